# Optimizing a Trainium2 kernel written in Bass

```python
import jax, jax.numpy as jnp
from jax import lax
import numpy as np

D_MODEL = 1024
BATCH = 8
SEQ = 2048
DEPTH = 2

GRID_W = 64
CTX_LEN = 256

A_HEADS = 8
A_KV_HEADS = 2
A_HEAD_DIM = 64
A_WINDOW = 128
A_BLOCK = 128
ROPE_BASE = 10000.0
B_HEADS = 4
B_DK = 128
B_DV = 128
C_HEADS = 4
C_DK = 96
C_DV = 192
C_GATE_RANK = 16
C_GATE_TAU = 16.0
D_GROUPS = 4
D_GROUP_DIM = 64
CHUNK = 64
N_EXPERTS = 16
N_GROUPS = 4
E_PER_GROUP = N_EXPERTS // N_GROUPS
TOP_K = 2
D_FF = 512

N_EVEN = (DEPTH + 1) // 2
N_ODD = DEPTH // 2
A_Q = A_HEADS * A_HEAD_DIM
A_KV = A_KV_HEADS * A_HEAD_DIM
B_KW = B_HEADS * B_DK
B_VW = B_HEADS * B_DV
C_KW = C_HEADS * C_DK
C_VW = C_HEADS * C_DV
D_W = D_GROUPS * D_GROUP_DIM
IN_EVEN = A_Q + 2 * A_KV + 3 * B_KW + 2 * B_VW
MIX_EVEN = A_Q + B_VW
IN_ODD = 2 * C_KW + 2 * C_VW + 2 * C_GATE_RANK + D_W
MIX_ODD = C_VW + D_W
DEEPNORM_ALPHA = (2 * DEPTH) ** 0.25
DEEPNORM_BETA = (8 * DEPTH) ** -0.25
LN_EPS = 1e-5
NEG = -1e30

kernel_name = "hybrid_dit_swa_hgrn2_gla_fnet_moe"


def layer_norm(x):
    xf = x.astype(jnp.float32)
    mu = jnp.mean(xf, -1, keepdims=True)
    var = jnp.mean(jnp.square(xf - mu), -1, keepdims=True)
    return ((xf - mu) * lax.rsqrt(var + LN_EPS)).astype(x.dtype)


def layer_norm_affine(x, g, b):
    xf = x.astype(jnp.float32)
    mu = jnp.mean(xf, -1, keepdims=True)
    var = jnp.mean(jnp.square(xf - mu), -1, keepdims=True)
    return ((xf - mu) * lax.rsqrt(var + LN_EPS) * g + b).astype(x.dtype)


def head_rms_norm(z, g, n_heads):
    B, T, W = z.shape
    zf = z.astype(jnp.float32).reshape(B, T, n_heads, W // n_heads)
    zf = zf * lax.rsqrt(jnp.mean(jnp.square(zf), -1, keepdims=True) + LN_EPS)
    return (zf.reshape(B, T, W) * g).astype(z.dtype)


def split_cols(p, sizes):
    offs = np.cumsum(sizes)[:-1]
    return jnp.split(p, [int(o) for o in offs], axis=-1)


def to_heads(z, n_heads):
    B, T, _ = z.shape
    return z.reshape(B, T, n_heads, -1).transpose(0, 2, 1, 3)


def from_heads(z):
    B, H, T, d = z.shape
    return z.transpose(0, 2, 1, 3).reshape(B, T, H * d)


def rope_half(x, pos):
    half = x.shape[-1] // 2
    inv = ROPE_BASE ** (-jnp.arange(half, dtype=jnp.float32) / half)
    ang = pos.astype(jnp.float32)[:, None] * inv[None, :]
    cos, sin = jnp.cos(ang), jnp.sin(ang)
    x1 = x[..., :half].astype(jnp.float32)
    x2 = x[..., half:].astype(jnp.float32)
    return jnp.concatenate([x1 * cos - x2 * sin, x1 * sin + x2 * cos], -1)


def axial_rope(x, rows, cols):
    r = x.shape[-1] // 2
    return jnp.concatenate([rope_half(x[..., :r], rows), rope_half(x[..., r:], cols)], -1).astype(x.dtype)


def window_attention(q, k, v, kc, vc, sink):
    B, Hkv, G, T, hd = q.shape
    nb = T // A_BLOCK
    scale = hd ** -0.5
    qb = q.reshape(B, Hkv, G, nb, A_BLOCK, hd)
    pad = ((0, 0), (0, 0), (A_BLOCK, A_BLOCK), (0, 0))
    kp = jnp.pad(k, pad).reshape(B, Hkv, nb + 2, A_BLOCK, hd)
    vp = jnp.pad(v, pad).reshape(B, Hkv, nb + 2, A_BLOCK, hd)
    kband = jnp.concatenate([kp[:, :, 0:nb], kp[:, :, 1:nb + 1], kp[:, :, 2:nb + 2]], axis=3)
    vband = jnp.concatenate([vp[:, :, 0:nb], vp[:, :, 1:nb + 1], vp[:, :, 2:nb + 2]], axis=3)
    qpos = jnp.arange(T).reshape(nb, A_BLOCK)
    kpos = jnp.arange(nb)[:, None] * A_BLOCK - A_BLOCK + jnp.arange(3 * A_BLOCK)[None, :]
    valid = ((kpos[:, None, :] >= 0) & (kpos[:, None, :] < T)
             & (jnp.abs(qpos[:, :, None] - kpos[:, None, :]) <= A_WINDOW))
    s_band = jnp.einsum('bhgnqd,bhnkd->bhgnqk', qb, kband).astype(jnp.float32) * scale
    s_band = jnp.where(valid, s_band, NEG)
    s_ctx = jnp.einsum('bhgnqd,bhcd->bhgnqc', qb, kc).astype(jnp.float32) * scale
    s_sink = jnp.broadcast_to(sink.astype(jnp.float32)[None, :, :, None, None, None], (B, Hkv, G, nb, A_BLOCK, 1))
    p = jax.nn.softmax(jnp.concatenate([s_sink, s_ctx, s_band], -1), -1)
    C = kc.shape[2]
    p_ctx = p[..., 1:1 + C].astype(v.dtype)
    p_band = p[..., 1 + C:].astype(v.dtype)
    o = (jnp.einsum('bhgnqc,bhcd->bhgnqd', p_ctx, vc)
         + jnp.einsum('bhgnqk,bhnkd->bhgnqd', p_band, vband))
    return o.reshape(B, Hkv, G, T, hd)


def context_attention(qc, kc, vc, sink):
    B, Hkv, G, C, hd = qc.shape
    s = jnp.einsum('bhgqd,bhkd->bhgqk', qc, kc).astype(jnp.float32) * hd ** -0.5
    s0 = jnp.broadcast_to(sink.astype(jnp.float32)[None, :, :, None, None], (B, Hkv, G, C, 1))
    p = jax.nn.softmax(jnp.concatenate([s0, s], -1), -1)[..., 1:]
    return jnp.einsum('bhgqk,bhkd->bhgqd', p.astype(vc.dtype), vc)


def chunk_gated_scan(q, k, v, log_f, s0):
    B, H, T, dk = q.shape
    dv = v.shape[-1]
    L = CHUNK
    N = T // L
    f32 = jnp.float32
    qf = q.astype(f32).reshape(B, H, N, L, dk)
    kf = k.astype(f32).reshape(B, H, N, L, dk)
    vf = v.astype(f32).reshape(B, H, N, L, dv)
    b = jnp.cumsum(log_f.astype(f32).reshape(B, H, N, L, dk), axis=3)
    g = b[:, :, :, L - 1:L, :]
    ref = b[:, :, :, L // 2:L // 2 + 1, :]
    att = jnp.einsum('bhnld,bhnmd->bhnlm', qf * jnp.exp(b - ref), kf * jnp.exp(ref - b))
    att = jnp.where(jnp.tril(jnp.ones((L, L), dtype=bool)), att, 0.0)
    o = jnp.einsum('bhnlm,bhnme->bhnle', att, vf)
    ds = jnp.einsum('bhnld,bhnle->nbhde', kf * jnp.exp(g - b), vf)
    decay = jnp.exp(g[:, :, :, 0, :]).transpose(2, 0, 1, 3)

    def step(s, inp):
        d, dsn = inp
        return s * d[..., None] + dsn, s

    s_last, s_start = lax.scan(step, s0, (decay, ds))
    o = o + jnp.einsum('bhnld,nbhde->bhnle', qf * jnp.exp(b), s_start)
    return o.reshape(B, H, T, dv).astype(v.dtype), s_last


def bidir_scan(q_c, v_c, kc_dirs, q_l, v_l, kl_dirs):
    B, H, _, dk = q_c.shape
    dv = v_c.shape[-1]
    s0 = jnp.zeros((B, H, dk, dv), jnp.float32)
    (kcf, lcf), (kcb, lcb) = kc_dirs
    (klf, llf), (klb, llb) = kl_dirs
    fl = lambda a: jnp.flip(a, axis=2)
    oc_f, sc_f = chunk_gated_scan(q_c, kcf, v_c, lcf, s0)
    oc_b, sc_b = chunk_gated_scan(fl(q_c), fl(kcb), fl(v_c), fl(lcb), s0)
    ol_f, _ = chunk_gated_scan(q_l, klf, v_l, llf, sc_f)
    ol_b, _ = chunk_gated_scan(fl(q_l), fl(klb), fl(v_l), fl(llb), sc_b)
    return oc_f + fl(oc_b), ol_f + fl(ol_b)


def fourier_mix(z):
    B, T, W = z.shape
    zf = z.astype(jnp.float32).reshape(B, T, D_GROUPS, D_GROUP_DIM)
    y = jnp.fft.fft2(zf, axes=(1, 3), norm='ortho').real
    return y.reshape(B, T, W).astype(z.dtype)


def mixer_even(u, uc, w_in, sink, lb_logits, idx, g_norm, w_out, rows, cols, need_ctx):
    sizes = (A_Q, A_KV, A_KV, B_KW, B_KW, B_KW, B_VW, B_VW)
    aq, ak, av, ff, fb, hq, hi, hg = split_cols(u @ w_in, sizes)
    caq, cak, cav, cff, cfb, chq, chi, chg = split_cols(uc @ w_in, sizes)
    G = A_HEADS // A_KV_HEADS
    sink = sink.reshape(A_KV_HEADS, G)
    q = axial_rope(to_heads(aq, A_HEADS), rows, cols)
    B, _, T, hd = q.shape
    q = q.reshape(B, A_KV_HEADS, G, T, hd)
    k = axial_rope(to_heads(ak, A_KV_HEADS), rows, cols)
    v = to_heads(av, A_KV_HEADS)
    kc = to_heads(cak, A_KV_HEADS)
    vc = to_heads(cav, A_KV_HEADS)
    o_att = from_heads(window_attention(q, k, v, kc, vc, sink).reshape(B, A_HEADS, T, hd))
    lb = jnp.cumsum(jax.nn.softmax(lb_logits.astype(jnp.float32), axis=1), axis=1)[:, idx]

    def gates(z, lb_d):
        f = lb_d + (1.0 - lb_d) * jax.nn.sigmoid(z.astype(jnp.float32))
        return to_heads((1.0 - f).astype(z.dtype), B_HEADS), to_heads(jnp.log(f), B_HEADS)

    o_c, o_l = bidir_scan(to_heads(chq, B_HEADS), to_heads(chi, B_HEADS), (gates(cff, lb[0]), gates(cfb, lb[1])),
                          to_heads(hq, B_HEADS), to_heads(hi, B_HEADS), (gates(ff, lb[0]), gates(fb, lb[1])))
    o_rec = head_rms_norm(from_heads(o_l), g_norm, B_HEADS) * jax.nn.silu(hg)
    y = jnp.concatenate([o_att, o_rec], -1) @ w_out
    if not need_ctx:
        return y, None
    qc = to_heads(caq, A_HEADS)
    C = qc.shape[2]
    qc = qc.reshape(B, A_KV_HEADS, G, C, hd)
    o_att_c = from_heads(context_attention(qc, kc, vc, sink).reshape(B, A_HEADS, C, hd))
    o_rec_c = head_rms_norm(from_heads(o_c), g_norm, B_HEADS) * jax.nn.silu(chg)
    yc = jnp.concatenate([o_att_c, o_rec_c], -1) @ w_out
    return y, yc


def mixer_odd(u, uc, w_in, gate_w, gate_b, g_norm, w_out, need_ctx):
    sizes = (C_KW, C_KW, C_VW, C_GATE_RANK, C_GATE_RANK, C_VW, D_W)

    def gla_inputs(p):
        q, k, v, rf, rb, g, z = split_cols(p, sizes)

        def decay(r, d):
            zz = (r @ gate_w[d] + gate_b[d]).astype(jnp.float32)
            return to_heads(jax.nn.log_sigmoid(zz) / C_GATE_TAU, C_HEADS)

        kh = to_heads(k, C_HEADS)
        return (to_heads(q * (C_DK ** -0.5), C_HEADS), to_heads(v, C_HEADS),
                ((kh, decay(rf, 0)), (kh, decay(rb, 1))), g, z)

    q, v, kd, g, z = gla_inputs(u @ w_in)
    qc, vc, kdc, gc, zc = gla_inputs(uc @ w_in)
    o_c, o_l = bidir_scan(qc, vc, kdc, q, v, kd)
    o_gla = head_rms_norm(from_heads(o_l), g_norm, C_HEADS) * jax.nn.silu(g)
    y = jnp.concatenate([o_gla, fourier_mix(z)], -1) @ w_out
    if not need_ctx:
        return y, None
    o_gla_c = head_rms_norm(from_heads(o_c), g_norm, C_HEADS) * jax.nn.silu(gc)
    yc = jnp.concatenate([o_gla_c, fourier_mix(zc)], -1) @ w_out
    return y, yc


def moe(t, w_router, b_router, w1, w3, w2):
    N = t.shape[0]
    probs = jax.nn.softmax((t @ w_router).astype(jnp.float32), -1)
    sel = probs + b_router.astype(jnp.float32)
    grp_score = jnp.sum(lax.top_k(sel.reshape(N, N_GROUPS, E_PER_GROUP), TOP_K)[0], -1)
    best = jnp.argmax(grp_score, -1)
    in_grp = (jnp.arange(N_EXPERTS) // E_PER_GROUP)[None, :] == best[:, None]
    _, idx = lax.top_k(jnp.where(in_grp, sel, NEG), TOP_K)
    w = jnp.take_along_axis(probs, idx, -1)
    w = w / jnp.sum(w, -1, keepdims=True)
    gates = jnp.sum(jax.nn.one_hot(idx, N_EXPERTS, dtype=jnp.float32) * w[..., None], axis=1)
    y = jnp.zeros(t.shape, jnp.float32)
    for e in range(N_EXPERTS):
        h = jax.nn.silu(t @ w1[e]) * (t @ w3[e])
        y = y + gates[:, e:e + 1] * (h @ w2[e])
    return y.astype(t.dtype)


def modulate(x, shift, scale):
    return x * (1.0 + scale) + shift


def setup_inputs(seed: int = 0) -> dict:
    key = jax.random.key(seed)
    ks = jax.random.split(key, 24)
    f32 = jnp.float32
    D = D_MODEL

    def nrm(k, shape, scale):
        return jax.random.normal(k, shape, f32) * scale

    return {
        'x': nrm(ks[0], (BATCH, SEQ, D), 1.0),
        'c': nrm(ks[1], (BATCH, D), 1.0),
        'ctx': nrm(ks[2], (BATCH, CTX_LEN, D), 1.0),
        'c_ctx': nrm(ks[3], (D,), 1.0),
        'w_ada': nrm(ks[4], (DEPTH, D, 6 * D), 0.5 * D ** -0.5),
        'b_ada': nrm(ks[5], (DEPTH, 6 * D), 0.02),
        'ln_g': 1.0 + nrm(ks[6], (DEPTH, 2, D), 0.02),
        'ln_b': nrm(ks[7], (DEPTH, 2, D), 0.02),
        'w_in_even': nrm(ks[8], (N_EVEN, D, IN_EVEN), D ** -0.5),
        'attn_sink': nrm(ks[9], (N_EVEN, A_HEADS), 0.5),
        'hgrn_lb_logits': nrm(ks[10], (2, N_EVEN + 1, B_KW), 0.5),
        'hgrn_norm': 1.0 + nrm(ks[11], (N_EVEN, B_VW), 0.02),
        'w_out_even': nrm(ks[12], (N_EVEN, MIX_EVEN, D), DEEPNORM_BETA * MIX_EVEN ** -0.5),
        'w_in_odd': nrm(ks[13], (N_ODD, D, IN_ODD), D ** -0.5),
        'gla_gate_w': nrm(ks[14], (N_ODD, 2, C_GATE_RANK, C_KW), C_GATE_RANK ** -0.5),
        'gla_gate_b': nrm(ks[15], (N_ODD, 2, C_KW), 0.5),
        'gla_norm': 1.0 + nrm(ks[16], (N_ODD, C_VW), 0.02),
        'w_out_odd': nrm(ks[17], (N_ODD, MIX_ODD, D), DEEPNORM_BETA * MIX_ODD ** -0.5),
        'w_router': nrm(ks[18], (D, N_EXPERTS), D ** -0.5),
        'b_router': nrm(ks[19], (N_EXPERTS,), 0.01),
        'w_expert_gate': nrm(ks[20], (DEPTH, N_EXPERTS, D, D_FF), D ** -0.5),
        'w_expert_up': nrm(ks[21], (DEPTH, N_EXPERTS, D, D_FF), D ** -0.5),
        'w_expert_down': nrm(ks[22], (DEPTH, N_EXPERTS, D_FF, D), DEEPNORM_BETA * D_FF ** -0.5),
    }


def reference(x, c, ctx, c_ctx, w_ada, b_ada, ln_g, ln_b, w_in_even, attn_sink, hgrn_lb_logits, hgrn_norm,
              w_out_even, w_in_odd, gla_gate_w, gla_gate_b, gla_norm, w_out_odd, w_router, b_router,
              w_expert_gate, w_expert_up, w_expert_down):
    B, T, D = x.shape
    C = ctx.shape[1]
    ROWS = T // GRID_W
    rows = jnp.repeat(jnp.arange(ROWS), GRID_W)
    cols = jnp.tile(jnp.arange(GRID_W), ROWS)
    h = x
    hc = ctx
    for l in range(DEPTH):
        need_ctx = l < DEPTH - 1
        mod = (jax.nn.silu(c) @ w_ada[l] + b_ada[l])[:, None, :]
        mod_c = (jax.nn.silu(c_ctx) @ w_ada[l] + b_ada[l])[None, None, :]
        sh1, sc1, g1, sh2, sc2, g2 = jnp.split(mod, 6, axis=-1)
        csh1, csc1, cg1, csh2, csc2, cg2 = jnp.split(mod_c, 6, axis=-1)
        u = modulate(layer_norm(h), sh1, sc1)
        uc = modulate(layer_norm(hc), csh1, csc1)
        if l % 2 == 0:
            i = l // 2
            m, mc = mixer_even(u, uc, w_in_even[i], attn_sink[i], hgrn_lb_logits, i, hgrn_norm[i],
                               w_out_even[i], rows, cols, need_ctx)
        else:
            i = l // 2
            m, mc = mixer_odd(u, uc, w_in_odd[i], gla_gate_w[i], gla_gate_b[i], gla_norm[i], w_out_odd[i], need_ctx)
        h = layer_norm_affine(DEEPNORM_ALPHA * h + g1 * m, ln_g[l, 0], ln_b[l, 0])
        u = modulate(layer_norm(h), sh2, sc2)
        if need_ctx:
            hc = layer_norm_affine(DEEPNORM_ALPHA * hc + cg1 * mc, ln_g[l, 0], ln_b[l, 0])
            uc = modulate(layer_norm(hc), csh2, csc2)
            tok = jnp.concatenate([uc, u], axis=1).reshape(-1, D)
            y = moe(tok, w_router, b_router, w_expert_gate[l], w_expert_up[l], w_expert_down[l]).reshape(B, C + T, D)
            yc = y[:, :C]
            y = y[:, C:]
            hc = layer_norm_affine(DEEPNORM_ALPHA * hc + cg2 * yc, ln_g[l, 1], ln_b[l, 1])
        else:
            y = moe(u.reshape(-1, D), w_router, b_router, w_expert_gate[l], w_expert_up[l],
                    w_expert_down[l]).reshape(B, T, D)
        h = layer_norm_affine(DEEPNORM_ALPHA * h + g2 * y, ln_g[l, 1], ln_b[l, 1])
    return h
```

```python
import contextlib
import math
import numpy as np
import concourse.bass as bass
import concourse.mybir as mybir
from concourse.bass_utils import run_bass_kernel_spmd

F32 = mybir.dt.float32
BF16 = mybir.dt.bfloat16
I32 = mybir.dt.int32
AF = mybir.ActivationFunctionType
ALU = mybir.AluOpType
AX = mybir.AxisListType

ENG = ("pe", "act", "dve", "pool", "sp")


class Trk:
    __slots__ = ("w", "rs", "dsem", "dcnt", "name")

    def __init__(self, name=""):
        self.w = None
        self.rs = []
        self.dsem = None
        self.dcnt = 0
        self.name = name


class Sched:
    SEM_CHUNK = 20000

    def __init__(self, nc):
        self.nc = nc
        self.ops = {e: [] for e in ENG}
        self.waited = {e: {} for e in ENG}
        self.stack = contextlib.ExitStack()
        self.nsem = 0
        self.dma_ev = {}

    def sbuf(self, name, shape, dt):
        return self.stack.enter_context(self.nc.sbuf_tensor(name, list(shape), dt))

    def psum(self, name, shape, dt=F32):
        return self.stack.enter_context(self.nc.psum_tensor(name, list(shape), dt))

    def new_sem(self, name):
        self.nsem += 1
        return self.stack.enter_context(self.nc.semaphore(f"{name}_{self.nsem}"))

    def _filter(self, engine, deps):
        waits = []
        wd = self.waited[engine]
        for ev in deps:
            if ev[0] == "e":
                _, f, idx = ev
                if engine == "pe" and f == "pe":
                    continue
                if idx <= wd.get(f, -1):
                    continue
                wd[f] = idx
                self.ops[f][idx][2] = True
                waits.append(ev)
            else:
                _, sem, val = ev
                k = id(sem)
                if val <= wd.get(k, 0):
                    continue
                wd[k] = val
                waits.append(ev)
        return waits

    def _deps(self, engine, r, w):
        deps = []
        for t in r:
            if t.w is not None:
                deps.append(t.w)
        for t in w:
            if t.w is not None:
                deps.append(t.w)
            deps.extend(t.rs)
        return self._filter(engine, deps)

    def _post(self, ev, r, w):
        for t in w:
            t.w = ev
            t.rs = []
        for t in r:
            if t in w:
                continue
            if ev[0] == "e":
                t.rs = [x for x in t.rs if not (x[0] == "e" and x[1] == ev[1])]
            else:
                t.rs = [x for x in t.rs if not (x[0] == "d" and x[1] is ev[1])]
            t.rs.append(ev)

    def op(self, engine, fn, r=(), w=()):
        r = list(r)
        w = list(w)
        waits = self._deps(engine, r, w)
        idx = len(self.ops[engine])
        self.ops[engine].append([fn, waits, False, None])
        self._post(("e", engine, idx), r, w)

    def dma(self, out, in_, r=(), w=(), q="sp", **kw):
        r = list(r)
        w = list(w)
        waits = self._deps(q, r, w)
        t0 = w[0]
        if t0.dsem is None or t0.dcnt > 60000:
            t0.dsem = self.new_sem("d")
            t0.dcnt = 0
        t0.dcnt += 16
        ev = ("d", t0.dsem, t0.dcnt)
        self.dma_ev[id(t0.dsem)] = ev

        def fn(eng, out=out, in_=in_, kw=kw):
            return eng.dma_start(out=out, in_=in_, **kw)
        self.ops[q].append([fn, waits, False, t0.dsem])
        self._post(ev, r, w)

    def barrier(self):
        last = {}
        for f in ("pe", "act", "dve", "pool"):
            j = len(self.ops[f]) - 1
            while j >= 0 and (self.ops[f][j][0] is None or self.ops[f][j][3] is not None):
                j -= 1
            last[f] = j
        dm = list(self.dma_ev.values())
        for e in ENG:
            deps = [("e", f, last[f]) for f in ("pe", "act", "dve", "pool") if f != e and last[f] >= 0]
            deps += dm
            waits = self._filter(e, deps)
            self.ops[e].append([None, waits, False, None])

    def wait_all(self, engine, trks):
        waits = self._deps(engine, list(trks), [])
        self.ops[engine].append([None, waits, False, None])

    def emit(self):
        nc = self.nc
        cum = {}
        sems = {}
        for e in ENG:
            c = 0
            arr = []
            for rec in self.ops[e]:
                if rec[2]:
                    c += 1
                arr.append(c)
            cum[e] = arr
            sems[e] = [self.new_sem(f"s{e}") for _ in range(c // self.SEM_CHUNK + 1)]
        CH = self.SEM_CHUNK

        def semval(f, idx):
            c = cum[f][idx]
            ch = (c - 1) // CH
            return sems[f][ch], c - ch * CH

        def run(e, eng):
            for i, (fn, waits, sig, dsem) in enumerate(self.ops[e]):
                for ev in waits:
                    if ev[0] == "e":
                        s, v = semval(ev[1], ev[2])
                        eng.wait_ge(s, v)
                    else:
                        eng.wait_ge(ev[1], ev[2])
                if fn is None:
                    continue
                ins = fn(eng)
                if dsem is not None:
                    ins.then_inc(dsem, 16)
                elif sig:
                    s, v = semval(e, i)
                    ins.then_inc(s, 1)

        with nc.Block() as block:
            @block.tensor
            def _(eng):
                run("pe", eng)

            @block.scalar
            def _(eng):
                run("act", eng)

            @block.vector
            def _(eng):
                run("dve", eng)

            @block.gpsimd
            def _(eng):
                run("pool", eng)

            @block.sync
            def _(eng):
                run("sp", eng)

    def close(self):
        self.stack.close()


N = 2304
NT = 18
D = 1024
KC = 8
NLAT = 2048
ALPHA = 4.0 ** 0.25
EPS = 1e-5
TOKB = [(0, 512), (512, 512), (1024, 512), (1536, 512), (2048, 256)]
PI = math.pi

_SW = list(range(16, 32)) + list(range(0, 16)) + list(range(48, 64)) + list(range(32, 48))


def _cols0():
    cols = []
    for j in range(2):
        for blk in range(2):
            for hh in (4 * j + 2 * blk, 4 * j + 2 * blk + 1):
                cols += [hh * 64 + d for d in range(64)]
        for blk in range(2):
            for hh in (4 * j + 2 * blk, 4 * j + 2 * blk + 1):
                cols += [hh * 64 + d for d in _SW]
        cols += [512 + j * 64 + d for d in range(64)] * 2
        cols += [512 + j * 64 + d for d in _SW] * 2
    cols += list(range(640, 768))
    for h in range(4):
        cols += [768 + h * 128 + d for d in range(128)]
        cols += [1280 + h * 128 + d for d in range(128)]
        cols += [1792 + h * 128 + d for d in range(128)]
        cols += [2816 + h * 128 + d for d in range(128)]
    cols += list(range(2304, 2816))
    return cols


def _cols1():
    cols = []
    for h in range(4):
        cols += [h * 96 + d for d in range(96)]
        cols += [384 + h * 96 + d for d in range(96)]
        cols += [1568 + h * 192 + d for d in range(192)]
    cols += list(range(2336, 2592))
    cols += list(range(1536, 1568))
    cols += list(range(768, 1536))
    return cols


class K:
    pass


def build(dbg=(), stop=None):
    nc = bass.Bass("TRN2", target_bir_lowering=False)
    S = Sched(nc)
    k = K()
    k.nc = nc
    k.S = S
    k.dbg = set(dbg)
    k.sub = stop
    k.dbg_out = []

    def din(name, shape, dt=F32):
        return nc.dram_tensor(name, list(shape), dt, kind="ExternalInput").ap()

    x_d = din("x", [NLAT, D])
    ctx_d = din("ctx", [256, D])
    cv_d = din("cvec", [2, D])
    wada_d = din("w_ada", [2, D, 6 * D])
    bada_d = din("b_ada", [2, 6 * D])
    lng_d = din("ln_g", [2, 2, D])
    lnb_d = din("ln_b", [2, 2, D])
    w0_d = din("w0a", [D, 4224])
    sink_d = din("attn_sink", [1, 8])
    lbl_d = din("lb_logits", [2, 2, 512])
    hn_d = din("hgrn_norm", [1, 512])
    wo0_d = din("w_out_even", [D, D])
    w1_d = din("w1a", [D, 2592])
    gw_d = din("gla_gate_w", [2, 16, 384])
    gb_d = din("gla_gate_b", [2, 384])
    gn_d = din("gla_norm", [1, 768])
    wo1_d = din("w_out_odd", [D, D])
    wr_d = din("w_router", [D, 16])
    br_d = din("b_router", [1, 16])
    eg_d = din("w_expert_gate", [2, 16, D, 512])
    eu_d = din("w_expert_up", [2, 16, D, 512])
    ed_d = din("w_expert_down", [2, 16, 512, D])
    out_d = nc.dram_tensor("out", [NLAT, D], F32, kind="ExternalOutput").ap()
    hmid_d = nc.dram_tensor("hmid", [N, D], F32).ap()
    hout0_d = nc.dram_tensor("hout0", [N, D], F32).ap()
    mix_d = nc.dram_tensor("mixd", [10, 128, N], BF16).ap()
    t_hmid = [Trk() for _ in range(NT)]
    t_hout0 = [Trk() for _ in range(NT)]
    t_mixd = [Trk() for _ in range(10)]
    t_out = [Trk() for _ in range(16)]

    def dump(name, src_ap, shape, dt, r):
        if name not in k.dbg:
            return
        d = nc.dram_tensor("dbg_" + name, list(shape), dt, kind="ExternalOutput").ap()
        t = Trk()
        S.dma(d, src_ap, r=r, w=[t])
        k.dbg_out.append(t)

    PS = [S.psum(f"ps{i}", [128, 1024]) for i in range(4)]
    PT = [[Trk(), Trk()] for _ in range(4)]
    k.bank_i = 0
    k.pair_i = 0

    k.reserved = set()

    def bank():
        i = k.bank_i
        while i in k.reserved:
            i = (i + 1) % 8
        k.bank_i = (i + 1) % 8
        k.last_bank = i
        return PS[i // 2][:, (i % 2) * 512:(i % 2 + 1) * 512], PT[i // 2][i % 2]

    def pair():
        i = k.pair_i
        k.pair_i = (i + 1) % 4
        k.bank_i = (2 * i + 2) % 8
        return PS[i], PT[i]

    identF = S.sbuf("identF", [128, 128], F32); t_c = Trk()
    identB = S.sbuf("identB", [128, 128], BF16)
    onesF = S.sbuf("onesF", [128, 128], F32)
    onesB = S.sbuf("onesB", [128, 128], BF16)
    cst = S.sbuf("cst", [128, 8], F32)
    Mf = S.sbuf("Mf", [128, 128], BF16)
    Mb = S.sbuf("Mb", [128, 128], BF16)
    MP = S.sbuf("MP", [128, 512], BF16)
    MN = S.sbuf("MN", [128, 512], BF16)
    rmask = S.sbuf("rmask", [128, N], BF16)
    modT = S.sbuf("modT", [128, 2, 48, 2], F32); t_modT = Trk()
    mod_d = nc.dram_tensor("mod_d", [2, 2, 6 * D], F32).ap(); t_mod = Trk()
    bc = S.sbuf("bc", [128, 4, D], F32); t_bc = [Trk() for _ in range(4)]
    uT = S.sbuf("uT", [128, KC, N], BF16); t_uT = [Trk() for _ in range(NT)]
    wbuf = S.sbuf("wbuf", [128, 5, 4096], BF16); t_wb = [Trk() for _ in range(5)]
    RB = S.sbuf("RB", [128, 6, N], F32); t_rb = [Trk() for _ in range(6)]
    HB = S.sbuf("HB", [128, 8, N], BF16); t_hb = [Trk() for _ in range(8)]
    sm = S.sbuf("sm", [128, 1024], F32)
    gates = S.sbuf("gates", [128, NT, 16], F32); t_gates = [Trk() for _ in range(NT)]
    wrt = S.sbuf("wrt", [128, KC, 16], F32); t_wr = Trk()
    brb = S.sbuf("brb", [128, 16], F32)

    S.op("pool", lambda e: e.memset(identF[:], 0.0), w=[t_c])
    S.op("pool", lambda e: e.affine_select(out=identF[:], in_=identF[:], pattern=[[-1, 128]], compare_op=ALU.not_equal,
                                           fill=1.0, base=0, channel_multiplier=1), r=[t_c], w=[t_c])
    S.op("pool", lambda e: e.tensor_copy(out=identB[:], in_=identF[:]), r=[t_c], w=[t_c])
    S.op("pool", lambda e: e.memset(onesF[:], 1.0), w=[t_c])
    S.op("pool", lambda e: e.memset(onesB[:], 1.0), w=[t_c])
    for j_, v_ in enumerate((EPS, 1.0, -PI, 0.0, PI)):
        S.op("pool", lambda e, j_=j_, v_=v_: e.memset(cst[:, j_:j_ + 1], v_), w=[t_c])
    S.op("pool", lambda e: e.memset(Mf[:], 1.0), w=[t_c])
    S.op("pool", lambda e: e.affine_select(out=Mf[:], in_=Mf[:], pattern=[[1, 128]], compare_op=ALU.is_ge, fill=0.0,
                                           base=0, channel_multiplier=-1), r=[t_c], w=[t_c])
    S.op("pool", lambda e: e.memset(Mf[0:64, 64:128], 0.0), r=[t_c], w=[t_c])
    S.op("pool", lambda e: e.memset(Mb[:], 1.0), w=[t_c])
    S.op("pool", lambda e: e.affine_select(out=Mb[:], in_=Mb[:], pattern=[[-1, 128]], compare_op=ALU.is_ge, fill=0.0,
                                           base=0, channel_multiplier=1), r=[t_c], w=[t_c])
    S.op("pool", lambda e: e.memset(Mb[64:128, 0:64], 0.0), r=[t_c], w=[t_c])
    S.op("pool", lambda e: e.memset(MP[:], 1.0), w=[t_c])
    S.op("pool", lambda e: e.affine_select(out=MP[:], in_=MP[:], pattern=[[0, 4], [-1, 128]], compare_op=ALU.is_ge,
                                           fill=0.0, base=0, channel_multiplier=1), r=[t_c], w=[t_c])
    S.op("pool", lambda e: e.memset(MN[:], 1.0), w=[t_c])
    S.op("pool", lambda e: e.affine_select(out=MN[:], in_=MN[:], pattern=[[0, 4], [1, 128]], compare_op=ALU.is_ge,
                                           fill=0.0, base=0, channel_multiplier=-1), r=[t_c], w=[t_c])
    S.op("pool", lambda e: e.memset(rmask[:], 1.0), w=[t_c])
    S.op("pool", lambda e: e.memset(rmask[:].rearrange("p (c l) -> p c l", l=64)[:, :, 0:1], 0.0), r=[t_c], w=[t_c])
    S.dma(wrt[:], wr_d.rearrange("(k p) n -> p k n", p=128), w=[t_wr])
    S.dma(brb[:], br_d[0:1, :].to_broadcast([128, 16]), w=[t_c])
    S.barrier()

    cs = RB[0:2, 4, 0:1024]
    sg_ = RB[0:2, 4, 1024:2048]
    csT = sm[:, 520:536].rearrange("p (k r) -> p k r", r=2)
    t_cs = Trk(); t_csT = Trk()
    k.t_bb = [[Trk(), Trk()], [Trk(), Trk()]]
    S.dma(cs, cv_d[:, :], w=[t_cs])
    S.op("act", lambda e: e.activation(out=sg_, in_=cs, func=AF.Sigmoid), r=[t_cs], w=[t_csT])
    S.op("dve", lambda e: e.tensor_tensor(out=cs, in0=cs, in1=sg_, op=ALU.mult), r=[t_cs, t_csT], w=[t_cs])
    pb, tb = bank()
    for kk in range(KC):
        S.op("pe", lambda e, kk=kk, pb=pb: e.transpose(out=pb[:, 2 * kk:2 * kk + 2], in_=cs[:, kk * 128:(kk + 1) * 128],
                                                       identity=identF[0:2, 0:2]), r=[t_cs, t_c], w=[tb])
    S.op("dve", lambda e, pb=pb: e.tensor_copy(out=csT, in_=pb[:, 0:16].rearrange("p (k r) -> p k r", r=2)), r=[tb], w=[t_csT])
    t_wa = [Trk(), Trk()]
    t_mb = [Trk(), Trk()]
    for l in range(2):
        pT, tT = bank()
        k.reserved = {k.last_bank}
        for j in range(12):
            slot = (l * 12 + j) % 2
            wa = RB[:, slot * 2:slot * 2 + 2, :].rearrange("p a n -> p (a n)")[:, 0:4096].rearrange("p (k n) -> p k n", n=512)
            mb = RB[0:2, 5, slot * 512:(slot + 1) * 512]
            S.dma(wa, wada_d[l, :, j * 512:(j + 1) * 512].rearrange("(k p) n -> p k n", p=128), w=[t_wa[slot]])
            for r_ in range(2):
                S.dma(RB[r_:r_ + 1, 5, 1024 + slot * 512:1024 + (slot + 1) * 512], bada_d[l:l + 1, j * 512:(j + 1) * 512],
                      w=[k.t_bb[slot][r_]])
            pb, tb = bank()
            for kk in range(KC):
                S.op("pe", lambda e, kk=kk, pb=pb, wa=wa: e.matmul(pb[0:2, :], lhsT=csT[:, kk, :], rhs=wa[:, kk, :],
                                                                   start=(kk == 0), stop=(kk == KC - 1)),
                     r=[t_csT, t_wa[slot]], w=[tb])
            bb = RB[0:2, 5, 1024 + slot * 512:1024 + (slot + 1) * 512]
            S.op("dve", lambda e, pb=pb, mb=mb, bb=bb: e.tensor_tensor(out=mb, in0=pb[0:2, :], in1=bb, op=ALU.add),
                 r=[tb] + k.t_bb[slot], w=[t_mb[slot]])
            if j in (2, 3, 8, 9):
                S.op("dve", lambda e, mb=mb: e.tensor_scalar(out=mb, in0=mb, scalar1=1.0, scalar2=None, op0=ALU.add),
                     r=[t_mb[slot]], w=[t_mb[slot]])
            S.dma(mod_d[l, :, j * 512:(j + 1) * 512], mb, r=[t_mb[slot]], w=[t_mod])
            for q_ in range(4):
                jj = j * 4 + q_
                S.op("pe", lambda e, jj=jj, q_=q_, mb=mb, pT=pT: e.transpose(out=pT[:, 2 * jj:2 * jj + 2], in_=mb[:, q_ * 128:(q_ + 1) * 128],
                                                                             identity=identF[0:2, 0:2]), r=[t_mb[slot], t_c], w=[tT])
        S.op("dve", lambda e, l=l, pT=pT: e.tensor_copy(out=modT[:, l, :, :], in_=pT[:, 0:96].rearrange("p (j r) -> p j r", r=2)),
             r=[tT], w=[t_modT])
        k.reserved = set()
        dump(f"mod{l}", mod_d[l, :, :], [2, 6 * D], F32, [t_mod])
    S.barrier()

    def bcast_rows(l, which):
        gi = 2 if which == "mix" else 5
        li = 0 if which == "mix" else 1
        S.dma(bc[:, 0, :], mod_d[l, 0:1, gi * D:(gi + 1) * D].to_broadcast([128, D]), r=[t_mod], w=[t_bc[0]])
        S.dma(bc[:, 1, :], mod_d[l, 1:2, gi * D:(gi + 1) * D].to_broadcast([128, D]), r=[t_mod], w=[t_bc[1]])
        S.dma(bc[:, 2, :], lng_d[l, li:li + 1, :].to_broadcast([128, D]), w=[t_bc[2]])
        S.dma(bc[:, 3, :], lnb_d[l, li:li + 1, :].to_broadcast([128, D]), w=[t_bc[3]])

    def ln_stats(src, t_src, st_ap, mv_ap, rs_ap, nb_ap, t_st):
        for j in range(2):
            S.op("dve", lambda e, j=j: e.bn_stats(out=st_ap[:, j, :], in_=src[:, j * 512:(j + 1) * 512]), r=[t_src], w=[t_st])
        S.op("dve", lambda e: e.bn_aggr(out=mv_ap, in_=st_ap), r=[t_st], w=[t_st])
        S.op("act", lambda e: e.activation(out=rs_ap, in_=mv_ap[:, 1:2], func=AF.Sqrt, bias=cst[:, 0:1]), r=[t_st, t_c], w=[t_st])
        S.op("dve", lambda e: e.reciprocal(out=rs_ap, in_=rs_ap), r=[t_st], w=[t_st])
        S.op("dve", lambda e: e.tensor_scalar(out=nb_ap, in0=mv_ap[:, 0:1], scalar1=rs_ap, scalar2=-1.0, op0=ALU.mult, op1=ALU.mult),
             r=[t_st], w=[t_st])

    k.st_i = 0

    def st_slot():
        i = k.st_i
        k.st_i = (i + 1) % 8
        base = i * 24
        return (sm[:, base:base + 12].rearrange("p (a b) -> p a b", b=6), sm[:, base + 12:base + 14],
                sm[:, base + 14:base + 15], sm[:, base + 15:base + 16], k.t_st[i])
    k.t_st = [Trk() for _ in range(8)]

    def ln_to_uT(l, src, t_src, xn, t_xn, i, which, router, uf=None, t_uf=None):
        st_ap, mv_ap, rs_ap, nb_ap, t_st = st_slot()
        ln_stats(src, t_src, st_ap, mv_ap, rs_ap, nb_ap, t_st)
        S.op("act", lambda e: e.activation(out=xn, in_=src, func=AF.Identity, scale=rs_ap, bias=nb_ap), r=[t_src, t_st], w=[t_xn])
        r_ = 1 if i < 2 else 0
        pp, tp = pair()
        for kk in range(KC):
            S.op("pe", lambda e, kk=kk: e.transpose(out=pp[:, kk * 128:(kk + 1) * 128], in_=xn[:, kk * 128:(kk + 1) * 128],
                                                    identity=identF[:]), r=[t_xn, t_c], w=[tp[kk // 4]])
        for kk in range(KC):
            sc_ap = modT[:, l, (which + 1) * 8 + kk, r_:r_ + 1]
            sh_ap = modT[:, l, which * 8 + kk, r_:r_ + 1]
            if router:
                o_ap = uf[:, kk, :]
                tw = t_uf
            else:
                o_ap = uT[:, kk, i * 128:(i + 1) * 128]
                tw = t_uT[i]
            if kk % 2 == 0:
                S.op("act", lambda e, kk=kk, o_ap=o_ap, sc_ap=sc_ap, sh_ap=sh_ap: e.activation(
                    out=o_ap, in_=pp[:, kk * 128:(kk + 1) * 128], func=AF.Identity, scale=sc_ap, bias=sh_ap),
                    r=[tp[kk // 4], t_modT], w=[tw])
            else:
                S.op("dve", lambda e, kk=kk, o_ap=o_ap, sc_ap=sc_ap, sh_ap=sh_ap: e.tensor_scalar(
                    out=o_ap, in0=pp[:, kk * 128:(kk + 1) * 128], scalar1=sc_ap, scalar2=sh_ap, op0=ALU.mult, op1=ALU.add),
                    r=[tp[kk // 4], t_modT], w=[tw])
        if router:
            S.op("pool", lambda e: e.tensor_copy(out=uT[:, :, i * 128:(i + 1) * 128], in_=uf[:, :, :]), r=[t_uf], w=[t_uT[i]])
            route(i, uf, t_uf)

    def route(i, uf, t_uf):
        pb, tb = bank()
        for kk in range(KC):
            S.op("pe", lambda e, kk=kk: e.matmul(pb[:, 0:16], lhsT=uf[:, kk, :], rhs=wrt[:, kk, :], start=(kk == 0),
                                                 stop=(kk == KC - 1)), r=[t_uf, t_wr], w=[tb])
        base = 192 + (i % 2) * 160
        t_r = k.t_route[i % 2]
        lg = sm[:, base:base + 16]
        pr = sm[:, base + 16:base + 32]
        sl = sm[:, base + 32:base + 48]
        s2 = sm[:, base + 48:base + 64]
        eq = sm[:, base + 64:base + 80]
        g4 = sm[:, base + 80:base + 84]
        g4b = sm[:, base + 84:base + 88]
        g4c = sm[:, base + 88:base + 92]
        sc1 = sm[:, base + 92:base + 93]
        sc2 = sm[:, base + 93:base + 94]
        eq2 = sm[:, base + 96:base + 112]
        BIG = 1.0e4

        def dv(fn, extra_r=()):
            S.op("dve", fn, r=[t_r] + list(extra_r), w=[t_r])
        S.op("dve", lambda e: e.tensor_copy(out=lg, in_=pb[:, 0:16]), r=[tb], w=[t_r])
        dv(lambda e: e.tensor_reduce(out=sc1, in_=lg, axis=AX.X, op=ALU.max))
        dv(lambda e: e.tensor_scalar(out=sc1, in0=sc1, scalar1=-1.0, scalar2=None, op0=ALU.mult))
        S.op("act", lambda e: e.activation(out=pr, in_=lg, func=AF.Exp, bias=sc1, scale=1.0), r=[t_r], w=[t_r])
        dv(lambda e: e.tensor_reduce(out=sc2, in_=pr, axis=AX.X, op=ALU.add))
        dv(lambda e: e.reciprocal(out=sc2, in_=sc2))
        dv(lambda e: e.tensor_scalar(out=pr, in0=pr, scalar1=sc2, scalar2=None, op0=ALU.mult))
        dv(lambda e: e.tensor_tensor(out=sl, in0=pr, in1=brb[:], op=ALU.add), extra_r=[t_c])
        sl3 = sl.rearrange("p (g e) -> p g e", e=4)
        s23 = s2.rearrange("p (g e) -> p g e", e=4)
        eq3 = eq.rearrange("p (g e) -> p g e", e=4)
        dv(lambda e: e.tensor_reduce(out=g4, in_=sl3, axis=AX.X, op=ALU.max))
        dv(lambda e: e.tensor_tensor(out=eq3, in0=sl3, in1=g4.unsqueeze(2).to_broadcast([128, 4, 4]), op=ALU.is_equal))
        dv(lambda e: e.scalar_tensor_tensor(out=s2, in0=eq, scalar=-BIG, in1=sl, op0=ALU.mult, op1=ALU.add))
        dv(lambda e: e.tensor_reduce(out=g4b, in_=s23, axis=AX.X, op=ALU.max))
        dv(lambda e: e.tensor_tensor(out=g4, in0=g4, in1=g4b, op=ALU.add))
        dv(lambda e: e.tensor_reduce(out=sc1, in_=g4, axis=AX.X, op=ALU.max))
        dv(lambda e: e.tensor_scalar(out=g4c, in0=g4, scalar1=sc1, scalar2=None, op0=ALU.is_equal))
        dv(lambda e: e.tensor_scalar(out=g4c, in0=g4c, scalar1=-1.0, scalar2=BIG, op0=ALU.add, op1=ALU.mult))
        dv(lambda e: e.tensor_tensor(out=s23, in0=sl3, in1=g4c.unsqueeze(2).to_broadcast([128, 4, 4]), op=ALU.add))
        dv(lambda e: e.tensor_reduce(out=sc1, in_=s2, axis=AX.X, op=ALU.max))
        dv(lambda e: e.tensor_scalar(out=eq, in0=s2, scalar1=sc1, scalar2=None, op0=ALU.is_equal))
        dv(lambda e: e.scalar_tensor_tensor(out=s2, in0=eq, scalar=-BIG, in1=s2, op0=ALU.mult, op1=ALU.add))
        dv(lambda e: e.tensor_reduce(out=sc1, in_=s2, axis=AX.X, op=ALU.max))
        dv(lambda e: e.tensor_scalar(out=eq2, in0=s2, scalar1=sc1, scalar2=None, op0=ALU.is_equal))
        dv(lambda e: e.tensor_tensor(out=eq, in0=eq, in1=eq2, op=ALU.add))
        dv(lambda e: e.tensor_tensor(out=eq, in0=eq, in1=pr, op=ALU.mult))
        dv(lambda e: e.tensor_reduce(out=sc2, in_=eq, axis=AX.X, op=ALU.add))
        dv(lambda e: e.reciprocal(out=sc2, in_=sc2))
        S.op("dve", lambda e: e.tensor_scalar(out=gates[:, i, :], in0=eq, scalar1=sc2, scalar2=None, op0=ALU.mult),
             r=[t_r], w=[t_gates[i]])
    k.t_route = [Trk(), Trk()]
    k.t_tile = [Trk() for _ in range(12)]

    k.wslot = 0

    def load_w(src_ap, nslots=1, parts=128):
        s0 = k.wslot
        if s0 + nslots > 5:
            s0 = 0
        k.wslot = (s0 + nslots) % 5
        a, b = src_ap.shape[1], src_ap.shape[2]
        dst = wbuf[0:parts, s0:s0 + nslots, :].rearrange("p s n -> p (s n)")[:, 0:a * b].rearrange("p (a b) -> p a b", b=b)
        trks = t_wb[s0:s0 + nslots]
        S.dma(dst, src_ap, w=trks, q="pool")
        return dst, trks

    def proj_fm(wv, t_w, c0, M, evac, toks=TOKB):
        for (t0, nt) in toks:
            pb, tb = bank()
            for kk in range(KC):
                S.op("pe", lambda e, kk=kk, pb=pb, t0=t0, nt=nt: e.matmul(pb[0:M, 0:nt], lhsT=wv[:, kk, c0:c0 + M],
                                                                          rhs=uT[:, kk, t0:t0 + nt], start=(kk == 0), stop=(kk == KC - 1)),
                     r=t_w + t_uT[t0 // 128:(t0 + nt) // 128], w=[tb])
            evac(pb, tb, t0, nt)

    def proj_tm(wv, t_w, c0, ncol, evac, tiles=range(NT)):
        for i in tiles:
            pb, tb = bank()
            for kk in range(KC):
                S.op("pe", lambda e, kk=kk, pb=pb, i=i: e.matmul(pb[:, 0:ncol], lhsT=uT[:, kk, i * 128:(i + 1) * 128],
                                                                 rhs=wv[:, kk, c0:c0 + ncol], start=(kk == 0), stop=(kk == KC - 1)),
                     r=t_w + [t_uT[i]], w=[tb])
            evac(pb, tb, i)

    k.proj_fm = proj_fm
    k.proj_tm = proj_tm
    k.load_w = load_w
    k.bank = bank
    k.pair = pair
    k.dump = dump
    k.ln_to_uT = ln_to_uT
    k.ln_stats = ln_stats
    k.st_slot = st_slot
    k.bcast_rows = bcast_rows
    for nm in ("x_d ctx_d w0_d sink_d lbl_d hn_d wo0_d w1_d gw_d gb_d gn_d wo1_d eg_d eu_d ed_d out_d hmid_d hout0_d mix_d "
               "t_hmid t_hout0 t_mixd t_out identF identB onesF onesB cst Mf Mb MP MN rmask modT t_modT mod_d t_mod bc t_bc uT t_uT "
               "wbuf t_wb RB t_rb HB t_hb sm gates t_gates t_c PS PT").split():
        setattr(k, nm, locals()[nm])

    for ph, l in [("B", 0), ("M", 0), ("D", 0), ("E", 0), ("B", 1), ("M", 1), ("D", 1), ("E", 1)]:
        if ph == "B":
            phase_B(k, l)
        elif ph == "M":
            (mixer_even if l == 0 else mixer_odd)(k)
        elif ph == "D":
            phase_D(k, l)
        else:
            phase_E(k, l)
        S.barrier()
        if stop is not None and stop[0:2] == f"{ph}{l}":
            break

    S.wait_all("sp", t_out + k.dbg_out)
    S.emit()
    S.close()
    return nc


def phase_B(k, l):
    S = k.S
    src_d = None
    for i in range(NT):
        slot = i % 2
        ht = k.RB[:, slot, 0:1024]
        xn = k.RB[:, slot, 1024:2048]
        t_ht = k.t_rb[slot]
        t_xn = k.t_rb[2 + slot]
        if l == 0:
            src = k.ctx_d[i * 128:(i + 1) * 128, :] if i < 2 else k.x_d[(i - 2) * 128:(i - 1) * 128, :]
            S.dma(ht, src, w=[t_ht])
        else:
            S.dma(ht, k.hout0_d[i * 128:(i + 1) * 128, :], r=[k.t_hout0[i]], w=[t_ht])
        xn = k.RB[:, 2 + slot, 0:1024]
        k.ln_to_uT(l, ht, t_ht, xn, t_xn, i, 0, False)
    k.dump(f"uT{l}", k.uT[:, :, :], [128, KC, N], BF16, k.t_uT)


def mixer_even(k):
    pass


def row_to_col(k, src, nr, n, dst, t_src, t_dst):
    S = k.S
    pb, tb = k.bank()
    S.op("pe", lambda e: e.transpose(out=pb[0:n, 0:nr], in_=src, identity=k.identF[0:nr, 0:nr]), r=[t_src, k.t_c], w=[tb])
    S.op("dve", lambda e: e.tensor_copy(out=dst, in_=pb[0:n, 0:nr]), r=[tb], w=[t_dst])


def proj_block(k, wv, t_w, c0, M, t0, nt):
    S = k.S
    pb, tb = k.bank()
    for kk in range(KC):
        S.op("pe", lambda e, kk=kk: e.matmul(pb[0:M, 0:nt], lhsT=wv[:, kk, c0:c0 + M], rhs=k.uT[:, kk, t0:t0 + nt],
                                             start=(kk == 0), stop=(kk == KC - 1)),
             r=t_w + k.t_uT[t0 // 128:(t0 + nt) // 128], w=[tb])
    return pb, tb


def gated_scan(k, dk, dvh, nh, q_ap, t_q, k_ap, t_k, A, t_A, B, t_B, make_logf, v_fn, t_v, o_acc, t_o, qt, t_qt, kt, t_kt):
    S = k.S
    sc = k.scn
    t_sc = k.t_scn
    NC_ = N // 64
    dvt = dvh * nh
    rr = sc["rr"][0:dk, :]
    gg = sc["gg"][0:dk, :]
    X1 = sc["X1"][0:dk, :]
    X2 = sc["X2"][0:dk, :]
    EG = sc["EG"][0:dk, :]
    B3 = B.rearrange("p (c l) -> p c l", l=64)
    for d in range(2):
        make_logf(d)
        S.op("dve", lambda e: e.tensor_tensor_scan(out=B, data0=k.rmask[0:dk, :], data1=A, initial=0.0, op0=ALU.mult, op1=ALU.add),
             r=[t_A, k.t_c], w=[t_B])
        S.op("pool", lambda e: e.tensor_copy(out=gg, in_=B3[:, :, 63]), r=[t_B], w=[t_sc])
        if d == 1:
            S.op("pool", lambda e: e.tensor_tensor(out=B, in0=B, in1=A, op=ALU.subtract), r=[t_B, t_A], w=[t_B])
        S.op("pool", lambda e: e.tensor_copy(out=rr, in_=B3[:, :, 32]), r=[t_B], w=[t_sc])
        S.op("dve", lambda e: e.tensor_tensor(out=B3, in0=B3, in1=rr.unsqueeze(2).to_broadcast([dk, NC_, 64]), op=ALU.subtract),
             r=[t_B, t_sc], w=[t_B])
        sgn = 1.0 if d == 0 else -1.0
        S.op("act", lambda e, sgn=sgn: e.activation(out=A, in_=B, func=AF.Exp, scale=sgn), r=[t_B], w=[t_A])
        S.op("dve", lambda e: e.tensor_tensor(out=qt, in0=q_ap, in1=A, op=ALU.mult), r=[t_q, t_A], w=[t_qt])
        S.op("act", lambda e, sgn=sgn: e.activation(out=A, in_=B, func=AF.Exp, scale=-sgn), r=[t_B, t_qt], w=[t_A])
        S.op("dve", lambda e: e.tensor_tensor(out=kt, in0=k_ap, in1=A, op=ALU.mult), r=[t_k, t_A], w=[t_kt])
        S.op("act", lambda e: e.activation(out=X1, in_=rr, func=AF.Exp), r=[t_sc], w=[t_sc])
        S.op("act", lambda e: e.activation(out=EG, in_=gg, func=AF.Exp), r=[t_sc], w=[t_sc])
        S.op("dve", lambda e: e.tensor_tensor(out=X2, in0=gg, in1=rr, op=ALU.subtract), r=[t_sc], w=[t_sc])
        S.op("act", lambda e: e.activation(out=X2, in_=X2, func=AF.Exp), r=[t_sc], w=[t_sc])
        a_s, c_s = (X1, X2) if d == 0 else (X2, X1)
        if d == 0:
            order = [(i, hf) for i in range(NT) for hf in (0, 1)]
        else:
            order = [(i, hf) for i in (1, 0) for hf in (1, 0)] + [(i, hf) for i in range(NT - 1, 1, -1) for hf in (1, 0)]
        Sst = sc["Sst"][0:dk, 0:dvt]
        t_S = k.t_Sst
        S.op("pool", lambda e: e.memset(Sst, 0.0), w=[t_S])
        S.op("pool", lambda e: e.memset(sc["Sbf"][0:dk, 0, 0:dvt], 0.0), w=[k.t_Sbf[0]])
        sb_i = 0
        M_ = k.Mf if d == 0 else k.Mb
        po = None
        for n_, (i, hf) in enumerate(order):
            c = 2 * i + hf
            first_in_tile = (n_ % 2 == 0)
            vt = v_fn(i)
            if first_in_tile:
                tk0 = i * 128
                pA, tA = k.bank()
                S.op("pe", lambda e, pA=pA, tk0=tk0: e.matmul(pA[:, 0:128], lhsT=kt[:, tk0:tk0 + 128], rhs=qt[:, tk0:tk0 + 128],
                                                              start=True, stop=True), r=[t_kt, t_qt], w=[tA])
                am_i = k.am_i
                k.am_i = (am_i + 1) % 2
                Am = sc["Am"][:, am_i, :]
                S.op("dve", lambda e, pA=pA, Am=Am, M_=M_: e.tensor_tensor(out=Am, in0=pA[:, 0:128], in1=M_[:], op=ALU.mult),
                     r=[tA, k.t_c], w=[k.t_Am[am_i]])
                pK, tK = k.bank()
                S.op("pe", lambda e, pK=pK, tk0=tk0: e.matmul(pK[:, 0:dk], lhsT=kt[:, tk0:tk0 + 128], rhs=k.identB[0:dk, 0:dk],
                                                              start=True, stop=True), r=[t_kt, k.t_c], w=[tK])
                ktok = sc["ktok"][:, am_i, 0:dk]
                S.op("act", lambda e, pK=pK, ktok=ktok: e.activation(out=ktok, in_=pK[:, 0:dk], func=AF.Copy), r=[tK], w=[k.t_ktok[am_i]])
                po, tpo = k.bank()
            rows = slice(hf * 64, hf * 64 + 64)
            tok0 = c * 64
            Sb = sc["Sbf"][0:dk, sb_i, 0:dvt]
            for a in range(nh):
                S.op("pe", lambda e, a=a, po=po, vt=vt, Am=Am, rows=rows, hf=hf: e.matmul(
                    po[0:dvh, a * 128 + hf * 64:a * 128 + hf * 64 + 64], lhsT=vt[rows, a * dvh:(a + 1) * dvh], rhs=Am[rows, hf * 64:hf * 64 + 64],
                    start=True, stop=False), r=t_v + [k.t_Am[am_i]], w=[tpo])
                S.op("pe", lambda e, a=a, po=po, Sb=Sb, tok0=tok0, hf=hf: e.matmul(
                    po[0:dvh, a * 128 + hf * 64:a * 128 + hf * 64 + 64], lhsT=Sb[:, a * dvh:(a + 1) * dvh], rhs=qt[:, tok0:tok0 + 64],
                    start=False, stop=True), r=[k.t_Sbf[sb_i], t_qt], w=[tpo])
            if n_ < len(order) - 1:
                pS, tS = k.bank()
                S.op("pe", lambda e, pS=pS, ktok=ktok, vt=vt, rows=rows: e.matmul(pS[0:dk, 0:dvt], lhsT=ktok[rows, :], rhs=vt[rows, 0:dvt],
                                                                                  start=True, stop=True), r=[k.t_ktok[am_i]] + t_v, w=[tS])
                S.op("pool", lambda e, c=c: e.tensor_scalar(out=Sst, in0=Sst, scalar1=EG[:, c:c + 1], scalar2=None, op0=ALU.mult),
                     r=[t_S, t_sc], w=[t_S])
                S.op("dve", lambda e, c=c, pS=pS, c_s=c_s: e.scalar_tensor_tensor(out=Sst, in0=pS[0:dk, 0:dvt], scalar=c_s[:, c:c + 1], in1=Sst,
                                                                         op0=ALU.mult, op1=ALU.add), r=[tS, t_S, t_sc], w=[t_S])
                ni, nhf = order[n_ + 1]
                cn = 2 * ni + nhf
                sb_i = 1 - sb_i
                Sbn = sc["Sbf"][0:dk, sb_i, 0:dvt]
                S.op("act", lambda e, cn=cn, Sbn=Sbn, a_s=a_s: e.activation(out=Sbn, in_=Sst, func=AF.Identity, scale=a_s[:, cn:cn + 1]),
                     r=[t_S, t_sc], w=[k.t_Sbf[sb_i]])
            if not first_in_tile:
                tk0 = i * 128
                for a in range(nh):
                    if d == 0:
                        S.op("act", lambda e, a=a, po=po, tk0=tk0: e.activation(out=o_acc[a][:, tk0:tk0 + 128], in_=po[0:dvh, a * 128:(a + 1) * 128],
                                                                                func=AF.Copy), r=[tpo], w=[t_o[a]])
                    else:
                        S.op("dve", lambda e, a=a, po=po, tk0=tk0: e.tensor_tensor(out=o_acc[a][:, tk0:tk0 + 128], in0=po[0:dvh, a * 128:(a + 1) * 128],
                                                                                   in1=o_acc[a][:, tk0:tk0 + 128], op=ALU.add), r=[tpo, t_o[a]], w=[t_o[a]])


def rms_gate_out(k, dvh, nh, o_acc, t_o, A, t_A, B, t_B, gsil, t_gs, gn_cols, t_gn, mixrow, t_mix, chunk_ids):
    S = k.S
    dv = dvh * nh
    for (t0, nt) in TOKB:
        pb, tb = k.bank()
        for a in range(nh):
            S.op("act", lambda e, a=a, t0=t0, nt=nt: e.activation(out=A[0:dvh, a * 512:a * 512 + nt], in_=o_acc[a][:, t0:t0 + nt], func=AF.Square),
                 r=[t_o[a]], w=[t_A])
        for a in range(nh):
            S.op("pe", lambda e, a=a, pb=pb, nt=nt: e.matmul(pb[0:dvh, 0:nt], lhsT=k.onesF[0:dvh, 0:dvh], rhs=A[0:dvh, a * 512:a * 512 + nt],
                                                             start=(a == 0), stop=(a == nh - 1)), r=[t_A, k.t_c], w=[tb])
        S.op("act", lambda e, pb=pb, nt=nt: e.activation(out=B[0:dvh, 0:nt], in_=pb[0:dvh, 0:nt], func=AF.Sqrt, scale=1.0 / dv, bias=k.cst[0:dvh, 0:1]),
             r=[tb, k.t_c], w=[t_B])
        S.op("dve", lambda e, nt=nt: e.reciprocal(out=B[0:dvh, 0:nt], in_=B[0:dvh, 0:nt]), r=[t_B], w=[t_B])
        for a in range(nh):
            S.op("dve", lambda e, a=a, t0=t0, nt=nt: e.tensor_tensor(out=B[0:dvh, 512 + a * 512:512 + a * 512 + nt], in0=o_acc[a][:, t0:t0 + nt],
                                                                     in1=B[0:dvh, 0:nt], op=ALU.mult), r=[t_o[a], t_B], w=[t_B])
            S.op("dve", lambda e, a=a, t0=t0, nt=nt: e.scalar_tensor_tensor(out=mixrow[a][0:dvh, t0:t0 + nt], in0=B[0:dvh, 512 + a * 512:512 + a * 512 + nt],
                                                                            scalar=gn_cols[a], in1=gsil[a][0:dvh, t0:t0 + nt], op0=ALU.mult, op1=ALU.mult),
                 r=[t_B, t_gn, t_gs[a]], w=[t_mix[a]])
    for a in range(nh):
        S.dma(k.mix_d[chunk_ids[a], 0:dvh, :], mixrow[a][0:dvh, :], r=[t_mix[a]], w=[k.t_mixd[chunk_ids[a]]])


def scan_setup(k):
    S = k.S
    if hasattr(k, "scn"):
        return
    sc = {}
    sc["rr"] = S.sbuf("sc_rr", [128, 36], F32)
    sc["gg"] = S.sbuf("sc_gg", [128, 36], F32)
    sc["X1"] = S.sbuf("sc_X1", [128, 36], F32)
    sc["X2"] = S.sbuf("sc_X2", [128, 36], F32)
    sc["EG"] = S.sbuf("sc_EG", [128, 36], F32)
    sc["Sst"] = S.sbuf("sc_Sst", [128, 192], F32)
    sc["Sbf"] = S.sbuf("sc_Sbf", [128, 2, 192], BF16)
    sc["Am"] = S.sbuf("sc_Am", [128, 2, 128], BF16)
    sc["ktok"] = S.sbuf("sc_ktok", [128, 2, 128], BF16)
    sc["col"] = S.sbuf("sc_col", [128, 64], F32)
    sc["row"] = S.sbuf("sc_row", [8, 768], F32)
    k.scn = sc
    k.t_scn = Trk()
    k.t_Sst = Trk()
    k.t_Sbf = [Trk(), Trk()]
    k.t_Am = [Trk(), Trk()]
    k.t_ktok = [Trk(), Trk()]
    k.t_col = Trk()
    k.t_row = Trk()
    k.am_i = 0


ATOK = [(0, 256), (256, 512), (768, 512), (1280, 512), (1792, 512)]


def mixer_even(k):
    S = k.S
    scan_setup(k)
    sc = k.scn
    RB, HB, t_rb, t_hb = k.RB, k.HB, k.t_rb, k.t_hb
    Ct = RB[:, 4, 0:2048]
    St = RB[:, 5, 0:2048]
    col = sc["col"]
    t_col = k.t_col
    ci = col[:, 0:8].bitcast(I32)
    S.op("pool", lambda e: e.iota(ci[:, 0:1], pattern=[[0, 1]], base=0, channel_multiplier=1), w=[t_col])
    S.op("dve", lambda e: e.tensor_single_scalar(out=ci[:, 1:2], in_=ci[:, 0:1], scalar=15, op=ALU.bitwise_and), r=[t_col], w=[t_col])
    S.op("dve", lambda e: e.tensor_scalar(out=ci[:, 2:3], in0=ci[:, 0:1], scalar1=5, scalar2=1, op0=ALU.logical_shift_right, op1=ALU.bitwise_and),
         r=[t_col], w=[t_col])
    S.op("dve", lambda e: e.tensor_scalar(out=ci[:, 3:4], in0=ci[:, 0:1], scalar1=4, scalar2=1, op0=ALU.logical_shift_right, op1=ALU.bitwise_and),
         r=[t_col], w=[t_col])
    S.op("dve", lambda e: e.tensor_copy(out=col[:, 8:11], in_=ci[:, 1:4]), r=[t_col], w=[t_col])
    S.op("act", lambda e: e.activation(out=col[:, 11:12], in_=col[:, 8:9], func=AF.Exp, scale=-math.log(10000.0) / 16.0), r=[t_col], w=[t_col])
    S.op("dve", lambda e: e.tensor_tensor(out=col[:, 13:14], in0=col[:, 11:12], in1=col[:, 9:10], op=ALU.mult), r=[t_col], w=[t_col])
    S.op("dve", lambda e: e.tensor_tensor(out=col[:, 12:13], in0=col[:, 11:12], in1=col[:, 13:14], op=ALU.subtract), r=[t_col], w=[t_col])
    S.op("dve", lambda e: e.tensor_scalar(out=col[:, 14:15], in0=col[:, 10:11], scalar1=2.0, scalar2=-1.0, op0=ALU.mult, op1=ALU.add),
         r=[t_col], w=[t_col])
    ri = RB[:, 0, 0:2048].bitcast(I32)
    qi = RB[:, 1, 0:2048].bitcast(I32)
    S.op("pool", lambda e: e.iota(ri, pattern=[[1, 32], [0, 64]], base=0, channel_multiplier=0), w=[t_rb[0]])
    S.op("pool", lambda e: e.iota(qi, pattern=[[0, 32], [1, 64]], base=0, channel_multiplier=0), w=[t_rb[1]])
    rf = RB[:, 2, 0:2048]
    qf = RB[:, 3, 0:2048]
    S.op("dve", lambda e: e.tensor_copy(out=rf, in_=ri), r=[t_rb[0]], w=[t_rb[2]])
    S.op("dve", lambda e: e.tensor_copy(out=qf, in_=qi), r=[t_rb[1]], w=[t_rb[3]])
    ang = RB[:, 0, 0:2048]
    S.op("dve", lambda e: e.tensor_scalar(out=ang, in0=rf, scalar1=col[:, 12:13], scalar2=None, op0=ALU.mult), r=[t_rb[2], t_col], w=[t_rb[0]])
    S.op("dve", lambda e: e.scalar_tensor_tensor(out=ang, in0=qf, scalar=col[:, 13:14], in1=ang, op0=ALU.mult, op1=ALU.add),
         r=[t_rb[3], t_rb[0], t_col], w=[t_rb[0]])
    def range_reduce(dst, t_dst, add, tmpi, t_tmpi, tmpf_, t_tmpf):
        S.op("dve", lambda e: e.tensor_scalar(out=tmpi, in0=ang, scalar1=add, scalar2=1.0 / (2 * PI), op0=ALU.add, op1=ALU.mult),
             r=[t_rb[0]], w=[t_tmpi])
        S.op("dve", lambda e: e.tensor_copy(out=tmpf_, in_=tmpi), r=[t_tmpi], w=[t_tmpf])
        S.op("dve", lambda e: e.scalar_tensor_tensor(out=dst, in0=tmpf_, scalar=-2 * PI, in1=ang, op0=ALU.mult, op1=ALU.add),
             r=[t_tmpf, t_rb[0]], w=[t_dst])
        if add != 0.0:
            S.op("dve", lambda e: e.tensor_scalar(out=dst, in0=dst, scalar1=add, scalar2=None, op0=ALU.add), r=[t_dst], w=[t_dst])
        S.op("dve", lambda e: e.tensor_scalar(out=tmpf_, in0=dst, scalar1=PI, scalar2=-2 * PI, op0=ALU.is_gt, op1=ALU.mult),
             r=[t_dst], w=[t_tmpf])
        S.op("dve", lambda e: e.tensor_tensor(out=dst, in0=dst, in1=tmpf_, op=ALU.add), r=[t_dst, t_tmpf], w=[t_dst])
        S.op("dve", lambda e: e.tensor_scalar(out=tmpf_, in0=dst, scalar1=-PI, scalar2=2 * PI, op0=ALU.is_lt, op1=ALU.mult),
             r=[t_dst], w=[t_tmpf])
        S.op("dve", lambda e: e.tensor_tensor(out=dst, in0=dst, in1=tmpf_, op=ALU.add), r=[t_dst, t_tmpf], w=[t_dst])
        S.op("dve", lambda e: e.tensor_scalar(out=dst, in0=dst, scalar1=PI, scalar2=-PI, op0=ALU.min, op1=ALU.max), r=[t_dst], w=[t_dst])
    m1 = RB[:, 1, 0:2048]
    tmpi = RB[:, 2, 0:2048].bitcast(I32)
    tmpf_ = RB[:, 3, 0:2048]
    range_reduce(m1, t_rb[1], 0.0, tmpi, t_rb[2], tmpf_, t_rb[3])
    S.op("act", lambda e: e.activation(out=St, in_=m1, func=AF.Sin, scale=col[:, 14:15]), r=[t_rb[1], t_col], w=[t_rb[5]])
    range_reduce(m1, t_rb[1], PI / 2, tmpi, t_rb[2], tmpf_, t_rb[3])
    S.op("act", lambda e: e.activation(out=Ct, in_=m1, func=AF.Sin), r=[t_rb[1]], w=[t_rb[4]])
    k.dump("ropeC", Ct, [128, 2048], F32, [t_rb[4]])
    k.dump("ropeS", St, [128, 2048], F32, [t_rb[5]])
    if k.sub == "M0a":
        return
    S.dma(col[:, 16:24], k.sink_d[0:1, :].to_broadcast([128, 8]), w=[t_col])
    S.op("act", lambda e: e.activation(out=col[:, 16:24], in_=col[:, 16:24], func=AF.Exp), r=[t_col], w=[t_col])

    qT = HB[:, 0:2, :]
    kAB = [HB[:, 2, :], HB[:, 3, :]]
    vdup = HB[:, 4, :].rearrange("p (i c) -> p i c", c=128)
    mixA = [HB[:, 5, :], HB[:, 6, :]]
    et = [HB[:, 7, ei * 512:(ei + 1) * 512] for ei in range(4)]
    S.op("pool", lambda e: e.memset(kAB[0][64:128, :], 0.0), w=[t_hb[2]])
    S.op("pool", lambda e: e.memset(kAB[1][0:64, :], 0.0), w=[t_hb[3]])
    t_et = [Trk() for _ in range(4)]
    k.et_i = 0
    tmpf = [RB[:, 0, 0:512], RB[:, 0, 512:1024], RB[:, 1, 0:512], RB[:, 1, 512:1024]]
    t_tmp = [Trk() for _ in range(4)]
    dn = RB[:, 2, 0:512]
    t_dn = t_rb[2]
    wv_v, t_wv = k.load_w(k.w0_d[:, 1536:1664].rearrange("(k p) n -> p k n", p=128))
    for j in range(2):
        base = j * 768
        wq, t_wq = k.load_w(k.w0_d[:, base:base + 512].rearrange("(k p) n -> p k n", p=128))
        wk, t_wk = k.load_w(k.w0_d[:, base + 512:base + 768].rearrange("(k p) n -> p k n", p=128))
        tmp_i = 0
        for (t0, nt) in ATOK:
            for blk in range(3):
                if blk < 2:
                    pq, tq = proj_block(k, wq, t_wq, blk * 128, 128, t0, nt)
                    dst = qT[:, blk, t0:t0 + nt]
                    tdst = t_hb[blk]
                else:
                    pq, tq = proj_block(k, wk, t_wk, 0, 128, t0, nt)
                    dst = None
                if t0 == 0:
                    if blk < 2:
                        S.op("act", lambda e, pq=pq, dst=dst, nt=nt: e.activation(out=dst, in_=pq[:, 0:nt], func=AF.Copy), r=[tq], w=[tdst])
                    else:
                        S.op("act", lambda e, pq=pq, nt=nt, t0=t0: e.activation(out=kAB[0][0:64, t0:t0 + nt], in_=pq[0:64, 0:nt], func=AF.Copy), r=[tq], w=[t_hb[2]])
                        S.op("act", lambda e, pq=pq, nt=nt, t0=t0: e.activation(out=kAB[1][64:128, t0:t0 + nt], in_=pq[64:128, 0:nt], func=AF.Copy), r=[tq], w=[t_hb[3]])
                    continue
                if blk < 2:
                    ps_, ts_ = proj_block(k, wq, t_wq, 256 + blk * 128, 128, t0, nt)
                else:
                    ps_, ts_ = proj_block(k, wk, t_wk, 128, 128, t0, nt)
                l0 = t0 - 256
                ta, tb_ = tmp_i % 4, (tmp_i + 1) % 4
                tmp_i += 2
                S.op("dve", lambda e, pq=pq, ta=ta, l0=l0, nt=nt: e.tensor_tensor(out=tmpf[ta][:, 0:nt], in0=pq[:, 0:nt], in1=Ct[:, l0:l0 + nt], op=ALU.mult),
                     r=[tq, t_rb[4]], w=[t_tmp[ta]])
                S.op("dve", lambda e, ps_=ps_, tb_=tb_, l0=l0, nt=nt: e.tensor_tensor(out=tmpf[tb_][:, 0:nt], in0=ps_[:, 0:nt], in1=St[:, l0:l0 + nt], op=ALU.mult),
                     r=[ts_, t_rb[5]], w=[t_tmp[tb_]])
                if blk < 2:
                    S.op("pool", lambda e, dst=dst, ta=ta, tb_=tb_, nt=nt: e.tensor_tensor(out=dst, in0=tmpf[ta][:, 0:nt], in1=tmpf[tb_][:, 0:nt], op=ALU.add),
                         r=[t_tmp[ta], t_tmp[tb_]], w=[tdst])
                else:
                    S.op("pool", lambda e, ta=ta, tb_=tb_, nt=nt, t0=t0: e.tensor_tensor(out=kAB[0][0:64, t0:t0 + nt], in0=tmpf[ta][0:64, 0:nt], in1=tmpf[tb_][0:64, 0:nt], op=ALU.add),
                         r=[t_tmp[ta], t_tmp[tb_]], w=[t_hb[2]])
                    S.op("pool", lambda e, ta=ta, tb_=tb_, nt=nt, t0=t0: e.tensor_tensor(out=kAB[1][64:128, t0:t0 + nt], in0=tmpf[ta][64:128, 0:nt], in1=tmpf[tb_][64:128, 0:nt], op=ALU.add),
                         r=[t_tmp[ta], t_tmp[tb_]], w=[t_hb[3]])

        if k.sub == "M0p":
            k.dump("qT0", qT, [128, 2, N], BF16, [t_hb[0], t_hb[1]])
            return

        def ev_v(pb, tb, i, j=j):
            S.op("act", lambda e: e.activation(out=vdup[:, i, 0:64], in_=pb[:, j * 64:(j + 1) * 64], func=AF.Copy), r=[tb], w=[t_hb[4]])
            S.op("dve", lambda e: e.tensor_copy(out=vdup[:, i, 64:128], in_=pb[:, j * 64:(j + 1) * 64]), r=[tb], w=[t_hb[4]])
        k.proj_tm(wv_v, t_wv, 0, 128, ev_v)
        if j == 0:
            k.dump("qT0", qT, [128, 2, N], BF16, [t_hb[0], t_hb[1]])
            k.dump("kT0", HB[:, 2:4, :], [128, 2, N], BF16, [t_hb[2], t_hb[3]])
        if k.sub == "M0v":
            return
        for qb in range(NT):
            q0 = qb * 128
            if (k.sub == "M0q1" and qb == 1) or (k.sub == "M0q3" and qb == 3):
                k.dump("mixA", HB[:, 5:7, :], [128, 2, N], BF16, [t_hb[5], t_hb[6]])
                return
            if qb < 2:
                chunks = [(0, None), (1, None)]
            else:
                n_ = qb - 2
                chunks = [(0, None), (1, None)]
                if n_ > 0:
                    chunks.append((qb - 1, k.MP))
                chunks.append((qb, None))
                if n_ < 15:
                    chunks.append((qb + 1, k.MN))
            po, tpo = k.bank()
            pd, tpd = k.bank()
            for ci_, (kc, msk) in enumerate(chunks):
                pss, tss = k.bank()
                for hh in range(4):
                    blk, half = hh // 2, hh % 2
                    rows = slice(half * 64, half * 64 + 64)
                    S.op("pe", lambda e, pss=pss, hh=hh, blk=blk, half=half, kc=kc, q0=q0: e.matmul(
                        pss[:, hh * 128:(hh + 1) * 128], lhsT=kAB[half][:, kc * 128:(kc + 1) * 128], rhs=qT[:, blk, q0:q0 + 128],
                        start=True, stop=True), r=[t_hb[2 + half], t_hb[blk]], w=[tss])
                ei = k.et_i
                k.et_i = (ei + 1) % 4
                S.op("act", lambda e, pss=pss, ei=ei: e.activation(out=et[ei], in_=pss[:, :], func=AF.Exp, scale=0.125), r=[tss], w=[t_et[ei]])
                if msk is not None:
                    S.op("dve", lambda e, ei=ei, msk=msk: e.tensor_tensor(out=et[ei], in0=et[ei], in1=msk[:], op=ALU.mult),
                         r=[t_et[ei], k.t_c], w=[t_et[ei]])
                if k.sub == "M0qa":
                    k.dump("et", HB[:, 7, :], [128, N], BF16, t_et)
                    return
                nch = len(chunks)
                S.op("pe", lambda e, ei=ei, kc=kc, ci_=ci_, po=po, nch=nch: e.matmul(
                    po[:, :], lhsT=vdup[:, kc, :], rhs=et[ei][:, :], start=(ci_ == 0), stop=(ci_ == nch - 1)),
                    r=[t_hb[4], t_et[ei]], w=[tpo])
                S.op("pe", lambda e, ei=ei, ci_=ci_, pd=pd, nch=nch: e.matmul(
                    pd[:, :], lhsT=k.onesB[:], rhs=et[ei][:, :], start=(ci_ == 0), stop=(ci_ == nch - 1)),
                    r=[k.t_c, t_et[ei]], w=[tpd])
            if k.sub == "M0qb":
                S.op("dve", lambda e, po=po: e.tensor_copy(out=RB[:, 0, 0:512], in_=po[:, :]), r=[tpo], w=[t_rb[0]])
                S.op("dve", lambda e, pd=pd: e.tensor_copy(out=RB[:, 0, 512:1024], in_=pd[:, :]), r=[tpd], w=[t_rb[0]])
                k.dump("popd", RB[:, 0, 0:1024], [128, 1024], F32, [t_rb[0]])
                return
            for hh in range(4):
                S.op("dve", lambda e, hh=hh, pd=pd, j=j: e.tensor_scalar(out=dn[:, hh * 128:(hh + 1) * 128], in0=pd[:, hh * 128:(hh + 1) * 128],
                                                                    scalar1=col[:, 16 + 4 * j + hh:17 + 4 * j + hh], scalar2=None, op0=ALU.add),
                     r=[tpd, t_col], w=[t_dn])
            S.op("dve", lambda e: e.reciprocal(out=dn, in_=dn), r=[t_dn], w=[t_dn])
            for hh in range(4):
                blk, half = hh // 2, hh % 2
                rows = slice(half * 64, half * 64 + 64)
                S.op("dve", lambda e, hh=hh, blk=blk, rows=rows, po=po, q0=q0: e.tensor_tensor(
                    out=mixA[blk][rows, q0:q0 + 128], in0=po[rows, hh * 128:(hh + 1) * 128], in1=dn[rows, hh * 128:(hh + 1) * 128], op=ALU.mult),
                    r=[tpo, t_dn], w=[t_hb[5 + blk]])
        for blk in range(2):
            S.dma(k.mix_d[2 * j + blk, :, :], mixA[blk], r=[t_hb[5 + blk]], w=[k.t_mixd[2 * j + blk]])
    k.dump("mixd_att", k.mix_d[0:4, :, :], [4, 128, N], BF16, k.t_mixd[0:4])
    S.barrier()
    if k.sub == "M0b":
        return

    row = sc["row"]
    t_row = k.t_row
    S.dma(row[0:4, 0:512], k.lbl_d.rearrange("r a n -> (r a) n"), w=[t_row])
    for h in range(4):
        row_to_col(k, row[0:4, h * 128:(h + 1) * 128], 4, 128, col[:, 24 + 4 * h:28 + 4 * h], t_row, t_col)
    lbT = col[:, 24:40].rearrange("p (h r a) -> p h r a", r=2, a=2)
    lbv = col[:, 44:52].rearrange("p (h r) -> p h r", r=2)
    omv = col[:, 52:60].rearrange("p (h r) -> p h r", r=2)
    S.op("dve", lambda e: e.tensor_tensor(out=lbv, in0=lbT[:, :, :, 0], in1=lbT[:, :, :, 1], op=ALU.subtract), r=[t_col], w=[t_col])
    S.op("act", lambda e: e.activation(out=lbv, in_=lbv, func=AF.Sigmoid), r=[t_col], w=[t_col])
    S.op("dve", lambda e: e.tensor_scalar(out=omv, in0=lbv, scalar1=-1.0, scalar2=1.0, op0=ALU.mult, op1=ALU.add), r=[t_col], w=[t_col])
    S.dma(row[0:1, 0:512], k.hn_d[:, :], w=[t_row])
    for h in range(4):
        row_to_col(k, row[0:1, h * 128:(h + 1) * 128], 1, 128, col[:, 40 + h:41 + h], t_row, t_col)

    qrow, Krow, A, B, oacc = RB[:, 0, :], RB[:, 1, :], RB[:, 2, :], RB[:, 3, :], RB[:, 4, :]
    qt, kt, gsil, mixrow = HB[:, 0, :], HB[:, 1, :], HB[:, 2, :], HB[:, 4, :]
    vtm = HB[:, 3, :].rearrange("p (i c) -> p i c", c=128)
    for h in range(4):
        base = 1664 + 512 * h
        wg, t_wg = k.load_w(k.w0_d[:, base:base + 512].rearrange("(k p) n -> p k n", p=128))
        wvv, t_wvv = k.load_w(k.w0_d[:, 3712 + 128 * h:3712 + 128 * (h + 1)].rearrange("(k p) n -> p k n", p=128))

        def ev_q(pb, tb, t0, nt):
            S.op("act", lambda e: e.activation(out=qrow[:, t0:t0 + nt], in_=pb[:, 0:nt], func=AF.Copy), r=[tb], w=[t_rb[0]])
        k.proj_fm(wg, t_wg, 256, 128, ev_q)

        def ev_g(pb, tb, t0, nt):
            S.op("act", lambda e: e.activation(out=B[:, t0:t0 + nt], in_=pb[:, 0:nt], func=AF.Sigmoid), r=[tb], w=[t_rb[3]])
            S.op("dve", lambda e: e.tensor_tensor(out=gsil[:, t0:t0 + nt], in0=pb[:, 0:nt], in1=B[:, t0:t0 + nt], op=ALU.mult),
                 r=[tb, t_rb[3]], w=[t_hb[2]])
        k.proj_fm(wg, t_wg, 384, 128, ev_g)

        def ev_v2(pb, tb, i):
            S.op("act", lambda e: e.activation(out=vtm[:, i, :], in_=pb[:, 0:128], func=AF.Copy), r=[tb], w=[t_hb[3]])
        k.proj_tm(wvv, t_wvv, 0, 128, ev_v2)

        def make_logf(d, h=h, wg=wg, t_wg=t_wg):
            def ev_z(pb, tb, t0, nt):
                S.op("act", lambda e: e.activation(out=A[:, t0:t0 + nt], in_=pb[:, 0:nt], func=AF.Sigmoid), r=[tb], w=[t_rb[2]])
            k.proj_fm(wg, t_wg, d * 128, 128, ev_z)
            S.op("dve", lambda e: e.tensor_scalar(out=A, in0=A, scalar1=omv[:, h, d:d + 1], scalar2=lbv[:, h, d:d + 1], op0=ALU.mult, op1=ALU.add),
                 r=[t_rb[2], t_col], w=[t_rb[2]])
            S.op("pool", lambda e: e.tensor_scalar(out=Krow, in0=A, scalar1=-1.0, scalar2=1.0, op0=ALU.mult, op1=ALU.add),
                 r=[t_rb[2]], w=[t_rb[1]])
            S.op("act", lambda e: e.activation(out=A, in_=A, func=AF.Ln), r=[t_rb[2]], w=[t_rb[2]])
        gated_scan(k, 128, 128, 1, qrow, t_rb[0], Krow, t_rb[1], A, t_rb[2], B, t_rb[3], make_logf,
                   lambda i: vtm[:, i, :], [t_hb[3]], [oacc], [t_rb[4]], qt, t_hb[0], kt, t_hb[1])
        if h == 0:
            k.dump("oacc0", oacc, [128, N], F32, [t_rb[4]])
        rms_gate_out(k, 128, 1, [oacc], [t_rb[4]], A, t_rb[2], B, t_rb[3], [gsil], [t_hb[2]], [col[:, 40 + h:41 + h]], t_col,
                     [mixrow], [t_hb[4]], [4 + h])
    k.dump("mixd0", k.mix_d[0:8, :, :], [8, 128, N], BF16, k.t_mixd[0:8])


def phase_D(k, l):
    S = k.S
    RB, HB = k.RB, k.HB
    k.bcast_rows(l, "mix")
    if l == 0:
        wo, t_wo = k.load_w(k.wo0_d.rearrange("(c p) n -> p c n", p=128), nslots=2)
        chunks = [(c, 128, wo, t_wo, c) for c in range(8)]
        tiles = list(range(NT))
        nch = 8
    else:
        wo, t_wo = k.load_w(k.wo1_d[0:768, :].rearrange("(c p) n -> p c n", p=96), nslots=2, parts=96)
        wf, t_wf = k.load_w(k.wo1_d[768:1024, :].rearrange("(c p) n -> p c n", p=128), nslots=1)
        chunks = [(c, 96, wo, t_wo, c) for c in range(8)] + [(8 + c, 128, wf, t_wf, c) for c in range(2)]
        tiles = list(range(2, NT))
        nch = 10
    tb_ = [RB[:, r, hf * 1024:(hf + 1) * 1024] for r in range(6) for hf in range(2)]
    tt = k.t_tile
    for n_, i in enumerate(tiles):
        s6 = (n_ % 2) * 6
        ht, tmp, xn1, hnew, xn2, ufb = [tb_[s6 + q] for q in range(6)]
        t_ht, t_tmp, t_xn1, t_hn, t_xn2, t_uf = [tt[s6 + q] for q in range(6)]
        uf = ufb.rearrange("p (k n) -> p k n", n=128)
        mt = HB[:, n_ % 2, 0:nch * 128].rearrange("p (c n) -> p c n", n=128)
        t_mt = k.t_hb[n_ % 2]
        S.dma(mt, k.mix_d[0:nch, :, i * 128:(i + 1) * 128].rearrange("c p n -> p c n"), r=k.t_mixd[0:nch], w=[t_mt])
        if l == 0:
            src = k.ctx_d[i * 128:(i + 1) * 128, :] if i < 2 else k.x_d[(i - 2) * 128:(i - 1) * 128, :]
            S.dma(ht, src, w=[t_ht])
        else:
            S.dma(ht, k.hout0_d[i * 128:(i + 1) * 128, :], r=[k.t_hout0[i]], w=[t_ht])
        pp, tp = k.pair()
        for hf in range(2):
            for ci_, (c, KR, wv, t_wv, wc) in enumerate(chunks):
                S.op("pe", lambda e, hf=hf, c=c, KR=KR, wv=wv, wc=wc, ci_=ci_, pp=pp, mt=mt: e.matmul(
                    pp[:, hf * 512:(hf + 1) * 512], lhsT=mt[0:KR, c, :], rhs=wv[0:KR, wc, hf * 512:(hf + 1) * 512],
                    start=(ci_ == 0), stop=(ci_ == len(chunks) - 1)), r=[t_mt] + t_wv, w=[tp[hf]])
        r_ = 1 if i < 2 else 0
        for hf in range(2):
            S.op("dve", lambda e, hf=hf, pp=pp, tmp=tmp, r_=r_: e.tensor_tensor(out=tmp[:, hf * 512:(hf + 1) * 512], in0=pp[:, hf * 512:(hf + 1) * 512],
                                                                               in1=k.bc[:, r_, hf * 512:(hf + 1) * 512], op=ALU.mult),
                 r=[tp[hf], k.t_bc[r_]], w=[t_tmp])
        S.op("dve", lambda e, ht=ht, tmp=tmp: e.scalar_tensor_tensor(out=tmp, in0=ht, scalar=ALPHA, in1=tmp, op0=ALU.mult, op1=ALU.add),
             r=[t_ht, t_tmp], w=[t_tmp])
        st_ap, mv_ap, rs_ap, nb_ap, t_st = k.st_slot()
        k.ln_stats(tmp, t_tmp, st_ap, mv_ap, rs_ap, nb_ap, t_st)
        S.op("act", lambda e, tmp=tmp, xn1=xn1, rs_ap=rs_ap, nb_ap=nb_ap: e.activation(out=xn1, in_=tmp, func=AF.Identity, scale=rs_ap, bias=nb_ap),
             r=[t_tmp, t_st], w=[t_xn1])
        S.op("dve", lambda e, xn1=xn1: e.tensor_tensor(out=xn1, in0=xn1, in1=k.bc[:, 2, :], op=ALU.mult), r=[t_xn1, k.t_bc[2]], w=[t_xn1])
        S.op("pool", lambda e, xn1=xn1, hnew=hnew: e.tensor_tensor(out=hnew, in0=xn1, in1=k.bc[:, 3, :], op=ALU.add), r=[t_xn1, k.t_bc[3]], w=[t_hn])
        S.dma(k.hmid_d[i * 128:(i + 1) * 128, :], hnew, r=[t_hn], w=[k.t_hmid[i]])
        k.ln_to_uT(l, hnew, t_hn, xn2, t_xn2, i, 3, True, uf, t_uf)
    k.dump(f"hmid{l}", k.hmid_d[:, :], [N, D], F32, k.t_hmid)
    k.dump(f"u2T{l}", k.uT[:, :, :], [128, KC, N], BF16, k.t_uT)
    k.dump(f"gates{l}", k.gates[:, :, :], [128, NT, 16], F32, k.t_gates)


def phase_E(k, l):
    S = k.S
    RB = k.RB
    k.bcast_rows(l, "moe")
    if l == 0:
        halves = [list(range(0, 9)), list(range(9, 18))]
        bsz = 384
    else:
        halves = [list(range(2, 10)), list(range(10, 18))]
        bsz = 512
    yacc = RB[:, 0:4, :].rearrange("p a n -> p (a n)").rearrange("p (t d) -> p t d", d=1024)
    t_y = k.t_tile[0:9]
    hTb = RB[:, 4, :].bitcast(BF16)
    hT = [hTb[:, q * 2048:(q + 1) * 2048].rearrange("p (f n) -> p f n", n=512) for q in range(2)]
    t_hT = [k.t_tile[9], k.t_tile[10]]
    sg = [RB[:, 5, q * 512:(q + 1) * 512] for q in range(2)]
    t_sg = [k.t_rb[4], k.t_rb[5]]
    Hb = RB[:, 5, 1024:2048]
    t_H = k.t_tile[11]
    dst_d = k.hout0_d if l == 0 else k.out_d
    k.h_i = 0
    k.s_i = 0
    for tiles in halves:
        tok0 = tiles[0] * 128
        ntok = len(tiles) * 128
        blocks = [(tok0 + b0, bsz) for b0 in range(0, ntok, bsz)]
        for e_ in range(16):
            w1, t_w1 = k.load_w(k.eg_d[l, e_].rearrange("(c p) n -> p c n", p=128))
            w3, t_w3 = k.load_w(k.eu_d[l, e_].rearrange("(c p) n -> p c n", p=128))
            w2, t_w2 = k.load_w(k.ed_d[l, e_].rearrange("(c p) n -> p c n", p=128))
            for (b0, nb) in blocks:
                hi = k.h_i
                k.h_i = 1 - hi
                hTc = hT[hi]
                for f in range(4):
                    p1, tp1 = k.bank()
                    for kk in range(KC):
                        S.op("pe", lambda e, kk=kk, f=f, p1=p1, w1=w1, b0=b0, nb=nb: e.matmul(
                            p1[:, 0:nb], lhsT=w1[:, kk, f * 128:(f + 1) * 128], rhs=k.uT[:, kk, b0:b0 + nb], start=(kk == 0), stop=(kk == KC - 1)),
                            r=t_w1 + k.t_uT[b0 // 128:(b0 + nb) // 128], w=[tp1])
                    p3, tp3 = k.bank()
                    for kk in range(KC):
                        S.op("pe", lambda e, kk=kk, f=f, p3=p3, w3=w3, b0=b0, nb=nb: e.matmul(
                            p3[:, 0:nb], lhsT=w3[:, kk, f * 128:(f + 1) * 128], rhs=k.uT[:, kk, b0:b0 + nb], start=(kk == 0), stop=(kk == KC - 1)),
                            r=t_w3 + k.t_uT[b0 // 128:(b0 + nb) // 128], w=[tp3])
                    si = k.s_i
                    k.s_i = 1 - si
                    S.op("act", lambda e, p1=p1, si=si, nb=nb: e.activation(out=sg[si][:, 0:nb], in_=p1[:, 0:nb], func=AF.Sigmoid), r=[tp1], w=[t_sg[si]])
                    S.op("dve", lambda e, p1=p1, si=si, nb=nb: e.tensor_tensor(out=sg[si][:, 0:nb], in0=p1[:, 0:nb], in1=sg[si][:, 0:nb], op=ALU.mult),
                         r=[tp1, t_sg[si]], w=[t_sg[si]])
                    S.op("dve", lambda e, p3=p3, si=si, nb=nb, f=f, hTc=hTc: e.tensor_tensor(out=hTc[:, f, 0:nb], in0=p3[:, 0:nb], in1=sg[si][:, 0:nb], op=ALU.mult),
                         r=[tp3, t_sg[si]], w=[t_hT[hi]])
                for tl in range(nb // 128):
                    gi = (b0 // 128) + tl
                    yi = gi - tiles[0]
                    for dh in range(2):
                        py, tpy = k.bank()
                        for f in range(4):
                            S.op("pe", lambda e, f=f, py=py, hTc=hTc, tl=tl, dh=dh, w2=w2: e.matmul(
                                py[:, :], lhsT=hTc[:, f, tl * 128:(tl + 1) * 128], rhs=w2[:, f, dh * 512:(dh + 1) * 512], start=(f == 0), stop=(f == 3)),
                                r=[t_hT[hi]] + t_w2, w=[tpy])
                        ya = yacc[:, yi, dh * 512:(dh + 1) * 512]
                        gs = k.gates[:, gi, e_:e_ + 1]
                        if e_ == 0:
                            S.op("dve", lambda e, py=py, ya=ya, gs=gs: e.tensor_scalar(out=ya, in0=py[:, :], scalar1=gs, scalar2=None, op0=ALU.mult),
                                 r=[tpy, k.t_gates[gi]], w=[t_y[yi]])
                        else:
                            S.op("dve", lambda e, py=py, ya=ya, gs=gs: e.scalar_tensor_tensor(out=ya, in0=py[:, :], scalar=gs, in1=ya, op0=ALU.mult, op1=ALU.add),
                                 r=[tpy, k.t_gates[gi], t_y[yi]], w=[t_y[yi]])
        for yi, gi in enumerate(tiles):
            yt = yacc[:, yi, :]
            r_ = 1 if gi < 2 else 0
            if l == 0 and yi == 0 and tiles[0] == 0:
                k.dump("ymoe_t0", yt, [128, D], F32, [t_y[yi]])
            S.dma(Hb, k.hmid_d[gi * 128:(gi + 1) * 128, :], r=[k.t_hmid[gi]], w=[t_H])
            S.op("dve", lambda e, yt=yt, r_=r_: e.tensor_tensor(out=yt, in0=yt, in1=k.bc[:, r_, :], op=ALU.mult), r=[t_y[yi], k.t_bc[r_]], w=[t_y[yi]])
            S.op("dve", lambda e, yt=yt: e.scalar_tensor_tensor(out=yt, in0=Hb, scalar=ALPHA, in1=yt, op0=ALU.mult, op1=ALU.add),
                 r=[t_H, t_y[yi]], w=[t_y[yi]])
            st_ap, mv_ap, rs_ap, nb_ap, t_st = k.st_slot()
            k.ln_stats(yt, t_y[yi], st_ap, mv_ap, rs_ap, nb_ap, t_st)
            S.op("act", lambda e, yt=yt, rs_ap=rs_ap, nb_ap=nb_ap: e.activation(out=Hb, in_=yt, func=AF.Identity, scale=rs_ap, bias=nb_ap),
                 r=[t_y[yi], t_st], w=[t_H])
            S.op("dve", lambda e: e.tensor_tensor(out=Hb, in0=Hb, in1=k.bc[:, 2, :], op=ALU.mult), r=[t_H, k.t_bc[2]], w=[t_H])
            S.op("pool", lambda e, yt=yt: e.tensor_tensor(out=yt, in0=Hb, in1=k.bc[:, 3, :], op=ALU.add), r=[t_H, k.t_bc[3]], w=[t_y[yi]])
            if l == 0:
                S.dma(k.hout0_d[gi * 128:(gi + 1) * 128, :], yt, r=[t_y[yi]], w=[k.t_hout0[gi]])
            else:
                S.dma(k.out_d[(gi - 2) * 128:(gi - 1) * 128, :], yt, r=[t_y[yi]], w=[k.t_out[gi - 2]])
    if l == 0:
        k.dump("hout0", k.hout0_d[:, :], [N, D], F32, k.t_hout0)


def mixer_odd(k):
    S = k.S
    scan_setup(k)
    sc = k.scn
    RB, HB, t_rb, t_hb = k.RB, k.HB, k.t_rb, k.t_hb
    col, t_col, row, t_row = sc["col"], k.t_col, sc["row"], k.t_row
    LATB = [(256, 512), (768, 512), (1280, 512), (1792, 512)]
    BC = sc["Am"][:, 0, :]
    BS = sc["Am"][:, 1, :]
    ci = col[:, 0:32].bitcast(I32)
    S.op("pool", lambda e: e.iota(ci[:, 0:1], pattern=[[0, 1]], base=0, channel_multiplier=1), w=[t_col])
    S.op("dve", lambda e: e.tensor_single_scalar(out=ci[:, 1:2], in_=ci[:, 0:1], scalar=63, op=ALU.bitwise_and), r=[t_col], w=[t_col])
    S.op("dve", lambda e: e.tensor_copy(out=col[:, 32:33], in_=ci[:, 1:2]), r=[t_col], w=[t_col])
    S.op("pool", lambda e: e.iota(ci[:, 2:18], pattern=[[128, 16]], base=0, channel_multiplier=1), r=[t_col], w=[t_col])
    S.op("dve", lambda e: e.tensor_copy(out=col[:, 40:56], in_=ci[:, 2:18]), r=[t_col], w=[t_col])
    qi = RB[:, 0, 0:128].bitcast(I32)
    qf = RB[:, 0, 128:256]
    ki = RB[:, 0, 256:384].bitcast(I32)
    kci = RB[:, 0, 384:512].bitcast(I32)
    tq = t_rb[0]
    S.op("pool", lambda e: e.iota(qi, pattern=[[1, 128]], base=0, channel_multiplier=0), w=[tq])
    S.op("dve", lambda e: e.tensor_single_scalar(out=qi, in_=qi, scalar=63, op=ALU.bitwise_and), r=[tq], w=[tq])
    S.op("dve", lambda e: e.tensor_copy(out=qf, in_=qi), r=[tq], w=[tq])
    S.op("dve", lambda e: e.tensor_scalar(out=ki, in0=qf, scalar1=col[:, 32:33], scalar2=None, op0=ALU.mult), r=[tq, t_col], w=[tq])
    S.op("dve", lambda e: e.tensor_single_scalar(out=ki, in_=ki, scalar=63, op=ALU.bitwise_and), r=[tq], w=[tq])
    S.op("dve", lambda e: e.tensor_scalar(out=kci, in0=ki, scalar1=16, scalar2=None, op0=ALU.add), r=[tq], w=[tq])
    S.op("dve", lambda e: e.tensor_single_scalar(out=kci, in_=kci, scalar=63, op=ALU.bitwise_and), r=[tq], w=[tq])
    S.op("act", lambda e: e.activation(out=BS, in_=ki, func=AF.Sin, scale=-2 * PI / 64, bias=k.cst[:, 4:5]), r=[tq, k.t_c], w=[k.t_Am[1]])
    S.op("act", lambda e: e.activation(out=BC, in_=kci, func=AF.Sin, scale=-2 * PI / 64, bias=k.cst[:, 4:5]), r=[tq, k.t_c], w=[k.t_Am[0]])
    for M_, tM in ((BC, k.t_Am[0]), (BS, k.t_Am[1])):
        S.op("pool", lambda e, M_=M_: e.memset(M_[0:64, 64:128], 0.0), r=[tM], w=[tM])
        S.op("pool", lambda e, M_=M_: e.memset(M_[64:128, 0:64], 0.0), r=[tM], w=[tM])
    if k.sub == "M1a":
        k.dump("BCS", sc["Am"][:, :, :], [128, 2, 128], BF16, k.t_Am)
        return
    wz, t_wz = k.load_w(k.w1_d[:, 1536:1792].rearrange("(k p) n -> p k n", p=128))
    zT = HB[:, 2:4, :]
    zc = HB[:, 4:6, :].rearrange("p a n -> p (a n)")[:, 0:4096].rearrange("p (i c) -> p i c", c=256)
    zs = HB[:, 6:8, :].rearrange("p a n -> p (a n)")[:, 0:4096].rearrange("p (i c) -> p i c", c=256)
    for m in range(2):
        for (t0, nt) in LATB:
            pb, tb = proj_block(k, wz, t_wz, m * 128, 128, t0, nt)
            S.op("act", lambda e, pb=pb, m=m, t0=t0, nt=nt: e.activation(out=zT[:, m, t0:t0 + nt], in_=pb[:, 0:nt], func=AF.Copy), r=[tb], w=[t_hb[2 + m]])
    if k.sub == "M1z":
        k.dump("zT", HB[:, 2:4, :], [128, 2, N], BF16, [t_hb[2], t_hb[3]])
        return
    for a in range(16):
        tok = (a + 2) * 128
        pb, tb = k.bank()
        for m in range(2):
            S.op("pe", lambda e, pb=pb, m=m, tok=tok: e.matmul(pb[:, m * 128:(m + 1) * 128], lhsT=zT[:, m, tok:tok + 128], rhs=BC[:, :], start=True, stop=True),
                 r=[t_hb[2 + m], k.t_Am[0]], w=[tb])
            S.op("pe", lambda e, pb=pb, m=m, tok=tok: e.matmul(pb[:, 256 + m * 128:256 + (m + 1) * 128], lhsT=zT[:, m, tok:tok + 128], rhs=BS[:, :], start=True, stop=True),
                 r=[t_hb[2 + m], k.t_Am[1]], w=[tb])
        S.op("act", lambda e, pb=pb, a=a: e.activation(out=zc[:, a, :], in_=pb[:, 0:256], func=AF.Copy), r=[tb], w=[t_hb[4], t_hb[5]])
        if k.sub != "M1d":
            S.op("act", lambda e, pb=pb, a=a: e.activation(out=zs[:, a, :], in_=pb[:, 256:512], func=AF.Copy, scale=-1.0), r=[tb], w=[t_hb[6], t_hb[7]])
        if k.sub in ("M1c", "M1d") and a == 0:
            k.dump("zc", HB[:, 4:6, :], [128, 2, N], BF16, [t_hb[4], t_hb[5]])
            return
    S.barrier()
    if k.sub == "M1b":
        k.dump("zc", HB[:, 4:6, :], [128, 2, N], BF16, [t_hb[4], t_hb[5]])
        return
    fidx = RB[:, 0, 0:2048]
    fi_i = RB[:, 1, 0:2048].bitcast(I32)
    S.op("pool", lambda e: e.iota(fi_i, pattern=[[1, 2048]], base=0, channel_multiplier=0), w=[t_rb[1]])
    S.op("dve", lambda e: e.tensor_copy(out=fidx, in_=fi_i), r=[t_rb[1]], w=[t_rb[0]])
    tabs = []
    for q in range(2):
        rowb = RB[:, 2 + q, :].bitcast(BF16)
        tabs.append((rowb[:, 0:2048], rowb[:, 2048:4096], t_rb[2 + q]))
    kib = [RB[:, 4, 0:2048].bitcast(I32), RB[:, 5, 0:2048].bitcast(I32)]
    banks = [(k.PS[i // 2][:, (i % 2) * 512:(i % 2 + 1) * 512], k.PT[i // 2][i % 2]) for i in range(8)]
    for a in range(16):
        Cb, Sb, t_tab = tabs[a % 2]
        S.op("dve", lambda e, a=a: e.tensor_scalar(out=kib[0], in0=fidx, scalar1=col[:, 40 + a:41 + a], scalar2=None, op0=ALU.mult),
             r=[t_rb[0], t_col], w=[t_rb[4]])
        S.op("dve", lambda e: e.tensor_single_scalar(out=kib[0], in_=kib[0], scalar=2047, op=ALU.bitwise_and), r=[t_rb[4]], w=[t_rb[4]])
        S.op("dve", lambda e: e.tensor_scalar(out=kib[1], in0=kib[0], scalar1=512, scalar2=None, op0=ALU.add), r=[t_rb[4]], w=[t_rb[5]])
        S.op("dve", lambda e: e.tensor_single_scalar(out=kib[1], in_=kib[1], scalar=2047, op=ALU.bitwise_and), r=[t_rb[5]], w=[t_rb[5]])
        S.op("act", lambda e, Sb=Sb: e.activation(out=Sb, in_=kib[0], func=AF.Sin, scale=-2 * PI / 2048, bias=k.cst[:, 4:5]), r=[t_rb[4], k.t_c], w=[t_tab])
        S.op("act", lambda e, Cb=Cb: e.activation(out=Cb, in_=kib[1], func=AF.Sin, scale=-2 * PI / 2048, bias=k.cst[:, 4:5]), r=[t_rb[5], k.t_c], w=[t_tab])
        for m in range(2):
            for fb in range(4):
                pbk, tbk = banks[m * 4 + fb]
                S.op("pe", lambda e, pbk=pbk, a=a, m=m, fb=fb, Cb=Cb: e.matmul(pbk[:, :], lhsT=zc[:, a, m * 128:(m + 1) * 128], rhs=Cb[:, fb * 512:(fb + 1) * 512],
                                                                            start=(a == 0), stop=False), r=[t_hb[4], t_hb[5], t_tab], w=[tbk])
                S.op("pe", lambda e, pbk=pbk, a=a, m=m, fb=fb, Sb=Sb: e.matmul(pbk[:, :], lhsT=zs[:, a, m * 128:(m + 1) * 128], rhs=Sb[:, fb * 512:(fb + 1) * 512],
                                                                            start=False, stop=(a == 15)), r=[t_hb[6], t_hb[7], t_tab], w=[tbk])
    fsc = 1.0 / math.sqrt(2048.0 * 64.0)
    for m in range(2):
        for fb in range(4):
            pbk, tbk = banks[m * 4 + fb]
            S.op("act", lambda e, pbk=pbk, m=m, fb=fb: e.activation(out=HB[:, m, fb * 512:(fb + 1) * 512], in_=pbk[:, :], func=AF.Copy, scale=fsc), r=[tbk], w=[t_hb[m]])
        S.dma(k.mix_d[8 + m, :, 256:2304], HB[:, m, 0:2048], r=[t_hb[m]], w=[k.t_mixd[8 + m]])
    k.dump("mixd_f", k.mix_d[8:10, :, :], [2, 128, N], BF16, k.t_mixd[8:10])
    S.barrier()
    if k.sub == "M1f":
        return

    gw = S.sbuf("gla_gw", [16, 2, 384], F32)
    t_gw = Trk()
    S.dma(gw[:, :, :], k.gw_d.rearrange("r k n -> k r n"), w=[t_gw])
    S.dma(row[0:2, 0:384], k.gb_d[:, :], w=[t_row])
    for h in range(4):
        row_to_col(k, row[0:2, h * 96:(h + 1) * 96], 2, 96, col[0:96, 2 * h:2 * h + 2], t_row, t_col)
    S.op("dve", lambda e: e.tensor_scalar(out=col[0:96, 0:8], in0=col[0:96, 0:8], scalar1=-1.0, scalar2=None, op0=ALU.mult), r=[t_col], w=[t_col])
    S.dma(row[0:1, 0:768], k.gn_d[:, :], r=[t_col], w=[t_row])
    for c in range(8):
        row_to_col(k, row[0:1, c * 96:(c + 1) * 96], 1, 96, col[0:96, 8 + c:9 + c], t_row, t_col)
    qrow, krow, A, B = RB[0:96, 0, :], RB[0:96, 1, :], RB[0:96, 2, :], RB[0:96, 3, :]
    oacc = [RB[0:96, 4, :], RB[0:96, 5, :]]
    qt, kt = HB[0:96, 0, :], HB[0:96, 1, :]
    gsil = [HB[0:96, 2, :], HB[0:96, 3, :]]
    vt = HB[:, 4:6, :].rearrange("p a n -> p (a n)")[:, 0:3456].rearrange("p (i c) -> p i c", c=192)
    for h in range(4):
        base = 384 * h
        wg, t_wg = k.load_w(k.w1_d[:, base:base + 384].rearrange("(k p) n -> p k n", p=128))
        wv, t_wvv = k.load_w(k.w1_d[:, 1824 + 192 * h:1824 + 192 * (h + 1)].rearrange("(k p) n -> p k n", p=128))
        wR, t_wR = k.load_w(k.w1_d[:, 1792:1824].rearrange("(k p) n -> p k n", p=128))

        def ev_q(pb, tb, t0, nt):
            S.op("act", lambda e: e.activation(out=qrow[:, t0:t0 + nt], in_=pb[0:96, 0:nt], func=AF.Copy, scale=96.0 ** -0.5), r=[tb], w=[t_rb[0]])
        k.proj_fm(wg, t_wg, 0, 96, ev_q)

        def ev_k(pb, tb, t0, nt):
            S.op("act", lambda e: e.activation(out=krow[:, t0:t0 + nt], in_=pb[0:96, 0:nt], func=AF.Copy), r=[tb], w=[t_rb[1]])
        k.proj_fm(wg, t_wg, 96, 96, ev_k)
        for a in range(2):
            def ev_g(pb, tb, t0, nt, a=a):
                S.op("act", lambda e: e.activation(out=B[:, t0:t0 + nt], in_=pb[0:96, 0:nt], func=AF.Sigmoid), r=[tb], w=[t_rb[3]])
                S.op("dve", lambda e: e.tensor_tensor(out=gsil[a][:, t0:t0 + nt], in0=pb[0:96, 0:nt], in1=B[:, t0:t0 + nt], op=ALU.mult),
                     r=[tb, t_rb[3]], w=[t_hb[2 + a]])
            k.proj_fm(wg, t_wg, 192 + 96 * a, 96, ev_g)

        def ev_v(pb, tb, i):
            S.op("act", lambda e: e.activation(out=vt[:, i, :], in_=pb[:, 0:192], func=AF.Copy), r=[tb], w=[t_hb[4], t_hb[5]])
        k.proj_tm(wv, t_wvv, 0, 192, ev_v)

        def make_logf(d, h=h, wR=wR, t_wR=t_wR):
            for (t0, nt) in TOKB:
                pr, tr = proj_block(k, wR, t_wR, d * 16, 16, t0, nt)
                S.op("act", lambda e, pr=pr, t0=t0, nt=nt: e.activation(out=B[0:16, t0:t0 + nt], in_=pr[0:16, 0:nt], func=AF.Copy), r=[tr], w=[t_rb[3]])
                pz, tz = k.bank()
                S.op("pe", lambda e, pz=pz, t0=t0, nt=nt: e.matmul(pz[0:96, 0:nt], lhsT=gw[0:16, d, h * 96:(h + 1) * 96], rhs=B[0:16, t0:t0 + nt],
                                                                   start=True, stop=True), r=[t_gw, t_rb[3]], w=[tz])
                S.op("act", lambda e, pz=pz, t0=t0, nt=nt: e.activation(out=A[:, t0:t0 + nt], in_=pz[0:96, 0:nt], func=AF.Exp, scale=-1.0,
                                                                        bias=col[0:96, 2 * h + d:2 * h + d + 1]), r=[tz, t_col], w=[t_rb[2]])
            S.op("act", lambda e: e.activation(out=A, in_=A, func=AF.Ln, bias=k.cst[0:96, 1:2]), r=[t_rb[2], k.t_c], w=[t_rb[2]])
            S.op("dve", lambda e: e.tensor_scalar(out=A, in0=A, scalar1=-1.0 / 16.0, scalar2=None, op0=ALU.mult), r=[t_rb[2]], w=[t_rb[2]])
        gated_scan(k, 96, 96, 2, qrow, t_rb[0], krow, t_rb[1], A, t_rb[2], B, t_rb[3], make_logf,
                   lambda i: vt[:, i, :], [t_hb[4], t_hb[5]], oacc, [t_rb[4], t_rb[5]], qt, t_hb[0], kt, t_hb[1])
        if h == 0:
            k.dump("gla_o0", RB[0:96, 4:6, :], [96, 2, N], F32, [t_rb[4], t_rb[5]])
        rms_gate_out(k, 96, 2, oacc, [t_rb[4], t_rb[5]], A, t_rb[2], B, t_rb[3], gsil, [t_hb[2], t_hb[3]],
                     [col[0:96, 8 + 2 * h:9 + 2 * h], col[0:96, 9 + 2 * h:10 + 2 * h]], t_col, [qt, kt], [t_hb[0], t_hb[1]], [2 * h, 2 * h + 1])
    k.dump("mixd1", k.mix_d[0:10, :, :], [10, 128, N], BF16, k.t_mixd[0:10])


_NC_CACHE = {}


def _f32(a):
    return np.ascontiguousarray(np.asarray(a, dtype=np.float32))


def kernel(x, c, ctx, c_ctx, w_ada, b_ada, ln_g, ln_b, w_in_even, attn_sink, hgrn_lb_logits, hgrn_norm,
           w_out_even, w_in_odd, gla_gate_w, gla_gate_b, gla_norm, w_out_odd, w_router, b_router,
           w_expert_gate, w_expert_up, w_expert_down):
    x = _f32(x); c = _f32(c); ctx = _f32(ctx); c_ctx = _f32(c_ctx)
    w0a = np.ascontiguousarray(_f32(w_in_even)[0][:, _cols0()])
    w1a = np.ascontiguousarray(_f32(w_in_odd)[0][:, _cols1()])
    shared = {
        "w_ada": _f32(w_ada), "b_ada": _f32(b_ada), "ln_g": _f32(ln_g), "ln_b": _f32(ln_b),
        "w0a": w0a, "attn_sink": _f32(attn_sink), "lb_logits": _f32(hgrn_lb_logits), "hgrn_norm": _f32(hgrn_norm),
        "w_out_even": _f32(w_out_even)[0], "w1a": w1a, "gla_gate_w": _f32(gla_gate_w)[0], "gla_gate_b": _f32(gla_gate_b)[0],
        "gla_norm": _f32(gla_norm), "w_out_odd": _f32(w_out_odd)[0], "w_router": _f32(w_router),
        "b_router": _f32(b_router)[None, :], "w_expert_gate": _f32(w_expert_gate), "w_expert_up": _f32(w_expert_up),
        "w_expert_down": _f32(w_expert_down),
    }
    nb = x.shape[0]
    in_maps = []
    for b in range(nb):
        m = dict(shared)
        m["x"] = np.ascontiguousarray(x[b])
        m["ctx"] = np.ascontiguousarray(ctx[b])
        m["cvec"] = np.ascontiguousarray(np.stack([c[b], c_ctx], 0))
        in_maps.append(m)
    if "nc" not in _NC_CACHE:
        _NC_CACHE["nc"] = build()
    res = run_bass_kernel_spmd(_NC_CACHE["nc"], in_maps, core_ids=list(range(nb)))
    return np.stack([np.asarray(r["out"], dtype=np.float32) for r in res.results], 0)
```

```python
import contextlib
import math
import numpy as np
import concourse.bass as bass
import concourse.mybir as mybir
from concourse.bass_utils import run_bass_kernel_spmd

F32 = mybir.dt.float32
BF16 = mybir.dt.bfloat16
I32 = mybir.dt.int32
AF = mybir.ActivationFunctionType
ALU = mybir.AluOpType
AX = mybir.AxisListType

ENG = ("pe", "act", "dve", "pool", "sp")


class Trk:
    __slots__ = ("w", "rs", "dsem", "dcnt", "name")

    def __init__(self, name=""):
        self.w = None
        self.rs = []
        self.dsem = None
        self.dcnt = 0
        self.name = name


class Sched:
    SEM_CHUNK = 20000

    def __init__(self, nc):
        self.nc = nc
        self.ops = {e: [] for e in ENG}
        self.waited = {e: {} for e in ENG}
        self.stack = contextlib.ExitStack()
        self.nsem = 0
        self.dma_ev = {}

    def sbuf(self, name, shape, dt):
        return self.stack.enter_context(self.nc.sbuf_tensor(name, list(shape), dt))

    def psum(self, name, shape, dt=F32):
        return self.stack.enter_context(self.nc.psum_tensor(name, list(shape), dt))

    def new_sem(self, name):
        self.nsem += 1
        return self.stack.enter_context(self.nc.semaphore(f"{name}_{self.nsem}"))

    def _filter(self, engine, deps):
        waits = []
        wd = self.waited[engine]
        for ev in deps:
            if ev[0] == "e":
                _, f, idx = ev
                if engine == "pe" and f == "pe":
                    continue
                if idx <= wd.get(f, -1):
                    continue
                wd[f] = idx
                self.ops[f][idx][2] = True
                waits.append(ev)
            else:
                _, sem, val = ev
                k = id(sem)
                if val <= wd.get(k, 0):
                    continue
                wd[k] = val
                waits.append(ev)
        return waits

    def _deps(self, engine, r, w):
        deps = []
        for t in r:
            if t.w is not None:
                deps.append(t.w)
        for t in w:
            if t.w is not None:
                deps.append(t.w)
            deps.extend(t.rs)
        return self._filter(engine, deps)

    def _post(self, ev, r, w):
        for t in w:
            t.w = ev
            t.rs = []
        for t in r:
            if t in w:
                continue
            if ev[0] == "e":
                t.rs = [x for x in t.rs if not (x[0] == "e" and x[1] == ev[1])]
            else:
                t.rs = [x for x in t.rs if not (x[0] == "d" and x[1] is ev[1])]
            t.rs.append(ev)

    def op(self, engine, fn, r=(), w=()):
        r = list(r)
        w = list(w)
        waits = self._deps(engine, r, w)
        idx = len(self.ops[engine])
        self.ops[engine].append([fn, waits, False, None])
        self._post(("e", engine, idx), r, w)

    def dma(self, out, in_, r=(), w=(), q="sp", **kw):
        r = list(r)
        w = list(w)
        waits = self._deps(q, r, w)
        t0 = w[0]
        if t0.dsem is None or t0.dcnt > 60000:
            t0.dsem = self.new_sem("d")
            t0.dcnt = 0
        t0.dcnt += 16
        ev = ("d", t0.dsem, t0.dcnt)
        self.dma_ev[id(t0.dsem)] = ev

        def fn(eng, out=out, in_=in_, kw=kw):
            return eng.dma_start(out=out, in_=in_, **kw)
        self.ops[q].append([fn, waits, False, t0.dsem])
        self._post(ev, r, w)

    def barrier(self):
        last = {}
        for f in ("pe", "act", "dve", "pool"):
            j = len(self.ops[f]) - 1
            while j >= 0 and (self.ops[f][j][0] is None or self.ops[f][j][3] is not None):
                j -= 1
            last[f] = j
        dm = list(self.dma_ev.values())
        for e in ENG:
            deps = [("e", f, last[f]) for f in ("pe", "act", "dve", "pool") if f != e and last[f] >= 0]
            deps += dm
            waits = self._filter(e, deps)
            self.ops[e].append([None, waits, False, None])

    def wait_all(self, engine, trks):
        waits = self._deps(engine, list(trks), [])
        self.ops[engine].append([None, waits, False, None])

    def emit(self):
        nc = self.nc
        cum = {}
        sems = {}
        for e in ENG:
            c = 0
            arr = []
            for rec in self.ops[e]:
                if rec[2]:
                    c += 1
                arr.append(c)
            cum[e] = arr
            sems[e] = [self.new_sem(f"s{e}") for _ in range(c // self.SEM_CHUNK + 1)]
        CH = self.SEM_CHUNK

        def semval(f, idx):
            c = cum[f][idx]
            ch = (c - 1) // CH
            return sems[f][ch], c - ch * CH

        def run(e, eng):
            for i, (fn, waits, sig, dsem) in enumerate(self.ops[e]):
                for ev in waits:
                    if ev[0] == "e":
                        s, v = semval(ev[1], ev[2])
                        eng.wait_ge(s, v)
                    else:
                        eng.wait_ge(ev[1], ev[2])
                if fn is None:
                    continue
                ins = fn(eng)
                if dsem is not None:
                    ins.then_inc(dsem, 16)
                elif sig:
                    s, v = semval(e, i)
                    ins.then_inc(s, 1)

        with nc.Block() as block:
            @block.tensor
            def _(eng):
                run("pe", eng)

            @block.scalar
            def _(eng):
                run("act", eng)

            @block.vector
            def _(eng):
                run("dve", eng)

            @block.gpsimd
            def _(eng):
                run("pool", eng)

            @block.sync
            def _(eng):
                run("sp", eng)

    def close(self):
        self.stack.close()


N = 2304
NT = 18
D = 1024
KC = 8
NLAT = 2048
ALPHA = 4.0 ** 0.25
EPS = 1e-5
TOKB = [(0, 512), (512, 512), (1024, 512), (1536, 512), (2048, 256)]
PI = math.pi

_SW = list(range(16, 32)) + list(range(0, 16)) + list(range(48, 64)) + list(range(32, 48))


def _cols0():
    cols = []
    for j in range(2):
        for blk in range(2):
            for hh in (4 * j + 2 * blk, 4 * j + 2 * blk + 1):
                cols += [hh * 64 + d for d in range(64)]
        for blk in range(2):
            for hh in (4 * j + 2 * blk, 4 * j + 2 * blk + 1):
                cols += [hh * 64 + d for d in _SW]
        cols += [512 + j * 64 + d for d in range(64)] * 2
        cols += [512 + j * 64 + d for d in _SW] * 2
    cols += list(range(640, 768))
    for h in range(4):
        cols += [768 + h * 128 + d for d in range(128)]
        cols += [1280 + h * 128 + d for d in range(128)]
        cols += [1792 + h * 128 + d for d in range(128)]
        cols += [2816 + h * 128 + d for d in range(128)]
    cols += list(range(2304, 2816))
    return cols


def _cols1():
    cols = []
    for h in range(4):
        cols += [h * 96 + d for d in range(96)]
        cols += [384 + h * 96 + d for d in range(96)]
        cols += [1568 + h * 192 + d for d in range(192)]
    cols += list(range(2336, 2592))
    cols += list(range(1536, 1568))
    cols += list(range(768, 1536))
    return cols


class K:
    pass


def build(dbg=(), stop=None):
    nc = bass.Bass("TRN2", target_bir_lowering=False)
    S = Sched(nc)
    k = K()
    k.nc = nc
    k.S = S
    k.dbg = set(dbg)
    k.sub = stop
    k.dbg_out = []

    def din(name, shape, dt=F32):
        return nc.dram_tensor(name, list(shape), dt, kind="ExternalInput").ap()

    x_d = din("x", [NLAT, D])
    ctx_d = din("ctx", [256, D])
    cv_d = din("cvec", [2, D])
    wada_d = din("w_ada", [2, D, 6 * D])
    bada_d = din("b_ada", [2, 6 * D])
    lng_d = din("ln_g", [2, 2, D])
    lnb_d = din("ln_b", [2, 2, D])
    w0_d = din("w0a", [D, 4224])
    sink_d = din("attn_sink", [1, 8])
    lbl_d = din("lb_logits", [2, 2, 512])
    hn_d = din("hgrn_norm", [1, 512])
    wo0_d = din("w_out_even", [D, D])
    w1_d = din("w1a", [D, 2592])
    gw_d = din("gla_gate_w", [2, 16, 384])
    gb_d = din("gla_gate_b", [2, 384])
    gn_d = din("gla_norm", [1, 768])
    wo1_d = din("w_out_odd", [D, D])
    wr_d = din("w_router", [D, 16])
    br_d = din("b_router", [1, 16])
    eg_d = din("w_expert_gate", [2, 16, D, 512])
    eu_d = din("w_expert_up", [2, 16, D, 512])
    ed_d = din("w_expert_down", [2, 16, 512, D])
    out_d = nc.dram_tensor("out", [NLAT, D], F32, kind="ExternalOutput").ap()
    hmid_d = nc.dram_tensor("hmid", [N, D], F32).ap()
    hout0_d = nc.dram_tensor("hout0", [N, D], F32).ap()
    mix_d = nc.dram_tensor("mixd", [10, 128, N], BF16).ap()
    t_hmid = [Trk() for _ in range(NT)]
    t_hout0 = [Trk() for _ in range(NT)]
    t_mixd = [Trk() for _ in range(10)]
    t_out = [Trk() for _ in range(16)]

    def dump(name, src_ap, shape, dt, r):
        if name not in k.dbg:
            return
        d = nc.dram_tensor("dbg_" + name, list(shape), dt, kind="ExternalOutput").ap()
        t = Trk()
        S.dma(d, src_ap, r=r, w=[t])
        k.dbg_out.append(t)

    PS = [S.psum(f"ps{i}", [128, 1024]) for i in range(4)]
    PT = [[Trk(), Trk()] for _ in range(4)]
    k.bank_i = 0
    k.pair_i = 0

    k.reserved = set()

    def bank():
        i = k.bank_i
        while i in k.reserved:
            i = (i + 1) % 8
        k.bank_i = (i + 1) % 8
        k.last_bank = i
        return PS[i // 2][:, (i % 2) * 512:(i % 2 + 1) * 512], PT[i // 2][i % 2]

    def pair():
        i = k.pair_i
        k.pair_i = (i + 1) % 4
        k.bank_i = (2 * i + 2) % 8
        return PS[i], PT[i]

    identF = S.sbuf("identF", [128, 128], F32); t_c = Trk()
    identB = S.sbuf("identB", [128, 128], BF16)
    onesF = S.sbuf("onesF", [128, 128], F32)
    onesB = S.sbuf("onesB", [128, 128], BF16)
    cst = S.sbuf("cst", [128, 8], F32)
    Mf = S.sbuf("Mf", [128, 128], BF16)
    Mb = S.sbuf("Mb", [128, 128], BF16)
    MP = S.sbuf("MP", [128, 512], BF16)
    MN = S.sbuf("MN", [128, 512], BF16)
    rmask = S.sbuf("rmask", [128, N], BF16)
    modT = S.sbuf("modT", [128, 2, 48, 2], F32); t_modT = Trk()
    mod_d = nc.dram_tensor("mod_d", [2, 2, 6 * D], F32).ap(); t_mod = Trk()
    bc = S.sbuf("bc", [128, 4, D], F32); t_bc = [Trk() for _ in range(4)]
    uT = S.sbuf("uT", [128, KC, N], BF16); t_uT = [Trk() for _ in range(NT)]
    wbuf = S.sbuf("wbuf", [128, 5, 4096], BF16); t_wb = [Trk() for _ in range(5)]
    RB = S.sbuf("RB", [128, 6, N], F32); t_rb = [Trk() for _ in range(6)]
    HB = S.sbuf("HB", [128, 8, N], BF16); t_hb = [Trk() for _ in range(8)]
    sm = S.sbuf("sm", [128, 640], F32)
    gates = S.sbuf("gates", [128, NT, 16], F32); t_gates = [Trk() for _ in range(NT)]
    wrt = S.sbuf("wrt", [128, KC, 16], F32); t_wr = Trk()
    brb = S.sbuf("brb", [128, 16], F32)

    S.op("pool", lambda e: e.memset(identF[:], 0.0), w=[t_c])
    S.op("pool", lambda e: e.affine_select(out=identF[:], in_=identF[:], pattern=[[-1, 128]], compare_op=ALU.not_equal,
                                           fill=1.0, base=0, channel_multiplier=1), r=[t_c], w=[t_c])
    S.op("pool", lambda e: e.tensor_copy(out=identB[:], in_=identF[:]), r=[t_c], w=[t_c])
    S.op("pool", lambda e: e.memset(onesF[:], 1.0), w=[t_c])
    S.op("pool", lambda e: e.memset(onesB[:], 1.0), w=[t_c])
    for j_, v_ in enumerate((EPS, 1.0, -PI, 0.0, PI)):
        S.op("pool", lambda e, j_=j_, v_=v_: e.memset(cst[:, j_:j_ + 1], v_), w=[t_c])
    S.op("pool", lambda e: e.memset(Mf[:], 1.0), w=[t_c])
    S.op("pool", lambda e: e.affine_select(out=Mf[:], in_=Mf[:], pattern=[[1, 128]], compare_op=ALU.is_ge, fill=0.0,
                                           base=0, channel_multiplier=-1), r=[t_c], w=[t_c])
    S.op("pool", lambda e: e.memset(Mf[0:64, 64:128], 0.0), r=[t_c], w=[t_c])
    S.op("pool", lambda e: e.memset(Mb[:], 1.0), w=[t_c])
    S.op("pool", lambda e: e.affine_select(out=Mb[:], in_=Mb[:], pattern=[[-1, 128]], compare_op=ALU.is_ge, fill=0.0,
                                           base=0, channel_multiplier=1), r=[t_c], w=[t_c])
    S.op("pool", lambda e: e.memset(Mb[64:128, 0:64], 0.0), r=[t_c], w=[t_c])
    S.op("pool", lambda e: e.memset(MP[:], 1.0), w=[t_c])
    S.op("pool", lambda e: e.affine_select(out=MP[:], in_=MP[:], pattern=[[0, 4], [-1, 128]], compare_op=ALU.is_ge,
                                           fill=0.0, base=0, channel_multiplier=1), r=[t_c], w=[t_c])
    S.op("pool", lambda e: e.memset(MN[:], 1.0), w=[t_c])
    S.op("pool", lambda e: e.affine_select(out=MN[:], in_=MN[:], pattern=[[0, 4], [1, 128]], compare_op=ALU.is_ge,
                                           fill=0.0, base=0, channel_multiplier=-1), r=[t_c], w=[t_c])
    S.op("pool", lambda e: e.memset(rmask[:], 1.0), w=[t_c])
    S.op("pool", lambda e: e.memset(rmask[:].rearrange("p (c l) -> p c l", l=64)[:, :, 0:1], 0.0), r=[t_c], w=[t_c])
    S.dma(wrt[:], wr_d.rearrange("(k p) n -> p k n", p=128), w=[t_wr])
    S.dma(brb[:], br_d[0:1, :].to_broadcast([128, 16]), w=[t_c])
    S.barrier()

    cs = RB[0:2, 4, 0:1024]
    sg_ = RB[0:2, 4, 1024:2048]
    csT = sm[:, 520:536].rearrange("p (k r) -> p k r", r=2)
    t_cs = Trk(); t_csT = Trk()
    k.t_bb = [[Trk(), Trk()], [Trk(), Trk()]]
    S.dma(cs, cv_d[:, :], w=[t_cs])
    S.op("act", lambda e: e.activation(out=sg_, in_=cs, func=AF.Sigmoid), r=[t_cs], w=[t_csT])
    S.op("dve", lambda e: e.tensor_tensor(out=cs, in0=cs, in1=sg_, op=ALU.mult), r=[t_cs, t_csT], w=[t_cs])
    pb, tb = bank()
    for kk in range(KC):
        S.op("pe", lambda e, kk=kk, pb=pb: e.transpose(out=pb[:, 2 * kk:2 * kk + 2], in_=cs[:, kk * 128:(kk + 1) * 128],
                                                       identity=identF[0:2, 0:2]), r=[t_cs, t_c], w=[tb])
    S.op("dve", lambda e, pb=pb: e.tensor_copy(out=csT, in_=pb[:, 0:16].rearrange("p (k r) -> p k r", r=2)), r=[tb], w=[t_csT])
    t_wa = [Trk(), Trk()]
    t_mb = [Trk(), Trk()]
    for l in range(2):
        pT, tT = bank()
        k.reserved = {k.last_bank}
        for j in range(12):
            slot = (l * 12 + j) % 2
            wa = RB[:, slot * 2:slot * 2 + 2, :].rearrange("p a n -> p (a n)")[:, 0:4096].rearrange("p (k n) -> p k n", n=512)
            mb = RB[0:2, 5, slot * 512:(slot + 1) * 512]
            S.dma(wa, wada_d[l, :, j * 512:(j + 1) * 512].rearrange("(k p) n -> p k n", p=128), w=[t_wa[slot]])
            for r_ in range(2):
                S.dma(RB[r_:r_ + 1, 5, 1024 + slot * 512:1024 + (slot + 1) * 512], bada_d[l:l + 1, j * 512:(j + 1) * 512],
                      w=[k.t_bb[slot][r_]])
            pb, tb = bank()
            for kk in range(KC):
                S.op("pe", lambda e, kk=kk, pb=pb, wa=wa: e.matmul(pb[0:2, :], lhsT=csT[:, kk, :], rhs=wa[:, kk, :],
                                                                   start=(kk == 0), stop=(kk == KC - 1)),
                     r=[t_csT, t_wa[slot]], w=[tb])
            bb = RB[0:2, 5, 1024 + slot * 512:1024 + (slot + 1) * 512]
            S.op("dve", lambda e, pb=pb, mb=mb, bb=bb: e.tensor_tensor(out=mb, in0=pb[0:2, :], in1=bb, op=ALU.add),
                 r=[tb] + k.t_bb[slot], w=[t_mb[slot]])
            if j in (2, 3, 8, 9):
                S.op("dve", lambda e, mb=mb: e.tensor_scalar(out=mb, in0=mb, scalar1=1.0, scalar2=None, op0=ALU.add),
                     r=[t_mb[slot]], w=[t_mb[slot]])
            S.dma(mod_d[l, :, j * 512:(j + 1) * 512], mb, r=[t_mb[slot]], w=[t_mod])
            for q_ in range(4):
                jj = j * 4 + q_
                S.op("pe", lambda e, jj=jj, q_=q_, mb=mb, pT=pT: e.transpose(out=pT[:, 2 * jj:2 * jj + 2], in_=mb[:, q_ * 128:(q_ + 1) * 128],
                                                                             identity=identF[0:2, 0:2]), r=[t_mb[slot], t_c], w=[tT])
        S.op("dve", lambda e, l=l, pT=pT: e.tensor_copy(out=modT[:, l, :, :], in_=pT[:, 0:96].rearrange("p (j r) -> p j r", r=2)),
             r=[tT], w=[t_modT])
        k.reserved = set()
        dump(f"mod{l}", mod_d[l, :, :], [2, 6 * D], F32, [t_mod])
    S.barrier()

    def bcast_rows(l, which):
        gi = 2 if which == "mix" else 5
        li = 0 if which == "mix" else 1
        S.dma(bc[:, 0, :], mod_d[l, 0:1, gi * D:(gi + 1) * D].to_broadcast([128, D]), r=[t_mod], w=[t_bc[0]])
        S.dma(bc[:, 1, :], mod_d[l, 1:2, gi * D:(gi + 1) * D].to_broadcast([128, D]), r=[t_mod], w=[t_bc[1]])
        S.dma(bc[:, 2, :], lng_d[l, li:li + 1, :].to_broadcast([128, D]), w=[t_bc[2]])
        S.dma(bc[:, 3, :], lnb_d[l, li:li + 1, :].to_broadcast([128, D]), w=[t_bc[3]])

    def ln_stats(src, t_src, st_ap, mv_ap, rs_ap, nb_ap, t_st):
        for j in range(2):
            S.op("dve", lambda e, j=j: e.bn_stats(out=st_ap[:, j, :], in_=src[:, j * 512:(j + 1) * 512]), r=[t_src], w=[t_st])
        S.op("dve", lambda e: e.bn_aggr(out=mv_ap, in_=st_ap), r=[t_st], w=[t_st])
        S.op("act", lambda e: e.activation(out=rs_ap, in_=mv_ap[:, 1:2], func=AF.Sqrt, bias=cst[:, 0:1]), r=[t_st, t_c], w=[t_st])
        S.op("dve", lambda e: e.reciprocal(out=rs_ap, in_=rs_ap), r=[t_st], w=[t_st])
        S.op("dve", lambda e: e.tensor_scalar(out=nb_ap, in0=mv_ap[:, 0:1], scalar1=rs_ap, scalar2=-1.0, op0=ALU.mult, op1=ALU.mult),
             r=[t_st], w=[t_st])

    k.st_i = 0

    def st_slot():
        i = k.st_i
        k.st_i = (i + 1) % 8
        base = i * 24
        return (sm[:, base:base + 12].rearrange("p (a b) -> p a b", b=6), sm[:, base + 12:base + 14],
                sm[:, base + 14:base + 15], sm[:, base + 15:base + 16], k.t_st[i])
    k.t_st = [Trk() for _ in range(8)]

    def ln_to_uT(l, src, t_src, xn, t_xn, i, which, router, uf=None, t_uf=None):
        st_ap, mv_ap, rs_ap, nb_ap, t_st = st_slot()
        ln_stats(src, t_src, st_ap, mv_ap, rs_ap, nb_ap, t_st)
        S.op("act", lambda e: e.activation(out=xn, in_=src, func=AF.Identity, scale=rs_ap, bias=nb_ap), r=[t_src, t_st], w=[t_xn])
        r_ = 1 if i < 2 else 0
        pp, tp = pair()
        for kk in range(KC):
            S.op("pe", lambda e, kk=kk: e.transpose(out=pp[:, kk * 128:(kk + 1) * 128], in_=xn[:, kk * 128:(kk + 1) * 128],
                                                    identity=identF[:]), r=[t_xn, t_c], w=[tp[kk // 4]])
        for kk in range(KC):
            sc_ap = modT[:, l, (which + 1) * 8 + kk, r_:r_ + 1]
            sh_ap = modT[:, l, which * 8 + kk, r_:r_ + 1]
            if router:
                o_ap = uf[:, kk, :]
                tw = t_uf
            else:
                o_ap = uT[:, kk, i * 128:(i + 1) * 128]
                tw = t_uT[i]
            if kk % 2 == 0:
                S.op("act", lambda e, kk=kk, o_ap=o_ap, sc_ap=sc_ap, sh_ap=sh_ap: e.activation(
                    out=o_ap, in_=pp[:, kk * 128:(kk + 1) * 128], func=AF.Identity, scale=sc_ap, bias=sh_ap),
                    r=[tp[kk // 4], t_modT], w=[tw])
            else:
                S.op("dve", lambda e, kk=kk, o_ap=o_ap, sc_ap=sc_ap, sh_ap=sh_ap: e.tensor_scalar(
                    out=o_ap, in0=pp[:, kk * 128:(kk + 1) * 128], scalar1=sc_ap, scalar2=sh_ap, op0=ALU.mult, op1=ALU.add),
                    r=[tp[kk // 4], t_modT], w=[tw])
        if router:
            S.op("pool", lambda e: e.tensor_copy(out=uT[:, :, i * 128:(i + 1) * 128], in_=uf[:, :, :]), r=[t_uf], w=[t_uT[i]])
            route(i, uf, t_uf)

    def route(i, uf, t_uf):
        pb, tb = bank()
        for kk in range(KC):
            S.op("pe", lambda e, kk=kk: e.matmul(pb[:, 0:16], lhsT=uf[:, kk, :], rhs=wrt[:, kk, :], start=(kk == 0),
                                                 stop=(kk == KC - 1)), r=[t_uf, t_wr], w=[tb])
        base = 192 + (i % 2) * 160
        t_r = k.t_route[i % 2]
        lg = sm[:, base:base + 16]
        pr = sm[:, base + 16:base + 32]
        sl = sm[:, base + 32:base + 48]
        s2 = sm[:, base + 48:base + 64]
        eq = sm[:, base + 64:base + 80]
        g4 = sm[:, base + 80:base + 84]
        g4b = sm[:, base + 84:base + 88]
        g4c = sm[:, base + 88:base + 92]
        sc1 = sm[:, base + 92:base + 93]
        sc2 = sm[:, base + 93:base + 94]
        eq2 = sm[:, base + 96:base + 112]
        BIG = 1.0e4

        def dv(fn, extra_r=()):
            S.op("dve", fn, r=[t_r] + list(extra_r), w=[t_r])
        S.op("dve", lambda e: e.tensor_copy(out=lg, in_=pb[:, 0:16]), r=[tb], w=[t_r])
        dv(lambda e: e.tensor_reduce(out=sc1, in_=lg, axis=AX.X, op=ALU.max))
        dv(lambda e: e.tensor_scalar(out=sc1, in0=sc1, scalar1=-1.0, scalar2=None, op0=ALU.mult))
        S.op("act", lambda e: e.activation(out=pr, in_=lg, func=AF.Exp, bias=sc1, scale=1.0), r=[t_r], w=[t_r])
        dv(lambda e: e.tensor_reduce(out=sc2, in_=pr, axis=AX.X, op=ALU.add))
        dv(lambda e: e.reciprocal(out=sc2, in_=sc2))
        dv(lambda e: e.tensor_scalar(out=pr, in0=pr, scalar1=sc2, scalar2=None, op0=ALU.mult))
        dv(lambda e: e.tensor_tensor(out=sl, in0=pr, in1=brb[:], op=ALU.add), extra_r=[t_c])
        sl3 = sl.rearrange("p (g e) -> p g e", e=4)
        s23 = s2.rearrange("p (g e) -> p g e", e=4)
        eq3 = eq.rearrange("p (g e) -> p g e", e=4)
        dv(lambda e: e.tensor_reduce(out=g4, in_=sl3, axis=AX.X, op=ALU.max))
        dv(lambda e: e.tensor_tensor(out=eq3, in0=sl3, in1=g4.unsqueeze(2).to_broadcast([128, 4, 4]), op=ALU.is_equal))
        dv(lambda e: e.scalar_tensor_tensor(out=s2, in0=eq, scalar=-BIG, in1=sl, op0=ALU.mult, op1=ALU.add))
        dv(lambda e: e.tensor_reduce(out=g4b, in_=s23, axis=AX.X, op=ALU.max))
        dv(lambda e: e.tensor_tensor(out=g4, in0=g4, in1=g4b, op=ALU.add))
        dv(lambda e: e.tensor_reduce(out=sc1, in_=g4, axis=AX.X, op=ALU.max))
        dv(lambda e: e.tensor_scalar(out=g4c, in0=g4, scalar1=sc1, scalar2=None, op0=ALU.is_equal))
        dv(lambda e: e.tensor_scalar(out=g4c, in0=g4c, scalar1=-1.0, scalar2=BIG, op0=ALU.add, op1=ALU.mult))
        dv(lambda e: e.tensor_tensor(out=s23, in0=sl3, in1=g4c.unsqueeze(2).to_broadcast([128, 4, 4]), op=ALU.add))
        dv(lambda e: e.tensor_reduce(out=sc1, in_=s2, axis=AX.X, op=ALU.max))
        dv(lambda e: e.tensor_scalar(out=eq, in0=s2, scalar1=sc1, scalar2=None, op0=ALU.is_equal))
        dv(lambda e: e.scalar_tensor_tensor(out=s2, in0=eq, scalar=-BIG, in1=s2, op0=ALU.mult, op1=ALU.add))
        dv(lambda e: e.tensor_reduce(out=sc1, in_=s2, axis=AX.X, op=ALU.max))
        dv(lambda e: e.tensor_scalar(out=eq2, in0=s2, scalar1=sc1, scalar2=None, op0=ALU.is_equal))
        dv(lambda e: e.tensor_tensor(out=eq, in0=eq, in1=eq2, op=ALU.add))
        dv(lambda e: e.tensor_tensor(out=eq, in0=eq, in1=pr, op=ALU.mult))
        dv(lambda e: e.tensor_reduce(out=sc2, in_=eq, axis=AX.X, op=ALU.add))
        dv(lambda e: e.reciprocal(out=sc2, in_=sc2))
        S.op("dve", lambda e: e.tensor_scalar(out=gates[:, i, :], in0=eq, scalar1=sc2, scalar2=None, op0=ALU.mult),
             r=[t_r], w=[t_gates[i]])
    k.t_route = [Trk(), Trk()]
    k.t_tile = [Trk() for _ in range(12)]

    k.wslot = 0

    def load_w(src_ap, nslots=1, parts=128):
        s0 = k.wslot
        if s0 + nslots > 5:
            s0 = 0
        k.wslot = (s0 + nslots) % 5
        a, b = src_ap.shape[1], src_ap.shape[2]
        dst = wbuf[0:parts, s0:s0 + nslots, :].rearrange("p s n -> p (s n)")[:, 0:a * b].rearrange("p (a b) -> p a b", b=b)
        trks = t_wb[s0:s0 + nslots]
        S.dma(dst, src_ap, w=trks, q="pool")
        return dst, trks

    def proj_fm(wv, t_w, c0, M, evac, toks=TOKB):
        for (t0, nt) in toks:
            pb, tb = bank()
            for kk in range(KC):
                S.op("pe", lambda e, kk=kk, pb=pb, t0=t0, nt=nt: e.matmul(pb[0:M, 0:nt], lhsT=wv[:, kk, c0:c0 + M],
                                                                          rhs=uT[:, kk, t0:t0 + nt], start=(kk == 0), stop=(kk == KC - 1)),
                     r=t_w + t_uT[t0 // 128:(t0 + nt) // 128], w=[tb])
            evac(pb, tb, t0, nt)

    def proj_tm(wv, t_w, c0, ncol, evac, tiles=range(NT)):
        for i in tiles:
            pb, tb = bank()
            for kk in range(KC):
                S.op("pe", lambda e, kk=kk, pb=pb, i=i: e.matmul(pb[:, 0:ncol], lhsT=uT[:, kk, i * 128:(i + 1) * 128],
                                                                 rhs=wv[:, kk, c0:c0 + ncol], start=(kk == 0), stop=(kk == KC - 1)),
                     r=t_w + [t_uT[i]], w=[tb])
            evac(pb, tb, i)

    k.proj_fm = proj_fm
    k.proj_tm = proj_tm
    k.load_w = load_w
    k.bank = bank
    k.pair = pair
    k.dump = dump
    k.ln_to_uT = ln_to_uT
    k.ln_stats = ln_stats
    k.st_slot = st_slot
    k.bcast_rows = bcast_rows
    for nm in ("x_d ctx_d w0_d sink_d lbl_d hn_d wo0_d w1_d gw_d gb_d gn_d wo1_d eg_d eu_d ed_d out_d hmid_d hout0_d mix_d "
               "t_hmid t_hout0 t_mixd t_out identF identB onesF onesB cst Mf Mb MP MN rmask modT t_modT mod_d t_mod bc t_bc uT t_uT "
               "wbuf t_wb RB t_rb HB t_hb sm gates t_gates t_c PS PT").split():
        setattr(k, nm, locals()[nm])

    for ph, l in [("B", 0), ("M", 0), ("D", 0), ("E", 0), ("B", 1), ("M", 1), ("D", 1), ("E", 1)]:
        if ph == "B":
            phase_B(k, l)
        elif ph == "M":
            (mixer_even if l == 0 else mixer_odd)(k)
        elif ph == "D":
            phase_D(k, l)
        else:
            phase_E(k, l)
        S.barrier()
        if stop is not None and stop[0:2] == f"{ph}{l}":
            break

    S.wait_all("sp", t_out + k.dbg_out)
    S.emit()
    S.close()
    return nc


def phase_B(k, l):
    S = k.S
    src_d = None
    for i in range(NT):
        slot = i % 2
        ht = k.RB[:, slot, 0:1024]
        xn = k.RB[:, slot, 1024:2048]
        t_ht = k.t_rb[slot]
        t_xn = k.t_rb[2 + slot]
        if l == 0:
            src = k.ctx_d[i * 128:(i + 1) * 128, :] if i < 2 else k.x_d[(i - 2) * 128:(i - 1) * 128, :]
            S.dma(ht, src, w=[t_ht])
        else:
            S.dma(ht, k.hout0_d[i * 128:(i + 1) * 128, :], r=[k.t_hout0[i]], w=[t_ht])
        xn = k.RB[:, 2 + slot, 0:1024]
        k.ln_to_uT(l, ht, t_ht, xn, t_xn, i, 0, False)
    k.dump(f"uT{l}", k.uT[:, :, :], [128, KC, N], BF16, k.t_uT)


def mixer_even(k):
    pass


def row_to_col(k, src, nr, n, dst, t_src, t_dst):
    S = k.S
    pb, tb = k.bank()
    S.op("pe", lambda e: e.transpose(out=pb[0:n, 0:nr], in_=src, identity=k.identF[0:nr, 0:nr]), r=[t_src, k.t_c], w=[tb])
    S.op("dve", lambda e: e.tensor_copy(out=dst, in_=pb[0:n, 0:nr]), r=[tb], w=[t_dst])


def proj_block(k, wv, t_w, c0, M, t0, nt):
    S = k.S
    pb, tb = k.bank()
    for kk in range(KC):
        S.op("pe", lambda e, kk=kk: e.matmul(pb[0:M, 0:nt], lhsT=wv[:, kk, c0:c0 + M], rhs=k.uT[:, kk, t0:t0 + nt],
                                             start=(kk == 0), stop=(kk == KC - 1)),
             r=t_w + k.t_uT[t0 // 128:(t0 + nt) // 128], w=[tb])
    return pb, tb


def gated_scan(k, dk, dvh, nh, q_ap, t_q, k_ap, t_k, A, t_A, B, t_B, make_logf, v_fn, t_v, o_acc, t_o, qts, t_qts, kts, t_kts):
    S = k.S
    sc = k.scn
    NC_ = N // 64
    dvt = dvh * nh
    B3 = B.rearrange("p (c l) -> p c l", l=64)
    for a in range(nh):
        S.op("pool", lambda e, a=a: e.memset(o_acc[a], 0.0), w=[t_o[a]])
    D_ = []
    for d in range(2):
        qt, kt, t_qt, t_kt = qts[d], kts[d], t_qts[d], t_kts[d]
        t_sc = k.t_scn[d]
        rr = sc["rr"][0:dk, d, :]
        gg = sc["gg"][0:dk, d, :]
        X1 = sc["X1"][0:dk, d, :]
        X2 = sc["X2"][0:dk, d, :]
        EG = sc["EG"][0:dk, d, :]
        make_logf(d)
        S.op("dve", lambda e: e.tensor_tensor_scan(out=B, data0=k.rmask[0:dk, :], data1=A, initial=0.0, op0=ALU.mult, op1=ALU.add),
             r=[t_A, k.t_c], w=[t_B])
        S.op("pool", lambda e, gg=gg: e.tensor_copy(out=gg, in_=B3[:, :, 63]), r=[t_B], w=[t_sc])
        if d == 1:
            S.op("pool", lambda e: e.tensor_tensor(out=B, in0=B, in1=A, op=ALU.subtract), r=[t_B, t_A], w=[t_B])
        S.op("pool", lambda e, rr=rr: e.tensor_copy(out=rr, in_=B3[:, :, 32]), r=[t_B], w=[t_sc])
        S.op("dve", lambda e, rr=rr: e.tensor_tensor(out=B3, in0=B3, in1=rr.unsqueeze(2).to_broadcast([dk, NC_, 64]), op=ALU.subtract),
             r=[t_B, t_sc], w=[t_B])
        sgn = 1.0 if d == 0 else -1.0
        S.op("act", lambda e, sgn=sgn: e.activation(out=A, in_=B, func=AF.Exp, scale=sgn), r=[t_B], w=[t_A])
        S.op("dve", lambda e, qt=qt: e.tensor_tensor(out=qt, in0=q_ap, in1=A, op=ALU.mult), r=[t_q, t_A], w=[t_qt])
        S.op("act", lambda e, sgn=sgn: e.activation(out=A, in_=B, func=AF.Exp, scale=-sgn), r=[t_B, t_qt], w=[t_A])
        S.op("dve", lambda e, kt=kt: e.tensor_tensor(out=kt, in0=k_ap, in1=A, op=ALU.mult), r=[t_k, t_A], w=[t_kt])
        S.op("act", lambda e, X1=X1, rr=rr: e.activation(out=X1, in_=rr, func=AF.Exp), r=[t_sc], w=[t_sc])
        S.op("act", lambda e, EG=EG, gg=gg: e.activation(out=EG, in_=gg, func=AF.Exp), r=[t_sc], w=[t_sc])
        S.op("dve", lambda e, X2=X2, gg=gg, rr=rr: e.tensor_tensor(out=X2, in0=gg, in1=rr, op=ALU.subtract), r=[t_sc], w=[t_sc])
        S.op("act", lambda e, X2=X2: e.activation(out=X2, in_=X2, func=AF.Exp), r=[t_sc], w=[t_sc])
        a_s, c_s = (X1, X2) if d == 0 else (X2, X1)
        if d == 0:
            tiles = [(i, (0, 1)) for i in range(NT)]
        else:
            tiles = [(i, (1, 0)) for i in (1, 0)] + [(i, (1, 0)) for i in range(NT - 1, 1, -1)]
        st = {"d": d, "qt": qt, "kt": kt, "t_qt": t_qt, "t_kt": t_kt, "t_sc": t_sc, "EG": EG, "a_s": a_s, "c_s": c_s,
              "M": k.Mf if d == 0 else k.Mb, "tiles": tiles, "s_i": 0, "sb_i": 0, "am_i": 0, "pend": None, "nchunk": 0, "ds_i": 0}
        S.op("pool", lambda e, d=d: e.memset(sc["Sst"][0:dk, d, 0, 0:dvt], 0.0), w=[k.t_Sst[d][0]])
        S.op("pool", lambda e, d=d: e.memset(sc["Sbf"][0:dk, d, 0, 0:dvt], 0.0), w=[k.t_Sbf[d][0]])
        D_.append(st)

    def stage1(st, i):
        d = st["d"]
        tk0 = i * 128
        vt = v_fn(i)
        am_i = st["am_i"]
        st["am_i"] = 1 - am_i
        Am = sc["Am"][:, d, am_i, :]
        ktok = sc["ktok"][:, d, am_i, 0:dk]
        t_Am = k.t_Am2[d][am_i]
        t_kk = k.t_ktok2[d][am_i]
        qt, kt = st["qt"], st["kt"]
        pA, tA = k.bank()
        S.op("pe", lambda e: e.matmul(pA[:, 0:128], lhsT=kt[:, tk0:tk0 + 128], rhs=qt[:, tk0:tk0 + 128], start=True, stop=True),
             r=[st["t_kt"], st["t_qt"]], w=[tA])
        M_ = st["M"]
        S.op("dve", lambda e: e.tensor_tensor(out=Am, in0=pA[:, 0:128], in1=M_[:], op=ALU.mult), r=[tA, k.t_c], w=[t_Am])
        pK, tK = k.bank()
        S.op("pe", lambda e: e.matmul(pK[:, 0:dk], lhsT=kt[:, tk0:tk0 + 128], rhs=k.identB[0:dk, 0:dk], start=True, stop=True),
             r=[st["t_kt"], k.t_c], w=[tK])
        S.op("act", lambda e: e.activation(out=ktok, in_=pK[:, 0:dk], func=AF.Copy), r=[tK], w=[t_kk])
        pI, tI = k.bank()
        for a in range(nh):
            S.op("pe", lambda e, a=a: e.matmul(pI[0:dvh, a * 128:(a + 1) * 128], lhsT=vt[:, a * dvh:(a + 1) * dvh], rhs=Am[:, :], start=True, stop=True),
                 r=t_v + [t_Am], w=[tI])
        pS = [None, None]
        c_s = st["c_s"]
        for hf in range(2):
            pS_, tS_ = k.bank()
            rows = slice(hf * 64, hf * 64 + 64)
            S.op("pe", lambda e, pS_=pS_, rows=rows: e.matmul(pS_[0:dk, 0:dvt], lhsT=ktok[rows, :], rhs=vt[rows, 0:dvt], start=True, stop=True),
                 r=[t_kk] + t_v, w=[tS_])
            ds_i = st["ds_i"]
            st["ds_i"] = (ds_i + 1) % 4
            dSs = sc["dSs"][0:dk, d, ds_i, 0:dvt]
            c = 2 * i + hf
            S.op("act", lambda e, pS_=pS_, dSs=dSs, c=c: e.activation(out=dSs, in_=pS_[0:dk, 0:dvt], func=AF.Identity, scale=c_s[:, c:c + 1]),
                 r=[tS_, st["t_sc"]], w=[k.t_dSs[d][ds_i]])
            pS[hf] = (dSs, k.t_dSs[d][ds_i])
        for a in range(nh):
            S.op("dve", lambda e, a=a: e.tensor_tensor(out=o_acc[a][:, tk0:tk0 + 128], in0=pI[0:dvh, a * 128:(a + 1) * 128],
                                                       in1=o_acc[a][:, tk0:tk0 + 128], op=ALU.add), r=[tI, t_o[a]], w=[t_o[a]])
        st["pend"] = (i, pS)

    def stage2_chunk(st, i, hf, pS, po, tpo, last):
        d = st["d"]
        c = 2 * i + hf
        tok0 = c * 64
        qt = st["qt"]
        sb_i = st["sb_i"]
        Sb = sc["Sbf"][0:dk, d, sb_i, 0:dvt]
        for a in range(nh):
            S.op("pe", lambda e, a=a: e.matmul(po[0:dvh, a * 128 + hf * 64:a * 128 + hf * 64 + 64], lhsT=Sb[:, a * dvh:(a + 1) * dvh],
                                               rhs=qt[:, tok0:tok0 + 64], start=True, stop=True), r=[k.t_Sbf[d][sb_i], st["t_qt"]], w=[tpo])
        if last:
            return
        s_i = st["s_i"]
        Sc = sc["Sst"][0:dk, d, s_i, 0:dvt]
        Sn = sc["Sst"][0:dk, d, 1 - s_i, 0:dvt]
        EG, a_s = st["EG"], st["a_s"]
        dSs, t_dS = pS[hf]
        S.op("dve", lambda e: e.scalar_tensor_tensor(out=Sn, in0=Sc, scalar=EG[:, c:c + 1], in1=dSs, op0=ALU.mult, op1=ALU.add),
             r=[k.t_Sst[d][s_i], t_dS, st["t_sc"]], w=[k.t_Sst[d][1 - s_i]])
        st["s_i"] = 1 - s_i
        n_ = st["nchunk"] + 1
        ti, (h0, h1) = st["tiles"][n_ // 2]
        cn = 2 * ti + (h0 if n_ % 2 == 0 else h1)
        Sbn = sc["Sbf"][0:dk, d, 1 - sb_i, 0:dvt]
        S.op("act", lambda e: e.activation(out=Sbn, in_=Sn, func=AF.Identity, scale=a_s[:, cn:cn + 1]),
             r=[k.t_Sst[d][1 - s_i], st["t_sc"]], w=[k.t_Sbf[d][1 - sb_i]])
        st["sb_i"] = 1 - sb_i

    nT = NT
    for step in range(nT + 1):
        if step < nT:
            for st in D_:
                stage1(st, st["tiles"][step][0])
        if step >= 1:
            pend = []
            for st in D_:
                i, (h0, h1) = st["tiles"][step - 1]
                po, tpo = k.bank()
                pend.append((st, i, (h0, h1), po, tpo))
            for which in range(2):
                for (st, i, hfs, po, tpo) in pend:
                    pS = st["pS_prev"]
                    last = (st["nchunk"] == 2 * nT - 1)
                    stage2_chunk(st, i, hfs[which], pS, po, tpo, last)
                    st["nchunk"] += 1
            for (st, i, hfs, po, tpo) in pend:
                tk0 = i * 128
                for a in range(nh):
                    S.op("dve", lambda e, a=a, po=po, tk0=tk0: e.tensor_tensor(out=o_acc[a][:, tk0:tk0 + 128], in0=po[0:dvh, a * 128:(a + 1) * 128],
                                                                               in1=o_acc[a][:, tk0:tk0 + 128], op=ALU.add), r=[tpo, t_o[a]], w=[t_o[a]])
        for st in D_:
            if st["pend"] is not None:
                st["pS_prev"] = st["pend"][1]


def rms_gate_out(k, dvh, nh, o_acc, t_o, A, t_A, B, t_B, gsil, t_gs, gn_cols, t_gn, mixrow, t_mix, chunk_ids):
    S = k.S
    dv = dvh * nh
    for (t0, nt) in TOKB:
        pb, tb = k.bank()
        for a in range(nh):
            S.op("act", lambda e, a=a, t0=t0, nt=nt: e.activation(out=A[0:dvh, a * 512:a * 512 + nt], in_=o_acc[a][:, t0:t0 + nt], func=AF.Square),
                 r=[t_o[a]], w=[t_A])
        for a in range(nh):
            S.op("pe", lambda e, a=a, pb=pb, nt=nt: e.matmul(pb[0:dvh, 0:nt], lhsT=k.onesF[0:dvh, 0:dvh], rhs=A[0:dvh, a * 512:a * 512 + nt],
                                                             start=(a == 0), stop=(a == nh - 1)), r=[t_A, k.t_c], w=[tb])
        S.op("act", lambda e, pb=pb, nt=nt: e.activation(out=B[0:dvh, 0:nt], in_=pb[0:dvh, 0:nt], func=AF.Sqrt, scale=1.0 / dv, bias=k.cst[0:dvh, 0:1]),
             r=[tb, k.t_c], w=[t_B])
        S.op("dve", lambda e, nt=nt: e.reciprocal(out=B[0:dvh, 0:nt], in_=B[0:dvh, 0:nt]), r=[t_B], w=[t_B])
        for a in range(nh):
            S.op("dve", lambda e, a=a, t0=t0, nt=nt: e.tensor_tensor(out=B[0:dvh, 512 + a * 512:512 + a * 512 + nt], in0=o_acc[a][:, t0:t0 + nt],
                                                                     in1=B[0:dvh, 0:nt], op=ALU.mult), r=[t_o[a], t_B], w=[t_B])
            S.op("dve", lambda e, a=a, t0=t0, nt=nt: e.scalar_tensor_tensor(out=mixrow[a][0:dvh, t0:t0 + nt], in0=B[0:dvh, 512 + a * 512:512 + a * 512 + nt],
                                                                            scalar=gn_cols[a], in1=gsil[a][0:dvh, t0:t0 + nt], op0=ALU.mult, op1=ALU.mult),
                 r=[t_B, t_gn, t_gs[a]], w=[t_mix[a]])
    for a in range(nh):
        S.dma(k.mix_d[chunk_ids[a], 0:dvh, :], mixrow[a][0:dvh, :], r=[t_mix[a]], w=[k.t_mixd[chunk_ids[a]]])


def scan_setup(k):
    S = k.S
    if hasattr(k, "scn"):
        return
    sc = {}
    for nm in ("rr", "gg", "X1", "X2", "EG"):
        sc[nm] = S.sbuf("sc_" + nm, [128, 2, 36], F32)
    sc["Sst"] = S.sbuf("sc_Sst", [128, 2, 2, 192], F32)
    sc["dSs"] = S.sbuf("sc_dSs", [128, 2, 4, 192], BF16)
    sc["Sbf"] = S.sbuf("sc_Sbf", [128, 2, 2, 192], BF16)
    sc["Am"] = S.sbuf("sc_Am", [128, 2, 2, 128], BF16)
    sc["ktok"] = S.sbuf("sc_ktok", [128, 2, 2, 128], BF16)
    sc["col"] = S.sbuf("sc_col", [128, 64], F32)
    sc["row"] = k.RB[0:8, 5, 0:768]
    k.scn = sc
    k.t_scn = [Trk(), Trk()]
    k.t_Sst = [[Trk(), Trk()], [Trk(), Trk()]]
    k.t_dSs = [[Trk() for _ in range(4)], [Trk() for _ in range(4)]]
    k.t_Sbf = [[Trk(), Trk()], [Trk(), Trk()]]
    k.t_Am2 = [[Trk(), Trk()], [Trk(), Trk()]]
    k.t_ktok2 = [[Trk(), Trk()], [Trk(), Trk()]]
    k.t_Am = [k.t_Am2[0][0], k.t_Am2[0][1]]
    k.t_col = Trk()
    k.t_row = k.t_rb[5]


ATOK = [(0, 256), (256, 512), (768, 512), (1280, 512), (1792, 512)]


def mixer_even(k):
    S = k.S
    scan_setup(k)
    sc = k.scn
    RB, HB, t_rb, t_hb = k.RB, k.HB, k.t_rb, k.t_hb
    Ct = RB[:, 4, 0:2048]
    St = RB[:, 5, 0:2048]
    col = sc["col"]
    t_col = k.t_col
    ci = col[:, 0:8].bitcast(I32)
    S.op("pool", lambda e: e.iota(ci[:, 0:1], pattern=[[0, 1]], base=0, channel_multiplier=1), w=[t_col])
    S.op("dve", lambda e: e.tensor_single_scalar(out=ci[:, 1:2], in_=ci[:, 0:1], scalar=15, op=ALU.bitwise_and), r=[t_col], w=[t_col])
    S.op("dve", lambda e: e.tensor_scalar(out=ci[:, 2:3], in0=ci[:, 0:1], scalar1=5, scalar2=1, op0=ALU.logical_shift_right, op1=ALU.bitwise_and),
         r=[t_col], w=[t_col])
    S.op("dve", lambda e: e.tensor_scalar(out=ci[:, 3:4], in0=ci[:, 0:1], scalar1=4, scalar2=1, op0=ALU.logical_shift_right, op1=ALU.bitwise_and),
         r=[t_col], w=[t_col])
    S.op("dve", lambda e: e.tensor_copy(out=col[:, 8:11], in_=ci[:, 1:4]), r=[t_col], w=[t_col])
    S.op("act", lambda e: e.activation(out=col[:, 11:12], in_=col[:, 8:9], func=AF.Exp, scale=-math.log(10000.0) / 16.0), r=[t_col], w=[t_col])
    S.op("dve", lambda e: e.tensor_tensor(out=col[:, 13:14], in0=col[:, 11:12], in1=col[:, 9:10], op=ALU.mult), r=[t_col], w=[t_col])
    S.op("dve", lambda e: e.tensor_tensor(out=col[:, 12:13], in0=col[:, 11:12], in1=col[:, 13:14], op=ALU.subtract), r=[t_col], w=[t_col])
    S.op("dve", lambda e: e.tensor_scalar(out=col[:, 14:15], in0=col[:, 10:11], scalar1=2.0, scalar2=-1.0, op0=ALU.mult, op1=ALU.add),
         r=[t_col], w=[t_col])
    ri = RB[:, 0, 0:2048].bitcast(I32)
    qi = RB[:, 1, 0:2048].bitcast(I32)
    S.op("pool", lambda e: e.iota(ri, pattern=[[1, 32], [0, 64]], base=0, channel_multiplier=0), w=[t_rb[0]])
    S.op("pool", lambda e: e.iota(qi, pattern=[[0, 32], [1, 64]], base=0, channel_multiplier=0), w=[t_rb[1]])
    rf = RB[:, 2, 0:2048]
    qf = RB[:, 3, 0:2048]
    S.op("dve", lambda e: e.tensor_copy(out=rf, in_=ri), r=[t_rb[0]], w=[t_rb[2]])
    S.op("dve", lambda e: e.tensor_copy(out=qf, in_=qi), r=[t_rb[1]], w=[t_rb[3]])
    ang = RB[:, 0, 0:2048]
    S.op("dve", lambda e: e.tensor_scalar(out=ang, in0=rf, scalar1=col[:, 12:13], scalar2=None, op0=ALU.mult), r=[t_rb[2], t_col], w=[t_rb[0]])
    S.op("dve", lambda e: e.scalar_tensor_tensor(out=ang, in0=qf, scalar=col[:, 13:14], in1=ang, op0=ALU.mult, op1=ALU.add),
         r=[t_rb[3], t_rb[0], t_col], w=[t_rb[0]])
    def range_reduce(dst, t_dst, add, tmpi, t_tmpi, tmpf_, t_tmpf):
        S.op("dve", lambda e: e.tensor_scalar(out=tmpi, in0=ang, scalar1=add, scalar2=1.0 / (2 * PI), op0=ALU.add, op1=ALU.mult),
             r=[t_rb[0]], w=[t_tmpi])
        S.op("dve", lambda e: e.tensor_copy(out=tmpf_, in_=tmpi), r=[t_tmpi], w=[t_tmpf])
        S.op("dve", lambda e: e.scalar_tensor_tensor(out=dst, in0=tmpf_, scalar=-2 * PI, in1=ang, op0=ALU.mult, op1=ALU.add),
             r=[t_tmpf, t_rb[0]], w=[t_dst])
        if add != 0.0:
            S.op("dve", lambda e: e.tensor_scalar(out=dst, in0=dst, scalar1=add, scalar2=None, op0=ALU.add), r=[t_dst], w=[t_dst])
        S.op("dve", lambda e: e.tensor_scalar(out=tmpf_, in0=dst, scalar1=PI, scalar2=-2 * PI, op0=ALU.is_gt, op1=ALU.mult),
             r=[t_dst], w=[t_tmpf])
        S.op("dve", lambda e: e.tensor_tensor(out=dst, in0=dst, in1=tmpf_, op=ALU.add), r=[t_dst, t_tmpf], w=[t_dst])
        S.op("dve", lambda e: e.tensor_scalar(out=tmpf_, in0=dst, scalar1=-PI, scalar2=2 * PI, op0=ALU.is_lt, op1=ALU.mult),
             r=[t_dst], w=[t_tmpf])
        S.op("dve", lambda e: e.tensor_tensor(out=dst, in0=dst, in1=tmpf_, op=ALU.add), r=[t_dst, t_tmpf], w=[t_dst])
        S.op("dve", lambda e: e.tensor_scalar(out=dst, in0=dst, scalar1=PI, scalar2=-PI, op0=ALU.min, op1=ALU.max), r=[t_dst], w=[t_dst])
    m1 = RB[:, 1, 0:2048]
    tmpi = RB[:, 2, 0:2048].bitcast(I32)
    tmpf_ = RB[:, 3, 0:2048]
    range_reduce(m1, t_rb[1], 0.0, tmpi, t_rb[2], tmpf_, t_rb[3])
    S.op("act", lambda e: e.activation(out=St, in_=m1, func=AF.Sin, scale=col[:, 14:15]), r=[t_rb[1], t_col], w=[t_rb[5]])
    range_reduce(m1, t_rb[1], PI / 2, tmpi, t_rb[2], tmpf_, t_rb[3])
    S.op("act", lambda e: e.activation(out=Ct, in_=m1, func=AF.Sin), r=[t_rb[1]], w=[t_rb[4]])
    k.dump("ropeC", Ct, [128, 2048], F32, [t_rb[4]])
    k.dump("ropeS", St, [128, 2048], F32, [t_rb[5]])
    if k.sub == "M0a":
        return
    S.dma(col[:, 16:24], k.sink_d[0:1, :].to_broadcast([128, 8]), w=[t_col])
    S.op("act", lambda e: e.activation(out=col[:, 16:24], in_=col[:, 16:24], func=AF.Exp), r=[t_col], w=[t_col])

    qT = HB[:, 0:2, :]
    kAB = [HB[:, 2, :], HB[:, 3, :]]
    vdup = HB[:, 4, :].rearrange("p (i c) -> p i c", c=128)
    mixA = [HB[:, 5, :], HB[:, 6, :]]
    et = [HB[:, 7, ei * 512:(ei + 1) * 512] for ei in range(4)]
    S.op("pool", lambda e: e.memset(kAB[0][64:128, :], 0.0), w=[t_hb[2]])
    S.op("pool", lambda e: e.memset(kAB[1][0:64, :], 0.0), w=[t_hb[3]])
    t_et = [Trk() for _ in range(4)]
    k.et_i = 0
    tmpf = [RB[:, 0, 0:512], RB[:, 0, 512:1024], RB[:, 1, 0:512], RB[:, 1, 512:1024]]
    t_tmp = [Trk() for _ in range(4)]
    dn = RB[:, 2, 0:512]
    t_dn = t_rb[2]
    wv_v, t_wv = k.load_w(k.w0_d[:, 1536:1664].rearrange("(k p) n -> p k n", p=128))
    for j in range(2):
        base = j * 768
        wq, t_wq = k.load_w(k.w0_d[:, base:base + 512].rearrange("(k p) n -> p k n", p=128))
        wk, t_wk = k.load_w(k.w0_d[:, base + 512:base + 768].rearrange("(k p) n -> p k n", p=128))
        tmp_i = 0
        for (t0, nt) in ATOK:
            for blk in range(3):
                if blk < 2:
                    pq, tq = proj_block(k, wq, t_wq, blk * 128, 128, t0, nt)
                    dst = qT[:, blk, t0:t0 + nt]
                    tdst = t_hb[blk]
                else:
                    pq, tq = proj_block(k, wk, t_wk, 0, 128, t0, nt)
                    dst = None
                if t0 == 0:
                    if blk < 2:
                        S.op("act", lambda e, pq=pq, dst=dst, nt=nt: e.activation(out=dst, in_=pq[:, 0:nt], func=AF.Copy), r=[tq], w=[tdst])
                    else:
                        S.op("act", lambda e, pq=pq, nt=nt, t0=t0: e.activation(out=kAB[0][0:64, t0:t0 + nt], in_=pq[0:64, 0:nt], func=AF.Copy), r=[tq], w=[t_hb[2]])
                        S.op("act", lambda e, pq=pq, nt=nt, t0=t0: e.activation(out=kAB[1][64:128, t0:t0 + nt], in_=pq[64:128, 0:nt], func=AF.Copy), r=[tq], w=[t_hb[3]])
                    continue
                if blk < 2:
                    ps_, ts_ = proj_block(k, wq, t_wq, 256 + blk * 128, 128, t0, nt)
                else:
                    ps_, ts_ = proj_block(k, wk, t_wk, 128, 128, t0, nt)
                l0 = t0 - 256
                ta, tb_ = tmp_i % 4, (tmp_i + 1) % 4
                tmp_i += 2
                S.op("dve", lambda e, pq=pq, ta=ta, l0=l0, nt=nt: e.tensor_tensor(out=tmpf[ta][:, 0:nt], in0=pq[:, 0:nt], in1=Ct[:, l0:l0 + nt], op=ALU.mult),
                     r=[tq, t_rb[4]], w=[t_tmp[ta]])
                S.op("dve", lambda e, ps_=ps_, tb_=tb_, l0=l0, nt=nt: e.tensor_tensor(out=tmpf[tb_][:, 0:nt], in0=ps_[:, 0:nt], in1=St[:, l0:l0 + nt], op=ALU.mult),
                     r=[ts_, t_rb[5]], w=[t_tmp[tb_]])
                if blk < 2:
                    S.op("pool", lambda e, dst=dst, ta=ta, tb_=tb_, nt=nt: e.tensor_tensor(out=dst, in0=tmpf[ta][:, 0:nt], in1=tmpf[tb_][:, 0:nt], op=ALU.add),
                         r=[t_tmp[ta], t_tmp[tb_]], w=[tdst])
                else:
                    S.op("pool", lambda e, ta=ta, tb_=tb_, nt=nt, t0=t0: e.tensor_tensor(out=kAB[0][0:64, t0:t0 + nt], in0=tmpf[ta][0:64, 0:nt], in1=tmpf[tb_][0:64, 0:nt], op=ALU.add),
                         r=[t_tmp[ta], t_tmp[tb_]], w=[t_hb[2]])
                    S.op("pool", lambda e, ta=ta, tb_=tb_, nt=nt, t0=t0: e.tensor_tensor(out=kAB[1][64:128, t0:t0 + nt], in0=tmpf[ta][64:128, 0:nt], in1=tmpf[tb_][64:128, 0:nt], op=ALU.add),
                         r=[t_tmp[ta], t_tmp[tb_]], w=[t_hb[3]])

        if k.sub == "M0p":
            k.dump("qT0", qT, [128, 2, N], BF16, [t_hb[0], t_hb[1]])
            return

        def ev_v(pb, tb, i, j=j):
            S.op("act", lambda e: e.activation(out=vdup[:, i, 0:64], in_=pb[:, j * 64:(j + 1) * 64], func=AF.Copy), r=[tb], w=[t_hb[4]])
            S.op("dve", lambda e: e.tensor_copy(out=vdup[:, i, 64:128], in_=pb[:, j * 64:(j + 1) * 64]), r=[tb], w=[t_hb[4]])
        k.proj_tm(wv_v, t_wv, 0, 128, ev_v)
        if j == 0:
            k.dump("qT0", qT, [128, 2, N], BF16, [t_hb[0], t_hb[1]])
            k.dump("kT0", HB[:, 2:4, :], [128, 2, N], BF16, [t_hb[2], t_hb[3]])
        if k.sub == "M0v":
            return
        for qb in range(NT):
            q0 = qb * 128
            if (k.sub == "M0q1" and qb == 1) or (k.sub == "M0q3" and qb == 3):
                k.dump("mixA", HB[:, 5:7, :], [128, 2, N], BF16, [t_hb[5], t_hb[6]])
                return
            if qb < 2:
                chunks = [(0, None), (1, None)]
            else:
                n_ = qb - 2
                chunks = [(0, None), (1, None)]
                if n_ > 0:
                    chunks.append((qb - 1, k.MP))
                chunks.append((qb, None))
                if n_ < 15:
                    chunks.append((qb + 1, k.MN))
            po, tpo = k.bank()
            pd, tpd = k.bank()
            for ci_, (kc, msk) in enumerate(chunks):
                pss, tss = k.bank()
                for hh in range(4):
                    blk, half = hh // 2, hh % 2
                    rows = slice(half * 64, half * 64 + 64)
                    S.op("pe", lambda e, pss=pss, hh=hh, blk=blk, half=half, kc=kc, q0=q0: e.matmul(
                        pss[:, hh * 128:(hh + 1) * 128], lhsT=kAB[half][:, kc * 128:(kc + 1) * 128], rhs=qT[:, blk, q0:q0 + 128],
                        start=True, stop=True), r=[t_hb[2 + half], t_hb[blk]], w=[tss])
                ei = k.et_i
                k.et_i = (ei + 1) % 4
                S.op("act", lambda e, pss=pss, ei=ei: e.activation(out=et[ei], in_=pss[:, :], func=AF.Exp, scale=0.125), r=[tss], w=[t_et[ei]])
                if msk is not None:
                    S.op("dve", lambda e, ei=ei, msk=msk: e.tensor_tensor(out=et[ei], in0=et[ei], in1=msk[:], op=ALU.mult),
                         r=[t_et[ei], k.t_c], w=[t_et[ei]])
                if k.sub == "M0qa":
                    k.dump("et", HB[:, 7, :], [128, N], BF16, t_et)
                    return
                nch = len(chunks)
                S.op("pe", lambda e, ei=ei, kc=kc, ci_=ci_, po=po, nch=nch: e.matmul(
                    po[:, :], lhsT=vdup[:, kc, :], rhs=et[ei][:, :], start=(ci_ == 0), stop=(ci_ == nch - 1)),
                    r=[t_hb[4], t_et[ei]], w=[tpo])
                S.op("pe", lambda e, ei=ei, ci_=ci_, pd=pd, nch=nch: e.matmul(
                    pd[:, :], lhsT=k.onesB[:], rhs=et[ei][:, :], start=(ci_ == 0), stop=(ci_ == nch - 1)),
                    r=[k.t_c, t_et[ei]], w=[tpd])
            if k.sub == "M0qb":
                S.op("dve", lambda e, po=po: e.tensor_copy(out=RB[:, 0, 0:512], in_=po[:, :]), r=[tpo], w=[t_rb[0]])
                S.op("dve", lambda e, pd=pd: e.tensor_copy(out=RB[:, 0, 512:1024], in_=pd[:, :]), r=[tpd], w=[t_rb[0]])
                k.dump("popd", RB[:, 0, 0:1024], [128, 1024], F32, [t_rb[0]])
                return
            for hh in range(4):
                S.op("dve", lambda e, hh=hh, pd=pd, j=j: e.tensor_scalar(out=dn[:, hh * 128:(hh + 1) * 128], in0=pd[:, hh * 128:(hh + 1) * 128],
                                                                    scalar1=col[:, 16 + 4 * j + hh:17 + 4 * j + hh], scalar2=None, op0=ALU.add),
                     r=[tpd, t_col], w=[t_dn])
            S.op("dve", lambda e: e.reciprocal(out=dn, in_=dn), r=[t_dn], w=[t_dn])
            for hh in range(4):
                blk, half = hh // 2, hh % 2
                rows = slice(half * 64, half * 64 + 64)
                S.op("dve", lambda e, hh=hh, blk=blk, rows=rows, po=po, q0=q0: e.tensor_tensor(
                    out=mixA[blk][rows, q0:q0 + 128], in0=po[rows, hh * 128:(hh + 1) * 128], in1=dn[rows, hh * 128:(hh + 1) * 128], op=ALU.mult),
                    r=[tpo, t_dn], w=[t_hb[5 + blk]])
        for blk in range(2):
            S.dma(k.mix_d[2 * j + blk, :, :], mixA[blk], r=[t_hb[5 + blk]], w=[k.t_mixd[2 * j + blk]])
    k.dump("mixd_att", k.mix_d[0:4, :, :], [4, 128, N], BF16, k.t_mixd[0:4])
    S.barrier()
    if k.sub == "M0b":
        return

    row = sc["row"]
    t_row = k.t_row
    S.dma(row[0:4, 0:512], k.lbl_d.rearrange("r a n -> (r a) n"), w=[t_row])
    for h in range(4):
        row_to_col(k, row[0:4, h * 128:(h + 1) * 128], 4, 128, col[:, 24 + 4 * h:28 + 4 * h], t_row, t_col)
    lbT = col[:, 24:40].rearrange("p (h r a) -> p h r a", r=2, a=2)
    lbv = col[:, 44:52].rearrange("p (h r) -> p h r", r=2)
    omv = col[:, 52:60].rearrange("p (h r) -> p h r", r=2)
    S.op("dve", lambda e: e.tensor_tensor(out=lbv, in0=lbT[:, :, :, 0], in1=lbT[:, :, :, 1], op=ALU.subtract), r=[t_col], w=[t_col])
    S.op("act", lambda e: e.activation(out=lbv, in_=lbv, func=AF.Sigmoid), r=[t_col], w=[t_col])
    S.op("dve", lambda e: e.tensor_scalar(out=omv, in0=lbv, scalar1=-1.0, scalar2=1.0, op0=ALU.mult, op1=ALU.add), r=[t_col], w=[t_col])
    S.dma(row[0:1, 0:512], k.hn_d[:, :], w=[t_row])
    for h in range(4):
        row_to_col(k, row[0:1, h * 128:(h + 1) * 128], 1, 128, col[:, 40 + h:41 + h], t_row, t_col)

    qrow, Krow, A, B, oacc = RB[:, 0, :], RB[:, 1, :], RB[:, 2, :], RB[:, 3, :], RB[:, 4, :]
    gsil, mixrow = HB[:, 2, :], HB[:, 4, :]
    qts, kts = [HB[:, 0, :], HB[:, 5, :]], [HB[:, 1, :], HB[:, 6, :]]
    vtm = HB[:, 3, :].rearrange("p (i c) -> p i c", c=128)
    for h in range(4):
        base = 1664 + 512 * h
        wg, t_wg = k.load_w(k.w0_d[:, base:base + 512].rearrange("(k p) n -> p k n", p=128))
        wvv, t_wvv = k.load_w(k.w0_d[:, 3712 + 128 * h:3712 + 128 * (h + 1)].rearrange("(k p) n -> p k n", p=128))

        def ev_q(pb, tb, t0, nt):
            S.op("act", lambda e: e.activation(out=qrow[:, t0:t0 + nt], in_=pb[:, 0:nt], func=AF.Copy), r=[tb], w=[t_rb[0]])
        k.proj_fm(wg, t_wg, 256, 128, ev_q)

        def ev_g(pb, tb, t0, nt):
            S.op("act", lambda e: e.activation(out=B[:, t0:t0 + nt], in_=pb[:, 0:nt], func=AF.Sigmoid), r=[tb], w=[t_rb[3]])
            S.op("dve", lambda e: e.tensor_tensor(out=gsil[:, t0:t0 + nt], in0=pb[:, 0:nt], in1=B[:, t0:t0 + nt], op=ALU.mult),
                 r=[tb, t_rb[3]], w=[t_hb[2]])
        k.proj_fm(wg, t_wg, 384, 128, ev_g)

        def ev_v2(pb, tb, i):
            S.op("act", lambda e: e.activation(out=vtm[:, i, :], in_=pb[:, 0:128], func=AF.Copy), r=[tb], w=[t_hb[3]])
        k.proj_tm(wvv, t_wvv, 0, 128, ev_v2)

        def make_logf(d, h=h, wg=wg, t_wg=t_wg):
            def ev_z(pb, tb, t0, nt):
                S.op("act", lambda e: e.activation(out=A[:, t0:t0 + nt], in_=pb[:, 0:nt], func=AF.Sigmoid), r=[tb], w=[t_rb[2]])
            k.proj_fm(wg, t_wg, d * 128, 128, ev_z)
            S.op("dve", lambda e: e.tensor_scalar(out=A, in0=A, scalar1=omv[:, h, d:d + 1], scalar2=lbv[:, h, d:d + 1], op0=ALU.mult, op1=ALU.add),
                 r=[t_rb[2], t_col], w=[t_rb[2]])
            S.op("pool", lambda e: e.tensor_scalar(out=Krow, in0=A, scalar1=-1.0, scalar2=1.0, op0=ALU.mult, op1=ALU.add),
                 r=[t_rb[2]], w=[t_rb[1]])
            S.op("act", lambda e: e.activation(out=A, in_=A, func=AF.Ln), r=[t_rb[2]], w=[t_rb[2]])
        gated_scan(k, 128, 128, 1, qrow, t_rb[0], Krow, t_rb[1], A, t_rb[2], B, t_rb[3], make_logf,
                   lambda i: vtm[:, i, :], [t_hb[3]], [oacc], [t_rb[4]], qts, [t_hb[0], t_hb[5]], kts, [t_hb[1], t_hb[6]])
        if h == 0:
            k.dump("oacc0", oacc, [128, N], F32, [t_rb[4]])
        rms_gate_out(k, 128, 1, [oacc], [t_rb[4]], A, t_rb[2], B, t_rb[3], [gsil], [t_hb[2]], [col[:, 40 + h:41 + h]], t_col,
                     [mixrow], [t_hb[4]], [4 + h])
    k.dump("mixd0", k.mix_d[0:8, :, :], [8, 128, N], BF16, k.t_mixd[0:8])


def phase_D(k, l):
    S = k.S
    RB, HB = k.RB, k.HB
    k.bcast_rows(l, "mix")
    if l == 0:
        wo, t_wo = k.load_w(k.wo0_d.rearrange("(c p) n -> p c n", p=128), nslots=2)
        chunks = [(c, 128, wo, t_wo, c) for c in range(8)]
        tiles = list(range(NT))
        nch = 8
    else:
        wo, t_wo = k.load_w(k.wo1_d[0:768, :].rearrange("(c p) n -> p c n", p=96), nslots=2, parts=96)
        wf, t_wf = k.load_w(k.wo1_d[768:1024, :].rearrange("(c p) n -> p c n", p=128), nslots=1)
        chunks = [(c, 96, wo, t_wo, c) for c in range(8)] + [(8 + c, 128, wf, t_wf, c) for c in range(2)]
        tiles = list(range(2, NT))
        nch = 10
    tb_ = [RB[:, r, hf * 1024:(hf + 1) * 1024] for r in range(6) for hf in range(2)]
    tt = k.t_tile
    for n_, i in enumerate(tiles):
        s6 = (n_ % 2) * 6
        ht, tmp, xn1, hnew, xn2, ufb = [tb_[s6 + q] for q in range(6)]
        t_ht, t_tmp, t_xn1, t_hn, t_xn2, t_uf = [tt[s6 + q] for q in range(6)]
        uf = ufb.rearrange("p (k n) -> p k n", n=128)
        mt = HB[:, n_ % 2, 0:nch * 128].rearrange("p (c n) -> p c n", n=128)
        t_mt = k.t_hb[n_ % 2]
        S.dma(mt, k.mix_d[0:nch, :, i * 128:(i + 1) * 128].rearrange("c p n -> p c n"), r=k.t_mixd[0:nch], w=[t_mt])
        if l == 0:
            src = k.ctx_d[i * 128:(i + 1) * 128, :] if i < 2 else k.x_d[(i - 2) * 128:(i - 1) * 128, :]
            S.dma(ht, src, w=[t_ht])
        else:
            S.dma(ht, k.hout0_d[i * 128:(i + 1) * 128, :], r=[k.t_hout0[i]], w=[t_ht])
        pp, tp = k.pair()
        for hf in range(2):
            for ci_, (c, KR, wv, t_wv, wc) in enumerate(chunks):
                S.op("pe", lambda e, hf=hf, c=c, KR=KR, wv=wv, wc=wc, ci_=ci_, pp=pp, mt=mt: e.matmul(
                    pp[:, hf * 512:(hf + 1) * 512], lhsT=mt[0:KR, c, :], rhs=wv[0:KR, wc, hf * 512:(hf + 1) * 512],
                    start=(ci_ == 0), stop=(ci_ == len(chunks) - 1)), r=[t_mt] + t_wv, w=[tp[hf]])
        r_ = 1 if i < 2 else 0
        for hf in range(2):
            S.op("dve", lambda e, hf=hf, pp=pp, tmp=tmp, r_=r_: e.tensor_tensor(out=tmp[:, hf * 512:(hf + 1) * 512], in0=pp[:, hf * 512:(hf + 1) * 512],
                                                                               in1=k.bc[:, r_, hf * 512:(hf + 1) * 512], op=ALU.mult),
                 r=[tp[hf], k.t_bc[r_]], w=[t_tmp])
        S.op("dve", lambda e, ht=ht, tmp=tmp: e.scalar_tensor_tensor(out=tmp, in0=ht, scalar=ALPHA, in1=tmp, op0=ALU.mult, op1=ALU.add),
             r=[t_ht, t_tmp], w=[t_tmp])
        st_ap, mv_ap, rs_ap, nb_ap, t_st = k.st_slot()
        k.ln_stats(tmp, t_tmp, st_ap, mv_ap, rs_ap, nb_ap, t_st)
        S.op("act", lambda e, tmp=tmp, xn1=xn1, rs_ap=rs_ap, nb_ap=nb_ap: e.activation(out=xn1, in_=tmp, func=AF.Identity, scale=rs_ap, bias=nb_ap),
             r=[t_tmp, t_st], w=[t_xn1])
        S.op("dve", lambda e, xn1=xn1: e.tensor_tensor(out=xn1, in0=xn1, in1=k.bc[:, 2, :], op=ALU.mult), r=[t_xn1, k.t_bc[2]], w=[t_xn1])
        S.op("pool", lambda e, xn1=xn1, hnew=hnew: e.tensor_tensor(out=hnew, in0=xn1, in1=k.bc[:, 3, :], op=ALU.add), r=[t_xn1, k.t_bc[3]], w=[t_hn])
        S.dma(k.hmid_d[i * 128:(i + 1) * 128, :], hnew, r=[t_hn], w=[k.t_hmid[i]])
        k.ln_to_uT(l, hnew, t_hn, xn2, t_xn2, i, 3, True, uf, t_uf)
    k.dump(f"hmid{l}", k.hmid_d[:, :], [N, D], F32, k.t_hmid)
    k.dump(f"u2T{l}", k.uT[:, :, :], [128, KC, N], BF16, k.t_uT)
    k.dump(f"gates{l}", k.gates[:, :, :], [128, NT, 16], F32, k.t_gates)


def phase_E(k, l):
    S = k.S
    RB = k.RB
    k.bcast_rows(l, "moe")
    if l == 0:
        halves = [list(range(0, 9)), list(range(9, 18))]
        bsz = 384
    else:
        halves = [list(range(2, 10)), list(range(10, 18))]
        bsz = 512
    yacc = RB[:, 0:4, :].rearrange("p a n -> p (a n)").rearrange("p (t d) -> p t d", d=1024)
    t_y = k.t_tile[0:9]
    hTb = RB[:, 4, :].bitcast(BF16)
    hT = [hTb[:, q * 2048:(q + 1) * 2048].rearrange("p (f n) -> p f n", n=512) for q in range(2)]
    t_hT = [k.t_tile[9], k.t_tile[10]]
    sg = [RB[:, 5, q * 512:(q + 1) * 512] for q in range(2)]
    t_sg = [k.t_rb[4], k.t_rb[5]]
    Hb = RB[:, 5, 1024:2048]
    t_H = k.t_tile[11]
    dst_d = k.hout0_d if l == 0 else k.out_d
    k.h_i = 0
    k.s_i = 0
    for tiles in halves:
        tok0 = tiles[0] * 128
        ntok = len(tiles) * 128
        blocks = [(tok0 + b0, bsz) for b0 in range(0, ntok, bsz)]
        for e_ in range(16):
            w1, t_w1 = k.load_w(k.eg_d[l, e_].rearrange("(c p) n -> p c n", p=128))
            w3, t_w3 = k.load_w(k.eu_d[l, e_].rearrange("(c p) n -> p c n", p=128))
            w2, t_w2 = k.load_w(k.ed_d[l, e_].rearrange("(c p) n -> p c n", p=128))
            for (b0, nb) in blocks:
                hi = k.h_i
                k.h_i = 1 - hi
                hTc = hT[hi]
                for f in range(4):
                    p1, tp1 = k.bank()
                    for kk in range(KC):
                        S.op("pe", lambda e, kk=kk, f=f, p1=p1, w1=w1, b0=b0, nb=nb: e.matmul(
                            p1[:, 0:nb], lhsT=w1[:, kk, f * 128:(f + 1) * 128], rhs=k.uT[:, kk, b0:b0 + nb], start=(kk == 0), stop=(kk == KC - 1)),
                            r=t_w1 + k.t_uT[b0 // 128:(b0 + nb) // 128], w=[tp1])
                    p3, tp3 = k.bank()
                    for kk in range(KC):
                        S.op("pe", lambda e, kk=kk, f=f, p3=p3, w3=w3, b0=b0, nb=nb: e.matmul(
                            p3[:, 0:nb], lhsT=w3[:, kk, f * 128:(f + 1) * 128], rhs=k.uT[:, kk, b0:b0 + nb], start=(kk == 0), stop=(kk == KC - 1)),
                            r=t_w3 + k.t_uT[b0 // 128:(b0 + nb) // 128], w=[tp3])
                    si = k.s_i
                    k.s_i = 1 - si
                    S.op("act", lambda e, p1=p1, si=si, nb=nb: e.activation(out=sg[si][:, 0:nb], in_=p1[:, 0:nb], func=AF.Sigmoid), r=[tp1], w=[t_sg[si]])
                    S.op("dve", lambda e, p1=p1, si=si, nb=nb: e.tensor_tensor(out=sg[si][:, 0:nb], in0=p1[:, 0:nb], in1=sg[si][:, 0:nb], op=ALU.mult),
                         r=[tp1, t_sg[si]], w=[t_sg[si]])
                    S.op("dve", lambda e, p3=p3, si=si, nb=nb, f=f, hTc=hTc: e.tensor_tensor(out=hTc[:, f, 0:nb], in0=p3[:, 0:nb], in1=sg[si][:, 0:nb], op=ALU.mult),
                         r=[tp3, t_sg[si]], w=[t_hT[hi]])
                for tl in range(nb // 128):
                    gi = (b0 // 128) + tl
                    yi = gi - tiles[0]
                    for dh in range(2):
                        py, tpy = k.bank()
                        for f in range(4):
                            S.op("pe", lambda e, f=f, py=py, hTc=hTc, tl=tl, dh=dh, w2=w2: e.matmul(
                                py[:, :], lhsT=hTc[:, f, tl * 128:(tl + 1) * 128], rhs=w2[:, f, dh * 512:(dh + 1) * 512], start=(f == 0), stop=(f == 3)),
                                r=[t_hT[hi]] + t_w2, w=[tpy])
                        ya = yacc[:, yi, dh * 512:(dh + 1) * 512]
                        gs = k.gates[:, gi, e_:e_ + 1]
                        if e_ == 0:
                            S.op("dve", lambda e, py=py, ya=ya, gs=gs: e.tensor_scalar(out=ya, in0=py[:, :], scalar1=gs, scalar2=None, op0=ALU.mult),
                                 r=[tpy, k.t_gates[gi]], w=[t_y[yi]])
                        else:
                            S.op("dve", lambda e, py=py, ya=ya, gs=gs: e.scalar_tensor_tensor(out=ya, in0=py[:, :], scalar=gs, in1=ya, op0=ALU.mult, op1=ALU.add),
                                 r=[tpy, k.t_gates[gi], t_y[yi]], w=[t_y[yi]])
        for yi, gi in enumerate(tiles):
            yt = yacc[:, yi, :]
            r_ = 1 if gi < 2 else 0
            if l == 0 and yi == 0 and tiles[0] == 0:
                k.dump("ymoe_t0", yt, [128, D], F32, [t_y[yi]])
            S.dma(Hb, k.hmid_d[gi * 128:(gi + 1) * 128, :], r=[k.t_hmid[gi]], w=[t_H])
            S.op("dve", lambda e, yt=yt, r_=r_: e.tensor_tensor(out=yt, in0=yt, in1=k.bc[:, r_, :], op=ALU.mult), r=[t_y[yi], k.t_bc[r_]], w=[t_y[yi]])
            S.op("dve", lambda e, yt=yt: e.scalar_tensor_tensor(out=yt, in0=Hb, scalar=ALPHA, in1=yt, op0=ALU.mult, op1=ALU.add),
                 r=[t_H, t_y[yi]], w=[t_y[yi]])
            st_ap, mv_ap, rs_ap, nb_ap, t_st = k.st_slot()
            k.ln_stats(yt, t_y[yi], st_ap, mv_ap, rs_ap, nb_ap, t_st)
            S.op("act", lambda e, yt=yt, rs_ap=rs_ap, nb_ap=nb_ap: e.activation(out=Hb, in_=yt, func=AF.Identity, scale=rs_ap, bias=nb_ap),
                 r=[t_y[yi], t_st], w=[t_H])
            S.op("dve", lambda e: e.tensor_tensor(out=Hb, in0=Hb, in1=k.bc[:, 2, :], op=ALU.mult), r=[t_H, k.t_bc[2]], w=[t_H])
            S.op("pool", lambda e, yt=yt: e.tensor_tensor(out=yt, in0=Hb, in1=k.bc[:, 3, :], op=ALU.add), r=[t_H, k.t_bc[3]], w=[t_y[yi]])
            if l == 0:
                S.dma(k.hout0_d[gi * 128:(gi + 1) * 128, :], yt, r=[t_y[yi]], w=[k.t_hout0[gi]])
            else:
                S.dma(k.out_d[(gi - 2) * 128:(gi - 1) * 128, :], yt, r=[t_y[yi]], w=[k.t_out[gi - 2]])
    if l == 0:
        k.dump("hout0", k.hout0_d[:, :], [N, D], F32, k.t_hout0)


def mixer_odd(k):
    S = k.S
    scan_setup(k)
    sc = k.scn
    RB, HB, t_rb, t_hb = k.RB, k.HB, k.t_rb, k.t_hb
    col, t_col, row, t_row = sc["col"], k.t_col, sc["row"], k.t_row
    LATB = [(256, 512), (768, 512), (1280, 512), (1792, 512)]
    BC = sc["Am"][:, 0, 0, :]
    BS = sc["Am"][:, 0, 1, :]
    ci = col[:, 0:32].bitcast(I32)
    S.op("pool", lambda e: e.iota(ci[:, 0:1], pattern=[[0, 1]], base=0, channel_multiplier=1), w=[t_col])
    S.op("dve", lambda e: e.tensor_single_scalar(out=ci[:, 1:2], in_=ci[:, 0:1], scalar=63, op=ALU.bitwise_and), r=[t_col], w=[t_col])
    S.op("dve", lambda e: e.tensor_copy(out=col[:, 32:33], in_=ci[:, 1:2]), r=[t_col], w=[t_col])
    S.op("pool", lambda e: e.iota(ci[:, 2:18], pattern=[[128, 16]], base=0, channel_multiplier=1), r=[t_col], w=[t_col])
    S.op("dve", lambda e: e.tensor_copy(out=col[:, 40:56], in_=ci[:, 2:18]), r=[t_col], w=[t_col])
    qi = RB[:, 0, 0:128].bitcast(I32)
    qf = RB[:, 0, 128:256]
    ki = RB[:, 0, 256:384].bitcast(I32)
    kci = RB[:, 0, 384:512].bitcast(I32)
    tq = t_rb[0]
    S.op("pool", lambda e: e.iota(qi, pattern=[[1, 128]], base=0, channel_multiplier=0), w=[tq])
    S.op("dve", lambda e: e.tensor_single_scalar(out=qi, in_=qi, scalar=63, op=ALU.bitwise_and), r=[tq], w=[tq])
    S.op("dve", lambda e: e.tensor_copy(out=qf, in_=qi), r=[tq], w=[tq])
    S.op("dve", lambda e: e.tensor_scalar(out=ki, in0=qf, scalar1=col[:, 32:33], scalar2=None, op0=ALU.mult), r=[tq, t_col], w=[tq])
    S.op("dve", lambda e: e.tensor_single_scalar(out=ki, in_=ki, scalar=63, op=ALU.bitwise_and), r=[tq], w=[tq])
    S.op("dve", lambda e: e.tensor_scalar(out=kci, in0=ki, scalar1=16, scalar2=None, op0=ALU.add), r=[tq], w=[tq])
    S.op("dve", lambda e: e.tensor_single_scalar(out=kci, in_=kci, scalar=63, op=ALU.bitwise_and), r=[tq], w=[tq])
    S.op("act", lambda e: e.activation(out=BS, in_=ki, func=AF.Sin, scale=-2 * PI / 64, bias=k.cst[:, 4:5]), r=[tq, k.t_c], w=[k.t_Am[1]])
    S.op("act", lambda e: e.activation(out=BC, in_=kci, func=AF.Sin, scale=-2 * PI / 64, bias=k.cst[:, 4:5]), r=[tq, k.t_c], w=[k.t_Am[0]])
    for M_, tM in ((BC, k.t_Am[0]), (BS, k.t_Am[1])):
        S.op("pool", lambda e, M_=M_: e.memset(M_[0:64, 64:128], 0.0), r=[tM], w=[tM])
        S.op("pool", lambda e, M_=M_: e.memset(M_[64:128, 0:64], 0.0), r=[tM], w=[tM])
    if k.sub == "M1a":
        k.dump("BCS", sc["Am"][:, 0, :, :], [128, 2, 128], BF16, k.t_Am)
        return
    wz, t_wz = k.load_w(k.w1_d[:, 1536:1792].rearrange("(k p) n -> p k n", p=128))
    zT = HB[:, 2:4, :]
    zc = HB[:, 4:6, :].rearrange("p a n -> p (a n)")[:, 0:4096].rearrange("p (i c) -> p i c", c=256)
    zs = HB[:, 6:8, :].rearrange("p a n -> p (a n)")[:, 0:4096].rearrange("p (i c) -> p i c", c=256)
    for m in range(2):
        for (t0, nt) in LATB:
            pb, tb = proj_block(k, wz, t_wz, m * 128, 128, t0, nt)
            S.op("act", lambda e, pb=pb, m=m, t0=t0, nt=nt: e.activation(out=zT[:, m, t0:t0 + nt], in_=pb[:, 0:nt], func=AF.Copy), r=[tb], w=[t_hb[2 + m]])
    if k.sub == "M1z":
        k.dump("zT", HB[:, 2:4, :], [128, 2, N], BF16, [t_hb[2], t_hb[3]])
        return
    for a in range(16):
        tok = (a + 2) * 128
        pb, tb = k.bank()
        for m in range(2):
            S.op("pe", lambda e, pb=pb, m=m, tok=tok: e.matmul(pb[:, m * 128:(m + 1) * 128], lhsT=zT[:, m, tok:tok + 128], rhs=BC[:, :], start=True, stop=True),
                 r=[t_hb[2 + m], k.t_Am[0]], w=[tb])
            S.op("pe", lambda e, pb=pb, m=m, tok=tok: e.matmul(pb[:, 256 + m * 128:256 + (m + 1) * 128], lhsT=zT[:, m, tok:tok + 128], rhs=BS[:, :], start=True, stop=True),
                 r=[t_hb[2 + m], k.t_Am[1]], w=[tb])
        S.op("act", lambda e, pb=pb, a=a: e.activation(out=zc[:, a, :], in_=pb[:, 0:256], func=AF.Copy), r=[tb], w=[t_hb[4], t_hb[5]])
        if k.sub != "M1d":
            S.op("act", lambda e, pb=pb, a=a: e.activation(out=zs[:, a, :], in_=pb[:, 256:512], func=AF.Copy, scale=-1.0), r=[tb], w=[t_hb[6], t_hb[7]])
        if k.sub in ("M1c", "M1d") and a == 0:
            k.dump("zc", HB[:, 4:6, :], [128, 2, N], BF16, [t_hb[4], t_hb[5]])
            return
    S.barrier()
    if k.sub == "M1b":
        k.dump("zc", HB[:, 4:6, :], [128, 2, N], BF16, [t_hb[4], t_hb[5]])
        return
    fidx = RB[:, 0, 0:2048]
    fi_i = RB[:, 1, 0:2048].bitcast(I32)
    S.op("pool", lambda e: e.iota(fi_i, pattern=[[1, 2048]], base=0, channel_multiplier=0), w=[t_rb[1]])
    S.op("dve", lambda e: e.tensor_copy(out=fidx, in_=fi_i), r=[t_rb[1]], w=[t_rb[0]])
    tabs = []
    for q in range(2):
        rowb = RB[:, 2 + q, :].bitcast(BF16)
        tabs.append((rowb[:, 0:2048], rowb[:, 2048:4096], t_rb[2 + q]))
    kib = [RB[:, 4, 0:2048].bitcast(I32), RB[:, 5, 0:2048].bitcast(I32)]
    banks = [(k.PS[i // 2][:, (i % 2) * 512:(i % 2 + 1) * 512], k.PT[i // 2][i % 2]) for i in range(8)]
    for a in range(16):
        Cb, Sb, t_tab = tabs[a % 2]
        S.op("dve", lambda e, a=a: e.tensor_scalar(out=kib[0], in0=fidx, scalar1=col[:, 40 + a:41 + a], scalar2=None, op0=ALU.mult),
             r=[t_rb[0], t_col], w=[t_rb[4]])
        S.op("dve", lambda e: e.tensor_single_scalar(out=kib[0], in_=kib[0], scalar=2047, op=ALU.bitwise_and), r=[t_rb[4]], w=[t_rb[4]])
        S.op("dve", lambda e: e.tensor_scalar(out=kib[1], in0=kib[0], scalar1=512, scalar2=None, op0=ALU.add), r=[t_rb[4]], w=[t_rb[5]])
        S.op("dve", lambda e: e.tensor_single_scalar(out=kib[1], in_=kib[1], scalar=2047, op=ALU.bitwise_and), r=[t_rb[5]], w=[t_rb[5]])
        S.op("act", lambda e, Sb=Sb: e.activation(out=Sb, in_=kib[0], func=AF.Sin, scale=-2 * PI / 2048, bias=k.cst[:, 4:5]), r=[t_rb[4], k.t_c], w=[t_tab])
        S.op("act", lambda e, Cb=Cb: e.activation(out=Cb, in_=kib[1], func=AF.Sin, scale=-2 * PI / 2048, bias=k.cst[:, 4:5]), r=[t_rb[5], k.t_c], w=[t_tab])
        for m in range(2):
            for fb in range(4):
                pbk, tbk = banks[m * 4 + fb]
                S.op("pe", lambda e, pbk=pbk, a=a, m=m, fb=fb, Cb=Cb: e.matmul(pbk[:, :], lhsT=zc[:, a, m * 128:(m + 1) * 128], rhs=Cb[:, fb * 512:(fb + 1) * 512],
                                                                            start=(a == 0), stop=False), r=[t_hb[4], t_hb[5], t_tab], w=[tbk])
                S.op("pe", lambda e, pbk=pbk, a=a, m=m, fb=fb, Sb=Sb: e.matmul(pbk[:, :], lhsT=zs[:, a, m * 128:(m + 1) * 128], rhs=Sb[:, fb * 512:(fb + 1) * 512],
                                                                            start=False, stop=(a == 15)), r=[t_hb[6], t_hb[7], t_tab], w=[tbk])
    fsc = 1.0 / math.sqrt(2048.0 * 64.0)
    for m in range(2):
        for fb in range(4):
            pbk, tbk = banks[m * 4 + fb]
            S.op("act", lambda e, pbk=pbk, m=m, fb=fb: e.activation(out=HB[:, m, fb * 512:(fb + 1) * 512], in_=pbk[:, :], func=AF.Copy, scale=fsc), r=[tbk], w=[t_hb[m]])
        S.dma(k.mix_d[8 + m, :, 256:2304], HB[:, m, 0:2048], r=[t_hb[m]], w=[k.t_mixd[8 + m]])
    k.dump("mixd_f", k.mix_d[8:10, :, :], [2, 128, N], BF16, k.t_mixd[8:10])
    S.barrier()
    if k.sub == "M1f":
        return

    gw = k.bc[0:16, 0, 0:768].rearrange("p (r n) -> p r n", n=384)
    t_gw = k.t_bc[0]
    S.dma(gw[:, :, :], k.gw_d.rearrange("r k n -> k r n"), w=[t_gw])
    S.dma(row[0:2, 0:384], k.gb_d[:, :], w=[t_row])
    for h in range(4):
        row_to_col(k, row[0:2, h * 96:(h + 1) * 96], 2, 96, col[0:96, 2 * h:2 * h + 2], t_row, t_col)
    S.op("dve", lambda e: e.tensor_scalar(out=col[0:96, 0:8], in0=col[0:96, 0:8], scalar1=-1.0, scalar2=None, op0=ALU.mult), r=[t_col], w=[t_col])
    S.dma(row[0:1, 0:768], k.gn_d[:, :], r=[t_col], w=[t_row])
    for c in range(8):
        row_to_col(k, row[0:1, c * 96:(c + 1) * 96], 1, 96, col[0:96, 8 + c:9 + c], t_row, t_col)
    qrow, krow, A, B = RB[0:96, 0, :], RB[0:96, 1, :], RB[0:96, 2, :], RB[0:96, 3, :]
    oacc = [RB[0:96, 4, :], RB[0:96, 5, :]]
    qt, kt = HB[0:96, 0, :], HB[0:96, 1, :]
    qts, kts = [HB[0:96, 0, :], HB[0:96, 6, :]], [HB[0:96, 1, :], HB[0:96, 7, :]]
    gsil = [HB[0:96, 2, :], HB[0:96, 3, :]]
    vt = HB[:, 4:6, :].rearrange("p a n -> p (a n)")[:, 0:3456].rearrange("p (i c) -> p i c", c=192)
    for h in range(4):
        base = 384 * h
        wg, t_wg = k.load_w(k.w1_d[:, base:base + 384].rearrange("(k p) n -> p k n", p=128))
        wv, t_wvv = k.load_w(k.w1_d[:, 1824 + 192 * h:1824 + 192 * (h + 1)].rearrange("(k p) n -> p k n", p=128))
        wR, t_wR = k.load_w(k.w1_d[:, 1792:1824].rearrange("(k p) n -> p k n", p=128))

        def ev_q(pb, tb, t0, nt):
            S.op("act", lambda e: e.activation(out=qrow[:, t0:t0 + nt], in_=pb[0:96, 0:nt], func=AF.Copy, scale=96.0 ** -0.5), r=[tb], w=[t_rb[0]])
        k.proj_fm(wg, t_wg, 0, 96, ev_q)

        def ev_k(pb, tb, t0, nt):
            S.op("act", lambda e: e.activation(out=krow[:, t0:t0 + nt], in_=pb[0:96, 0:nt], func=AF.Copy), r=[tb], w=[t_rb[1]])
        k.proj_fm(wg, t_wg, 96, 96, ev_k)
        for a in range(2):
            def ev_g(pb, tb, t0, nt, a=a):
                S.op("act", lambda e: e.activation(out=B[:, t0:t0 + nt], in_=pb[0:96, 0:nt], func=AF.Sigmoid), r=[tb], w=[t_rb[3]])
                S.op("dve", lambda e: e.tensor_tensor(out=gsil[a][:, t0:t0 + nt], in0=pb[0:96, 0:nt], in1=B[:, t0:t0 + nt], op=ALU.mult),
                     r=[tb, t_rb[3]], w=[t_hb[2 + a]])
            k.proj_fm(wg, t_wg, 192 + 96 * a, 96, ev_g)

        def ev_v(pb, tb, i):
            S.op("act", lambda e: e.activation(out=vt[:, i, :], in_=pb[:, 0:192], func=AF.Copy), r=[tb], w=[t_hb[4], t_hb[5]])
        k.proj_tm(wv, t_wvv, 0, 192, ev_v)

        def make_logf(d, h=h, wR=wR, t_wR=t_wR):
            for (t0, nt) in TOKB:
                pr, tr = proj_block(k, wR, t_wR, d * 16, 16, t0, nt)
                S.op("act", lambda e, pr=pr, t0=t0, nt=nt: e.activation(out=B[0:16, t0:t0 + nt], in_=pr[0:16, 0:nt], func=AF.Copy), r=[tr], w=[t_rb[3]])
                pz, tz = k.bank()
                S.op("pe", lambda e, pz=pz, t0=t0, nt=nt: e.matmul(pz[0:96, 0:nt], lhsT=gw[0:16, d, h * 96:(h + 1) * 96], rhs=B[0:16, t0:t0 + nt],
                                                                   start=True, stop=True), r=[t_gw, t_rb[3]], w=[tz])
                S.op("act", lambda e, pz=pz, t0=t0, nt=nt: e.activation(out=A[:, t0:t0 + nt], in_=pz[0:96, 0:nt], func=AF.Exp, scale=-1.0,
                                                                        bias=col[0:96, 2 * h + d:2 * h + d + 1]), r=[tz, t_col], w=[t_rb[2]])
            S.op("act", lambda e: e.activation(out=A, in_=A, func=AF.Ln, bias=k.cst[0:96, 1:2]), r=[t_rb[2], k.t_c], w=[t_rb[2]])
            S.op("dve", lambda e: e.tensor_scalar(out=A, in0=A, scalar1=-1.0 / 16.0, scalar2=None, op0=ALU.mult), r=[t_rb[2]], w=[t_rb[2]])
        gated_scan(k, 96, 96, 2, qrow, t_rb[0], krow, t_rb[1], A, t_rb[2], B, t_rb[3], make_logf,
                   lambda i: vt[:, i, :], [t_hb[4], t_hb[5]], oacc, [t_rb[4], t_rb[5]], qts, [t_hb[0], t_hb[6]], kts, [t_hb[1], t_hb[7]])
        if h == 0:
            k.dump("gla_o0", RB[0:96, 4:6, :], [96, 2, N], F32, [t_rb[4], t_rb[5]])
        rms_gate_out(k, 96, 2, oacc, [t_rb[4], t_rb[5]], A, t_rb[2], B, t_rb[3], gsil, [t_hb[2], t_hb[3]],
                     [col[0:96, 8 + 2 * h:9 + 2 * h], col[0:96, 9 + 2 * h:10 + 2 * h]], t_col, [qt, kt], [t_hb[0], t_hb[1]], [2 * h, 2 * h + 1])
    k.dump("mixd1", k.mix_d[0:10, :, :], [10, 128, N], BF16, k.t_mixd[0:10])


_NC_CACHE = {}


def _f32(a):
    return np.ascontiguousarray(np.asarray(a, dtype=np.float32))


def kernel(x, c, ctx, c_ctx, w_ada, b_ada, ln_g, ln_b, w_in_even, attn_sink, hgrn_lb_logits, hgrn_norm,
           w_out_even, w_in_odd, gla_gate_w, gla_gate_b, gla_norm, w_out_odd, w_router, b_router,
           w_expert_gate, w_expert_up, w_expert_down):
    x = _f32(x); c = _f32(c); ctx = _f32(ctx); c_ctx = _f32(c_ctx)
    w0a = np.ascontiguousarray(_f32(w_in_even)[0][:, _cols0()])
    w1a = np.ascontiguousarray(_f32(w_in_odd)[0][:, _cols1()])
    shared = {
        "w_ada": _f32(w_ada), "b_ada": _f32(b_ada), "ln_g": _f32(ln_g), "ln_b": _f32(ln_b),
        "w0a": w0a, "attn_sink": _f32(attn_sink), "lb_logits": _f32(hgrn_lb_logits), "hgrn_norm": _f32(hgrn_norm),
        "w_out_even": _f32(w_out_even)[0], "w1a": w1a, "gla_gate_w": _f32(gla_gate_w)[0], "gla_gate_b": _f32(gla_gate_b)[0],
        "gla_norm": _f32(gla_norm), "w_out_odd": _f32(w_out_odd)[0], "w_router": _f32(w_router),
        "b_router": _f32(b_router)[None, :], "w_expert_gate": _f32(w_expert_gate), "w_expert_up": _f32(w_expert_up),
        "w_expert_down": _f32(w_expert_down),
    }
    nb = x.shape[0]
    in_maps = []
    for b in range(nb):
        m = dict(shared)
        m["x"] = np.ascontiguousarray(x[b])
        m["ctx"] = np.ascontiguousarray(ctx[b])
        m["cvec"] = np.ascontiguousarray(np.stack([c[b], c_ctx], 0))
        in_maps.append(m)
    if "nc" not in _NC_CACHE:
        _NC_CACHE["nc"] = build()
    res = run_bass_kernel_spmd(_NC_CACHE["nc"], in_maps, core_ids=list(range(nb)))
    return np.stack([np.asarray(r["out"], dtype=np.float32) for r in res.results], 0)
```

```python
import contextlib
import math
import numpy as np
import concourse.bass as bass
import concourse.mybir as mybir
from concourse.bass_utils import run_bass_kernel_spmd

F32 = mybir.dt.float32
BF16 = mybir.dt.bfloat16
I32 = mybir.dt.int32
AF = mybir.ActivationFunctionType
ALU = mybir.AluOpType
AX = mybir.AxisListType

ENG = ("pe", "act", "dve", "pool", "sp")


class Trk:
    __slots__ = ("w", "rs", "dsem", "dcnt", "name")

    def __init__(self, name=""):
        self.w = None
        self.rs = []
        self.dsem = None
        self.dcnt = 0
        self.name = name


class Sched:
    SEM_CHUNK = 20000

    def __init__(self, nc):
        self.nc = nc
        self.ops = {e: [] for e in ENG}
        self.waited = {e: {} for e in ENG}
        self.stack = contextlib.ExitStack()
        self.nsem = 0
        self.dma_ev = {}

    def sbuf(self, name, shape, dt):
        return self.stack.enter_context(self.nc.sbuf_tensor(name, list(shape), dt))

    def psum(self, name, shape, dt=F32):
        return self.stack.enter_context(self.nc.psum_tensor(name, list(shape), dt))

    def new_sem(self, name):
        self.nsem += 1
        return self.stack.enter_context(self.nc.semaphore(f"{name}_{self.nsem}"))

    def _filter(self, engine, deps):
        waits = []
        wd = self.waited[engine]
        for ev in deps:
            if ev[0] == "e":
                _, f, idx = ev
                if engine == "pe" and f == "pe":
                    continue
                if idx <= wd.get(f, -1):
                    continue
                wd[f] = idx
                self.ops[f][idx][2] = True
                waits.append(ev)
            else:
                _, sem, val = ev
                k = id(sem)
                if val <= wd.get(k, 0):
                    continue
                wd[k] = val
                waits.append(ev)
        return waits

    def _deps(self, engine, r, w):
        deps = []
        for t in r:
            if t.w is not None:
                deps.append(t.w)
        for t in w:
            if t.w is not None:
                deps.append(t.w)
            deps.extend(t.rs)
        return self._filter(engine, deps)

    def _post(self, ev, r, w):
        for t in w:
            t.w = ev
            t.rs = []
        for t in r:
            if t in w:
                continue
            if ev[0] == "e":
                t.rs = [x for x in t.rs if not (x[0] == "e" and x[1] == ev[1])]
            else:
                t.rs = [x for x in t.rs if not (x[0] == "d" and x[1] is ev[1])]
            t.rs.append(ev)

    def op(self, engine, fn, r=(), w=()):
        r = list(r)
        w = list(w)
        waits = self._deps(engine, r, w)
        idx = len(self.ops[engine])
        self.ops[engine].append([fn, waits, False, None])
        self._post(("e", engine, idx), r, w)

    def dma(self, out, in_, r=(), w=(), q="sp", **kw):
        r = list(r)
        w = list(w)
        waits = self._deps(q, r, w)
        t0 = w[0]
        if t0.dsem is None or t0.dcnt > 60000:
            t0.dsem = self.new_sem("d")
            t0.dcnt = 0
        t0.dcnt += 16
        ev = ("d", t0.dsem, t0.dcnt)
        self.dma_ev[id(t0.dsem)] = ev

        def fn(eng, out=out, in_=in_, kw=kw):
            return eng.dma_start(out=out, in_=in_, **kw)
        self.ops[q].append([fn, waits, False, t0.dsem])
        self._post(ev, r, w)

    def barrier(self):
        last = {}
        for f in ("pe", "act", "dve", "pool"):
            j = len(self.ops[f]) - 1
            while j >= 0 and (self.ops[f][j][0] is None or self.ops[f][j][3] is not None):
                j -= 1
            last[f] = j
        dm = list(self.dma_ev.values())
        for e in ENG:
            deps = [("e", f, last[f]) for f in ("pe", "act", "dve", "pool") if f != e and last[f] >= 0]
            deps += dm
            waits = self._filter(e, deps)
            self.ops[e].append([None, waits, False, None])

    def wait_all(self, engine, trks):
        waits = self._deps(engine, list(trks), [])
        self.ops[engine].append([None, waits, False, None])

    def emit(self):
        nc = self.nc
        cum = {}
        sems = {}
        for e in ENG:
            c = 0
            arr = []
            for rec in self.ops[e]:
                if rec[2]:
                    c += 1
                arr.append(c)
            cum[e] = arr
            sems[e] = [self.new_sem(f"s{e}") for _ in range(c // self.SEM_CHUNK + 1)]
        CH = self.SEM_CHUNK

        def semval(f, idx):
            c = cum[f][idx]
            ch = (c - 1) // CH
            return sems[f][ch], c - ch * CH

        def run(e, eng):
            for i, (fn, waits, sig, dsem) in enumerate(self.ops[e]):
                for ev in waits:
                    if ev[0] == "e":
                        s, v = semval(ev[1], ev[2])
                        eng.wait_ge(s, v)
                    else:
                        eng.wait_ge(ev[1], ev[2])
                if fn is None:
                    continue
                ins = fn(eng)
                if dsem is not None:
                    ins.then_inc(dsem, 16)
                elif sig:
                    s, v = semval(e, i)
                    ins.then_inc(s, 1)

        with nc.Block() as block:
            @block.tensor
            def _(eng):
                run("pe", eng)

            @block.scalar
            def _(eng):
                run("act", eng)

            @block.vector
            def _(eng):
                run("dve", eng)

            @block.gpsimd
            def _(eng):
                run("pool", eng)

            @block.sync
            def _(eng):
                run("sp", eng)

    def close(self):
        self.stack.close()


N = 2304
NT = 18
D = 1024
KC = 8
NLAT = 2048
ALPHA = 4.0 ** 0.25
EPS = 1e-5
TOKB = [(0, 512), (512, 512), (1024, 512), (1536, 512), (2048, 256)]
PI = math.pi

_SW = list(range(16, 32)) + list(range(0, 16)) + list(range(48, 64)) + list(range(32, 48))


def _cols0():
    cols = []
    for j in range(2):
        for blk in range(2):
            for hh in (4 * j + 2 * blk, 4 * j + 2 * blk + 1):
                cols += [hh * 64 + d for d in range(64)]
        for blk in range(2):
            for hh in (4 * j + 2 * blk, 4 * j + 2 * blk + 1):
                cols += [hh * 64 + d for d in _SW]
        cols += [512 + j * 64 + d for d in range(64)] * 2
        cols += [512 + j * 64 + d for d in _SW] * 2
    cols += list(range(640, 768))
    for h in range(4):
        cols += [768 + h * 128 + d for d in range(128)]
        cols += [1280 + h * 128 + d for d in range(128)]
        cols += [1792 + h * 128 + d for d in range(128)]
        cols += [2816 + h * 128 + d for d in range(128)]
    cols += list(range(2304, 2816))
    return cols


def _cols1():
    cols = []
    for h in range(4):
        cols += [h * 96 + d for d in range(96)]
        cols += [384 + h * 96 + d for d in range(96)]
        cols += [1568 + h * 192 + d for d in range(192)]
    cols += list(range(2336, 2592))
    cols += list(range(1536, 1568))
    cols += list(range(768, 1536))
    return cols


class K:
    pass


def build(dbg=(), stop=None):
    nc = bass.Bass("TRN2", target_bir_lowering=False)
    S = Sched(nc)
    k = K()
    k.nc = nc
    k.S = S
    k.dbg = set(dbg)
    k.sub = stop
    k.dbg_out = []

    def din(name, shape, dt=F32):
        return nc.dram_tensor(name, list(shape), dt, kind="ExternalInput").ap()

    x_d = din("x", [NLAT, D])
    ctx_d = din("ctx", [256, D])
    cv_d = din("cvec", [2, D])
    wada_d = din("w_ada", [2, D, 6 * D])
    bada_d = din("b_ada", [2, 6 * D])
    lng_d = din("ln_g", [2, 2, D])
    lnb_d = din("ln_b", [2, 2, D])
    w0_d = din("w0a", [D, 4224])
    sink_d = din("attn_sink", [1, 8])
    lbl_d = din("lb_logits", [2, 2, 512])
    hn_d = din("hgrn_norm", [1, 512])
    wo0_d = din("w_out_even", [D, D])
    w1_d = din("w1a", [D, 2592])
    gw_d = din("gla_gate_w", [2, 16, 384])
    gb_d = din("gla_gate_b", [2, 384])
    gn_d = din("gla_norm", [1, 768])
    wo1_d = din("w_out_odd", [D, D])
    wr_d = din("w_router", [D, 16])
    br_d = din("b_router", [1, 16])
    eg_d = din("w_expert_gate", [2, 16, D, 512])
    eu_d = din("w_expert_up", [2, 16, D, 512])
    ed_d = din("w_expert_down", [2, 16, 512, D])
    out_d = nc.dram_tensor("out", [NLAT, D], F32, kind="ExternalOutput").ap()
    hmid_d = nc.dram_tensor("hmid", [N, D], F32).ap()
    hout0_d = nc.dram_tensor("hout0", [N, D], F32).ap()
    mix_d = nc.dram_tensor("mixd", [10, 128, N], BF16).ap()
    t_hmid = [Trk() for _ in range(NT)]
    t_hout0 = [Trk() for _ in range(NT)]
    t_mixd = [Trk() for _ in range(10)]
    t_out = [Trk() for _ in range(16)]

    def dump(name, src_ap, shape, dt, r):
        if name not in k.dbg:
            return
        d = nc.dram_tensor("dbg_" + name, list(shape), dt, kind="ExternalOutput").ap()
        t = Trk()
        S.dma(d, src_ap, r=r, w=[t])
        k.dbg_out.append(t)

    PS = [S.psum(f"ps{i}", [128, 1024]) for i in range(4)]
    PT = [[Trk(), Trk()] for _ in range(4)]
    k.bank_i = 0
    k.pair_i = 0

    k.reserved = set()

    def bank():
        i = k.bank_i
        while i in k.reserved:
            i = (i + 1) % 8
        k.bank_i = (i + 1) % 8
        k.last_bank = i
        return PS[i // 2][:, (i % 2) * 512:(i % 2 + 1) * 512], PT[i // 2][i % 2]

    def pair():
        i = k.pair_i
        k.pair_i = (i + 1) % 4
        k.bank_i = (2 * i + 2) % 8
        return PS[i], PT[i]

    identF = S.sbuf("identF", [128, 128], F32); t_c = Trk()
    identB = S.sbuf("identB", [128, 128], BF16)
    onesF = S.sbuf("onesF", [128, 128], F32)
    onesB = S.sbuf("onesB", [128, 128], BF16)
    cst = S.sbuf("cst", [128, 8], F32)
    Mf = S.sbuf("Mf", [128, 128], BF16)
    Mb = S.sbuf("Mb", [128, 128], BF16)
    MP = S.sbuf("MP", [128, 512], BF16)
    MN = S.sbuf("MN", [128, 512], BF16)
    rmask = S.sbuf("rmask", [128, N], BF16)
    modT = S.sbuf("modT", [128, 2, 48, 2], F32); t_modT = Trk()
    mod_d = nc.dram_tensor("mod_d", [2, 2, 6 * D], F32).ap(); t_mod = Trk()
    bc = S.sbuf("bc", [128, 4, D], F32); t_bc = [Trk() for _ in range(4)]
    uT = S.sbuf("uT", [128, KC, N], BF16); t_uT = [Trk() for _ in range(NT)]
    wbuf = S.sbuf("wbuf", [128, 5, 4096], BF16); t_wb = [Trk() for _ in range(5)]
    RB = S.sbuf("RB", [128, 6, N], F32); t_rb = [Trk() for _ in range(6)]
    HB = S.sbuf("HB", [128, 8, N], BF16); t_hb = [Trk() for _ in range(8)]
    sm = S.sbuf("sm", [128, 640], F32)
    gates = S.sbuf("gates", [128, NT, 16], F32); t_gates = [Trk() for _ in range(NT)]
    wrt = S.sbuf("wrt", [128, KC, 16], F32); t_wr = Trk()
    brb = S.sbuf("brb", [128, 16], F32)

    S.op("pool", lambda e: e.memset(identF[:], 0.0), w=[t_c])
    S.op("pool", lambda e: e.affine_select(out=identF[:], in_=identF[:], pattern=[[-1, 128]], compare_op=ALU.not_equal,
                                           fill=1.0, base=0, channel_multiplier=1), r=[t_c], w=[t_c])
    S.op("pool", lambda e: e.tensor_copy(out=identB[:], in_=identF[:]), r=[t_c], w=[t_c])
    S.op("pool", lambda e: e.memset(onesF[:], 1.0), w=[t_c])
    S.op("pool", lambda e: e.memset(onesB[:], 1.0), w=[t_c])
    for j_, v_ in enumerate((EPS, 1.0, -PI, 0.0, PI)):
        S.op("pool", lambda e, j_=j_, v_=v_: e.memset(cst[:, j_:j_ + 1], v_), w=[t_c])
    S.op("pool", lambda e: e.memset(Mf[:], 1.0), w=[t_c])
    S.op("pool", lambda e: e.affine_select(out=Mf[:], in_=Mf[:], pattern=[[1, 128]], compare_op=ALU.is_ge, fill=0.0,
                                           base=0, channel_multiplier=-1), r=[t_c], w=[t_c])
    S.op("pool", lambda e: e.memset(Mf[0:64, 64:128], 0.0), r=[t_c], w=[t_c])
    S.op("pool", lambda e: e.memset(Mb[:], 1.0), w=[t_c])
    S.op("pool", lambda e: e.affine_select(out=Mb[:], in_=Mb[:], pattern=[[-1, 128]], compare_op=ALU.is_ge, fill=0.0,
                                           base=0, channel_multiplier=1), r=[t_c], w=[t_c])
    S.op("pool", lambda e: e.memset(Mb[64:128, 0:64], 0.0), r=[t_c], w=[t_c])
    S.op("pool", lambda e: e.memset(MP[:], 1.0), w=[t_c])
    S.op("pool", lambda e: e.affine_select(out=MP[:], in_=MP[:], pattern=[[0, 4], [-1, 128]], compare_op=ALU.is_ge,
                                           fill=0.0, base=0, channel_multiplier=1), r=[t_c], w=[t_c])
    S.op("pool", lambda e: e.memset(MN[:], 1.0), w=[t_c])
    S.op("pool", lambda e: e.affine_select(out=MN[:], in_=MN[:], pattern=[[0, 4], [1, 128]], compare_op=ALU.is_ge,
                                           fill=0.0, base=0, channel_multiplier=-1), r=[t_c], w=[t_c])
    S.op("pool", lambda e: e.memset(rmask[:], 1.0), w=[t_c])
    S.op("pool", lambda e: e.memset(rmask[:].rearrange("p (c l) -> p c l", l=64)[:, :, 0:1], 0.0), r=[t_c], w=[t_c])
    S.dma(wrt[:], wr_d.rearrange("(k p) n -> p k n", p=128), w=[t_wr])
    S.dma(brb[:], br_d[0:1, :].to_broadcast([128, 16]), w=[t_c])
    S.barrier()

    cs = RB[0:2, 4, 0:1024]
    sg_ = RB[0:2, 4, 1024:2048]
    csT = sm[:, 520:536].rearrange("p (k r) -> p k r", r=2)
    t_cs = Trk(); t_csT = Trk()
    k.t_bb = [[Trk(), Trk()], [Trk(), Trk()]]
    S.dma(cs, cv_d[:, :], w=[t_cs])
    S.op("act", lambda e: e.activation(out=sg_, in_=cs, func=AF.Sigmoid), r=[t_cs], w=[t_csT])
    S.op("dve", lambda e: e.tensor_tensor(out=cs, in0=cs, in1=sg_, op=ALU.mult), r=[t_cs, t_csT], w=[t_cs])
    pb, tb = bank()
    for kk in range(KC):
        S.op("pe", lambda e, kk=kk, pb=pb: e.transpose(out=pb[:, 2 * kk:2 * kk + 2], in_=cs[:, kk * 128:(kk + 1) * 128],
                                                       identity=identF[0:2, 0:2]), r=[t_cs, t_c], w=[tb])
    S.op("dve", lambda e, pb=pb: e.tensor_copy(out=csT, in_=pb[:, 0:16].rearrange("p (k r) -> p k r", r=2)), r=[tb], w=[t_csT])
    t_wa = [Trk(), Trk()]
    t_mb = [Trk(), Trk()]
    for l in range(2):
        pT, tT = bank()
        k.reserved = {k.last_bank}
        for j in range(12):
            slot = (l * 12 + j) % 2
            wa = RB[:, slot * 2:slot * 2 + 2, :].rearrange("p a n -> p (a n)")[:, 0:4096].rearrange("p (k n) -> p k n", n=512)
            mb = RB[0:2, 5, slot * 512:(slot + 1) * 512]
            S.dma(wa, wada_d[l, :, j * 512:(j + 1) * 512].rearrange("(k p) n -> p k n", p=128), w=[t_wa[slot]])
            for r_ in range(2):
                S.dma(RB[r_:r_ + 1, 5, 1024 + slot * 512:1024 + (slot + 1) * 512], bada_d[l:l + 1, j * 512:(j + 1) * 512],
                      w=[k.t_bb[slot][r_]])
            pb, tb = bank()
            for kk in range(KC):
                S.op("pe", lambda e, kk=kk, pb=pb, wa=wa: e.matmul(pb[0:2, :], lhsT=csT[:, kk, :], rhs=wa[:, kk, :],
                                                                   start=(kk == 0), stop=(kk == KC - 1)),
                     r=[t_csT, t_wa[slot]], w=[tb])
            bb = RB[0:2, 5, 1024 + slot * 512:1024 + (slot + 1) * 512]
            S.op("dve", lambda e, pb=pb, mb=mb, bb=bb: e.tensor_tensor(out=mb, in0=pb[0:2, :], in1=bb, op=ALU.add),
                 r=[tb] + k.t_bb[slot], w=[t_mb[slot]])
            if j in (2, 3, 8, 9):
                S.op("dve", lambda e, mb=mb: e.tensor_scalar(out=mb, in0=mb, scalar1=1.0, scalar2=None, op0=ALU.add),
                     r=[t_mb[slot]], w=[t_mb[slot]])
            S.dma(mod_d[l, :, j * 512:(j + 1) * 512], mb, r=[t_mb[slot]], w=[t_mod])
            for q_ in range(4):
                jj = j * 4 + q_
                S.op("pe", lambda e, jj=jj, q_=q_, mb=mb, pT=pT: e.transpose(out=pT[:, 2 * jj:2 * jj + 2], in_=mb[:, q_ * 128:(q_ + 1) * 128],
                                                                             identity=identF[0:2, 0:2]), r=[t_mb[slot], t_c], w=[tT])
        S.op("dve", lambda e, l=l, pT=pT: e.tensor_copy(out=modT[:, l, :, :], in_=pT[:, 0:96].rearrange("p (j r) -> p j r", r=2)),
             r=[tT], w=[t_modT])
        k.reserved = set()
        dump(f"mod{l}", mod_d[l, :, :], [2, 6 * D], F32, [t_mod])
    S.barrier()

    def bcast_rows(l, which):
        gi = 2 if which == "mix" else 5
        li = 0 if which == "mix" else 1
        S.dma(bc[:, 0, :], mod_d[l, 0:1, gi * D:(gi + 1) * D].to_broadcast([128, D]), r=[t_mod], w=[t_bc[0]])
        S.dma(bc[:, 1, :], mod_d[l, 1:2, gi * D:(gi + 1) * D].to_broadcast([128, D]), r=[t_mod], w=[t_bc[1]])
        S.dma(bc[:, 2, :], lng_d[l, li:li + 1, :].to_broadcast([128, D]), w=[t_bc[2]])
        S.dma(bc[:, 3, :], lnb_d[l, li:li + 1, :].to_broadcast([128, D]), w=[t_bc[3]])

    def ln_stats(src, t_src, st_ap, mv_ap, rs_ap, nb_ap, t_st):
        for j in range(2):
            S.op("dve", lambda e, j=j: e.bn_stats(out=st_ap[:, j, :], in_=src[:, j * 512:(j + 1) * 512]), r=[t_src], w=[t_st])
        S.op("dve", lambda e: e.bn_aggr(out=mv_ap, in_=st_ap), r=[t_st], w=[t_st])
        S.op("act", lambda e: e.activation(out=rs_ap, in_=mv_ap[:, 1:2], func=AF.Sqrt, bias=cst[:, 0:1]), r=[t_st, t_c], w=[t_st])
        S.op("dve", lambda e: e.reciprocal(out=rs_ap, in_=rs_ap), r=[t_st], w=[t_st])
        S.op("dve", lambda e: e.tensor_scalar(out=nb_ap, in0=mv_ap[:, 0:1], scalar1=rs_ap, scalar2=-1.0, op0=ALU.mult, op1=ALU.mult),
             r=[t_st], w=[t_st])

    k.st_i = 0

    def st_slot():
        i = k.st_i
        k.st_i = (i + 1) % 8
        base = i * 24
        return (sm[:, base:base + 12].rearrange("p (a b) -> p a b", b=6), sm[:, base + 12:base + 14],
                sm[:, base + 14:base + 15], sm[:, base + 15:base + 16], k.t_st[i])
    k.t_st = [Trk() for _ in range(8)]

    def ln_part1(src, t_src, xn, t_xn):
        st_ap, mv_ap, rs_ap, nb_ap, t_st = st_slot()
        ln_stats(src, t_src, st_ap, mv_ap, rs_ap, nb_ap, t_st)
        S.op("act", lambda e: e.activation(out=xn, in_=src, func=AF.Identity, scale=rs_ap, bias=nb_ap), r=[t_src, t_st], w=[t_xn])

    def ln_part2(l, xn, t_xn, i, which, router, uf=None, t_uf=None):
        r_ = 1 if i < 2 else 0
        pp, tp = pair()
        for kk in range(KC):
            S.op("pe", lambda e, kk=kk: e.transpose(out=pp[:, kk * 128:(kk + 1) * 128], in_=xn[:, kk * 128:(kk + 1) * 128],
                                                    identity=identF[:]), r=[t_xn, t_c], w=[tp[kk // 4]])
        for kk in range(KC):
            sc_ap = modT[:, l, (which + 1) * 8 + kk, r_:r_ + 1]
            sh_ap = modT[:, l, which * 8 + kk, r_:r_ + 1]
            if router:
                o_ap = uf[:, kk, :]
                tw = t_uf
            else:
                o_ap = uT[:, kk, i * 128:(i + 1) * 128]
                tw = t_uT[i]
            if kk % 2 == 0:
                S.op("act", lambda e, kk=kk, o_ap=o_ap, sc_ap=sc_ap, sh_ap=sh_ap: e.activation(
                    out=o_ap, in_=pp[:, kk * 128:(kk + 1) * 128], func=AF.Identity, scale=sc_ap, bias=sh_ap),
                    r=[tp[kk // 4], t_modT], w=[tw])
            else:
                S.op("dve", lambda e, kk=kk, o_ap=o_ap, sc_ap=sc_ap, sh_ap=sh_ap: e.tensor_scalar(
                    out=o_ap, in0=pp[:, kk * 128:(kk + 1) * 128], scalar1=sc_ap, scalar2=sh_ap, op0=ALU.mult, op1=ALU.add),
                    r=[tp[kk // 4], t_modT], w=[tw])
        if router:
            S.op("pool", lambda e: e.tensor_copy(out=uT[:, :, i * 128:(i + 1) * 128], in_=uf[:, :, :]), r=[t_uf], w=[t_uT[i]])

    def ln_to_uT(l, src, t_src, xn, t_xn, i, which, router, uf=None, t_uf=None):
        ln_part1(src, t_src, xn, t_xn)
        ln_part2(l, xn, t_xn, i, which, router, uf, t_uf)
        if router:
            route(i, uf, t_uf)

    def route(i, uf, t_uf):
        pb, tb = bank()
        for kk in range(KC):
            S.op("pe", lambda e, kk=kk: e.matmul(pb[:, 0:16], lhsT=uf[:, kk, :], rhs=wrt[:, kk, :], start=(kk == 0),
                                                 stop=(kk == KC - 1)), r=[t_uf, t_wr], w=[tb])
        base = 192 + (i % 2) * 160
        t_r = k.t_route[i % 2]
        lg = sm[:, base:base + 16]
        pr = sm[:, base + 16:base + 32]
        sl = sm[:, base + 32:base + 48]
        s2 = sm[:, base + 48:base + 64]
        eq = sm[:, base + 64:base + 80]
        g4 = sm[:, base + 80:base + 84]
        g4b = sm[:, base + 84:base + 88]
        g4c = sm[:, base + 88:base + 92]
        sc1 = sm[:, base + 92:base + 93]
        sc2 = sm[:, base + 93:base + 94]
        eq2 = sm[:, base + 96:base + 112]
        BIG = 1.0e4

        def dv(fn, extra_r=()):
            S.op("dve", fn, r=[t_r] + list(extra_r), w=[t_r])
        S.op("dve", lambda e: e.tensor_copy(out=lg, in_=pb[:, 0:16]), r=[tb], w=[t_r])
        dv(lambda e: e.tensor_reduce(out=sc1, in_=lg, axis=AX.X, op=ALU.max))
        dv(lambda e: e.tensor_scalar(out=sc1, in0=sc1, scalar1=-1.0, scalar2=None, op0=ALU.mult))
        S.op("act", lambda e: e.activation(out=pr, in_=lg, func=AF.Exp, bias=sc1, scale=1.0), r=[t_r], w=[t_r])
        dv(lambda e: e.tensor_reduce(out=sc2, in_=pr, axis=AX.X, op=ALU.add))
        dv(lambda e: e.reciprocal(out=sc2, in_=sc2))
        dv(lambda e: e.tensor_scalar(out=pr, in0=pr, scalar1=sc2, scalar2=None, op0=ALU.mult))
        dv(lambda e: e.tensor_tensor(out=sl, in0=pr, in1=brb[:], op=ALU.add), extra_r=[t_c])
        sl3 = sl.rearrange("p (g e) -> p g e", e=4)
        s23 = s2.rearrange("p (g e) -> p g e", e=4)
        eq3 = eq.rearrange("p (g e) -> p g e", e=4)
        dv(lambda e: e.tensor_reduce(out=g4, in_=sl3, axis=AX.X, op=ALU.max))
        dv(lambda e: e.tensor_tensor(out=eq3, in0=sl3, in1=g4.unsqueeze(2).to_broadcast([128, 4, 4]), op=ALU.is_equal))
        dv(lambda e: e.scalar_tensor_tensor(out=s2, in0=eq, scalar=-BIG, in1=sl, op0=ALU.mult, op1=ALU.add))
        dv(lambda e: e.tensor_reduce(out=g4b, in_=s23, axis=AX.X, op=ALU.max))
        dv(lambda e: e.tensor_tensor(out=g4, in0=g4, in1=g4b, op=ALU.add))
        dv(lambda e: e.tensor_reduce(out=sc1, in_=g4, axis=AX.X, op=ALU.max))
        dv(lambda e: e.tensor_scalar(out=g4c, in0=g4, scalar1=sc1, scalar2=None, op0=ALU.is_equal))
        dv(lambda e: e.tensor_scalar(out=g4c, in0=g4c, scalar1=-1.0, scalar2=BIG, op0=ALU.add, op1=ALU.mult))
        dv(lambda e: e.tensor_tensor(out=s23, in0=sl3, in1=g4c.unsqueeze(2).to_broadcast([128, 4, 4]), op=ALU.add))
        dv(lambda e: e.tensor_reduce(out=sc1, in_=s2, axis=AX.X, op=ALU.max))
        dv(lambda e: e.tensor_scalar(out=eq, in0=s2, scalar1=sc1, scalar2=None, op0=ALU.is_equal))
        dv(lambda e: e.scalar_tensor_tensor(out=s2, in0=eq, scalar=-BIG, in1=s2, op0=ALU.mult, op1=ALU.add))
        dv(lambda e: e.tensor_reduce(out=sc1, in_=s2, axis=AX.X, op=ALU.max))
        dv(lambda e: e.tensor_scalar(out=eq2, in0=s2, scalar1=sc1, scalar2=None, op0=ALU.is_equal))
        dv(lambda e: e.tensor_tensor(out=eq, in0=eq, in1=eq2, op=ALU.add))
        dv(lambda e: e.tensor_tensor(out=eq, in0=eq, in1=pr, op=ALU.mult))
        dv(lambda e: e.tensor_reduce(out=sc2, in_=eq, axis=AX.X, op=ALU.add))
        dv(lambda e: e.reciprocal(out=sc2, in_=sc2))
        S.op("dve", lambda e: e.tensor_scalar(out=gates[:, i, :], in0=eq, scalar1=sc2, scalar2=None, op0=ALU.mult),
             r=[t_r], w=[t_gates[i]])
    k.t_route = [Trk(), Trk()]
    k.t_tile = [Trk() for _ in range(12)]

    k.wslot = 0

    def load_w(src_ap, nslots=1, parts=128):
        s0 = k.wslot
        if s0 + nslots > 5:
            s0 = 0
        k.wslot = (s0 + nslots) % 5
        a, b = src_ap.shape[1], src_ap.shape[2]
        dst = wbuf[0:parts, s0:s0 + nslots, :].rearrange("p s n -> p (s n)")[:, 0:a * b].rearrange("p (a b) -> p a b", b=b)
        trks = t_wb[s0:s0 + nslots]
        S.dma(dst, src_ap, w=trks, q="pool")
        return dst, trks

    def proj_fm(wv, t_w, c0, M, evac, toks=TOKB):
        for (t0, nt) in toks:
            pb, tb = bank()
            for kk in range(KC):
                S.op("pe", lambda e, kk=kk, pb=pb, t0=t0, nt=nt: e.matmul(pb[0:M, 0:nt], lhsT=wv[:, kk, c0:c0 + M],
                                                                          rhs=uT[:, kk, t0:t0 + nt], start=(kk == 0), stop=(kk == KC - 1)),
                     r=t_w + t_uT[t0 // 128:(t0 + nt) // 128], w=[tb])
            evac(pb, tb, t0, nt)

    def proj_tm(wv, t_w, c0, ncol, evac, tiles=range(NT)):
        for i in tiles:
            pb, tb = bank()
            for kk in range(KC):
                S.op("pe", lambda e, kk=kk, pb=pb, i=i: e.matmul(pb[:, 0:ncol], lhsT=uT[:, kk, i * 128:(i + 1) * 128],
                                                                 rhs=wv[:, kk, c0:c0 + ncol], start=(kk == 0), stop=(kk == KC - 1)),
                     r=t_w + [t_uT[i]], w=[tb])
            evac(pb, tb, i)

    k.proj_fm = proj_fm
    k.proj_tm = proj_tm
    k.load_w = load_w
    k.bank = bank
    k.pair = pair
    k.dump = dump
    k.ln_to_uT = ln_to_uT
    k.ln_part1 = ln_part1
    k.ln_part2 = ln_part2
    k.route = route
    k.ln_stats = ln_stats
    k.st_slot = st_slot
    k.bcast_rows = bcast_rows
    for nm in ("x_d ctx_d w0_d sink_d lbl_d hn_d wo0_d w1_d gw_d gb_d gn_d wo1_d eg_d eu_d ed_d out_d hmid_d hout0_d mix_d "
               "t_hmid t_hout0 t_mixd t_out identF identB onesF onesB cst Mf Mb MP MN rmask modT t_modT mod_d t_mod bc t_bc uT t_uT "
               "wbuf t_wb RB t_rb HB t_hb sm gates t_gates t_c PS PT").split():
        setattr(k, nm, locals()[nm])

    for ph, l in [("B", 0), ("M", 0), ("D", 0), ("E", 0), ("B", 1), ("M", 1), ("D", 1), ("E", 1)]:
        if ph == "B":
            phase_B(k, l)
        elif ph == "M":
            (mixer_even if l == 0 else mixer_odd)(k)
        elif ph == "D":
            phase_D(k, l)
        else:
            phase_E(k, l)
        S.barrier()
        if stop is not None and stop[0:2] == f"{ph}{l}":
            break

    S.wait_all("sp", t_out + k.dbg_out)
    S.emit()
    S.close()
    return nc


def phase_B(k, l):
    S = k.S
    bufs = {}

    def stA(i):
        slot = i % 2
        ht = k.RB[:, slot, 0:1024]
        t_ht = k.t_tile[slot]
        xn = k.RB[:, 2 + slot, 0:1024]
        t_xn = k.t_tile[2 + slot]
        if l == 0:
            src = k.ctx_d[i * 128:(i + 1) * 128, :] if i < 2 else k.x_d[(i - 2) * 128:(i - 1) * 128, :]
            S.dma(ht, src, w=[t_ht])
        else:
            S.dma(ht, k.hout0_d[i * 128:(i + 1) * 128, :], r=[k.t_hout0[i]], w=[t_ht])
        k.ln_part1(ht, t_ht, xn, t_xn)
        bufs[i] = (xn, t_xn)

    def stB(i):
        xn, t_xn = bufs.pop(i)
        k.ln_part2(l, xn, t_xn, i, 0, False)
    for n in range(NT + 1):
        if n < NT:
            stA(n)
        if n >= 1:
            stB(n - 1)
    k.dump(f"uT{l}", k.uT[:, :, :], [128, KC, N], BF16, k.t_uT)


def row_to_col(k, src, nr, n, dst, t_src, t_dst):
    S = k.S
    pb, tb = k.bank()
    S.op("pe", lambda e: e.transpose(out=pb[0:n, 0:nr], in_=src, identity=k.identF[0:nr, 0:nr]), r=[t_src, k.t_c], w=[tb])
    S.op("dve", lambda e: e.tensor_copy(out=dst, in_=pb[0:n, 0:nr]), r=[tb], w=[t_dst])


def proj_block(k, wv, t_w, c0, M, t0, nt):
    S = k.S
    pb, tb = k.bank()
    for kk in range(KC):
        S.op("pe", lambda e, kk=kk: e.matmul(pb[0:M, 0:nt], lhsT=wv[:, kk, c0:c0 + M], rhs=k.uT[:, kk, t0:t0 + nt],
                                             start=(kk == 0), stop=(kk == KC - 1)),
             r=t_w + k.t_uT[t0 // 128:(t0 + nt) // 128], w=[tb])
    return pb, tb


def gated_scan(k, dk, dvh, nh, q_ap, t_q, k_ap, t_k, A, t_A, B, t_B, make_logf, v_fn, t_v, o_acc, t_o, qts, t_qts, kts, t_kts):
    S = k.S
    sc = k.scn
    NC_ = N // 64
    dvt = dvh * nh
    B3 = B.rearrange("p (c l) -> p c l", l=64)
    for a in range(nh):
        S.op("pool", lambda e, a=a: e.memset(o_acc[a], 0.0), w=[t_o[a]])
    D_ = []
    for d in range(2):
        qt, kt, t_qt, t_kt = qts[d], kts[d], t_qts[d], t_kts[d]
        t_sc = k.t_scn[d]
        rr = sc["rr"][0:dk, d, :]
        gg = sc["gg"][0:dk, d, :]
        X1 = sc["X1"][0:dk, d, :]
        X2 = sc["X2"][0:dk, d, :]
        EG = sc["EG"][0:dk, d, :]
        make_logf(d)
        S.op("dve", lambda e: e.tensor_tensor_scan(out=B, data0=k.rmask[0:dk, :], data1=A, initial=0.0, op0=ALU.mult, op1=ALU.add),
             r=[t_A, k.t_c], w=[t_B])
        S.op("pool", lambda e, gg=gg: e.tensor_copy(out=gg, in_=B3[:, :, 63]), r=[t_B], w=[t_sc])
        if d == 1:
            S.op("pool", lambda e: e.tensor_tensor(out=B, in0=B, in1=A, op=ALU.subtract), r=[t_B, t_A], w=[t_B])
        S.op("pool", lambda e, rr=rr: e.tensor_copy(out=rr, in_=B3[:, :, 32]), r=[t_B], w=[t_sc])
        S.op("dve", lambda e, rr=rr: e.tensor_tensor(out=B3, in0=B3, in1=rr.unsqueeze(2).to_broadcast([dk, NC_, 64]), op=ALU.subtract),
             r=[t_B, t_sc], w=[t_B])
        sgn = 1.0 if d == 0 else -1.0
        S.op("act", lambda e, sgn=sgn: e.activation(out=A, in_=B, func=AF.Exp, scale=sgn), r=[t_B], w=[t_A])
        S.op("dve", lambda e, qt=qt: e.tensor_tensor(out=qt, in0=q_ap, in1=A, op=ALU.mult), r=[t_q, t_A], w=[t_qt])
        S.op("act", lambda e, sgn=sgn: e.activation(out=A, in_=B, func=AF.Exp, scale=-sgn), r=[t_B, t_qt], w=[t_A])
        S.op("dve", lambda e, kt=kt: e.tensor_tensor(out=kt, in0=k_ap, in1=A, op=ALU.mult), r=[t_k, t_A], w=[t_kt])
        S.op("act", lambda e, X1=X1, rr=rr: e.activation(out=X1, in_=rr, func=AF.Exp), r=[t_sc], w=[t_sc])
        S.op("act", lambda e, EG=EG, gg=gg: e.activation(out=EG, in_=gg, func=AF.Exp), r=[t_sc], w=[t_sc])
        S.op("dve", lambda e, X2=X2, gg=gg, rr=rr: e.tensor_tensor(out=X2, in0=gg, in1=rr, op=ALU.subtract), r=[t_sc], w=[t_sc])
        S.op("act", lambda e, X2=X2: e.activation(out=X2, in_=X2, func=AF.Exp), r=[t_sc], w=[t_sc])
        a_s, c_s = (X1, X2) if d == 0 else (X2, X1)
        if d == 0:
            tiles = [(i, (0, 1)) for i in range(NT)]
        else:
            tiles = [(i, (1, 0)) for i in (1, 0)] + [(i, (1, 0)) for i in range(NT - 1, 1, -1)]
        st = {"d": d, "qt": qt, "kt": kt, "t_qt": t_qt, "t_kt": t_kt, "t_sc": t_sc, "EG": EG, "a_s": a_s, "c_s": c_s,
              "M": k.Mf if d == 0 else k.Mb, "tiles": tiles, "s_i": 0, "sb_i": 0, "am_i": 0, "pend": None, "nchunk": 0, "ds_i": 0}
        S.op("pool", lambda e, d=d: e.memset(sc["Sst"][0:dk, d, 0, 0:dvt], 0.0), w=[k.t_Sst[d][0]])
        S.op("pool", lambda e, d=d: e.memset(sc["Sbf"][0:dk, d, 0, 0:dvt], 0.0), w=[k.t_Sbf[d][0]])
        D_.append(st)

    def stage1(st, i):
        d = st["d"]
        tk0 = i * 128
        vt = v_fn(i)
        am_i = st["am_i"]
        st["am_i"] = 1 - am_i
        Am = sc["Am"][:, d, am_i, :]
        ktok = sc["ktok"][:, d, am_i, 0:dk]
        t_Am = k.t_Am2[d][am_i]
        t_kk = k.t_ktok2[d][am_i]
        qt, kt = st["qt"], st["kt"]
        pA, tA = k.bank()
        S.op("pe", lambda e: e.matmul(pA[:, 0:128], lhsT=kt[:, tk0:tk0 + 128], rhs=qt[:, tk0:tk0 + 128], start=True, stop=True),
             r=[st["t_kt"], st["t_qt"]], w=[tA])
        M_ = st["M"]
        S.op("dve", lambda e: e.tensor_tensor(out=Am, in0=pA[:, 0:128], in1=M_[:], op=ALU.mult), r=[tA, k.t_c], w=[t_Am])
        pK, tK = k.bank()
        S.op("pe", lambda e: e.matmul(pK[:, 0:dk], lhsT=kt[:, tk0:tk0 + 128], rhs=k.identB[0:dk, 0:dk], start=True, stop=True),
             r=[st["t_kt"], k.t_c], w=[tK])
        S.op("act", lambda e: e.activation(out=ktok, in_=pK[:, 0:dk], func=AF.Copy), r=[tK], w=[t_kk])
        pI, tI = k.bank()
        for a in range(nh):
            S.op("pe", lambda e, a=a: e.matmul(pI[0:dvh, a * 128:(a + 1) * 128], lhsT=vt[:, a * dvh:(a + 1) * dvh], rhs=Am[:, :], start=True, stop=True),
                 r=t_v + [t_Am], w=[tI])
        pS = [None, None]
        c_s = st["c_s"]
        for hf in range(2):
            pS_, tS_ = k.bank()
            rows = slice(hf * 64, hf * 64 + 64)
            S.op("pe", lambda e, pS_=pS_, rows=rows: e.matmul(pS_[0:dk, 0:dvt], lhsT=ktok[rows, :], rhs=vt[rows, 0:dvt], start=True, stop=True),
                 r=[t_kk] + t_v, w=[tS_])
            ds_i = st["ds_i"]
            st["ds_i"] = (ds_i + 1) % 4
            dSs = sc["dSs"][0:dk, d, ds_i, 0:dvt]
            c = 2 * i + hf
            S.op("act", lambda e, pS_=pS_, dSs=dSs, c=c: e.activation(out=dSs, in_=pS_[0:dk, 0:dvt], func=AF.Identity, scale=c_s[:, c:c + 1]),
                 r=[tS_, st["t_sc"]], w=[k.t_dSs[d][ds_i]])
            pS[hf] = (dSs, k.t_dSs[d][ds_i])
        for a in range(nh):
            S.op("dve", lambda e, a=a: e.tensor_tensor(out=o_acc[a][:, tk0:tk0 + 128], in0=pI[0:dvh, a * 128:(a + 1) * 128],
                                                       in1=o_acc[a][:, tk0:tk0 + 128], op=ALU.add), r=[tI, t_o[a]], w=[t_o[a]])
        st["pend"] = (i, pS)

    def stage2_chunk(st, i, hf, pS, po, tpo, last):
        d = st["d"]
        c = 2 * i + hf
        tok0 = c * 64
        qt = st["qt"]
        sb_i = st["sb_i"]
        Sb = sc["Sbf"][0:dk, d, sb_i, 0:dvt]
        for a in range(nh):
            S.op("pe", lambda e, a=a: e.matmul(po[0:dvh, a * 128 + hf * 64:a * 128 + hf * 64 + 64], lhsT=Sb[:, a * dvh:(a + 1) * dvh],
                                               rhs=qt[:, tok0:tok0 + 64], start=True, stop=True), r=[k.t_Sbf[d][sb_i], st["t_qt"]], w=[tpo])
        if last:
            return
        s_i = st["s_i"]
        Sc = sc["Sst"][0:dk, d, s_i, 0:dvt]
        Sn = sc["Sst"][0:dk, d, 1 - s_i, 0:dvt]
        EG, a_s = st["EG"], st["a_s"]
        dSs, t_dS = pS[hf]
        S.op("dve", lambda e: e.scalar_tensor_tensor(out=Sn, in0=Sc, scalar=EG[:, c:c + 1], in1=dSs, op0=ALU.mult, op1=ALU.add),
             r=[k.t_Sst[d][s_i], t_dS, st["t_sc"]], w=[k.t_Sst[d][1 - s_i]])
        st["s_i"] = 1 - s_i
        n_ = st["nchunk"] + 1
        ti, (h0, h1) = st["tiles"][n_ // 2]
        cn = 2 * ti + (h0 if n_ % 2 == 0 else h1)
        Sbn = sc["Sbf"][0:dk, d, 1 - sb_i, 0:dvt]
        S.op("act", lambda e: e.activation(out=Sbn, in_=Sn, func=AF.Identity, scale=a_s[:, cn:cn + 1]),
             r=[k.t_Sst[d][1 - s_i], st["t_sc"]], w=[k.t_Sbf[d][1 - sb_i]])
        st["sb_i"] = 1 - sb_i

    nT = NT
    for step in range(nT + 1):
        if step < nT:
            for st in D_:
                stage1(st, st["tiles"][step][0])
        if step >= 1:
            pend = []
            for st in D_:
                i, (h0, h1) = st["tiles"][step - 1]
                po, tpo = k.bank()
                pend.append((st, i, (h0, h1), po, tpo))
            for which in range(2):
                for (st, i, hfs, po, tpo) in pend:
                    pS = st["pS_prev"]
                    last = (st["nchunk"] == 2 * nT - 1)
                    stage2_chunk(st, i, hfs[which], pS, po, tpo, last)
                    st["nchunk"] += 1
            for (st, i, hfs, po, tpo) in pend:
                tk0 = i * 128
                for a in range(nh):
                    S.op("dve", lambda e, a=a, po=po, tk0=tk0: e.tensor_tensor(out=o_acc[a][:, tk0:tk0 + 128], in0=po[0:dvh, a * 128:(a + 1) * 128],
                                                                               in1=o_acc[a][:, tk0:tk0 + 128], op=ALU.add), r=[tpo, t_o[a]], w=[t_o[a]])
        for st in D_:
            if st["pend"] is not None:
                st["pS_prev"] = st["pend"][1]


def rms_gate_out(k, dvh, nh, o_acc, t_o, A, t_A, B, t_B, gsil, t_gs, gn_cols, t_gn, mixrow, t_mix, chunk_ids):
    S = k.S
    dv = dvh * nh
    for (t0, nt) in TOKB:
        pb, tb = k.bank()
        for a in range(nh):
            S.op("act", lambda e, a=a, t0=t0, nt=nt: e.activation(out=A[0:dvh, a * 512:a * 512 + nt], in_=o_acc[a][:, t0:t0 + nt], func=AF.Square),
                 r=[t_o[a]], w=[t_A])
        for a in range(nh):
            S.op("pe", lambda e, a=a, pb=pb, nt=nt: e.matmul(pb[0:dvh, 0:nt], lhsT=k.onesF[0:dvh, 0:dvh], rhs=A[0:dvh, a * 512:a * 512 + nt],
                                                             start=(a == 0), stop=(a == nh - 1)), r=[t_A, k.t_c], w=[tb])
        S.op("act", lambda e, pb=pb, nt=nt: e.activation(out=B[0:dvh, 0:nt], in_=pb[0:dvh, 0:nt], func=AF.Sqrt, scale=1.0 / dv, bias=k.cst[0:dvh, 0:1]),
             r=[tb, k.t_c], w=[t_B])
        S.op("dve", lambda e, nt=nt: e.reciprocal(out=B[0:dvh, 0:nt], in_=B[0:dvh, 0:nt]), r=[t_B], w=[t_B])
        for a in range(nh):
            S.op("dve", lambda e, a=a, t0=t0, nt=nt: e.tensor_tensor(out=B[0:dvh, 512 + a * 512:512 + a * 512 + nt], in0=o_acc[a][:, t0:t0 + nt],
                                                                     in1=B[0:dvh, 0:nt], op=ALU.mult), r=[t_o[a], t_B], w=[t_B])
            S.op("dve", lambda e, a=a, t0=t0, nt=nt: e.scalar_tensor_tensor(out=mixrow[a][0:dvh, t0:t0 + nt], in0=B[0:dvh, 512 + a * 512:512 + a * 512 + nt],
                                                                            scalar=gn_cols[a], in1=gsil[a][0:dvh, t0:t0 + nt], op0=ALU.mult, op1=ALU.mult),
                 r=[t_B, t_gn, t_gs[a]], w=[t_mix[a]])
    for a in range(nh):
        S.dma(k.mix_d[chunk_ids[a], 0:dvh, :], mixrow[a][0:dvh, :], r=[t_mix[a]], w=[k.t_mixd[chunk_ids[a]]])


def scan_setup(k):
    S = k.S
    if hasattr(k, "scn"):
        return
    sc = {}
    for nm in ("rr", "gg", "X1", "X2", "EG"):
        sc[nm] = S.sbuf("sc_" + nm, [128, 2, 36], F32)
    sc["Sst"] = S.sbuf("sc_Sst", [128, 2, 2, 192], F32)
    sc["dSs"] = S.sbuf("sc_dSs", [128, 2, 4, 192], BF16)
    sc["Sbf"] = S.sbuf("sc_Sbf", [128, 2, 2, 192], BF16)
    sc["Am"] = S.sbuf("sc_Am", [128, 2, 2, 128], BF16)
    sc["ktok"] = S.sbuf("sc_ktok", [128, 2, 2, 128], BF16)
    sc["col"] = S.sbuf("sc_col", [128, 64], F32)
    sc["row"] = k.RB[0:8, 5, 0:768]
    k.scn = sc
    k.t_scn = [Trk(), Trk()]
    k.t_Sst = [[Trk(), Trk()], [Trk(), Trk()]]
    k.t_dSs = [[Trk() for _ in range(4)], [Trk() for _ in range(4)]]
    k.t_Sbf = [[Trk(), Trk()], [Trk(), Trk()]]
    k.t_Am2 = [[Trk(), Trk()], [Trk(), Trk()]]
    k.t_ktok2 = [[Trk(), Trk()], [Trk(), Trk()]]
    k.t_Am = [k.t_Am2[0][0], k.t_Am2[0][1]]
    k.t_col = Trk()
    k.t_row = k.t_rb[5]


ATOK = [(0, 256), (256, 512), (768, 512), (1280, 512), (1792, 512)]


def mixer_even(k):
    S = k.S
    scan_setup(k)
    sc = k.scn
    RB, HB, t_rb, t_hb = k.RB, k.HB, k.t_rb, k.t_hb
    Ct = RB[:, 4, 0:2048]
    St = RB[:, 5, 0:2048]
    col = sc["col"]
    t_col = k.t_col
    ci = col[:, 0:8].bitcast(I32)
    S.op("pool", lambda e: e.iota(ci[:, 0:1], pattern=[[0, 1]], base=0, channel_multiplier=1), w=[t_col])
    S.op("dve", lambda e: e.tensor_single_scalar(out=ci[:, 1:2], in_=ci[:, 0:1], scalar=15, op=ALU.bitwise_and), r=[t_col], w=[t_col])
    S.op("dve", lambda e: e.tensor_scalar(out=ci[:, 2:3], in0=ci[:, 0:1], scalar1=5, scalar2=1, op0=ALU.logical_shift_right, op1=ALU.bitwise_and),
         r=[t_col], w=[t_col])
    S.op("dve", lambda e: e.tensor_scalar(out=ci[:, 3:4], in0=ci[:, 0:1], scalar1=4, scalar2=1, op0=ALU.logical_shift_right, op1=ALU.bitwise_and),
         r=[t_col], w=[t_col])
    S.op("dve", lambda e: e.tensor_copy(out=col[:, 8:11], in_=ci[:, 1:4]), r=[t_col], w=[t_col])
    S.op("act", lambda e: e.activation(out=col[:, 11:12], in_=col[:, 8:9], func=AF.Exp, scale=-math.log(10000.0) / 16.0), r=[t_col], w=[t_col])
    S.op("dve", lambda e: e.tensor_tensor(out=col[:, 13:14], in0=col[:, 11:12], in1=col[:, 9:10], op=ALU.mult), r=[t_col], w=[t_col])
    S.op("dve", lambda e: e.tensor_tensor(out=col[:, 12:13], in0=col[:, 11:12], in1=col[:, 13:14], op=ALU.subtract), r=[t_col], w=[t_col])
    S.op("dve", lambda e: e.tensor_scalar(out=col[:, 14:15], in0=col[:, 10:11], scalar1=2.0, scalar2=-1.0, op0=ALU.mult, op1=ALU.add),
         r=[t_col], w=[t_col])
    ri = RB[:, 0, 0:2048].bitcast(I32)
    qi = RB[:, 1, 0:2048].bitcast(I32)
    S.op("pool", lambda e: e.iota(ri, pattern=[[1, 32], [0, 64]], base=0, channel_multiplier=0), w=[t_rb[0]])
    S.op("pool", lambda e: e.iota(qi, pattern=[[0, 32], [1, 64]], base=0, channel_multiplier=0), w=[t_rb[1]])
    rf = RB[:, 2, 0:2048]
    qf = RB[:, 3, 0:2048]
    S.op("dve", lambda e: e.tensor_copy(out=rf, in_=ri), r=[t_rb[0]], w=[t_rb[2]])
    S.op("dve", lambda e: e.tensor_copy(out=qf, in_=qi), r=[t_rb[1]], w=[t_rb[3]])
    ang = RB[:, 0, 0:2048]
    S.op("dve", lambda e: e.tensor_scalar(out=ang, in0=rf, scalar1=col[:, 12:13], scalar2=None, op0=ALU.mult), r=[t_rb[2], t_col], w=[t_rb[0]])
    S.op("dve", lambda e: e.scalar_tensor_tensor(out=ang, in0=qf, scalar=col[:, 13:14], in1=ang, op0=ALU.mult, op1=ALU.add),
         r=[t_rb[3], t_rb[0], t_col], w=[t_rb[0]])
    def range_reduce(dst, t_dst, add, tmpi, t_tmpi, tmpf_, t_tmpf):
        S.op("dve", lambda e: e.tensor_scalar(out=tmpi, in0=ang, scalar1=add, scalar2=1.0 / (2 * PI), op0=ALU.add, op1=ALU.mult),
             r=[t_rb[0]], w=[t_tmpi])
        S.op("dve", lambda e: e.tensor_copy(out=tmpf_, in_=tmpi), r=[t_tmpi], w=[t_tmpf])
        S.op("dve", lambda e: e.scalar_tensor_tensor(out=dst, in0=tmpf_, scalar=-2 * PI, in1=ang, op0=ALU.mult, op1=ALU.add),
             r=[t_tmpf, t_rb[0]], w=[t_dst])
        if add != 0.0:
            S.op("dve", lambda e: e.tensor_scalar(out=dst, in0=dst, scalar1=add, scalar2=None, op0=ALU.add), r=[t_dst], w=[t_dst])
        S.op("dve", lambda e: e.tensor_scalar(out=tmpf_, in0=dst, scalar1=PI, scalar2=-2 * PI, op0=ALU.is_gt, op1=ALU.mult),
             r=[t_dst], w=[t_tmpf])
        S.op("dve", lambda e: e.tensor_tensor(out=dst, in0=dst, in1=tmpf_, op=ALU.add), r=[t_dst, t_tmpf], w=[t_dst])
        S.op("dve", lambda e: e.tensor_scalar(out=tmpf_, in0=dst, scalar1=-PI, scalar2=2 * PI, op0=ALU.is_lt, op1=ALU.mult),
             r=[t_dst], w=[t_tmpf])
        S.op("dve", lambda e: e.tensor_tensor(out=dst, in0=dst, in1=tmpf_, op=ALU.add), r=[t_dst, t_tmpf], w=[t_dst])
        S.op("dve", lambda e: e.tensor_scalar(out=dst, in0=dst, scalar1=PI, scalar2=-PI, op0=ALU.min, op1=ALU.max), r=[t_dst], w=[t_dst])
    m1 = RB[:, 1, 0:2048]
    tmpi = RB[:, 2, 0:2048].bitcast(I32)
    tmpf_ = RB[:, 3, 0:2048]
    range_reduce(m1, t_rb[1], 0.0, tmpi, t_rb[2], tmpf_, t_rb[3])
    S.op("act", lambda e: e.activation(out=St, in_=m1, func=AF.Sin, scale=col[:, 14:15]), r=[t_rb[1], t_col], w=[t_rb[5]])
    range_reduce(m1, t_rb[1], PI / 2, tmpi, t_rb[2], tmpf_, t_rb[3])
    S.op("act", lambda e: e.activation(out=Ct, in_=m1, func=AF.Sin), r=[t_rb[1]], w=[t_rb[4]])
    k.dump("ropeC", Ct, [128, 2048], F32, [t_rb[4]])
    k.dump("ropeS", St, [128, 2048], F32, [t_rb[5]])
    if k.sub == "M0a":
        return
    S.dma(col[:, 16:24], k.sink_d[0:1, :].to_broadcast([128, 8]), w=[t_col])
    S.op("act", lambda e: e.activation(out=col[:, 16:24], in_=col[:, 16:24], func=AF.Exp), r=[t_col], w=[t_col])

    qT = HB[:, 0:2, :]
    kAB = [HB[:, 2, :], HB[:, 3, :]]
    vdup = HB[:, 4, :].rearrange("p (i c) -> p i c", c=128)
    mixA = [HB[:, 5, :], HB[:, 6, :]]
    et = [HB[:, 7, ei * 512:(ei + 1) * 512] for ei in range(4)] + [RB[:, 3, 0:256].bitcast(BF16)]
    S.op("pool", lambda e: e.memset(kAB[0][64:128, :], 0.0), w=[t_hb[2]])
    S.op("pool", lambda e: e.memset(kAB[1][0:64, :], 0.0), w=[t_hb[3]])
    t_et = [Trk() for _ in range(5)]
    k.et_i = 0
    tmpf = [RB[:, 0, 0:512], RB[:, 0, 512:1024], RB[:, 1, 0:512], RB[:, 1, 512:1024]]
    t_tmp = [Trk() for _ in range(4)]
    dn = RB[:, 2, 0:512]
    t_dn = t_rb[2]
    wv_v, t_wv = k.load_w(k.w0_d[:, 1536:1664].rearrange("(k p) n -> p k n", p=128))
    for j in range(2):
        base = j * 768
        wq, t_wq = k.load_w(k.w0_d[:, base:base + 512].rearrange("(k p) n -> p k n", p=128))
        wk, t_wk = k.load_w(k.w0_d[:, base + 512:base + 768].rearrange("(k p) n -> p k n", p=128))
        tmp_i = 0
        for (t0, nt) in ATOK:
            for blk in range(3):
                if blk < 2:
                    pq, tq = proj_block(k, wq, t_wq, blk * 128, 128, t0, nt)
                    dst = qT[:, blk, t0:t0 + nt]
                    tdst = t_hb[blk]
                else:
                    pq, tq = proj_block(k, wk, t_wk, 0, 128, t0, nt)
                    dst = None
                if t0 == 0:
                    if blk < 2:
                        S.op("act", lambda e, pq=pq, dst=dst, nt=nt: e.activation(out=dst, in_=pq[:, 0:nt], func=AF.Copy), r=[tq], w=[tdst])
                    else:
                        S.op("act", lambda e, pq=pq, nt=nt, t0=t0: e.activation(out=kAB[0][0:64, t0:t0 + nt], in_=pq[0:64, 0:nt], func=AF.Copy), r=[tq], w=[t_hb[2]])
                        S.op("act", lambda e, pq=pq, nt=nt, t0=t0: e.activation(out=kAB[1][64:128, t0:t0 + nt], in_=pq[64:128, 0:nt], func=AF.Copy), r=[tq], w=[t_hb[3]])
                    continue
                if blk < 2:
                    ps_, ts_ = proj_block(k, wq, t_wq, 256 + blk * 128, 128, t0, nt)
                else:
                    ps_, ts_ = proj_block(k, wk, t_wk, 128, 128, t0, nt)
                l0 = t0 - 256
                ta, tb_ = tmp_i % 4, (tmp_i + 1) % 4
                tmp_i += 2
                S.op("dve", lambda e, pq=pq, ta=ta, l0=l0, nt=nt: e.tensor_tensor(out=tmpf[ta][:, 0:nt], in0=pq[:, 0:nt], in1=Ct[:, l0:l0 + nt], op=ALU.mult),
                     r=[tq, t_rb[4]], w=[t_tmp[ta]])
                S.op("dve", lambda e, ps_=ps_, tb_=tb_, l0=l0, nt=nt: e.tensor_tensor(out=tmpf[tb_][:, 0:nt], in0=ps_[:, 0:nt], in1=St[:, l0:l0 + nt], op=ALU.mult),
                     r=[ts_, t_rb[5]], w=[t_tmp[tb_]])
                if blk < 2:
                    S.op("pool", lambda e, dst=dst, ta=ta, tb_=tb_, nt=nt: e.tensor_tensor(out=dst, in0=tmpf[ta][:, 0:nt], in1=tmpf[tb_][:, 0:nt], op=ALU.add),
                         r=[t_tmp[ta], t_tmp[tb_]], w=[tdst])
                else:
                    S.op("pool", lambda e, ta=ta, tb_=tb_, nt=nt, t0=t0: e.tensor_tensor(out=kAB[0][0:64, t0:t0 + nt], in0=tmpf[ta][0:64, 0:nt], in1=tmpf[tb_][0:64, 0:nt], op=ALU.add),
                         r=[t_tmp[ta], t_tmp[tb_]], w=[t_hb[2]])
                    S.op("pool", lambda e, ta=ta, tb_=tb_, nt=nt, t0=t0: e.tensor_tensor(out=kAB[1][64:128, t0:t0 + nt], in0=tmpf[ta][64:128, 0:nt], in1=tmpf[tb_][64:128, 0:nt], op=ALU.add),
                         r=[t_tmp[ta], t_tmp[tb_]], w=[t_hb[3]])

        if k.sub == "M0p":
            k.dump("qT0", qT, [128, 2, N], BF16, [t_hb[0], t_hb[1]])
            return

        def ev_v(pb, tb, i, j=j):
            S.op("act", lambda e: e.activation(out=vdup[:, i, 0:64], in_=pb[:, j * 64:(j + 1) * 64], func=AF.Copy), r=[tb], w=[t_hb[4]])
            S.op("dve", lambda e: e.tensor_copy(out=vdup[:, i, 64:128], in_=pb[:, j * 64:(j + 1) * 64]), r=[tb], w=[t_hb[4]])
        k.proj_tm(wv_v, t_wv, 0, 128, ev_v)
        if j == 0:
            k.dump("qT0", qT, [128, 2, N], BF16, [t_hb[0], t_hb[1]])
            k.dump("kT0", HB[:, 2:4, :], [128, 2, N], BF16, [t_hb[2], t_hb[3]])
        if k.sub == "M0v":
            return
        for qb in range(NT):
            q0 = qb * 128
            if (k.sub == "M0q1" and qb == 1) or (k.sub == "M0q3" and qb == 3):
                k.dump("mixA", HB[:, 5:7, :], [128, 2, N], BF16, [t_hb[5], t_hb[6]])
                return
            if qb < 2:
                chunks = [(0, None), (1, None)]
            else:
                n_ = qb - 2
                chunks = [(0, None), (1, None)]
                if n_ > 0:
                    chunks.append((qb - 1, k.MP))
                chunks.append((qb, None))
                if n_ < 15:
                    chunks.append((qb + 1, k.MN))
            po, tpo = k.bank()
            pd, tpd = k.bank()
            nch = len(chunks)
            pss_l = []
            for ci_, (kc, msk) in enumerate(chunks):
                pss, tss = k.bank()
                for hh in range(4):
                    blk, half = hh // 2, hh % 2
                    S.op("pe", lambda e, pss=pss, hh=hh, blk=blk, half=half, kc=kc, q0=q0: e.matmul(
                        pss[:, hh * 128:(hh + 1) * 128], lhsT=kAB[half][:, kc * 128:(kc + 1) * 128], rhs=qT[:, blk, q0:q0 + 128],
                        start=True, stop=True), r=[t_hb[2 + half], t_hb[blk]], w=[tss])
                pss_l.append((pss, tss))
            for ci_, (kc, msk) in enumerate(chunks):
                pss, tss = pss_l[ci_]
                ei = ci_
                S.op("act", lambda e, pss=pss, ei=ei: e.activation(out=et[ei], in_=pss[:, :], func=AF.Exp, scale=0.125), r=[tss], w=[t_et[ei]])
                if msk is not None:
                    S.op("dve", lambda e, ei=ei, msk=msk: e.tensor_tensor(out=et[ei], in0=et[ei], in1=msk[:], op=ALU.mult),
                         r=[t_et[ei], k.t_c], w=[t_et[ei]])
            for ci_, (kc, msk) in enumerate(chunks):
                ei = ci_
                S.op("pe", lambda e, ei=ei, kc=kc, ci_=ci_, po=po, nch=nch: e.matmul(
                    po[:, :], lhsT=vdup[:, kc, :], rhs=et[ei][:, :], start=(ci_ == 0), stop=(ci_ == nch - 1)),
                    r=[t_hb[4], t_et[ei]], w=[tpo])
                S.op("pe", lambda e, ei=ei, ci_=ci_, pd=pd, nch=nch: e.matmul(
                    pd[:, :], lhsT=k.onesB[:], rhs=et[ei][:, :], start=(ci_ == 0), stop=(ci_ == nch - 1)),
                    r=[k.t_c, t_et[ei]], w=[tpd])
            if k.sub == "M0qb":
                S.op("dve", lambda e, po=po: e.tensor_copy(out=RB[:, 0, 0:512], in_=po[:, :]), r=[tpo], w=[t_rb[0]])
                S.op("dve", lambda e, pd=pd: e.tensor_copy(out=RB[:, 0, 512:1024], in_=pd[:, :]), r=[tpd], w=[t_rb[0]])
                k.dump("popd", RB[:, 0, 0:1024], [128, 1024], F32, [t_rb[0]])
                return
            for hh in range(4):
                S.op("dve", lambda e, hh=hh, pd=pd, j=j: e.tensor_scalar(out=dn[:, hh * 128:(hh + 1) * 128], in0=pd[:, hh * 128:(hh + 1) * 128],
                                                                    scalar1=col[:, 16 + 4 * j + hh:17 + 4 * j + hh], scalar2=None, op0=ALU.add),
                     r=[tpd, t_col], w=[t_dn])
            S.op("dve", lambda e: e.reciprocal(out=dn, in_=dn), r=[t_dn], w=[t_dn])
            for hh in range(4):
                blk, half = hh // 2, hh % 2
                rows = slice(half * 64, half * 64 + 64)
                S.op("dve", lambda e, hh=hh, blk=blk, rows=rows, po=po, q0=q0: e.tensor_tensor(
                    out=mixA[blk][rows, q0:q0 + 128], in0=po[rows, hh * 128:(hh + 1) * 128], in1=dn[rows, hh * 128:(hh + 1) * 128], op=ALU.mult),
                    r=[tpo, t_dn], w=[t_hb[5 + blk]])
        for blk in range(2):
            S.dma(k.mix_d[2 * j + blk, :, :], mixA[blk], r=[t_hb[5 + blk]], w=[k.t_mixd[2 * j + blk]])
    k.dump("mixd_att", k.mix_d[0:4, :, :], [4, 128, N], BF16, k.t_mixd[0:4])
    S.barrier()
    if k.sub == "M0b":
        return

    row = sc["row"]
    t_row = k.t_row
    S.dma(row[0:4, 0:512], k.lbl_d.rearrange("r a n -> (r a) n"), w=[t_row])
    for h in range(4):
        row_to_col(k, row[0:4, h * 128:(h + 1) * 128], 4, 128, col[:, 24 + 4 * h:28 + 4 * h], t_row, t_col)
    lbT = col[:, 24:40].rearrange("p (h r a) -> p h r a", r=2, a=2)
    lbv = col[:, 44:52].rearrange("p (h r) -> p h r", r=2)
    omv = col[:, 52:60].rearrange("p (h r) -> p h r", r=2)
    S.op("dve", lambda e: e.tensor_tensor(out=lbv, in0=lbT[:, :, :, 0], in1=lbT[:, :, :, 1], op=ALU.subtract), r=[t_col], w=[t_col])
    S.op("act", lambda e: e.activation(out=lbv, in_=lbv, func=AF.Sigmoid), r=[t_col], w=[t_col])
    S.op("dve", lambda e: e.tensor_scalar(out=omv, in0=lbv, scalar1=-1.0, scalar2=1.0, op0=ALU.mult, op1=ALU.add), r=[t_col], w=[t_col])
    S.dma(row[0:1, 0:512], k.hn_d[:, :], w=[t_row])
    for h in range(4):
        row_to_col(k, row[0:1, h * 128:(h + 1) * 128], 1, 128, col[:, 40 + h:41 + h], t_row, t_col)

    qrow, Krow, A, B, oacc = RB[:, 0, :], RB[:, 1, :], RB[:, 2, :], RB[:, 3, :], RB[:, 4, :]
    gsil, mixrow = HB[:, 2, :], HB[:, 4, :]
    qts, kts = [HB[:, 0, :], HB[:, 5, :]], [HB[:, 1, :], HB[:, 6, :]]
    vtm = HB[:, 3, :].rearrange("p (i c) -> p i c", c=128)
    for h in range(4):
        base = 1664 + 512 * h
        wg, t_wg = k.load_w(k.w0_d[:, base:base + 512].rearrange("(k p) n -> p k n", p=128))
        wvv, t_wvv = k.load_w(k.w0_d[:, 3712 + 128 * h:3712 + 128 * (h + 1)].rearrange("(k p) n -> p k n", p=128))

        def ev_q(pb, tb, t0, nt):
            S.op("act", lambda e: e.activation(out=qrow[:, t0:t0 + nt], in_=pb[:, 0:nt], func=AF.Copy), r=[tb], w=[t_rb[0]])
        k.proj_fm(wg, t_wg, 256, 128, ev_q)

        def ev_g(pb, tb, t0, nt):
            S.op("act", lambda e: e.activation(out=B[:, t0:t0 + nt], in_=pb[:, 0:nt], func=AF.Sigmoid), r=[tb], w=[t_rb[3]])
            S.op("dve", lambda e: e.tensor_tensor(out=gsil[:, t0:t0 + nt], in0=pb[:, 0:nt], in1=B[:, t0:t0 + nt], op=ALU.mult),
                 r=[tb, t_rb[3]], w=[t_hb[2]])
        k.proj_fm(wg, t_wg, 384, 128, ev_g)

        def ev_v2(pb, tb, i):
            S.op("act", lambda e: e.activation(out=vtm[:, i, :], in_=pb[:, 0:128], func=AF.Copy), r=[tb], w=[t_hb[3]])
        k.proj_tm(wvv, t_wvv, 0, 128, ev_v2)

        def make_logf(d, h=h, wg=wg, t_wg=t_wg):
            def ev_z(pb, tb, t0, nt):
                S.op("act", lambda e: e.activation(out=A[:, t0:t0 + nt], in_=pb[:, 0:nt], func=AF.Sigmoid), r=[tb], w=[t_rb[2]])
            k.proj_fm(wg, t_wg, d * 128, 128, ev_z)
            S.op("dve", lambda e: e.tensor_scalar(out=A, in0=A, scalar1=omv[:, h, d:d + 1], scalar2=lbv[:, h, d:d + 1], op0=ALU.mult, op1=ALU.add),
                 r=[t_rb[2], t_col], w=[t_rb[2]])
            S.op("pool", lambda e: e.tensor_scalar(out=Krow, in0=A, scalar1=-1.0, scalar2=1.0, op0=ALU.mult, op1=ALU.add),
                 r=[t_rb[2]], w=[t_rb[1]])
            S.op("act", lambda e: e.activation(out=A, in_=A, func=AF.Ln), r=[t_rb[2]], w=[t_rb[2]])
        gated_scan(k, 128, 128, 1, qrow, t_rb[0], Krow, t_rb[1], A, t_rb[2], B, t_rb[3], make_logf,
                   lambda i: vtm[:, i, :], [t_hb[3]], [oacc], [t_rb[4]], qts, [t_hb[0], t_hb[5]], kts, [t_hb[1], t_hb[6]])
        if h == 0:
            k.dump("oacc0", oacc, [128, N], F32, [t_rb[4]])
        rms_gate_out(k, 128, 1, [oacc], [t_rb[4]], A, t_rb[2], B, t_rb[3], [gsil], [t_hb[2]], [col[:, 40 + h:41 + h]], t_col,
                     [mixrow], [t_hb[4]], [4 + h])
    k.dump("mixd0", k.mix_d[0:8, :, :], [8, 128, N], BF16, k.t_mixd[0:8])


def phase_D(k, l):
    S = k.S
    RB, HB = k.RB, k.HB
    k.bcast_rows(l, "mix")
    if l == 0:
        wo, t_wo = k.load_w(k.wo0_d.rearrange("(c p) n -> p c n", p=128), nslots=2)
        chunks = [(c, 128, wo, t_wo, c) for c in range(8)]
        tiles = list(range(NT))
        nch = 8
    else:
        wo, t_wo = k.load_w(k.wo1_d[0:768, :].rearrange("(c p) n -> p c n", p=96), nslots=2, parts=96)
        wf, t_wf = k.load_w(k.wo1_d[768:1024, :].rearrange("(c p) n -> p c n", p=128), nslots=1)
        chunks = [(c, 96, wo, t_wo, c) for c in range(8)] + [(8 + c, 128, wf, t_wf, c) for c in range(2)]
        tiles = list(range(2, NT))
        nch = 10
    tb_ = [RB[:, r, hf * 1024:(hf + 1) * 1024] for r in range(6) for hf in range(2)]
    tt = k.t_tile
    nchunks = len(chunks)

    def bufs_for(n_):
        s6 = (n_ % 2) * 6
        return [tb_[s6 + q] for q in range(6)], [tt[s6 + q] for q in range(6)]

    def stA(n_):
        i = tiles[n_]
        (ht, tmp, xn1, hnew, xn2, ufb), (t_ht, t_tmp, t_xn1, t_hn, t_xn2, t_uf) = bufs_for(n_)
        mt = HB[:, n_ % 2, 0:nch * 128].rearrange("p (c n) -> p c n", n=128)
        t_mt = k.t_hb[n_ % 2]
        S.dma(mt, k.mix_d[0:nch, :, i * 128:(i + 1) * 128].rearrange("c p n -> p c n"), r=k.t_mixd[0:nch], w=[t_mt])
        if l == 0:
            src = k.ctx_d[i * 128:(i + 1) * 128, :] if i < 2 else k.x_d[(i - 2) * 128:(i - 1) * 128, :]
            S.dma(ht, src, w=[t_ht])
        else:
            S.dma(ht, k.hout0_d[i * 128:(i + 1) * 128, :], r=[k.t_hout0[i]], w=[t_ht])
        pp, tp = k.pair()
        for hf in range(2):
            for ci_, (c, KR, wv, t_wv, wc) in enumerate(chunks):
                S.op("pe", lambda e, hf=hf, c=c, KR=KR, wv=wv, wc=wc, ci_=ci_: e.matmul(
                    pp[:, hf * 512:(hf + 1) * 512], lhsT=mt[0:KR, c, :], rhs=wv[0:KR, wc, hf * 512:(hf + 1) * 512],
                    start=(ci_ == 0), stop=(ci_ == nchunks - 1)), r=[t_mt] + t_wv, w=[tp[hf]])
        r_ = 1 if i < 2 else 0
        for hf in range(2):
            S.op("dve", lambda e, hf=hf: e.tensor_tensor(out=tmp[:, hf * 512:(hf + 1) * 512], in0=pp[:, hf * 512:(hf + 1) * 512],
                                                         in1=k.bc[:, r_, hf * 512:(hf + 1) * 512], op=ALU.mult),
                 r=[tp[hf], k.t_bc[r_]], w=[t_tmp])
        S.op("dve", lambda e: e.scalar_tensor_tensor(out=tmp, in0=ht, scalar=ALPHA, in1=tmp, op0=ALU.mult, op1=ALU.add),
             r=[t_ht, t_tmp], w=[t_tmp])
        st_ap, mv_ap, rs_ap, nb_ap, t_st = k.st_slot()
        k.ln_stats(tmp, t_tmp, st_ap, mv_ap, rs_ap, nb_ap, t_st)
        S.op("act", lambda e: e.activation(out=xn1, in_=tmp, func=AF.Identity, scale=rs_ap, bias=nb_ap), r=[t_tmp, t_st], w=[t_xn1])
        S.op("pool", lambda e: e.tensor_tensor(out=xn1, in0=xn1, in1=k.bc[:, 2, :], op=ALU.mult), r=[t_xn1, k.t_bc[2]], w=[t_xn1])
        S.op("pool", lambda e: e.tensor_tensor(out=hnew, in0=xn1, in1=k.bc[:, 3, :], op=ALU.add), r=[t_xn1, k.t_bc[3]], w=[t_hn])
        S.dma(k.hmid_d[i * 128:(i + 1) * 128, :], hnew, r=[t_hn], w=[k.t_hmid[i]])
        k.ln_part1(hnew, t_hn, xn2, t_xn2)

    def stB(n_):
        i = tiles[n_]
        (ht, tmp, xn1, hnew, xn2, ufb), (t_ht, t_tmp, t_xn1, t_hn, t_xn2, t_uf) = bufs_for(n_)
        uf = ufb.rearrange("p (k n) -> p k n", n=128)
        k.ln_part2(l, xn2, t_xn2, i, 3, True, uf, t_uf)

    def stC(n_):
        i = tiles[n_]
        (ht, tmp, xn1, hnew, xn2, ufb), (t_ht, t_tmp, t_xn1, t_hn, t_xn2, t_uf) = bufs_for(n_)
        uf = ufb.rearrange("p (k n) -> p k n", n=128)
        k.route(i, uf, t_uf)
    nT_ = len(tiles)
    for n in range(nT_ + 2):
        if n < nT_:
            stA(n)
        if 1 <= n <= nT_:
            stB(n - 1)
        if n >= 2:
            stC(n - 2)
    k.dump(f"hmid{l}", k.hmid_d[:, :], [N, D], F32, k.t_hmid)
    k.dump(f"u2T{l}", k.uT[:, :, :], [128, KC, N], BF16, k.t_uT)
    k.dump(f"gates{l}", k.gates[:, :, :], [128, NT, 16], F32, k.t_gates)


def phase_E(k, l):
    S = k.S
    RB = k.RB
    k.bcast_rows(l, "moe")
    if l == 0:
        halves = [list(range(0, 9)), list(range(9, 18))]
        bsz = 384
    else:
        halves = [list(range(2, 10)), list(range(10, 18))]
        bsz = 512
    yacc = RB[:, 0:4, :].rearrange("p a n -> p (a n)").rearrange("p (t d) -> p t d", d=1024)
    t_y = k.t_tile[0:9]
    hTb = RB[:, 4, :].bitcast(BF16)
    hT = [hTb[:, q * 2048:(q + 1) * 2048].rearrange("p (f n) -> p f n", n=512) for q in range(2)]
    t_hT = [k.t_tile[9], k.t_tile[10]]
    sg = [RB[:, 5, q * 512:(q + 1) * 512] for q in range(2)]
    t_sg = [k.t_rb[4], k.t_rb[5]]
    Hb = RB[:, 5, 1024:2048]
    t_H = k.t_tile[11]
    dst_d = k.hout0_d if l == 0 else k.out_d
    k.h_i = 0
    k.s_i = 0
    for tiles in halves:
        tok0 = tiles[0] * 128
        ntok = len(tiles) * 128
        blocks = [(tok0 + b0, bsz) for b0 in range(0, ntok, bsz)]
        for e_ in range(16):
            w1, t_w1 = k.load_w(k.eg_d[l, e_].rearrange("(c p) n -> p c n", p=128))
            w3, t_w3 = k.load_w(k.eu_d[l, e_].rearrange("(c p) n -> p c n", p=128))
            w2, t_w2 = k.load_w(k.ed_d[l, e_].rearrange("(c p) n -> p c n", p=128))
            for (b0, nb) in blocks:
                hi = k.h_i
                k.h_i = 1 - hi
                hTc = hT[hi]
                for f in range(4):
                    p1, tp1 = k.bank()
                    for kk in range(KC):
                        S.op("pe", lambda e, kk=kk, f=f, p1=p1, w1=w1, b0=b0, nb=nb: e.matmul(
                            p1[:, 0:nb], lhsT=w1[:, kk, f * 128:(f + 1) * 128], rhs=k.uT[:, kk, b0:b0 + nb], start=(kk == 0), stop=(kk == KC - 1)),
                            r=t_w1 + k.t_uT[b0 // 128:(b0 + nb) // 128], w=[tp1])
                    p3, tp3 = k.bank()
                    for kk in range(KC):
                        S.op("pe", lambda e, kk=kk, f=f, p3=p3, w3=w3, b0=b0, nb=nb: e.matmul(
                            p3[:, 0:nb], lhsT=w3[:, kk, f * 128:(f + 1) * 128], rhs=k.uT[:, kk, b0:b0 + nb], start=(kk == 0), stop=(kk == KC - 1)),
                            r=t_w3 + k.t_uT[b0 // 128:(b0 + nb) // 128], w=[tp3])
                    si = k.s_i
                    k.s_i = 1 - si
                    S.op("act", lambda e, p1=p1, si=si, nb=nb: e.activation(out=sg[si][:, 0:nb], in_=p1[:, 0:nb], func=AF.Sigmoid), r=[tp1], w=[t_sg[si]])
                    S.op("dve", lambda e, p1=p1, si=si, nb=nb: e.tensor_tensor(out=sg[si][:, 0:nb], in0=p1[:, 0:nb], in1=sg[si][:, 0:nb], op=ALU.mult),
                         r=[tp1, t_sg[si]], w=[t_sg[si]])
                    S.op("dve", lambda e, p3=p3, si=si, nb=nb, f=f, hTc=hTc: e.tensor_tensor(out=hTc[:, f, 0:nb], in0=p3[:, 0:nb], in1=sg[si][:, 0:nb], op=ALU.mult),
                         r=[tp3, t_sg[si]], w=[t_hT[hi]])
                for tl in range(nb // 128):
                    gi = (b0 // 128) + tl
                    yi = gi - tiles[0]
                    for dh in range(2):
                        py, tpy = k.bank()
                        for f in range(4):
                            S.op("pe", lambda e, f=f, py=py, hTc=hTc, tl=tl, dh=dh, w2=w2: e.matmul(
                                py[:, :], lhsT=hTc[:, f, tl * 128:(tl + 1) * 128], rhs=w2[:, f, dh * 512:(dh + 1) * 512], start=(f == 0), stop=(f == 3)),
                                r=[t_hT[hi]] + t_w2, w=[tpy])
                        ya = yacc[:, yi, dh * 512:(dh + 1) * 512]
                        gs = k.gates[:, gi, e_:e_ + 1]
                        if e_ == 0:
                            S.op("dve", lambda e, py=py, ya=ya, gs=gs: e.tensor_scalar(out=ya, in0=py[:, :], scalar1=gs, scalar2=None, op0=ALU.mult),
                                 r=[tpy, k.t_gates[gi]], w=[t_y[yi]])
                        else:
                            S.op("dve", lambda e, py=py, ya=ya, gs=gs: e.scalar_tensor_tensor(out=ya, in0=py[:, :], scalar=gs, in1=ya, op0=ALU.mult, op1=ALU.add),
                                 r=[tpy, k.t_gates[gi], t_y[yi]], w=[t_y[yi]])
        for yi, gi in enumerate(tiles):
            yt = yacc[:, yi, :]
            r_ = 1 if gi < 2 else 0
            if l == 0 and yi == 0 and tiles[0] == 0:
                k.dump("ymoe_t0", yt, [128, D], F32, [t_y[yi]])
            S.dma(Hb, k.hmid_d[gi * 128:(gi + 1) * 128, :], r=[k.t_hmid[gi]], w=[t_H])
            S.op("dve", lambda e, yt=yt, r_=r_: e.tensor_tensor(out=yt, in0=yt, in1=k.bc[:, r_, :], op=ALU.mult), r=[t_y[yi], k.t_bc[r_]], w=[t_y[yi]])
            S.op("dve", lambda e, yt=yt: e.scalar_tensor_tensor(out=yt, in0=Hb, scalar=ALPHA, in1=yt, op0=ALU.mult, op1=ALU.add),
                 r=[t_H, t_y[yi]], w=[t_y[yi]])
            st_ap, mv_ap, rs_ap, nb_ap, t_st = k.st_slot()
            k.ln_stats(yt, t_y[yi], st_ap, mv_ap, rs_ap, nb_ap, t_st)
            S.op("act", lambda e, yt=yt, rs_ap=rs_ap, nb_ap=nb_ap: e.activation(out=Hb, in_=yt, func=AF.Identity, scale=rs_ap, bias=nb_ap),
                 r=[t_y[yi], t_st], w=[t_H])
            S.op("dve", lambda e: e.tensor_tensor(out=Hb, in0=Hb, in1=k.bc[:, 2, :], op=ALU.mult), r=[t_H, k.t_bc[2]], w=[t_H])
            S.op("pool", lambda e, yt=yt: e.tensor_tensor(out=yt, in0=Hb, in1=k.bc[:, 3, :], op=ALU.add), r=[t_H, k.t_bc[3]], w=[t_y[yi]])
            if l == 0:
                S.dma(k.hout0_d[gi * 128:(gi + 1) * 128, :], yt, r=[t_y[yi]], w=[k.t_hout0[gi]])
            else:
                S.dma(k.out_d[(gi - 2) * 128:(gi - 1) * 128, :], yt, r=[t_y[yi]], w=[k.t_out[gi - 2]])
    if l == 0:
        k.dump("hout0", k.hout0_d[:, :], [N, D], F32, k.t_hout0)


def mixer_odd(k):
    S = k.S
    scan_setup(k)
    sc = k.scn
    RB, HB, t_rb, t_hb = k.RB, k.HB, k.t_rb, k.t_hb
    col, t_col, row, t_row = sc["col"], k.t_col, sc["row"], k.t_row
    LATB = [(256, 512), (768, 512), (1280, 512), (1792, 512)]
    BC = sc["Am"][:, 0, 0, :]
    BS = sc["Am"][:, 0, 1, :]
    ci = col[:, 0:32].bitcast(I32)
    S.op("pool", lambda e: e.iota(ci[:, 0:1], pattern=[[0, 1]], base=0, channel_multiplier=1), w=[t_col])
    S.op("dve", lambda e: e.tensor_single_scalar(out=ci[:, 1:2], in_=ci[:, 0:1], scalar=63, op=ALU.bitwise_and), r=[t_col], w=[t_col])
    S.op("dve", lambda e: e.tensor_copy(out=col[:, 32:33], in_=ci[:, 1:2]), r=[t_col], w=[t_col])
    S.op("pool", lambda e: e.iota(ci[:, 2:18], pattern=[[128, 16]], base=0, channel_multiplier=1), r=[t_col], w=[t_col])
    S.op("dve", lambda e: e.tensor_copy(out=col[:, 40:56], in_=ci[:, 2:18]), r=[t_col], w=[t_col])
    qi = RB[:, 0, 0:128].bitcast(I32)
    qf = RB[:, 0, 128:256]
    ki = RB[:, 0, 256:384].bitcast(I32)
    kci = RB[:, 0, 384:512].bitcast(I32)
    tq = t_rb[0]
    S.op("pool", lambda e: e.iota(qi, pattern=[[1, 128]], base=0, channel_multiplier=0), w=[tq])
    S.op("dve", lambda e: e.tensor_single_scalar(out=qi, in_=qi, scalar=63, op=ALU.bitwise_and), r=[tq], w=[tq])
    S.op("dve", lambda e: e.tensor_copy(out=qf, in_=qi), r=[tq], w=[tq])
    S.op("dve", lambda e: e.tensor_scalar(out=ki, in0=qf, scalar1=col[:, 32:33], scalar2=None, op0=ALU.mult), r=[tq, t_col], w=[tq])
    S.op("dve", lambda e: e.tensor_single_scalar(out=ki, in_=ki, scalar=63, op=ALU.bitwise_and), r=[tq], w=[tq])
    S.op("dve", lambda e: e.tensor_scalar(out=kci, in0=ki, scalar1=16, scalar2=None, op0=ALU.add), r=[tq], w=[tq])
    S.op("dve", lambda e: e.tensor_single_scalar(out=kci, in_=kci, scalar=63, op=ALU.bitwise_and), r=[tq], w=[tq])
    S.op("act", lambda e: e.activation(out=BS, in_=ki, func=AF.Sin, scale=-2 * PI / 64, bias=k.cst[:, 4:5]), r=[tq, k.t_c], w=[k.t_Am[1]])
    S.op("act", lambda e: e.activation(out=BC, in_=kci, func=AF.Sin, scale=-2 * PI / 64, bias=k.cst[:, 4:5]), r=[tq, k.t_c], w=[k.t_Am[0]])
    for M_, tM in ((BC, k.t_Am[0]), (BS, k.t_Am[1])):
        S.op("pool", lambda e, M_=M_: e.memset(M_[0:64, 64:128], 0.0), r=[tM], w=[tM])
        S.op("pool", lambda e, M_=M_: e.memset(M_[64:128, 0:64], 0.0), r=[tM], w=[tM])
    if k.sub == "M1a":
        k.dump("BCS", sc["Am"][:, 0, :, :], [128, 2, 128], BF16, k.t_Am)
        return
    wz, t_wz = k.load_w(k.w1_d[:, 1536:1792].rearrange("(k p) n -> p k n", p=128))
    zT = HB[:, 2:4, :]
    zc = HB[:, 4:6, :].rearrange("p a n -> p (a n)")[:, 0:4096].rearrange("p (i c) -> p i c", c=256)
    zs = HB[:, 6:8, :].rearrange("p a n -> p (a n)")[:, 0:4096].rearrange("p (i c) -> p i c", c=256)
    for m in range(2):
        for (t0, nt) in LATB:
            pb, tb = proj_block(k, wz, t_wz, m * 128, 128, t0, nt)
            S.op("act", lambda e, pb=pb, m=m, t0=t0, nt=nt: e.activation(out=zT[:, m, t0:t0 + nt], in_=pb[:, 0:nt], func=AF.Copy), r=[tb], w=[t_hb[2 + m]])
    if k.sub == "M1z":
        k.dump("zT", HB[:, 2:4, :], [128, 2, N], BF16, [t_hb[2], t_hb[3]])
        return
    for a in range(16):
        tok = (a + 2) * 128
        pb, tb = k.bank()
        for m in range(2):
            S.op("pe", lambda e, pb=pb, m=m, tok=tok: e.matmul(pb[:, m * 128:(m + 1) * 128], lhsT=zT[:, m, tok:tok + 128], rhs=BC[:, :], start=True, stop=True),
                 r=[t_hb[2 + m], k.t_Am[0]], w=[tb])
            S.op("pe", lambda e, pb=pb, m=m, tok=tok: e.matmul(pb[:, 256 + m * 128:256 + (m + 1) * 128], lhsT=zT[:, m, tok:tok + 128], rhs=BS[:, :], start=True, stop=True),
                 r=[t_hb[2 + m], k.t_Am[1]], w=[tb])
        S.op("act", lambda e, pb=pb, a=a: e.activation(out=zc[:, a, :], in_=pb[:, 0:256], func=AF.Copy), r=[tb], w=[t_hb[4], t_hb[5]])
        if k.sub != "M1d":
            S.op("act", lambda e, pb=pb, a=a: e.activation(out=zs[:, a, :], in_=pb[:, 256:512], func=AF.Copy, scale=-1.0), r=[tb], w=[t_hb[6], t_hb[7]])
        if k.sub in ("M1c", "M1d") and a == 0:
            k.dump("zc", HB[:, 4:6, :], [128, 2, N], BF16, [t_hb[4], t_hb[5]])
            return
    S.barrier()
    if k.sub == "M1b":
        k.dump("zc", HB[:, 4:6, :], [128, 2, N], BF16, [t_hb[4], t_hb[5]])
        return
    fidx = RB[:, 0, 0:2048]
    fi_i = RB[:, 1, 0:2048].bitcast(I32)
    S.op("pool", lambda e: e.iota(fi_i, pattern=[[1, 2048]], base=0, channel_multiplier=0), w=[t_rb[1]])
    S.op("dve", lambda e: e.tensor_copy(out=fidx, in_=fi_i), r=[t_rb[1]], w=[t_rb[0]])
    tabs = []
    for q in range(2):
        rowb = RB[:, 2 + q, :].bitcast(BF16)
        tabs.append((rowb[:, 0:2048], rowb[:, 2048:4096], t_rb[2 + q]))
    kib = [RB[:, 4, 0:2048].bitcast(I32), RB[:, 5, 0:2048].bitcast(I32)]
    banks = [(k.PS[i // 2][:, (i % 2) * 512:(i % 2 + 1) * 512], k.PT[i // 2][i % 2]) for i in range(8)]
    for a in range(16):
        Cb, Sb, t_tab = tabs[a % 2]
        S.op("dve", lambda e, a=a: e.tensor_scalar(out=kib[0], in0=fidx, scalar1=col[:, 40 + a:41 + a], scalar2=None, op0=ALU.mult),
             r=[t_rb[0], t_col], w=[t_rb[4]])
        S.op("dve", lambda e: e.tensor_single_scalar(out=kib[0], in_=kib[0], scalar=2047, op=ALU.bitwise_and), r=[t_rb[4]], w=[t_rb[4]])
        S.op("dve", lambda e: e.tensor_scalar(out=kib[1], in0=kib[0], scalar1=512, scalar2=None, op0=ALU.add), r=[t_rb[4]], w=[t_rb[5]])
        S.op("dve", lambda e: e.tensor_single_scalar(out=kib[1], in_=kib[1], scalar=2047, op=ALU.bitwise_and), r=[t_rb[5]], w=[t_rb[5]])
        S.op("act", lambda e, Sb=Sb: e.activation(out=Sb, in_=kib[0], func=AF.Sin, scale=-2 * PI / 2048, bias=k.cst[:, 4:5]), r=[t_rb[4], k.t_c], w=[t_tab])
        S.op("act", lambda e, Cb=Cb: e.activation(out=Cb, in_=kib[1], func=AF.Sin, scale=-2 * PI / 2048, bias=k.cst[:, 4:5]), r=[t_rb[5], k.t_c], w=[t_tab])
        for m in range(2):
            for fb in range(4):
                pbk, tbk = banks[m * 4 + fb]
                S.op("pe", lambda e, pbk=pbk, a=a, m=m, fb=fb, Cb=Cb: e.matmul(pbk[:, :], lhsT=zc[:, a, m * 128:(m + 1) * 128], rhs=Cb[:, fb * 512:(fb + 1) * 512],
                                                                            start=(a == 0), stop=False), r=[t_hb[4], t_hb[5], t_tab], w=[tbk])
                S.op("pe", lambda e, pbk=pbk, a=a, m=m, fb=fb, Sb=Sb: e.matmul(pbk[:, :], lhsT=zs[:, a, m * 128:(m + 1) * 128], rhs=Sb[:, fb * 512:(fb + 1) * 512],
                                                                            start=False, stop=(a == 15)), r=[t_hb[6], t_hb[7], t_tab], w=[tbk])
    fsc = 1.0 / math.sqrt(2048.0 * 64.0)
    for m in range(2):
        for fb in range(4):
            pbk, tbk = banks[m * 4 + fb]
            S.op("act", lambda e, pbk=pbk, m=m, fb=fb: e.activation(out=HB[:, m, fb * 512:(fb + 1) * 512], in_=pbk[:, :], func=AF.Copy, scale=fsc), r=[tbk], w=[t_hb[m]])
        S.dma(k.mix_d[8 + m, :, 256:2304], HB[:, m, 0:2048], r=[t_hb[m]], w=[k.t_mixd[8 + m]])
    k.dump("mixd_f", k.mix_d[8:10, :, :], [2, 128, N], BF16, k.t_mixd[8:10])
    S.barrier()
    if k.sub == "M1f":
        return

    gw = k.bc[0:16, 0, 0:768].rearrange("p (r n) -> p r n", n=384)
    t_gw = k.t_bc[0]
    S.dma(gw[:, :, :], k.gw_d.rearrange("r k n -> k r n"), w=[t_gw])
    S.dma(row[0:2, 0:384], k.gb_d[:, :], w=[t_row])
    for h in range(4):
        row_to_col(k, row[0:2, h * 96:(h + 1) * 96], 2, 96, col[0:96, 2 * h:2 * h + 2], t_row, t_col)
    S.op("dve", lambda e: e.tensor_scalar(out=col[0:96, 0:8], in0=col[0:96, 0:8], scalar1=-1.0, scalar2=None, op0=ALU.mult), r=[t_col], w=[t_col])
    S.dma(row[0:1, 0:768], k.gn_d[:, :], r=[t_col], w=[t_row])
    for c in range(8):
        row_to_col(k, row[0:1, c * 96:(c + 1) * 96], 1, 96, col[0:96, 8 + c:9 + c], t_row, t_col)
    qrow, krow, A, B = RB[0:96, 0, :], RB[0:96, 1, :], RB[0:96, 2, :], RB[0:96, 3, :]
    oacc = [RB[0:96, 4, :], RB[0:96, 5, :]]
    qt, kt = HB[0:96, 0, :], HB[0:96, 1, :]
    qts, kts = [HB[0:96, 0, :], HB[0:96, 6, :]], [HB[0:96, 1, :], HB[0:96, 7, :]]
    gsil = [HB[0:96, 2, :], HB[0:96, 3, :]]
    vt = HB[:, 4:6, :].rearrange("p a n -> p (a n)")[:, 0:3456].rearrange("p (i c) -> p i c", c=192)
    for h in range(4):
        base = 384 * h
        wg, t_wg = k.load_w(k.w1_d[:, base:base + 384].rearrange("(k p) n -> p k n", p=128))
        wv, t_wvv = k.load_w(k.w1_d[:, 1824 + 192 * h:1824 + 192 * (h + 1)].rearrange("(k p) n -> p k n", p=128))
        wR, t_wR = k.load_w(k.w1_d[:, 1792:1824].rearrange("(k p) n -> p k n", p=128))

        def ev_q(pb, tb, t0, nt):
            S.op("act", lambda e: e.activation(out=qrow[:, t0:t0 + nt], in_=pb[0:96, 0:nt], func=AF.Copy, scale=96.0 ** -0.5), r=[tb], w=[t_rb[0]])
        k.proj_fm(wg, t_wg, 0, 96, ev_q)

        def ev_k(pb, tb, t0, nt):
            S.op("act", lambda e: e.activation(out=krow[:, t0:t0 + nt], in_=pb[0:96, 0:nt], func=AF.Copy), r=[tb], w=[t_rb[1]])
        k.proj_fm(wg, t_wg, 96, 96, ev_k)
        for a in range(2):
            def ev_g(pb, tb, t0, nt, a=a):
                S.op("act", lambda e: e.activation(out=B[:, t0:t0 + nt], in_=pb[0:96, 0:nt], func=AF.Sigmoid), r=[tb], w=[t_rb[3]])
                S.op("dve", lambda e: e.tensor_tensor(out=gsil[a][:, t0:t0 + nt], in0=pb[0:96, 0:nt], in1=B[:, t0:t0 + nt], op=ALU.mult),
                     r=[tb, t_rb[3]], w=[t_hb[2 + a]])
            k.proj_fm(wg, t_wg, 192 + 96 * a, 96, ev_g)

        def ev_v(pb, tb, i):
            S.op("act", lambda e: e.activation(out=vt[:, i, :], in_=pb[:, 0:192], func=AF.Copy), r=[tb], w=[t_hb[4], t_hb[5]])
        k.proj_tm(wv, t_wvv, 0, 192, ev_v)

        def make_logf(d, h=h, wR=wR, t_wR=t_wR):
            for (t0, nt) in TOKB:
                pr, tr = proj_block(k, wR, t_wR, d * 16, 16, t0, nt)
                S.op("act", lambda e, pr=pr, t0=t0, nt=nt: e.activation(out=B[0:16, t0:t0 + nt], in_=pr[0:16, 0:nt], func=AF.Copy), r=[tr], w=[t_rb[3]])
                pz, tz = k.bank()
                S.op("pe", lambda e, pz=pz, t0=t0, nt=nt: e.matmul(pz[0:96, 0:nt], lhsT=gw[0:16, d, h * 96:(h + 1) * 96], rhs=B[0:16, t0:t0 + nt],
                                                                   start=True, stop=True), r=[t_gw, t_rb[3]], w=[tz])
                S.op("act", lambda e, pz=pz, t0=t0, nt=nt: e.activation(out=A[:, t0:t0 + nt], in_=pz[0:96, 0:nt], func=AF.Exp, scale=-1.0,
                                                                        bias=col[0:96, 2 * h + d:2 * h + d + 1]), r=[tz, t_col], w=[t_rb[2]])
            S.op("act", lambda e: e.activation(out=A, in_=A, func=AF.Ln, bias=k.cst[0:96, 1:2]), r=[t_rb[2], k.t_c], w=[t_rb[2]])
            S.op("dve", lambda e: e.tensor_scalar(out=A, in0=A, scalar1=-1.0 / 16.0, scalar2=None, op0=ALU.mult), r=[t_rb[2]], w=[t_rb[2]])
        gated_scan(k, 96, 96, 2, qrow, t_rb[0], krow, t_rb[1], A, t_rb[2], B, t_rb[3], make_logf,
                   lambda i: vt[:, i, :], [t_hb[4], t_hb[5]], oacc, [t_rb[4], t_rb[5]], qts, [t_hb[0], t_hb[6]], kts, [t_hb[1], t_hb[7]])
        if h == 0:
            k.dump("gla_o0", RB[0:96, 4:6, :], [96, 2, N], F32, [t_rb[4], t_rb[5]])
        rms_gate_out(k, 96, 2, oacc, [t_rb[4], t_rb[5]], A, t_rb[2], B, t_rb[3], gsil, [t_hb[2], t_hb[3]],
                     [col[0:96, 8 + 2 * h:9 + 2 * h], col[0:96, 9 + 2 * h:10 + 2 * h]], t_col, [qt, kt], [t_hb[0], t_hb[1]], [2 * h, 2 * h + 1])
    k.dump("mixd1", k.mix_d[0:10, :, :], [10, 128, N], BF16, k.t_mixd[0:10])


_NC_CACHE = {}


def _f32(a):
    return np.ascontiguousarray(np.asarray(a, dtype=np.float32))


def kernel(x, c, ctx, c_ctx, w_ada, b_ada, ln_g, ln_b, w_in_even, attn_sink, hgrn_lb_logits, hgrn_norm,
           w_out_even, w_in_odd, gla_gate_w, gla_gate_b, gla_norm, w_out_odd, w_router, b_router,
           w_expert_gate, w_expert_up, w_expert_down):
    x = _f32(x); c = _f32(c); ctx = _f32(ctx); c_ctx = _f32(c_ctx)
    w0a = np.ascontiguousarray(_f32(w_in_even)[0][:, _cols0()])
    w1a = np.ascontiguousarray(_f32(w_in_odd)[0][:, _cols1()])
    shared = {
        "w_ada": _f32(w_ada), "b_ada": _f32(b_ada), "ln_g": _f32(ln_g), "ln_b": _f32(ln_b),
        "w0a": w0a, "attn_sink": _f32(attn_sink), "lb_logits": _f32(hgrn_lb_logits), "hgrn_norm": _f32(hgrn_norm),
        "w_out_even": _f32(w_out_even)[0], "w1a": w1a, "gla_gate_w": _f32(gla_gate_w)[0], "gla_gate_b": _f32(gla_gate_b)[0],
        "gla_norm": _f32(gla_norm), "w_out_odd": _f32(w_out_odd)[0], "w_router": _f32(w_router),
        "b_router": _f32(b_router)[None, :], "w_expert_gate": _f32(w_expert_gate), "w_expert_up": _f32(w_expert_up),
        "w_expert_down": _f32(w_expert_down),
    }
    nb = x.shape[0]
    in_maps = []
    for b in range(nb):
        m = dict(shared)
        m["x"] = np.ascontiguousarray(x[b])
        m["ctx"] = np.ascontiguousarray(ctx[b])
        m["cvec"] = np.ascontiguousarray(np.stack([c[b], c_ctx], 0))
        in_maps.append(m)
    if "nc" not in _NC_CACHE:
        _NC_CACHE["nc"] = build()
    res = run_bass_kernel_spmd(_NC_CACHE["nc"], in_maps, core_ids=list(range(nb)))
    return np.stack([np.asarray(r["out"], dtype=np.float32) for r in res.results], 0)
```

```python
import contextlib
import math
import numpy as np
import concourse.bass as bass
import concourse.mybir as mybir
from concourse.bass_utils import run_bass_kernel_spmd

F32 = mybir.dt.float32
BF16 = mybir.dt.bfloat16
I32 = mybir.dt.int32
AF = mybir.ActivationFunctionType
ALU = mybir.AluOpType
AX = mybir.AxisListType

ENG = ("pe", "act", "dve", "pool", "sp")


class Trk:
    __slots__ = ("w", "rs", "dsem", "dcnt", "name")

    def __init__(self, name=""):
        self.w = None
        self.rs = []
        self.dsem = None
        self.dcnt = 0
        self.name = name


class Sched:
    SEM_CHUNK = 20000

    def __init__(self, nc):
        self.nc = nc
        self.ops = {e: [] for e in ENG}
        self.waited = {e: {} for e in ENG}
        self.stack = contextlib.ExitStack()
        self.nsem = 0
        self.dma_ev = {}

    def sbuf(self, name, shape, dt):
        return self.stack.enter_context(self.nc.sbuf_tensor(name, list(shape), dt))

    def psum(self, name, shape, dt=F32):
        return self.stack.enter_context(self.nc.psum_tensor(name, list(shape), dt))

    def new_sem(self, name):
        self.nsem += 1
        return self.stack.enter_context(self.nc.semaphore(f"{name}_{self.nsem}"))

    def _filter(self, engine, deps):
        waits = []
        wd = self.waited[engine]
        for ev in deps:
            if ev[0] == "e":
                _, f, idx = ev
                if engine == "pe" and f == "pe":
                    continue
                if idx <= wd.get(f, -1):
                    continue
                wd[f] = idx
                self.ops[f][idx][2] = True
                waits.append(ev)
            else:
                _, sem, val = ev
                k = id(sem)
                if val <= wd.get(k, 0):
                    continue
                wd[k] = val
                waits.append(ev)
        return waits

    def _deps(self, engine, r, w):
        deps = []
        for t in r:
            if t.w is not None:
                deps.append(t.w)
        for t in w:
            if t.w is not None:
                deps.append(t.w)
            deps.extend(t.rs)
        return self._filter(engine, deps)

    def _post(self, ev, r, w):
        for t in w:
            t.w = ev
            t.rs = []
        for t in r:
            if t in w:
                continue
            if ev[0] == "e":
                t.rs = [x for x in t.rs if not (x[0] == "e" and x[1] == ev[1])]
            else:
                t.rs = [x for x in t.rs if not (x[0] == "d" and x[1] is ev[1])]
            t.rs.append(ev)

    def op(self, engine, fn, r=(), w=()):
        r = list(r)
        w = list(w)
        waits = self._deps(engine, r, w)
        idx = len(self.ops[engine])
        self.ops[engine].append([fn, waits, False, None])
        self._post(("e", engine, idx), r, w)

    def dma(self, out, in_, r=(), w=(), q="sp", **kw):
        r = list(r)
        w = list(w)
        waits = self._deps(q, r, w)
        t0 = w[0]
        if t0.dsem is None or t0.dcnt > 60000:
            t0.dsem = self.new_sem("d")
            t0.dcnt = 0
        t0.dcnt += 16
        ev = ("d", t0.dsem, t0.dcnt)
        self.dma_ev[id(t0.dsem)] = ev

        def fn(eng, out=out, in_=in_, kw=kw):
            return eng.dma_start(out=out, in_=in_, **kw)
        self.ops[q].append([fn, waits, False, t0.dsem])
        self._post(ev, r, w)

    def barrier(self):
        last = {}
        for f in ("pe", "act", "dve", "pool"):
            j = len(self.ops[f]) - 1
            while j >= 0 and (self.ops[f][j][0] is None or self.ops[f][j][3] is not None):
                j -= 1
            last[f] = j
        dm = list(self.dma_ev.values())
        for e in ENG:
            deps = [("e", f, last[f]) for f in ("pe", "act", "dve", "pool") if f != e and last[f] >= 0]
            deps += dm
            waits = self._filter(e, deps)
            self.ops[e].append([None, waits, False, None])

    def wait_all(self, engine, trks):
        waits = self._deps(engine, list(trks), [])
        self.ops[engine].append([None, waits, False, None])

    def emit(self):
        nc = self.nc
        cum = {}
        sems = {}
        for e in ENG:
            c = 0
            arr = []
            for rec in self.ops[e]:
                if rec[2]:
                    c += 1
                arr.append(c)
            cum[e] = arr
            sems[e] = [self.new_sem(f"s{e}") for _ in range(c // self.SEM_CHUNK + 1)]
        CH = self.SEM_CHUNK

        def semval(f, idx):
            c = cum[f][idx]
            ch = (c - 1) // CH
            return sems[f][ch], c - ch * CH

        def run(e, eng):
            for i, (fn, waits, sig, dsem) in enumerate(self.ops[e]):
                for ev in waits:
                    if ev[0] == "e":
                        s, v = semval(ev[1], ev[2])
                        eng.wait_ge(s, v)
                    else:
                        eng.wait_ge(ev[1], ev[2])
                if fn is None:
                    continue
                ins = fn(eng)
                if dsem is not None:
                    ins.then_inc(dsem, 16)
                elif sig:
                    s, v = semval(e, i)
                    ins.then_inc(s, 1)

        with nc.Block() as block:
            @block.tensor
            def _(eng):
                run("pe", eng)

            @block.scalar
            def _(eng):
                run("act", eng)

            @block.vector
            def _(eng):
                run("dve", eng)

            @block.gpsimd
            def _(eng):
                run("pool", eng)

            @block.sync
            def _(eng):
                run("sp", eng)

    def close(self):
        self.stack.close()


N = 2304
NT = 18
D = 1024
KC = 8
NLAT = 2048
ALPHA = 4.0 ** 0.25
EPS = 1e-5
TOKB = [(0, 512), (512, 512), (1024, 512), (1536, 512), (2048, 256)]
PI = math.pi

_SW = list(range(16, 32)) + list(range(0, 16)) + list(range(48, 64)) + list(range(32, 48))


def _cols0():
    cols = []
    for j in range(2):
        for blk in range(2):
            for hh in (4 * j + 2 * blk, 4 * j + 2 * blk + 1):
                cols += [hh * 64 + d for d in range(64)]
        for blk in range(2):
            for hh in (4 * j + 2 * blk, 4 * j + 2 * blk + 1):
                cols += [hh * 64 + d for d in _SW]
        cols += [512 + j * 64 + d for d in range(64)] * 2
        cols += [512 + j * 64 + d for d in _SW] * 2
    cols += list(range(640, 768))
    for h in range(4):
        cols += [768 + h * 128 + d for d in range(128)]
        cols += [1280 + h * 128 + d for d in range(128)]
        cols += [1792 + h * 128 + d for d in range(128)]
        cols += [2816 + h * 128 + d for d in range(128)]
    cols += list(range(2304, 2816))
    return cols


def _cols1():
    cols = []
    for h in range(4):
        cols += [h * 96 + d for d in range(96)]
        cols += [384 + h * 96 + d for d in range(96)]
        cols += [1568 + h * 192 + d for d in range(192)]
    cols += list(range(2336, 2592))
    cols += list(range(1536, 1568))
    cols += list(range(768, 1536))
    return cols


class K:
    pass


def build(dbg=(), stop=None):
    nc = bass.Bass("TRN2", target_bir_lowering=False)
    S = Sched(nc)
    k = K()
    k.nc = nc
    k.S = S
    k.dbg = set(dbg)
    k.sub = stop
    k.dbg_out = []

    def din(name, shape, dt=F32):
        return nc.dram_tensor(name, list(shape), dt, kind="ExternalInput").ap()

    x_d = din("x", [NLAT, D])
    ctx_d = din("ctx", [256, D])
    cv_d = din("cvec", [2, D])
    wada_d = din("w_ada", [2, D, 6 * D])
    bada_d = din("b_ada", [2, 6 * D])
    lng_d = din("ln_g", [2, 2, D])
    lnb_d = din("ln_b", [2, 2, D])
    w0_d = din("w0a", [D, 4224])
    sink_d = din("attn_sink", [1, 8])
    lbl_d = din("lb_logits", [2, 2, 512])
    hn_d = din("hgrn_norm", [1, 512])
    wo0_d = din("w_out_even", [D, D])
    w1_d = din("w1a", [D, 2592])
    gw_d = din("gla_gate_w", [2, 16, 384])
    gb_d = din("gla_gate_b", [2, 384])
    gn_d = din("gla_norm", [1, 768])
    wo1_d = din("w_out_odd", [D, D])
    wr_d = din("w_router", [D, 16])
    br_d = din("b_router", [1, 16])
    eg_d = din("w_expert_gate", [2, 16, D, 512])
    eu_d = din("w_expert_up", [2, 16, D, 512])
    ed_d = din("w_expert_down", [2, 16, 512, D])
    out_d = nc.dram_tensor("out", [NLAT, D], F32, kind="ExternalOutput").ap()
    hmid_d = nc.dram_tensor("hmid", [N, D], F32).ap()
    hout0_d = nc.dram_tensor("hout0", [N, D], F32).ap()
    mix_d = nc.dram_tensor("mixd", [10, 128, N], BF16).ap()
    t_hmid = [Trk() for _ in range(NT)]
    t_hout0 = [Trk() for _ in range(NT)]
    t_mixd = [Trk() for _ in range(10)]
    t_out = [Trk() for _ in range(16)]

    def dump(name, src_ap, shape, dt, r):
        if name not in k.dbg:
            return
        d = nc.dram_tensor("dbg_" + name, list(shape), dt, kind="ExternalOutput").ap()
        t = Trk()
        S.dma(d, src_ap, r=r, w=[t])
        k.dbg_out.append(t)

    PS = [S.psum(f"ps{i}", [128, 1024]) for i in range(4)]
    PT = [[Trk(), Trk()] for _ in range(4)]
    k.bank_i = 0
    k.pair_i = 0

    k.reserved = set()

    def bank():
        i = k.bank_i
        while i in k.reserved:
            i = (i + 1) % 8
        k.bank_i = (i + 1) % 8
        k.last_bank = i
        return PS[i // 2][:, (i % 2) * 512:(i % 2 + 1) * 512], PT[i // 2][i % 2]

    def pair():
        i = k.pair_i
        k.pair_i = (i + 1) % 4
        k.bank_i = (2 * i + 2) % 8
        return PS[i], PT[i]

    identF = S.sbuf("identF", [128, 128], F32); t_c = Trk()
    identB = S.sbuf("identB", [128, 128], BF16)
    onesF = S.sbuf("onesF", [128, 128], F32)
    onesB = S.sbuf("onesB", [128, 128], BF16)
    cst = S.sbuf("cst", [128, 8], F32)
    Mf = S.sbuf("Mf", [128, 128], BF16)
    Mb = S.sbuf("Mb", [128, 128], BF16)
    MP = S.sbuf("MP", [128, 512], BF16)
    MN = S.sbuf("MN", [128, 512], BF16)
    rmask = S.sbuf("rmask", [128, N], BF16)
    modT = S.sbuf("modT", [128, 2, 48, 2], F32); t_modT = Trk()
    mod_d = nc.dram_tensor("mod_d", [2, 2, 6 * D], F32).ap(); t_mod = Trk()
    bc = S.sbuf("bc", [128, 4, D], F32); t_bc = [Trk() for _ in range(4)]
    uT = S.sbuf("uT", [128, KC, N], BF16); t_uT = [Trk() for _ in range(NT)]
    wbuf = S.sbuf("wbuf", [128, 5, 4096], BF16); t_wb = [Trk() for _ in range(5)]
    RB = S.sbuf("RB", [128, 6, N], F32); t_rb = [Trk() for _ in range(6)]
    HB = S.sbuf("HB", [128, 8, N], BF16); t_hb = [Trk() for _ in range(8)]
    sm = S.sbuf("sm", [128, 640], F32)
    gates = S.sbuf("gates", [128, NT, 16], F32); t_gates = [Trk() for _ in range(NT)]
    wrt = S.sbuf("wrt", [128, KC, 16], F32); t_wr = Trk()
    brb = S.sbuf("brb", [128, 16], F32)

    S.op("pool", lambda e: e.memset(identF[:], 0.0), w=[t_c])
    S.op("pool", lambda e: e.affine_select(out=identF[:], in_=identF[:], pattern=[[-1, 128]], compare_op=ALU.not_equal,
                                           fill=1.0, base=0, channel_multiplier=1), r=[t_c], w=[t_c])
    S.op("pool", lambda e: e.tensor_copy(out=identB[:], in_=identF[:]), r=[t_c], w=[t_c])
    S.op("pool", lambda e: e.memset(onesF[:], 1.0), w=[t_c])
    S.op("pool", lambda e: e.memset(onesB[:], 1.0), w=[t_c])
    for j_, v_ in enumerate((EPS, 1.0, -PI, 0.0, PI)):
        S.op("pool", lambda e, j_=j_, v_=v_: e.memset(cst[:, j_:j_ + 1], v_), w=[t_c])
    S.op("pool", lambda e: e.memset(Mf[:], 1.0), w=[t_c])
    S.op("pool", lambda e: e.affine_select(out=Mf[:], in_=Mf[:], pattern=[[1, 128]], compare_op=ALU.is_ge, fill=0.0,
                                           base=0, channel_multiplier=-1), r=[t_c], w=[t_c])
    S.op("pool", lambda e: e.memset(Mf[0:64, 64:128], 0.0), r=[t_c], w=[t_c])
    S.op("pool", lambda e: e.memset(Mb[:], 1.0), w=[t_c])
    S.op("pool", lambda e: e.affine_select(out=Mb[:], in_=Mb[:], pattern=[[-1, 128]], compare_op=ALU.is_ge, fill=0.0,
                                           base=0, channel_multiplier=1), r=[t_c], w=[t_c])
    S.op("pool", lambda e: e.memset(Mb[64:128, 0:64], 0.0), r=[t_c], w=[t_c])
    S.op("pool", lambda e: e.memset(MP[:], 1.0), w=[t_c])
    S.op("pool", lambda e: e.affine_select(out=MP[:], in_=MP[:], pattern=[[0, 4], [-1, 128]], compare_op=ALU.is_ge,
                                           fill=0.0, base=0, channel_multiplier=1), r=[t_c], w=[t_c])
    S.op("pool", lambda e: e.memset(MN[:], 1.0), w=[t_c])
    S.op("pool", lambda e: e.affine_select(out=MN[:], in_=MN[:], pattern=[[0, 4], [1, 128]], compare_op=ALU.is_ge,
                                           fill=0.0, base=0, channel_multiplier=-1), r=[t_c], w=[t_c])
    S.op("pool", lambda e: e.memset(rmask[:], 1.0), w=[t_c])
    S.op("pool", lambda e: e.memset(rmask[:].rearrange("p (c l) -> p c l", l=64)[:, :, 0:1], 0.0), r=[t_c], w=[t_c])
    S.dma(wrt[:], wr_d.rearrange("(k p) n -> p k n", p=128), w=[t_wr])
    S.dma(brb[:], br_d[0:1, :].to_broadcast([128, 16]), w=[t_c])
    S.barrier()

    cs = RB[0:2, 4, 0:1024]
    sg_ = RB[0:2, 4, 1024:2048]
    csT = sm[:, 520:536].rearrange("p (k r) -> p k r", r=2)
    t_cs = Trk(); t_csT = Trk()
    k.t_bb = [[Trk(), Trk()], [Trk(), Trk()]]
    S.dma(cs, cv_d[:, :], w=[t_cs])
    S.op("act", lambda e: e.activation(out=sg_, in_=cs, func=AF.Sigmoid), r=[t_cs], w=[t_csT])
    S.op("dve", lambda e: e.tensor_tensor(out=cs, in0=cs, in1=sg_, op=ALU.mult), r=[t_cs, t_csT], w=[t_cs])
    pb, tb = bank()
    for kk in range(KC):
        S.op("pe", lambda e, kk=kk, pb=pb: e.transpose(out=pb[:, 2 * kk:2 * kk + 2], in_=cs[:, kk * 128:(kk + 1) * 128],
                                                       identity=identF[0:2, 0:2]), r=[t_cs, t_c], w=[tb])
    S.op("dve", lambda e, pb=pb: e.tensor_copy(out=csT, in_=pb[:, 0:16].rearrange("p (k r) -> p k r", r=2)), r=[tb], w=[t_csT])
    t_wa = [Trk(), Trk()]
    t_mb = [Trk(), Trk()]
    for l in range(2):
        pT, tT = bank()
        k.reserved = {k.last_bank}
        for j in range(12):
            slot = (l * 12 + j) % 2
            wa = RB[:, slot * 2:slot * 2 + 2, :].rearrange("p a n -> p (a n)")[:, 0:4096].rearrange("p (k n) -> p k n", n=512)
            mb = RB[0:2, 5, slot * 512:(slot + 1) * 512]
            S.dma(wa, wada_d[l, :, j * 512:(j + 1) * 512].rearrange("(k p) n -> p k n", p=128), w=[t_wa[slot]])
            for r_ in range(2):
                S.dma(RB[r_:r_ + 1, 5, 1024 + slot * 512:1024 + (slot + 1) * 512], bada_d[l:l + 1, j * 512:(j + 1) * 512],
                      w=[k.t_bb[slot][r_]])
            pb, tb = bank()
            for kk in range(KC):
                S.op("pe", lambda e, kk=kk, pb=pb, wa=wa: e.matmul(pb[0:2, :], lhsT=csT[:, kk, :], rhs=wa[:, kk, :],
                                                                   start=(kk == 0), stop=(kk == KC - 1)),
                     r=[t_csT, t_wa[slot]], w=[tb])
            bb = RB[0:2, 5, 1024 + slot * 512:1024 + (slot + 1) * 512]
            S.op("dve", lambda e, pb=pb, mb=mb, bb=bb: e.tensor_tensor(out=mb, in0=pb[0:2, :], in1=bb, op=ALU.add),
                 r=[tb] + k.t_bb[slot], w=[t_mb[slot]])
            if j in (2, 3, 8, 9):
                S.op("dve", lambda e, mb=mb: e.tensor_scalar(out=mb, in0=mb, scalar1=1.0, scalar2=None, op0=ALU.add),
                     r=[t_mb[slot]], w=[t_mb[slot]])
            S.dma(mod_d[l, :, j * 512:(j + 1) * 512], mb, r=[t_mb[slot]], w=[t_mod])
            for q_ in range(4):
                jj = j * 4 + q_
                S.op("pe", lambda e, jj=jj, q_=q_, mb=mb, pT=pT: e.transpose(out=pT[:, 2 * jj:2 * jj + 2], in_=mb[:, q_ * 128:(q_ + 1) * 128],
                                                                             identity=identF[0:2, 0:2]), r=[t_mb[slot], t_c], w=[tT])
        S.op("dve", lambda e, l=l, pT=pT: e.tensor_copy(out=modT[:, l, :, :], in_=pT[:, 0:96].rearrange("p (j r) -> p j r", r=2)),
             r=[tT], w=[t_modT])
        k.reserved = set()
        dump(f"mod{l}", mod_d[l, :, :], [2, 6 * D], F32, [t_mod])
    S.barrier()

    def bcast_rows(l, which):
        gi = 2 if which == "mix" else 5
        li = 0 if which == "mix" else 1
        S.dma(bc[:, 0, :], mod_d[l, 0:1, gi * D:(gi + 1) * D].to_broadcast([128, D]), r=[t_mod], w=[t_bc[0]])
        S.dma(bc[:, 1, :], mod_d[l, 1:2, gi * D:(gi + 1) * D].to_broadcast([128, D]), r=[t_mod], w=[t_bc[1]])
        S.dma(bc[:, 2, :], lng_d[l, li:li + 1, :].to_broadcast([128, D]), w=[t_bc[2]])
        S.dma(bc[:, 3, :], lnb_d[l, li:li + 1, :].to_broadcast([128, D]), w=[t_bc[3]])

    def ln_stats(src, t_src, st_ap, mv_ap, rs_ap, nb_ap, t_st):
        for j in range(2):
            S.op("dve", lambda e, j=j: e.bn_stats(out=st_ap[:, j, :], in_=src[:, j * 512:(j + 1) * 512]), r=[t_src], w=[t_st])
        S.op("dve", lambda e: e.bn_aggr(out=mv_ap, in_=st_ap), r=[t_st], w=[t_st])
        S.op("act", lambda e: e.activation(out=rs_ap, in_=mv_ap[:, 1:2], func=AF.Sqrt, bias=cst[:, 0:1]), r=[t_st, t_c], w=[t_st])
        S.op("dve", lambda e: e.reciprocal(out=rs_ap, in_=rs_ap), r=[t_st], w=[t_st])
        S.op("dve", lambda e: e.tensor_scalar(out=nb_ap, in0=mv_ap[:, 0:1], scalar1=rs_ap, scalar2=-1.0, op0=ALU.mult, op1=ALU.mult),
             r=[t_st], w=[t_st])

    k.st_i = 0

    def st_slot():
        i = k.st_i
        k.st_i = (i + 1) % 8
        base = i * 24
        return (sm[:, base:base + 12].rearrange("p (a b) -> p a b", b=6), sm[:, base + 12:base + 14],
                sm[:, base + 14:base + 15], sm[:, base + 15:base + 16], k.t_st[i])
    k.t_st = [Trk() for _ in range(8)]

    def ln_part1(src, t_src, xn, t_xn):
        st_ap, mv_ap, rs_ap, nb_ap, t_st = st_slot()
        ln_stats(src, t_src, st_ap, mv_ap, rs_ap, nb_ap, t_st)
        S.op("act", lambda e: e.activation(out=xn, in_=src, func=AF.Identity, scale=rs_ap, bias=nb_ap), r=[t_src, t_st], w=[t_xn])

    def ln_part2(l, xn, t_xn, i, which, router, uf=None, t_uf=None):
        r_ = 1 if i < 2 else 0
        pp, tp = pair()
        for kk in range(KC):
            S.op("pe", lambda e, kk=kk: e.transpose(out=pp[:, kk * 128:(kk + 1) * 128], in_=xn[:, kk * 128:(kk + 1) * 128],
                                                    identity=identF[:]), r=[t_xn, t_c], w=[tp[kk // 4]])
        for kk in range(KC):
            sc_ap = modT[:, l, (which + 1) * 8 + kk, r_:r_ + 1]
            sh_ap = modT[:, l, which * 8 + kk, r_:r_ + 1]
            if router:
                o_ap = uf[:, kk, :]
                tw = t_uf
            else:
                o_ap = uT[:, kk, i * 128:(i + 1) * 128]
                tw = t_uT[i]
            if kk % 2 == 0:
                S.op("act", lambda e, kk=kk, o_ap=o_ap, sc_ap=sc_ap, sh_ap=sh_ap: e.activation(
                    out=o_ap, in_=pp[:, kk * 128:(kk + 1) * 128], func=AF.Identity, scale=sc_ap, bias=sh_ap),
                    r=[tp[kk // 4], t_modT], w=[tw])
            else:
                S.op("dve", lambda e, kk=kk, o_ap=o_ap, sc_ap=sc_ap, sh_ap=sh_ap: e.tensor_scalar(
                    out=o_ap, in0=pp[:, kk * 128:(kk + 1) * 128], scalar1=sc_ap, scalar2=sh_ap, op0=ALU.mult, op1=ALU.add),
                    r=[tp[kk // 4], t_modT], w=[tw])
        if router:
            S.op("pool", lambda e: e.tensor_copy(out=uT[:, :, i * 128:(i + 1) * 128], in_=uf[:, :, :]), r=[t_uf], w=[t_uT[i]])

    def ln_to_uT(l, src, t_src, xn, t_xn, i, which, router, uf=None, t_uf=None):
        ln_part1(src, t_src, xn, t_xn)
        ln_part2(l, xn, t_xn, i, which, router, uf, t_uf)
        if router:
            route(i, uf, t_uf)

    def route(i, uf, t_uf):
        pb, tb = bank()
        for kk in range(KC):
            S.op("pe", lambda e, kk=kk: e.matmul(pb[:, 0:16], lhsT=uf[:, kk, :], rhs=wrt[:, kk, :], start=(kk == 0),
                                                 stop=(kk == KC - 1)), r=[t_uf, t_wr], w=[tb])
        S.op("act", lambda e: e.activation(out=gates[:, i, :], in_=pb[:, 0:16], func=AF.Copy), r=[tb], w=[t_gates[i]])

    def route_all(t0, nt, scr, t_scr):
        n16 = nt * 16
        o = [0]

        def take(n):
            a = scr[:, o[0]:o[0] + n]
            o[0] += n
            return a
        lg = gates[:, t0:t0 + nt, :]
        pr, sl, s2, eq, eq2 = [take(n16).rearrange("p (t e) -> p t e", e=16) for _ in range(5)]
        g4, g4b, g4c = [take(nt * 4).rearrange("p (t g) -> p t g", g=4) for _ in range(3)]
        sc1, sc2 = take(nt), take(nt)
        BIG = 1.0e4
        tg = t_gates[t0:t0 + nt]

        def dv(fn):
            S.op("dve", fn, r=t_scr + [k.t_c] + tg, w=t_scr)

        def bc16(a):
            return a.unsqueeze(2).to_broadcast([128, nt, 16])

        def g4v(a):
            return a.rearrange("p t (g e) -> p (t g) e", e=4)

        def g4f(a):
            return a.rearrange("p t g -> p (t g)")
        dv(lambda e: e.tensor_reduce(out=sc1, in_=lg, axis=AX.X, op=ALU.max))
        dv(lambda e: e.tensor_tensor(out=pr, in0=lg, in1=bc16(sc1), op=ALU.subtract))
        S.op("act", lambda e: e.activation(out=pr, in_=pr, func=AF.Exp), r=t_scr, w=t_scr)
        dv(lambda e: e.tensor_reduce(out=sc2, in_=pr, axis=AX.X, op=ALU.add))
        dv(lambda e: e.reciprocal(out=sc2, in_=sc2))
        dv(lambda e: e.tensor_tensor(out=pr, in0=pr, in1=bc16(sc2), op=ALU.mult))
        dv(lambda e: e.tensor_tensor(out=sl, in0=pr, in1=brb[:].unsqueeze(1).to_broadcast([128, nt, 16]), op=ALU.add))
        dv(lambda e: e.tensor_reduce(out=g4f(g4), in_=g4v(sl), axis=AX.X, op=ALU.max))
        dv(lambda e: e.tensor_tensor(out=g4v(eq), in0=g4v(sl), in1=g4f(g4).unsqueeze(2).to_broadcast([128, nt * 4, 4]), op=ALU.is_equal))
        dv(lambda e: e.scalar_tensor_tensor(out=s2.rearrange("p t e -> p (t e)"), in0=eq.rearrange("p t e -> p (t e)"), scalar=-BIG,
                                            in1=sl.rearrange("p t e -> p (t e)"), op0=ALU.mult, op1=ALU.add))
        dv(lambda e: e.tensor_reduce(out=g4f(g4b), in_=g4v(s2), axis=AX.X, op=ALU.max))
        dv(lambda e: e.tensor_tensor(out=g4f(g4), in0=g4f(g4), in1=g4f(g4b), op=ALU.add))
        dv(lambda e: e.tensor_reduce(out=sc1, in_=g4, axis=AX.X, op=ALU.max))
        dv(lambda e: e.tensor_tensor(out=g4c, in0=g4, in1=sc1.unsqueeze(2).to_broadcast([128, nt, 4]), op=ALU.is_equal))
        dv(lambda e: e.tensor_scalar(out=g4f(g4c), in0=g4f(g4c), scalar1=-1.0, scalar2=BIG, op0=ALU.add, op1=ALU.mult))
        dv(lambda e: e.tensor_tensor(out=g4v(s2), in0=g4v(sl), in1=g4f(g4c).unsqueeze(2).to_broadcast([128, nt * 4, 4]), op=ALU.add))
        dv(lambda e: e.tensor_reduce(out=sc1, in_=s2, axis=AX.X, op=ALU.max))
        dv(lambda e: e.tensor_tensor(out=eq, in0=s2, in1=bc16(sc1), op=ALU.is_equal))
        dv(lambda e: e.scalar_tensor_tensor(out=s2.rearrange("p t e -> p (t e)"), in0=eq.rearrange("p t e -> p (t e)"), scalar=-BIG,
                                            in1=s2.rearrange("p t e -> p (t e)"), op0=ALU.mult, op1=ALU.add))
        dv(lambda e: e.tensor_reduce(out=sc1, in_=s2, axis=AX.X, op=ALU.max))
        dv(lambda e: e.tensor_tensor(out=eq2, in0=s2, in1=bc16(sc1), op=ALU.is_equal))
        dv(lambda e: e.tensor_tensor(out=eq, in0=eq, in1=eq2, op=ALU.add))
        dv(lambda e: e.tensor_tensor(out=eq, in0=eq, in1=pr, op=ALU.mult))
        dv(lambda e: e.tensor_reduce(out=sc2, in_=eq, axis=AX.X, op=ALU.add))
        dv(lambda e: e.reciprocal(out=sc2, in_=sc2))
        S.op("dve", lambda e: e.tensor_tensor(out=lg, in0=eq, in1=bc16(sc2), op=ALU.mult), r=t_scr, w=tg)
    k.route_all = route_all
    k.t_route = [Trk(), Trk()]
    k.t_tile = [Trk() for _ in range(12)]

    k.wslot = 0

    def load_w(src_ap, nslots=1, parts=128):
        s0 = k.wslot
        if s0 + nslots > 5:
            s0 = 0
        k.wslot = (s0 + nslots) % 5
        a, b = src_ap.shape[1], src_ap.shape[2]
        dst = wbuf[0:parts, s0:s0 + nslots, :].rearrange("p s n -> p (s n)")[:, 0:a * b].rearrange("p (a b) -> p a b", b=b)
        trks = t_wb[s0:s0 + nslots]
        S.dma(dst, src_ap, w=trks, q="pool")
        return dst, trks

    def proj_fm(wv, t_w, c0, M, evac, toks=TOKB):
        for (t0, nt) in toks:
            pb, tb = bank()
            for kk in range(KC):
                S.op("pe", lambda e, kk=kk, pb=pb, t0=t0, nt=nt: e.matmul(pb[0:M, 0:nt], lhsT=wv[:, kk, c0:c0 + M],
                                                                          rhs=uT[:, kk, t0:t0 + nt], start=(kk == 0), stop=(kk == KC - 1)),
                     r=t_w + t_uT[t0 // 128:(t0 + nt) // 128], w=[tb])
            evac(pb, tb, t0, nt)

    def proj_tm(wv, t_w, c0, ncol, evac, tiles=range(NT)):
        for i in tiles:
            pb, tb = bank()
            for kk in range(KC):
                S.op("pe", lambda e, kk=kk, pb=pb, i=i: e.matmul(pb[:, 0:ncol], lhsT=uT[:, kk, i * 128:(i + 1) * 128],
                                                                 rhs=wv[:, kk, c0:c0 + ncol], start=(kk == 0), stop=(kk == KC - 1)),
                     r=t_w + [t_uT[i]], w=[tb])
            evac(pb, tb, i)

    k.proj_fm = proj_fm
    k.proj_tm = proj_tm
    k.load_w = load_w
    k.bank = bank
    k.pair = pair
    k.dump = dump
    k.ln_to_uT = ln_to_uT
    k.ln_part1 = ln_part1
    k.ln_part2 = ln_part2
    k.route = route
    k.ln_stats = ln_stats
    k.st_slot = st_slot
    k.bcast_rows = bcast_rows
    for nm in ("x_d ctx_d w0_d sink_d lbl_d hn_d wo0_d w1_d gw_d gb_d gn_d wo1_d eg_d eu_d ed_d out_d hmid_d hout0_d mix_d "
               "t_hmid t_hout0 t_mixd t_out identF identB onesF onesB cst Mf Mb MP MN rmask modT t_modT mod_d t_mod bc t_bc uT t_uT "
               "wbuf t_wb RB t_rb HB t_hb sm gates t_gates t_c PS PT").split():
        setattr(k, nm, locals()[nm])

    for ph, l in [("B", 0), ("M", 0), ("D", 0), ("E", 0), ("B", 1), ("M", 1), ("D", 1), ("E", 1)]:
        if ph == "B":
            phase_B(k, l)
        elif ph == "M":
            (mixer_even if l == 0 else mixer_odd)(k)
        elif ph == "D":
            phase_D(k, l)
        else:
            phase_E(k, l)
        S.barrier()
        if stop is not None and stop[0:2] == f"{ph}{l}":
            break

    S.wait_all("sp", t_out + k.dbg_out)
    S.emit()
    S.close()
    return nc


def phase_B(k, l):
    S = k.S
    bufs = {}

    def stA(i):
        slot = i % 2
        ht = k.RB[:, slot, 0:1024]
        t_ht = k.t_tile[slot]
        xn = k.RB[:, 2 + slot, 0:1024]
        t_xn = k.t_tile[2 + slot]
        if l == 0:
            src = k.ctx_d[i * 128:(i + 1) * 128, :] if i < 2 else k.x_d[(i - 2) * 128:(i - 1) * 128, :]
            S.dma(ht, src, w=[t_ht])
        else:
            S.dma(ht, k.hout0_d[i * 128:(i + 1) * 128, :], r=[k.t_hout0[i]], w=[t_ht])
        k.ln_part1(ht, t_ht, xn, t_xn)
        bufs[i] = (xn, t_xn)

    def stB(i):
        xn, t_xn = bufs.pop(i)
        k.ln_part2(l, xn, t_xn, i, 0, False)
    for n in range(NT + 1):
        if n < NT:
            stA(n)
        if n >= 1:
            stB(n - 1)
    k.dump(f"uT{l}", k.uT[:, :, :], [128, KC, N], BF16, k.t_uT)


def row_to_col(k, src, nr, n, dst, t_src, t_dst):
    S = k.S
    pb, tb = k.bank()
    S.op("pe", lambda e: e.transpose(out=pb[0:n, 0:nr], in_=src, identity=k.identF[0:nr, 0:nr]), r=[t_src, k.t_c], w=[tb])
    S.op("dve", lambda e: e.tensor_copy(out=dst, in_=pb[0:n, 0:nr]), r=[tb], w=[t_dst])


def proj_block(k, wv, t_w, c0, M, t0, nt):
    S = k.S
    pb, tb = k.bank()
    for kk in range(KC):
        S.op("pe", lambda e, kk=kk: e.matmul(pb[0:M, 0:nt], lhsT=wv[:, kk, c0:c0 + M], rhs=k.uT[:, kk, t0:t0 + nt],
                                             start=(kk == 0), stop=(kk == KC - 1)),
             r=t_w + k.t_uT[t0 // 128:(t0 + nt) // 128], w=[tb])
    return pb, tb


def gated_scan(k, dk, dvh, nh, q_ap, t_q, k_ap, t_k, A, t_A, B, t_B, make_logf, v_fn, t_v, o_acc, t_o, qts, t_qts, kts, t_kts, L=64):
    S = k.S
    sc = k.scn
    NC_ = N // L
    cpt = 128 // L
    dvt = dvh * nh
    B3 = B.rearrange("p (c l) -> p c l", l=L)
    for a in range(nh):
        S.op("pool", lambda e, a=a: e.memset(o_acc[a], 0.0), w=[t_o[a]])
    D_ = []
    for d in range(2):
        qt, kt, t_qt, t_kt = qts[d], kts[d], t_qts[d], t_kts[d]
        t_sc = k.t_scn[d]
        rr = sc["rr"][0:dk, d, 0:NC_]
        gg = sc["gg"][0:dk, d, 0:NC_]
        X1 = sc["X1"][0:dk, d, 0:NC_]
        X2 = sc["X2"][0:dk, d, 0:NC_]
        EG = sc["EG"][0:dk, d, 0:NC_]
        make_logf(d)
        S.op("dve", lambda e: e.tensor_tensor_scan(out=B, data0=k.rmask[0:dk, :], data1=A, initial=0.0, op0=ALU.mult, op1=ALU.add),
             r=[t_A, k.t_c], w=[t_B])
        S.op("pool", lambda e, gg=gg: e.tensor_copy(out=gg, in_=B3[:, :, L - 1]), r=[t_B], w=[t_sc])
        if d == 1:
            S.op("pool", lambda e: e.tensor_tensor(out=B, in0=B, in1=A, op=ALU.subtract), r=[t_B, t_A], w=[t_B])
        S.op("pool", lambda e, rr=rr: e.tensor_copy(out=rr, in_=B3[:, :, L // 2]), r=[t_B], w=[t_sc])
        S.op("dve", lambda e, rr=rr: e.tensor_tensor(out=B3, in0=B3, in1=rr.unsqueeze(2).to_broadcast([dk, NC_, L]), op=ALU.subtract),
             r=[t_B, t_sc], w=[t_B])
        sgn = 1.0 if d == 0 else -1.0
        S.op("act", lambda e, sgn=sgn: e.activation(out=A, in_=B, func=AF.Exp, scale=sgn), r=[t_B], w=[t_A])
        S.op("dve", lambda e, qt=qt: e.tensor_tensor(out=qt, in0=q_ap, in1=A, op=ALU.mult), r=[t_q, t_A], w=[t_qt])
        S.op("act", lambda e, sgn=sgn: e.activation(out=A, in_=B, func=AF.Exp, scale=-sgn), r=[t_B, t_qt], w=[t_A])
        S.op("dve", lambda e, kt=kt: e.tensor_tensor(out=kt, in0=k_ap, in1=A, op=ALU.mult), r=[t_k, t_A], w=[t_kt])
        S.op("act", lambda e, X1=X1, rr=rr: e.activation(out=X1, in_=rr, func=AF.Exp), r=[t_sc], w=[t_sc])
        S.op("act", lambda e, EG=EG, gg=gg: e.activation(out=EG, in_=gg, func=AF.Exp), r=[t_sc], w=[t_sc])
        S.op("dve", lambda e, X2=X2, gg=gg, rr=rr: e.tensor_tensor(out=X2, in0=gg, in1=rr, op=ALU.subtract), r=[t_sc], w=[t_sc])
        S.op("act", lambda e, X2=X2: e.activation(out=X2, in_=X2, func=AF.Exp), r=[t_sc], w=[t_sc])
        a_s, c_s = (X1, X2) if d == 0 else (X2, X1)
        fo = tuple(range(cpt))
        bo = tuple(reversed(range(cpt)))
        if d == 0:
            tiles = [(i, fo) for i in range(NT)]
        else:
            tiles = [(i, bo) for i in (1, 0)] + [(i, bo) for i in range(NT - 1, 1, -1)]
        st = {"d": d, "qt": qt, "kt": kt, "t_qt": t_qt, "t_kt": t_kt, "t_sc": t_sc, "EG": EG, "a_s": a_s, "c_s": c_s,
              "M": k.Mf if d == 0 else k.Mb, "tiles": tiles, "s_i": 0, "sb_i": 0, "am_i": 0, "pend": None, "nchunk": 0, "ds_i": 0}
        S.op("pool", lambda e, d=d: e.memset(sc["Sst"][0:dk, d, 0, 0:dvt], 0.0), w=[k.t_Sst[d][0]])
        S.op("pool", lambda e, d=d: e.memset(sc["Sbf"][0:dk, d, 0, 0:dvt], 0.0), w=[k.t_Sbf[d][0]])
        D_.append(st)

    def stage1(st, i):
        d = st["d"]
        tk0 = i * 128
        vt = v_fn(i)
        am_i = st["am_i"]
        st["am_i"] = 1 - am_i
        Am = sc["Am"][:, d, am_i, :]
        ktok = sc["ktok"][:, d, am_i, 0:dk]
        t_Am = k.t_Am2[d][am_i]
        t_kk = k.t_ktok2[d][am_i]
        qt, kt = st["qt"], st["kt"]
        pA, tA = k.bank()
        S.op("pe", lambda e: e.matmul(pA[:, 0:128], lhsT=kt[:, tk0:tk0 + 128], rhs=qt[:, tk0:tk0 + 128], start=True, stop=True),
             r=[st["t_kt"], st["t_qt"]], w=[tA])
        M_ = st["M"]
        S.op("dve", lambda e: e.tensor_tensor(out=Am, in0=pA[:, 0:128], in1=M_[:], op=ALU.mult), r=[tA, k.t_c], w=[t_Am])
        pK, tK = k.bank()
        S.op("pe", lambda e: e.matmul(pK[:, 0:dk], lhsT=kt[:, tk0:tk0 + 128], rhs=k.identB[0:dk, 0:dk], start=True, stop=True),
             r=[st["t_kt"], k.t_c], w=[tK])
        S.op("act", lambda e: e.activation(out=ktok, in_=pK[:, 0:dk], func=AF.Copy), r=[tK], w=[t_kk])
        pI, tI = k.bank()
        for a in range(nh):
            S.op("pe", lambda e, a=a: e.matmul(pI[0:dvh, a * 128:(a + 1) * 128], lhsT=vt[:, a * dvh:(a + 1) * dvh], rhs=Am[:, :], start=True, stop=True),
                 r=t_v + [t_Am], w=[tI])
        pS = [None] * cpt
        c_s = st["c_s"]
        for hf in range(cpt):
            pS_, tS_ = k.bank()
            rows = slice(hf * L, hf * L + L)
            S.op("pe", lambda e, pS_=pS_, rows=rows: e.matmul(pS_[0:dk, 0:dvt], lhsT=ktok[rows, :], rhs=vt[rows, 0:dvt], start=True, stop=True),
                 r=[t_kk] + t_v, w=[tS_])
            ds_i = st["ds_i"]
            st["ds_i"] = (ds_i + 1) % 4
            dSs = sc["dSs"][0:dk, d, ds_i, 0:dvt]
            c = cpt * i + hf
            S.op("act", lambda e, pS_=pS_, dSs=dSs, c=c: e.activation(out=dSs, in_=pS_[0:dk, 0:dvt], func=AF.Identity, scale=c_s[:, c:c + 1]),
                 r=[tS_, st["t_sc"]], w=[k.t_dSs[d][ds_i]])
            pS[hf] = (dSs, k.t_dSs[d][ds_i])
        for a in range(nh):
            S.op("dve", lambda e, a=a: e.tensor_tensor(out=o_acc[a][:, tk0:tk0 + 128], in0=pI[0:dvh, a * 128:(a + 1) * 128],
                                                       in1=o_acc[a][:, tk0:tk0 + 128], op=ALU.add), r=[tI, t_o[a]], w=[t_o[a]])
        st["pend"] = (i, pS)

    def stage2_chunk(st, i, hf, pS, po, tpo, last):
        d = st["d"]
        c = cpt * i + hf
        tok0 = c * L
        qt = st["qt"]
        sb_i = st["sb_i"]
        Sb = sc["Sbf"][0:dk, d, sb_i, 0:dvt]
        for a in range(nh):
            S.op("pe", lambda e, a=a: e.matmul(po[0:dvh, a * 128 + hf * L:a * 128 + hf * L + L], lhsT=Sb[:, a * dvh:(a + 1) * dvh],
                                               rhs=qt[:, tok0:tok0 + L], start=True, stop=True), r=[k.t_Sbf[d][sb_i], st["t_qt"]], w=[tpo])
        if last:
            return
        s_i = st["s_i"]
        Sc = sc["Sst"][0:dk, d, s_i, 0:dvt]
        Sn = sc["Sst"][0:dk, d, 1 - s_i, 0:dvt]
        EG, a_s = st["EG"], st["a_s"]
        dSs, t_dS = pS[hf]
        S.op("dve", lambda e: e.scalar_tensor_tensor(out=Sn, in0=Sc, scalar=EG[:, c:c + 1], in1=dSs, op0=ALU.mult, op1=ALU.add),
             r=[k.t_Sst[d][s_i], t_dS, st["t_sc"]], w=[k.t_Sst[d][1 - s_i]])
        st["s_i"] = 1 - s_i
        n_ = st["nchunk"] + 1
        ti, hfo = st["tiles"][n_ // cpt]
        cn = cpt * ti + hfo[n_ % cpt]
        Sbn = sc["Sbf"][0:dk, d, 1 - sb_i, 0:dvt]
        S.op("act", lambda e: e.activation(out=Sbn, in_=Sn, func=AF.Identity, scale=a_s[:, cn:cn + 1]),
             r=[k.t_Sst[d][1 - s_i], st["t_sc"]], w=[k.t_Sbf[d][1 - sb_i]])
        st["sb_i"] = 1 - sb_i

    nT = NT
    for step in range(nT + 1):
        if step < nT:
            for st in D_:
                stage1(st, st["tiles"][step][0])
        if step >= 1:
            pend = []
            for st in D_:
                i, hfo = st["tiles"][step - 1]
                po, tpo = k.bank()
                pend.append((st, i, hfo, po, tpo))
            for which in range(cpt):
                for (st, i, hfs, po, tpo) in pend:
                    pS = st["pS_prev"]
                    last = (st["nchunk"] == cpt * nT - 1)
                    stage2_chunk(st, i, hfs[which], pS, po, tpo, last)
                    st["nchunk"] += 1
            for (st, i, hfs, po, tpo) in pend:
                tk0 = i * 128
                for a in range(nh):
                    S.op("dve", lambda e, a=a, po=po, tk0=tk0: e.tensor_tensor(out=o_acc[a][:, tk0:tk0 + 128], in0=po[0:dvh, a * 128:(a + 1) * 128],
                                                                               in1=o_acc[a][:, tk0:tk0 + 128], op=ALU.add), r=[tpo, t_o[a]], w=[t_o[a]])
        for st in D_:
            if st["pend"] is not None:
                st["pS_prev"] = st["pend"][1]


def rms_gate_out(k, dvh, nh, o_acc, t_o, A, t_A, B, t_B, gsil, t_gs, gn_cols, t_gn, mixrow, t_mix, chunk_ids):
    S = k.S
    dv = dvh * nh
    for (t0, nt) in TOKB:
        pb, tb = k.bank()
        for a in range(nh):
            S.op("act", lambda e, a=a, t0=t0, nt=nt: e.activation(out=A[0:dvh, a * 512:a * 512 + nt], in_=o_acc[a][:, t0:t0 + nt], func=AF.Square),
                 r=[t_o[a]], w=[t_A])
        for a in range(nh):
            S.op("pe", lambda e, a=a, pb=pb, nt=nt: e.matmul(pb[0:dvh, 0:nt], lhsT=k.onesF[0:dvh, 0:dvh], rhs=A[0:dvh, a * 512:a * 512 + nt],
                                                             start=(a == 0), stop=(a == nh - 1)), r=[t_A, k.t_c], w=[tb])
        S.op("act", lambda e, pb=pb, nt=nt: e.activation(out=B[0:dvh, 0:nt], in_=pb[0:dvh, 0:nt], func=AF.Sqrt, scale=1.0 / dv, bias=k.cst[0:dvh, 0:1]),
             r=[tb, k.t_c], w=[t_B])
        S.op("dve", lambda e, nt=nt: e.reciprocal(out=B[0:dvh, 0:nt], in_=B[0:dvh, 0:nt]), r=[t_B], w=[t_B])
        for a in range(nh):
            S.op("dve", lambda e, a=a, t0=t0, nt=nt: e.tensor_tensor(out=B[0:dvh, 512 + a * 512:512 + a * 512 + nt], in0=o_acc[a][:, t0:t0 + nt],
                                                                     in1=B[0:dvh, 0:nt], op=ALU.mult), r=[t_o[a], t_B], w=[t_B])
            S.op("dve", lambda e, a=a, t0=t0, nt=nt: e.scalar_tensor_tensor(out=mixrow[a][0:dvh, t0:t0 + nt], in0=B[0:dvh, 512 + a * 512:512 + a * 512 + nt],
                                                                            scalar=gn_cols[a], in1=gsil[a][0:dvh, t0:t0 + nt], op0=ALU.mult, op1=ALU.mult),
                 r=[t_B, t_gn, t_gs[a]], w=[t_mix[a]])
    for a in range(nh):
        S.dma(k.mix_d[chunk_ids[a], 0:dvh, :], mixrow[a][0:dvh, :], r=[t_mix[a]], w=[k.t_mixd[chunk_ids[a]]])


def scan_setup(k):
    S = k.S
    if hasattr(k, "scn"):
        return
    sc = {}
    for nm in ("rr", "gg", "X1", "X2", "EG"):
        sc[nm] = S.sbuf("sc_" + nm, [128, 2, 36], F32)
    sc["Sst"] = S.sbuf("sc_Sst", [128, 2, 2, 192], F32)
    sc["dSs"] = S.sbuf("sc_dSs", [128, 2, 4, 192], BF16)
    sc["Sbf"] = S.sbuf("sc_Sbf", [128, 2, 2, 192], BF16)
    sc["Am"] = S.sbuf("sc_Am", [128, 2, 2, 128], BF16)
    sc["ktok"] = S.sbuf("sc_ktok", [128, 2, 2, 128], BF16)
    sc["col"] = S.sbuf("sc_col", [128, 64], F32)
    sc["row"] = k.RB[0:8, 5, 0:768]
    k.scn = sc
    k.t_scn = [Trk(), Trk()]
    k.t_Sst = [[Trk(), Trk()], [Trk(), Trk()]]
    k.t_dSs = [[Trk() for _ in range(4)], [Trk() for _ in range(4)]]
    k.t_Sbf = [[Trk(), Trk()], [Trk(), Trk()]]
    k.t_Am2 = [[Trk(), Trk()], [Trk(), Trk()]]
    k.t_ktok2 = [[Trk(), Trk()], [Trk(), Trk()]]
    k.t_Am = [k.t_Am2[0][0], k.t_Am2[0][1]]
    k.t_col = Trk()
    k.t_row = k.t_rb[5]


ATOK = [(0, 256), (256, 512), (768, 512), (1280, 512), (1792, 512)]


def mixer_even(k):
    S = k.S
    scan_setup(k)
    sc = k.scn
    RB, HB, t_rb, t_hb = k.RB, k.HB, k.t_rb, k.t_hb
    Ct = RB[:, 4, 0:2048]
    St = RB[:, 5, 0:2048]
    col = sc["col"]
    t_col = k.t_col
    ci = col[:, 0:8].bitcast(I32)
    S.op("pool", lambda e: e.iota(ci[:, 0:1], pattern=[[0, 1]], base=0, channel_multiplier=1), w=[t_col])
    S.op("dve", lambda e: e.tensor_single_scalar(out=ci[:, 1:2], in_=ci[:, 0:1], scalar=15, op=ALU.bitwise_and), r=[t_col], w=[t_col])
    S.op("dve", lambda e: e.tensor_scalar(out=ci[:, 2:3], in0=ci[:, 0:1], scalar1=5, scalar2=1, op0=ALU.logical_shift_right, op1=ALU.bitwise_and),
         r=[t_col], w=[t_col])
    S.op("dve", lambda e: e.tensor_scalar(out=ci[:, 3:4], in0=ci[:, 0:1], scalar1=4, scalar2=1, op0=ALU.logical_shift_right, op1=ALU.bitwise_and),
         r=[t_col], w=[t_col])
    S.op("dve", lambda e: e.tensor_copy(out=col[:, 8:11], in_=ci[:, 1:4]), r=[t_col], w=[t_col])
    S.op("act", lambda e: e.activation(out=col[:, 11:12], in_=col[:, 8:9], func=AF.Exp, scale=-math.log(10000.0) / 16.0), r=[t_col], w=[t_col])
    S.op("dve", lambda e: e.tensor_tensor(out=col[:, 13:14], in0=col[:, 11:12], in1=col[:, 9:10], op=ALU.mult), r=[t_col], w=[t_col])
    S.op("dve", lambda e: e.tensor_tensor(out=col[:, 12:13], in0=col[:, 11:12], in1=col[:, 13:14], op=ALU.subtract), r=[t_col], w=[t_col])
    S.op("dve", lambda e: e.tensor_scalar(out=col[:, 14:15], in0=col[:, 10:11], scalar1=2.0, scalar2=-1.0, op0=ALU.mult, op1=ALU.add),
         r=[t_col], w=[t_col])
    ri = RB[:, 0, 0:2048].bitcast(I32)
    qi = RB[:, 1, 0:2048].bitcast(I32)
    S.op("pool", lambda e: e.iota(ri, pattern=[[1, 32], [0, 64]], base=0, channel_multiplier=0), w=[t_rb[0]])
    S.op("pool", lambda e: e.iota(qi, pattern=[[0, 32], [1, 64]], base=0, channel_multiplier=0), w=[t_rb[1]])
    rf = RB[:, 2, 0:2048]
    qf = RB[:, 3, 0:2048]
    S.op("dve", lambda e: e.tensor_copy(out=rf, in_=ri), r=[t_rb[0]], w=[t_rb[2]])
    S.op("dve", lambda e: e.tensor_copy(out=qf, in_=qi), r=[t_rb[1]], w=[t_rb[3]])
    ang = RB[:, 0, 0:2048]
    S.op("dve", lambda e: e.tensor_scalar(out=ang, in0=rf, scalar1=col[:, 12:13], scalar2=None, op0=ALU.mult), r=[t_rb[2], t_col], w=[t_rb[0]])
    S.op("dve", lambda e: e.scalar_tensor_tensor(out=ang, in0=qf, scalar=col[:, 13:14], in1=ang, op0=ALU.mult, op1=ALU.add),
         r=[t_rb[3], t_rb[0], t_col], w=[t_rb[0]])
    def range_reduce(dst, t_dst, add, tmpi, t_tmpi, tmpf_, t_tmpf):
        S.op("dve", lambda e: e.tensor_scalar(out=tmpi, in0=ang, scalar1=add, scalar2=1.0 / (2 * PI), op0=ALU.add, op1=ALU.mult),
             r=[t_rb[0]], w=[t_tmpi])
        S.op("dve", lambda e: e.tensor_copy(out=tmpf_, in_=tmpi), r=[t_tmpi], w=[t_tmpf])
        S.op("dve", lambda e: e.scalar_tensor_tensor(out=dst, in0=tmpf_, scalar=-2 * PI, in1=ang, op0=ALU.mult, op1=ALU.add),
             r=[t_tmpf, t_rb[0]], w=[t_dst])
        if add != 0.0:
            S.op("dve", lambda e: e.tensor_scalar(out=dst, in0=dst, scalar1=add, scalar2=None, op0=ALU.add), r=[t_dst], w=[t_dst])
        S.op("dve", lambda e: e.tensor_scalar(out=tmpf_, in0=dst, scalar1=PI, scalar2=-2 * PI, op0=ALU.is_gt, op1=ALU.mult),
             r=[t_dst], w=[t_tmpf])
        S.op("dve", lambda e: e.tensor_tensor(out=dst, in0=dst, in1=tmpf_, op=ALU.add), r=[t_dst, t_tmpf], w=[t_dst])
        S.op("dve", lambda e: e.tensor_scalar(out=tmpf_, in0=dst, scalar1=-PI, scalar2=2 * PI, op0=ALU.is_lt, op1=ALU.mult),
             r=[t_dst], w=[t_tmpf])
        S.op("dve", lambda e: e.tensor_tensor(out=dst, in0=dst, in1=tmpf_, op=ALU.add), r=[t_dst, t_tmpf], w=[t_dst])
        S.op("dve", lambda e: e.tensor_scalar(out=dst, in0=dst, scalar1=PI, scalar2=-PI, op0=ALU.min, op1=ALU.max), r=[t_dst], w=[t_dst])
    m1 = RB[:, 1, 0:2048]
    tmpi = RB[:, 2, 0:2048].bitcast(I32)
    tmpf_ = RB[:, 3, 0:2048]
    range_reduce(m1, t_rb[1], 0.0, tmpi, t_rb[2], tmpf_, t_rb[3])
    S.op("act", lambda e: e.activation(out=St, in_=m1, func=AF.Sin, scale=col[:, 14:15]), r=[t_rb[1], t_col], w=[t_rb[5]])
    range_reduce(m1, t_rb[1], PI / 2, tmpi, t_rb[2], tmpf_, t_rb[3])
    S.op("act", lambda e: e.activation(out=Ct, in_=m1, func=AF.Sin), r=[t_rb[1]], w=[t_rb[4]])
    k.dump("ropeC", Ct, [128, 2048], F32, [t_rb[4]])
    k.dump("ropeS", St, [128, 2048], F32, [t_rb[5]])
    if k.sub == "M0a":
        return
    S.dma(col[:, 16:24], k.sink_d[0:1, :].to_broadcast([128, 8]), w=[t_col])
    S.op("act", lambda e: e.activation(out=col[:, 16:24], in_=col[:, 16:24], func=AF.Exp), r=[t_col], w=[t_col])

    qT = HB[:, 0:2, :]
    kAB = [HB[:, 2, :], HB[:, 3, :]]
    vdup = HB[:, 4, :].rearrange("p (i c) -> p i c", c=128)
    mixA = [HB[:, 5, :], HB[:, 6, :]]
    et = [HB[:, 7, ei * 512:(ei + 1) * 512] for ei in range(4)] + [RB[:, 3, 0:256].bitcast(BF16)]
    S.op("pool", lambda e: e.memset(kAB[0][64:128, :], 0.0), w=[t_hb[2]])
    S.op("pool", lambda e: e.memset(kAB[1][0:64, :], 0.0), w=[t_hb[3]])
    t_et = [Trk() for _ in range(5)]
    k.et_i = 0
    tmpf = [RB[:, 0, 0:512], RB[:, 0, 512:1024], RB[:, 1, 0:512], RB[:, 1, 512:1024]]
    t_tmp = [Trk() for _ in range(4)]
    dn = RB[:, 2, 0:512]
    t_dn = t_rb[2]
    wv_v, t_wv = k.load_w(k.w0_d[:, 1536:1664].rearrange("(k p) n -> p k n", p=128))
    for j in range(2):
        base = j * 768
        wq, t_wq = k.load_w(k.w0_d[:, base:base + 512].rearrange("(k p) n -> p k n", p=128))
        wk, t_wk = k.load_w(k.w0_d[:, base + 512:base + 768].rearrange("(k p) n -> p k n", p=128))
        tmp_i = 0
        for (t0, nt) in ATOK:
            for blk in range(3):
                if blk < 2:
                    pq, tq = proj_block(k, wq, t_wq, blk * 128, 128, t0, nt)
                    dst = qT[:, blk, t0:t0 + nt]
                    tdst = t_hb[blk]
                else:
                    pq, tq = proj_block(k, wk, t_wk, 0, 128, t0, nt)
                    dst = None
                if t0 == 0:
                    if blk < 2:
                        S.op("act", lambda e, pq=pq, dst=dst, nt=nt: e.activation(out=dst, in_=pq[:, 0:nt], func=AF.Copy), r=[tq], w=[tdst])
                    else:
                        S.op("act", lambda e, pq=pq, nt=nt, t0=t0: e.activation(out=kAB[0][0:64, t0:t0 + nt], in_=pq[0:64, 0:nt], func=AF.Copy), r=[tq], w=[t_hb[2]])
                        S.op("act", lambda e, pq=pq, nt=nt, t0=t0: e.activation(out=kAB[1][64:128, t0:t0 + nt], in_=pq[64:128, 0:nt], func=AF.Copy), r=[tq], w=[t_hb[3]])
                    continue
                if blk < 2:
                    ps_, ts_ = proj_block(k, wq, t_wq, 256 + blk * 128, 128, t0, nt)
                else:
                    ps_, ts_ = proj_block(k, wk, t_wk, 128, 128, t0, nt)
                l0 = t0 - 256
                ta, tb_ = tmp_i % 4, (tmp_i + 1) % 4
                tmp_i += 2
                S.op("dve", lambda e, pq=pq, ta=ta, l0=l0, nt=nt: e.tensor_tensor(out=tmpf[ta][:, 0:nt], in0=pq[:, 0:nt], in1=Ct[:, l0:l0 + nt], op=ALU.mult),
                     r=[tq, t_rb[4]], w=[t_tmp[ta]])
                S.op("dve", lambda e, ps_=ps_, tb_=tb_, l0=l0, nt=nt: e.tensor_tensor(out=tmpf[tb_][:, 0:nt], in0=ps_[:, 0:nt], in1=St[:, l0:l0 + nt], op=ALU.mult),
                     r=[ts_, t_rb[5]], w=[t_tmp[tb_]])
                if blk < 2:
                    S.op("pool", lambda e, dst=dst, ta=ta, tb_=tb_, nt=nt: e.tensor_tensor(out=dst, in0=tmpf[ta][:, 0:nt], in1=tmpf[tb_][:, 0:nt], op=ALU.add),
                         r=[t_tmp[ta], t_tmp[tb_]], w=[tdst])
                else:
                    S.op("pool", lambda e, ta=ta, tb_=tb_, nt=nt, t0=t0: e.tensor_tensor(out=kAB[0][0:64, t0:t0 + nt], in0=tmpf[ta][0:64, 0:nt], in1=tmpf[tb_][0:64, 0:nt], op=ALU.add),
                         r=[t_tmp[ta], t_tmp[tb_]], w=[t_hb[2]])
                    S.op("pool", lambda e, ta=ta, tb_=tb_, nt=nt, t0=t0: e.tensor_tensor(out=kAB[1][64:128, t0:t0 + nt], in0=tmpf[ta][64:128, 0:nt], in1=tmpf[tb_][64:128, 0:nt], op=ALU.add),
                         r=[t_tmp[ta], t_tmp[tb_]], w=[t_hb[3]])

        if k.sub == "M0p":
            k.dump("qT0", qT, [128, 2, N], BF16, [t_hb[0], t_hb[1]])
            return

        def ev_v(pb, tb, i, j=j):
            S.op("act", lambda e: e.activation(out=vdup[:, i, 0:64], in_=pb[:, j * 64:(j + 1) * 64], func=AF.Copy), r=[tb], w=[t_hb[4]])
            S.op("dve", lambda e: e.tensor_copy(out=vdup[:, i, 64:128], in_=pb[:, j * 64:(j + 1) * 64]), r=[tb], w=[t_hb[4]])
        k.proj_tm(wv_v, t_wv, 0, 128, ev_v)
        if j == 0:
            k.dump("qT0", qT, [128, 2, N], BF16, [t_hb[0], t_hb[1]])
            k.dump("kT0", HB[:, 2:4, :], [128, 2, N], BF16, [t_hb[2], t_hb[3]])
        if k.sub == "M0v":
            return
        for qb in range(NT):
            q0 = qb * 128
            if (k.sub == "M0q1" and qb == 1) or (k.sub == "M0q3" and qb == 3):
                k.dump("mixA", HB[:, 5:7, :], [128, 2, N], BF16, [t_hb[5], t_hb[6]])
                return
            if qb < 2:
                chunks = [(0, None), (1, None)]
            else:
                n_ = qb - 2
                chunks = [(0, None), (1, None)]
                if n_ > 0:
                    chunks.append((qb - 1, k.MP))
                chunks.append((qb, None))
                if n_ < 15:
                    chunks.append((qb + 1, k.MN))
            po, tpo = k.bank()
            pd, tpd = k.bank()
            nch = len(chunks)
            pss_l = []
            for ci_, (kc, msk) in enumerate(chunks):
                pss, tss = k.bank()
                for hh in range(4):
                    blk, half = hh // 2, hh % 2
                    S.op("pe", lambda e, pss=pss, hh=hh, blk=blk, half=half, kc=kc, q0=q0: e.matmul(
                        pss[:, hh * 128:(hh + 1) * 128], lhsT=kAB[half][:, kc * 128:(kc + 1) * 128], rhs=qT[:, blk, q0:q0 + 128],
                        start=True, stop=True), r=[t_hb[2 + half], t_hb[blk]], w=[tss])
                pss_l.append((pss, tss))
            for ci_, (kc, msk) in enumerate(chunks):
                pss, tss = pss_l[ci_]
                ei = ci_
                S.op("act", lambda e, pss=pss, ei=ei: e.activation(out=et[ei], in_=pss[:, :], func=AF.Exp, scale=0.125), r=[tss], w=[t_et[ei]])
                if msk is not None:
                    S.op("dve", lambda e, ei=ei, msk=msk: e.tensor_tensor(out=et[ei], in0=et[ei], in1=msk[:], op=ALU.mult),
                         r=[t_et[ei], k.t_c], w=[t_et[ei]])
            for ci_, (kc, msk) in enumerate(chunks):
                ei = ci_
                S.op("pe", lambda e, ei=ei, kc=kc, ci_=ci_, po=po, nch=nch: e.matmul(
                    po[:, :], lhsT=vdup[:, kc, :], rhs=et[ei][:, :], start=(ci_ == 0), stop=(ci_ == nch - 1)),
                    r=[t_hb[4], t_et[ei]], w=[tpo])
                S.op("pe", lambda e, ei=ei, ci_=ci_, pd=pd, nch=nch: e.matmul(
                    pd[:, :], lhsT=k.onesB[:], rhs=et[ei][:, :], start=(ci_ == 0), stop=(ci_ == nch - 1)),
                    r=[k.t_c, t_et[ei]], w=[tpd])
            if k.sub == "M0qb":
                S.op("dve", lambda e, po=po: e.tensor_copy(out=RB[:, 0, 0:512], in_=po[:, :]), r=[tpo], w=[t_rb[0]])
                S.op("dve", lambda e, pd=pd: e.tensor_copy(out=RB[:, 0, 512:1024], in_=pd[:, :]), r=[tpd], w=[t_rb[0]])
                k.dump("popd", RB[:, 0, 0:1024], [128, 1024], F32, [t_rb[0]])
                return
            for hh in range(4):
                S.op("dve", lambda e, hh=hh, pd=pd, j=j: e.tensor_scalar(out=dn[:, hh * 128:(hh + 1) * 128], in0=pd[:, hh * 128:(hh + 1) * 128],
                                                                    scalar1=col[:, 16 + 4 * j + hh:17 + 4 * j + hh], scalar2=None, op0=ALU.add),
                     r=[tpd, t_col], w=[t_dn])
            S.op("dve", lambda e: e.reciprocal(out=dn, in_=dn), r=[t_dn], w=[t_dn])
            for hh in range(4):
                blk, half = hh // 2, hh % 2
                rows = slice(half * 64, half * 64 + 64)
                S.op("dve", lambda e, hh=hh, blk=blk, rows=rows, po=po, q0=q0: e.tensor_tensor(
                    out=mixA[blk][rows, q0:q0 + 128], in0=po[rows, hh * 128:(hh + 1) * 128], in1=dn[rows, hh * 128:(hh + 1) * 128], op=ALU.mult),
                    r=[tpo, t_dn], w=[t_hb[5 + blk]])
        for blk in range(2):
            S.dma(k.mix_d[2 * j + blk, :, :], mixA[blk], r=[t_hb[5 + blk]], w=[k.t_mixd[2 * j + blk]])
    k.dump("mixd_att", k.mix_d[0:4, :, :], [4, 128, N], BF16, k.t_mixd[0:4])
    S.barrier()
    if k.sub == "M0b":
        return

    row = sc["row"]
    t_row = k.t_row
    S.dma(row[0:4, 0:512], k.lbl_d.rearrange("r a n -> (r a) n"), w=[t_row])
    for h in range(4):
        row_to_col(k, row[0:4, h * 128:(h + 1) * 128], 4, 128, col[:, 24 + 4 * h:28 + 4 * h], t_row, t_col)
    lbT = col[:, 24:40].rearrange("p (h r a) -> p h r a", r=2, a=2)
    lbv = col[:, 44:52].rearrange("p (h r) -> p h r", r=2)
    omv = col[:, 52:60].rearrange("p (h r) -> p h r", r=2)
    S.op("dve", lambda e: e.tensor_tensor(out=lbv, in0=lbT[:, :, :, 0], in1=lbT[:, :, :, 1], op=ALU.subtract), r=[t_col], w=[t_col])
    S.op("act", lambda e: e.activation(out=lbv, in_=lbv, func=AF.Sigmoid), r=[t_col], w=[t_col])
    S.op("dve", lambda e: e.tensor_scalar(out=omv, in0=lbv, scalar1=-1.0, scalar2=1.0, op0=ALU.mult, op1=ALU.add), r=[t_col], w=[t_col])
    S.dma(row[0:1, 0:512], k.hn_d[:, :], w=[t_row])
    for h in range(4):
        row_to_col(k, row[0:1, h * 128:(h + 1) * 128], 1, 128, col[:, 40 + h:41 + h], t_row, t_col)

    qrow, Krow, A, B, oacc = RB[:, 0, :], RB[:, 1, :], RB[:, 2, :], RB[:, 3, :], RB[:, 4, :]
    gsil, mixrow = HB[:, 2, :], HB[:, 4, :]
    qts, kts = [HB[:, 0, :], HB[:, 5, :]], [HB[:, 1, :], HB[:, 6, :]]
    vtm = HB[:, 3, :].rearrange("p (i c) -> p i c", c=128)
    for h in range(4):
        base = 1664 + 512 * h
        wg, t_wg = k.load_w(k.w0_d[:, base:base + 512].rearrange("(k p) n -> p k n", p=128))
        wvv, t_wvv = k.load_w(k.w0_d[:, 3712 + 128 * h:3712 + 128 * (h + 1)].rearrange("(k p) n -> p k n", p=128))

        def ev_q(pb, tb, t0, nt):
            S.op("act", lambda e: e.activation(out=qrow[:, t0:t0 + nt], in_=pb[:, 0:nt], func=AF.Copy), r=[tb], w=[t_rb[0]])
        k.proj_fm(wg, t_wg, 256, 128, ev_q)

        def ev_g(pb, tb, t0, nt):
            S.op("act", lambda e: e.activation(out=B[:, t0:t0 + nt], in_=pb[:, 0:nt], func=AF.Sigmoid), r=[tb], w=[t_rb[3]])
            S.op("dve", lambda e: e.tensor_tensor(out=gsil[:, t0:t0 + nt], in0=pb[:, 0:nt], in1=B[:, t0:t0 + nt], op=ALU.mult),
                 r=[tb, t_rb[3]], w=[t_hb[2]])
        k.proj_fm(wg, t_wg, 384, 128, ev_g)

        def ev_v2(pb, tb, i):
            S.op("act", lambda e: e.activation(out=vtm[:, i, :], in_=pb[:, 0:128], func=AF.Copy), r=[tb], w=[t_hb[3]])
        k.proj_tm(wvv, t_wvv, 0, 128, ev_v2)

        def make_logf(d, h=h, wg=wg, t_wg=t_wg):
            def ev_z(pb, tb, t0, nt):
                S.op("act", lambda e: e.activation(out=A[:, t0:t0 + nt], in_=pb[:, 0:nt], func=AF.Sigmoid), r=[tb], w=[t_rb[2]])
            k.proj_fm(wg, t_wg, d * 128, 128, ev_z)
            S.op("dve", lambda e: e.tensor_scalar(out=A, in0=A, scalar1=omv[:, h, d:d + 1], scalar2=lbv[:, h, d:d + 1], op0=ALU.mult, op1=ALU.add),
                 r=[t_rb[2], t_col], w=[t_rb[2]])
            S.op("pool", lambda e: e.tensor_scalar(out=Krow, in0=A, scalar1=-1.0, scalar2=1.0, op0=ALU.mult, op1=ALU.add),
                 r=[t_rb[2]], w=[t_rb[1]])
            S.op("act", lambda e: e.activation(out=A, in_=A, func=AF.Ln), r=[t_rb[2]], w=[t_rb[2]])
        gated_scan(k, 128, 128, 1, qrow, t_rb[0], Krow, t_rb[1], A, t_rb[2], B, t_rb[3], make_logf,
                   lambda i: vtm[:, i, :], [t_hb[3]], [oacc], [t_rb[4]], qts, [t_hb[0], t_hb[5]], kts, [t_hb[1], t_hb[6]])
        if h == 0:
            k.dump("oacc0", oacc, [128, N], F32, [t_rb[4]])
        rms_gate_out(k, 128, 1, [oacc], [t_rb[4]], A, t_rb[2], B, t_rb[3], [gsil], [t_hb[2]], [col[:, 40 + h:41 + h]], t_col,
                     [mixrow], [t_hb[4]], [4 + h])
    k.dump("mixd0", k.mix_d[0:8, :, :], [8, 128, N], BF16, k.t_mixd[0:8])


def phase_D(k, l):
    S = k.S
    RB, HB = k.RB, k.HB
    k.bcast_rows(l, "mix")
    if l == 0:
        wo, t_wo = k.load_w(k.wo0_d.rearrange("(c p) n -> p c n", p=128), nslots=2)
        chunks = [(c, 128, wo, t_wo, c) for c in range(8)]
        tiles = list(range(NT))
        nch = 8
    else:
        wo, t_wo = k.load_w(k.wo1_d[0:768, :].rearrange("(c p) n -> p c n", p=96), nslots=2, parts=96)
        wf, t_wf = k.load_w(k.wo1_d[768:1024, :].rearrange("(c p) n -> p c n", p=128), nslots=1)
        chunks = [(c, 96, wo, t_wo, c) for c in range(8)] + [(8 + c, 128, wf, t_wf, c) for c in range(2)]
        tiles = list(range(2, NT))
        nch = 10
    tb_ = [RB[:, r, hf * 1024:(hf + 1) * 1024] for r in range(6) for hf in range(2)]
    tt = k.t_tile
    nchunks = len(chunks)

    def bufs_for(n_):
        s6 = (n_ % 2) * 6
        return [tb_[s6 + q] for q in range(6)], [tt[s6 + q] for q in range(6)]

    def stA(n_):
        i = tiles[n_]
        (ht, tmp, xn1, hnew, xn2, ufb), (t_ht, t_tmp, t_xn1, t_hn, t_xn2, t_uf) = bufs_for(n_)
        mt = HB[:, n_ % 2, 0:nch * 128].rearrange("p (c n) -> p c n", n=128)
        t_mt = k.t_hb[n_ % 2]
        S.dma(mt, k.mix_d[0:nch, :, i * 128:(i + 1) * 128].rearrange("c p n -> p c n"), r=k.t_mixd[0:nch], w=[t_mt])
        if l == 0:
            src = k.ctx_d[i * 128:(i + 1) * 128, :] if i < 2 else k.x_d[(i - 2) * 128:(i - 1) * 128, :]
            S.dma(ht, src, w=[t_ht])
        else:
            S.dma(ht, k.hout0_d[i * 128:(i + 1) * 128, :], r=[k.t_hout0[i]], w=[t_ht])
        pp, tp = k.pair()
        for hf in range(2):
            for ci_, (c, KR, wv, t_wv, wc) in enumerate(chunks):
                S.op("pe", lambda e, hf=hf, c=c, KR=KR, wv=wv, wc=wc, ci_=ci_: e.matmul(
                    pp[:, hf * 512:(hf + 1) * 512], lhsT=mt[0:KR, c, :], rhs=wv[0:KR, wc, hf * 512:(hf + 1) * 512],
                    start=(ci_ == 0), stop=(ci_ == nchunks - 1)), r=[t_mt] + t_wv, w=[tp[hf]])
        r_ = 1 if i < 2 else 0
        for hf in range(2):
            S.op("dve", lambda e, hf=hf: e.tensor_tensor(out=tmp[:, hf * 512:(hf + 1) * 512], in0=pp[:, hf * 512:(hf + 1) * 512],
                                                         in1=k.bc[:, r_, hf * 512:(hf + 1) * 512], op=ALU.mult),
                 r=[tp[hf], k.t_bc[r_]], w=[t_tmp])
        S.op("dve", lambda e: e.scalar_tensor_tensor(out=tmp, in0=ht, scalar=ALPHA, in1=tmp, op0=ALU.mult, op1=ALU.add),
             r=[t_ht, t_tmp], w=[t_tmp])
        st_ap, mv_ap, rs_ap, nb_ap, t_st = k.st_slot()
        k.ln_stats(tmp, t_tmp, st_ap, mv_ap, rs_ap, nb_ap, t_st)
        S.op("act", lambda e: e.activation(out=xn1, in_=tmp, func=AF.Identity, scale=rs_ap, bias=nb_ap), r=[t_tmp, t_st], w=[t_xn1])
        S.op("pool", lambda e: e.tensor_tensor(out=xn1, in0=xn1, in1=k.bc[:, 2, :], op=ALU.mult), r=[t_xn1, k.t_bc[2]], w=[t_xn1])
        S.op("pool", lambda e: e.tensor_tensor(out=hnew, in0=xn1, in1=k.bc[:, 3, :], op=ALU.add), r=[t_xn1, k.t_bc[3]], w=[t_hn])
        S.dma(k.hmid_d[i * 128:(i + 1) * 128, :], hnew, r=[t_hn], w=[k.t_hmid[i]])
        k.ln_part1(hnew, t_hn, xn2, t_xn2)

    def stB(n_):
        i = tiles[n_]
        (ht, tmp, xn1, hnew, xn2, ufb), (t_ht, t_tmp, t_xn1, t_hn, t_xn2, t_uf) = bufs_for(n_)
        uf = ufb.rearrange("p (k n) -> p k n", n=128)
        k.ln_part2(l, xn2, t_xn2, i, 3, True, uf, t_uf)

    def stC(n_):
        i = tiles[n_]
        (ht, tmp, xn1, hnew, xn2, ufb), (t_ht, t_tmp, t_xn1, t_hn, t_xn2, t_uf) = bufs_for(n_)
        uf = ufb.rearrange("p (k n) -> p k n", n=128)
        k.route(i, uf, t_uf)
    nT_ = len(tiles)
    for n in range(nT_ + 2):
        if n < nT_:
            stA(n)
        if 1 <= n <= nT_:
            stB(n - 1)
        if n >= 2:
            stC(n - 2)
    k.route_all(tiles[0], len(tiles), RB[:, 0, :], [k.t_tile[0], k.t_tile[1]])
    k.dump(f"hmid{l}", k.hmid_d[:, :], [N, D], F32, k.t_hmid)
    k.dump(f"u2T{l}", k.uT[:, :, :], [128, KC, N], BF16, k.t_uT)
    k.dump(f"gates{l}", k.gates[:, :, :], [128, NT, 16], F32, k.t_gates)


def phase_E(k, l):
    S = k.S
    RB = k.RB
    k.bcast_rows(l, "moe")
    if l == 0:
        halves = [list(range(0, 9)), list(range(9, 18))]
        bsz = 384
    else:
        halves = [list(range(2, 10)), list(range(10, 18))]
        bsz = 512
    yacc = RB[:, 0:4, :].rearrange("p a n -> p (a n)").rearrange("p (t d) -> p t d", d=1024)
    t_y = k.t_tile[0:9]
    hTb = RB[:, 4, :].bitcast(BF16)
    hT = [hTb[:, q * 2048:(q + 1) * 2048].rearrange("p (f n) -> p f n", n=512) for q in range(2)]
    t_hT = [k.t_tile[9], k.t_tile[10]]
    sg = [RB[:, 5, q * 512:(q + 1) * 512] for q in range(2)]
    t_sg = [k.t_rb[4], k.t_rb[5]]
    Hb = RB[:, 5, 1024:2048]
    t_H = k.t_tile[11]
    dst_d = k.hout0_d if l == 0 else k.out_d
    k.h_i = 0
    k.s_i = 0
    for tiles in halves:
        tok0 = tiles[0] * 128
        ntok = len(tiles) * 128
        blocks = [(tok0 + b0, bsz) for b0 in range(0, ntok, bsz)]
        for e_ in range(16):
            w1, t_w1 = k.load_w(k.eg_d[l, e_].rearrange("(c p) n -> p c n", p=128))
            w3, t_w3 = k.load_w(k.eu_d[l, e_].rearrange("(c p) n -> p c n", p=128))
            w2, t_w2 = k.load_w(k.ed_d[l, e_].rearrange("(c p) n -> p c n", p=128))
            for (b0, nb) in blocks:
                hi = k.h_i
                k.h_i = 1 - hi
                hTc = hT[hi]
                for f in range(4):
                    p1, tp1 = k.bank()
                    for kk in range(KC):
                        S.op("pe", lambda e, kk=kk, f=f, p1=p1, w1=w1, b0=b0, nb=nb: e.matmul(
                            p1[:, 0:nb], lhsT=w1[:, kk, f * 128:(f + 1) * 128], rhs=k.uT[:, kk, b0:b0 + nb], start=(kk == 0), stop=(kk == KC - 1)),
                            r=t_w1 + k.t_uT[b0 // 128:(b0 + nb) // 128], w=[tp1])
                    p3, tp3 = k.bank()
                    for kk in range(KC):
                        S.op("pe", lambda e, kk=kk, f=f, p3=p3, w3=w3, b0=b0, nb=nb: e.matmul(
                            p3[:, 0:nb], lhsT=w3[:, kk, f * 128:(f + 1) * 128], rhs=k.uT[:, kk, b0:b0 + nb], start=(kk == 0), stop=(kk == KC - 1)),
                            r=t_w3 + k.t_uT[b0 // 128:(b0 + nb) // 128], w=[tp3])
                    si = k.s_i
                    k.s_i = 1 - si
                    S.op("act", lambda e, p1=p1, si=si, nb=nb: e.activation(out=sg[si][:, 0:nb], in_=p1[:, 0:nb], func=AF.Sigmoid), r=[tp1], w=[t_sg[si]])
                    S.op("dve", lambda e, p1=p1, si=si, nb=nb: e.tensor_tensor(out=sg[si][:, 0:nb], in0=p1[:, 0:nb], in1=sg[si][:, 0:nb], op=ALU.mult),
                         r=[tp1, t_sg[si]], w=[t_sg[si]])
                    S.op("dve", lambda e, p3=p3, si=si, nb=nb, f=f, hTc=hTc: e.tensor_tensor(out=hTc[:, f, 0:nb], in0=p3[:, 0:nb], in1=sg[si][:, 0:nb], op=ALU.mult),
                         r=[tp3, t_sg[si]], w=[t_hT[hi]])
                for tl in range(nb // 128):
                    gi = (b0 // 128) + tl
                    yi = gi - tiles[0]
                    for dh in range(2):
                        py, tpy = k.bank()
                        for f in range(4):
                            S.op("pe", lambda e, f=f, py=py, hTc=hTc, tl=tl, dh=dh, w2=w2: e.matmul(
                                py[:, :], lhsT=hTc[:, f, tl * 128:(tl + 1) * 128], rhs=w2[:, f, dh * 512:(dh + 1) * 512], start=(f == 0), stop=(f == 3)),
                                r=[t_hT[hi]] + t_w2, w=[tpy])
                        ya = yacc[:, yi, dh * 512:(dh + 1) * 512]
                        gs = k.gates[:, gi, e_:e_ + 1]
                        if e_ == 0:
                            S.op("dve", lambda e, py=py, ya=ya, gs=gs: e.tensor_scalar(out=ya, in0=py[:, :], scalar1=gs, scalar2=None, op0=ALU.mult),
                                 r=[tpy, k.t_gates[gi]], w=[t_y[yi]])
                        else:
                            S.op("dve", lambda e, py=py, ya=ya, gs=gs: e.scalar_tensor_tensor(out=ya, in0=py[:, :], scalar=gs, in1=ya, op0=ALU.mult, op1=ALU.add),
                                 r=[tpy, k.t_gates[gi], t_y[yi]], w=[t_y[yi]])
        for yi, gi in enumerate(tiles):
            yt = yacc[:, yi, :]
            r_ = 1 if gi < 2 else 0
            if l == 0 and yi == 0 and tiles[0] == 0:
                k.dump("ymoe_t0", yt, [128, D], F32, [t_y[yi]])
            S.dma(Hb, k.hmid_d[gi * 128:(gi + 1) * 128, :], r=[k.t_hmid[gi]], w=[t_H])
            S.op("dve", lambda e, yt=yt, r_=r_: e.tensor_tensor(out=yt, in0=yt, in1=k.bc[:, r_, :], op=ALU.mult), r=[t_y[yi], k.t_bc[r_]], w=[t_y[yi]])
            S.op("dve", lambda e, yt=yt: e.scalar_tensor_tensor(out=yt, in0=Hb, scalar=ALPHA, in1=yt, op0=ALU.mult, op1=ALU.add),
                 r=[t_H, t_y[yi]], w=[t_y[yi]])
            st_ap, mv_ap, rs_ap, nb_ap, t_st = k.st_slot()
            k.ln_stats(yt, t_y[yi], st_ap, mv_ap, rs_ap, nb_ap, t_st)
            S.op("act", lambda e, yt=yt, rs_ap=rs_ap, nb_ap=nb_ap: e.activation(out=Hb, in_=yt, func=AF.Identity, scale=rs_ap, bias=nb_ap),
                 r=[t_y[yi], t_st], w=[t_H])
            S.op("dve", lambda e: e.tensor_tensor(out=Hb, in0=Hb, in1=k.bc[:, 2, :], op=ALU.mult), r=[t_H, k.t_bc[2]], w=[t_H])
            S.op("pool", lambda e, yt=yt: e.tensor_tensor(out=yt, in0=Hb, in1=k.bc[:, 3, :], op=ALU.add), r=[t_H, k.t_bc[3]], w=[t_y[yi]])
            if l == 0:
                S.dma(k.hout0_d[gi * 128:(gi + 1) * 128, :], yt, r=[t_y[yi]], w=[k.t_hout0[gi]])
            else:
                S.dma(k.out_d[(gi - 2) * 128:(gi - 1) * 128, :], yt, r=[t_y[yi]], w=[k.t_out[gi - 2]])
    if l == 0:
        k.dump("hout0", k.hout0_d[:, :], [N, D], F32, k.t_hout0)


def mixer_odd(k):
    S = k.S
    scan_setup(k)
    sc = k.scn
    RB, HB, t_rb, t_hb = k.RB, k.HB, k.t_rb, k.t_hb
    col, t_col, row, t_row = sc["col"], k.t_col, sc["row"], k.t_row
    LATB = [(256, 512), (768, 512), (1280, 512), (1792, 512)]
    BC = sc["Am"][:, 0, 0, :]
    BS = sc["Am"][:, 0, 1, :]
    ci = col[:, 0:32].bitcast(I32)
    S.op("pool", lambda e: e.iota(ci[:, 0:1], pattern=[[0, 1]], base=0, channel_multiplier=1), w=[t_col])
    S.op("dve", lambda e: e.tensor_single_scalar(out=ci[:, 1:2], in_=ci[:, 0:1], scalar=63, op=ALU.bitwise_and), r=[t_col], w=[t_col])
    S.op("dve", lambda e: e.tensor_copy(out=col[:, 32:33], in_=ci[:, 1:2]), r=[t_col], w=[t_col])
    S.op("pool", lambda e: e.iota(ci[:, 2:18], pattern=[[128, 16]], base=0, channel_multiplier=1), r=[t_col], w=[t_col])
    S.op("dve", lambda e: e.tensor_copy(out=col[:, 40:56], in_=ci[:, 2:18]), r=[t_col], w=[t_col])
    qi = RB[:, 0, 0:128].bitcast(I32)
    qf = RB[:, 0, 128:256]
    ki = RB[:, 0, 256:384].bitcast(I32)
    kci = RB[:, 0, 384:512].bitcast(I32)
    tq = t_rb[0]
    S.op("pool", lambda e: e.iota(qi, pattern=[[1, 128]], base=0, channel_multiplier=0), w=[tq])
    S.op("dve", lambda e: e.tensor_single_scalar(out=qi, in_=qi, scalar=63, op=ALU.bitwise_and), r=[tq], w=[tq])
    S.op("dve", lambda e: e.tensor_copy(out=qf, in_=qi), r=[tq], w=[tq])
    S.op("dve", lambda e: e.tensor_scalar(out=ki, in0=qf, scalar1=col[:, 32:33], scalar2=None, op0=ALU.mult), r=[tq, t_col], w=[tq])
    S.op("dve", lambda e: e.tensor_single_scalar(out=ki, in_=ki, scalar=63, op=ALU.bitwise_and), r=[tq], w=[tq])
    S.op("dve", lambda e: e.tensor_scalar(out=kci, in0=ki, scalar1=16, scalar2=None, op0=ALU.add), r=[tq], w=[tq])
    S.op("dve", lambda e: e.tensor_single_scalar(out=kci, in_=kci, scalar=63, op=ALU.bitwise_and), r=[tq], w=[tq])
    S.op("act", lambda e: e.activation(out=BS, in_=ki, func=AF.Sin, scale=-2 * PI / 64, bias=k.cst[:, 4:5]), r=[tq, k.t_c], w=[k.t_Am[1]])
    S.op("act", lambda e: e.activation(out=BC, in_=kci, func=AF.Sin, scale=-2 * PI / 64, bias=k.cst[:, 4:5]), r=[tq, k.t_c], w=[k.t_Am[0]])
    for M_, tM in ((BC, k.t_Am[0]), (BS, k.t_Am[1])):
        S.op("pool", lambda e, M_=M_: e.memset(M_[0:64, 64:128], 0.0), r=[tM], w=[tM])
        S.op("pool", lambda e, M_=M_: e.memset(M_[64:128, 0:64], 0.0), r=[tM], w=[tM])
    if k.sub == "M1a":
        k.dump("BCS", sc["Am"][:, 0, :, :], [128, 2, 128], BF16, k.t_Am)
        return
    wz, t_wz = k.load_w(k.w1_d[:, 1536:1792].rearrange("(k p) n -> p k n", p=128))
    zT = HB[:, 2:4, :]
    zc = HB[:, 4:6, :].rearrange("p a n -> p (a n)")[:, 0:4096].rearrange("p (i c) -> p i c", c=256)
    zs = HB[:, 6:8, :].rearrange("p a n -> p (a n)")[:, 0:4096].rearrange("p (i c) -> p i c", c=256)
    for m in range(2):
        for (t0, nt) in LATB:
            pb, tb = proj_block(k, wz, t_wz, m * 128, 128, t0, nt)
            S.op("act", lambda e, pb=pb, m=m, t0=t0, nt=nt: e.activation(out=zT[:, m, t0:t0 + nt], in_=pb[:, 0:nt], func=AF.Copy), r=[tb], w=[t_hb[2 + m]])
    if k.sub == "M1z":
        k.dump("zT", HB[:, 2:4, :], [128, 2, N], BF16, [t_hb[2], t_hb[3]])
        return
    for a in range(16):
        tok = (a + 2) * 128
        pb, tb = k.bank()
        for m in range(2):
            S.op("pe", lambda e, pb=pb, m=m, tok=tok: e.matmul(pb[:, m * 128:(m + 1) * 128], lhsT=zT[:, m, tok:tok + 128], rhs=BC[:, :], start=True, stop=True),
                 r=[t_hb[2 + m], k.t_Am[0]], w=[tb])
            S.op("pe", lambda e, pb=pb, m=m, tok=tok: e.matmul(pb[:, 256 + m * 128:256 + (m + 1) * 128], lhsT=zT[:, m, tok:tok + 128], rhs=BS[:, :], start=True, stop=True),
                 r=[t_hb[2 + m], k.t_Am[1]], w=[tb])
        S.op("act", lambda e, pb=pb, a=a: e.activation(out=zc[:, a, :], in_=pb[:, 0:256], func=AF.Copy), r=[tb], w=[t_hb[4], t_hb[5]])
        if k.sub != "M1d":
            S.op("act", lambda e, pb=pb, a=a: e.activation(out=zs[:, a, :], in_=pb[:, 256:512], func=AF.Copy, scale=-1.0), r=[tb], w=[t_hb[6], t_hb[7]])
        if k.sub in ("M1c", "M1d") and a == 0:
            k.dump("zc", HB[:, 4:6, :], [128, 2, N], BF16, [t_hb[4], t_hb[5]])
            return
    S.barrier()
    if k.sub == "M1b":
        k.dump("zc", HB[:, 4:6, :], [128, 2, N], BF16, [t_hb[4], t_hb[5]])
        return
    fidx = RB[:, 0, 0:2048]
    fi_i = RB[:, 1, 0:2048].bitcast(I32)
    S.op("pool", lambda e: e.iota(fi_i, pattern=[[1, 2048]], base=0, channel_multiplier=0), w=[t_rb[1]])
    S.op("dve", lambda e: e.tensor_copy(out=fidx, in_=fi_i), r=[t_rb[1]], w=[t_rb[0]])
    tabs = []
    for q in range(2):
        rowb = RB[:, 2 + q, :].bitcast(BF16)
        tabs.append((rowb[:, 0:2048], rowb[:, 2048:4096], t_rb[2 + q]))
    kib = [RB[:, 4, 0:2048].bitcast(I32), RB[:, 5, 0:2048].bitcast(I32)]
    banks = [(k.PS[i // 2][:, (i % 2) * 512:(i % 2 + 1) * 512], k.PT[i // 2][i % 2]) for i in range(8)]
    for a in range(16):
        Cb, Sb, t_tab = tabs[a % 2]
        S.op("dve", lambda e, a=a: e.tensor_scalar(out=kib[0], in0=fidx, scalar1=col[:, 40 + a:41 + a], scalar2=None, op0=ALU.mult),
             r=[t_rb[0], t_col], w=[t_rb[4]])
        S.op("dve", lambda e: e.tensor_single_scalar(out=kib[0], in_=kib[0], scalar=2047, op=ALU.bitwise_and), r=[t_rb[4]], w=[t_rb[4]])
        S.op("dve", lambda e: e.tensor_scalar(out=kib[1], in0=kib[0], scalar1=512, scalar2=None, op0=ALU.add), r=[t_rb[4]], w=[t_rb[5]])
        S.op("dve", lambda e: e.tensor_single_scalar(out=kib[1], in_=kib[1], scalar=2047, op=ALU.bitwise_and), r=[t_rb[5]], w=[t_rb[5]])
        S.op("act", lambda e, Sb=Sb: e.activation(out=Sb, in_=kib[0], func=AF.Sin, scale=-2 * PI / 2048, bias=k.cst[:, 4:5]), r=[t_rb[4], k.t_c], w=[t_tab])
        S.op("act", lambda e, Cb=Cb: e.activation(out=Cb, in_=kib[1], func=AF.Sin, scale=-2 * PI / 2048, bias=k.cst[:, 4:5]), r=[t_rb[5], k.t_c], w=[t_tab])
        for m in range(2):
            for fb in range(4):
                pbk, tbk = banks[m * 4 + fb]
                S.op("pe", lambda e, pbk=pbk, a=a, m=m, fb=fb, Cb=Cb: e.matmul(pbk[:, :], lhsT=zc[:, a, m * 128:(m + 1) * 128], rhs=Cb[:, fb * 512:(fb + 1) * 512],
                                                                            start=(a == 0), stop=False), r=[t_hb[4], t_hb[5], t_tab], w=[tbk])
                S.op("pe", lambda e, pbk=pbk, a=a, m=m, fb=fb, Sb=Sb: e.matmul(pbk[:, :], lhsT=zs[:, a, m * 128:(m + 1) * 128], rhs=Sb[:, fb * 512:(fb + 1) * 512],
                                                                            start=False, stop=(a == 15)), r=[t_hb[6], t_hb[7], t_tab], w=[tbk])
    fsc = 1.0 / math.sqrt(2048.0 * 64.0)
    for m in range(2):
        for fb in range(4):
            pbk, tbk = banks[m * 4 + fb]
            S.op("act", lambda e, pbk=pbk, m=m, fb=fb: e.activation(out=HB[:, m, fb * 512:(fb + 1) * 512], in_=pbk[:, :], func=AF.Copy, scale=fsc), r=[tbk], w=[t_hb[m]])
        S.dma(k.mix_d[8 + m, :, 256:2304], HB[:, m, 0:2048], r=[t_hb[m]], w=[k.t_mixd[8 + m]])
    k.dump("mixd_f", k.mix_d[8:10, :, :], [2, 128, N], BF16, k.t_mixd[8:10])
    S.barrier()
    if k.sub == "M1f":
        return

    S.op("pool", lambda e: e.memset(k.rmask[:], 1.0), w=[k.t_c])
    S.op("pool", lambda e: e.memset(k.rmask[:].rearrange("p (c l) -> p c l", l=128)[:, :, 0:1], 0.0), r=[k.t_c], w=[k.t_c])
    S.op("pool", lambda e: e.memset(k.Mf[:], 1.0), w=[k.t_c])
    S.op("pool", lambda e: e.affine_select(out=k.Mf[:], in_=k.Mf[:], pattern=[[1, 128]], compare_op=ALU.is_ge, fill=0.0,
                                           base=0, channel_multiplier=-1), r=[k.t_c], w=[k.t_c])
    S.op("pool", lambda e: e.memset(k.Mb[:], 1.0), w=[k.t_c])
    S.op("pool", lambda e: e.affine_select(out=k.Mb[:], in_=k.Mb[:], pattern=[[-1, 128]], compare_op=ALU.is_ge, fill=0.0,
                                           base=0, channel_multiplier=1), r=[k.t_c], w=[k.t_c])
    gw = k.bc[0:16, 0, 0:768].rearrange("p (r n) -> p r n", n=384)
    t_gw = k.t_bc[0]
    S.dma(gw[:, :, :], k.gw_d.rearrange("r k n -> k r n"), w=[t_gw])
    S.dma(row[0:2, 0:384], k.gb_d[:, :], w=[t_row])
    for h in range(4):
        row_to_col(k, row[0:2, h * 96:(h + 1) * 96], 2, 96, col[0:96, 2 * h:2 * h + 2], t_row, t_col)
    S.op("dve", lambda e: e.tensor_scalar(out=col[0:96, 0:8], in0=col[0:96, 0:8], scalar1=-1.0, scalar2=None, op0=ALU.mult), r=[t_col], w=[t_col])
    S.dma(row[0:1, 0:768], k.gn_d[:, :], r=[t_col], w=[t_row])
    for c in range(8):
        row_to_col(k, row[0:1, c * 96:(c + 1) * 96], 1, 96, col[0:96, 8 + c:9 + c], t_row, t_col)
    qrow, krow, A, B = RB[0:96, 0, :], RB[0:96, 1, :], RB[0:96, 2, :], RB[0:96, 3, :]
    oacc = [RB[0:96, 4, :], RB[0:96, 5, :]]
    qt, kt = HB[0:96, 0, :], HB[0:96, 1, :]
    qts, kts = [HB[0:96, 0, :], HB[0:96, 6, :]], [HB[0:96, 1, :], HB[0:96, 7, :]]
    gsil = [HB[0:96, 2, :], HB[0:96, 3, :]]
    vt = HB[:, 4:6, :].rearrange("p a n -> p (a n)")[:, 0:3456].rearrange("p (i c) -> p i c", c=192)
    for h in range(4):
        base = 384 * h
        wg, t_wg = k.load_w(k.w1_d[:, base:base + 384].rearrange("(k p) n -> p k n", p=128))
        wv, t_wvv = k.load_w(k.w1_d[:, 1824 + 192 * h:1824 + 192 * (h + 1)].rearrange("(k p) n -> p k n", p=128))
        wR, t_wR = k.load_w(k.w1_d[:, 1792:1824].rearrange("(k p) n -> p k n", p=128))

        def ev_q(pb, tb, t0, nt):
            S.op("act", lambda e: e.activation(out=qrow[:, t0:t0 + nt], in_=pb[0:96, 0:nt], func=AF.Copy, scale=96.0 ** -0.5), r=[tb], w=[t_rb[0]])
        k.proj_fm(wg, t_wg, 0, 96, ev_q)

        def ev_k(pb, tb, t0, nt):
            S.op("act", lambda e: e.activation(out=krow[:, t0:t0 + nt], in_=pb[0:96, 0:nt], func=AF.Copy), r=[tb], w=[t_rb[1]])
        k.proj_fm(wg, t_wg, 96, 96, ev_k)
        for a in range(2):
            def ev_g(pb, tb, t0, nt, a=a):
                S.op("act", lambda e: e.activation(out=B[:, t0:t0 + nt], in_=pb[0:96, 0:nt], func=AF.Sigmoid), r=[tb], w=[t_rb[3]])
                S.op("dve", lambda e: e.tensor_tensor(out=gsil[a][:, t0:t0 + nt], in0=pb[0:96, 0:nt], in1=B[:, t0:t0 + nt], op=ALU.mult),
                     r=[tb, t_rb[3]], w=[t_hb[2 + a]])
            k.proj_fm(wg, t_wg, 192 + 96 * a, 96, ev_g)

        def ev_v(pb, tb, i):
            S.op("act", lambda e: e.activation(out=vt[:, i, :], in_=pb[:, 0:192], func=AF.Copy), r=[tb], w=[t_hb[4], t_hb[5]])
        k.proj_tm(wv, t_wvv, 0, 192, ev_v)

        def make_logf(d, h=h, wR=wR, t_wR=t_wR):
            for (t0, nt) in TOKB:
                pr, tr = proj_block(k, wR, t_wR, d * 16, 16, t0, nt)
                S.op("act", lambda e, pr=pr, t0=t0, nt=nt: e.activation(out=B[0:16, t0:t0 + nt], in_=pr[0:16, 0:nt], func=AF.Copy), r=[tr], w=[t_rb[3]])
                pz, tz = k.bank()
                S.op("pe", lambda e, pz=pz, t0=t0, nt=nt: e.matmul(pz[0:96, 0:nt], lhsT=gw[0:16, d, h * 96:(h + 1) * 96], rhs=B[0:16, t0:t0 + nt],
                                                                   start=True, stop=True), r=[t_gw, t_rb[3]], w=[tz])
                S.op("act", lambda e, pz=pz, t0=t0, nt=nt: e.activation(out=A[:, t0:t0 + nt], in_=pz[0:96, 0:nt], func=AF.Exp, scale=-1.0,
                                                                        bias=col[0:96, 2 * h + d:2 * h + d + 1]), r=[tz, t_col], w=[t_rb[2]])
            S.op("act", lambda e: e.activation(out=A, in_=A, func=AF.Ln, bias=k.cst[0:96, 1:2]), r=[t_rb[2], k.t_c], w=[t_rb[2]])
            S.op("dve", lambda e: e.tensor_scalar(out=A, in0=A, scalar1=-1.0 / 16.0, scalar2=None, op0=ALU.mult), r=[t_rb[2]], w=[t_rb[2]])
        gated_scan(k, 96, 96, 2, qrow, t_rb[0], krow, t_rb[1], A, t_rb[2], B, t_rb[3], make_logf,
                   lambda i: vt[:, i, :], [t_hb[4], t_hb[5]], oacc, [t_rb[4], t_rb[5]], qts, [t_hb[0], t_hb[6]], kts, [t_hb[1], t_hb[7]], L=128)
        if h == 0:
            k.dump("gla_o0", RB[0:96, 4:6, :], [96, 2, N], F32, [t_rb[4], t_rb[5]])
        rms_gate_out(k, 96, 2, oacc, [t_rb[4], t_rb[5]], A, t_rb[2], B, t_rb[3], gsil, [t_hb[2], t_hb[3]],
                     [col[0:96, 8 + 2 * h:9 + 2 * h], col[0:96, 9 + 2 * h:10 + 2 * h]], t_col, [qt, kt], [t_hb[0], t_hb[1]], [2 * h, 2 * h + 1])
    k.dump("mixd1", k.mix_d[0:10, :, :], [10, 128, N], BF16, k.t_mixd[0:10])


_NC_CACHE = {}


def _f32(a):
    return np.ascontiguousarray(np.asarray(a, dtype=np.float32))


def kernel(x, c, ctx, c_ctx, w_ada, b_ada, ln_g, ln_b, w_in_even, attn_sink, hgrn_lb_logits, hgrn_norm,
           w_out_even, w_in_odd, gla_gate_w, gla_gate_b, gla_norm, w_out_odd, w_router, b_router,
           w_expert_gate, w_expert_up, w_expert_down):
    x = _f32(x); c = _f32(c); ctx = _f32(ctx); c_ctx = _f32(c_ctx)
    w0a = np.ascontiguousarray(_f32(w_in_even)[0][:, _cols0()])
    w1a = np.ascontiguousarray(_f32(w_in_odd)[0][:, _cols1()])
    shared = {
        "w_ada": _f32(w_ada), "b_ada": _f32(b_ada), "ln_g": _f32(ln_g), "ln_b": _f32(ln_b),
        "w0a": w0a, "attn_sink": _f32(attn_sink), "lb_logits": _f32(hgrn_lb_logits), "hgrn_norm": _f32(hgrn_norm),
        "w_out_even": _f32(w_out_even)[0], "w1a": w1a, "gla_gate_w": _f32(gla_gate_w)[0], "gla_gate_b": _f32(gla_gate_b)[0],
        "gla_norm": _f32(gla_norm), "w_out_odd": _f32(w_out_odd)[0], "w_router": _f32(w_router),
        "b_router": _f32(b_router)[None, :], "w_expert_gate": _f32(w_expert_gate), "w_expert_up": _f32(w_expert_up),
        "w_expert_down": _f32(w_expert_down),
    }
    nb = x.shape[0]
    in_maps = []
    for b in range(nb):
        m = dict(shared)
        m["x"] = np.ascontiguousarray(x[b])
        m["ctx"] = np.ascontiguousarray(ctx[b])
        m["cvec"] = np.ascontiguousarray(np.stack([c[b], c_ctx], 0))
        in_maps.append(m)
    if "nc" not in _NC_CACHE:
        _NC_CACHE["nc"] = build()
    res = run_bass_kernel_spmd(_NC_CACHE["nc"], in_maps, core_ids=list(range(nb)))
    return np.stack([np.asarray(r["out"], dtype=np.float32) for r in res.results], 0)
```

```python
import contextlib
import math
import numpy as np
import concourse.bass as bass
import concourse.mybir as mybir
from concourse.bass_utils import run_bass_kernel_spmd

F32 = mybir.dt.float32
BF16 = mybir.dt.bfloat16
I32 = mybir.dt.int32
AF = mybir.ActivationFunctionType
ALU = mybir.AluOpType
AX = mybir.AxisListType

ENG = ("pe", "act", "dve", "pool", "sp")


class Trk:
    __slots__ = ("w", "rs", "dsem", "dcnt", "name")

    def __init__(self, name=""):
        self.w = None
        self.rs = []
        self.dsem = None
        self.dcnt = 0
        self.name = name


class Sched:
    SEM_CHUNK = 20000

    def __init__(self, nc):
        self.nc = nc
        self.ops = {e: [] for e in ENG}
        self.waited = {e: {} for e in ENG}
        self.stack = contextlib.ExitStack()
        self.nsem = 0
        self.dma_ev = {}

    def sbuf(self, name, shape, dt):
        return self.stack.enter_context(self.nc.sbuf_tensor(name, list(shape), dt))

    def psum(self, name, shape, dt=F32):
        return self.stack.enter_context(self.nc.psum_tensor(name, list(shape), dt))

    def new_sem(self, name):
        self.nsem += 1
        return self.stack.enter_context(self.nc.semaphore(f"{name}_{self.nsem}"))

    def _filter(self, engine, deps):
        waits = []
        wd = self.waited[engine]
        for ev in deps:
            if ev[0] == "e":
                _, f, idx = ev
                if engine == "pe" and f == "pe":
                    continue
                if idx <= wd.get(f, -1):
                    continue
                wd[f] = idx
                self.ops[f][idx][2] = True
                waits.append(ev)
            else:
                _, sem, val = ev
                k = id(sem)
                if val <= wd.get(k, 0):
                    continue
                wd[k] = val
                waits.append(ev)
        return waits

    def _deps(self, engine, r, w):
        deps = []
        for t in r:
            if t.w is not None:
                deps.append(t.w)
        for t in w:
            if t.w is not None:
                deps.append(t.w)
            deps.extend(x for x in t.rs if not (x[0] == "e" and x[1] == engine))
        return self._filter(engine, deps)

    def _post(self, ev, r, w):
        for t in w:
            t.w = ev
            t.rs = []
        for t in r:
            if t in w:
                continue
            if ev[0] == "e":
                t.rs = [x for x in t.rs if not (x[0] == "e" and x[1] == ev[1])]
            else:
                t.rs = [x for x in t.rs if not (x[0] == "d" and x[1] is ev[1])]
            t.rs.append(ev)

    def op(self, engine, fn, r=(), w=()):
        r = list(r)
        w = list(w)
        waits = self._deps(engine, r, w)
        idx = len(self.ops[engine])
        self.ops[engine].append([fn, waits, False, None])
        self._post(("e", engine, idx), r, w)

    def dma(self, out, in_, r=(), w=(), q="sp", **kw):
        r = list(r)
        w = list(w)
        waits = self._deps(q, r, w)
        t0 = w[0]
        if t0.dsem is None or t0.dcnt > 60000:
            t0.dsem = self.new_sem("d")
            t0.dcnt = 0
        t0.dcnt += 16
        ev = ("d", t0.dsem, t0.dcnt)
        self.dma_ev[id(t0.dsem)] = ev

        def fn(eng, out=out, in_=in_, kw=kw):
            return eng.dma_start(out=out, in_=in_, **kw)
        self.ops[q].append([fn, waits, False, t0.dsem])
        self._post(ev, r, w)

    def barrier(self):
        last = {}
        for f in ("pe", "act", "dve", "pool"):
            j = len(self.ops[f]) - 1
            while j >= 0 and (self.ops[f][j][0] is None or self.ops[f][j][3] is not None):
                j -= 1
            last[f] = j
        dm = list(self.dma_ev.values())
        for e in ENG:
            deps = [("e", f, last[f]) for f in ("pe", "act", "dve", "pool") if f != e and last[f] >= 0]
            deps += dm
            waits = self._filter(e, deps)
            self.ops[e].append([None, waits, False, None])

    def wait_all(self, engine, trks):
        waits = self._deps(engine, list(trks), [])
        self.ops[engine].append([None, waits, False, None])

    def emit(self):
        nc = self.nc
        cum = {}
        sems = {}
        for e in ENG:
            c = 0
            arr = []
            for rec in self.ops[e]:
                if rec[2]:
                    c += 1
                arr.append(c)
            cum[e] = arr
            sems[e] = [self.new_sem(f"s{e}") for _ in range(c // self.SEM_CHUNK + 1)]
        CH = self.SEM_CHUNK

        def semval(f, idx):
            c = cum[f][idx]
            ch = (c - 1) // CH
            return sems[f][ch], c - ch * CH

        def run(e, eng):
            for i, (fn, waits, sig, dsem) in enumerate(self.ops[e]):
                for ev in waits:
                    if ev[0] == "e":
                        s, v = semval(ev[1], ev[2])
                        eng.wait_ge(s, v)
                    else:
                        eng.wait_ge(ev[1], ev[2])
                if fn is None:
                    continue
                ins = fn(eng)
                if dsem is not None:
                    ins.then_inc(dsem, 16)
                elif sig:
                    s, v = semval(e, i)
                    ins.then_inc(s, 1)

        with nc.Block() as block:
            @block.tensor
            def _(eng):
                run("pe", eng)

            @block.scalar
            def _(eng):
                run("act", eng)

            @block.vector
            def _(eng):
                run("dve", eng)

            @block.gpsimd
            def _(eng):
                run("pool", eng)

            @block.sync
            def _(eng):
                run("sp", eng)

    def close(self):
        self.stack.close()


N = 2304
NT = 18
D = 1024
KC = 8
NLAT = 2048
ALPHA = 4.0 ** 0.25
EPS = 1e-5
TOKB = [(0, 512), (512, 512), (1024, 512), (1536, 512), (2048, 256)]
PI = math.pi

_SW = list(range(16, 32)) + list(range(0, 16)) + list(range(48, 64)) + list(range(32, 48))


def _cols0():
    cols = []
    for j in range(2):
        for blk in range(2):
            for hh in (4 * j + 2 * blk, 4 * j + 2 * blk + 1):
                cols += [hh * 64 + d for d in range(64)]
        for blk in range(2):
            for hh in (4 * j + 2 * blk, 4 * j + 2 * blk + 1):
                cols += [hh * 64 + d for d in _SW]
        cols += [512 + j * 64 + d for d in range(64)] * 2
        cols += [512 + j * 64 + d for d in _SW] * 2
    cols += list(range(640, 768))
    for h in range(4):
        cols += [768 + h * 128 + d for d in range(128)]
        cols += [1280 + h * 128 + d for d in range(128)]
        cols += [1792 + h * 128 + d for d in range(128)]
        cols += [2816 + h * 128 + d for d in range(128)]
    cols += list(range(2304, 2816))
    return cols


def _cols1():
    cols = []
    for h in range(4):
        cols += [h * 96 + d for d in range(96)]
        cols += [384 + h * 96 + d for d in range(96)]
        cols += [1568 + h * 192 + d for d in range(192)]
    cols += list(range(2336, 2592))
    cols += list(range(1536, 1568))
    cols += list(range(768, 1536))
    return cols


class K:
    pass


def build(dbg=(), stop=None):
    nc = bass.Bass("TRN2", target_bir_lowering=False)
    S = Sched(nc)
    k = K()
    k.nc = nc
    k.S = S
    k.dbg = set(dbg)
    k.sub = stop
    k.dbg_out = []

    def din(name, shape, dt=F32):
        return nc.dram_tensor(name, list(shape), dt, kind="ExternalInput").ap()

    x_d = din("x", [NLAT, D])
    ctx_d = din("ctx", [256, D])
    cv_d = din("cvec", [2, D])
    wada_d = din("w_ada", [2, D, 6 * D])
    bada_d = din("b_ada", [2, 6 * D])
    lng_d = din("ln_g", [2, 2, D])
    lnb_d = din("ln_b", [2, 2, D])
    w0_d = din("w0a", [D, 4224])
    sink_d = din("attn_sink", [1, 8])
    lbl_d = din("lb_logits", [2, 2, 512])
    hn_d = din("hgrn_norm", [1, 512])
    wo0_d = din("w_out_even", [D, D])
    w1_d = din("w1a", [D, 2592])
    gw_d = din("gla_gate_w", [2, 16, 384])
    gb_d = din("gla_gate_b", [2, 384])
    gn_d = din("gla_norm", [1, 768])
    wo1_d = din("w_out_odd", [D, D])
    wr_d = din("w_router", [D, 16])
    br_d = din("b_router", [1, 16])
    eg_d = din("w_expert_gate", [2, 16, D, 512])
    eu_d = din("w_expert_up", [2, 16, D, 512])
    ed_d = din("w_expert_down", [2, 16, 512, D])
    out_d = nc.dram_tensor("out", [NLAT, D], F32, kind="ExternalOutput").ap()
    hmid_d = nc.dram_tensor("hmid", [N, D], F32).ap()
    hout0_d = nc.dram_tensor("hout0", [N, D], F32).ap()
    mix_d = nc.dram_tensor("mixd", [10, 128, N], BF16).ap()
    t_hmid = [Trk() for _ in range(NT)]
    t_hout0 = [Trk() for _ in range(NT)]
    t_mixd = [Trk() for _ in range(10)]
    t_out = [Trk() for _ in range(16)]

    def dump(name, src_ap, shape, dt, r):
        if name not in k.dbg:
            return
        d = nc.dram_tensor("dbg_" + name, list(shape), dt, kind="ExternalOutput").ap()
        t = Trk()
        S.dma(d, src_ap, r=r, w=[t])
        k.dbg_out.append(t)

    PS = [S.psum(f"ps{i}", [128, 1024]) for i in range(4)]
    PT = [[Trk(), Trk()] for _ in range(4)]
    k.bank_i = 0
    k.pair_i = 0

    k.reserved = set()

    def bank():
        i = k.bank_i
        while i in k.reserved:
            i = (i + 1) % 8
        k.bank_i = (i + 1) % 8
        k.last_bank = i
        return PS[i // 2][:, (i % 2) * 512:(i % 2 + 1) * 512], PT[i // 2][i % 2]

    def pair():
        i = k.pair_i
        k.pair_i = (i + 1) % 4
        k.bank_i = (2 * i + 2) % 8
        return PS[i], PT[i]

    identF = S.sbuf("identF", [128, 128], F32); t_c = Trk()
    identB = S.sbuf("identB", [128, 128], BF16)
    onesF = S.sbuf("onesF", [128, 128], F32)
    onesB = S.sbuf("onesB", [128, 128], BF16)
    cst = S.sbuf("cst", [128, 8], F32)
    Mf = S.sbuf("Mf", [128, 128], BF16)
    Mb = S.sbuf("Mb", [128, 128], BF16)
    MP = S.sbuf("MP", [128, 512], BF16)
    MN = S.sbuf("MN", [128, 512], BF16)
    rmask = S.sbuf("rmask", [128, N], BF16)
    modT = S.sbuf("modT", [128, 2, 48, 2], F32); t_modT = Trk()
    mod_d = nc.dram_tensor("mod_d", [2, 2, 6 * D], F32).ap(); t_mod = Trk()
    bc = S.sbuf("bc", [128, 4, D], F32); t_bc = [Trk() for _ in range(4)]
    uT = S.sbuf("uT", [128, KC, N], BF16); t_uT = [Trk() for _ in range(NT)]
    wbuf = S.sbuf("wbuf", [128, 5, 4096], BF16); t_wb = [Trk() for _ in range(5)]
    RB = S.sbuf("RB", [128, 6, N], F32); t_rb = [Trk() for _ in range(6)]
    HB = S.sbuf("HB", [128, 8, N], BF16); t_hb = [Trk() for _ in range(8)]
    sm = S.sbuf("sm", [128, 640], F32)
    gates = S.sbuf("gates", [128, NT, 16], F32); t_gates = [Trk() for _ in range(NT)]
    wrt = S.sbuf("wrt", [128, KC, 16], F32); t_wr = Trk()
    brb = S.sbuf("brb", [128, 16], F32)

    S.op("pool", lambda e: e.memset(identF[:], 0.0), w=[t_c])
    S.op("pool", lambda e: e.affine_select(out=identF[:], in_=identF[:], pattern=[[-1, 128]], compare_op=ALU.not_equal,
                                           fill=1.0, base=0, channel_multiplier=1), r=[t_c], w=[t_c])
    S.op("pool", lambda e: e.tensor_copy(out=identB[:], in_=identF[:]), r=[t_c], w=[t_c])
    S.op("pool", lambda e: e.memset(onesF[:], 1.0), w=[t_c])
    S.op("pool", lambda e: e.memset(onesB[:], 1.0), w=[t_c])
    for j_, v_ in enumerate((EPS, 1.0, -PI, 0.0, PI)):
        S.op("pool", lambda e, j_=j_, v_=v_: e.memset(cst[:, j_:j_ + 1], v_), w=[t_c])
    S.op("pool", lambda e: e.memset(Mf[:], 1.0), w=[t_c])
    S.op("pool", lambda e: e.affine_select(out=Mf[:], in_=Mf[:], pattern=[[1, 128]], compare_op=ALU.is_ge, fill=0.0,
                                           base=0, channel_multiplier=-1), r=[t_c], w=[t_c])
    S.op("pool", lambda e: e.memset(Mf[0:64, 64:128], 0.0), r=[t_c], w=[t_c])
    S.op("pool", lambda e: e.memset(Mb[:], 1.0), w=[t_c])
    S.op("pool", lambda e: e.affine_select(out=Mb[:], in_=Mb[:], pattern=[[-1, 128]], compare_op=ALU.is_ge, fill=0.0,
                                           base=0, channel_multiplier=1), r=[t_c], w=[t_c])
    S.op("pool", lambda e: e.memset(Mb[64:128, 0:64], 0.0), r=[t_c], w=[t_c])
    S.op("pool", lambda e: e.memset(MP[:], 1.0), w=[t_c])
    S.op("pool", lambda e: e.affine_select(out=MP[:], in_=MP[:], pattern=[[0, 4], [-1, 128]], compare_op=ALU.is_ge,
                                           fill=0.0, base=0, channel_multiplier=1), r=[t_c], w=[t_c])
    S.op("pool", lambda e: e.memset(MN[:], 1.0), w=[t_c])
    S.op("pool", lambda e: e.affine_select(out=MN[:], in_=MN[:], pattern=[[0, 4], [1, 128]], compare_op=ALU.is_ge,
                                           fill=0.0, base=0, channel_multiplier=-1), r=[t_c], w=[t_c])
    S.op("pool", lambda e: e.memset(rmask[:], 1.0), w=[t_c])
    S.op("pool", lambda e: e.memset(rmask[:].rearrange("p (c l) -> p c l", l=64)[:, :, 0:1], 0.0), r=[t_c], w=[t_c])
    S.dma(wrt[:], wr_d.rearrange("(k p) n -> p k n", p=128), w=[t_wr])
    S.dma(brb[:], br_d[0:1, :].to_broadcast([128, 16]), w=[t_c])
    S.barrier()

    cs = RB[0:2, 4, 0:1024]
    sg_ = RB[0:2, 4, 1024:2048]
    csT = sm[:, 520:536].rearrange("p (k r) -> p k r", r=2)
    t_cs = Trk(); t_csT = Trk()
    k.t_bb = [[Trk(), Trk()], [Trk(), Trk()]]
    S.dma(cs, cv_d[:, :], w=[t_cs])
    S.op("act", lambda e: e.activation(out=sg_, in_=cs, func=AF.Sigmoid), r=[t_cs], w=[t_csT])
    S.op("dve", lambda e: e.tensor_tensor(out=cs, in0=cs, in1=sg_, op=ALU.mult), r=[t_cs, t_csT], w=[t_cs])
    pb, tb = bank()
    for kk in range(KC):
        S.op("pe", lambda e, kk=kk, pb=pb: e.transpose(out=pb[:, 2 * kk:2 * kk + 2], in_=cs[:, kk * 128:(kk + 1) * 128],
                                                       identity=identF[0:2, 0:2]), r=[t_cs, t_c], w=[tb])
    S.op("dve", lambda e, pb=pb: e.tensor_copy(out=csT, in_=pb[:, 0:16].rearrange("p (k r) -> p k r", r=2)), r=[tb], w=[t_csT])
    t_wa = [Trk(), Trk()]
    t_mb = [Trk(), Trk()]
    for l in range(2):
        pT, tT = bank()
        k.reserved = {k.last_bank}
        for j in range(12):
            slot = (l * 12 + j) % 2
            wa = RB[:, slot * 2:slot * 2 + 2, :].rearrange("p a n -> p (a n)")[:, 0:4096].rearrange("p (k n) -> p k n", n=512)
            mb = RB[0:2, 5, slot * 512:(slot + 1) * 512]
            S.dma(wa, wada_d[l, :, j * 512:(j + 1) * 512].rearrange("(k p) n -> p k n", p=128), w=[t_wa[slot]])
            for r_ in range(2):
                S.dma(RB[r_:r_ + 1, 5, 1024 + slot * 512:1024 + (slot + 1) * 512], bada_d[l:l + 1, j * 512:(j + 1) * 512],
                      w=[k.t_bb[slot][r_]])
            pb, tb = bank()
            for kk in range(KC):
                S.op("pe", lambda e, kk=kk, pb=pb, wa=wa: e.matmul(pb[0:2, :], lhsT=csT[:, kk, :], rhs=wa[:, kk, :],
                                                                   start=(kk == 0), stop=(kk == KC - 1)),
                     r=[t_csT, t_wa[slot]], w=[tb])
            bb = RB[0:2, 5, 1024 + slot * 512:1024 + (slot + 1) * 512]
            S.op("dve", lambda e, pb=pb, mb=mb, bb=bb: e.tensor_tensor(out=mb, in0=pb[0:2, :], in1=bb, op=ALU.add),
                 r=[tb] + k.t_bb[slot], w=[t_mb[slot]])
            if j in (2, 3, 8, 9):
                S.op("dve", lambda e, mb=mb: e.tensor_scalar(out=mb, in0=mb, scalar1=1.0, scalar2=None, op0=ALU.add),
                     r=[t_mb[slot]], w=[t_mb[slot]])
            S.dma(mod_d[l, :, j * 512:(j + 1) * 512], mb, r=[t_mb[slot]], w=[t_mod])
            for q_ in range(4):
                jj = j * 4 + q_
                S.op("pe", lambda e, jj=jj, q_=q_, mb=mb, pT=pT: e.transpose(out=pT[:, 2 * jj:2 * jj + 2], in_=mb[:, q_ * 128:(q_ + 1) * 128],
                                                                             identity=identF[0:2, 0:2]), r=[t_mb[slot], t_c], w=[tT])
        S.op("dve", lambda e, l=l, pT=pT: e.tensor_copy(out=modT[:, l, :, :], in_=pT[:, 0:96].rearrange("p (j r) -> p j r", r=2)),
             r=[tT], w=[t_modT])
        k.reserved = set()
        dump(f"mod{l}", mod_d[l, :, :], [2, 6 * D], F32, [t_mod])
    S.barrier()

    def bcast_rows(l, which):
        gi = 2 if which == "mix" else 5
        li = 0 if which == "mix" else 1
        S.dma(bc[:, 0, :], mod_d[l, 0:1, gi * D:(gi + 1) * D].to_broadcast([128, D]), r=[t_mod], w=[t_bc[0]])
        S.dma(bc[:, 1, :], mod_d[l, 1:2, gi * D:(gi + 1) * D].to_broadcast([128, D]), r=[t_mod], w=[t_bc[1]])
        S.dma(bc[:, 2, :], lng_d[l, li:li + 1, :].to_broadcast([128, D]), w=[t_bc[2]])
        S.dma(bc[:, 3, :], lnb_d[l, li:li + 1, :].to_broadcast([128, D]), w=[t_bc[3]])

    def ln_stats(src, t_src, st_ap, mv_ap, rs_ap, nb_ap, t_st):
        for j in range(2):
            S.op("dve", lambda e, j=j: e.bn_stats(out=st_ap[:, j, :], in_=src[:, j * 512:(j + 1) * 512]), r=[t_src], w=[t_st])
        S.op("dve", lambda e: e.bn_aggr(out=mv_ap, in_=st_ap), r=[t_st], w=[t_st])
        S.op("act", lambda e: e.activation(out=rs_ap, in_=mv_ap[:, 1:2], func=AF.Sqrt, bias=cst[:, 0:1]), r=[t_st, t_c], w=[t_st])
        S.op("dve", lambda e: e.reciprocal(out=rs_ap, in_=rs_ap), r=[t_st], w=[t_st])
        S.op("dve", lambda e: e.tensor_scalar(out=nb_ap, in0=mv_ap[:, 0:1], scalar1=rs_ap, scalar2=-1.0, op0=ALU.mult, op1=ALU.mult),
             r=[t_st], w=[t_st])

    k.st_i = 0

    def st_slot():
        i = k.st_i
        k.st_i = (i + 1) % 8
        base = i * 24
        return (sm[:, base:base + 12].rearrange("p (a b) -> p a b", b=6), sm[:, base + 12:base + 14],
                sm[:, base + 14:base + 15], sm[:, base + 15:base + 16], k.t_st[i])
    k.t_st = [Trk() for _ in range(8)]

    def ln_part1(src, t_src, xn, t_xn):
        st_ap, mv_ap, rs_ap, nb_ap, t_st = st_slot()
        ln_stats(src, t_src, st_ap, mv_ap, rs_ap, nb_ap, t_st)
        S.op("act", lambda e: e.activation(out=xn, in_=src, func=AF.Identity, scale=rs_ap, bias=nb_ap), r=[t_src, t_st], w=[t_xn])

    def ln_part2(l, xn, t_xn, i, which, router, uf=None, t_uf=None):
        r_ = 1 if i < 2 else 0
        pp, tp = pair()
        for kk in range(KC):
            S.op("pe", lambda e, kk=kk: e.transpose(out=pp[:, kk * 128:(kk + 1) * 128], in_=xn[:, kk * 128:(kk + 1) * 128],
                                                    identity=identF[:]), r=[t_xn, t_c], w=[tp[kk // 4]])
        for kk in range(KC):
            sc_ap = modT[:, l, (which + 1) * 8 + kk, r_:r_ + 1]
            sh_ap = modT[:, l, which * 8 + kk, r_:r_ + 1]
            if router:
                o_ap = uf[:, kk, :]
                tw = t_uf
            else:
                o_ap = uT[:, kk, i * 128:(i + 1) * 128]
                tw = t_uT[i]
            if kk % 2 == 0:
                S.op("act", lambda e, kk=kk, o_ap=o_ap, sc_ap=sc_ap, sh_ap=sh_ap: e.activation(
                    out=o_ap, in_=pp[:, kk * 128:(kk + 1) * 128], func=AF.Identity, scale=sc_ap, bias=sh_ap),
                    r=[tp[kk // 4], t_modT], w=[tw])
            else:
                S.op("dve", lambda e, kk=kk, o_ap=o_ap, sc_ap=sc_ap, sh_ap=sh_ap: e.tensor_scalar(
                    out=o_ap, in0=pp[:, kk * 128:(kk + 1) * 128], scalar1=sc_ap, scalar2=sh_ap, op0=ALU.mult, op1=ALU.add),
                    r=[tp[kk // 4], t_modT], w=[tw])
        if router:
            S.op("pool", lambda e: e.tensor_copy(out=uT[:, :, i * 128:(i + 1) * 128], in_=uf[:, :, :]), r=[t_uf], w=[t_uT[i]])

    def ln_to_uT(l, src, t_src, xn, t_xn, i, which, router, uf=None, t_uf=None):
        ln_part1(src, t_src, xn, t_xn)
        ln_part2(l, xn, t_xn, i, which, router, uf, t_uf)
        if router:
            route(i, uf, t_uf)

    def route(i, uf, t_uf):
        pb, tb = bank()
        for kk in range(KC):
            S.op("pe", lambda e, kk=kk: e.matmul(pb[:, 0:16], lhsT=uf[:, kk, :], rhs=wrt[:, kk, :], start=(kk == 0),
                                                 stop=(kk == KC - 1)), r=[t_uf, t_wr], w=[tb])
        S.op("act", lambda e: e.activation(out=gates[:, i, :], in_=pb[:, 0:16], func=AF.Copy), r=[tb], w=[t_gates[i]])

    def route_all(t0, nt, scr, t_scr):
        n16 = nt * 16
        o = [0]

        def take(n):
            a = scr[:, o[0]:o[0] + n]
            o[0] += n
            return a
        lg = gates[:, t0:t0 + nt, :]
        pr, sl, s2, eq, eq2 = [take(n16).rearrange("p (t e) -> p t e", e=16) for _ in range(5)]
        g4, g4b, g4c = [take(nt * 4).rearrange("p (t g) -> p t g", g=4) for _ in range(3)]
        sc1, sc2 = take(nt), take(nt)
        BIG = 1.0e4
        tg = t_gates[t0:t0 + nt]

        def dv(fn):
            S.op("dve", fn, r=t_scr + [k.t_c] + tg, w=t_scr)

        def bc16(a):
            return a.unsqueeze(2).to_broadcast([128, nt, 16])

        def g4v(a):
            return a.rearrange("p t (g e) -> p (t g) e", e=4)

        def g4f(a):
            return a.rearrange("p t g -> p (t g)")
        dv(lambda e: e.tensor_reduce(out=sc1, in_=lg, axis=AX.X, op=ALU.max))
        dv(lambda e: e.tensor_tensor(out=pr, in0=lg, in1=bc16(sc1), op=ALU.subtract))
        S.op("act", lambda e: e.activation(out=pr, in_=pr, func=AF.Exp), r=t_scr, w=t_scr)
        dv(lambda e: e.tensor_reduce(out=sc2, in_=pr, axis=AX.X, op=ALU.add))
        dv(lambda e: e.reciprocal(out=sc2, in_=sc2))
        dv(lambda e: e.tensor_tensor(out=pr, in0=pr, in1=bc16(sc2), op=ALU.mult))
        dv(lambda e: e.tensor_tensor(out=sl, in0=pr, in1=brb[:].unsqueeze(1).to_broadcast([128, nt, 16]), op=ALU.add))
        dv(lambda e: e.tensor_reduce(out=g4f(g4), in_=g4v(sl), axis=AX.X, op=ALU.max))
        dv(lambda e: e.tensor_tensor(out=g4v(eq), in0=g4v(sl), in1=g4f(g4).unsqueeze(2).to_broadcast([128, nt * 4, 4]), op=ALU.is_equal))
        dv(lambda e: e.scalar_tensor_tensor(out=s2.rearrange("p t e -> p (t e)"), in0=eq.rearrange("p t e -> p (t e)"), scalar=-BIG,
                                            in1=sl.rearrange("p t e -> p (t e)"), op0=ALU.mult, op1=ALU.add))
        dv(lambda e: e.tensor_reduce(out=g4f(g4b), in_=g4v(s2), axis=AX.X, op=ALU.max))
        dv(lambda e: e.tensor_tensor(out=g4f(g4), in0=g4f(g4), in1=g4f(g4b), op=ALU.add))
        dv(lambda e: e.tensor_reduce(out=sc1, in_=g4, axis=AX.X, op=ALU.max))
        dv(lambda e: e.tensor_tensor(out=g4c, in0=g4, in1=sc1.unsqueeze(2).to_broadcast([128, nt, 4]), op=ALU.is_equal))
        dv(lambda e: e.tensor_scalar(out=g4f(g4c), in0=g4f(g4c), scalar1=-1.0, scalar2=BIG, op0=ALU.add, op1=ALU.mult))
        dv(lambda e: e.tensor_tensor(out=g4v(s2), in0=g4v(sl), in1=g4f(g4c).unsqueeze(2).to_broadcast([128, nt * 4, 4]), op=ALU.add))
        dv(lambda e: e.tensor_reduce(out=sc1, in_=s2, axis=AX.X, op=ALU.max))
        dv(lambda e: e.tensor_tensor(out=eq, in0=s2, in1=bc16(sc1), op=ALU.is_equal))
        dv(lambda e: e.scalar_tensor_tensor(out=s2.rearrange("p t e -> p (t e)"), in0=eq.rearrange("p t e -> p (t e)"), scalar=-BIG,
                                            in1=s2.rearrange("p t e -> p (t e)"), op0=ALU.mult, op1=ALU.add))
        dv(lambda e: e.tensor_reduce(out=sc1, in_=s2, axis=AX.X, op=ALU.max))
        dv(lambda e: e.tensor_tensor(out=eq2, in0=s2, in1=bc16(sc1), op=ALU.is_equal))
        dv(lambda e: e.tensor_tensor(out=eq, in0=eq, in1=eq2, op=ALU.add))
        dv(lambda e: e.tensor_tensor(out=eq, in0=eq, in1=pr, op=ALU.mult))
        dv(lambda e: e.tensor_reduce(out=sc2, in_=eq, axis=AX.X, op=ALU.add))
        dv(lambda e: e.reciprocal(out=sc2, in_=sc2))
        S.op("dve", lambda e: e.tensor_tensor(out=lg, in0=eq, in1=bc16(sc2), op=ALU.mult), r=t_scr, w=tg)
    k.route_all = route_all
    k.t_route = [Trk(), Trk()]
    k.t_tile = [Trk() for _ in range(12)]

    k.wslot = 0

    def load_w(src_ap, nslots=1, parts=128):
        s0 = k.wslot
        if s0 + nslots > 5:
            s0 = 0
        k.wslot = (s0 + nslots) % 5
        a, b = src_ap.shape[1], src_ap.shape[2]
        dst = wbuf[0:parts, s0:s0 + nslots, :].rearrange("p s n -> p (s n)")[:, 0:a * b].rearrange("p (a b) -> p a b", b=b)
        trks = t_wb[s0:s0 + nslots]
        S.dma(dst, src_ap, w=trks, q="pool")
        return dst, trks

    def proj_fm(wv, t_w, c0, M, evac, toks=TOKB):
        for (t0, nt) in toks:
            pb, tb = bank()
            for kk in range(KC):
                S.op("pe", lambda e, kk=kk, pb=pb, t0=t0, nt=nt: e.matmul(pb[0:M, 0:nt], lhsT=wv[:, kk, c0:c0 + M],
                                                                          rhs=uT[:, kk, t0:t0 + nt], start=(kk == 0), stop=(kk == KC - 1)),
                     r=t_w + t_uT[t0 // 128:(t0 + nt) // 128], w=[tb])
            evac(pb, tb, t0, nt)

    def proj_tm(wv, t_w, c0, ncol, evac, tiles=range(NT)):
        for i in tiles:
            pb, tb = bank()
            for kk in range(KC):
                S.op("pe", lambda e, kk=kk, pb=pb, i=i: e.matmul(pb[:, 0:ncol], lhsT=uT[:, kk, i * 128:(i + 1) * 128],
                                                                 rhs=wv[:, kk, c0:c0 + ncol], start=(kk == 0), stop=(kk == KC - 1)),
                     r=t_w + [t_uT[i]], w=[tb])
            evac(pb, tb, i)

    k.proj_fm = proj_fm
    k.proj_tm = proj_tm
    k.load_w = load_w
    k.bank = bank
    k.pair = pair
    k.dump = dump
    k.ln_to_uT = ln_to_uT
    k.ln_part1 = ln_part1
    k.ln_part2 = ln_part2
    k.route = route
    k.ln_stats = ln_stats
    k.st_slot = st_slot
    k.bcast_rows = bcast_rows
    for nm in ("x_d ctx_d w0_d sink_d lbl_d hn_d wo0_d w1_d gw_d gb_d gn_d wo1_d eg_d eu_d ed_d out_d hmid_d hout0_d mix_d "
               "t_hmid t_hout0 t_mixd t_out identF identB onesF onesB cst Mf Mb MP MN rmask modT t_modT mod_d t_mod bc t_bc uT t_uT "
               "wbuf t_wb RB t_rb HB t_hb sm gates t_gates t_c PS PT").split():
        setattr(k, nm, locals()[nm])

    for ph, l in [("B", 0), ("M", 0), ("D", 0), ("E", 0), ("B", 1), ("M", 1), ("D", 1), ("E", 1)]:
        if ph == "B":
            phase_B(k, l)
        elif ph == "M":
            (mixer_even if l == 0 else mixer_odd)(k)
        elif ph == "D":
            phase_D(k, l)
        else:
            phase_E(k, l)
        S.barrier()
        if stop is not None and stop[0:2] == f"{ph}{l}":
            break

    S.wait_all("sp", t_out + k.dbg_out)
    S.emit()
    S.close()
    return nc


def phase_B(k, l):
    S = k.S
    bufs = {}

    def stA(i):
        slot = i % 2
        ht = k.RB[:, slot, 0:1024]
        t_ht = k.t_tile[slot]
        xn = k.RB[:, 2 + slot, 0:1024]
        t_xn = k.t_tile[2 + slot]
        if l == 0:
            src = k.ctx_d[i * 128:(i + 1) * 128, :] if i < 2 else k.x_d[(i - 2) * 128:(i - 1) * 128, :]
            S.dma(ht, src, w=[t_ht])
        else:
            S.dma(ht, k.hout0_d[i * 128:(i + 1) * 128, :], r=[k.t_hout0[i]], w=[t_ht])
        k.ln_part1(ht, t_ht, xn, t_xn)
        bufs[i] = (xn, t_xn)

    def stB(i):
        xn, t_xn = bufs.pop(i)
        k.ln_part2(l, xn, t_xn, i, 0, False)
    for n in range(NT + 1):
        if n < NT:
            stA(n)
        if n >= 1:
            stB(n - 1)
    k.dump(f"uT{l}", k.uT[:, :, :], [128, KC, N], BF16, k.t_uT)


def row_to_col(k, src, nr, n, dst, t_src, t_dst):
    S = k.S
    pb, tb = k.bank()
    S.op("pe", lambda e: e.transpose(out=pb[0:n, 0:nr], in_=src, identity=k.identF[0:nr, 0:nr]), r=[t_src, k.t_c], w=[tb])
    S.op("dve", lambda e: e.tensor_copy(out=dst, in_=pb[0:n, 0:nr]), r=[tb], w=[t_dst])


def proj_block(k, wv, t_w, c0, M, t0, nt):
    S = k.S
    pb, tb = k.bank()
    for kk in range(KC):
        S.op("pe", lambda e, kk=kk: e.matmul(pb[0:M, 0:nt], lhsT=wv[:, kk, c0:c0 + M], rhs=k.uT[:, kk, t0:t0 + nt],
                                             start=(kk == 0), stop=(kk == KC - 1)),
             r=t_w + k.t_uT[t0 // 128:(t0 + nt) // 128], w=[tb])
    return pb, tb


def gated_scan(k, dk, dvh, nh, q_ap, t_q, k_ap, t_k, A, t_A, B, t_B, make_logf, v_fn, t_v, o_acc, t_o, qts, t_qts, kts, t_kts, L=64):
    S = k.S
    sc = k.scn
    NC_ = N // L
    cpt = 128 // L
    dvt = dvh * nh
    B3 = B.rearrange("p (c l) -> p c l", l=L)
    for a in range(nh):
        S.op("pool", lambda e, a=a: e.memset(o_acc[a], 0.0), w=[t_o[a]])
    D_ = []
    for d in range(2):
        qt, kt, t_qt, t_kt = qts[d], kts[d], t_qts[d], t_kts[d]
        t_sc = k.t_scn[d]
        rr = sc["rr"][0:dk, d, 0:NC_]
        gg = sc["gg"][0:dk, d, 0:NC_]
        X1 = sc["X1"][0:dk, d, 0:NC_]
        X2 = sc["X2"][0:dk, d, 0:NC_]
        EG = sc["EG"][0:dk, d, 0:NC_]
        make_logf(d)
        S.op("dve", lambda e: e.tensor_tensor_scan(out=B, data0=k.rmask[0:dk, :], data1=A, initial=0.0, op0=ALU.mult, op1=ALU.add),
             r=[t_A, k.t_c], w=[t_B])
        S.op("pool", lambda e, gg=gg: e.tensor_copy(out=gg, in_=B3[:, :, L - 1]), r=[t_B], w=[t_sc])
        if d == 1:
            S.op("pool", lambda e: e.tensor_tensor(out=B, in0=B, in1=A, op=ALU.subtract), r=[t_B, t_A], w=[t_B])
        S.op("pool", lambda e, rr=rr: e.tensor_copy(out=rr, in_=B3[:, :, L // 2]), r=[t_B], w=[t_sc])
        S.op("dve", lambda e, rr=rr: e.tensor_tensor(out=B3, in0=B3, in1=rr.unsqueeze(2).to_broadcast([dk, NC_, L]), op=ALU.subtract),
             r=[t_B, t_sc], w=[t_B])
        sgn = 1.0 if d == 0 else -1.0
        S.op("act", lambda e, sgn=sgn: e.activation(out=A, in_=B, func=AF.Exp, scale=sgn), r=[t_B], w=[t_A])
        S.op("dve", lambda e, qt=qt: e.tensor_tensor(out=qt, in0=q_ap, in1=A, op=ALU.mult), r=[t_q, t_A], w=[t_qt])
        S.op("act", lambda e, sgn=sgn: e.activation(out=A, in_=B, func=AF.Exp, scale=-sgn), r=[t_B, t_qt], w=[t_A])
        S.op("dve", lambda e, kt=kt: e.tensor_tensor(out=kt, in0=k_ap, in1=A, op=ALU.mult), r=[t_k, t_A], w=[t_kt])
        S.op("act", lambda e, X1=X1, rr=rr: e.activation(out=X1, in_=rr, func=AF.Exp), r=[t_sc], w=[t_sc])
        S.op("act", lambda e, EG=EG, gg=gg: e.activation(out=EG, in_=gg, func=AF.Exp), r=[t_sc], w=[t_sc])
        S.op("dve", lambda e, X2=X2, gg=gg, rr=rr: e.tensor_tensor(out=X2, in0=gg, in1=rr, op=ALU.subtract), r=[t_sc], w=[t_sc])
        S.op("act", lambda e, X2=X2: e.activation(out=X2, in_=X2, func=AF.Exp), r=[t_sc], w=[t_sc])
        a_s, c_s = (X1, X2) if d == 0 else (X2, X1)
        fo = tuple(range(cpt))
        bo = tuple(reversed(range(cpt)))
        if d == 0:
            tiles = [(i, fo) for i in range(NT)]
        else:
            tiles = [(i, bo) for i in (1, 0)] + [(i, bo) for i in range(NT - 1, 1, -1)]
        st = {"d": d, "qt": qt, "kt": kt, "t_qt": t_qt, "t_kt": t_kt, "t_sc": t_sc, "EG": EG, "a_s": a_s, "c_s": c_s,
              "M": k.Mf if d == 0 else k.Mb, "tiles": tiles, "s_i": 0, "sb_i": 0, "am_i": 0, "pend": None, "nchunk": 0, "ds_i": 0}
        S.op("pool", lambda e, d=d: e.memset(sc["Sst"][0:dk, d, 0, 0:dvt], 0.0), w=[k.t_Sst[d][0]])
        S.op("pool", lambda e, d=d: e.memset(sc["Sbf"][0:dk, d, 0, 0:dvt], 0.0), w=[k.t_Sbf[d][0]])
        D_.append(st)

    def stage1(st, i):
        d = st["d"]
        tk0 = i * 128
        vt = v_fn(i)
        am_i = st["am_i"]
        st["am_i"] = 1 - am_i
        Am = sc["Am"][:, d, am_i, :]
        ktok = sc["ktok"][:, d, am_i, 0:dk]
        t_Am = k.t_Am2[d][am_i]
        t_kk = k.t_ktok2[d][am_i]
        qt, kt = st["qt"], st["kt"]
        pA, tA = k.bank()
        S.op("pe", lambda e: e.matmul(pA[:, 0:128], lhsT=kt[:, tk0:tk0 + 128], rhs=qt[:, tk0:tk0 + 128], start=True, stop=True),
             r=[st["t_kt"], st["t_qt"]], w=[tA])
        M_ = st["M"]
        S.op("dve", lambda e: e.tensor_tensor(out=Am, in0=pA[:, 0:128], in1=M_[:], op=ALU.mult), r=[tA, k.t_c], w=[t_Am])
        pK, tK = k.bank()
        S.op("pe", lambda e: e.matmul(pK[:, 0:dk], lhsT=kt[:, tk0:tk0 + 128], rhs=k.identB[0:dk, 0:dk], start=True, stop=True),
             r=[st["t_kt"], k.t_c], w=[tK])
        S.op("act", lambda e: e.activation(out=ktok, in_=pK[:, 0:dk], func=AF.Copy), r=[tK], w=[t_kk])
        pI, tI = k.bank()
        for a in range(nh):
            S.op("pe", lambda e, a=a: e.matmul(pI[0:dvh, a * 128:(a + 1) * 128], lhsT=vt[:, a * dvh:(a + 1) * dvh], rhs=Am[:, :], start=True, stop=True),
                 r=t_v + [t_Am], w=[tI])
        pS = [None] * cpt
        c_s = st["c_s"]
        for hf in range(cpt):
            pS_, tS_ = k.bank()
            rows = slice(hf * L, hf * L + L)
            S.op("pe", lambda e, pS_=pS_, rows=rows: e.matmul(pS_[0:dk, 0:dvt], lhsT=ktok[rows, :], rhs=vt[rows, 0:dvt], start=True, stop=True),
                 r=[t_kk] + t_v, w=[tS_])
            ds_i = st["ds_i"]
            st["ds_i"] = (ds_i + 1) % 4
            dSs = sc["dSs"][0:dk, d, ds_i, 0:dvt]
            c = cpt * i + hf
            S.op("act", lambda e, pS_=pS_, dSs=dSs, c=c: e.activation(out=dSs, in_=pS_[0:dk, 0:dvt], func=AF.Identity, scale=c_s[:, c:c + 1]),
                 r=[tS_, st["t_sc"]], w=[k.t_dSs[d][ds_i]])
            pS[hf] = (dSs, k.t_dSs[d][ds_i])
        for a in range(nh):
            S.op("dve", lambda e, a=a: e.tensor_tensor(out=o_acc[a][:, tk0:tk0 + 128], in0=pI[0:dvh, a * 128:(a + 1) * 128],
                                                       in1=o_acc[a][:, tk0:tk0 + 128], op=ALU.add), r=[tI, t_o[a]], w=[t_o[a]])
        st["pend"] = (i, pS)

    def stage2_chunk(st, i, hf, pS, po, tpo, last):
        d = st["d"]
        c = cpt * i + hf
        tok0 = c * L
        qt = st["qt"]
        sb_i = st["sb_i"]
        Sb = sc["Sbf"][0:dk, d, sb_i, 0:dvt]
        for a in range(nh):
            S.op("pe", lambda e, a=a: e.matmul(po[0:dvh, a * 128 + hf * L:a * 128 + hf * L + L], lhsT=Sb[:, a * dvh:(a + 1) * dvh],
                                               rhs=qt[:, tok0:tok0 + L], start=True, stop=True), r=[k.t_Sbf[d][sb_i], st["t_qt"]], w=[tpo])
        if last:
            return
        s_i = st["s_i"]
        Sc = sc["Sst"][0:dk, d, s_i, 0:dvt]
        Sn = sc["Sst"][0:dk, d, 1 - s_i, 0:dvt]
        EG, a_s = st["EG"], st["a_s"]
        dSs, t_dS = pS[hf]
        S.op("dve", lambda e: e.scalar_tensor_tensor(out=Sn, in0=Sc, scalar=EG[:, c:c + 1], in1=dSs, op0=ALU.mult, op1=ALU.add),
             r=[k.t_Sst[d][s_i], t_dS, st["t_sc"]], w=[k.t_Sst[d][1 - s_i]])
        st["s_i"] = 1 - s_i
        n_ = st["nchunk"] + 1
        ti, hfo = st["tiles"][n_ // cpt]
        cn = cpt * ti + hfo[n_ % cpt]
        Sbn = sc["Sbf"][0:dk, d, 1 - sb_i, 0:dvt]
        S.op("act", lambda e: e.activation(out=Sbn, in_=Sn, func=AF.Identity, scale=a_s[:, cn:cn + 1]),
             r=[k.t_Sst[d][1 - s_i], st["t_sc"]], w=[k.t_Sbf[d][1 - sb_i]])
        st["sb_i"] = 1 - sb_i

    nT = NT
    for step in range(nT + 1):
        if step < nT:
            for st in D_:
                stage1(st, st["tiles"][step][0])
        if step >= 1:
            pend = []
            for st in D_:
                i, hfo = st["tiles"][step - 1]
                po, tpo = k.bank()
                pend.append((st, i, hfo, po, tpo))
            for which in range(cpt):
                for (st, i, hfs, po, tpo) in pend:
                    pS = st["pS_prev"]
                    last = (st["nchunk"] == cpt * nT - 1)
                    stage2_chunk(st, i, hfs[which], pS, po, tpo, last)
                    st["nchunk"] += 1
            for (st, i, hfs, po, tpo) in pend:
                tk0 = i * 128
                for a in range(nh):
                    S.op("dve", lambda e, a=a, po=po, tk0=tk0: e.tensor_tensor(out=o_acc[a][:, tk0:tk0 + 128], in0=po[0:dvh, a * 128:(a + 1) * 128],
                                                                               in1=o_acc[a][:, tk0:tk0 + 128], op=ALU.add), r=[tpo, t_o[a]], w=[t_o[a]])
        for st in D_:
            if st["pend"] is not None:
                st["pS_prev"] = st["pend"][1]


def rms_gate_out(k, dvh, nh, o_acc, t_o, A, t_A, B, t_B, gsil, t_gs, gn_cols, t_gn, mixrow, t_mix, chunk_ids):
    S = k.S
    dv = dvh * nh
    for (t0, nt) in TOKB:
        pb, tb = k.bank()
        for a in range(nh):
            S.op("act", lambda e, a=a, t0=t0, nt=nt: e.activation(out=A[0:dvh, a * 512:a * 512 + nt], in_=o_acc[a][:, t0:t0 + nt], func=AF.Square),
                 r=[t_o[a]], w=[t_A])
        for a in range(nh):
            S.op("pe", lambda e, a=a, pb=pb, nt=nt: e.matmul(pb[0:dvh, 0:nt], lhsT=k.onesF[0:dvh, 0:dvh], rhs=A[0:dvh, a * 512:a * 512 + nt],
                                                             start=(a == 0), stop=(a == nh - 1)), r=[t_A, k.t_c], w=[tb])
        S.op("act", lambda e, pb=pb, nt=nt: e.activation(out=B[0:dvh, 0:nt], in_=pb[0:dvh, 0:nt], func=AF.Sqrt, scale=1.0 / dv, bias=k.cst[0:dvh, 0:1]),
             r=[tb, k.t_c], w=[t_B])
        S.op("dve", lambda e, nt=nt: e.reciprocal(out=B[0:dvh, 0:nt], in_=B[0:dvh, 0:nt]), r=[t_B], w=[t_B])
        for a in range(nh):
            S.op("dve", lambda e, a=a, t0=t0, nt=nt: e.tensor_tensor(out=B[0:dvh, 512 + a * 512:512 + a * 512 + nt], in0=o_acc[a][:, t0:t0 + nt],
                                                                     in1=B[0:dvh, 0:nt], op=ALU.mult), r=[t_o[a], t_B], w=[t_B])
            S.op("dve", lambda e, a=a, t0=t0, nt=nt: e.scalar_tensor_tensor(out=mixrow[a][0:dvh, t0:t0 + nt], in0=B[0:dvh, 512 + a * 512:512 + a * 512 + nt],
                                                                            scalar=gn_cols[a], in1=gsil[a][0:dvh, t0:t0 + nt], op0=ALU.mult, op1=ALU.mult),
                 r=[t_B, t_gn, t_gs[a]], w=[t_mix[a]])
    for a in range(nh):
        S.dma(k.mix_d[chunk_ids[a], 0:dvh, :], mixrow[a][0:dvh, :], r=[t_mix[a]], w=[k.t_mixd[chunk_ids[a]]])


def scan_setup(k):
    S = k.S
    if hasattr(k, "scn"):
        return
    sc = {}
    for nm in ("rr", "gg", "X1", "X2", "EG"):
        sc[nm] = S.sbuf("sc_" + nm, [128, 2, 36], F32)
    sc["Sst"] = S.sbuf("sc_Sst", [128, 2, 2, 192], F32)
    sc["dSs"] = S.sbuf("sc_dSs", [128, 2, 4, 192], BF16)
    sc["Sbf"] = S.sbuf("sc_Sbf", [128, 2, 2, 192], BF16)
    sc["Am"] = S.sbuf("sc_Am", [128, 2, 2, 128], BF16)
    sc["ktok"] = S.sbuf("sc_ktok", [128, 2, 2, 128], BF16)
    sc["col"] = S.sbuf("sc_col", [128, 64], F32)
    sc["row"] = k.RB[0:8, 5, 0:768]
    k.scn = sc
    k.t_scn = [Trk(), Trk()]
    k.t_Sst = [[Trk(), Trk()], [Trk(), Trk()]]
    k.t_dSs = [[Trk() for _ in range(4)], [Trk() for _ in range(4)]]
    k.t_Sbf = [[Trk(), Trk()], [Trk(), Trk()]]
    k.t_Am2 = [[Trk(), Trk()], [Trk(), Trk()]]
    k.t_ktok2 = [[Trk(), Trk()], [Trk(), Trk()]]
    k.t_Am = [k.t_Am2[0][0], k.t_Am2[0][1]]
    k.t_col = Trk()
    k.t_row = k.t_rb[5]


ATOK = [(0, 256), (256, 512), (768, 512), (1280, 512), (1792, 512)]


def mixer_even(k):
    S = k.S
    scan_setup(k)
    sc = k.scn
    RB, HB, t_rb, t_hb = k.RB, k.HB, k.t_rb, k.t_hb
    Ct = RB[:, 4, 0:2048]
    St = RB[:, 5, 0:2048]
    col = sc["col"]
    t_col = k.t_col
    ci = col[:, 0:8].bitcast(I32)
    S.op("pool", lambda e: e.iota(ci[:, 0:1], pattern=[[0, 1]], base=0, channel_multiplier=1), w=[t_col])
    S.op("dve", lambda e: e.tensor_single_scalar(out=ci[:, 1:2], in_=ci[:, 0:1], scalar=15, op=ALU.bitwise_and), r=[t_col], w=[t_col])
    S.op("dve", lambda e: e.tensor_scalar(out=ci[:, 2:3], in0=ci[:, 0:1], scalar1=5, scalar2=1, op0=ALU.logical_shift_right, op1=ALU.bitwise_and),
         r=[t_col], w=[t_col])
    S.op("dve", lambda e: e.tensor_scalar(out=ci[:, 3:4], in0=ci[:, 0:1], scalar1=4, scalar2=1, op0=ALU.logical_shift_right, op1=ALU.bitwise_and),
         r=[t_col], w=[t_col])
    S.op("dve", lambda e: e.tensor_copy(out=col[:, 8:11], in_=ci[:, 1:4]), r=[t_col], w=[t_col])
    S.op("act", lambda e: e.activation(out=col[:, 11:12], in_=col[:, 8:9], func=AF.Exp, scale=-math.log(10000.0) / 16.0), r=[t_col], w=[t_col])
    S.op("dve", lambda e: e.tensor_tensor(out=col[:, 13:14], in0=col[:, 11:12], in1=col[:, 9:10], op=ALU.mult), r=[t_col], w=[t_col])
    S.op("dve", lambda e: e.tensor_tensor(out=col[:, 12:13], in0=col[:, 11:12], in1=col[:, 13:14], op=ALU.subtract), r=[t_col], w=[t_col])
    S.op("dve", lambda e: e.tensor_scalar(out=col[:, 14:15], in0=col[:, 10:11], scalar1=2.0, scalar2=-1.0, op0=ALU.mult, op1=ALU.add),
         r=[t_col], w=[t_col])
    ri = RB[:, 0, 0:2048].bitcast(I32)
    qi = RB[:, 1, 0:2048].bitcast(I32)
    S.op("pool", lambda e: e.iota(ri, pattern=[[1, 32], [0, 64]], base=0, channel_multiplier=0), w=[t_rb[0]])
    S.op("pool", lambda e: e.iota(qi, pattern=[[0, 32], [1, 64]], base=0, channel_multiplier=0), w=[t_rb[1]])
    rf = RB[:, 2, 0:2048]
    qf = RB[:, 3, 0:2048]
    S.op("dve", lambda e: e.tensor_copy(out=rf, in_=ri), r=[t_rb[0]], w=[t_rb[2]])
    S.op("dve", lambda e: e.tensor_copy(out=qf, in_=qi), r=[t_rb[1]], w=[t_rb[3]])
    ang = RB[:, 0, 0:2048]
    S.op("dve", lambda e: e.tensor_scalar(out=ang, in0=rf, scalar1=col[:, 12:13], scalar2=None, op0=ALU.mult), r=[t_rb[2], t_col], w=[t_rb[0]])
    S.op("dve", lambda e: e.scalar_tensor_tensor(out=ang, in0=qf, scalar=col[:, 13:14], in1=ang, op0=ALU.mult, op1=ALU.add),
         r=[t_rb[3], t_rb[0], t_col], w=[t_rb[0]])
    def range_reduce(dst, t_dst, add, tmpi, t_tmpi, tmpf_, t_tmpf):
        S.op("dve", lambda e: e.tensor_scalar(out=tmpi, in0=ang, scalar1=add, scalar2=1.0 / (2 * PI), op0=ALU.add, op1=ALU.mult),
             r=[t_rb[0]], w=[t_tmpi])
        S.op("dve", lambda e: e.tensor_copy(out=tmpf_, in_=tmpi), r=[t_tmpi], w=[t_tmpf])
        S.op("dve", lambda e: e.scalar_tensor_tensor(out=dst, in0=tmpf_, scalar=-2 * PI, in1=ang, op0=ALU.mult, op1=ALU.add),
             r=[t_tmpf, t_rb[0]], w=[t_dst])
        if add != 0.0:
            S.op("dve", lambda e: e.tensor_scalar(out=dst, in0=dst, scalar1=add, scalar2=None, op0=ALU.add), r=[t_dst], w=[t_dst])
        S.op("dve", lambda e: e.tensor_scalar(out=tmpf_, in0=dst, scalar1=PI, scalar2=-2 * PI, op0=ALU.is_gt, op1=ALU.mult),
             r=[t_dst], w=[t_tmpf])
        S.op("dve", lambda e: e.tensor_tensor(out=dst, in0=dst, in1=tmpf_, op=ALU.add), r=[t_dst, t_tmpf], w=[t_dst])
        S.op("dve", lambda e: e.tensor_scalar(out=tmpf_, in0=dst, scalar1=-PI, scalar2=2 * PI, op0=ALU.is_lt, op1=ALU.mult),
             r=[t_dst], w=[t_tmpf])
        S.op("dve", lambda e: e.tensor_tensor(out=dst, in0=dst, in1=tmpf_, op=ALU.add), r=[t_dst, t_tmpf], w=[t_dst])
        S.op("dve", lambda e: e.tensor_scalar(out=dst, in0=dst, scalar1=PI, scalar2=-PI, op0=ALU.min, op1=ALU.max), r=[t_dst], w=[t_dst])
    m1 = RB[:, 1, 0:2048]
    tmpi = RB[:, 2, 0:2048].bitcast(I32)
    tmpf_ = RB[:, 3, 0:2048]
    range_reduce(m1, t_rb[1], 0.0, tmpi, t_rb[2], tmpf_, t_rb[3])
    S.op("act", lambda e: e.activation(out=St, in_=m1, func=AF.Sin, scale=col[:, 14:15]), r=[t_rb[1], t_col], w=[t_rb[5]])
    range_reduce(m1, t_rb[1], PI / 2, tmpi, t_rb[2], tmpf_, t_rb[3])
    S.op("act", lambda e: e.activation(out=Ct, in_=m1, func=AF.Sin), r=[t_rb[1]], w=[t_rb[4]])
    k.dump("ropeC", Ct, [128, 2048], F32, [t_rb[4]])
    k.dump("ropeS", St, [128, 2048], F32, [t_rb[5]])
    if k.sub == "M0a":
        return
    S.dma(col[:, 16:24], k.sink_d[0:1, :].to_broadcast([128, 8]), w=[t_col])
    S.op("act", lambda e: e.activation(out=col[:, 16:24], in_=col[:, 16:24], func=AF.Exp), r=[t_col], w=[t_col])

    qT = HB[:, 0:2, :]
    kAB = [HB[:, 2, :], HB[:, 3, :]]
    vdup = HB[:, 4, :].rearrange("p (i c) -> p i c", c=128)
    mixA = [HB[:, 5, :], HB[:, 6, :]]
    et = [HB[:, 7, ei * 512:(ei + 1) * 512] for ei in range(4)] + [RB[:, 3, 0:256].bitcast(BF16)]
    S.op("pool", lambda e: e.memset(kAB[0][64:128, :], 0.0), w=[t_hb[2]])
    S.op("pool", lambda e: e.memset(kAB[1][0:64, :], 0.0), w=[t_hb[3]])
    t_et = [Trk() for _ in range(5)]
    k.et_i = 0
    tmpf = [RB[:, 0, 0:512], RB[:, 0, 512:1024], RB[:, 1, 0:512], RB[:, 1, 512:1024]]
    t_tmp = [Trk() for _ in range(4)]
    dn = RB[:, 2, 0:512]
    t_dn = t_rb[2]
    wv_v, t_wv = k.load_w(k.w0_d[:, 1536:1664].rearrange("(k p) n -> p k n", p=128))
    for j in range(2):
        base = j * 768
        wq, t_wq = k.load_w(k.w0_d[:, base:base + 512].rearrange("(k p) n -> p k n", p=128))
        wk, t_wk = k.load_w(k.w0_d[:, base + 512:base + 768].rearrange("(k p) n -> p k n", p=128))
        tmp_i = 0
        for (t0, nt) in ATOK:
            for blk in range(3):
                if blk < 2:
                    pq, tq = proj_block(k, wq, t_wq, blk * 128, 128, t0, nt)
                    dst = qT[:, blk, t0:t0 + nt]
                    tdst = t_hb[blk]
                else:
                    pq, tq = proj_block(k, wk, t_wk, 0, 128, t0, nt)
                    dst = None
                if t0 == 0:
                    if blk < 2:
                        S.op("act", lambda e, pq=pq, dst=dst, nt=nt: e.activation(out=dst, in_=pq[:, 0:nt], func=AF.Copy), r=[tq], w=[tdst])
                    else:
                        S.op("act", lambda e, pq=pq, nt=nt, t0=t0: e.activation(out=kAB[0][0:64, t0:t0 + nt], in_=pq[0:64, 0:nt], func=AF.Copy), r=[tq], w=[t_hb[2]])
                        S.op("act", lambda e, pq=pq, nt=nt, t0=t0: e.activation(out=kAB[1][64:128, t0:t0 + nt], in_=pq[64:128, 0:nt], func=AF.Copy), r=[tq], w=[t_hb[3]])
                    continue
                if blk < 2:
                    ps_, ts_ = proj_block(k, wq, t_wq, 256 + blk * 128, 128, t0, nt)
                else:
                    ps_, ts_ = proj_block(k, wk, t_wk, 128, 128, t0, nt)
                l0 = t0 - 256
                ta, tb_ = tmp_i % 4, (tmp_i + 1) % 4
                tmp_i += 2
                S.op("dve", lambda e, pq=pq, ta=ta, l0=l0, nt=nt: e.tensor_tensor(out=tmpf[ta][:, 0:nt], in0=pq[:, 0:nt], in1=Ct[:, l0:l0 + nt], op=ALU.mult),
                     r=[tq, t_rb[4]], w=[t_tmp[ta]])
                S.op("dve", lambda e, ps_=ps_, tb_=tb_, l0=l0, nt=nt: e.tensor_tensor(out=tmpf[tb_][:, 0:nt], in0=ps_[:, 0:nt], in1=St[:, l0:l0 + nt], op=ALU.mult),
                     r=[ts_, t_rb[5]], w=[t_tmp[tb_]])
                if blk < 2:
                    S.op("pool", lambda e, dst=dst, ta=ta, tb_=tb_, nt=nt: e.tensor_tensor(out=dst, in0=tmpf[ta][:, 0:nt], in1=tmpf[tb_][:, 0:nt], op=ALU.add),
                         r=[t_tmp[ta], t_tmp[tb_]], w=[tdst])
                else:
                    S.op("pool", lambda e, ta=ta, tb_=tb_, nt=nt, t0=t0: e.tensor_tensor(out=kAB[0][0:64, t0:t0 + nt], in0=tmpf[ta][0:64, 0:nt], in1=tmpf[tb_][0:64, 0:nt], op=ALU.add),
                         r=[t_tmp[ta], t_tmp[tb_]], w=[t_hb[2]])
                    S.op("pool", lambda e, ta=ta, tb_=tb_, nt=nt, t0=t0: e.tensor_tensor(out=kAB[1][64:128, t0:t0 + nt], in0=tmpf[ta][64:128, 0:nt], in1=tmpf[tb_][64:128, 0:nt], op=ALU.add),
                         r=[t_tmp[ta], t_tmp[tb_]], w=[t_hb[3]])

        if k.sub == "M0p":
            k.dump("qT0", qT, [128, 2, N], BF16, [t_hb[0], t_hb[1]])
            return

        def ev_v(pb, tb, i, j=j):
            S.op("act", lambda e: e.activation(out=vdup[:, i, 0:64], in_=pb[:, j * 64:(j + 1) * 64], func=AF.Copy), r=[tb], w=[t_hb[4]])
            S.op("dve", lambda e: e.tensor_copy(out=vdup[:, i, 64:128], in_=pb[:, j * 64:(j + 1) * 64]), r=[tb], w=[t_hb[4]])
        k.proj_tm(wv_v, t_wv, 0, 128, ev_v)
        if j == 0:
            k.dump("qT0", qT, [128, 2, N], BF16, [t_hb[0], t_hb[1]])
            k.dump("kT0", HB[:, 2:4, :], [128, 2, N], BF16, [t_hb[2], t_hb[3]])
        if k.sub == "M0v":
            return
        for qb in range(NT):
            q0 = qb * 128
            if (k.sub == "M0q1" and qb == 1) or (k.sub == "M0q3" and qb == 3):
                k.dump("mixA", HB[:, 5:7, :], [128, 2, N], BF16, [t_hb[5], t_hb[6]])
                return
            if qb < 2:
                chunks = [(0, None), (1, None)]
            else:
                n_ = qb - 2
                chunks = [(0, None), (1, None)]
                if n_ > 0:
                    chunks.append((qb - 1, k.MP))
                chunks.append((qb, None))
                if n_ < 15:
                    chunks.append((qb + 1, k.MN))
            po, tpo = k.bank()
            pd, tpd = k.bank()
            nch = len(chunks)
            pss_l = []
            for ci_, (kc, msk) in enumerate(chunks):
                pss, tss = k.bank()
                for hh in range(4):
                    blk, half = hh // 2, hh % 2
                    S.op("pe", lambda e, pss=pss, hh=hh, blk=blk, half=half, kc=kc, q0=q0: e.matmul(
                        pss[:, hh * 128:(hh + 1) * 128], lhsT=kAB[half][:, kc * 128:(kc + 1) * 128], rhs=qT[:, blk, q0:q0 + 128],
                        start=True, stop=True), r=[t_hb[2 + half], t_hb[blk]], w=[tss])
                pss_l.append((pss, tss))
            for ci_, (kc, msk) in enumerate(chunks):
                pss, tss = pss_l[ci_]
                ei = ci_
                S.op("act", lambda e, pss=pss, ei=ei: e.activation(out=et[ei], in_=pss[:, :], func=AF.Exp, scale=0.125), r=[tss], w=[t_et[ei]])
                if msk is not None:
                    S.op("dve", lambda e, ei=ei, msk=msk: e.tensor_tensor(out=et[ei], in0=et[ei], in1=msk[:], op=ALU.mult),
                         r=[t_et[ei], k.t_c], w=[t_et[ei]])
            for ci_, (kc, msk) in enumerate(chunks):
                ei = ci_
                S.op("pe", lambda e, ei=ei, kc=kc, ci_=ci_, po=po, nch=nch: e.matmul(
                    po[:, :], lhsT=vdup[:, kc, :], rhs=et[ei][:, :], start=(ci_ == 0), stop=(ci_ == nch - 1)),
                    r=[t_hb[4], t_et[ei]], w=[tpo])
                S.op("pe", lambda e, ei=ei, ci_=ci_, pd=pd, nch=nch: e.matmul(
                    pd[:, :], lhsT=k.onesB[:], rhs=et[ei][:, :], start=(ci_ == 0), stop=(ci_ == nch - 1)),
                    r=[k.t_c, t_et[ei]], w=[tpd])
            if k.sub == "M0qb":
                S.op("dve", lambda e, po=po: e.tensor_copy(out=RB[:, 0, 0:512], in_=po[:, :]), r=[tpo], w=[t_rb[0]])
                S.op("dve", lambda e, pd=pd: e.tensor_copy(out=RB[:, 0, 512:1024], in_=pd[:, :]), r=[tpd], w=[t_rb[0]])
                k.dump("popd", RB[:, 0, 0:1024], [128, 1024], F32, [t_rb[0]])
                return
            for hh in range(4):
                S.op("dve", lambda e, hh=hh, pd=pd, j=j: e.tensor_scalar(out=dn[:, hh * 128:(hh + 1) * 128], in0=pd[:, hh * 128:(hh + 1) * 128],
                                                                    scalar1=col[:, 16 + 4 * j + hh:17 + 4 * j + hh], scalar2=None, op0=ALU.add),
                     r=[tpd, t_col], w=[t_dn])
            S.op("dve", lambda e: e.reciprocal(out=dn, in_=dn), r=[t_dn], w=[t_dn])
            for hh in range(4):
                blk, half = hh // 2, hh % 2
                rows = slice(half * 64, half * 64 + 64)
                S.op("dve", lambda e, hh=hh, blk=blk, rows=rows, po=po, q0=q0: e.tensor_tensor(
                    out=mixA[blk][rows, q0:q0 + 128], in0=po[rows, hh * 128:(hh + 1) * 128], in1=dn[rows, hh * 128:(hh + 1) * 128], op=ALU.mult),
                    r=[tpo, t_dn], w=[t_hb[5 + blk]])
        for blk in range(2):
            S.dma(k.mix_d[2 * j + blk, :, :], mixA[blk], r=[t_hb[5 + blk]], w=[k.t_mixd[2 * j + blk]])
    k.dump("mixd_att", k.mix_d[0:4, :, :], [4, 128, N], BF16, k.t_mixd[0:4])
    S.barrier()
    if k.sub == "M0b":
        return

    row = sc["row"]
    t_row = k.t_row
    S.dma(row[0:4, 0:512], k.lbl_d.rearrange("r a n -> (r a) n"), w=[t_row])
    for h in range(4):
        row_to_col(k, row[0:4, h * 128:(h + 1) * 128], 4, 128, col[:, 24 + 4 * h:28 + 4 * h], t_row, t_col)
    lbT = col[:, 24:40].rearrange("p (h r a) -> p h r a", r=2, a=2)
    lbv = col[:, 44:52].rearrange("p (h r) -> p h r", r=2)
    omv = col[:, 52:60].rearrange("p (h r) -> p h r", r=2)
    S.op("dve", lambda e: e.tensor_tensor(out=lbv, in0=lbT[:, :, :, 0], in1=lbT[:, :, :, 1], op=ALU.subtract), r=[t_col], w=[t_col])
    S.op("act", lambda e: e.activation(out=lbv, in_=lbv, func=AF.Sigmoid), r=[t_col], w=[t_col])
    S.op("dve", lambda e: e.tensor_scalar(out=omv, in0=lbv, scalar1=-1.0, scalar2=1.0, op0=ALU.mult, op1=ALU.add), r=[t_col], w=[t_col])
    S.dma(row[0:1, 0:512], k.hn_d[:, :], w=[t_row])
    for h in range(4):
        row_to_col(k, row[0:1, h * 128:(h + 1) * 128], 1, 128, col[:, 40 + h:41 + h], t_row, t_col)

    qrow, Krow, A, B, oacc = RB[:, 0, :], RB[:, 1, :], RB[:, 2, :], RB[:, 3, :], RB[:, 4, :]
    gsil, mixrow = HB[:, 2, :], HB[:, 4, :]
    qts, kts = [HB[:, 0, :], HB[:, 5, :]], [HB[:, 1, :], HB[:, 6, :]]
    vtm = HB[:, 3, :].rearrange("p (i c) -> p i c", c=128)
    for h in range(4):
        base = 1664 + 512 * h
        wg, t_wg = k.load_w(k.w0_d[:, base:base + 512].rearrange("(k p) n -> p k n", p=128))
        wvv, t_wvv = k.load_w(k.w0_d[:, 3712 + 128 * h:3712 + 128 * (h + 1)].rearrange("(k p) n -> p k n", p=128))

        def ev_q(pb, tb, t0, nt):
            S.op("act", lambda e: e.activation(out=qrow[:, t0:t0 + nt], in_=pb[:, 0:nt], func=AF.Copy), r=[tb], w=[t_rb[0]])
        k.proj_fm(wg, t_wg, 256, 128, ev_q)

        def ev_g(pb, tb, t0, nt):
            S.op("act", lambda e: e.activation(out=B[:, t0:t0 + nt], in_=pb[:, 0:nt], func=AF.Sigmoid), r=[tb], w=[t_rb[3]])
            S.op("dve", lambda e: e.tensor_tensor(out=gsil[:, t0:t0 + nt], in0=pb[:, 0:nt], in1=B[:, t0:t0 + nt], op=ALU.mult),
                 r=[tb, t_rb[3]], w=[t_hb[2]])
        k.proj_fm(wg, t_wg, 384, 128, ev_g)

        def ev_v2(pb, tb, i):
            S.op("act", lambda e: e.activation(out=vtm[:, i, :], in_=pb[:, 0:128], func=AF.Copy), r=[tb], w=[t_hb[3]])
        k.proj_tm(wvv, t_wvv, 0, 128, ev_v2)

        def make_logf(d, h=h, wg=wg, t_wg=t_wg):
            def ev_z(pb, tb, t0, nt):
                S.op("act", lambda e: e.activation(out=A[:, t0:t0 + nt], in_=pb[:, 0:nt], func=AF.Sigmoid), r=[tb], w=[t_rb[2]])
            k.proj_fm(wg, t_wg, d * 128, 128, ev_z)
            S.op("dve", lambda e: e.tensor_scalar(out=A, in0=A, scalar1=omv[:, h, d:d + 1], scalar2=lbv[:, h, d:d + 1], op0=ALU.mult, op1=ALU.add),
                 r=[t_rb[2], t_col], w=[t_rb[2]])
            S.op("pool", lambda e: e.tensor_scalar(out=Krow, in0=A, scalar1=-1.0, scalar2=1.0, op0=ALU.mult, op1=ALU.add),
                 r=[t_rb[2]], w=[t_rb[1]])
            S.op("act", lambda e: e.activation(out=A, in_=A, func=AF.Ln), r=[t_rb[2]], w=[t_rb[2]])
        gated_scan(k, 128, 128, 1, qrow, t_rb[0], Krow, t_rb[1], A, t_rb[2], B, t_rb[3], make_logf,
                   lambda i: vtm[:, i, :], [t_hb[3]], [oacc], [t_rb[4]], qts, [t_hb[0], t_hb[5]], kts, [t_hb[1], t_hb[6]])
        if h == 0:
            k.dump("oacc0", oacc, [128, N], F32, [t_rb[4]])
        rms_gate_out(k, 128, 1, [oacc], [t_rb[4]], A, t_rb[2], B, t_rb[3], [gsil], [t_hb[2]], [col[:, 40 + h:41 + h]], t_col,
                     [mixrow], [t_hb[4]], [4 + h])
    k.dump("mixd0", k.mix_d[0:8, :, :], [8, 128, N], BF16, k.t_mixd[0:8])


def phase_D(k, l):
    S = k.S
    RB, HB = k.RB, k.HB
    k.bcast_rows(l, "mix")
    if l == 0:
        wo, t_wo = k.load_w(k.wo0_d.rearrange("(c p) n -> p c n", p=128), nslots=2)
        chunks = [(c, 128, wo, t_wo, c) for c in range(8)]
        tiles = list(range(NT))
        nch = 8
    else:
        wo, t_wo = k.load_w(k.wo1_d[0:768, :].rearrange("(c p) n -> p c n", p=96), nslots=2, parts=96)
        wf, t_wf = k.load_w(k.wo1_d[768:1024, :].rearrange("(c p) n -> p c n", p=128), nslots=1)
        chunks = [(c, 96, wo, t_wo, c) for c in range(8)] + [(8 + c, 128, wf, t_wf, c) for c in range(2)]
        tiles = list(range(2, NT))
        nch = 10
    tb_ = [RB[:, r, hf * 1024:(hf + 1) * 1024] for r in range(6) for hf in range(2)]
    tt = k.t_tile
    nchunks = len(chunks)

    def bufs_for(n_):
        s6 = (n_ % 2) * 6
        return [tb_[s6 + q] for q in range(6)], [tt[s6 + q] for q in range(6)]

    def stA(n_):
        i = tiles[n_]
        (ht, tmp, xn1, hnew, xn2, ufb), (t_ht, t_tmp, t_xn1, t_hn, t_xn2, t_uf) = bufs_for(n_)
        mt = HB[:, n_ % 2, 0:nch * 128].rearrange("p (c n) -> p c n", n=128)
        t_mt = k.t_hb[n_ % 2]
        S.dma(mt, k.mix_d[0:nch, :, i * 128:(i + 1) * 128].rearrange("c p n -> p c n"), r=k.t_mixd[0:nch], w=[t_mt])
        if l == 0:
            src = k.ctx_d[i * 128:(i + 1) * 128, :] if i < 2 else k.x_d[(i - 2) * 128:(i - 1) * 128, :]
            S.dma(ht, src, w=[t_ht])
        else:
            S.dma(ht, k.hout0_d[i * 128:(i + 1) * 128, :], r=[k.t_hout0[i]], w=[t_ht])
        pp, tp = k.pair()
        for hf in range(2):
            for ci_, (c, KR, wv, t_wv, wc) in enumerate(chunks):
                S.op("pe", lambda e, hf=hf, c=c, KR=KR, wv=wv, wc=wc, ci_=ci_: e.matmul(
                    pp[:, hf * 512:(hf + 1) * 512], lhsT=mt[0:KR, c, :], rhs=wv[0:KR, wc, hf * 512:(hf + 1) * 512],
                    start=(ci_ == 0), stop=(ci_ == nchunks - 1)), r=[t_mt] + t_wv, w=[tp[hf]])
        r_ = 1 if i < 2 else 0
        for hf in range(2):
            S.op("dve", lambda e, hf=hf: e.tensor_tensor(out=tmp[:, hf * 512:(hf + 1) * 512], in0=pp[:, hf * 512:(hf + 1) * 512],
                                                         in1=k.bc[:, r_, hf * 512:(hf + 1) * 512], op=ALU.mult),
                 r=[tp[hf], k.t_bc[r_]], w=[t_tmp])
        S.op("dve", lambda e: e.scalar_tensor_tensor(out=tmp, in0=ht, scalar=ALPHA, in1=tmp, op0=ALU.mult, op1=ALU.add),
             r=[t_ht, t_tmp], w=[t_tmp])
        st_ap, mv_ap, rs_ap, nb_ap, t_st = k.st_slot()
        k.ln_stats(tmp, t_tmp, st_ap, mv_ap, rs_ap, nb_ap, t_st)
        S.op("act", lambda e: e.activation(out=xn1, in_=tmp, func=AF.Identity, scale=rs_ap, bias=nb_ap), r=[t_tmp, t_st], w=[t_xn1])
        S.op("pool", lambda e: e.tensor_tensor(out=xn1, in0=xn1, in1=k.bc[:, 2, :], op=ALU.mult), r=[t_xn1, k.t_bc[2]], w=[t_xn1])
        S.op("pool", lambda e: e.tensor_tensor(out=hnew, in0=xn1, in1=k.bc[:, 3, :], op=ALU.add), r=[t_xn1, k.t_bc[3]], w=[t_hn])
        S.dma(k.hmid_d[i * 128:(i + 1) * 128, :], hnew, r=[t_hn], w=[k.t_hmid[i]])
        k.ln_part1(hnew, t_hn, xn2, t_xn2)

    def stB(n_):
        i = tiles[n_]
        (ht, tmp, xn1, hnew, xn2, ufb), (t_ht, t_tmp, t_xn1, t_hn, t_xn2, t_uf) = bufs_for(n_)
        uf = ufb.rearrange("p (k n) -> p k n", n=128)
        k.ln_part2(l, xn2, t_xn2, i, 3, True, uf, t_uf)

    def stC(n_):
        i = tiles[n_]
        (ht, tmp, xn1, hnew, xn2, ufb), (t_ht, t_tmp, t_xn1, t_hn, t_xn2, t_uf) = bufs_for(n_)
        uf = ufb.rearrange("p (k n) -> p k n", n=128)
        k.route(i, uf, t_uf)
    nT_ = len(tiles)
    for n in range(nT_ + 2):
        if n < nT_:
            stA(n)
        if 1 <= n <= nT_:
            stB(n - 1)
        if n >= 2:
            stC(n - 2)
    k.route_all(tiles[0], len(tiles), RB[:, 0, :], [k.t_tile[0], k.t_tile[1]])
    k.dump(f"hmid{l}", k.hmid_d[:, :], [N, D], F32, k.t_hmid)
    k.dump(f"u2T{l}", k.uT[:, :, :], [128, KC, N], BF16, k.t_uT)
    k.dump(f"gates{l}", k.gates[:, :, :], [128, NT, 16], F32, k.t_gates)


def phase_E(k, l):
    S = k.S
    RB = k.RB
    k.bcast_rows(l, "moe")
    if l == 0:
        halves = [list(range(0, 9)), list(range(9, 18))]
        bsz = 384
    else:
        halves = [list(range(2, 10)), list(range(10, 18))]
        bsz = 512
    yacc = RB[:, 0:4, :].rearrange("p a n -> p (a n)").rearrange("p (t d) -> p t d", d=1024)
    t_y = k.t_tile[0:9]
    hTb = RB[:, 4, :].bitcast(BF16)
    hT = [hTb[:, q * 2048:(q + 1) * 2048].rearrange("p (f n) -> p f n", n=512) for q in range(2)]
    t_hT = [k.t_tile[9], k.t_tile[10]]
    sg = [RB[:, 5, q * 512:(q + 1) * 512] for q in range(2)]
    t_sg = [k.t_rb[4], k.t_rb[5]]
    Hb = RB[:, 5, 1024:2048]
    t_H = k.t_tile[11]
    dst_d = k.hout0_d if l == 0 else k.out_d
    k.h_i = 0
    k.s_i = 0
    for tiles in halves:
        tok0 = tiles[0] * 128
        ntok = len(tiles) * 128
        blocks = [(tok0 + b0, bsz) for b0 in range(0, ntok, bsz)]
        for e_ in range(16):
            w1, t_w1 = k.load_w(k.eg_d[l, e_].rearrange("(c p) n -> p c n", p=128))
            w3, t_w3 = k.load_w(k.eu_d[l, e_].rearrange("(c p) n -> p c n", p=128))
            w2, t_w2 = k.load_w(k.ed_d[l, e_].rearrange("(c p) n -> p c n", p=128))
            for (b0, nb) in blocks:
                hi = k.h_i
                k.h_i = 1 - hi
                hTc = hT[hi]
                for f in range(4):
                    p1, tp1 = k.bank()
                    for kk in range(KC):
                        S.op("pe", lambda e, kk=kk, f=f, p1=p1, w1=w1, b0=b0, nb=nb: e.matmul(
                            p1[:, 0:nb], lhsT=w1[:, kk, f * 128:(f + 1) * 128], rhs=k.uT[:, kk, b0:b0 + nb], start=(kk == 0), stop=(kk == KC - 1)),
                            r=t_w1 + k.t_uT[b0 // 128:(b0 + nb) // 128], w=[tp1])
                    p3, tp3 = k.bank()
                    for kk in range(KC):
                        S.op("pe", lambda e, kk=kk, f=f, p3=p3, w3=w3, b0=b0, nb=nb: e.matmul(
                            p3[:, 0:nb], lhsT=w3[:, kk, f * 128:(f + 1) * 128], rhs=k.uT[:, kk, b0:b0 + nb], start=(kk == 0), stop=(kk == KC - 1)),
                            r=t_w3 + k.t_uT[b0 // 128:(b0 + nb) // 128], w=[tp3])
                    si = k.s_i
                    k.s_i = 1 - si
                    S.op("act", lambda e, p1=p1, si=si, nb=nb: e.activation(out=sg[si][:, 0:nb], in_=p1[:, 0:nb], func=AF.Sigmoid), r=[tp1], w=[t_sg[si]])
                    S.op("dve", lambda e, p1=p1, si=si, nb=nb: e.tensor_tensor(out=sg[si][:, 0:nb], in0=p1[:, 0:nb], in1=sg[si][:, 0:nb], op=ALU.mult),
                         r=[tp1, t_sg[si]], w=[t_sg[si]])
                    S.op("dve", lambda e, p3=p3, si=si, nb=nb, f=f, hTc=hTc: e.tensor_tensor(out=hTc[:, f, 0:nb], in0=p3[:, 0:nb], in1=sg[si][:, 0:nb], op=ALU.mult),
                         r=[tp3, t_sg[si]], w=[t_hT[hi]])
                for tl in range(nb // 128):
                    gi = (b0 // 128) + tl
                    yi = gi - tiles[0]
                    for dh in range(2):
                        py, tpy = k.bank()
                        for f in range(4):
                            S.op("pe", lambda e, f=f, py=py, hTc=hTc, tl=tl, dh=dh, w2=w2: e.matmul(
                                py[:, :], lhsT=hTc[:, f, tl * 128:(tl + 1) * 128], rhs=w2[:, f, dh * 512:(dh + 1) * 512], start=(f == 0), stop=(f == 3)),
                                r=[t_hT[hi]] + t_w2, w=[tpy])
                        ya = yacc[:, yi, dh * 512:(dh + 1) * 512]
                        gs = k.gates[:, gi, e_:e_ + 1]
                        if e_ == 0:
                            S.op("dve", lambda e, py=py, ya=ya, gs=gs: e.tensor_scalar(out=ya, in0=py[:, :], scalar1=gs, scalar2=None, op0=ALU.mult),
                                 r=[tpy, k.t_gates[gi]], w=[t_y[yi]])
                        else:
                            S.op("dve", lambda e, py=py, ya=ya, gs=gs: e.scalar_tensor_tensor(out=ya, in0=py[:, :], scalar=gs, in1=ya, op0=ALU.mult, op1=ALU.add),
                                 r=[tpy, k.t_gates[gi], t_y[yi]], w=[t_y[yi]])
        for yi, gi in enumerate(tiles):
            yt = yacc[:, yi, :]
            r_ = 1 if gi < 2 else 0
            if l == 0 and yi == 0 and tiles[0] == 0:
                k.dump("ymoe_t0", yt, [128, D], F32, [t_y[yi]])
            S.dma(Hb, k.hmid_d[gi * 128:(gi + 1) * 128, :], r=[k.t_hmid[gi]], w=[t_H])
            S.op("dve", lambda e, yt=yt, r_=r_: e.tensor_tensor(out=yt, in0=yt, in1=k.bc[:, r_, :], op=ALU.mult), r=[t_y[yi], k.t_bc[r_]], w=[t_y[yi]])
            S.op("dve", lambda e, yt=yt: e.scalar_tensor_tensor(out=yt, in0=Hb, scalar=ALPHA, in1=yt, op0=ALU.mult, op1=ALU.add),
                 r=[t_H, t_y[yi]], w=[t_y[yi]])
            st_ap, mv_ap, rs_ap, nb_ap, t_st = k.st_slot()
            k.ln_stats(yt, t_y[yi], st_ap, mv_ap, rs_ap, nb_ap, t_st)
            S.op("act", lambda e, yt=yt, rs_ap=rs_ap, nb_ap=nb_ap: e.activation(out=Hb, in_=yt, func=AF.Identity, scale=rs_ap, bias=nb_ap),
                 r=[t_y[yi], t_st], w=[t_H])
            S.op("dve", lambda e: e.tensor_tensor(out=Hb, in0=Hb, in1=k.bc[:, 2, :], op=ALU.mult), r=[t_H, k.t_bc[2]], w=[t_H])
            S.op("pool", lambda e, yt=yt: e.tensor_tensor(out=yt, in0=Hb, in1=k.bc[:, 3, :], op=ALU.add), r=[t_H, k.t_bc[3]], w=[t_y[yi]])
            if l == 0:
                S.dma(k.hout0_d[gi * 128:(gi + 1) * 128, :], yt, r=[t_y[yi]], w=[k.t_hout0[gi]])
            else:
                S.dma(k.out_d[(gi - 2) * 128:(gi - 1) * 128, :], yt, r=[t_y[yi]], w=[k.t_out[gi - 2]])
    if l == 0:
        k.dump("hout0", k.hout0_d[:, :], [N, D], F32, k.t_hout0)


def mixer_odd(k):
    S = k.S
    scan_setup(k)
    sc = k.scn
    RB, HB, t_rb, t_hb = k.RB, k.HB, k.t_rb, k.t_hb
    col, t_col, row, t_row = sc["col"], k.t_col, sc["row"], k.t_row
    LATB = [(256, 512), (768, 512), (1280, 512), (1792, 512)]
    BC = sc["Am"][:, 0, 0, :]
    BS = sc["Am"][:, 0, 1, :]
    ci = col[:, 0:32].bitcast(I32)
    S.op("pool", lambda e: e.iota(ci[:, 0:1], pattern=[[0, 1]], base=0, channel_multiplier=1), w=[t_col])
    S.op("dve", lambda e: e.tensor_single_scalar(out=ci[:, 1:2], in_=ci[:, 0:1], scalar=63, op=ALU.bitwise_and), r=[t_col], w=[t_col])
    S.op("dve", lambda e: e.tensor_copy(out=col[:, 32:33], in_=ci[:, 1:2]), r=[t_col], w=[t_col])
    S.op("pool", lambda e: e.iota(ci[:, 2:18], pattern=[[128, 16]], base=0, channel_multiplier=1), r=[t_col], w=[t_col])
    S.op("dve", lambda e: e.tensor_copy(out=col[:, 40:56], in_=ci[:, 2:18]), r=[t_col], w=[t_col])
    qi = RB[:, 0, 0:128].bitcast(I32)
    qf = RB[:, 0, 128:256]
    ki = RB[:, 0, 256:384].bitcast(I32)
    kci = RB[:, 0, 384:512].bitcast(I32)
    tq = t_rb[0]
    S.op("pool", lambda e: e.iota(qi, pattern=[[1, 128]], base=0, channel_multiplier=0), w=[tq])
    S.op("dve", lambda e: e.tensor_single_scalar(out=qi, in_=qi, scalar=63, op=ALU.bitwise_and), r=[tq], w=[tq])
    S.op("dve", lambda e: e.tensor_copy(out=qf, in_=qi), r=[tq], w=[tq])
    S.op("dve", lambda e: e.tensor_scalar(out=ki, in0=qf, scalar1=col[:, 32:33], scalar2=None, op0=ALU.mult), r=[tq, t_col], w=[tq])
    S.op("dve", lambda e: e.tensor_single_scalar(out=ki, in_=ki, scalar=63, op=ALU.bitwise_and), r=[tq], w=[tq])
    S.op("dve", lambda e: e.tensor_scalar(out=kci, in0=ki, scalar1=16, scalar2=None, op0=ALU.add), r=[tq], w=[tq])
    S.op("dve", lambda e: e.tensor_single_scalar(out=kci, in_=kci, scalar=63, op=ALU.bitwise_and), r=[tq], w=[tq])
    S.op("act", lambda e: e.activation(out=BS, in_=ki, func=AF.Sin, scale=-2 * PI / 64, bias=k.cst[:, 4:5]), r=[tq, k.t_c], w=[k.t_Am[1]])
    S.op("act", lambda e: e.activation(out=BC, in_=kci, func=AF.Sin, scale=-2 * PI / 64, bias=k.cst[:, 4:5]), r=[tq, k.t_c], w=[k.t_Am[0]])
    for M_, tM in ((BC, k.t_Am[0]), (BS, k.t_Am[1])):
        S.op("pool", lambda e, M_=M_: e.memset(M_[0:64, 64:128], 0.0), r=[tM], w=[tM])
        S.op("pool", lambda e, M_=M_: e.memset(M_[64:128, 0:64], 0.0), r=[tM], w=[tM])
    if k.sub == "M1a":
        k.dump("BCS", sc["Am"][:, 0, :, :], [128, 2, 128], BF16, k.t_Am)
        return
    wz, t_wz = k.load_w(k.w1_d[:, 1536:1792].rearrange("(k p) n -> p k n", p=128))
    zT = HB[:, 2:4, :]
    zc = HB[:, 4:6, :].rearrange("p a n -> p (a n)")[:, 0:4096].rearrange("p (i c) -> p i c", c=256)
    zs = HB[:, 6:8, :].rearrange("p a n -> p (a n)")[:, 0:4096].rearrange("p (i c) -> p i c", c=256)
    for m in range(2):
        for (t0, nt) in LATB:
            pb, tb = proj_block(k, wz, t_wz, m * 128, 128, t0, nt)
            S.op("act", lambda e, pb=pb, m=m, t0=t0, nt=nt: e.activation(out=zT[:, m, t0:t0 + nt], in_=pb[:, 0:nt], func=AF.Copy), r=[tb], w=[t_hb[2 + m]])
    if k.sub == "M1z":
        k.dump("zT", HB[:, 2:4, :], [128, 2, N], BF16, [t_hb[2], t_hb[3]])
        return
    for a in range(16):
        tok = (a + 2) * 128
        pb, tb = k.bank()
        for m in range(2):
            S.op("pe", lambda e, pb=pb, m=m, tok=tok: e.matmul(pb[:, m * 128:(m + 1) * 128], lhsT=zT[:, m, tok:tok + 128], rhs=BC[:, :], start=True, stop=True),
                 r=[t_hb[2 + m], k.t_Am[0]], w=[tb])
            S.op("pe", lambda e, pb=pb, m=m, tok=tok: e.matmul(pb[:, 256 + m * 128:256 + (m + 1) * 128], lhsT=zT[:, m, tok:tok + 128], rhs=BS[:, :], start=True, stop=True),
                 r=[t_hb[2 + m], k.t_Am[1]], w=[tb])
        S.op("act", lambda e, pb=pb, a=a: e.activation(out=zc[:, a, :], in_=pb[:, 0:256], func=AF.Copy), r=[tb], w=[t_hb[4], t_hb[5]])
        if k.sub != "M1d":
            S.op("act", lambda e, pb=pb, a=a: e.activation(out=zs[:, a, :], in_=pb[:, 256:512], func=AF.Copy, scale=-1.0), r=[tb], w=[t_hb[6], t_hb[7]])
        if k.sub in ("M1c", "M1d") and a == 0:
            k.dump("zc", HB[:, 4:6, :], [128, 2, N], BF16, [t_hb[4], t_hb[5]])
            return
    S.barrier()
    if k.sub == "M1b":
        k.dump("zc", HB[:, 4:6, :], [128, 2, N], BF16, [t_hb[4], t_hb[5]])
        return
    fidx = RB[:, 0, 0:2048]
    fi_i = RB[:, 1, 0:2048].bitcast(I32)
    S.op("pool", lambda e: e.iota(fi_i, pattern=[[1, 2048]], base=0, channel_multiplier=0), w=[t_rb[1]])
    S.op("dve", lambda e: e.tensor_copy(out=fidx, in_=fi_i), r=[t_rb[1]], w=[t_rb[0]])
    tabs = []
    for q in range(2):
        rowb = RB[:, 2 + q, :].bitcast(BF16)
        tabs.append((rowb[:, 0:2048], rowb[:, 2048:4096], t_rb[2 + q]))
    kib = [RB[:, 4, 0:2048].bitcast(I32), RB[:, 5, 0:2048].bitcast(I32)]
    banks = [(k.PS[i // 2][:, (i % 2) * 512:(i % 2 + 1) * 512], k.PT[i // 2][i % 2]) for i in range(8)]
    for a in range(16):
        Cb, Sb, t_tab = tabs[a % 2]
        S.op("dve", lambda e, a=a: e.tensor_scalar(out=kib[0], in0=fidx, scalar1=col[:, 40 + a:41 + a], scalar2=None, op0=ALU.mult),
             r=[t_rb[0], t_col], w=[t_rb[4]])
        S.op("dve", lambda e: e.tensor_single_scalar(out=kib[0], in_=kib[0], scalar=2047, op=ALU.bitwise_and), r=[t_rb[4]], w=[t_rb[4]])
        S.op("dve", lambda e: e.tensor_scalar(out=kib[1], in0=kib[0], scalar1=512, scalar2=None, op0=ALU.add), r=[t_rb[4]], w=[t_rb[5]])
        S.op("dve", lambda e: e.tensor_single_scalar(out=kib[1], in_=kib[1], scalar=2047, op=ALU.bitwise_and), r=[t_rb[5]], w=[t_rb[5]])
        S.op("act", lambda e, Sb=Sb: e.activation(out=Sb, in_=kib[0], func=AF.Sin, scale=-2 * PI / 2048, bias=k.cst[:, 4:5]), r=[t_rb[4], k.t_c], w=[t_tab])
        S.op("act", lambda e, Cb=Cb: e.activation(out=Cb, in_=kib[1], func=AF.Sin, scale=-2 * PI / 2048, bias=k.cst[:, 4:5]), r=[t_rb[5], k.t_c], w=[t_tab])
        for m in range(2):
            for fb in range(4):
                pbk, tbk = banks[m * 4 + fb]
                S.op("pe", lambda e, pbk=pbk, a=a, m=m, fb=fb, Cb=Cb: e.matmul(pbk[:, :], lhsT=zc[:, a, m * 128:(m + 1) * 128], rhs=Cb[:, fb * 512:(fb + 1) * 512],
                                                                            start=(a == 0), stop=False), r=[t_hb[4], t_hb[5], t_tab], w=[tbk])
                S.op("pe", lambda e, pbk=pbk, a=a, m=m, fb=fb, Sb=Sb: e.matmul(pbk[:, :], lhsT=zs[:, a, m * 128:(m + 1) * 128], rhs=Sb[:, fb * 512:(fb + 1) * 512],
                                                                            start=False, stop=(a == 15)), r=[t_hb[6], t_hb[7], t_tab], w=[tbk])
    fsc = 1.0 / math.sqrt(2048.0 * 64.0)
    for m in range(2):
        for fb in range(4):
            pbk, tbk = banks[m * 4 + fb]
            S.op("act", lambda e, pbk=pbk, m=m, fb=fb: e.activation(out=HB[:, m, fb * 512:(fb + 1) * 512], in_=pbk[:, :], func=AF.Copy, scale=fsc), r=[tbk], w=[t_hb[m]])
        S.dma(k.mix_d[8 + m, :, 256:2304], HB[:, m, 0:2048], r=[t_hb[m]], w=[k.t_mixd[8 + m]])
    k.dump("mixd_f", k.mix_d[8:10, :, :], [2, 128, N], BF16, k.t_mixd[8:10])
    S.barrier()
    if k.sub == "M1f":
        return

    S.op("pool", lambda e: e.memset(k.rmask[:], 1.0), w=[k.t_c])
    S.op("pool", lambda e: e.memset(k.rmask[:].rearrange("p (c l) -> p c l", l=128)[:, :, 0:1], 0.0), r=[k.t_c], w=[k.t_c])
    S.op("pool", lambda e: e.memset(k.Mf[:], 1.0), w=[k.t_c])
    S.op("pool", lambda e: e.affine_select(out=k.Mf[:], in_=k.Mf[:], pattern=[[1, 128]], compare_op=ALU.is_ge, fill=0.0,
                                           base=0, channel_multiplier=-1), r=[k.t_c], w=[k.t_c])
    S.op("pool", lambda e: e.memset(k.Mb[:], 1.0), w=[k.t_c])
    S.op("pool", lambda e: e.affine_select(out=k.Mb[:], in_=k.Mb[:], pattern=[[-1, 128]], compare_op=ALU.is_ge, fill=0.0,
                                           base=0, channel_multiplier=1), r=[k.t_c], w=[k.t_c])
    gw = k.bc[0:16, 0, 0:768].rearrange("p (r n) -> p r n", n=384)
    t_gw = k.t_bc[0]
    S.dma(gw[:, :, :], k.gw_d.rearrange("r k n -> k r n"), w=[t_gw])
    S.dma(row[0:2, 0:384], k.gb_d[:, :], w=[t_row])
    for h in range(4):
        row_to_col(k, row[0:2, h * 96:(h + 1) * 96], 2, 96, col[0:96, 2 * h:2 * h + 2], t_row, t_col)
    S.op("dve", lambda e: e.tensor_scalar(out=col[0:96, 0:8], in0=col[0:96, 0:8], scalar1=-1.0, scalar2=None, op0=ALU.mult), r=[t_col], w=[t_col])
    S.dma(row[0:1, 0:768], k.gn_d[:, :], r=[t_col], w=[t_row])
    for c in range(8):
        row_to_col(k, row[0:1, c * 96:(c + 1) * 96], 1, 96, col[0:96, 8 + c:9 + c], t_row, t_col)
    qrow, krow, A, B = RB[0:96, 0, :], RB[0:96, 1, :], RB[0:96, 2, :], RB[0:96, 3, :]
    oacc = [RB[0:96, 4, :], RB[0:96, 5, :]]
    qt, kt = HB[0:96, 0, :], HB[0:96, 1, :]
    qts, kts = [HB[0:96, 0, :], HB[0:96, 6, :]], [HB[0:96, 1, :], HB[0:96, 7, :]]
    gsil = [HB[0:96, 2, :], HB[0:96, 3, :]]
    vt = HB[:, 4:6, :].rearrange("p a n -> p (a n)")[:, 0:3456].rearrange("p (i c) -> p i c", c=192)
    for h in range(4):
        base = 384 * h
        wg, t_wg = k.load_w(k.w1_d[:, base:base + 384].rearrange("(k p) n -> p k n", p=128))
        wv, t_wvv = k.load_w(k.w1_d[:, 1824 + 192 * h:1824 + 192 * (h + 1)].rearrange("(k p) n -> p k n", p=128))
        wR, t_wR = k.load_w(k.w1_d[:, 1792:1824].rearrange("(k p) n -> p k n", p=128))

        def ev_q(pb, tb, t0, nt):
            S.op("act", lambda e: e.activation(out=qrow[:, t0:t0 + nt], in_=pb[0:96, 0:nt], func=AF.Copy, scale=96.0 ** -0.5), r=[tb], w=[t_rb[0]])
        k.proj_fm(wg, t_wg, 0, 96, ev_q)

        def ev_k(pb, tb, t0, nt):
            S.op("act", lambda e: e.activation(out=krow[:, t0:t0 + nt], in_=pb[0:96, 0:nt], func=AF.Copy), r=[tb], w=[t_rb[1]])
        k.proj_fm(wg, t_wg, 96, 96, ev_k)
        for a in range(2):
            def ev_g(pb, tb, t0, nt, a=a):
                S.op("act", lambda e: e.activation(out=B[:, t0:t0 + nt], in_=pb[0:96, 0:nt], func=AF.Sigmoid), r=[tb], w=[t_rb[3]])
                S.op("dve", lambda e: e.tensor_tensor(out=gsil[a][:, t0:t0 + nt], in0=pb[0:96, 0:nt], in1=B[:, t0:t0 + nt], op=ALU.mult),
                     r=[tb, t_rb[3]], w=[t_hb[2 + a]])
            k.proj_fm(wg, t_wg, 192 + 96 * a, 96, ev_g)

        def ev_v(pb, tb, i):
            S.op("act", lambda e: e.activation(out=vt[:, i, :], in_=pb[:, 0:192], func=AF.Copy), r=[tb], w=[t_hb[4], t_hb[5]])
        k.proj_tm(wv, t_wvv, 0, 192, ev_v)

        def make_logf(d, h=h, wR=wR, t_wR=t_wR):
            for (t0, nt) in TOKB:
                pr, tr = proj_block(k, wR, t_wR, d * 16, 16, t0, nt)
                S.op("act", lambda e, pr=pr, t0=t0, nt=nt: e.activation(out=B[0:16, t0:t0 + nt], in_=pr[0:16, 0:nt], func=AF.Copy), r=[tr], w=[t_rb[3]])
                pz, tz = k.bank()
                S.op("pe", lambda e, pz=pz, t0=t0, nt=nt: e.matmul(pz[0:96, 0:nt], lhsT=gw[0:16, d, h * 96:(h + 1) * 96], rhs=B[0:16, t0:t0 + nt],
                                                                   start=True, stop=True), r=[t_gw, t_rb[3]], w=[tz])
                S.op("act", lambda e, pz=pz, t0=t0, nt=nt: e.activation(out=A[:, t0:t0 + nt], in_=pz[0:96, 0:nt], func=AF.Exp, scale=-1.0,
                                                                        bias=col[0:96, 2 * h + d:2 * h + d + 1]), r=[tz, t_col], w=[t_rb[2]])
            S.op("act", lambda e: e.activation(out=A, in_=A, func=AF.Ln, bias=k.cst[0:96, 1:2]), r=[t_rb[2], k.t_c], w=[t_rb[2]])
            S.op("dve", lambda e: e.tensor_scalar(out=A, in0=A, scalar1=-1.0 / 16.0, scalar2=None, op0=ALU.mult), r=[t_rb[2]], w=[t_rb[2]])
        gated_scan(k, 96, 96, 2, qrow, t_rb[0], krow, t_rb[1], A, t_rb[2], B, t_rb[3], make_logf,
                   lambda i: vt[:, i, :], [t_hb[4], t_hb[5]], oacc, [t_rb[4], t_rb[5]], qts, [t_hb[0], t_hb[6]], kts, [t_hb[1], t_hb[7]], L=128)
        if h == 0:
            k.dump("gla_o0", RB[0:96, 4:6, :], [96, 2, N], F32, [t_rb[4], t_rb[5]])
        rms_gate_out(k, 96, 2, oacc, [t_rb[4], t_rb[5]], A, t_rb[2], B, t_rb[3], gsil, [t_hb[2], t_hb[3]],
                     [col[0:96, 8 + 2 * h:9 + 2 * h], col[0:96, 9 + 2 * h:10 + 2 * h]], t_col, [qt, kt], [t_hb[0], t_hb[1]], [2 * h, 2 * h + 1])
    k.dump("mixd1", k.mix_d[0:10, :, :], [10, 128, N], BF16, k.t_mixd[0:10])


_NC_CACHE = {}


def _f32(a):
    return np.ascontiguousarray(np.asarray(a, dtype=np.float32))


def kernel(x, c, ctx, c_ctx, w_ada, b_ada, ln_g, ln_b, w_in_even, attn_sink, hgrn_lb_logits, hgrn_norm,
           w_out_even, w_in_odd, gla_gate_w, gla_gate_b, gla_norm, w_out_odd, w_router, b_router,
           w_expert_gate, w_expert_up, w_expert_down):
    x = _f32(x); c = _f32(c); ctx = _f32(ctx); c_ctx = _f32(c_ctx)
    w0a = np.ascontiguousarray(_f32(w_in_even)[0][:, _cols0()])
    w1a = np.ascontiguousarray(_f32(w_in_odd)[0][:, _cols1()])
    shared = {
        "w_ada": _f32(w_ada), "b_ada": _f32(b_ada), "ln_g": _f32(ln_g), "ln_b": _f32(ln_b),
        "w0a": w0a, "attn_sink": _f32(attn_sink), "lb_logits": _f32(hgrn_lb_logits), "hgrn_norm": _f32(hgrn_norm),
        "w_out_even": _f32(w_out_even)[0], "w1a": w1a, "gla_gate_w": _f32(gla_gate_w)[0], "gla_gate_b": _f32(gla_gate_b)[0],
        "gla_norm": _f32(gla_norm), "w_out_odd": _f32(w_out_odd)[0], "w_router": _f32(w_router),
        "b_router": _f32(b_router)[None, :], "w_expert_gate": _f32(w_expert_gate), "w_expert_up": _f32(w_expert_up),
        "w_expert_down": _f32(w_expert_down),
    }
    nb = x.shape[0]
    in_maps = []
    for b in range(nb):
        m = dict(shared)
        m["x"] = np.ascontiguousarray(x[b])
        m["ctx"] = np.ascontiguousarray(ctx[b])
        m["cvec"] = np.ascontiguousarray(np.stack([c[b], c_ctx], 0))
        in_maps.append(m)
    if "nc" not in _NC_CACHE:
        _NC_CACHE["nc"] = build()
    res = run_bass_kernel_spmd(_NC_CACHE["nc"], in_maps, core_ids=list(range(nb)))
    return np.stack([np.asarray(r["out"], dtype=np.float32) for r in res.results], 0)
```

```python
import contextlib
import math
import numpy as np
import concourse.bass as bass
import concourse.mybir as mybir
from concourse.bass_utils import run_bass_kernel_spmd

F32 = mybir.dt.float32
BF16 = mybir.dt.bfloat16
I32 = mybir.dt.int32
AF = mybir.ActivationFunctionType
ALU = mybir.AluOpType
AX = mybir.AxisListType

ENG = ("pe", "act", "dve", "pool", "sp")


class Trk:
    __slots__ = ("w", "rs", "dsem", "dcnt", "name")

    def __init__(self, name=""):
        self.w = None
        self.rs = []
        self.dsem = None
        self.dcnt = 0
        self.name = name


class Sched:
    SEM_CHUNK = 20000

    def __init__(self, nc):
        self.nc = nc
        self.ops = {e: [] for e in ENG}
        self.waited = {e: {} for e in ENG}
        self.stack = contextlib.ExitStack()
        self.nsem = 0
        self.dma_ev = {}

    def sbuf(self, name, shape, dt):
        return self.stack.enter_context(self.nc.sbuf_tensor(name, list(shape), dt))

    def psum(self, name, shape, dt=F32):
        return self.stack.enter_context(self.nc.psum_tensor(name, list(shape), dt))

    def new_sem(self, name):
        self.nsem += 1
        return self.stack.enter_context(self.nc.semaphore(f"{name}_{self.nsem}"))

    def _filter(self, engine, deps):
        waits = []
        wd = self.waited[engine]
        for ev in deps:
            if ev[0] == "e":
                _, f, idx = ev
                if engine == "pe" and f == "pe":
                    continue
                if idx <= wd.get(f, -1):
                    continue
                wd[f] = idx
                self.ops[f][idx][2] = True
                waits.append(ev)
            else:
                _, sem, val = ev
                k = id(sem)
                if val <= wd.get(k, 0):
                    continue
                wd[k] = val
                waits.append(ev)
        return waits

    def _deps(self, engine, r, w):
        deps = []
        for t in r:
            if t.w is not None:
                deps.append(t.w)
        for t in w:
            if t.w is not None:
                deps.append(t.w)
            deps.extend(t.rs)
        return self._filter(engine, deps)

    def _post(self, ev, r, w):
        for t in w:
            t.w = ev
            t.rs = []
        for t in r:
            if t in w:
                continue
            if ev[0] == "e":
                t.rs = [x for x in t.rs if not (x[0] == "e" and x[1] == ev[1])]
            else:
                t.rs = [x for x in t.rs if not (x[0] == "d" and x[1] is ev[1])]
            t.rs.append(ev)

    def op(self, engine, fn, r=(), w=()):
        r = list(r)
        w = list(w)
        waits = self._deps(engine, r, w)
        idx = len(self.ops[engine])
        self.ops[engine].append([fn, waits, False, None])
        self._post(("e", engine, idx), r, w)

    def dma(self, out, in_, r=(), w=(), q="sp", **kw):
        r = list(r)
        w = list(w)
        waits = self._deps(q, r, w)
        t0 = w[0]
        if t0.dsem is None or t0.dcnt > 60000:
            t0.dsem = self.new_sem("d")
            t0.dcnt = 0
        t0.dcnt += 16
        ev = ("d", t0.dsem, t0.dcnt)
        self.dma_ev[id(t0.dsem)] = ev

        def fn(eng, out=out, in_=in_, kw=kw):
            return eng.dma_start(out=out, in_=in_, **kw)
        self.ops[q].append([fn, waits, False, t0.dsem])
        self._post(ev, r, w)

    def barrier(self):
        last = {}
        for f in ("pe", "act", "dve", "pool"):
            j = len(self.ops[f]) - 1
            while j >= 0 and (self.ops[f][j][0] is None or self.ops[f][j][3] is not None):
                j -= 1
            last[f] = j
        dm = list(self.dma_ev.values())
        for e in ENG:
            deps = [("e", f, last[f]) for f in ("pe", "act", "dve", "pool") if f != e and last[f] >= 0]
            deps += dm
            waits = self._filter(e, deps)
            self.ops[e].append([None, waits, False, None])

    def wait_all(self, engine, trks):
        waits = self._deps(engine, list(trks), [])
        self.ops[engine].append([None, waits, False, None])

    def emit(self):
        nc = self.nc
        cum = {}
        sems = {}
        for e in ENG:
            c = 0
            arr = []
            for rec in self.ops[e]:
                if rec[2]:
                    c += 1
                arr.append(c)
            cum[e] = arr
            sems[e] = [self.new_sem(f"s{e}") for _ in range(c // self.SEM_CHUNK + 1)]
        CH = self.SEM_CHUNK

        def semval(f, idx):
            c = cum[f][idx]
            ch = (c - 1) // CH
            return sems[f][ch], c - ch * CH

        def run(e, eng):
            for i, (fn, waits, sig, dsem) in enumerate(self.ops[e]):
                for ev in waits:
                    if ev[0] == "e":
                        s, v = semval(ev[1], ev[2])
                        eng.wait_ge(s, v)
                    else:
                        eng.wait_ge(ev[1], ev[2])
                if fn is None:
                    continue
                ins = fn(eng)
                if dsem is not None:
                    ins.then_inc(dsem, 16)
                elif sig:
                    s, v = semval(e, i)
                    ins.then_inc(s, 1)

        with nc.Block() as block:
            @block.tensor
            def _(eng):
                run("pe", eng)

            @block.scalar
            def _(eng):
                run("act", eng)

            @block.vector
            def _(eng):
                run("dve", eng)

            @block.gpsimd
            def _(eng):
                run("pool", eng)

            @block.sync
            def _(eng):
                run("sp", eng)

    def close(self):
        self.stack.close()


N = 2304
NT = 18
D = 1024
KC = 8
NLAT = 2048
ALPHA = 4.0 ** 0.25
EPS = 1e-5
TOKB = [(0, 512), (512, 512), (1024, 512), (1536, 512), (2048, 256)]
PI = math.pi

_SW = list(range(16, 32)) + list(range(0, 16)) + list(range(48, 64)) + list(range(32, 48))


def _cols0():
    cols = []
    for j in range(2):
        for blk in range(2):
            for hh in (4 * j + 2 * blk, 4 * j + 2 * blk + 1):
                cols += [hh * 64 + d for d in range(64)]
        for blk in range(2):
            for hh in (4 * j + 2 * blk, 4 * j + 2 * blk + 1):
                cols += [hh * 64 + d for d in _SW]
        cols += [512 + j * 64 + d for d in range(64)] * 2
        cols += [512 + j * 64 + d for d in _SW] * 2
    cols += list(range(640, 768))
    for h in range(4):
        cols += [768 + h * 128 + d for d in range(128)]
        cols += [1280 + h * 128 + d for d in range(128)]
        cols += [1792 + h * 128 + d for d in range(128)]
        cols += [2816 + h * 128 + d for d in range(128)]
    cols += list(range(2304, 2816))
    return cols


def _cols1():
    cols = []
    for h in range(4):
        cols += [h * 96 + d for d in range(96)]
        cols += [384 + h * 96 + d for d in range(96)]
        cols += [1568 + h * 192 + d for d in range(192)]
    cols += list(range(2336, 2592))
    cols += list(range(1536, 1568))
    cols += list(range(768, 1536))
    return cols


class K:
    pass


def build(dbg=(), stop=None):
    nc = bass.Bass("TRN2", target_bir_lowering=False)
    S = Sched(nc)
    k = K()
    k.nc = nc
    k.S = S
    k.dbg = set(dbg)
    k.sub = stop
    k.dbg_out = []

    def din(name, shape, dt=F32):
        return nc.dram_tensor(name, list(shape), dt, kind="ExternalInput").ap()

    x_d = din("x", [NLAT, D])
    ctx_d = din("ctx", [256, D])
    cv_d = din("cvec", [2, D])
    wada_d = din("w_ada", [2, D, 6 * D])
    bada_d = din("b_ada", [2, 6 * D])
    lng_d = din("ln_g", [2, 2, D])
    lnb_d = din("ln_b", [2, 2, D])
    w0_d = din("w0a", [D, 4224])
    sink_d = din("attn_sink", [1, 8])
    lbl_d = din("lb_logits", [2, 2, 512])
    hn_d = din("hgrn_norm", [1, 512])
    wo0_d = din("w_out_even", [D, D])
    w1_d = din("w1a", [D, 2592])
    gw_d = din("gla_gate_w", [2, 16, 384])
    gb_d = din("gla_gate_b", [2, 384])
    gn_d = din("gla_norm", [1, 768])
    wo1_d = din("w_out_odd", [D, D])
    wr_d = din("w_router", [D, 16])
    br_d = din("b_router", [1, 16])
    eg_d = din("w_expert_gate", [2, 16, D, 512])
    eu_d = din("w_expert_up", [2, 16, D, 512])
    ed_d = din("w_expert_down", [2, 16, 512, D])
    out_d = nc.dram_tensor("out", [NLAT, D], F32, kind="ExternalOutput").ap()
    hmid_d = nc.dram_tensor("hmid", [N, D], F32).ap()
    hout0_d = nc.dram_tensor("hout0", [N, D], F32).ap()
    mix_d = nc.dram_tensor("mixd", [10, 128, N], BF16).ap()
    t_hmid = [Trk() for _ in range(NT)]
    t_hout0 = [Trk() for _ in range(NT)]
    t_mixd = [Trk() for _ in range(10)]
    t_out = [Trk() for _ in range(16)]

    def dump(name, src_ap, shape, dt, r):
        if name not in k.dbg:
            return
        d = nc.dram_tensor("dbg_" + name, list(shape), dt, kind="ExternalOutput").ap()
        t = Trk()
        S.dma(d, src_ap, r=r, w=[t])
        k.dbg_out.append(t)

    PS = [S.psum(f"ps{i}", [128, 1024]) for i in range(4)]
    PT = [[Trk(), Trk()] for _ in range(4)]
    k.bank_i = 0
    k.pair_i = 0

    k.reserved = set()

    def bank():
        i = k.bank_i
        while i in k.reserved:
            i = (i + 1) % 8
        k.bank_i = (i + 1) % 8
        k.last_bank = i
        return PS[i // 2][:, (i % 2) * 512:(i % 2 + 1) * 512], PT[i // 2][i % 2]

    def pair():
        i = k.pair_i
        k.pair_i = (i + 1) % 4
        k.bank_i = (2 * i + 2) % 8
        return PS[i], PT[i]

    identF = S.sbuf("identF", [128, 128], F32); t_c = Trk()
    identB = S.sbuf("identB", [128, 128], BF16)
    onesF = S.sbuf("onesF", [128, 128], F32)
    onesB = S.sbuf("onesB", [128, 128], BF16)
    cst = S.sbuf("cst", [128, 8], F32)
    Mf = S.sbuf("Mf", [128, 128], BF16)
    Mb = S.sbuf("Mb", [128, 128], BF16)
    MP = S.sbuf("MP", [128, 512], BF16)
    MN = S.sbuf("MN", [128, 512], BF16)
    rmask = S.sbuf("rmask", [128, N], BF16)
    modT = S.sbuf("modT", [128, 2, 48, 2], F32); t_modT = Trk()
    mod_d = nc.dram_tensor("mod_d", [2, 2, 6 * D], F32).ap(); t_mod = Trk()
    bc = S.sbuf("bc", [128, 4, D], F32); t_bc = [Trk() for _ in range(4)]
    uT = S.sbuf("uT", [128, KC, N], BF16); t_uT = [Trk() for _ in range(NT)]
    wbuf = S.sbuf("wbuf", [128, 5, 4096], BF16); t_wb = [Trk() for _ in range(5)]
    RB = S.sbuf("RB", [128, 6, N], F32); t_rb = [Trk() for _ in range(6)]
    HB = S.sbuf("HB", [128, 8, N], BF16); t_hb = [Trk() for _ in range(8)]
    sm = S.sbuf("sm", [128, 640], F32)
    gates = S.sbuf("gates", [128, NT, 16], F32); t_gates = [Trk() for _ in range(NT)]
    wrt = S.sbuf("wrt", [128, KC, 16], F32); t_wr = Trk()
    brb = S.sbuf("brb", [128, 16], F32)

    S.op("pool", lambda e: e.memset(identF[:], 0.0), w=[t_c])
    S.op("pool", lambda e: e.affine_select(out=identF[:], in_=identF[:], pattern=[[-1, 128]], compare_op=ALU.not_equal,
                                           fill=1.0, base=0, channel_multiplier=1), r=[t_c], w=[t_c])
    S.op("pool", lambda e: e.tensor_copy(out=identB[:], in_=identF[:]), r=[t_c], w=[t_c])
    S.op("pool", lambda e: e.memset(onesF[:], 1.0), w=[t_c])
    S.op("pool", lambda e: e.memset(onesB[:], 1.0), w=[t_c])
    for j_, v_ in enumerate((EPS, 1.0, -PI, 0.0, PI)):
        S.op("pool", lambda e, j_=j_, v_=v_: e.memset(cst[:, j_:j_ + 1], v_), w=[t_c])
    S.op("pool", lambda e: e.memset(Mf[:], 1.0), w=[t_c])
    S.op("pool", lambda e: e.affine_select(out=Mf[:], in_=Mf[:], pattern=[[1, 128]], compare_op=ALU.is_ge, fill=0.0,
                                           base=0, channel_multiplier=-1), r=[t_c], w=[t_c])
    S.op("pool", lambda e: e.memset(Mf[0:64, 64:128], 0.0), r=[t_c], w=[t_c])
    S.op("pool", lambda e: e.memset(Mb[:], 1.0), w=[t_c])
    S.op("pool", lambda e: e.affine_select(out=Mb[:], in_=Mb[:], pattern=[[-1, 128]], compare_op=ALU.is_ge, fill=0.0,
                                           base=0, channel_multiplier=1), r=[t_c], w=[t_c])
    S.op("pool", lambda e: e.memset(Mb[64:128, 0:64], 0.0), r=[t_c], w=[t_c])
    S.op("pool", lambda e: e.memset(MP[:], 1.0), w=[t_c])
    S.op("pool", lambda e: e.affine_select(out=MP[:], in_=MP[:], pattern=[[0, 4], [-1, 128]], compare_op=ALU.is_ge,
                                           fill=0.0, base=0, channel_multiplier=1), r=[t_c], w=[t_c])
    S.op("pool", lambda e: e.memset(MN[:], 1.0), w=[t_c])
    S.op("pool", lambda e: e.affine_select(out=MN[:], in_=MN[:], pattern=[[0, 4], [1, 128]], compare_op=ALU.is_ge,
                                           fill=0.0, base=0, channel_multiplier=-1), r=[t_c], w=[t_c])
    S.op("pool", lambda e: e.memset(rmask[:], 1.0), w=[t_c])
    S.op("pool", lambda e: e.memset(rmask[:].rearrange("p (c l) -> p c l", l=64)[:, :, 0:1], 0.0), r=[t_c], w=[t_c])
    S.dma(wrt[:], wr_d.rearrange("(k p) n -> p k n", p=128), w=[t_wr])
    S.dma(brb[:], br_d[0:1, :].to_broadcast([128, 16]), w=[t_c])
    S.barrier()

    cs = RB[0:2, 4, 0:1024]
    sg_ = RB[0:2, 4, 1024:2048]
    csT = sm[:, 520:536].rearrange("p (k r) -> p k r", r=2)
    t_cs = Trk(); t_csT = Trk()
    k.t_bb = [[Trk(), Trk()], [Trk(), Trk()]]
    S.dma(cs, cv_d[:, :], w=[t_cs])
    S.op("act", lambda e: e.activation(out=sg_, in_=cs, func=AF.Sigmoid), r=[t_cs], w=[t_csT])
    S.op("dve", lambda e: e.tensor_tensor(out=cs, in0=cs, in1=sg_, op=ALU.mult), r=[t_cs, t_csT], w=[t_cs])
    pb, tb = bank()
    for kk in range(KC):
        S.op("pe", lambda e, kk=kk, pb=pb: e.transpose(out=pb[:, 2 * kk:2 * kk + 2], in_=cs[:, kk * 128:(kk + 1) * 128],
                                                       identity=identF[0:2, 0:2]), r=[t_cs, t_c], w=[tb])
    S.op("dve", lambda e, pb=pb: e.tensor_copy(out=csT, in_=pb[:, 0:16].rearrange("p (k r) -> p k r", r=2)), r=[tb], w=[t_csT])
    t_wa = [Trk(), Trk()]
    t_mb = [Trk(), Trk()]
    for l in range(2):
        pT, tT = bank()
        k.reserved = {k.last_bank}
        for j in range(12):
            slot = (l * 12 + j) % 2
            wa = RB[:, slot * 2:slot * 2 + 2, :].rearrange("p a n -> p (a n)")[:, 0:4096].rearrange("p (k n) -> p k n", n=512)
            mb = RB[0:2, 5, slot * 512:(slot + 1) * 512]
            S.dma(wa, wada_d[l, :, j * 512:(j + 1) * 512].rearrange("(k p) n -> p k n", p=128), w=[t_wa[slot]])
            for r_ in range(2):
                S.dma(RB[r_:r_ + 1, 5, 1024 + slot * 512:1024 + (slot + 1) * 512], bada_d[l:l + 1, j * 512:(j + 1) * 512],
                      w=[k.t_bb[slot][r_]])
            pb, tb = bank()
            for kk in range(KC):
                S.op("pe", lambda e, kk=kk, pb=pb, wa=wa: e.matmul(pb[0:2, :], lhsT=csT[:, kk, :], rhs=wa[:, kk, :],
                                                                   start=(kk == 0), stop=(kk == KC - 1)),
                     r=[t_csT, t_wa[slot]], w=[tb])
            bb = RB[0:2, 5, 1024 + slot * 512:1024 + (slot + 1) * 512]
            S.op("dve", lambda e, pb=pb, mb=mb, bb=bb: e.tensor_tensor(out=mb, in0=pb[0:2, :], in1=bb, op=ALU.add),
                 r=[tb] + k.t_bb[slot], w=[t_mb[slot]])
            if j in (2, 3, 8, 9):
                S.op("dve", lambda e, mb=mb: e.tensor_scalar(out=mb, in0=mb, scalar1=1.0, scalar2=None, op0=ALU.add),
                     r=[t_mb[slot]], w=[t_mb[slot]])
            S.dma(mod_d[l, :, j * 512:(j + 1) * 512], mb, r=[t_mb[slot]], w=[t_mod])
            for q_ in range(4):
                jj = j * 4 + q_
                S.op("pe", lambda e, jj=jj, q_=q_, mb=mb, pT=pT: e.transpose(out=pT[:, 2 * jj:2 * jj + 2], in_=mb[:, q_ * 128:(q_ + 1) * 128],
                                                                             identity=identF[0:2, 0:2]), r=[t_mb[slot], t_c], w=[tT])
        S.op("dve", lambda e, l=l, pT=pT: e.tensor_copy(out=modT[:, l, :, :], in_=pT[:, 0:96].rearrange("p (j r) -> p j r", r=2)),
             r=[tT], w=[t_modT])
        k.reserved = set()
        dump(f"mod{l}", mod_d[l, :, :], [2, 6 * D], F32, [t_mod])
    S.barrier()

    def bcast_rows(l, which):
        gi = 2 if which == "mix" else 5
        li = 0 if which == "mix" else 1
        S.dma(bc[:, 0, :], mod_d[l, 0:1, gi * D:(gi + 1) * D].to_broadcast([128, D]), r=[t_mod], w=[t_bc[0]])
        S.dma(bc[:, 1, :], mod_d[l, 1:2, gi * D:(gi + 1) * D].to_broadcast([128, D]), r=[t_mod], w=[t_bc[1]])
        S.dma(bc[:, 2, :], lng_d[l, li:li + 1, :].to_broadcast([128, D]), w=[t_bc[2]])
        S.dma(bc[:, 3, :], lnb_d[l, li:li + 1, :].to_broadcast([128, D]), w=[t_bc[3]])

    def ln_stats(src, t_src, st_ap, mv_ap, rs_ap, nb_ap, t_st):
        for j in range(2):
            S.op("dve", lambda e, j=j: e.bn_stats(out=st_ap[:, j, :], in_=src[:, j * 512:(j + 1) * 512]), r=[t_src], w=[t_st])
        S.op("dve", lambda e: e.bn_aggr(out=mv_ap, in_=st_ap), r=[t_st], w=[t_st])
        S.op("act", lambda e: e.activation(out=rs_ap, in_=mv_ap[:, 1:2], func=AF.Sqrt, bias=cst[:, 0:1]), r=[t_st, t_c], w=[t_st])
        S.op("dve", lambda e: e.reciprocal(out=rs_ap, in_=rs_ap), r=[t_st], w=[t_st])
        S.op("dve", lambda e: e.tensor_scalar(out=nb_ap, in0=mv_ap[:, 0:1], scalar1=rs_ap, scalar2=-1.0, op0=ALU.mult, op1=ALU.mult),
             r=[t_st], w=[t_st])

    k.st_i = 0

    def st_slot():
        i = k.st_i
        k.st_i = (i + 1) % 8
        base = i * 24
        return (sm[:, base:base + 12].rearrange("p (a b) -> p a b", b=6), sm[:, base + 12:base + 14],
                sm[:, base + 14:base + 15], sm[:, base + 15:base + 16], k.t_st[i])
    k.t_st = [Trk() for _ in range(8)]

    def ln_part1(src, t_src, xn, t_xn):
        st_ap, mv_ap, rs_ap, nb_ap, t_st = st_slot()
        ln_stats(src, t_src, st_ap, mv_ap, rs_ap, nb_ap, t_st)
        S.op("act", lambda e: e.activation(out=xn, in_=src, func=AF.Identity, scale=rs_ap, bias=nb_ap), r=[t_src, t_st], w=[t_xn])

    def ln_part2(l, xn, t_xn, i, which, router, uf=None, t_uf=None):
        r_ = 1 if i < 2 else 0
        pp, tp = pair()
        for kk in range(KC):
            S.op("pe", lambda e, kk=kk: e.transpose(out=pp[:, kk * 128:(kk + 1) * 128], in_=xn[:, kk * 128:(kk + 1) * 128],
                                                    identity=identF[:]), r=[t_xn, t_c], w=[tp[kk // 4]])
        for kk in range(KC):
            sc_ap = modT[:, l, (which + 1) * 8 + kk, r_:r_ + 1]
            sh_ap = modT[:, l, which * 8 + kk, r_:r_ + 1]
            if router:
                o_ap = uf[:, kk, :]
                tw = t_uf
            else:
                o_ap = uT[:, kk, i * 128:(i + 1) * 128]
                tw = t_uT[i]
            if kk % 2 == 0:
                S.op("act", lambda e, kk=kk, o_ap=o_ap, sc_ap=sc_ap, sh_ap=sh_ap: e.activation(
                    out=o_ap, in_=pp[:, kk * 128:(kk + 1) * 128], func=AF.Identity, scale=sc_ap, bias=sh_ap),
                    r=[tp[kk // 4], t_modT], w=[tw])
            else:
                S.op("dve", lambda e, kk=kk, o_ap=o_ap, sc_ap=sc_ap, sh_ap=sh_ap: e.tensor_scalar(
                    out=o_ap, in0=pp[:, kk * 128:(kk + 1) * 128], scalar1=sc_ap, scalar2=sh_ap, op0=ALU.mult, op1=ALU.add),
                    r=[tp[kk // 4], t_modT], w=[tw])
        if router:
            S.op("pool", lambda e: e.tensor_copy(out=uT[:, :, i * 128:(i + 1) * 128], in_=uf[:, :, :]), r=[t_uf], w=[t_uT[i]])

    def ln_to_uT(l, src, t_src, xn, t_xn, i, which, router, uf=None, t_uf=None):
        ln_part1(src, t_src, xn, t_xn)
        ln_part2(l, xn, t_xn, i, which, router, uf, t_uf)
        if router:
            route(i, uf, t_uf)

    def route(i, uf, t_uf):
        pb, tb = bank()
        for kk in range(KC):
            S.op("pe", lambda e, kk=kk: e.matmul(pb[:, 0:16], lhsT=uf[:, kk, :], rhs=wrt[:, kk, :], start=(kk == 0),
                                                 stop=(kk == KC - 1)), r=[t_uf, t_wr], w=[tb])
        S.op("act", lambda e: e.activation(out=gates[:, i, :], in_=pb[:, 0:16], func=AF.Copy), r=[tb], w=[t_gates[i]])

    def route_all(t0, nt, scr, t_scr):
        n16 = nt * 16
        o = [0]

        def take(n):
            a = scr[:, o[0]:o[0] + n]
            o[0] += n
            return a
        lg = gates[:, t0:t0 + nt, :]
        pr, sl, s2, eq, eq2 = [take(n16).rearrange("p (t e) -> p t e", e=16) for _ in range(5)]
        g4, g4b, g4c = [take(nt * 4).rearrange("p (t g) -> p t g", g=4) for _ in range(3)]
        sc1, sc2 = take(nt), take(nt)
        BIG = 1.0e4
        tg = t_gates[t0:t0 + nt]

        def dv(fn):
            S.op("dve", fn, r=t_scr + [k.t_c] + tg, w=t_scr)

        def bc16(a):
            return a.unsqueeze(2).to_broadcast([128, nt, 16])

        def g4v(a):
            return a.rearrange("p t (g e) -> p (t g) e", e=4)

        def g4f(a):
            return a.rearrange("p t g -> p (t g)")
        dv(lambda e: e.tensor_reduce(out=sc1, in_=lg, axis=AX.X, op=ALU.max))
        dv(lambda e: e.tensor_tensor(out=pr, in0=lg, in1=bc16(sc1), op=ALU.subtract))
        S.op("act", lambda e: e.activation(out=pr, in_=pr, func=AF.Exp), r=t_scr, w=t_scr)
        dv(lambda e: e.tensor_reduce(out=sc2, in_=pr, axis=AX.X, op=ALU.add))
        dv(lambda e: e.reciprocal(out=sc2, in_=sc2))
        dv(lambda e: e.tensor_tensor(out=pr, in0=pr, in1=bc16(sc2), op=ALU.mult))
        dv(lambda e: e.tensor_tensor(out=sl, in0=pr, in1=brb[:].unsqueeze(1).to_broadcast([128, nt, 16]), op=ALU.add))
        dv(lambda e: e.tensor_reduce(out=g4f(g4), in_=g4v(sl), axis=AX.X, op=ALU.max))
        dv(lambda e: e.tensor_tensor(out=g4v(eq), in0=g4v(sl), in1=g4f(g4).unsqueeze(2).to_broadcast([128, nt * 4, 4]), op=ALU.is_equal))
        dv(lambda e: e.scalar_tensor_tensor(out=s2.rearrange("p t e -> p (t e)"), in0=eq.rearrange("p t e -> p (t e)"), scalar=-BIG,
                                            in1=sl.rearrange("p t e -> p (t e)"), op0=ALU.mult, op1=ALU.add))
        dv(lambda e: e.tensor_reduce(out=g4f(g4b), in_=g4v(s2), axis=AX.X, op=ALU.max))
        dv(lambda e: e.tensor_tensor(out=g4f(g4), in0=g4f(g4), in1=g4f(g4b), op=ALU.add))
        dv(lambda e: e.tensor_reduce(out=sc1, in_=g4, axis=AX.X, op=ALU.max))
        dv(lambda e: e.tensor_tensor(out=g4c, in0=g4, in1=sc1.unsqueeze(2).to_broadcast([128, nt, 4]), op=ALU.is_equal))
        dv(lambda e: e.tensor_scalar(out=g4f(g4c), in0=g4f(g4c), scalar1=-1.0, scalar2=BIG, op0=ALU.add, op1=ALU.mult))
        dv(lambda e: e.tensor_tensor(out=g4v(s2), in0=g4v(sl), in1=g4f(g4c).unsqueeze(2).to_broadcast([128, nt * 4, 4]), op=ALU.add))
        dv(lambda e: e.tensor_reduce(out=sc1, in_=s2, axis=AX.X, op=ALU.max))
        dv(lambda e: e.tensor_tensor(out=eq, in0=s2, in1=bc16(sc1), op=ALU.is_equal))
        dv(lambda e: e.scalar_tensor_tensor(out=s2.rearrange("p t e -> p (t e)"), in0=eq.rearrange("p t e -> p (t e)"), scalar=-BIG,
                                            in1=s2.rearrange("p t e -> p (t e)"), op0=ALU.mult, op1=ALU.add))
        dv(lambda e: e.tensor_reduce(out=sc1, in_=s2, axis=AX.X, op=ALU.max))
        dv(lambda e: e.tensor_tensor(out=eq2, in0=s2, in1=bc16(sc1), op=ALU.is_equal))
        dv(lambda e: e.tensor_tensor(out=eq, in0=eq, in1=eq2, op=ALU.add))
        dv(lambda e: e.tensor_tensor(out=eq, in0=eq, in1=pr, op=ALU.mult))
        dv(lambda e: e.tensor_reduce(out=sc2, in_=eq, axis=AX.X, op=ALU.add))
        dv(lambda e: e.reciprocal(out=sc2, in_=sc2))
        S.op("dve", lambda e: e.tensor_tensor(out=lg, in0=eq, in1=bc16(sc2), op=ALU.mult), r=t_scr, w=tg)
    k.route_all = route_all
    k.t_route = [Trk(), Trk()]
    k.t_tile = [Trk() for _ in range(12)]

    k.wslot = 0

    def load_w(src_ap, nslots=1, parts=128):
        s0 = k.wslot
        if s0 + nslots > 5:
            s0 = 0
        k.wslot = (s0 + nslots) % 5
        a, b = src_ap.shape[1], src_ap.shape[2]
        dst = wbuf[0:parts, s0:s0 + nslots, :].rearrange("p s n -> p (s n)")[:, 0:a * b].rearrange("p (a b) -> p a b", b=b)
        trks = t_wb[s0:s0 + nslots]
        S.dma(dst, src_ap, w=trks, q="pool")
        return dst, trks

    def proj_fm(wv, t_w, c0, M, evac, toks=TOKB):
        for (t0, nt) in toks:
            pb, tb = bank()
            for kk in range(KC):
                S.op("pe", lambda e, kk=kk, pb=pb, t0=t0, nt=nt: e.matmul(pb[0:M, 0:nt], lhsT=wv[:, kk, c0:c0 + M],
                                                                          rhs=uT[:, kk, t0:t0 + nt], start=(kk == 0), stop=(kk == KC - 1)),
                     r=t_w + t_uT[t0 // 128:(t0 + nt) // 128], w=[tb])
            evac(pb, tb, t0, nt)

    def proj_tm(wv, t_w, c0, ncol, evac, tiles=range(NT)):
        for i in tiles:
            pb, tb = bank()
            for kk in range(KC):
                S.op("pe", lambda e, kk=kk, pb=pb, i=i: e.matmul(pb[:, 0:ncol], lhsT=uT[:, kk, i * 128:(i + 1) * 128],
                                                                 rhs=wv[:, kk, c0:c0 + ncol], start=(kk == 0), stop=(kk == KC - 1)),
                     r=t_w + [t_uT[i]], w=[tb])
            evac(pb, tb, i)

    k.proj_fm = proj_fm
    k.proj_tm = proj_tm
    k.load_w = load_w
    k.bank = bank
    k.pair = pair
    k.dump = dump
    k.ln_to_uT = ln_to_uT
    k.ln_part1 = ln_part1
    k.ln_part2 = ln_part2
    k.route = route
    k.ln_stats = ln_stats
    k.st_slot = st_slot
    k.bcast_rows = bcast_rows
    for nm in ("x_d ctx_d w0_d sink_d lbl_d hn_d wo0_d w1_d gw_d gb_d gn_d wo1_d eg_d eu_d ed_d out_d hmid_d hout0_d mix_d "
               "t_hmid t_hout0 t_mixd t_out identF identB onesF onesB cst Mf Mb MP MN rmask modT t_modT mod_d t_mod bc t_bc uT t_uT "
               "wbuf t_wb RB t_rb HB t_hb sm gates t_gates t_c PS PT").split():
        setattr(k, nm, locals()[nm])

    for ph, l in [("B", 0), ("M", 0), ("D", 0), ("E", 0), ("B", 1), ("M", 1), ("D", 1), ("E", 1)]:
        if ph == "B":
            phase_B(k, l)
        elif ph == "M":
            (mixer_even if l == 0 else mixer_odd)(k)
        elif ph == "D":
            phase_D(k, l)
        else:
            phase_E(k, l)
        S.barrier()
        if stop is not None and stop[0:2] == f"{ph}{l}":
            break

    S.wait_all("sp", t_out + k.dbg_out)
    S.emit()
    S.close()
    return nc


def phase_B(k, l):
    S = k.S
    bufs = {}

    def stA(i):
        slot = i % 2
        ht = k.RB[:, slot, 0:1024]
        t_ht = k.t_tile[slot]
        xn = k.RB[:, 2 + slot, 0:1024]
        t_xn = k.t_tile[2 + slot]
        if l == 0:
            src = k.ctx_d[i * 128:(i + 1) * 128, :] if i < 2 else k.x_d[(i - 2) * 128:(i - 1) * 128, :]
            S.dma(ht, src, w=[t_ht])
        else:
            S.dma(ht, k.hout0_d[i * 128:(i + 1) * 128, :], r=[k.t_hout0[i]], w=[t_ht])
        k.ln_part1(ht, t_ht, xn, t_xn)
        bufs[i] = (xn, t_xn)

    def stB(i):
        xn, t_xn = bufs.pop(i)
        k.ln_part2(l, xn, t_xn, i, 0, False)
    for n in range(NT + 1):
        if n < NT:
            stA(n)
        if n >= 1:
            stB(n - 1)
    k.dump(f"uT{l}", k.uT[:, :, :], [128, KC, N], BF16, k.t_uT)


def row_to_col(k, src, nr, n, dst, t_src, t_dst):
    S = k.S
    pb, tb = k.bank()
    S.op("pe", lambda e: e.transpose(out=pb[0:n, 0:nr], in_=src, identity=k.identF[0:nr, 0:nr]), r=[t_src, k.t_c], w=[tb])
    S.op("dve", lambda e: e.tensor_copy(out=dst, in_=pb[0:n, 0:nr]), r=[tb], w=[t_dst])


def proj_block(k, wv, t_w, c0, M, t0, nt):
    S = k.S
    pb, tb = k.bank()
    for kk in range(KC):
        S.op("pe", lambda e, kk=kk: e.matmul(pb[0:M, 0:nt], lhsT=wv[:, kk, c0:c0 + M], rhs=k.uT[:, kk, t0:t0 + nt],
                                             start=(kk == 0), stop=(kk == KC - 1)),
             r=t_w + k.t_uT[t0 // 128:(t0 + nt) // 128], w=[tb])
    return pb, tb


def gated_scan(k, dk, dvh, nh, q_ap, t_q, k_ap, t_k, A, t_A, B, t_B, make_logf, v_fn, t_v, o_acc, t_o, qts, t_qts, kts, t_kts, L=64):
    S = k.S
    sc = k.scn
    NC_ = N // L
    cpt = 128 // L
    dvt = dvh * nh
    B3 = B.rearrange("p (c l) -> p c l", l=L)
    for a in range(nh):
        S.op("pool", lambda e, a=a: e.memset(o_acc[a], 0.0), w=[t_o[a]])
    D_ = []
    for d in range(2):
        qt, kt, t_qt, t_kt = qts[d], kts[d], t_qts[d], t_kts[d]
        t_sc = k.t_scn[d]
        rr = sc["rr"][0:dk, d, 0:NC_]
        gg = sc["gg"][0:dk, d, 0:NC_]
        X1 = sc["X1"][0:dk, d, 0:NC_]
        X2 = sc["X2"][0:dk, d, 0:NC_]
        EG = sc["EG"][0:dk, d, 0:NC_]
        make_logf(d)
        S.op("dve", lambda e: e.tensor_tensor_scan(out=B, data0=k.rmask[0:dk, :], data1=A, initial=0.0, op0=ALU.mult, op1=ALU.add),
             r=[t_A, k.t_c], w=[t_B])
        S.op("pool", lambda e, gg=gg: e.tensor_copy(out=gg, in_=B3[:, :, L - 1]), r=[t_B], w=[t_sc])
        if d == 1:
            S.op("pool", lambda e: e.tensor_tensor(out=B, in0=B, in1=A, op=ALU.subtract), r=[t_B, t_A], w=[t_B])
        S.op("pool", lambda e, rr=rr: e.tensor_copy(out=rr, in_=B3[:, :, L // 2]), r=[t_B], w=[t_sc])
        S.op("dve", lambda e, rr=rr: e.tensor_tensor(out=B3, in0=B3, in1=rr.unsqueeze(2).to_broadcast([dk, NC_, L]), op=ALU.subtract),
             r=[t_B, t_sc], w=[t_B])
        sgn = 1.0 if d == 0 else -1.0
        S.op("act", lambda e, sgn=sgn: e.activation(out=A, in_=B, func=AF.Exp, scale=sgn), r=[t_B], w=[t_A])
        S.op("dve", lambda e, qt=qt: e.tensor_tensor(out=qt, in0=q_ap, in1=A, op=ALU.mult), r=[t_q, t_A], w=[t_qt])
        S.op("act", lambda e, sgn=sgn: e.activation(out=A, in_=B, func=AF.Exp, scale=-sgn), r=[t_B, t_qt], w=[t_A])
        S.op("dve", lambda e, kt=kt: e.tensor_tensor(out=kt, in0=k_ap, in1=A, op=ALU.mult), r=[t_k, t_A], w=[t_kt])
        S.op("act", lambda e, X1=X1, rr=rr: e.activation(out=X1, in_=rr, func=AF.Exp), r=[t_sc], w=[t_sc])
        S.op("act", lambda e, EG=EG, gg=gg: e.activation(out=EG, in_=gg, func=AF.Exp), r=[t_sc], w=[t_sc])
        S.op("dve", lambda e, X2=X2, gg=gg, rr=rr: e.tensor_tensor(out=X2, in0=gg, in1=rr, op=ALU.subtract), r=[t_sc], w=[t_sc])
        S.op("act", lambda e, X2=X2: e.activation(out=X2, in_=X2, func=AF.Exp), r=[t_sc], w=[t_sc])
        a_s, c_s = (X1, X2) if d == 0 else (X2, X1)
        fo = tuple(range(cpt))
        bo = tuple(reversed(range(cpt)))
        if d == 0:
            tiles = [(i, fo) for i in range(NT)]
        else:
            tiles = [(i, bo) for i in (1, 0)] + [(i, bo) for i in range(NT - 1, 1, -1)]
        st = {"d": d, "qt": qt, "kt": kt, "t_qt": t_qt, "t_kt": t_kt, "t_sc": t_sc, "EG": EG, "a_s": a_s, "c_s": c_s,
              "M": k.Mf if d == 0 else k.Mb, "tiles": tiles, "s_i": 0, "sb_i": 0, "am_i": 0, "pend": None, "nchunk": 0, "ds_i": 0}
        S.op("pool", lambda e, d=d: e.memset(sc["Sst"][0:dk, d, 0, 0:dvt], 0.0), w=[k.t_Sst[d][0]])
        S.op("pool", lambda e, d=d: e.memset(sc["Sbf"][0:dk, d, 0, 0:dvt], 0.0), w=[k.t_Sbf[d][0]])
        D_.append(st)

    def stage1(st, i):
        d = st["d"]
        tk0 = i * 128
        vt = v_fn(i)
        am_i = st["am_i"]
        st["am_i"] = 1 - am_i
        Am = sc["Am"][:, d, am_i, :]
        ktok = sc["ktok"][:, d, am_i, 0:dk]
        t_Am = k.t_Am2[d][am_i]
        t_kk = k.t_ktok2[d][am_i]
        qt, kt = st["qt"], st["kt"]
        pA, tA = k.bank()
        S.op("pe", lambda e: e.matmul(pA[:, 0:128], lhsT=kt[:, tk0:tk0 + 128], rhs=qt[:, tk0:tk0 + 128], start=True, stop=True),
             r=[st["t_kt"], st["t_qt"]], w=[tA])
        M_ = st["M"]
        S.op("dve", lambda e: e.tensor_tensor(out=Am, in0=pA[:, 0:128], in1=M_[:], op=ALU.mult), r=[tA, k.t_c], w=[t_Am])
        pK, tK = k.bank()
        S.op("pe", lambda e: e.matmul(pK[:, 0:dk], lhsT=kt[:, tk0:tk0 + 128], rhs=k.identB[0:dk, 0:dk], start=True, stop=True),
             r=[st["t_kt"], k.t_c], w=[tK])
        S.op("act", lambda e: e.activation(out=ktok, in_=pK[:, 0:dk], func=AF.Copy), r=[tK], w=[t_kk])
        pI, tI = k.bank()
        for a in range(nh):
            S.op("pe", lambda e, a=a: e.matmul(pI[0:dvh, a * 128:(a + 1) * 128], lhsT=vt[:, a * dvh:(a + 1) * dvh], rhs=Am[:, :], start=True, stop=True),
                 r=t_v + [t_Am], w=[tI])
        pS = [None] * cpt
        c_s = st["c_s"]
        for hf in range(cpt):
            pS_, tS_ = k.bank()
            rows = slice(hf * L, hf * L + L)
            S.op("pe", lambda e, pS_=pS_, rows=rows: e.matmul(pS_[0:dk, 0:dvt], lhsT=ktok[rows, :], rhs=vt[rows, 0:dvt], start=True, stop=True),
                 r=[t_kk] + t_v, w=[tS_])
            ds_i = st["ds_i"]
            st["ds_i"] = (ds_i + 1) % 4
            dSs = sc["dSs"][0:dk, d, ds_i, 0:dvt]
            c = cpt * i + hf
            S.op("act", lambda e, pS_=pS_, dSs=dSs, c=c: e.activation(out=dSs, in_=pS_[0:dk, 0:dvt], func=AF.Identity, scale=c_s[:, c:c + 1]),
                 r=[tS_, st["t_sc"]], w=[k.t_dSs[d][ds_i]])
            pS[hf] = (dSs, k.t_dSs[d][ds_i])
        for a in range(nh):
            S.op("dve", lambda e, a=a: e.tensor_tensor(out=o_acc[a][:, tk0:tk0 + 128], in0=pI[0:dvh, a * 128:(a + 1) * 128],
                                                       in1=o_acc[a][:, tk0:tk0 + 128], op=ALU.add), r=[tI, t_o[a]], w=[t_o[a]])
        st["pend"] = (i, pS)

    def stage2_chunk(st, i, hf, pS, po, tpo, last):
        d = st["d"]
        c = cpt * i + hf
        tok0 = c * L
        qt = st["qt"]
        sb_i = st["sb_i"]
        Sb = sc["Sbf"][0:dk, d, sb_i, 0:dvt]
        for a in range(nh):
            S.op("pe", lambda e, a=a: e.matmul(po[0:dvh, a * 128 + hf * L:a * 128 + hf * L + L], lhsT=Sb[:, a * dvh:(a + 1) * dvh],
                                               rhs=qt[:, tok0:tok0 + L], start=True, stop=True), r=[k.t_Sbf[d][sb_i], st["t_qt"]], w=[tpo])
        if last:
            return
        s_i = st["s_i"]
        Sc = sc["Sst"][0:dk, d, s_i, 0:dvt]
        Sn = sc["Sst"][0:dk, d, 1 - s_i, 0:dvt]
        EG, a_s = st["EG"], st["a_s"]
        dSs, t_dS = pS[hf]
        S.op("dve", lambda e: e.scalar_tensor_tensor(out=Sn, in0=Sc, scalar=EG[:, c:c + 1], in1=dSs, op0=ALU.mult, op1=ALU.add),
             r=[k.t_Sst[d][s_i], t_dS, st["t_sc"]], w=[k.t_Sst[d][1 - s_i]])
        st["s_i"] = 1 - s_i
        n_ = st["nchunk"] + 1
        ti, hfo = st["tiles"][n_ // cpt]
        cn = cpt * ti + hfo[n_ % cpt]
        Sbn = sc["Sbf"][0:dk, d, 1 - sb_i, 0:dvt]
        S.op("act", lambda e: e.activation(out=Sbn, in_=Sn, func=AF.Identity, scale=a_s[:, cn:cn + 1]),
             r=[k.t_Sst[d][1 - s_i], st["t_sc"]], w=[k.t_Sbf[d][1 - sb_i]])
        st["sb_i"] = 1 - sb_i

    nT = NT
    for step in range(nT + 1):
        if step < nT:
            for st in D_:
                stage1(st, st["tiles"][step][0])
        if step >= 1:
            pend = []
            for st in D_:
                i, hfo = st["tiles"][step - 1]
                po, tpo = k.bank()
                pend.append((st, i, hfo, po, tpo))
            for which in range(cpt):
                for (st, i, hfs, po, tpo) in pend:
                    pS = st["pS_prev"]
                    last = (st["nchunk"] == cpt * nT - 1)
                    stage2_chunk(st, i, hfs[which], pS, po, tpo, last)
                    st["nchunk"] += 1
            for (st, i, hfs, po, tpo) in pend:
                tk0 = i * 128
                for a in range(nh):
                    S.op("dve", lambda e, a=a, po=po, tk0=tk0: e.tensor_tensor(out=o_acc[a][:, tk0:tk0 + 128], in0=po[0:dvh, a * 128:(a + 1) * 128],
                                                                               in1=o_acc[a][:, tk0:tk0 + 128], op=ALU.add), r=[tpo, t_o[a]], w=[t_o[a]])
        for st in D_:
            if st["pend"] is not None:
                st["pS_prev"] = st["pend"][1]


def rms_gate_out(k, dvh, nh, o_acc, t_o, A, t_A, B, t_B, gsil, t_gs, gn_cols, t_gn, mixrow, t_mix, chunk_ids):
    S = k.S
    dv = dvh * nh
    for (t0, nt) in TOKB:
        pb, tb = k.bank()
        for a in range(nh):
            S.op("act", lambda e, a=a, t0=t0, nt=nt: e.activation(out=A[0:dvh, a * 512:a * 512 + nt], in_=o_acc[a][:, t0:t0 + nt], func=AF.Square),
                 r=[t_o[a]], w=[t_A])
        for a in range(nh):
            S.op("pe", lambda e, a=a, pb=pb, nt=nt: e.matmul(pb[0:dvh, 0:nt], lhsT=k.onesF[0:dvh, 0:dvh], rhs=A[0:dvh, a * 512:a * 512 + nt],
                                                             start=(a == 0), stop=(a == nh - 1)), r=[t_A, k.t_c], w=[tb])
        S.op("act", lambda e, pb=pb, nt=nt: e.activation(out=B[0:dvh, 0:nt], in_=pb[0:dvh, 0:nt], func=AF.Sqrt, scale=1.0 / dv, bias=k.cst[0:dvh, 0:1]),
             r=[tb, k.t_c], w=[t_B])
        S.op("dve", lambda e, nt=nt: e.reciprocal(out=B[0:dvh, 0:nt], in_=B[0:dvh, 0:nt]), r=[t_B], w=[t_B])
        for a in range(nh):
            S.op("dve", lambda e, a=a, t0=t0, nt=nt: e.tensor_tensor(out=B[0:dvh, 512 + a * 512:512 + a * 512 + nt], in0=o_acc[a][:, t0:t0 + nt],
                                                                     in1=B[0:dvh, 0:nt], op=ALU.mult), r=[t_o[a], t_B], w=[t_B])
            S.op("dve", lambda e, a=a, t0=t0, nt=nt: e.scalar_tensor_tensor(out=mixrow[a][0:dvh, t0:t0 + nt], in0=B[0:dvh, 512 + a * 512:512 + a * 512 + nt],
                                                                            scalar=gn_cols[a], in1=gsil[a][0:dvh, t0:t0 + nt], op0=ALU.mult, op1=ALU.mult),
                 r=[t_B, t_gn, t_gs[a]], w=[t_mix[a]])
    for a in range(nh):
        S.dma(k.mix_d[chunk_ids[a], 0:dvh, :], mixrow[a][0:dvh, :], r=[t_mix[a]], w=[k.t_mixd[chunk_ids[a]]])


def scan_setup(k):
    S = k.S
    if hasattr(k, "scn"):
        return
    sc = {}
    for nm in ("rr", "gg", "X1", "X2", "EG"):
        sc[nm] = S.sbuf("sc_" + nm, [128, 2, 36], F32)
    sc["Sst"] = S.sbuf("sc_Sst", [128, 2, 2, 192], F32)
    sc["dSs"] = S.sbuf("sc_dSs", [128, 2, 4, 192], BF16)
    sc["Sbf"] = S.sbuf("sc_Sbf", [128, 2, 2, 192], BF16)
    sc["Am"] = S.sbuf("sc_Am", [128, 2, 2, 128], BF16)
    sc["ktok"] = S.sbuf("sc_ktok", [128, 2, 2, 128], BF16)
    sc["col"] = S.sbuf("sc_col", [128, 64], F32)
    sc["row"] = k.RB[0:8, 5, 0:768]
    k.scn = sc
    k.t_scn = [Trk(), Trk()]
    k.t_Sst = [[Trk(), Trk()], [Trk(), Trk()]]
    k.t_dSs = [[Trk() for _ in range(4)], [Trk() for _ in range(4)]]
    k.t_Sbf = [[Trk(), Trk()], [Trk(), Trk()]]
    k.t_Am2 = [[Trk(), Trk()], [Trk(), Trk()]]
    k.t_ktok2 = [[Trk(), Trk()], [Trk(), Trk()]]
    k.t_Am = [k.t_Am2[0][0], k.t_Am2[0][1]]
    k.t_col = Trk()
    k.t_row = k.t_rb[5]


ATOK = [(0, 256), (256, 512), (768, 512), (1280, 512), (1792, 512)]


def mixer_even(k):
    S = k.S
    scan_setup(k)
    sc = k.scn
    RB, HB, t_rb, t_hb = k.RB, k.HB, k.t_rb, k.t_hb
    Ct = RB[:, 4, 0:2048]
    St = RB[:, 5, 0:2048]
    col = sc["col"]
    t_col = k.t_col
    ci = col[:, 0:8].bitcast(I32)
    S.op("pool", lambda e: e.iota(ci[:, 0:1], pattern=[[0, 1]], base=0, channel_multiplier=1), w=[t_col])
    S.op("dve", lambda e: e.tensor_single_scalar(out=ci[:, 1:2], in_=ci[:, 0:1], scalar=15, op=ALU.bitwise_and), r=[t_col], w=[t_col])
    S.op("dve", lambda e: e.tensor_scalar(out=ci[:, 2:3], in0=ci[:, 0:1], scalar1=5, scalar2=1, op0=ALU.logical_shift_right, op1=ALU.bitwise_and),
         r=[t_col], w=[t_col])
    S.op("dve", lambda e: e.tensor_scalar(out=ci[:, 3:4], in0=ci[:, 0:1], scalar1=4, scalar2=1, op0=ALU.logical_shift_right, op1=ALU.bitwise_and),
         r=[t_col], w=[t_col])
    S.op("dve", lambda e: e.tensor_copy(out=col[:, 8:11], in_=ci[:, 1:4]), r=[t_col], w=[t_col])
    S.op("act", lambda e: e.activation(out=col[:, 11:12], in_=col[:, 8:9], func=AF.Exp, scale=-math.log(10000.0) / 16.0), r=[t_col], w=[t_col])
    S.op("dve", lambda e: e.tensor_tensor(out=col[:, 13:14], in0=col[:, 11:12], in1=col[:, 9:10], op=ALU.mult), r=[t_col], w=[t_col])
    S.op("dve", lambda e: e.tensor_tensor(out=col[:, 12:13], in0=col[:, 11:12], in1=col[:, 13:14], op=ALU.subtract), r=[t_col], w=[t_col])
    S.op("dve", lambda e: e.tensor_scalar(out=col[:, 14:15], in0=col[:, 10:11], scalar1=2.0, scalar2=-1.0, op0=ALU.mult, op1=ALU.add),
         r=[t_col], w=[t_col])
    ri = RB[:, 0, 0:2048].bitcast(I32)
    qi = RB[:, 1, 0:2048].bitcast(I32)
    S.op("pool", lambda e: e.iota(ri, pattern=[[1, 32], [0, 64]], base=0, channel_multiplier=0), w=[t_rb[0]])
    S.op("pool", lambda e: e.iota(qi, pattern=[[0, 32], [1, 64]], base=0, channel_multiplier=0), w=[t_rb[1]])
    rf = RB[:, 2, 0:2048]
    qf = RB[:, 3, 0:2048]
    S.op("dve", lambda e: e.tensor_copy(out=rf, in_=ri), r=[t_rb[0]], w=[t_rb[2]])
    S.op("dve", lambda e: e.tensor_copy(out=qf, in_=qi), r=[t_rb[1]], w=[t_rb[3]])
    ang = RB[:, 0, 0:2048]
    S.op("dve", lambda e: e.tensor_scalar(out=ang, in0=rf, scalar1=col[:, 12:13], scalar2=None, op0=ALU.mult), r=[t_rb[2], t_col], w=[t_rb[0]])
    S.op("dve", lambda e: e.scalar_tensor_tensor(out=ang, in0=qf, scalar=col[:, 13:14], in1=ang, op0=ALU.mult, op1=ALU.add),
         r=[t_rb[3], t_rb[0], t_col], w=[t_rb[0]])
    def range_reduce(dst, t_dst, add, tmpi, t_tmpi, tmpf_, t_tmpf):
        S.op("dve", lambda e: e.tensor_scalar(out=tmpi, in0=ang, scalar1=add, scalar2=1.0 / (2 * PI), op0=ALU.add, op1=ALU.mult),
             r=[t_rb[0]], w=[t_tmpi])
        S.op("dve", lambda e: e.tensor_copy(out=tmpf_, in_=tmpi), r=[t_tmpi], w=[t_tmpf])
        S.op("dve", lambda e: e.scalar_tensor_tensor(out=dst, in0=tmpf_, scalar=-2 * PI, in1=ang, op0=ALU.mult, op1=ALU.add),
             r=[t_tmpf, t_rb[0]], w=[t_dst])
        if add != 0.0:
            S.op("dve", lambda e: e.tensor_scalar(out=dst, in0=dst, scalar1=add, scalar2=None, op0=ALU.add), r=[t_dst], w=[t_dst])
        S.op("dve", lambda e: e.tensor_scalar(out=tmpf_, in0=dst, scalar1=PI, scalar2=-2 * PI, op0=ALU.is_gt, op1=ALU.mult),
             r=[t_dst], w=[t_tmpf])
        S.op("dve", lambda e: e.tensor_tensor(out=dst, in0=dst, in1=tmpf_, op=ALU.add), r=[t_dst, t_tmpf], w=[t_dst])
        S.op("dve", lambda e: e.tensor_scalar(out=tmpf_, in0=dst, scalar1=-PI, scalar2=2 * PI, op0=ALU.is_lt, op1=ALU.mult),
             r=[t_dst], w=[t_tmpf])
        S.op("dve", lambda e: e.tensor_tensor(out=dst, in0=dst, in1=tmpf_, op=ALU.add), r=[t_dst, t_tmpf], w=[t_dst])
        S.op("dve", lambda e: e.tensor_scalar(out=dst, in0=dst, scalar1=PI, scalar2=-PI, op0=ALU.min, op1=ALU.max), r=[t_dst], w=[t_dst])
    m1 = RB[:, 1, 0:2048]
    tmpi = RB[:, 2, 0:2048].bitcast(I32)
    tmpf_ = RB[:, 3, 0:2048]
    range_reduce(m1, t_rb[1], 0.0, tmpi, t_rb[2], tmpf_, t_rb[3])
    S.op("act", lambda e: e.activation(out=St, in_=m1, func=AF.Sin, scale=col[:, 14:15]), r=[t_rb[1], t_col], w=[t_rb[5]])
    range_reduce(m1, t_rb[1], PI / 2, tmpi, t_rb[2], tmpf_, t_rb[3])
    S.op("act", lambda e: e.activation(out=Ct, in_=m1, func=AF.Sin), r=[t_rb[1]], w=[t_rb[4]])
    k.dump("ropeC", Ct, [128, 2048], F32, [t_rb[4]])
    k.dump("ropeS", St, [128, 2048], F32, [t_rb[5]])
    if k.sub == "M0a":
        return
    S.dma(col[:, 16:24], k.sink_d[0:1, :].to_broadcast([128, 8]), w=[t_col])
    S.op("act", lambda e: e.activation(out=col[:, 16:24], in_=col[:, 16:24], func=AF.Exp), r=[t_col], w=[t_col])

    qT = HB[:, 0:2, :]
    kAB = [HB[:, 2, :], HB[:, 3, :]]
    vdup = HB[:, 4, :].rearrange("p (i c) -> p i c", c=128)
    mixA = [HB[:, 5, :], HB[:, 6, :]]
    et = [HB[:, 7, ei * 512:(ei + 1) * 512] for ei in range(4)] + [RB[:, 3, 0:256].bitcast(BF16)]
    S.op("pool", lambda e: e.memset(kAB[0][64:128, :], 0.0), w=[t_hb[2]])
    S.op("pool", lambda e: e.memset(kAB[1][0:64, :], 0.0), w=[t_hb[3]])
    t_et = [Trk() for _ in range(5)]
    k.et_i = 0
    tmpf = [RB[:, 0, 0:512], RB[:, 0, 512:1024], RB[:, 1, 0:512], RB[:, 1, 512:1024]]
    t_tmp = [Trk() for _ in range(4)]
    dn = RB[:, 2, 0:512]
    t_dn = t_rb[2]
    wv_v, t_wv = k.load_w(k.w0_d[:, 1536:1664].rearrange("(k p) n -> p k n", p=128))
    for j in range(2):
        base = j * 768
        wq, t_wq = k.load_w(k.w0_d[:, base:base + 512].rearrange("(k p) n -> p k n", p=128))
        wk, t_wk = k.load_w(k.w0_d[:, base + 512:base + 768].rearrange("(k p) n -> p k n", p=128))
        tmp_i = 0
        for (t0, nt) in ATOK:
            for blk in range(3):
                if blk < 2:
                    pq, tq = proj_block(k, wq, t_wq, blk * 128, 128, t0, nt)
                    dst = qT[:, blk, t0:t0 + nt]
                    tdst = t_hb[blk]
                else:
                    pq, tq = proj_block(k, wk, t_wk, 0, 128, t0, nt)
                    dst = None
                if t0 == 0:
                    if blk < 2:
                        S.op("act", lambda e, pq=pq, dst=dst, nt=nt: e.activation(out=dst, in_=pq[:, 0:nt], func=AF.Copy), r=[tq], w=[tdst])
                    else:
                        S.op("act", lambda e, pq=pq, nt=nt, t0=t0: e.activation(out=kAB[0][0:64, t0:t0 + nt], in_=pq[0:64, 0:nt], func=AF.Copy), r=[tq], w=[t_hb[2]])
                        S.op("act", lambda e, pq=pq, nt=nt, t0=t0: e.activation(out=kAB[1][64:128, t0:t0 + nt], in_=pq[64:128, 0:nt], func=AF.Copy), r=[tq], w=[t_hb[3]])
                    continue
                if blk < 2:
                    ps_, ts_ = proj_block(k, wq, t_wq, 256 + blk * 128, 128, t0, nt)
                else:
                    ps_, ts_ = proj_block(k, wk, t_wk, 128, 128, t0, nt)
                l0 = t0 - 256
                ta, tb_ = tmp_i % 4, (tmp_i + 1) % 4
                tmp_i += 2
                S.op("dve", lambda e, pq=pq, ta=ta, l0=l0, nt=nt: e.tensor_tensor(out=tmpf[ta][:, 0:nt], in0=pq[:, 0:nt], in1=Ct[:, l0:l0 + nt], op=ALU.mult),
                     r=[tq, t_rb[4]], w=[t_tmp[ta]])
                S.op("dve", lambda e, ps_=ps_, tb_=tb_, l0=l0, nt=nt: e.tensor_tensor(out=tmpf[tb_][:, 0:nt], in0=ps_[:, 0:nt], in1=St[:, l0:l0 + nt], op=ALU.mult),
                     r=[ts_, t_rb[5]], w=[t_tmp[tb_]])
                if blk < 2:
                    S.op("pool", lambda e, dst=dst, ta=ta, tb_=tb_, nt=nt: e.tensor_tensor(out=dst, in0=tmpf[ta][:, 0:nt], in1=tmpf[tb_][:, 0:nt], op=ALU.add),
                         r=[t_tmp[ta], t_tmp[tb_]], w=[tdst])
                else:
                    S.op("pool", lambda e, ta=ta, tb_=tb_, nt=nt, t0=t0: e.tensor_tensor(out=kAB[0][0:64, t0:t0 + nt], in0=tmpf[ta][0:64, 0:nt], in1=tmpf[tb_][0:64, 0:nt], op=ALU.add),
                         r=[t_tmp[ta], t_tmp[tb_]], w=[t_hb[2]])
                    S.op("pool", lambda e, ta=ta, tb_=tb_, nt=nt, t0=t0: e.tensor_tensor(out=kAB[1][64:128, t0:t0 + nt], in0=tmpf[ta][64:128, 0:nt], in1=tmpf[tb_][64:128, 0:nt], op=ALU.add),
                         r=[t_tmp[ta], t_tmp[tb_]], w=[t_hb[3]])

        if k.sub == "M0p":
            k.dump("qT0", qT, [128, 2, N], BF16, [t_hb[0], t_hb[1]])
            return

        def ev_v(pb, tb, i, j=j):
            S.op("act", lambda e: e.activation(out=vdup[:, i, 0:64], in_=pb[:, j * 64:(j + 1) * 64], func=AF.Copy), r=[tb], w=[t_hb[4]])
            S.op("dve", lambda e: e.tensor_copy(out=vdup[:, i, 64:128], in_=pb[:, j * 64:(j + 1) * 64]), r=[tb], w=[t_hb[4]])
        k.proj_tm(wv_v, t_wv, 0, 128, ev_v)
        if j == 0:
            k.dump("qT0", qT, [128, 2, N], BF16, [t_hb[0], t_hb[1]])
            k.dump("kT0", HB[:, 2:4, :], [128, 2, N], BF16, [t_hb[2], t_hb[3]])
        if k.sub == "M0v":
            return
        for qb in range(NT):
            q0 = qb * 128
            if (k.sub == "M0q1" and qb == 1) or (k.sub == "M0q3" and qb == 3):
                k.dump("mixA", HB[:, 5:7, :], [128, 2, N], BF16, [t_hb[5], t_hb[6]])
                return
            if qb < 2:
                chunks = [(0, None), (1, None)]
            else:
                n_ = qb - 2
                chunks = [(0, None), (1, None)]
                if n_ > 0:
                    chunks.append((qb - 1, k.MP))
                chunks.append((qb, None))
                if n_ < 15:
                    chunks.append((qb + 1, k.MN))
            po, tpo = k.bank()
            pd, tpd = k.bank()
            nch = len(chunks)
            pss_l = []
            for ci_, (kc, msk) in enumerate(chunks):
                pss, tss = k.bank()
                for hh in range(4):
                    blk, half = hh // 2, hh % 2
                    S.op("pe", lambda e, pss=pss, hh=hh, blk=blk, half=half, kc=kc, q0=q0: e.matmul(
                        pss[:, hh * 128:(hh + 1) * 128], lhsT=kAB[half][:, kc * 128:(kc + 1) * 128], rhs=qT[:, blk, q0:q0 + 128],
                        start=True, stop=True), r=[t_hb[2 + half], t_hb[blk]], w=[tss])
                pss_l.append((pss, tss))
            for ci_, (kc, msk) in enumerate(chunks):
                pss, tss = pss_l[ci_]
                ei = ci_
                S.op("act", lambda e, pss=pss, ei=ei: e.activation(out=et[ei], in_=pss[:, :], func=AF.Exp, scale=0.125), r=[tss], w=[t_et[ei]])
                if msk is not None:
                    S.op("dve", lambda e, ei=ei, msk=msk: e.tensor_tensor(out=et[ei], in0=et[ei], in1=msk[:], op=ALU.mult),
                         r=[t_et[ei], k.t_c], w=[t_et[ei]])
            for ci_, (kc, msk) in enumerate(chunks):
                ei = ci_
                S.op("pe", lambda e, ei=ei, kc=kc, ci_=ci_, po=po, nch=nch: e.matmul(
                    po[:, :], lhsT=vdup[:, kc, :], rhs=et[ei][:, :], start=(ci_ == 0), stop=(ci_ == nch - 1)),
                    r=[t_hb[4], t_et[ei]], w=[tpo])
                S.op("pe", lambda e, ei=ei, ci_=ci_, pd=pd, nch=nch: e.matmul(
                    pd[:, :], lhsT=k.onesB[:], rhs=et[ei][:, :], start=(ci_ == 0), stop=(ci_ == nch - 1)),
                    r=[k.t_c, t_et[ei]], w=[tpd])
            if k.sub == "M0qb":
                S.op("dve", lambda e, po=po: e.tensor_copy(out=RB[:, 0, 0:512], in_=po[:, :]), r=[tpo], w=[t_rb[0]])
                S.op("dve", lambda e, pd=pd: e.tensor_copy(out=RB[:, 0, 512:1024], in_=pd[:, :]), r=[tpd], w=[t_rb[0]])
                k.dump("popd", RB[:, 0, 0:1024], [128, 1024], F32, [t_rb[0]])
                return
            for hh in range(4):
                S.op("dve", lambda e, hh=hh, pd=pd, j=j: e.tensor_scalar(out=dn[:, hh * 128:(hh + 1) * 128], in0=pd[:, hh * 128:(hh + 1) * 128],
                                                                    scalar1=col[:, 16 + 4 * j + hh:17 + 4 * j + hh], scalar2=None, op0=ALU.add),
                     r=[tpd, t_col], w=[t_dn])
            S.op("dve", lambda e: e.reciprocal(out=dn, in_=dn), r=[t_dn], w=[t_dn])
            for hh in range(4):
                blk, half = hh // 2, hh % 2
                rows = slice(half * 64, half * 64 + 64)
                S.op("dve", lambda e, hh=hh, blk=blk, rows=rows, po=po, q0=q0: e.tensor_tensor(
                    out=mixA[blk][rows, q0:q0 + 128], in0=po[rows, hh * 128:(hh + 1) * 128], in1=dn[rows, hh * 128:(hh + 1) * 128], op=ALU.mult),
                    r=[tpo, t_dn], w=[t_hb[5 + blk]])
        for blk in range(2):
            S.dma(k.mix_d[2 * j + blk, :, :], mixA[blk], r=[t_hb[5 + blk]], w=[k.t_mixd[2 * j + blk]])
    k.dump("mixd_att", k.mix_d[0:4, :, :], [4, 128, N], BF16, k.t_mixd[0:4])
    S.barrier()
    if k.sub == "M0b":
        return

    row = sc["row"]
    t_row = k.t_row
    S.dma(row[0:4, 0:512], k.lbl_d.rearrange("r a n -> (r a) n"), w=[t_row])
    for h in range(4):
        row_to_col(k, row[0:4, h * 128:(h + 1) * 128], 4, 128, col[:, 24 + 4 * h:28 + 4 * h], t_row, t_col)
    lbT = col[:, 24:40].rearrange("p (h r a) -> p h r a", r=2, a=2)
    lbv = col[:, 44:52].rearrange("p (h r) -> p h r", r=2)
    omv = col[:, 52:60].rearrange("p (h r) -> p h r", r=2)
    S.op("dve", lambda e: e.tensor_tensor(out=lbv, in0=lbT[:, :, :, 0], in1=lbT[:, :, :, 1], op=ALU.subtract), r=[t_col], w=[t_col])
    S.op("act", lambda e: e.activation(out=lbv, in_=lbv, func=AF.Sigmoid), r=[t_col], w=[t_col])
    S.op("dve", lambda e: e.tensor_scalar(out=omv, in0=lbv, scalar1=-1.0, scalar2=1.0, op0=ALU.mult, op1=ALU.add), r=[t_col], w=[t_col])
    S.dma(row[0:1, 0:512], k.hn_d[:, :], w=[t_row])
    for h in range(4):
        row_to_col(k, row[0:1, h * 128:(h + 1) * 128], 1, 128, col[:, 40 + h:41 + h], t_row, t_col)

    qrow, Krow, A, B, oacc = RB[:, 0, :], RB[:, 1, :], RB[:, 2, :], RB[:, 3, :], RB[:, 4, :]
    gsil, mixrow = HB[:, 2, :], HB[:, 4, :]
    qts, kts = [HB[:, 0, :], HB[:, 5, :]], [HB[:, 1, :], HB[:, 6, :]]
    vtm = HB[:, 3, :].rearrange("p (i c) -> p i c", c=128)
    for h in range(4):
        base = 1664 + 512 * h
        wg, t_wg = k.load_w(k.w0_d[:, base:base + 512].rearrange("(k p) n -> p k n", p=128))
        wvv, t_wvv = k.load_w(k.w0_d[:, 3712 + 128 * h:3712 + 128 * (h + 1)].rearrange("(k p) n -> p k n", p=128))

        def ev_q(pb, tb, t0, nt):
            S.op("act", lambda e: e.activation(out=qrow[:, t0:t0 + nt], in_=pb[:, 0:nt], func=AF.Copy), r=[tb], w=[t_rb[0]])
        k.proj_fm(wg, t_wg, 256, 128, ev_q)

        def ev_g(pb, tb, t0, nt):
            S.op("act", lambda e: e.activation(out=B[:, t0:t0 + nt], in_=pb[:, 0:nt], func=AF.Sigmoid), r=[tb], w=[t_rb[3]])
            S.op("dve", lambda e: e.tensor_tensor(out=gsil[:, t0:t0 + nt], in0=pb[:, 0:nt], in1=B[:, t0:t0 + nt], op=ALU.mult),
                 r=[tb, t_rb[3]], w=[t_hb[2]])
        k.proj_fm(wg, t_wg, 384, 128, ev_g)

        def ev_v2(pb, tb, i):
            S.op("act", lambda e: e.activation(out=vtm[:, i, :], in_=pb[:, 0:128], func=AF.Copy), r=[tb], w=[t_hb[3]])
        k.proj_tm(wvv, t_wvv, 0, 128, ev_v2)

        def make_logf(d, h=h, wg=wg, t_wg=t_wg):
            def ev_z(pb, tb, t0, nt):
                S.op("act", lambda e: e.activation(out=A[:, t0:t0 + nt], in_=pb[:, 0:nt], func=AF.Sigmoid), r=[tb], w=[t_rb[2]])
            k.proj_fm(wg, t_wg, d * 128, 128, ev_z)
            S.op("dve", lambda e: e.tensor_scalar(out=A, in0=A, scalar1=omv[:, h, d:d + 1], scalar2=lbv[:, h, d:d + 1], op0=ALU.mult, op1=ALU.add),
                 r=[t_rb[2], t_col], w=[t_rb[2]])
            S.op("pool", lambda e: e.tensor_scalar(out=Krow, in0=A, scalar1=-1.0, scalar2=1.0, op0=ALU.mult, op1=ALU.add),
                 r=[t_rb[2]], w=[t_rb[1]])
            S.op("act", lambda e: e.activation(out=A, in_=A, func=AF.Ln), r=[t_rb[2]], w=[t_rb[2]])
        gated_scan(k, 128, 128, 1, qrow, t_rb[0], Krow, t_rb[1], A, t_rb[2], B, t_rb[3], make_logf,
                   lambda i: vtm[:, i, :], [t_hb[3]], [oacc], [t_rb[4]], qts, [t_hb[0], t_hb[5]], kts, [t_hb[1], t_hb[6]])
        if h == 0:
            k.dump("oacc0", oacc, [128, N], F32, [t_rb[4]])
        rms_gate_out(k, 128, 1, [oacc], [t_rb[4]], A, t_rb[2], B, t_rb[3], [gsil], [t_hb[2]], [col[:, 40 + h:41 + h]], t_col,
                     [mixrow], [t_hb[4]], [4 + h])
    k.dump("mixd0", k.mix_d[0:8, :, :], [8, 128, N], BF16, k.t_mixd[0:8])


def phase_D(k, l):
    S = k.S
    RB, HB = k.RB, k.HB
    k.bcast_rows(l, "mix")
    if l == 0:
        wo, t_wo = k.load_w(k.wo0_d.rearrange("(c p) n -> p c n", p=128), nslots=2)
        chunks = [(c, 128, wo, t_wo, c) for c in range(8)]
        tiles = list(range(NT))
        nch = 8
    else:
        wo, t_wo = k.load_w(k.wo1_d[0:768, :].rearrange("(c p) n -> p c n", p=96), nslots=2, parts=96)
        wf, t_wf = k.load_w(k.wo1_d[768:1024, :].rearrange("(c p) n -> p c n", p=128), nslots=1)
        chunks = [(c, 96, wo, t_wo, c) for c in range(8)] + [(8 + c, 128, wf, t_wf, c) for c in range(2)]
        tiles = list(range(2, NT))
        nch = 10
    tb_ = [RB[:, r, hf * 1024:(hf + 1) * 1024] for r in range(6) for hf in range(2)]
    tt = k.t_tile
    nchunks = len(chunks)

    def bufs_for(n_):
        s6 = (n_ % 2) * 6
        return [tb_[s6 + q] for q in range(6)], [tt[s6 + q] for q in range(6)]

    def stA(n_):
        i = tiles[n_]
        (ht, tmp, xn1, hnew, xn2, ufb), (t_ht, t_tmp, t_xn1, t_hn, t_xn2, t_uf) = bufs_for(n_)
        mt = HB[:, n_ % 2, 0:nch * 128].rearrange("p (c n) -> p c n", n=128)
        t_mt = k.t_hb[n_ % 2]
        S.dma(mt, k.mix_d[0:nch, :, i * 128:(i + 1) * 128].rearrange("c p n -> p c n"), r=k.t_mixd[0:nch], w=[t_mt])
        if l == 0:
            src = k.ctx_d[i * 128:(i + 1) * 128, :] if i < 2 else k.x_d[(i - 2) * 128:(i - 1) * 128, :]
            S.dma(ht, src, w=[t_ht])
        else:
            S.dma(ht, k.hout0_d[i * 128:(i + 1) * 128, :], r=[k.t_hout0[i]], w=[t_ht])
        pp, tp = k.pair()
        for hf in range(2):
            for ci_, (c, KR, wv, t_wv, wc) in enumerate(chunks):
                S.op("pe", lambda e, hf=hf, c=c, KR=KR, wv=wv, wc=wc, ci_=ci_: e.matmul(
                    pp[:, hf * 512:(hf + 1) * 512], lhsT=mt[0:KR, c, :], rhs=wv[0:KR, wc, hf * 512:(hf + 1) * 512],
                    start=(ci_ == 0), stop=(ci_ == nchunks - 1)), r=[t_mt] + t_wv, w=[tp[hf]])
        r_ = 1 if i < 2 else 0
        for hf in range(2):
            S.op("dve", lambda e, hf=hf: e.tensor_tensor(out=tmp[:, hf * 512:(hf + 1) * 512], in0=pp[:, hf * 512:(hf + 1) * 512],
                                                         in1=k.bc[:, r_, hf * 512:(hf + 1) * 512], op=ALU.mult),
                 r=[tp[hf], k.t_bc[r_]], w=[t_tmp])
        S.op("dve", lambda e: e.scalar_tensor_tensor(out=tmp, in0=ht, scalar=ALPHA, in1=tmp, op0=ALU.mult, op1=ALU.add),
             r=[t_ht, t_tmp], w=[t_tmp])
        st_ap, mv_ap, rs_ap, nb_ap, t_st = k.st_slot()
        k.ln_stats(tmp, t_tmp, st_ap, mv_ap, rs_ap, nb_ap, t_st)
        S.op("act", lambda e: e.activation(out=xn1, in_=tmp, func=AF.Identity, scale=rs_ap, bias=nb_ap), r=[t_tmp, t_st], w=[t_xn1])
        S.op("pool", lambda e: e.tensor_tensor(out=xn1, in0=xn1, in1=k.bc[:, 2, :], op=ALU.mult), r=[t_xn1, k.t_bc[2]], w=[t_xn1])
        S.op("pool", lambda e: e.tensor_tensor(out=hnew, in0=xn1, in1=k.bc[:, 3, :], op=ALU.add), r=[t_xn1, k.t_bc[3]], w=[t_hn])
        k.ln_part1(hnew, t_hn, xn2, t_xn2)

    def stB(n_):
        i = tiles[n_]
        (ht, tmp, xn1, hnew, xn2, ufb), (t_ht, t_tmp, t_xn1, t_hn, t_xn2, t_uf) = bufs_for(n_)
        uf = ufb.rearrange("p (k n) -> p k n", n=128)
        S.dma(k.hmid_d[i * 128:(i + 1) * 128, :], hnew, r=[t_hn], w=[k.t_hmid[i]])
        k.ln_part2(l, xn2, t_xn2, i, 3, True, uf, t_uf)

    def stC(n_):
        i = tiles[n_]
        (ht, tmp, xn1, hnew, xn2, ufb), (t_ht, t_tmp, t_xn1, t_hn, t_xn2, t_uf) = bufs_for(n_)
        uf = ufb.rearrange("p (k n) -> p k n", n=128)
        k.route(i, uf, t_uf)
    nT_ = len(tiles)
    for n in range(nT_ + 2):
        if n < nT_:
            stA(n)
        if 1 <= n <= nT_:
            stB(n - 1)
        if n >= 2:
            stC(n - 2)
    k.route_all(tiles[0], len(tiles), RB[:, 0, :], [k.t_tile[0], k.t_tile[1]])
    k.dump(f"hmid{l}", k.hmid_d[:, :], [N, D], F32, k.t_hmid)
    k.dump(f"u2T{l}", k.uT[:, :, :], [128, KC, N], BF16, k.t_uT)
    k.dump(f"gates{l}", k.gates[:, :, :], [128, NT, 16], F32, k.t_gates)


def phase_E(k, l):
    S = k.S
    RB = k.RB
    k.bcast_rows(l, "moe")
    if l == 0:
        halves = [list(range(0, 9)), list(range(9, 18))]
        bsz = 384
    else:
        halves = [list(range(2, 10)), list(range(10, 18))]
        bsz = 512
    yacc = RB[:, 0:4, :].rearrange("p a n -> p (a n)").rearrange("p (t d) -> p t d", d=1024)
    t_y = k.t_tile[0:9]
    hTb = RB[:, 4, :].bitcast(BF16)
    hT = [hTb[:, q * 2048:(q + 1) * 2048].rearrange("p (f n) -> p f n", n=512) for q in range(2)]
    t_hT = [k.t_tile[9], k.t_tile[10]]
    sg = [RB[:, 5, q * 512:(q + 1) * 512] for q in range(2)]
    t_sg = [k.t_rb[4], k.t_rb[5]]
    Hb = RB[:, 5, 1024:2048]
    t_H = k.t_tile[11]
    dst_d = k.hout0_d if l == 0 else k.out_d
    k.h_i = 0
    k.s_i = 0
    for tiles in halves:
        tok0 = tiles[0] * 128
        ntok = len(tiles) * 128
        blocks = [(tok0 + b0, bsz) for b0 in range(0, ntok, bsz)]
        for e_ in range(16):
            w1, t_w1 = k.load_w(k.eg_d[l, e_].rearrange("(c p) n -> p c n", p=128))
            w3, t_w3 = k.load_w(k.eu_d[l, e_].rearrange("(c p) n -> p c n", p=128))
            w2, t_w2 = k.load_w(k.ed_d[l, e_].rearrange("(c p) n -> p c n", p=128))
            for (b0, nb) in blocks:
                hi = k.h_i
                k.h_i = 1 - hi
                hTc = hT[hi]
                for f in range(4):
                    p1, tp1 = k.bank()
                    for kk in range(KC):
                        S.op("pe", lambda e, kk=kk, f=f, p1=p1, w1=w1, b0=b0, nb=nb: e.matmul(
                            p1[:, 0:nb], lhsT=w1[:, kk, f * 128:(f + 1) * 128], rhs=k.uT[:, kk, b0:b0 + nb], start=(kk == 0), stop=(kk == KC - 1)),
                            r=t_w1 + k.t_uT[b0 // 128:(b0 + nb) // 128], w=[tp1])
                    p3, tp3 = k.bank()
                    for kk in range(KC):
                        S.op("pe", lambda e, kk=kk, f=f, p3=p3, w3=w3, b0=b0, nb=nb: e.matmul(
                            p3[:, 0:nb], lhsT=w3[:, kk, f * 128:(f + 1) * 128], rhs=k.uT[:, kk, b0:b0 + nb], start=(kk == 0), stop=(kk == KC - 1)),
                            r=t_w3 + k.t_uT[b0 // 128:(b0 + nb) // 128], w=[tp3])
                    si = k.s_i
                    k.s_i = 1 - si
                    S.op("act", lambda e, p1=p1, si=si, nb=nb: e.activation(out=sg[si][:, 0:nb], in_=p1[:, 0:nb], func=AF.Sigmoid), r=[tp1], w=[t_sg[si]])
                    S.op("dve", lambda e, p1=p1, si=si, nb=nb: e.tensor_tensor(out=sg[si][:, 0:nb], in0=p1[:, 0:nb], in1=sg[si][:, 0:nb], op=ALU.mult),
                         r=[tp1, t_sg[si]], w=[t_sg[si]])
                    S.op("dve", lambda e, p3=p3, si=si, nb=nb, f=f, hTc=hTc: e.tensor_tensor(out=hTc[:, f, 0:nb], in0=p3[:, 0:nb], in1=sg[si][:, 0:nb], op=ALU.mult),
                         r=[tp3, t_sg[si]], w=[t_hT[hi]])
                for tl in range(nb // 128):
                    gi = (b0 // 128) + tl
                    yi = gi - tiles[0]
                    for dh in range(2):
                        py, tpy = k.bank()
                        for f in range(4):
                            S.op("pe", lambda e, f=f, py=py, hTc=hTc, tl=tl, dh=dh, w2=w2: e.matmul(
                                py[:, :], lhsT=hTc[:, f, tl * 128:(tl + 1) * 128], rhs=w2[:, f, dh * 512:(dh + 1) * 512], start=(f == 0), stop=(f == 3)),
                                r=[t_hT[hi]] + t_w2, w=[tpy])
                        ya = yacc[:, yi, dh * 512:(dh + 1) * 512]
                        gs = k.gates[:, gi, e_:e_ + 1]
                        if e_ == 0:
                            S.op("dve", lambda e, py=py, ya=ya, gs=gs: e.tensor_scalar(out=ya, in0=py[:, :], scalar1=gs, scalar2=None, op0=ALU.mult),
                                 r=[tpy, k.t_gates[gi]], w=[t_y[yi]])
                        else:
                            S.op("dve", lambda e, py=py, ya=ya, gs=gs: e.scalar_tensor_tensor(out=ya, in0=py[:, :], scalar=gs, in1=ya, op0=ALU.mult, op1=ALU.add),
                                 r=[tpy, k.t_gates[gi], t_y[yi]], w=[t_y[yi]])
        Hbs = [RB[:, 5, 1024:2048], RB[:, 5, 0:1024]]
        t_Hs = [[k.t_tile[11]], [k.t_rb[4], k.t_rb[5]]]

        def fin_load(yi):
            gi = tiles[yi]
            S.dma(Hbs[yi % 2], k.hmid_d[gi * 128:(gi + 1) * 128, :], r=[k.t_hmid[gi]], w=t_Hs[yi % 2])

        def fin_store(yi):
            gi = tiles[yi]
            yt = yacc[:, yi, :]
            if l == 0:
                S.dma(k.hout0_d[gi * 128:(gi + 1) * 128, :], yt, r=[t_y[yi]], w=[k.t_hout0[gi]])
            else:
                S.dma(k.out_d[(gi - 2) * 128:(gi - 1) * 128, :], yt, r=[t_y[yi]], w=[k.t_out[gi - 2]])
        fin_load(0)
        for yi, gi in enumerate(tiles):
            yt = yacc[:, yi, :]
            Hb = Hbs[yi % 2]
            t_H = t_Hs[yi % 2]
            r_ = 1 if gi < 2 else 0
            if l == 0 and yi == 0 and tiles[0] == 0:
                k.dump("ymoe_t0", yt, [128, D], F32, [t_y[yi]])
            if yi + 1 < len(tiles):
                fin_load(yi + 1)
            S.op("dve", lambda e, yt=yt, r_=r_: e.tensor_tensor(out=yt, in0=yt, in1=k.bc[:, r_, :], op=ALU.mult), r=[t_y[yi], k.t_bc[r_]], w=[t_y[yi]])
            S.op("dve", lambda e, yt=yt, Hb=Hb: e.scalar_tensor_tensor(out=yt, in0=Hb, scalar=ALPHA, in1=yt, op0=ALU.mult, op1=ALU.add),
                 r=t_H + [t_y[yi]], w=[t_y[yi]])
            st_ap, mv_ap, rs_ap, nb_ap, t_st = k.st_slot()
            k.ln_stats(yt, t_y[yi], st_ap, mv_ap, rs_ap, nb_ap, t_st)
            S.op("act", lambda e, yt=yt, Hb=Hb, rs_ap=rs_ap, nb_ap=nb_ap: e.activation(out=Hb, in_=yt, func=AF.Identity, scale=rs_ap, bias=nb_ap),
                 r=[t_y[yi], t_st], w=t_H)
            S.op("pool", lambda e, Hb=Hb: e.tensor_tensor(out=Hb, in0=Hb, in1=k.bc[:, 2, :], op=ALU.mult), r=t_H + [k.t_bc[2]], w=t_H)
            S.op("pool", lambda e, yt=yt, Hb=Hb: e.tensor_tensor(out=yt, in0=Hb, in1=k.bc[:, 3, :], op=ALU.add), r=t_H + [k.t_bc[3]], w=[t_y[yi]])
            if yi >= 1:
                fin_store(yi - 1)
        fin_store(len(tiles) - 1)
    if l == 0:
        k.dump("hout0", k.hout0_d[:, :], [N, D], F32, k.t_hout0)


def mixer_odd(k):
    S = k.S
    scan_setup(k)
    sc = k.scn
    RB, HB, t_rb, t_hb = k.RB, k.HB, k.t_rb, k.t_hb
    col, t_col, row, t_row = sc["col"], k.t_col, sc["row"], k.t_row
    LATB = [(256, 512), (768, 512), (1280, 512), (1792, 512)]
    BC = sc["Am"][:, 0, 0, :]
    BS = sc["Am"][:, 0, 1, :]
    ci = col[:, 0:32].bitcast(I32)
    S.op("pool", lambda e: e.iota(ci[:, 0:1], pattern=[[0, 1]], base=0, channel_multiplier=1), w=[t_col])
    S.op("dve", lambda e: e.tensor_single_scalar(out=ci[:, 1:2], in_=ci[:, 0:1], scalar=63, op=ALU.bitwise_and), r=[t_col], w=[t_col])
    S.op("dve", lambda e: e.tensor_copy(out=col[:, 32:33], in_=ci[:, 1:2]), r=[t_col], w=[t_col])
    S.op("pool", lambda e: e.iota(ci[:, 2:18], pattern=[[128, 16]], base=0, channel_multiplier=1), r=[t_col], w=[t_col])
    S.op("dve", lambda e: e.tensor_copy(out=col[:, 40:56], in_=ci[:, 2:18]), r=[t_col], w=[t_col])
    qi = RB[:, 0, 0:128].bitcast(I32)
    qf = RB[:, 0, 128:256]
    ki = RB[:, 0, 256:384].bitcast(I32)
    kci = RB[:, 0, 384:512].bitcast(I32)
    tq = t_rb[0]
    S.op("pool", lambda e: e.iota(qi, pattern=[[1, 128]], base=0, channel_multiplier=0), w=[tq])
    S.op("dve", lambda e: e.tensor_single_scalar(out=qi, in_=qi, scalar=63, op=ALU.bitwise_and), r=[tq], w=[tq])
    S.op("dve", lambda e: e.tensor_copy(out=qf, in_=qi), r=[tq], w=[tq])
    S.op("dve", lambda e: e.tensor_scalar(out=ki, in0=qf, scalar1=col[:, 32:33], scalar2=None, op0=ALU.mult), r=[tq, t_col], w=[tq])
    S.op("dve", lambda e: e.tensor_single_scalar(out=ki, in_=ki, scalar=63, op=ALU.bitwise_and), r=[tq], w=[tq])
    S.op("dve", lambda e: e.tensor_scalar(out=kci, in0=ki, scalar1=16, scalar2=None, op0=ALU.add), r=[tq], w=[tq])
    S.op("dve", lambda e: e.tensor_single_scalar(out=kci, in_=kci, scalar=63, op=ALU.bitwise_and), r=[tq], w=[tq])
    S.op("act", lambda e: e.activation(out=BS, in_=ki, func=AF.Sin, scale=-2 * PI / 64, bias=k.cst[:, 4:5]), r=[tq, k.t_c], w=[k.t_Am[1]])
    S.op("act", lambda e: e.activation(out=BC, in_=kci, func=AF.Sin, scale=-2 * PI / 64, bias=k.cst[:, 4:5]), r=[tq, k.t_c], w=[k.t_Am[0]])
    for M_, tM in ((BC, k.t_Am[0]), (BS, k.t_Am[1])):
        S.op("pool", lambda e, M_=M_: e.memset(M_[0:64, 64:128], 0.0), r=[tM], w=[tM])
        S.op("pool", lambda e, M_=M_: e.memset(M_[64:128, 0:64], 0.0), r=[tM], w=[tM])
    if k.sub == "M1a":
        k.dump("BCS", sc["Am"][:, 0, :, :], [128, 2, 128], BF16, k.t_Am)
        return
    wz, t_wz = k.load_w(k.w1_d[:, 1536:1792].rearrange("(k p) n -> p k n", p=128))
    zT = HB[:, 2:4, :]
    zc = HB[:, 4:6, :].rearrange("p a n -> p (a n)")[:, 0:4096].rearrange("p (i c) -> p i c", c=256)
    zs = HB[:, 6:8, :].rearrange("p a n -> p (a n)")[:, 0:4096].rearrange("p (i c) -> p i c", c=256)
    for m in range(2):
        for (t0, nt) in LATB:
            pb, tb = proj_block(k, wz, t_wz, m * 128, 128, t0, nt)
            S.op("act", lambda e, pb=pb, m=m, t0=t0, nt=nt: e.activation(out=zT[:, m, t0:t0 + nt], in_=pb[:, 0:nt], func=AF.Copy), r=[tb], w=[t_hb[2 + m]])
    if k.sub == "M1z":
        k.dump("zT", HB[:, 2:4, :], [128, 2, N], BF16, [t_hb[2], t_hb[3]])
        return
    for a in range(16):
        tok = (a + 2) * 128
        pb, tb = k.bank()
        for m in range(2):
            S.op("pe", lambda e, pb=pb, m=m, tok=tok: e.matmul(pb[:, m * 128:(m + 1) * 128], lhsT=zT[:, m, tok:tok + 128], rhs=BC[:, :], start=True, stop=True),
                 r=[t_hb[2 + m], k.t_Am[0]], w=[tb])
            S.op("pe", lambda e, pb=pb, m=m, tok=tok: e.matmul(pb[:, 256 + m * 128:256 + (m + 1) * 128], lhsT=zT[:, m, tok:tok + 128], rhs=BS[:, :], start=True, stop=True),
                 r=[t_hb[2 + m], k.t_Am[1]], w=[tb])
        S.op("act", lambda e, pb=pb, a=a: e.activation(out=zc[:, a, :], in_=pb[:, 0:256], func=AF.Copy), r=[tb], w=[t_hb[4], t_hb[5]])
        if k.sub != "M1d":
            S.op("act", lambda e, pb=pb, a=a: e.activation(out=zs[:, a, :], in_=pb[:, 256:512], func=AF.Copy, scale=-1.0), r=[tb], w=[t_hb[6], t_hb[7]])
        if k.sub in ("M1c", "M1d") and a == 0:
            k.dump("zc", HB[:, 4:6, :], [128, 2, N], BF16, [t_hb[4], t_hb[5]])
            return
    S.barrier()
    if k.sub == "M1b":
        k.dump("zc", HB[:, 4:6, :], [128, 2, N], BF16, [t_hb[4], t_hb[5]])
        return
    fidx = RB[:, 0, 0:2048]
    fi_i = RB[:, 1, 0:2048].bitcast(I32)
    S.op("pool", lambda e: e.iota(fi_i, pattern=[[1, 2048]], base=0, channel_multiplier=0), w=[t_rb[1]])
    S.op("dve", lambda e: e.tensor_copy(out=fidx, in_=fi_i), r=[t_rb[1]], w=[t_rb[0]])
    tabs = []
    for q in range(2):
        rowb = RB[:, 2 + q, :].bitcast(BF16)
        tabs.append((rowb[:, 0:2048], rowb[:, 2048:4096], t_rb[2 + q]))
    kib = [RB[:, 4, 0:2048].bitcast(I32), RB[:, 5, 0:2048].bitcast(I32)]
    banks = [(k.PS[i // 2][:, (i % 2) * 512:(i % 2 + 1) * 512], k.PT[i // 2][i % 2]) for i in range(8)]
    for a in range(16):
        Cb, Sb, t_tab = tabs[a % 2]
        S.op("dve", lambda e, a=a: e.tensor_scalar(out=kib[0], in0=fidx, scalar1=col[:, 40 + a:41 + a], scalar2=None, op0=ALU.mult),
             r=[t_rb[0], t_col], w=[t_rb[4]])
        S.op("dve", lambda e: e.tensor_single_scalar(out=kib[0], in_=kib[0], scalar=2047, op=ALU.bitwise_and), r=[t_rb[4]], w=[t_rb[4]])
        S.op("dve", lambda e: e.tensor_scalar(out=kib[1], in0=kib[0], scalar1=512, scalar2=None, op0=ALU.add), r=[t_rb[4]], w=[t_rb[5]])
        S.op("dve", lambda e: e.tensor_single_scalar(out=kib[1], in_=kib[1], scalar=2047, op=ALU.bitwise_and), r=[t_rb[5]], w=[t_rb[5]])
        S.op("act", lambda e, Sb=Sb: e.activation(out=Sb, in_=kib[0], func=AF.Sin, scale=-2 * PI / 2048, bias=k.cst[:, 4:5]), r=[t_rb[4], k.t_c], w=[t_tab])
        S.op("act", lambda e, Cb=Cb: e.activation(out=Cb, in_=kib[1], func=AF.Sin, scale=-2 * PI / 2048, bias=k.cst[:, 4:5]), r=[t_rb[5], k.t_c], w=[t_tab])
        for m in range(2):
            for fb in range(4):
                pbk, tbk = banks[m * 4 + fb]
                S.op("pe", lambda e, pbk=pbk, a=a, m=m, fb=fb, Cb=Cb: e.matmul(pbk[:, :], lhsT=zc[:, a, m * 128:(m + 1) * 128], rhs=Cb[:, fb * 512:(fb + 1) * 512],
                                                                            start=(a == 0), stop=False), r=[t_hb[4], t_hb[5], t_tab], w=[tbk])
                S.op("pe", lambda e, pbk=pbk, a=a, m=m, fb=fb, Sb=Sb: e.matmul(pbk[:, :], lhsT=zs[:, a, m * 128:(m + 1) * 128], rhs=Sb[:, fb * 512:(fb + 1) * 512],
                                                                            start=False, stop=(a == 15)), r=[t_hb[6], t_hb[7], t_tab], w=[tbk])
    fsc = 1.0 / math.sqrt(2048.0 * 64.0)
    for m in range(2):
        for fb in range(4):
            pbk, tbk = banks[m * 4 + fb]
            S.op("act", lambda e, pbk=pbk, m=m, fb=fb: e.activation(out=HB[:, m, fb * 512:(fb + 1) * 512], in_=pbk[:, :], func=AF.Copy, scale=fsc), r=[tbk], w=[t_hb[m]])
        S.dma(k.mix_d[8 + m, :, 256:2304], HB[:, m, 0:2048], r=[t_hb[m]], w=[k.t_mixd[8 + m]])
    k.dump("mixd_f", k.mix_d[8:10, :, :], [2, 128, N], BF16, k.t_mixd[8:10])
    S.barrier()
    if k.sub == "M1f":
        return

    S.op("pool", lambda e: e.memset(k.rmask[:], 1.0), w=[k.t_c])
    S.op("pool", lambda e: e.memset(k.rmask[:].rearrange("p (c l) -> p c l", l=128)[:, :, 0:1], 0.0), r=[k.t_c], w=[k.t_c])
    S.op("pool", lambda e: e.memset(k.Mf[:], 1.0), w=[k.t_c])
    S.op("pool", lambda e: e.affine_select(out=k.Mf[:], in_=k.Mf[:], pattern=[[1, 128]], compare_op=ALU.is_ge, fill=0.0,
                                           base=0, channel_multiplier=-1), r=[k.t_c], w=[k.t_c])
    S.op("pool", lambda e: e.memset(k.Mb[:], 1.0), w=[k.t_c])
    S.op("pool", lambda e: e.affine_select(out=k.Mb[:], in_=k.Mb[:], pattern=[[-1, 128]], compare_op=ALU.is_ge, fill=0.0,
                                           base=0, channel_multiplier=1), r=[k.t_c], w=[k.t_c])
    gw = k.bc[0:16, 0, 0:768].rearrange("p (r n) -> p r n", n=384)
    t_gw = k.t_bc[0]
    S.dma(gw[:, :, :], k.gw_d.rearrange("r k n -> k r n"), w=[t_gw])
    S.dma(row[0:2, 0:384], k.gb_d[:, :], w=[t_row])
    for h in range(4):
        row_to_col(k, row[0:2, h * 96:(h + 1) * 96], 2, 96, col[0:96, 2 * h:2 * h + 2], t_row, t_col)
    S.op("dve", lambda e: e.tensor_scalar(out=col[0:96, 0:8], in0=col[0:96, 0:8], scalar1=-1.0, scalar2=None, op0=ALU.mult), r=[t_col], w=[t_col])
    S.dma(row[0:1, 0:768], k.gn_d[:, :], r=[t_col], w=[t_row])
    for c in range(8):
        row_to_col(k, row[0:1, c * 96:(c + 1) * 96], 1, 96, col[0:96, 8 + c:9 + c], t_row, t_col)
    qrow, krow, A, B = RB[0:96, 0, :], RB[0:96, 1, :], RB[0:96, 2, :], RB[0:96, 3, :]
    oacc = [RB[0:96, 4, :], RB[0:96, 5, :]]
    qt, kt = HB[0:96, 0, :], HB[0:96, 1, :]
    qts, kts = [HB[0:96, 0, :], HB[0:96, 6, :]], [HB[0:96, 1, :], HB[0:96, 7, :]]
    gsil = [HB[0:96, 2, :], HB[0:96, 3, :]]
    vt = HB[:, 4:6, :].rearrange("p a n -> p (a n)")[:, 0:3456].rearrange("p (i c) -> p i c", c=192)
    for h in range(4):
        base = 384 * h
        wg, t_wg = k.load_w(k.w1_d[:, base:base + 384].rearrange("(k p) n -> p k n", p=128))
        wv, t_wvv = k.load_w(k.w1_d[:, 1824 + 192 * h:1824 + 192 * (h + 1)].rearrange("(k p) n -> p k n", p=128))
        wR, t_wR = k.load_w(k.w1_d[:, 1792:1824].rearrange("(k p) n -> p k n", p=128))

        def ev_q(pb, tb, t0, nt):
            S.op("act", lambda e: e.activation(out=qrow[:, t0:t0 + nt], in_=pb[0:96, 0:nt], func=AF.Copy, scale=96.0 ** -0.5), r=[tb], w=[t_rb[0]])
        k.proj_fm(wg, t_wg, 0, 96, ev_q)

        def ev_k(pb, tb, t0, nt):
            S.op("act", lambda e: e.activation(out=krow[:, t0:t0 + nt], in_=pb[0:96, 0:nt], func=AF.Copy), r=[tb], w=[t_rb[1]])
        k.proj_fm(wg, t_wg, 96, 96, ev_k)
        for a in range(2):
            def ev_g(pb, tb, t0, nt, a=a):
                S.op("act", lambda e: e.activation(out=B[:, t0:t0 + nt], in_=pb[0:96, 0:nt], func=AF.Sigmoid), r=[tb], w=[t_rb[3]])
                S.op("dve", lambda e: e.tensor_tensor(out=gsil[a][:, t0:t0 + nt], in0=pb[0:96, 0:nt], in1=B[:, t0:t0 + nt], op=ALU.mult),
                     r=[tb, t_rb[3]], w=[t_hb[2 + a]])
            k.proj_fm(wg, t_wg, 192 + 96 * a, 96, ev_g)

        def ev_v(pb, tb, i):
            S.op("act", lambda e: e.activation(out=vt[:, i, :], in_=pb[:, 0:192], func=AF.Copy), r=[tb], w=[t_hb[4], t_hb[5]])
        k.proj_tm(wv, t_wvv, 0, 192, ev_v)

        def make_logf(d, h=h, wR=wR, t_wR=t_wR):
            for (t0, nt) in TOKB:
                pr, tr = proj_block(k, wR, t_wR, d * 16, 16, t0, nt)
                S.op("act", lambda e, pr=pr, t0=t0, nt=nt: e.activation(out=B[0:16, t0:t0 + nt], in_=pr[0:16, 0:nt], func=AF.Copy), r=[tr], w=[t_rb[3]])
                pz, tz = k.bank()
                S.op("pe", lambda e, pz=pz, t0=t0, nt=nt: e.matmul(pz[0:96, 0:nt], lhsT=gw[0:16, d, h * 96:(h + 1) * 96], rhs=B[0:16, t0:t0 + nt],
                                                                   start=True, stop=True), r=[t_gw, t_rb[3]], w=[tz])
                S.op("act", lambda e, pz=pz, t0=t0, nt=nt: e.activation(out=A[:, t0:t0 + nt], in_=pz[0:96, 0:nt], func=AF.Exp, scale=-1.0,
                                                                        bias=col[0:96, 2 * h + d:2 * h + d + 1]), r=[tz, t_col], w=[t_rb[2]])
            S.op("act", lambda e: e.activation(out=A, in_=A, func=AF.Ln, bias=k.cst[0:96, 1:2]), r=[t_rb[2], k.t_c], w=[t_rb[2]])
            S.op("dve", lambda e: e.tensor_scalar(out=A, in0=A, scalar1=-1.0 / 16.0, scalar2=None, op0=ALU.mult), r=[t_rb[2]], w=[t_rb[2]])
        gated_scan(k, 96, 96, 2, qrow, t_rb[0], krow, t_rb[1], A, t_rb[2], B, t_rb[3], make_logf,
                   lambda i: vt[:, i, :], [t_hb[4], t_hb[5]], oacc, [t_rb[4], t_rb[5]], qts, [t_hb[0], t_hb[6]], kts, [t_hb[1], t_hb[7]], L=128)
        if h == 0:
            k.dump("gla_o0", RB[0:96, 4:6, :], [96, 2, N], F32, [t_rb[4], t_rb[5]])
        rms_gate_out(k, 96, 2, oacc, [t_rb[4], t_rb[5]], A, t_rb[2], B, t_rb[3], gsil, [t_hb[2], t_hb[3]],
                     [col[0:96, 8 + 2 * h:9 + 2 * h], col[0:96, 9 + 2 * h:10 + 2 * h]], t_col, [qt, kt], [t_hb[0], t_hb[1]], [2 * h, 2 * h + 1])
    k.dump("mixd1", k.mix_d[0:10, :, :], [10, 128, N], BF16, k.t_mixd[0:10])


_NC_CACHE = {}


def _f32(a):
    return np.ascontiguousarray(np.asarray(a, dtype=np.float32))


def kernel(x, c, ctx, c_ctx, w_ada, b_ada, ln_g, ln_b, w_in_even, attn_sink, hgrn_lb_logits, hgrn_norm,
           w_out_even, w_in_odd, gla_gate_w, gla_gate_b, gla_norm, w_out_odd, w_router, b_router,
           w_expert_gate, w_expert_up, w_expert_down):
    x = _f32(x); c = _f32(c); ctx = _f32(ctx); c_ctx = _f32(c_ctx)
    w0a = np.ascontiguousarray(_f32(w_in_even)[0][:, _cols0()])
    w1a = np.ascontiguousarray(_f32(w_in_odd)[0][:, _cols1()])
    shared = {
        "w_ada": _f32(w_ada), "b_ada": _f32(b_ada), "ln_g": _f32(ln_g), "ln_b": _f32(ln_b),
        "w0a": w0a, "attn_sink": _f32(attn_sink), "lb_logits": _f32(hgrn_lb_logits), "hgrn_norm": _f32(hgrn_norm),
        "w_out_even": _f32(w_out_even)[0], "w1a": w1a, "gla_gate_w": _f32(gla_gate_w)[0], "gla_gate_b": _f32(gla_gate_b)[0],
        "gla_norm": _f32(gla_norm), "w_out_odd": _f32(w_out_odd)[0], "w_router": _f32(w_router),
        "b_router": _f32(b_router)[None, :], "w_expert_gate": _f32(w_expert_gate), "w_expert_up": _f32(w_expert_up),
        "w_expert_down": _f32(w_expert_down),
    }
    nb = x.shape[0]
    in_maps = []
    for b in range(nb):
        m = dict(shared)
        m["x"] = np.ascontiguousarray(x[b])
        m["ctx"] = np.ascontiguousarray(ctx[b])
        m["cvec"] = np.ascontiguousarray(np.stack([c[b], c_ctx], 0))
        in_maps.append(m)
    if "nc" not in _NC_CACHE:
        _NC_CACHE["nc"] = build()
    res = run_bass_kernel_spmd(_NC_CACHE["nc"], in_maps, core_ids=list(range(nb)))
    return np.stack([np.asarray(r["out"], dtype=np.float32) for r in res.results], 0)
```

```python
import contextlib
import math
import numpy as np
import concourse.bass as bass
import concourse.mybir as mybir
from concourse.bass_utils import run_bass_kernel_spmd

F32 = mybir.dt.float32
BF16 = mybir.dt.bfloat16
I32 = mybir.dt.int32
AF = mybir.ActivationFunctionType
ALU = mybir.AluOpType
AX = mybir.AxisListType

ENG = ("pe", "act", "dve", "pool", "sp")


class Trk:
    __slots__ = ("w", "rs", "dsem", "dcnt", "name")

    def __init__(self, name=""):
        self.w = None
        self.rs = []
        self.dsem = None
        self.dcnt = 0
        self.name = name


class Sched:
    SEM_CHUNK = 20000

    def __init__(self, nc):
        self.nc = nc
        self.ops = {e: [] for e in ENG}
        self.waited = {e: {} for e in ENG}
        self.stack = contextlib.ExitStack()
        self.nsem = 0
        self.dma_ev = {}

    def sbuf(self, name, shape, dt):
        return self.stack.enter_context(self.nc.sbuf_tensor(name, list(shape), dt))

    def psum(self, name, shape, dt=F32):
        return self.stack.enter_context(self.nc.psum_tensor(name, list(shape), dt))

    def new_sem(self, name):
        self.nsem += 1
        return self.stack.enter_context(self.nc.semaphore(f"{name}_{self.nsem}"))

    def _filter(self, engine, deps):
        waits = []
        wd = self.waited[engine]
        for ev in deps:
            if ev[0] == "e":
                _, f, idx = ev
                if engine == "pe" and f == "pe":
                    continue
                if idx <= wd.get(f, -1):
                    continue
                wd[f] = idx
                self.ops[f][idx][2] = True
                waits.append(ev)
            else:
                _, sem, val = ev
                k = id(sem)
                if val <= wd.get(k, 0):
                    continue
                wd[k] = val
                waits.append(ev)
        return waits

    def _deps(self, engine, r, w):
        deps = []
        for t in r:
            if t.w is not None:
                deps.append(t.w)
        for t in w:
            if t.w is not None:
                deps.append(t.w)
            deps.extend(t.rs)
        return self._filter(engine, deps)

    def _post(self, ev, r, w):
        for t in w:
            t.w = ev
            t.rs = []
        for t in r:
            if t in w:
                continue
            if ev[0] == "e":
                t.rs = [x for x in t.rs if not (x[0] == "e" and x[1] == ev[1])]
            else:
                t.rs = [x for x in t.rs if not (x[0] == "d" and x[1] is ev[1])]
            t.rs.append(ev)

    def op(self, engine, fn, r=(), w=()):
        r = list(r)
        w = list(w)
        waits = self._deps(engine, r, w)
        idx = len(self.ops[engine])
        self.ops[engine].append([fn, waits, False, None])
        self._post(("e", engine, idx), r, w)

    def dma(self, out, in_, r=(), w=(), q="sp", **kw):
        r = list(r)
        w = list(w)
        waits = self._deps(q, r, w)
        t0 = w[0]
        if t0.dsem is None or t0.dcnt > 60000:
            t0.dsem = self.new_sem("d")
            t0.dcnt = 0
        t0.dcnt += 16
        ev = ("d", t0.dsem, t0.dcnt)
        self.dma_ev[id(t0.dsem)] = ev

        def fn(eng, out=out, in_=in_, kw=kw):
            return eng.dma_start(out=out, in_=in_, **kw)
        self.ops[q].append([fn, waits, False, t0.dsem])
        self._post(ev, r, w)

    def barrier(self):
        last = {}
        for f in ("pe", "act", "dve", "pool"):
            j = len(self.ops[f]) - 1
            while j >= 0 and (self.ops[f][j][0] is None or self.ops[f][j][3] is not None):
                j -= 1
            last[f] = j
        dm = list(self.dma_ev.values())
        for e in ENG:
            deps = [("e", f, last[f]) for f in ("pe", "act", "dve", "pool") if f != e and last[f] >= 0]
            deps += dm
            waits = self._filter(e, deps)
            self.ops[e].append([None, waits, False, None])

    def wait_all(self, engine, trks):
        waits = self._deps(engine, list(trks), [])
        self.ops[engine].append([None, waits, False, None])

    def emit(self):
        nc = self.nc
        cum = {}
        sems = {}
        for e in ENG:
            c = 0
            arr = []
            for rec in self.ops[e]:
                if rec[2]:
                    c += 1
                arr.append(c)
            cum[e] = arr
            sems[e] = [self.new_sem(f"s{e}") for _ in range(c // self.SEM_CHUNK + 1)]
        CH = self.SEM_CHUNK

        def semval(f, idx):
            c = cum[f][idx]
            ch = (c - 1) // CH
            return sems[f][ch], c - ch * CH

        def run(e, eng):
            for i, (fn, waits, sig, dsem) in enumerate(self.ops[e]):
                for ev in waits:
                    if ev[0] == "e":
                        s, v = semval(ev[1], ev[2])
                        eng.wait_ge(s, v)
                    else:
                        eng.wait_ge(ev[1], ev[2])
                if fn is None:
                    continue
                ins = fn(eng)
                if dsem is not None:
                    ins.then_inc(dsem, 16)
                elif sig:
                    s, v = semval(e, i)
                    ins.then_inc(s, 1)

        with nc.Block() as block:
            @block.tensor
            def _(eng):
                run("pe", eng)

            @block.scalar
            def _(eng):
                run("act", eng)

            @block.vector
            def _(eng):
                run("dve", eng)

            @block.gpsimd
            def _(eng):
                run("pool", eng)

            @block.sync
            def _(eng):
                run("sp", eng)

    def close(self):
        self.stack.close()


N = 2304
NT = 18
D = 1024
KC = 8
NLAT = 2048
ALPHA = 4.0 ** 0.25
EPS = 1e-5
TOKB = [(0, 512), (512, 512), (1024, 512), (1536, 512), (2048, 256)]
PI = math.pi

_SW = list(range(16, 32)) + list(range(0, 16)) + list(range(48, 64)) + list(range(32, 48))


def _cols0():
    cols = []
    for j in range(2):
        for blk in range(2):
            for hh in (4 * j + 2 * blk, 4 * j + 2 * blk + 1):
                cols += [hh * 64 + d for d in range(64)]
        for blk in range(2):
            for hh in (4 * j + 2 * blk, 4 * j + 2 * blk + 1):
                cols += [hh * 64 + d for d in _SW]
        cols += [512 + j * 64 + d for d in range(64)] * 2
        cols += [512 + j * 64 + d for d in _SW] * 2
    cols += list(range(640, 768))
    for h in range(4):
        cols += [768 + h * 128 + d for d in range(128)]
        cols += [1280 + h * 128 + d for d in range(128)]
        cols += [1792 + h * 128 + d for d in range(128)]
        cols += [2816 + h * 128 + d for d in range(128)]
    cols += list(range(2304, 2816))
    return cols


def _cols1():
    cols = []
    for h in range(4):
        cols += [h * 96 + d for d in range(96)]
        cols += [384 + h * 96 + d for d in range(96)]
        cols += [1568 + h * 192 + d for d in range(192)]
    cols += list(range(2336, 2592))
    cols += list(range(1536, 1568))
    cols += list(range(768, 1536))
    return cols


class K:
    pass


def build(dbg=(), stop=None):
    nc = bass.Bass("TRN2", target_bir_lowering=False)
    S = Sched(nc)
    k = K()
    k.nc = nc
    k.S = S
    k.dbg = set(dbg)
    k.sub = stop
    k.dbg_out = []

    def din(name, shape, dt=F32):
        return nc.dram_tensor(name, list(shape), dt, kind="ExternalInput").ap()

    x_d = din("x", [NLAT, D])
    ctx_d = din("ctx", [256, D])
    cv_d = din("cvec", [2, D])
    wada_d = din("w_ada", [2, D, 6 * D])
    bada_d = din("b_ada", [2, 6 * D])
    lng_d = din("ln_g", [2, 2, D])
    lnb_d = din("ln_b", [2, 2, D])
    w0_d = din("w0a", [D, 4224])
    sink_d = din("attn_sink", [1, 8])
    lbl_d = din("lb_logits", [2, 2, 512])
    hn_d = din("hgrn_norm", [1, 512])
    wo0_d = din("w_out_even", [D, D])
    w1_d = din("w1a", [D, 2592])
    gw_d = din("gla_gate_w", [2, 16, 384])
    gb_d = din("gla_gate_b", [2, 384])
    gn_d = din("gla_norm", [1, 768])
    wo1_d = din("w_out_odd", [D, D])
    wr_d = din("w_router", [D, 16])
    br_d = din("b_router", [1, 16])
    eg_d = din("w_expert_gate", [2, 16, D, 512])
    eu_d = din("w_expert_up", [2, 16, D, 512])
    ed_d = din("w_expert_down", [2, 16, 512, D])
    out_d = nc.dram_tensor("out", [NLAT, D], F32, kind="ExternalOutput").ap()
    hmid_d = nc.dram_tensor("hmid", [N, D], F32).ap()
    hout0_d = nc.dram_tensor("hout0", [N, D], F32).ap()
    mix_d = nc.dram_tensor("mixd", [10, 128, N], BF16).ap()
    t_hmid = [Trk() for _ in range(NT)]
    t_hout0 = [Trk() for _ in range(NT)]
    t_mixd = [Trk() for _ in range(10)]
    t_out = [Trk() for _ in range(16)]

    def dump(name, src_ap, shape, dt, r):
        if name not in k.dbg:
            return
        d = nc.dram_tensor("dbg_" + name, list(shape), dt, kind="ExternalOutput").ap()
        t = Trk()
        S.dma(d, src_ap, r=r, w=[t])
        k.dbg_out.append(t)

    PS = [S.psum(f"ps{i}", [128, 1024]) for i in range(4)]
    PT = [[Trk(), Trk()] for _ in range(4)]
    k.bank_i = 0
    k.pair_i = 0

    k.reserved = set()

    def bank():
        i = k.bank_i
        while i in k.reserved:
            i = (i + 1) % 8
        k.bank_i = (i + 1) % 8
        k.last_bank = i
        return PS[i // 2][:, (i % 2) * 512:(i % 2 + 1) * 512], PT[i // 2][i % 2]

    def pair():
        i = k.pair_i
        k.pair_i = (i + 1) % 4
        k.bank_i = (2 * i + 2) % 8
        return PS[i], PT[i]

    identF = S.sbuf("identF", [128, 128], F32); t_c = Trk()
    identB = S.sbuf("identB", [128, 128], BF16)
    onesF = S.sbuf("onesF", [128, 128], F32)
    onesB = S.sbuf("onesB", [128, 128], BF16)
    cst = S.sbuf("cst", [128, 8], F32)
    Mf = S.sbuf("Mf", [128, 128], BF16)
    Mb = S.sbuf("Mb", [128, 128], BF16)
    MP = S.sbuf("MP", [128, 512], BF16)
    MN = S.sbuf("MN", [128, 512], BF16)
    rmask = S.sbuf("rmask", [128, N], BF16)
    modT = S.sbuf("modT", [128, 2, 48, 2], F32); t_modT = Trk()
    mod_d = nc.dram_tensor("mod_d", [2, 2, 6 * D], F32).ap(); t_mod = Trk()
    bc = S.sbuf("bc", [128, 4, D], F32); t_bc = [Trk() for _ in range(4)]
    uT = S.sbuf("uT", [128, KC, N], BF16); t_uT = [Trk() for _ in range(NT)]
    wbuf = S.sbuf("wbuf", [128, 5, 4096], BF16); t_wb = [Trk() for _ in range(5)]
    RB = S.sbuf("RB", [128, 6, N], F32); t_rb = [Trk() for _ in range(6)]
    HB = S.sbuf("HB", [128, 8, N], BF16); t_hb = [Trk() for _ in range(8)]
    sm = S.sbuf("sm", [128, 640], F32)
    gates = S.sbuf("gates", [128, NT, 16], F32); t_gates = [Trk() for _ in range(NT)]
    wrt = S.sbuf("wrt", [128, KC, 16], F32); t_wr = Trk()
    brb = S.sbuf("brb", [128, 16], F32)

    S.op("pool", lambda e: e.memset(identF[:], 0.0), w=[t_c])
    S.op("pool", lambda e: e.affine_select(out=identF[:], in_=identF[:], pattern=[[-1, 128]], compare_op=ALU.not_equal,
                                           fill=1.0, base=0, channel_multiplier=1), r=[t_c], w=[t_c])
    S.op("pool", lambda e: e.tensor_copy(out=identB[:], in_=identF[:]), r=[t_c], w=[t_c])
    S.op("pool", lambda e: e.memset(onesF[:], 1.0), w=[t_c])
    S.op("pool", lambda e: e.memset(onesB[:], 1.0), w=[t_c])
    for j_, v_ in enumerate((EPS, 1.0, -PI, 0.0, PI)):
        S.op("pool", lambda e, j_=j_, v_=v_: e.memset(cst[:, j_:j_ + 1], v_), w=[t_c])
    S.op("pool", lambda e: e.memset(Mf[:], 1.0), w=[t_c])
    S.op("pool", lambda e: e.affine_select(out=Mf[:], in_=Mf[:], pattern=[[1, 128]], compare_op=ALU.is_ge, fill=0.0,
                                           base=0, channel_multiplier=-1), r=[t_c], w=[t_c])
    S.op("pool", lambda e: e.memset(Mf[0:64, 64:128], 0.0), r=[t_c], w=[t_c])
    S.op("pool", lambda e: e.memset(Mb[:], 1.0), w=[t_c])
    S.op("pool", lambda e: e.affine_select(out=Mb[:], in_=Mb[:], pattern=[[-1, 128]], compare_op=ALU.is_ge, fill=0.0,
                                           base=0, channel_multiplier=1), r=[t_c], w=[t_c])
    S.op("pool", lambda e: e.memset(Mb[64:128, 0:64], 0.0), r=[t_c], w=[t_c])
    S.op("pool", lambda e: e.memset(MP[:], 1.0), w=[t_c])
    S.op("pool", lambda e: e.affine_select(out=MP[:], in_=MP[:], pattern=[[0, 4], [-1, 128]], compare_op=ALU.is_ge,
                                           fill=0.0, base=0, channel_multiplier=1), r=[t_c], w=[t_c])
    S.op("pool", lambda e: e.memset(MN[:], 1.0), w=[t_c])
    S.op("pool", lambda e: e.affine_select(out=MN[:], in_=MN[:], pattern=[[0, 4], [1, 128]], compare_op=ALU.is_ge,
                                           fill=0.0, base=0, channel_multiplier=-1), r=[t_c], w=[t_c])
    S.op("pool", lambda e: e.memset(rmask[:], 1.0), w=[t_c])
    S.op("pool", lambda e: e.memset(rmask[:].rearrange("p (c l) -> p c l", l=64)[:, :, 0:1], 0.0), r=[t_c], w=[t_c])
    S.dma(wrt[:], wr_d.rearrange("(k p) n -> p k n", p=128), w=[t_wr])
    S.dma(brb[:], br_d[0:1, :].to_broadcast([128, 16]), w=[t_c])
    S.barrier()

    cs = bc[0:2, 0, 0:1024]
    sg_ = bc[0:2, 1, 0:1024]
    csT = sm[:, 520:536].rearrange("p (k r) -> p k r", r=2)
    t_cs = Trk(); t_csT = Trk()
    k.t_bb = [[Trk(), Trk()], [Trk(), Trk()]]
    S.dma(cs, cv_d[:, :], w=[t_cs])
    S.op("act", lambda e: e.activation(out=sg_, in_=cs, func=AF.Sigmoid), r=[t_cs], w=[t_csT])
    S.op("dve", lambda e: e.tensor_tensor(out=cs, in0=cs, in1=sg_, op=ALU.mult), r=[t_cs, t_csT], w=[t_cs])
    pb, tb = bank()
    for kk in range(KC):
        S.op("pe", lambda e, kk=kk, pb=pb: e.transpose(out=pb[:, 2 * kk:2 * kk + 2], in_=cs[:, kk * 128:(kk + 1) * 128],
                                                       identity=identF[0:2, 0:2]), r=[t_cs, t_c], w=[tb])
    S.op("dve", lambda e, pb=pb: e.tensor_copy(out=csT, in_=pb[:, 0:16].rearrange("p (k r) -> p k r", r=2)), r=[tb], w=[t_csT])
    t_wa = [Trk(), Trk()]
    t_mb = [Trk(), Trk()]
    k.t_modTg = [[Trk() for _ in range(6)] for _ in range(2)]
    mbuf = bc[0:2, 2, :]

    def ada_block(l, j):
        slot = j % 2
        wa = RB[:, 4 + slot, 0:2048].rearrange("p (k n) -> p k n", n=256)
        mb = mbuf[:, slot * 256:(slot + 1) * 256]
        bb = mbuf[:, 512 + slot * 256:512 + (slot + 1) * 256]
        S.dma(wa, wada_d[l, :, j * 256:(j + 1) * 256].rearrange("(k p) n -> p k n", p=128), w=[t_wa[slot]])
        for r_ in range(2):
            S.dma(mbuf[r_:r_ + 1, 512 + slot * 256:512 + (slot + 1) * 256], bada_d[l:l + 1, j * 256:(j + 1) * 256], w=[k.t_bb[slot][r_]])
        pb, tb = bank()
        for kk in range(KC):
            S.op("pe", lambda e, kk=kk: e.matmul(pb[0:2, 0:256], lhsT=csT[:, kk, :], rhs=wa[:, kk, :], start=(kk == 0), stop=(kk == KC - 1)),
                 r=[t_csT, t_wa[slot]], w=[tb])
        S.op("dve", lambda e: e.tensor_tensor(out=mb, in0=pb[0:2, 0:256], in1=bb, op=ALU.add), r=[tb] + k.t_bb[slot], w=[t_mb[slot]])
        which = j // 4
        if which in (1, 4):
            S.op("dve", lambda e: e.tensor_scalar(out=mb, in0=mb, scalar1=1.0, scalar2=None, op0=ALU.add), r=[t_mb[slot]], w=[t_mb[slot]])
        S.dma(mod_d[l, :, j * 256:(j + 1) * 256], mb, r=[t_mb[slot]], w=[t_mod])
        pT, tT = bank()
        for q_ in range(2):
            S.op("pe", lambda e, q_=q_: e.transpose(out=pT[:, 2 * q_:2 * q_ + 2], in_=mb[:, q_ * 128:(q_ + 1) * 128], identity=identF[0:2, 0:2]),
                 r=[t_mb[slot], t_c], w=[tT])
        S.op("dve", lambda e: e.tensor_copy(out=modT[:, l, 2 * j:2 * j + 2, :], in_=pT[:, 0:4].rearrange("p (j r) -> p j r", r=2)),
             r=[tT], w=[k.t_modTg[l][which]])
    k.ada_blocks = [(l, j) for l in range(2) for j in range(24)]
    k.ada_block = ada_block
    for _ in range(8):
        ada_block(*k.ada_blocks.pop(0))

    def bcast_rows(l, which):
        gi = 2 if which == "mix" else 5
        li = 0 if which == "mix" else 1
        S.dma(bc[:, 0, :], mod_d[l, 0:1, gi * D:(gi + 1) * D].to_broadcast([128, D]), r=[t_mod], w=[t_bc[0]])
        S.dma(bc[:, 1, :], mod_d[l, 1:2, gi * D:(gi + 1) * D].to_broadcast([128, D]), r=[t_mod], w=[t_bc[1]])
        S.dma(bc[:, 2, :], lng_d[l, li:li + 1, :].to_broadcast([128, D]), w=[t_bc[2]])
        S.dma(bc[:, 3, :], lnb_d[l, li:li + 1, :].to_broadcast([128, D]), w=[t_bc[3]])

    def ln_stats(src, t_src, st_ap, mv_ap, rs_ap, nb_ap, t_st):
        for j in range(2):
            S.op("dve", lambda e, j=j: e.bn_stats(out=st_ap[:, j, :], in_=src[:, j * 512:(j + 1) * 512]), r=[t_src], w=[t_st])
        S.op("dve", lambda e: e.bn_aggr(out=mv_ap, in_=st_ap), r=[t_st], w=[t_st])
        S.op("act", lambda e: e.activation(out=rs_ap, in_=mv_ap[:, 1:2], func=AF.Sqrt, bias=cst[:, 0:1]), r=[t_st, t_c], w=[t_st])
        S.op("dve", lambda e: e.reciprocal(out=rs_ap, in_=rs_ap), r=[t_st], w=[t_st])
        S.op("dve", lambda e: e.tensor_scalar(out=nb_ap, in0=mv_ap[:, 0:1], scalar1=rs_ap, scalar2=-1.0, op0=ALU.mult, op1=ALU.mult),
             r=[t_st], w=[t_st])

    k.st_i = 0

    def st_slot():
        i = k.st_i
        k.st_i = (i + 1) % 8
        base = i * 24
        return (sm[:, base:base + 12].rearrange("p (a b) -> p a b", b=6), sm[:, base + 12:base + 14],
                sm[:, base + 14:base + 15], sm[:, base + 15:base + 16], k.t_st[i])
    k.t_st = [Trk() for _ in range(8)]

    def ln_part1(src, t_src, xn, t_xn):
        st_ap, mv_ap, rs_ap, nb_ap, t_st = st_slot()
        ln_stats(src, t_src, st_ap, mv_ap, rs_ap, nb_ap, t_st)
        S.op("act", lambda e: e.activation(out=xn, in_=src, func=AF.Identity, scale=rs_ap, bias=nb_ap), r=[t_src, t_st], w=[t_xn])

    def ln_part2(l, xn, t_xn, i, which, router, uf=None, t_uf=None):
        r_ = 1 if i < 2 else 0
        pp, tp = pair()
        for kk in range(KC):
            S.op("pe", lambda e, kk=kk: e.transpose(out=pp[:, kk * 128:(kk + 1) * 128], in_=xn[:, kk * 128:(kk + 1) * 128],
                                                    identity=identF[:]), r=[t_xn, t_c], w=[tp[kk // 4]])
        for kk in range(KC):
            sc_ap = modT[:, l, (which + 1) * 8 + kk, r_:r_ + 1]
            sh_ap = modT[:, l, which * 8 + kk, r_:r_ + 1]
            if router:
                o_ap = uf[:, kk, :]
                tw = t_uf
            else:
                o_ap = uT[:, kk, i * 128:(i + 1) * 128]
                tw = t_uT[i]
            if kk % 2 == 0:
                S.op("act", lambda e, kk=kk, o_ap=o_ap, sc_ap=sc_ap, sh_ap=sh_ap: e.activation(
                    out=o_ap, in_=pp[:, kk * 128:(kk + 1) * 128], func=AF.Identity, scale=sc_ap, bias=sh_ap),
                    r=[tp[kk // 4], k.t_modTg[l][which], k.t_modTg[l][which + 1]], w=[tw])
            else:
                S.op("dve", lambda e, kk=kk, o_ap=o_ap, sc_ap=sc_ap, sh_ap=sh_ap: e.tensor_scalar(
                    out=o_ap, in0=pp[:, kk * 128:(kk + 1) * 128], scalar1=sc_ap, scalar2=sh_ap, op0=ALU.mult, op1=ALU.add),
                    r=[tp[kk // 4], k.t_modTg[l][which], k.t_modTg[l][which + 1]], w=[tw])
        if router:
            S.op("pool", lambda e: e.tensor_copy(out=uT[:, :, i * 128:(i + 1) * 128], in_=uf[:, :, :]), r=[t_uf], w=[t_uT[i]])

    def ln_to_uT(l, src, t_src, xn, t_xn, i, which, router, uf=None, t_uf=None):
        ln_part1(src, t_src, xn, t_xn)
        ln_part2(l, xn, t_xn, i, which, router, uf, t_uf)
        if router:
            route(i, uf, t_uf)

    def route(i, uf, t_uf):
        pb, tb = bank()
        for kk in range(KC):
            S.op("pe", lambda e, kk=kk: e.matmul(pb[:, 0:16], lhsT=uf[:, kk, :], rhs=wrt[:, kk, :], start=(kk == 0),
                                                 stop=(kk == KC - 1)), r=[t_uf, t_wr], w=[tb])
        S.op("act", lambda e: e.activation(out=gates[:, i, :], in_=pb[:, 0:16], func=AF.Copy), r=[tb], w=[t_gates[i]])

    def route_all(t0, nt, scr, t_scr):
        n16 = nt * 16
        o = [0]

        def take(n):
            a = scr[:, o[0]:o[0] + n]
            o[0] += n
            return a
        lg = gates[:, t0:t0 + nt, :]
        pr, sl, s2, eq, eq2 = [take(n16).rearrange("p (t e) -> p t e", e=16) for _ in range(5)]
        g4, g4b, g4c = [take(nt * 4).rearrange("p (t g) -> p t g", g=4) for _ in range(3)]
        sc1, sc2 = take(nt), take(nt)
        BIG = 1.0e4
        tg = t_gates[t0:t0 + nt]

        def dv(fn):
            S.op("dve", fn, r=t_scr + [k.t_c] + tg, w=t_scr)

        def bc16(a):
            return a.unsqueeze(2).to_broadcast([128, nt, 16])

        def g4v(a):
            return a.rearrange("p t (g e) -> p (t g) e", e=4)

        def g4f(a):
            return a.rearrange("p t g -> p (t g)")
        dv(lambda e: e.tensor_reduce(out=sc1, in_=lg, axis=AX.X, op=ALU.max))
        dv(lambda e: e.tensor_tensor(out=pr, in0=lg, in1=bc16(sc1), op=ALU.subtract))
        S.op("act", lambda e: e.activation(out=pr, in_=pr, func=AF.Exp), r=t_scr, w=t_scr)
        dv(lambda e: e.tensor_reduce(out=sc2, in_=pr, axis=AX.X, op=ALU.add))
        dv(lambda e: e.reciprocal(out=sc2, in_=sc2))
        dv(lambda e: e.tensor_tensor(out=pr, in0=pr, in1=bc16(sc2), op=ALU.mult))
        dv(lambda e: e.tensor_tensor(out=sl, in0=pr, in1=brb[:].unsqueeze(1).to_broadcast([128, nt, 16]), op=ALU.add))
        dv(lambda e: e.tensor_reduce(out=g4f(g4), in_=g4v(sl), axis=AX.X, op=ALU.max))
        dv(lambda e: e.tensor_tensor(out=g4v(eq), in0=g4v(sl), in1=g4f(g4).unsqueeze(2).to_broadcast([128, nt * 4, 4]), op=ALU.is_equal))
        dv(lambda e: e.scalar_tensor_tensor(out=s2.rearrange("p t e -> p (t e)"), in0=eq.rearrange("p t e -> p (t e)"), scalar=-BIG,
                                            in1=sl.rearrange("p t e -> p (t e)"), op0=ALU.mult, op1=ALU.add))
        dv(lambda e: e.tensor_reduce(out=g4f(g4b), in_=g4v(s2), axis=AX.X, op=ALU.max))
        dv(lambda e: e.tensor_tensor(out=g4f(g4), in0=g4f(g4), in1=g4f(g4b), op=ALU.add))
        dv(lambda e: e.tensor_reduce(out=sc1, in_=g4, axis=AX.X, op=ALU.max))
        dv(lambda e: e.tensor_tensor(out=g4c, in0=g4, in1=sc1.unsqueeze(2).to_broadcast([128, nt, 4]), op=ALU.is_equal))
        dv(lambda e: e.tensor_scalar(out=g4f(g4c), in0=g4f(g4c), scalar1=-1.0, scalar2=BIG, op0=ALU.add, op1=ALU.mult))
        dv(lambda e: e.tensor_tensor(out=g4v(s2), in0=g4v(sl), in1=g4f(g4c).unsqueeze(2).to_broadcast([128, nt * 4, 4]), op=ALU.add))
        dv(lambda e: e.tensor_reduce(out=sc1, in_=s2, axis=AX.X, op=ALU.max))
        dv(lambda e: e.tensor_tensor(out=eq, in0=s2, in1=bc16(sc1), op=ALU.is_equal))
        dv(lambda e: e.scalar_tensor_tensor(out=s2.rearrange("p t e -> p (t e)"), in0=eq.rearrange("p t e -> p (t e)"), scalar=-BIG,
                                            in1=s2.rearrange("p t e -> p (t e)"), op0=ALU.mult, op1=ALU.add))
        dv(lambda e: e.tensor_reduce(out=sc1, in_=s2, axis=AX.X, op=ALU.max))
        dv(lambda e: e.tensor_tensor(out=eq2, in0=s2, in1=bc16(sc1), op=ALU.is_equal))
        dv(lambda e: e.tensor_tensor(out=eq, in0=eq, in1=eq2, op=ALU.add))
        dv(lambda e: e.tensor_tensor(out=eq, in0=eq, in1=pr, op=ALU.mult))
        dv(lambda e: e.tensor_reduce(out=sc2, in_=eq, axis=AX.X, op=ALU.add))
        dv(lambda e: e.reciprocal(out=sc2, in_=sc2))
        S.op("dve", lambda e: e.tensor_tensor(out=lg, in0=eq, in1=bc16(sc2), op=ALU.mult), r=t_scr, w=tg)
    k.route_all = route_all
    k.t_route = [Trk(), Trk()]
    k.t_tile = [Trk() for _ in range(12)]

    k.wslot = 0

    def load_w(src_ap, nslots=1, parts=128):
        s0 = k.wslot
        if s0 + nslots > 5:
            s0 = 0
        k.wslot = (s0 + nslots) % 5
        a, b = src_ap.shape[1], src_ap.shape[2]
        dst = wbuf[0:parts, s0:s0 + nslots, :].rearrange("p s n -> p (s n)")[:, 0:a * b].rearrange("p (a b) -> p a b", b=b)
        trks = t_wb[s0:s0 + nslots]
        S.dma(dst, src_ap, w=trks, q="pool")
        return dst, trks

    def proj_fm(wv, t_w, c0, M, evac, toks=TOKB):
        for (t0, nt) in toks:
            pb, tb = bank()
            for kk in range(KC):
                S.op("pe", lambda e, kk=kk, pb=pb, t0=t0, nt=nt: e.matmul(pb[0:M, 0:nt], lhsT=wv[:, kk, c0:c0 + M],
                                                                          rhs=uT[:, kk, t0:t0 + nt], start=(kk == 0), stop=(kk == KC - 1)),
                     r=t_w + t_uT[t0 // 128:(t0 + nt) // 128], w=[tb])
            evac(pb, tb, t0, nt)

    def proj_tm(wv, t_w, c0, ncol, evac, tiles=range(NT)):
        for i in tiles:
            pb, tb = bank()
            for kk in range(KC):
                S.op("pe", lambda e, kk=kk, pb=pb, i=i: e.matmul(pb[:, 0:ncol], lhsT=uT[:, kk, i * 128:(i + 1) * 128],
                                                                 rhs=wv[:, kk, c0:c0 + ncol], start=(kk == 0), stop=(kk == KC - 1)),
                     r=t_w + [t_uT[i]], w=[tb])
            evac(pb, tb, i)

    k.proj_fm = proj_fm
    k.proj_tm = proj_tm
    k.load_w = load_w
    k.bank = bank
    k.pair = pair
    k.dump = dump
    k.ln_to_uT = ln_to_uT
    k.ln_part1 = ln_part1
    k.ln_part2 = ln_part2
    k.route = route
    k.ln_stats = ln_stats
    k.st_slot = st_slot
    k.bcast_rows = bcast_rows
    for nm in ("x_d ctx_d w0_d sink_d lbl_d hn_d wo0_d w1_d gw_d gb_d gn_d wo1_d eg_d eu_d ed_d out_d hmid_d hout0_d mix_d "
               "t_hmid t_hout0 t_mixd t_out identF identB onesF onesB cst Mf Mb MP MN rmask modT t_modT mod_d t_mod bc t_bc uT t_uT "
               "wbuf t_wb RB t_rb HB t_hb sm gates t_gates t_c PS PT").split():
        setattr(k, nm, locals()[nm])

    for ph, l in [("B", 0), ("M", 0), ("D", 0), ("E", 0), ("B", 1), ("M", 1), ("D", 1), ("E", 1)]:
        if ph == "B":
            phase_B(k, l)
        elif ph == "M":
            (mixer_even if l == 0 else mixer_odd)(k)
        elif ph == "D":
            phase_D(k, l)
        else:
            phase_E(k, l)
        S.barrier()
        if stop is not None and stop[0:2] == f"{ph}{l}":
            break

    S.wait_all("sp", t_out + k.dbg_out)
    S.emit()
    S.close()
    return nc


def phase_B(k, l):
    S = k.S
    bufs = {}

    def stA(i):
        slot = i % 2
        ht = k.RB[:, slot, 0:1024]
        t_ht = k.t_tile[slot]
        xn = k.RB[:, 2 + slot, 0:1024]
        t_xn = k.t_tile[2 + slot]
        if l == 0:
            src = k.ctx_d[i * 128:(i + 1) * 128, :] if i < 2 else k.x_d[(i - 2) * 128:(i - 1) * 128, :]
            S.dma(ht, src, w=[t_ht])
        else:
            S.dma(ht, k.hout0_d[i * 128:(i + 1) * 128, :], r=[k.t_hout0[i]], w=[t_ht])
        k.ln_part1(ht, t_ht, xn, t_xn)
        bufs[i] = (xn, t_xn)

    def stB(i):
        xn, t_xn = bufs.pop(i)
        k.ln_part2(l, xn, t_xn, i, 0, False)
    for n in range(NT + 1):
        if n < NT:
            stA(n)
        if n >= 1:
            stB(n - 1)
        if l == 0:
            for _ in range(3):
                if k.ada_blocks:
                    k.ada_block(*k.ada_blocks.pop(0))
    while l == 0 and k.ada_blocks:
        k.ada_block(*k.ada_blocks.pop(0))
    if l == 0:
        for l_ in range(2):
            k.dump(f"mod{l_}", k.mod_d[l_, :, :], [2, 6 * D], F32, [k.t_mod])
    k.dump(f"uT{l}", k.uT[:, :, :], [128, KC, N], BF16, k.t_uT)


def row_to_col(k, src, nr, n, dst, t_src, t_dst):
    S = k.S
    pb, tb = k.bank()
    S.op("pe", lambda e: e.transpose(out=pb[0:n, 0:nr], in_=src, identity=k.identF[0:nr, 0:nr]), r=[t_src, k.t_c], w=[tb])
    S.op("dve", lambda e: e.tensor_copy(out=dst, in_=pb[0:n, 0:nr]), r=[tb], w=[t_dst])


def proj_block(k, wv, t_w, c0, M, t0, nt):
    S = k.S
    pb, tb = k.bank()
    for kk in range(KC):
        S.op("pe", lambda e, kk=kk: e.matmul(pb[0:M, 0:nt], lhsT=wv[:, kk, c0:c0 + M], rhs=k.uT[:, kk, t0:t0 + nt],
                                             start=(kk == 0), stop=(kk == KC - 1)),
             r=t_w + k.t_uT[t0 // 128:(t0 + nt) // 128], w=[tb])
    return pb, tb


def gated_scan(k, dk, dvh, nh, q_ap, t_q, k_ap, t_k, A, t_A, B, t_B, make_logf, v_fn, t_v, o_acc, t_o, qts, t_qts, kts, t_kts, L=64):
    S = k.S
    sc = k.scn
    NC_ = N // L
    cpt = 128 // L
    dvt = dvh * nh
    B3 = B.rearrange("p (c l) -> p c l", l=L)
    for a in range(nh):
        S.op("pool", lambda e, a=a: e.memset(o_acc[a], 0.0), w=[t_o[a]])
    D_ = []
    for d in range(2):
        qt, kt, t_qt, t_kt = qts[d], kts[d], t_qts[d], t_kts[d]
        t_sc = k.t_scn[d]
        rr = sc["rr"][0:dk, d, 0:NC_]
        gg = sc["gg"][0:dk, d, 0:NC_]
        X1 = sc["X1"][0:dk, d, 0:NC_]
        X2 = sc["X2"][0:dk, d, 0:NC_]
        EG = sc["EG"][0:dk, d, 0:NC_]
        make_logf(d)
        S.op("dve", lambda e: e.tensor_tensor_scan(out=B, data0=k.rmask[0:dk, :], data1=A, initial=0.0, op0=ALU.mult, op1=ALU.add),
             r=[t_A, k.t_c], w=[t_B])
        S.op("pool", lambda e, gg=gg: e.tensor_copy(out=gg, in_=B3[:, :, L - 1]), r=[t_B], w=[t_sc])
        if d == 1:
            S.op("pool", lambda e: e.tensor_tensor(out=B, in0=B, in1=A, op=ALU.subtract), r=[t_B, t_A], w=[t_B])
        S.op("pool", lambda e, rr=rr: e.tensor_copy(out=rr, in_=B3[:, :, L // 2]), r=[t_B], w=[t_sc])
        S.op("dve", lambda e, rr=rr: e.tensor_tensor(out=B3, in0=B3, in1=rr.unsqueeze(2).to_broadcast([dk, NC_, L]), op=ALU.subtract),
             r=[t_B, t_sc], w=[t_B])
        sgn = 1.0 if d == 0 else -1.0
        S.op("act", lambda e, sgn=sgn: e.activation(out=A, in_=B, func=AF.Exp, scale=sgn), r=[t_B], w=[t_A])
        S.op("dve", lambda e, qt=qt: e.tensor_tensor(out=qt, in0=q_ap, in1=A, op=ALU.mult), r=[t_q, t_A], w=[t_qt])
        S.op("act", lambda e, sgn=sgn: e.activation(out=A, in_=B, func=AF.Exp, scale=-sgn), r=[t_B, t_qt], w=[t_A])
        S.op("dve", lambda e, kt=kt: e.tensor_tensor(out=kt, in0=k_ap, in1=A, op=ALU.mult), r=[t_k, t_A], w=[t_kt])
        S.op("act", lambda e, X1=X1, rr=rr: e.activation(out=X1, in_=rr, func=AF.Exp), r=[t_sc], w=[t_sc])
        S.op("act", lambda e, EG=EG, gg=gg: e.activation(out=EG, in_=gg, func=AF.Exp), r=[t_sc], w=[t_sc])
        S.op("dve", lambda e, X2=X2, gg=gg, rr=rr: e.tensor_tensor(out=X2, in0=gg, in1=rr, op=ALU.subtract), r=[t_sc], w=[t_sc])
        S.op("act", lambda e, X2=X2: e.activation(out=X2, in_=X2, func=AF.Exp), r=[t_sc], w=[t_sc])
        a_s, c_s = (X1, X2) if d == 0 else (X2, X1)
        fo = tuple(range(cpt))
        bo = tuple(reversed(range(cpt)))
        if d == 0:
            tiles = [(i, fo) for i in range(NT)]
        else:
            tiles = [(i, bo) for i in (1, 0)] + [(i, bo) for i in range(NT - 1, 1, -1)]
        st = {"d": d, "qt": qt, "kt": kt, "t_qt": t_qt, "t_kt": t_kt, "t_sc": t_sc, "EG": EG, "a_s": a_s, "c_s": c_s,
              "M": k.Mf if d == 0 else k.Mb, "tiles": tiles, "s_i": 0, "sb_i": 0, "am_i": 0, "pend": None, "nchunk": 0, "ds_i": 0}
        S.op("pool", lambda e, d=d: e.memset(sc["Sst"][0:dk, d, 0, 0:dvt], 0.0), w=[k.t_Sst[d][0]])
        S.op("pool", lambda e, d=d: e.memset(sc["Sbf"][0:dk, d, 0, 0:dvt], 0.0), w=[k.t_Sbf[d][0]])
        D_.append(st)

    def stage1(st, i):
        d = st["d"]
        tk0 = i * 128
        vt = v_fn(i)
        am_i = st["am_i"]
        st["am_i"] = 1 - am_i
        Am = sc["Am"][:, d, am_i, :]
        ktok = sc["ktok"][:, d, am_i, 0:dk]
        t_Am = k.t_Am2[d][am_i]
        t_kk = k.t_ktok2[d][am_i]
        qt, kt = st["qt"], st["kt"]
        pA, tA = k.bank()
        S.op("pe", lambda e: e.matmul(pA[:, 0:128], lhsT=kt[:, tk0:tk0 + 128], rhs=qt[:, tk0:tk0 + 128], start=True, stop=True),
             r=[st["t_kt"], st["t_qt"]], w=[tA])
        M_ = st["M"]
        S.op("dve", lambda e: e.tensor_tensor(out=Am, in0=pA[:, 0:128], in1=M_[:], op=ALU.mult), r=[tA, k.t_c], w=[t_Am])
        pK, tK = k.bank()
        S.op("pe", lambda e: e.matmul(pK[:, 0:dk], lhsT=kt[:, tk0:tk0 + 128], rhs=k.identB[0:dk, 0:dk], start=True, stop=True),
             r=[st["t_kt"], k.t_c], w=[tK])
        S.op("act", lambda e: e.activation(out=ktok, in_=pK[:, 0:dk], func=AF.Copy), r=[tK], w=[t_kk])
        pI, tI = k.bank()
        for a in range(nh):
            S.op("pe", lambda e, a=a: e.matmul(pI[0:dvh, a * 128:(a + 1) * 128], lhsT=vt[:, a * dvh:(a + 1) * dvh], rhs=Am[:, :], start=True, stop=True),
                 r=t_v + [t_Am], w=[tI])
        pS = [None] * cpt
        c_s = st["c_s"]
        for hf in range(cpt):
            pS_, tS_ = k.bank()
            rows = slice(hf * L, hf * L + L)
            S.op("pe", lambda e, pS_=pS_, rows=rows: e.matmul(pS_[0:dk, 0:dvt], lhsT=ktok[rows, :], rhs=vt[rows, 0:dvt], start=True, stop=True),
                 r=[t_kk] + t_v, w=[tS_])
            ds_i = st["ds_i"]
            st["ds_i"] = (ds_i + 1) % 4
            dSs = sc["dSs"][0:dk, d, ds_i, 0:dvt]
            c = cpt * i + hf
            S.op("act", lambda e, pS_=pS_, dSs=dSs, c=c: e.activation(out=dSs, in_=pS_[0:dk, 0:dvt], func=AF.Identity, scale=c_s[:, c:c + 1]),
                 r=[tS_, st["t_sc"]], w=[k.t_dSs[d][ds_i]])
            pS[hf] = (dSs, k.t_dSs[d][ds_i])
        for a in range(nh):
            S.op("dve", lambda e, a=a: e.tensor_tensor(out=o_acc[a][:, tk0:tk0 + 128], in0=pI[0:dvh, a * 128:(a + 1) * 128],
                                                       in1=o_acc[a][:, tk0:tk0 + 128], op=ALU.add), r=[tI, t_o[a]], w=[t_o[a]])
        st["pend"] = (i, pS)

    def stage2_chunk(st, i, hf, pS, po, tpo, last):
        d = st["d"]
        c = cpt * i + hf
        tok0 = c * L
        qt = st["qt"]
        sb_i = st["sb_i"]
        Sb = sc["Sbf"][0:dk, d, sb_i, 0:dvt]
        for a in range(nh):
            S.op("pe", lambda e, a=a: e.matmul(po[0:dvh, a * 128 + hf * L:a * 128 + hf * L + L], lhsT=Sb[:, a * dvh:(a + 1) * dvh],
                                               rhs=qt[:, tok0:tok0 + L], start=True, stop=True), r=[k.t_Sbf[d][sb_i], st["t_qt"]], w=[tpo])
        if last:
            return
        s_i = st["s_i"]
        Sc = sc["Sst"][0:dk, d, s_i, 0:dvt]
        Sn = sc["Sst"][0:dk, d, 1 - s_i, 0:dvt]
        EG, a_s = st["EG"], st["a_s"]
        dSs, t_dS = pS[hf]
        S.op("dve", lambda e: e.scalar_tensor_tensor(out=Sn, in0=Sc, scalar=EG[:, c:c + 1], in1=dSs, op0=ALU.mult, op1=ALU.add),
             r=[k.t_Sst[d][s_i], t_dS, st["t_sc"]], w=[k.t_Sst[d][1 - s_i]])
        st["s_i"] = 1 - s_i
        n_ = st["nchunk"] + 1
        ti, hfo = st["tiles"][n_ // cpt]
        cn = cpt * ti + hfo[n_ % cpt]
        Sbn = sc["Sbf"][0:dk, d, 1 - sb_i, 0:dvt]
        S.op("act", lambda e: e.activation(out=Sbn, in_=Sn, func=AF.Identity, scale=a_s[:, cn:cn + 1]),
             r=[k.t_Sst[d][1 - s_i], st["t_sc"]], w=[k.t_Sbf[d][1 - sb_i]])
        st["sb_i"] = 1 - sb_i

    nT = NT
    for step in range(nT + 1):
        if step < nT:
            for st in D_:
                stage1(st, st["tiles"][step][0])
        if step >= 1:
            pend = []
            for st in D_:
                i, hfo = st["tiles"][step - 1]
                po, tpo = k.bank()
                pend.append((st, i, hfo, po, tpo))
            for which in range(cpt):
                for (st, i, hfs, po, tpo) in pend:
                    pS = st["pS_prev"]
                    last = (st["nchunk"] == cpt * nT - 1)
                    stage2_chunk(st, i, hfs[which], pS, po, tpo, last)
                    st["nchunk"] += 1
            for (st, i, hfs, po, tpo) in pend:
                tk0 = i * 128
                for a in range(nh):
                    S.op("dve", lambda e, a=a, po=po, tk0=tk0: e.tensor_tensor(out=o_acc[a][:, tk0:tk0 + 128], in0=po[0:dvh, a * 128:(a + 1) * 128],
                                                                               in1=o_acc[a][:, tk0:tk0 + 128], op=ALU.add), r=[tpo, t_o[a]], w=[t_o[a]])
        for st in D_:
            if st["pend"] is not None:
                st["pS_prev"] = st["pend"][1]


def rms_gate_out(k, dvh, nh, o_acc, t_o, A, t_A, B, t_B, gsil, t_gs, gn_cols, t_gn, mixrow, t_mix, chunk_ids):
    S = k.S
    dv = dvh * nh
    for (t0, nt) in TOKB:
        pb, tb = k.bank()
        for a in range(nh):
            S.op("act", lambda e, a=a, t0=t0, nt=nt: e.activation(out=A[0:dvh, a * 512:a * 512 + nt], in_=o_acc[a][:, t0:t0 + nt], func=AF.Square),
                 r=[t_o[a]], w=[t_A])
        for a in range(nh):
            S.op("pe", lambda e, a=a, pb=pb, nt=nt: e.matmul(pb[0:dvh, 0:nt], lhsT=k.onesF[0:dvh, 0:dvh], rhs=A[0:dvh, a * 512:a * 512 + nt],
                                                             start=(a == 0), stop=(a == nh - 1)), r=[t_A, k.t_c], w=[tb])
        S.op("act", lambda e, pb=pb, nt=nt: e.activation(out=B[0:dvh, 0:nt], in_=pb[0:dvh, 0:nt], func=AF.Sqrt, scale=1.0 / dv, bias=k.cst[0:dvh, 0:1]),
             r=[tb, k.t_c], w=[t_B])
        S.op("dve", lambda e, nt=nt: e.reciprocal(out=B[0:dvh, 0:nt], in_=B[0:dvh, 0:nt]), r=[t_B], w=[t_B])
        for a in range(nh):
            S.op("dve", lambda e, a=a, t0=t0, nt=nt: e.tensor_tensor(out=B[0:dvh, 512 + a * 512:512 + a * 512 + nt], in0=o_acc[a][:, t0:t0 + nt],
                                                                     in1=B[0:dvh, 0:nt], op=ALU.mult), r=[t_o[a], t_B], w=[t_B])
            S.op("dve", lambda e, a=a, t0=t0, nt=nt: e.scalar_tensor_tensor(out=mixrow[a][0:dvh, t0:t0 + nt], in0=B[0:dvh, 512 + a * 512:512 + a * 512 + nt],
                                                                            scalar=gn_cols[a], in1=gsil[a][0:dvh, t0:t0 + nt], op0=ALU.mult, op1=ALU.mult),
                 r=[t_B, t_gn, t_gs[a]], w=[t_mix[a]])
    for a in range(nh):
        S.dma(k.mix_d[chunk_ids[a], 0:dvh, :], mixrow[a][0:dvh, :], r=[t_mix[a]], w=[k.t_mixd[chunk_ids[a]]])


def scan_setup(k):
    S = k.S
    if hasattr(k, "scn"):
        return
    sc = {}
    for nm in ("rr", "gg", "X1", "X2", "EG"):
        sc[nm] = S.sbuf("sc_" + nm, [128, 2, 36], F32)
    sc["Sst"] = S.sbuf("sc_Sst", [128, 2, 2, 192], F32)
    sc["dSs"] = S.sbuf("sc_dSs", [128, 2, 4, 192], BF16)
    sc["Sbf"] = S.sbuf("sc_Sbf", [128, 2, 2, 192], BF16)
    sc["Am"] = S.sbuf("sc_Am", [128, 2, 2, 128], BF16)
    sc["ktok"] = S.sbuf("sc_ktok", [128, 2, 2, 128], BF16)
    sc["col"] = S.sbuf("sc_col", [128, 64], F32)
    sc["row"] = k.RB[0:8, 5, 0:768]
    k.scn = sc
    k.t_scn = [Trk(), Trk()]
    k.t_Sst = [[Trk(), Trk()], [Trk(), Trk()]]
    k.t_dSs = [[Trk() for _ in range(4)], [Trk() for _ in range(4)]]
    k.t_Sbf = [[Trk(), Trk()], [Trk(), Trk()]]
    k.t_Am2 = [[Trk(), Trk()], [Trk(), Trk()]]
    k.t_ktok2 = [[Trk(), Trk()], [Trk(), Trk()]]
    k.t_Am = [k.t_Am2[0][0], k.t_Am2[0][1]]
    k.t_col = Trk()
    k.t_row = k.t_rb[5]


ATOK = [(0, 256), (256, 512), (768, 512), (1280, 512), (1792, 512)]


def mixer_even(k):
    S = k.S
    scan_setup(k)
    sc = k.scn
    RB, HB, t_rb, t_hb = k.RB, k.HB, k.t_rb, k.t_hb
    Ct = RB[:, 4, 0:2048]
    St = RB[:, 5, 0:2048]
    col = sc["col"]
    t_col = k.t_col
    ci = col[:, 0:8].bitcast(I32)
    S.op("pool", lambda e: e.iota(ci[:, 0:1], pattern=[[0, 1]], base=0, channel_multiplier=1), w=[t_col])
    S.op("dve", lambda e: e.tensor_single_scalar(out=ci[:, 1:2], in_=ci[:, 0:1], scalar=15, op=ALU.bitwise_and), r=[t_col], w=[t_col])
    S.op("dve", lambda e: e.tensor_scalar(out=ci[:, 2:3], in0=ci[:, 0:1], scalar1=5, scalar2=1, op0=ALU.logical_shift_right, op1=ALU.bitwise_and),
         r=[t_col], w=[t_col])
    S.op("dve", lambda e: e.tensor_scalar(out=ci[:, 3:4], in0=ci[:, 0:1], scalar1=4, scalar2=1, op0=ALU.logical_shift_right, op1=ALU.bitwise_and),
         r=[t_col], w=[t_col])
    S.op("dve", lambda e: e.tensor_copy(out=col[:, 8:11], in_=ci[:, 1:4]), r=[t_col], w=[t_col])
    S.op("act", lambda e: e.activation(out=col[:, 11:12], in_=col[:, 8:9], func=AF.Exp, scale=-math.log(10000.0) / 16.0), r=[t_col], w=[t_col])
    S.op("dve", lambda e: e.tensor_tensor(out=col[:, 13:14], in0=col[:, 11:12], in1=col[:, 9:10], op=ALU.mult), r=[t_col], w=[t_col])
    S.op("dve", lambda e: e.tensor_tensor(out=col[:, 12:13], in0=col[:, 11:12], in1=col[:, 13:14], op=ALU.subtract), r=[t_col], w=[t_col])
    S.op("dve", lambda e: e.tensor_scalar(out=col[:, 14:15], in0=col[:, 10:11], scalar1=2.0, scalar2=-1.0, op0=ALU.mult, op1=ALU.add),
         r=[t_col], w=[t_col])
    ri = RB[:, 0, 0:2048].bitcast(I32)
    qi = RB[:, 1, 0:2048].bitcast(I32)
    S.op("pool", lambda e: e.iota(ri, pattern=[[1, 32], [0, 64]], base=0, channel_multiplier=0), w=[t_rb[0]])
    S.op("pool", lambda e: e.iota(qi, pattern=[[0, 32], [1, 64]], base=0, channel_multiplier=0), w=[t_rb[1]])
    rf = RB[:, 2, 0:2048]
    qf = RB[:, 3, 0:2048]
    S.op("dve", lambda e: e.tensor_copy(out=rf, in_=ri), r=[t_rb[0]], w=[t_rb[2]])
    S.op("dve", lambda e: e.tensor_copy(out=qf, in_=qi), r=[t_rb[1]], w=[t_rb[3]])
    ang = RB[:, 0, 0:2048]
    S.op("dve", lambda e: e.tensor_scalar(out=ang, in0=rf, scalar1=col[:, 12:13], scalar2=None, op0=ALU.mult), r=[t_rb[2], t_col], w=[t_rb[0]])
    S.op("dve", lambda e: e.scalar_tensor_tensor(out=ang, in0=qf, scalar=col[:, 13:14], in1=ang, op0=ALU.mult, op1=ALU.add),
         r=[t_rb[3], t_rb[0], t_col], w=[t_rb[0]])
    def range_reduce(dst, t_dst, add, tmpi, t_tmpi, tmpf_, t_tmpf):
        S.op("dve", lambda e: e.tensor_scalar(out=tmpi, in0=ang, scalar1=add, scalar2=1.0 / (2 * PI), op0=ALU.add, op1=ALU.mult),
             r=[t_rb[0]], w=[t_tmpi])
        S.op("dve", lambda e: e.tensor_copy(out=tmpf_, in_=tmpi), r=[t_tmpi], w=[t_tmpf])
        S.op("dve", lambda e: e.scalar_tensor_tensor(out=dst, in0=tmpf_, scalar=-2 * PI, in1=ang, op0=ALU.mult, op1=ALU.add),
             r=[t_tmpf, t_rb[0]], w=[t_dst])
        if add != 0.0:
            S.op("dve", lambda e: e.tensor_scalar(out=dst, in0=dst, scalar1=add, scalar2=None, op0=ALU.add), r=[t_dst], w=[t_dst])
        S.op("dve", lambda e: e.tensor_scalar(out=tmpf_, in0=dst, scalar1=PI, scalar2=-2 * PI, op0=ALU.is_gt, op1=ALU.mult),
             r=[t_dst], w=[t_tmpf])
        S.op("dve", lambda e: e.tensor_tensor(out=dst, in0=dst, in1=tmpf_, op=ALU.add), r=[t_dst, t_tmpf], w=[t_dst])
        S.op("dve", lambda e: e.tensor_scalar(out=tmpf_, in0=dst, scalar1=-PI, scalar2=2 * PI, op0=ALU.is_lt, op1=ALU.mult),
             r=[t_dst], w=[t_tmpf])
        S.op("dve", lambda e: e.tensor_tensor(out=dst, in0=dst, in1=tmpf_, op=ALU.add), r=[t_dst, t_tmpf], w=[t_dst])
        S.op("dve", lambda e: e.tensor_scalar(out=dst, in0=dst, scalar1=PI, scalar2=-PI, op0=ALU.min, op1=ALU.max), r=[t_dst], w=[t_dst])
    m1 = RB[:, 1, 0:2048]
    tmpi = RB[:, 2, 0:2048].bitcast(I32)
    tmpf_ = RB[:, 3, 0:2048]
    range_reduce(m1, t_rb[1], 0.0, tmpi, t_rb[2], tmpf_, t_rb[3])
    S.op("act", lambda e: e.activation(out=St, in_=m1, func=AF.Sin, scale=col[:, 14:15]), r=[t_rb[1], t_col], w=[t_rb[5]])
    range_reduce(m1, t_rb[1], PI / 2, tmpi, t_rb[2], tmpf_, t_rb[3])
    S.op("act", lambda e: e.activation(out=Ct, in_=m1, func=AF.Sin), r=[t_rb[1]], w=[t_rb[4]])
    k.dump("ropeC", Ct, [128, 2048], F32, [t_rb[4]])
    k.dump("ropeS", St, [128, 2048], F32, [t_rb[5]])
    if k.sub == "M0a":
        return
    S.dma(col[:, 16:24], k.sink_d[0:1, :].to_broadcast([128, 8]), w=[t_col])
    S.op("act", lambda e: e.activation(out=col[:, 16:24], in_=col[:, 16:24], func=AF.Exp), r=[t_col], w=[t_col])

    qT = HB[:, 0:2, :]
    kAB = [HB[:, 2, :], HB[:, 3, :]]
    vdup = HB[:, 4, :].rearrange("p (i c) -> p i c", c=128)
    mixA = [HB[:, 5, :], HB[:, 6, :]]
    et = [HB[:, 7, ei * 512:(ei + 1) * 512] for ei in range(4)] + [RB[:, 3, 0:256].bitcast(BF16)]
    S.op("pool", lambda e: e.memset(kAB[0][64:128, :], 0.0), w=[t_hb[2]])
    S.op("pool", lambda e: e.memset(kAB[1][0:64, :], 0.0), w=[t_hb[3]])
    t_et = [Trk() for _ in range(5)]
    k.et_i = 0
    tmpf = [RB[:, 0, 0:512], RB[:, 0, 512:1024], RB[:, 1, 0:512], RB[:, 1, 512:1024]]
    t_tmp = [Trk() for _ in range(4)]
    dn = RB[:, 2, 0:512]
    t_dn = t_rb[2]
    wv_v, t_wv = k.load_w(k.w0_d[:, 1536:1664].rearrange("(k p) n -> p k n", p=128))
    for j in range(2):
        base = j * 768
        wq, t_wq = k.load_w(k.w0_d[:, base:base + 512].rearrange("(k p) n -> p k n", p=128))
        wk, t_wk = k.load_w(k.w0_d[:, base + 512:base + 768].rearrange("(k p) n -> p k n", p=128))
        tmp_i = 0
        for (t0, nt) in ATOK:
            for blk in range(3):
                if blk < 2:
                    pq, tq = proj_block(k, wq, t_wq, blk * 128, 128, t0, nt)
                    dst = qT[:, blk, t0:t0 + nt]
                    tdst = t_hb[blk]
                else:
                    pq, tq = proj_block(k, wk, t_wk, 0, 128, t0, nt)
                    dst = None
                if t0 == 0:
                    if blk < 2:
                        S.op("act", lambda e, pq=pq, dst=dst, nt=nt: e.activation(out=dst, in_=pq[:, 0:nt], func=AF.Copy), r=[tq], w=[tdst])
                    else:
                        S.op("act", lambda e, pq=pq, nt=nt, t0=t0: e.activation(out=kAB[0][0:64, t0:t0 + nt], in_=pq[0:64, 0:nt], func=AF.Copy), r=[tq], w=[t_hb[2]])
                        S.op("act", lambda e, pq=pq, nt=nt, t0=t0: e.activation(out=kAB[1][64:128, t0:t0 + nt], in_=pq[64:128, 0:nt], func=AF.Copy), r=[tq], w=[t_hb[3]])
                    continue
                if blk < 2:
                    ps_, ts_ = proj_block(k, wq, t_wq, 256 + blk * 128, 128, t0, nt)
                else:
                    ps_, ts_ = proj_block(k, wk, t_wk, 128, 128, t0, nt)
                l0 = t0 - 256
                ta, tb_ = tmp_i % 4, (tmp_i + 1) % 4
                tmp_i += 2
                S.op("dve", lambda e, pq=pq, ta=ta, l0=l0, nt=nt: e.tensor_tensor(out=tmpf[ta][:, 0:nt], in0=pq[:, 0:nt], in1=Ct[:, l0:l0 + nt], op=ALU.mult),
                     r=[tq, t_rb[4]], w=[t_tmp[ta]])
                S.op("dve", lambda e, ps_=ps_, tb_=tb_, l0=l0, nt=nt: e.tensor_tensor(out=tmpf[tb_][:, 0:nt], in0=ps_[:, 0:nt], in1=St[:, l0:l0 + nt], op=ALU.mult),
                     r=[ts_, t_rb[5]], w=[t_tmp[tb_]])
                if blk < 2:
                    S.op("pool", lambda e, dst=dst, ta=ta, tb_=tb_, nt=nt: e.tensor_tensor(out=dst, in0=tmpf[ta][:, 0:nt], in1=tmpf[tb_][:, 0:nt], op=ALU.add),
                         r=[t_tmp[ta], t_tmp[tb_]], w=[tdst])
                else:
                    S.op("pool", lambda e, ta=ta, tb_=tb_, nt=nt, t0=t0: e.tensor_tensor(out=kAB[0][0:64, t0:t0 + nt], in0=tmpf[ta][0:64, 0:nt], in1=tmpf[tb_][0:64, 0:nt], op=ALU.add),
                         r=[t_tmp[ta], t_tmp[tb_]], w=[t_hb[2]])
                    S.op("pool", lambda e, ta=ta, tb_=tb_, nt=nt, t0=t0: e.tensor_tensor(out=kAB[1][64:128, t0:t0 + nt], in0=tmpf[ta][64:128, 0:nt], in1=tmpf[tb_][64:128, 0:nt], op=ALU.add),
                         r=[t_tmp[ta], t_tmp[tb_]], w=[t_hb[3]])

        if k.sub == "M0p":
            k.dump("qT0", qT, [128, 2, N], BF16, [t_hb[0], t_hb[1]])
            return

        def ev_v(pb, tb, i, j=j):
            S.op("act", lambda e: e.activation(out=vdup[:, i, 0:64], in_=pb[:, j * 64:(j + 1) * 64], func=AF.Copy), r=[tb], w=[t_hb[4]])
            S.op("dve", lambda e: e.tensor_copy(out=vdup[:, i, 64:128], in_=pb[:, j * 64:(j + 1) * 64]), r=[tb], w=[t_hb[4]])
        k.proj_tm(wv_v, t_wv, 0, 128, ev_v)
        if j == 0:
            k.dump("qT0", qT, [128, 2, N], BF16, [t_hb[0], t_hb[1]])
            k.dump("kT0", HB[:, 2:4, :], [128, 2, N], BF16, [t_hb[2], t_hb[3]])
        if k.sub == "M0v":
            return
        for qb in range(NT):
            q0 = qb * 128
            if (k.sub == "M0q1" and qb == 1) or (k.sub == "M0q3" and qb == 3):
                k.dump("mixA", HB[:, 5:7, :], [128, 2, N], BF16, [t_hb[5], t_hb[6]])
                return
            if qb < 2:
                chunks = [(0, None), (1, None)]
            else:
                n_ = qb - 2
                chunks = [(0, None), (1, None)]
                if n_ > 0:
                    chunks.append((qb - 1, k.MP))
                chunks.append((qb, None))
                if n_ < 15:
                    chunks.append((qb + 1, k.MN))
            po, tpo = k.bank()
            pd, tpd = k.bank()
            nch = len(chunks)
            pss_l = []
            for ci_, (kc, msk) in enumerate(chunks):
                pss, tss = k.bank()
                for hh in range(4):
                    blk, half = hh // 2, hh % 2
                    S.op("pe", lambda e, pss=pss, hh=hh, blk=blk, half=half, kc=kc, q0=q0: e.matmul(
                        pss[:, hh * 128:(hh + 1) * 128], lhsT=kAB[half][:, kc * 128:(kc + 1) * 128], rhs=qT[:, blk, q0:q0 + 128],
                        start=True, stop=True), r=[t_hb[2 + half], t_hb[blk]], w=[tss])
                pss_l.append((pss, tss))
            for ci_, (kc, msk) in enumerate(chunks):
                pss, tss = pss_l[ci_]
                ei = ci_
                S.op("act", lambda e, pss=pss, ei=ei: e.activation(out=et[ei], in_=pss[:, :], func=AF.Exp, scale=0.125), r=[tss], w=[t_et[ei]])
                if msk is not None:
                    S.op("dve", lambda e, ei=ei, msk=msk: e.tensor_tensor(out=et[ei], in0=et[ei], in1=msk[:], op=ALU.mult),
                         r=[t_et[ei], k.t_c], w=[t_et[ei]])
            for ci_, (kc, msk) in enumerate(chunks):
                ei = ci_
                S.op("pe", lambda e, ei=ei, kc=kc, ci_=ci_, po=po, nch=nch: e.matmul(
                    po[:, :], lhsT=vdup[:, kc, :], rhs=et[ei][:, :], start=(ci_ == 0), stop=(ci_ == nch - 1)),
                    r=[t_hb[4], t_et[ei]], w=[tpo])
                S.op("pe", lambda e, ei=ei, ci_=ci_, pd=pd, nch=nch: e.matmul(
                    pd[:, :], lhsT=k.onesB[:], rhs=et[ei][:, :], start=(ci_ == 0), stop=(ci_ == nch - 1)),
                    r=[k.t_c, t_et[ei]], w=[tpd])
            if k.sub == "M0qb":
                S.op("dve", lambda e, po=po: e.tensor_copy(out=RB[:, 0, 0:512], in_=po[:, :]), r=[tpo], w=[t_rb[0]])
                S.op("dve", lambda e, pd=pd: e.tensor_copy(out=RB[:, 0, 512:1024], in_=pd[:, :]), r=[tpd], w=[t_rb[0]])
                k.dump("popd", RB[:, 0, 0:1024], [128, 1024], F32, [t_rb[0]])
                return
            for hh in range(4):
                S.op("dve", lambda e, hh=hh, pd=pd, j=j: e.tensor_scalar(out=dn[:, hh * 128:(hh + 1) * 128], in0=pd[:, hh * 128:(hh + 1) * 128],
                                                                    scalar1=col[:, 16 + 4 * j + hh:17 + 4 * j + hh], scalar2=None, op0=ALU.add),
                     r=[tpd, t_col], w=[t_dn])
            S.op("dve", lambda e: e.reciprocal(out=dn, in_=dn), r=[t_dn], w=[t_dn])
            for hh in range(4):
                blk, half = hh // 2, hh % 2
                rows = slice(half * 64, half * 64 + 64)
                S.op("dve", lambda e, hh=hh, blk=blk, rows=rows, po=po, q0=q0: e.tensor_tensor(
                    out=mixA[blk][rows, q0:q0 + 128], in0=po[rows, hh * 128:(hh + 1) * 128], in1=dn[rows, hh * 128:(hh + 1) * 128], op=ALU.mult),
                    r=[tpo, t_dn], w=[t_hb[5 + blk]])
        for blk in range(2):
            S.dma(k.mix_d[2 * j + blk, :, :], mixA[blk], r=[t_hb[5 + blk]], w=[k.t_mixd[2 * j + blk]])
    k.dump("mixd_att", k.mix_d[0:4, :, :], [4, 128, N], BF16, k.t_mixd[0:4])
    S.barrier()
    if k.sub == "M0b":
        return

    row = sc["row"]
    t_row = k.t_row
    S.dma(row[0:4, 0:512], k.lbl_d.rearrange("r a n -> (r a) n"), w=[t_row])
    for h in range(4):
        row_to_col(k, row[0:4, h * 128:(h + 1) * 128], 4, 128, col[:, 24 + 4 * h:28 + 4 * h], t_row, t_col)
    lbT = col[:, 24:40].rearrange("p (h r a) -> p h r a", r=2, a=2)
    lbv = col[:, 44:52].rearrange("p (h r) -> p h r", r=2)
    omv = col[:, 52:60].rearrange("p (h r) -> p h r", r=2)
    S.op("dve", lambda e: e.tensor_tensor(out=lbv, in0=lbT[:, :, :, 0], in1=lbT[:, :, :, 1], op=ALU.subtract), r=[t_col], w=[t_col])
    S.op("act", lambda e: e.activation(out=lbv, in_=lbv, func=AF.Sigmoid), r=[t_col], w=[t_col])
    S.op("dve", lambda e: e.tensor_scalar(out=omv, in0=lbv, scalar1=-1.0, scalar2=1.0, op0=ALU.mult, op1=ALU.add), r=[t_col], w=[t_col])
    S.dma(row[0:1, 0:512], k.hn_d[:, :], w=[t_row])
    for h in range(4):
        row_to_col(k, row[0:1, h * 128:(h + 1) * 128], 1, 128, col[:, 40 + h:41 + h], t_row, t_col)

    qrow, Krow, A, B, oacc = RB[:, 0, :], RB[:, 1, :], RB[:, 2, :], RB[:, 3, :], RB[:, 4, :]
    gsil, mixrow = HB[:, 2, :], HB[:, 4, :]
    qts, kts = [HB[:, 0, :], HB[:, 5, :]], [HB[:, 1, :], HB[:, 6, :]]
    vtm = HB[:, 3, :].rearrange("p (i c) -> p i c", c=128)
    for h in range(4):
        base = 1664 + 512 * h
        wg, t_wg = k.load_w(k.w0_d[:, base:base + 512].rearrange("(k p) n -> p k n", p=128))
        wvv, t_wvv = k.load_w(k.w0_d[:, 3712 + 128 * h:3712 + 128 * (h + 1)].rearrange("(k p) n -> p k n", p=128))

        def ev_q(pb, tb, t0, nt):
            S.op("act", lambda e: e.activation(out=qrow[:, t0:t0 + nt], in_=pb[:, 0:nt], func=AF.Copy), r=[tb], w=[t_rb[0]])
        k.proj_fm(wg, t_wg, 256, 128, ev_q)

        def ev_g(pb, tb, t0, nt):
            S.op("act", lambda e: e.activation(out=B[:, t0:t0 + nt], in_=pb[:, 0:nt], func=AF.Sigmoid), r=[tb], w=[t_rb[3]])
            S.op("dve", lambda e: e.tensor_tensor(out=gsil[:, t0:t0 + nt], in0=pb[:, 0:nt], in1=B[:, t0:t0 + nt], op=ALU.mult),
                 r=[tb, t_rb[3]], w=[t_hb[2]])
        k.proj_fm(wg, t_wg, 384, 128, ev_g)

        def ev_v2(pb, tb, i):
            S.op("act", lambda e: e.activation(out=vtm[:, i, :], in_=pb[:, 0:128], func=AF.Copy), r=[tb], w=[t_hb[3]])
        k.proj_tm(wvv, t_wvv, 0, 128, ev_v2)

        def make_logf(d, h=h, wg=wg, t_wg=t_wg):
            def ev_z(pb, tb, t0, nt):
                S.op("act", lambda e: e.activation(out=A[:, t0:t0 + nt], in_=pb[:, 0:nt], func=AF.Sigmoid), r=[tb], w=[t_rb[2]])
            k.proj_fm(wg, t_wg, d * 128, 128, ev_z)
            S.op("dve", lambda e: e.tensor_scalar(out=A, in0=A, scalar1=omv[:, h, d:d + 1], scalar2=lbv[:, h, d:d + 1], op0=ALU.mult, op1=ALU.add),
                 r=[t_rb[2], t_col], w=[t_rb[2]])
            S.op("pool", lambda e: e.tensor_scalar(out=Krow, in0=A, scalar1=-1.0, scalar2=1.0, op0=ALU.mult, op1=ALU.add),
                 r=[t_rb[2]], w=[t_rb[1]])
            S.op("act", lambda e: e.activation(out=A, in_=A, func=AF.Ln), r=[t_rb[2]], w=[t_rb[2]])
        gated_scan(k, 128, 128, 1, qrow, t_rb[0], Krow, t_rb[1], A, t_rb[2], B, t_rb[3], make_logf,
                   lambda i: vtm[:, i, :], [t_hb[3]], [oacc], [t_rb[4]], qts, [t_hb[0], t_hb[5]], kts, [t_hb[1], t_hb[6]])
        if h == 0:
            k.dump("oacc0", oacc, [128, N], F32, [t_rb[4]])
        rms_gate_out(k, 128, 1, [oacc], [t_rb[4]], A, t_rb[2], B, t_rb[3], [gsil], [t_hb[2]], [col[:, 40 + h:41 + h]], t_col,
                     [mixrow], [t_hb[4]], [4 + h])
    k.dump("mixd0", k.mix_d[0:8, :, :], [8, 128, N], BF16, k.t_mixd[0:8])


def phase_D(k, l):
    S = k.S
    RB, HB = k.RB, k.HB
    k.bcast_rows(l, "mix")
    if l == 0:
        wo, t_wo = k.load_w(k.wo0_d.rearrange("(c p) n -> p c n", p=128), nslots=2)
        chunks = [(c, 128, wo, t_wo, c) for c in range(8)]
        tiles = list(range(NT))
        nch = 8
    else:
        wo, t_wo = k.load_w(k.wo1_d[0:768, :].rearrange("(c p) n -> p c n", p=96), nslots=2, parts=96)
        wf, t_wf = k.load_w(k.wo1_d[768:1024, :].rearrange("(c p) n -> p c n", p=128), nslots=1)
        chunks = [(c, 96, wo, t_wo, c) for c in range(8)] + [(8 + c, 128, wf, t_wf, c) for c in range(2)]
        tiles = list(range(2, NT))
        nch = 10
    tb_ = [RB[:, r, hf * 1024:(hf + 1) * 1024] for r in range(6) for hf in range(2)]
    tt = k.t_tile
    nchunks = len(chunks)

    def bufs_for(n_):
        s6 = (n_ % 2) * 6
        return [tb_[s6 + q] for q in range(6)], [tt[s6 + q] for q in range(6)]

    def stA(n_):
        i = tiles[n_]
        (ht, tmp, xn1, hnew, xn2, ufb), (t_ht, t_tmp, t_xn1, t_hn, t_xn2, t_uf) = bufs_for(n_)
        mt = HB[:, n_ % 2, 0:nch * 128].rearrange("p (c n) -> p c n", n=128)
        t_mt = k.t_hb[n_ % 2]
        S.dma(mt, k.mix_d[0:nch, :, i * 128:(i + 1) * 128].rearrange("c p n -> p c n"), r=k.t_mixd[0:nch], w=[t_mt])
        if l == 0:
            src = k.ctx_d[i * 128:(i + 1) * 128, :] if i < 2 else k.x_d[(i - 2) * 128:(i - 1) * 128, :]
            S.dma(ht, src, w=[t_ht])
        else:
            S.dma(ht, k.hout0_d[i * 128:(i + 1) * 128, :], r=[k.t_hout0[i]], w=[t_ht])
        pp, tp = k.pair()
        for hf in range(2):
            for ci_, (c, KR, wv, t_wv, wc) in enumerate(chunks):
                S.op("pe", lambda e, hf=hf, c=c, KR=KR, wv=wv, wc=wc, ci_=ci_: e.matmul(
                    pp[:, hf * 512:(hf + 1) * 512], lhsT=mt[0:KR, c, :], rhs=wv[0:KR, wc, hf * 512:(hf + 1) * 512],
                    start=(ci_ == 0), stop=(ci_ == nchunks - 1)), r=[t_mt] + t_wv, w=[tp[hf]])
        r_ = 1 if i < 2 else 0
        for hf in range(2):
            S.op("dve", lambda e, hf=hf: e.tensor_tensor(out=tmp[:, hf * 512:(hf + 1) * 512], in0=pp[:, hf * 512:(hf + 1) * 512],
                                                         in1=k.bc[:, r_, hf * 512:(hf + 1) * 512], op=ALU.mult),
                 r=[tp[hf], k.t_bc[r_]], w=[t_tmp])
        S.op("dve", lambda e: e.scalar_tensor_tensor(out=tmp, in0=ht, scalar=ALPHA, in1=tmp, op0=ALU.mult, op1=ALU.add),
             r=[t_ht, t_tmp], w=[t_tmp])
        st_ap, mv_ap, rs_ap, nb_ap, t_st = k.st_slot()
        k.ln_stats(tmp, t_tmp, st_ap, mv_ap, rs_ap, nb_ap, t_st)
        S.op("act", lambda e: e.activation(out=xn1, in_=tmp, func=AF.Identity, scale=rs_ap, bias=nb_ap), r=[t_tmp, t_st], w=[t_xn1])
        S.op("pool", lambda e: e.tensor_tensor(out=xn1, in0=xn1, in1=k.bc[:, 2, :], op=ALU.mult), r=[t_xn1, k.t_bc[2]], w=[t_xn1])
        S.op("pool", lambda e: e.tensor_tensor(out=hnew, in0=xn1, in1=k.bc[:, 3, :], op=ALU.add), r=[t_xn1, k.t_bc[3]], w=[t_hn])
        k.ln_part1(hnew, t_hn, xn2, t_xn2)

    def stB(n_):
        i = tiles[n_]
        (ht, tmp, xn1, hnew, xn2, ufb), (t_ht, t_tmp, t_xn1, t_hn, t_xn2, t_uf) = bufs_for(n_)
        uf = ufb.rearrange("p (k n) -> p k n", n=128)
        S.dma(k.hmid_d[i * 128:(i + 1) * 128, :], hnew, r=[t_hn], w=[k.t_hmid[i]])
        k.ln_part2(l, xn2, t_xn2, i, 3, True, uf, t_uf)

    def stC(n_):
        i = tiles[n_]
        (ht, tmp, xn1, hnew, xn2, ufb), (t_ht, t_tmp, t_xn1, t_hn, t_xn2, t_uf) = bufs_for(n_)
        uf = ufb.rearrange("p (k n) -> p k n", n=128)
        k.route(i, uf, t_uf)
    nT_ = len(tiles)
    for n in range(nT_ + 2):
        if n < nT_:
            stA(n)
        if 1 <= n <= nT_:
            stB(n - 1)
        if n >= 2:
            stC(n - 2)
    k.route_all(tiles[0], len(tiles), RB[:, 0, :], [k.t_tile[0], k.t_tile[1]])
    k.dump(f"hmid{l}", k.hmid_d[:, :], [N, D], F32, k.t_hmid)
    k.dump(f"u2T{l}", k.uT[:, :, :], [128, KC, N], BF16, k.t_uT)
    k.dump(f"gates{l}", k.gates[:, :, :], [128, NT, 16], F32, k.t_gates)


def phase_E(k, l):
    S = k.S
    RB = k.RB
    k.bcast_rows(l, "moe")
    if l == 0:
        halves = [list(range(0, 9)), list(range(9, 18))]
        bsz = 384
    else:
        halves = [list(range(2, 10)), list(range(10, 18))]
        bsz = 512
    yacc = RB[:, 0:4, :].rearrange("p a n -> p (a n)").rearrange("p (t d) -> p t d", d=1024)
    t_y = k.t_tile[0:9]
    hTb = RB[:, 4, :].bitcast(BF16)
    hT = [hTb[:, q * 2048:(q + 1) * 2048].rearrange("p (f n) -> p f n", n=512) for q in range(2)]
    t_hT = [k.t_tile[9], k.t_tile[10]]
    sg = [RB[:, 5, q * 512:(q + 1) * 512] for q in range(2)]
    t_sg = [k.t_rb[4], k.t_rb[5]]
    Hb = RB[:, 5, 1024:2048]
    t_H = k.t_tile[11]
    dst_d = k.hout0_d if l == 0 else k.out_d
    k.h_i = 0
    k.s_i = 0
    for tiles in halves:
        tok0 = tiles[0] * 128
        ntok = len(tiles) * 128
        blocks = [(tok0 + b0, bsz) for b0 in range(0, ntok, bsz)]
        for e_ in range(16):
            w1, t_w1 = k.load_w(k.eg_d[l, e_].rearrange("(c p) n -> p c n", p=128))
            w3, t_w3 = k.load_w(k.eu_d[l, e_].rearrange("(c p) n -> p c n", p=128))
            w2, t_w2 = k.load_w(k.ed_d[l, e_].rearrange("(c p) n -> p c n", p=128))
            for (b0, nb) in blocks:
                hi = k.h_i
                k.h_i = 1 - hi
                hTc = hT[hi]
                for f in range(4):
                    p1, tp1 = k.bank()
                    for kk in range(KC):
                        S.op("pe", lambda e, kk=kk, f=f, p1=p1, w1=w1, b0=b0, nb=nb: e.matmul(
                            p1[:, 0:nb], lhsT=w1[:, kk, f * 128:(f + 1) * 128], rhs=k.uT[:, kk, b0:b0 + nb], start=(kk == 0), stop=(kk == KC - 1)),
                            r=t_w1 + k.t_uT[b0 // 128:(b0 + nb) // 128], w=[tp1])
                    p3, tp3 = k.bank()
                    for kk in range(KC):
                        S.op("pe", lambda e, kk=kk, f=f, p3=p3, w3=w3, b0=b0, nb=nb: e.matmul(
                            p3[:, 0:nb], lhsT=w3[:, kk, f * 128:(f + 1) * 128], rhs=k.uT[:, kk, b0:b0 + nb], start=(kk == 0), stop=(kk == KC - 1)),
                            r=t_w3 + k.t_uT[b0 // 128:(b0 + nb) // 128], w=[tp3])
                    si = k.s_i
                    k.s_i = 1 - si
                    S.op("act", lambda e, p1=p1, si=si, nb=nb: e.activation(out=sg[si][:, 0:nb], in_=p1[:, 0:nb], func=AF.Sigmoid), r=[tp1], w=[t_sg[si]])
                    S.op("dve", lambda e, p1=p1, si=si, nb=nb: e.tensor_tensor(out=sg[si][:, 0:nb], in0=p1[:, 0:nb], in1=sg[si][:, 0:nb], op=ALU.mult),
                         r=[tp1, t_sg[si]], w=[t_sg[si]])
                    S.op("dve", lambda e, p3=p3, si=si, nb=nb, f=f, hTc=hTc: e.tensor_tensor(out=hTc[:, f, 0:nb], in0=p3[:, 0:nb], in1=sg[si][:, 0:nb], op=ALU.mult),
                         r=[tp3, t_sg[si]], w=[t_hT[hi]])
                for tl in range(nb // 128):
                    gi = (b0 // 128) + tl
                    yi = gi - tiles[0]
                    for dh in range(2):
                        py, tpy = k.bank()
                        for f in range(4):
                            S.op("pe", lambda e, f=f, py=py, hTc=hTc, tl=tl, dh=dh, w2=w2: e.matmul(
                                py[:, :], lhsT=hTc[:, f, tl * 128:(tl + 1) * 128], rhs=w2[:, f, dh * 512:(dh + 1) * 512], start=(f == 0), stop=(f == 3)),
                                r=[t_hT[hi]] + t_w2, w=[tpy])
                        ya = yacc[:, yi, dh * 512:(dh + 1) * 512]
                        gs = k.gates[:, gi, e_:e_ + 1]
                        if e_ == 0:
                            S.op("dve", lambda e, py=py, ya=ya, gs=gs: e.tensor_scalar(out=ya, in0=py[:, :], scalar1=gs, scalar2=None, op0=ALU.mult),
                                 r=[tpy, k.t_gates[gi]], w=[t_y[yi]])
                        else:
                            S.op("dve", lambda e, py=py, ya=ya, gs=gs: e.scalar_tensor_tensor(out=ya, in0=py[:, :], scalar=gs, in1=ya, op0=ALU.mult, op1=ALU.add),
                                 r=[tpy, k.t_gates[gi], t_y[yi]], w=[t_y[yi]])
        Hbs = [RB[:, 5, 1024:2048], RB[:, 5, 0:1024]]
        t_Hs = [[k.t_tile[11]], [k.t_rb[4], k.t_rb[5]]]

        def fin_load(yi):
            gi = tiles[yi]
            S.dma(Hbs[yi % 2], k.hmid_d[gi * 128:(gi + 1) * 128, :], r=[k.t_hmid[gi]], w=t_Hs[yi % 2])

        def fin_store(yi):
            gi = tiles[yi]
            yt = yacc[:, yi, :]
            if l == 0:
                S.dma(k.hout0_d[gi * 128:(gi + 1) * 128, :], yt, r=[t_y[yi]], w=[k.t_hout0[gi]])
            else:
                S.dma(k.out_d[(gi - 2) * 128:(gi - 1) * 128, :], yt, r=[t_y[yi]], w=[k.t_out[gi - 2]])
        fin_load(0)
        for yi, gi in enumerate(tiles):
            yt = yacc[:, yi, :]
            Hb = Hbs[yi % 2]
            t_H = t_Hs[yi % 2]
            r_ = 1 if gi < 2 else 0
            if l == 0 and yi == 0 and tiles[0] == 0:
                k.dump("ymoe_t0", yt, [128, D], F32, [t_y[yi]])
            if yi + 1 < len(tiles):
                fin_load(yi + 1)
            S.op("dve", lambda e, yt=yt, r_=r_: e.tensor_tensor(out=yt, in0=yt, in1=k.bc[:, r_, :], op=ALU.mult), r=[t_y[yi], k.t_bc[r_]], w=[t_y[yi]])
            S.op("dve", lambda e, yt=yt, Hb=Hb: e.scalar_tensor_tensor(out=yt, in0=Hb, scalar=ALPHA, in1=yt, op0=ALU.mult, op1=ALU.add),
                 r=t_H + [t_y[yi]], w=[t_y[yi]])
            st_ap, mv_ap, rs_ap, nb_ap, t_st = k.st_slot()
            k.ln_stats(yt, t_y[yi], st_ap, mv_ap, rs_ap, nb_ap, t_st)
            S.op("act", lambda e, yt=yt, Hb=Hb, rs_ap=rs_ap, nb_ap=nb_ap: e.activation(out=Hb, in_=yt, func=AF.Identity, scale=rs_ap, bias=nb_ap),
                 r=[t_y[yi], t_st], w=t_H)
            S.op("pool", lambda e, Hb=Hb: e.tensor_tensor(out=Hb, in0=Hb, in1=k.bc[:, 2, :], op=ALU.mult), r=t_H + [k.t_bc[2]], w=t_H)
            S.op("pool", lambda e, yt=yt, Hb=Hb: e.tensor_tensor(out=yt, in0=Hb, in1=k.bc[:, 3, :], op=ALU.add), r=t_H + [k.t_bc[3]], w=[t_y[yi]])
            if yi >= 1:
                fin_store(yi - 1)
        fin_store(len(tiles) - 1)
    if l == 0:
        k.dump("hout0", k.hout0_d[:, :], [N, D], F32, k.t_hout0)


def mixer_odd(k):
    S = k.S
    scan_setup(k)
    sc = k.scn
    RB, HB, t_rb, t_hb = k.RB, k.HB, k.t_rb, k.t_hb
    col, t_col, row, t_row = sc["col"], k.t_col, sc["row"], k.t_row
    LATB = [(256, 512), (768, 512), (1280, 512), (1792, 512)]
    BC = sc["Am"][:, 0, 0, :]
    BS = sc["Am"][:, 0, 1, :]
    ci = col[:, 0:32].bitcast(I32)
    S.op("pool", lambda e: e.iota(ci[:, 0:1], pattern=[[0, 1]], base=0, channel_multiplier=1), w=[t_col])
    S.op("dve", lambda e: e.tensor_single_scalar(out=ci[:, 1:2], in_=ci[:, 0:1], scalar=63, op=ALU.bitwise_and), r=[t_col], w=[t_col])
    S.op("dve", lambda e: e.tensor_copy(out=col[:, 32:33], in_=ci[:, 1:2]), r=[t_col], w=[t_col])
    S.op("pool", lambda e: e.iota(ci[:, 2:18], pattern=[[128, 16]], base=0, channel_multiplier=1), r=[t_col], w=[t_col])
    S.op("dve", lambda e: e.tensor_copy(out=col[:, 40:56], in_=ci[:, 2:18]), r=[t_col], w=[t_col])
    qi = RB[:, 0, 0:128].bitcast(I32)
    qf = RB[:, 0, 128:256]
    ki = RB[:, 0, 256:384].bitcast(I32)
    kci = RB[:, 0, 384:512].bitcast(I32)
    tq = t_rb[0]
    S.op("pool", lambda e: e.iota(qi, pattern=[[1, 128]], base=0, channel_multiplier=0), w=[tq])
    S.op("dve", lambda e: e.tensor_single_scalar(out=qi, in_=qi, scalar=63, op=ALU.bitwise_and), r=[tq], w=[tq])
    S.op("dve", lambda e: e.tensor_copy(out=qf, in_=qi), r=[tq], w=[tq])
    S.op("dve", lambda e: e.tensor_scalar(out=ki, in0=qf, scalar1=col[:, 32:33], scalar2=None, op0=ALU.mult), r=[tq, t_col], w=[tq])
    S.op("dve", lambda e: e.tensor_single_scalar(out=ki, in_=ki, scalar=63, op=ALU.bitwise_and), r=[tq], w=[tq])
    S.op("dve", lambda e: e.tensor_scalar(out=kci, in0=ki, scalar1=16, scalar2=None, op0=ALU.add), r=[tq], w=[tq])
    S.op("dve", lambda e: e.tensor_single_scalar(out=kci, in_=kci, scalar=63, op=ALU.bitwise_and), r=[tq], w=[tq])
    S.op("act", lambda e: e.activation(out=BS, in_=ki, func=AF.Sin, scale=-2 * PI / 64, bias=k.cst[:, 4:5]), r=[tq, k.t_c], w=[k.t_Am[1]])
    S.op("act", lambda e: e.activation(out=BC, in_=kci, func=AF.Sin, scale=-2 * PI / 64, bias=k.cst[:, 4:5]), r=[tq, k.t_c], w=[k.t_Am[0]])
    for M_, tM in ((BC, k.t_Am[0]), (BS, k.t_Am[1])):
        S.op("pool", lambda e, M_=M_: e.memset(M_[0:64, 64:128], 0.0), r=[tM], w=[tM])
        S.op("pool", lambda e, M_=M_: e.memset(M_[64:128, 0:64], 0.0), r=[tM], w=[tM])
    if k.sub == "M1a":
        k.dump("BCS", sc["Am"][:, 0, :, :], [128, 2, 128], BF16, k.t_Am)
        return
    wz, t_wz = k.load_w(k.w1_d[:, 1536:1792].rearrange("(k p) n -> p k n", p=128))
    zT = HB[:, 2:4, :]
    zc = HB[:, 4:6, :].rearrange("p a n -> p (a n)")[:, 0:4096].rearrange("p (i c) -> p i c", c=256)
    zs = HB[:, 6:8, :].rearrange("p a n -> p (a n)")[:, 0:4096].rearrange("p (i c) -> p i c", c=256)
    for m in range(2):
        for (t0, nt) in LATB:
            pb, tb = proj_block(k, wz, t_wz, m * 128, 128, t0, nt)
            S.op("act", lambda e, pb=pb, m=m, t0=t0, nt=nt: e.activation(out=zT[:, m, t0:t0 + nt], in_=pb[:, 0:nt], func=AF.Copy), r=[tb], w=[t_hb[2 + m]])
    if k.sub == "M1z":
        k.dump("zT", HB[:, 2:4, :], [128, 2, N], BF16, [t_hb[2], t_hb[3]])
        return
    for a in range(16):
        tok = (a + 2) * 128
        pb, tb = k.bank()
        for m in range(2):
            S.op("pe", lambda e, pb=pb, m=m, tok=tok: e.matmul(pb[:, m * 128:(m + 1) * 128], lhsT=zT[:, m, tok:tok + 128], rhs=BC[:, :], start=True, stop=True),
                 r=[t_hb[2 + m], k.t_Am[0]], w=[tb])
            S.op("pe", lambda e, pb=pb, m=m, tok=tok: e.matmul(pb[:, 256 + m * 128:256 + (m + 1) * 128], lhsT=zT[:, m, tok:tok + 128], rhs=BS[:, :], start=True, stop=True),
                 r=[t_hb[2 + m], k.t_Am[1]], w=[tb])
        S.op("act", lambda e, pb=pb, a=a: e.activation(out=zc[:, a, :], in_=pb[:, 0:256], func=AF.Copy), r=[tb], w=[t_hb[4], t_hb[5]])
        if k.sub != "M1d":
            S.op("act", lambda e, pb=pb, a=a: e.activation(out=zs[:, a, :], in_=pb[:, 256:512], func=AF.Copy, scale=-1.0), r=[tb], w=[t_hb[6], t_hb[7]])
        if k.sub in ("M1c", "M1d") and a == 0:
            k.dump("zc", HB[:, 4:6, :], [128, 2, N], BF16, [t_hb[4], t_hb[5]])
            return
    S.barrier()
    if k.sub == "M1b":
        k.dump("zc", HB[:, 4:6, :], [128, 2, N], BF16, [t_hb[4], t_hb[5]])
        return
    fidx = RB[:, 0, 0:2048]
    fi_i = RB[:, 1, 0:2048].bitcast(I32)
    S.op("pool", lambda e: e.iota(fi_i, pattern=[[1, 2048]], base=0, channel_multiplier=0), w=[t_rb[1]])
    S.op("dve", lambda e: e.tensor_copy(out=fidx, in_=fi_i), r=[t_rb[1]], w=[t_rb[0]])
    tabs = []
    for q in range(2):
        rowb = RB[:, 2 + q, :].bitcast(BF16)
        tabs.append((rowb[:, 0:2048], rowb[:, 2048:4096], t_rb[2 + q]))
    kib = [RB[:, 4, 0:2048].bitcast(I32), RB[:, 5, 0:2048].bitcast(I32)]
    banks = [(k.PS[i // 2][:, (i % 2) * 512:(i % 2 + 1) * 512], k.PT[i // 2][i % 2]) for i in range(8)]
    for a in range(16):
        Cb, Sb, t_tab = tabs[a % 2]
        S.op("dve", lambda e, a=a: e.tensor_scalar(out=kib[0], in0=fidx, scalar1=col[:, 40 + a:41 + a], scalar2=None, op0=ALU.mult),
             r=[t_rb[0], t_col], w=[t_rb[4]])
        S.op("dve", lambda e: e.tensor_single_scalar(out=kib[0], in_=kib[0], scalar=2047, op=ALU.bitwise_and), r=[t_rb[4]], w=[t_rb[4]])
        S.op("dve", lambda e: e.tensor_scalar(out=kib[1], in0=kib[0], scalar1=512, scalar2=None, op0=ALU.add), r=[t_rb[4]], w=[t_rb[5]])
        S.op("dve", lambda e: e.tensor_single_scalar(out=kib[1], in_=kib[1], scalar=2047, op=ALU.bitwise_and), r=[t_rb[5]], w=[t_rb[5]])
        S.op("act", lambda e, Sb=Sb: e.activation(out=Sb, in_=kib[0], func=AF.Sin, scale=-2 * PI / 2048, bias=k.cst[:, 4:5]), r=[t_rb[4], k.t_c], w=[t_tab])
        S.op("act", lambda e, Cb=Cb: e.activation(out=Cb, in_=kib[1], func=AF.Sin, scale=-2 * PI / 2048, bias=k.cst[:, 4:5]), r=[t_rb[5], k.t_c], w=[t_tab])
        for m in range(2):
            for fb in range(4):
                pbk, tbk = banks[m * 4 + fb]
                S.op("pe", lambda e, pbk=pbk, a=a, m=m, fb=fb, Cb=Cb: e.matmul(pbk[:, :], lhsT=zc[:, a, m * 128:(m + 1) * 128], rhs=Cb[:, fb * 512:(fb + 1) * 512],
                                                                            start=(a == 0), stop=False), r=[t_hb[4], t_hb[5], t_tab], w=[tbk])
                S.op("pe", lambda e, pbk=pbk, a=a, m=m, fb=fb, Sb=Sb: e.matmul(pbk[:, :], lhsT=zs[:, a, m * 128:(m + 1) * 128], rhs=Sb[:, fb * 512:(fb + 1) * 512],
                                                                            start=False, stop=(a == 15)), r=[t_hb[6], t_hb[7], t_tab], w=[tbk])
    fsc = 1.0 / math.sqrt(2048.0 * 64.0)
    for m in range(2):
        for fb in range(4):
            pbk, tbk = banks[m * 4 + fb]
            S.op("act", lambda e, pbk=pbk, m=m, fb=fb: e.activation(out=HB[:, m, fb * 512:(fb + 1) * 512], in_=pbk[:, :], func=AF.Copy, scale=fsc), r=[tbk], w=[t_hb[m]])
        S.dma(k.mix_d[8 + m, :, 256:2304], HB[:, m, 0:2048], r=[t_hb[m]], w=[k.t_mixd[8 + m]])
    k.dump("mixd_f", k.mix_d[8:10, :, :], [2, 128, N], BF16, k.t_mixd[8:10])
    S.barrier()
    if k.sub == "M1f":
        return

    S.op("pool", lambda e: e.memset(k.rmask[:], 1.0), w=[k.t_c])
    S.op("pool", lambda e: e.memset(k.rmask[:].rearrange("p (c l) -> p c l", l=128)[:, :, 0:1], 0.0), r=[k.t_c], w=[k.t_c])
    S.op("pool", lambda e: e.memset(k.Mf[:], 1.0), w=[k.t_c])
    S.op("pool", lambda e: e.affine_select(out=k.Mf[:], in_=k.Mf[:], pattern=[[1, 128]], compare_op=ALU.is_ge, fill=0.0,
                                           base=0, channel_multiplier=-1), r=[k.t_c], w=[k.t_c])
    S.op("pool", lambda e: e.memset(k.Mb[:], 1.0), w=[k.t_c])
    S.op("pool", lambda e: e.affine_select(out=k.Mb[:], in_=k.Mb[:], pattern=[[-1, 128]], compare_op=ALU.is_ge, fill=0.0,
                                           base=0, channel_multiplier=1), r=[k.t_c], w=[k.t_c])
    gw = k.bc[0:16, 0, 0:768].rearrange("p (r n) -> p r n", n=384)
    t_gw = k.t_bc[0]
    S.dma(gw[:, :, :], k.gw_d.rearrange("r k n -> k r n"), w=[t_gw])
    S.dma(row[0:2, 0:384], k.gb_d[:, :], w=[t_row])
    for h in range(4):
        row_to_col(k, row[0:2, h * 96:(h + 1) * 96], 2, 96, col[0:96, 2 * h:2 * h + 2], t_row, t_col)
    S.op("dve", lambda e: e.tensor_scalar(out=col[0:96, 0:8], in0=col[0:96, 0:8], scalar1=-1.0, scalar2=None, op0=ALU.mult), r=[t_col], w=[t_col])
    S.dma(row[0:1, 0:768], k.gn_d[:, :], r=[t_col], w=[t_row])
    for c in range(8):
        row_to_col(k, row[0:1, c * 96:(c + 1) * 96], 1, 96, col[0:96, 8 + c:9 + c], t_row, t_col)
    qrow, krow, A, B = RB[0:96, 0, :], RB[0:96, 1, :], RB[0:96, 2, :], RB[0:96, 3, :]
    oacc = [RB[0:96, 4, :], RB[0:96, 5, :]]
    qt, kt = HB[0:96, 0, :], HB[0:96, 1, :]
    qts, kts = [HB[0:96, 0, :], HB[0:96, 6, :]], [HB[0:96, 1, :], HB[0:96, 7, :]]
    gsil = [HB[0:96, 2, :], HB[0:96, 3, :]]
    vt = HB[:, 4:6, :].rearrange("p a n -> p (a n)")[:, 0:3456].rearrange("p (i c) -> p i c", c=192)
    for h in range(4):
        base = 384 * h
        wg, t_wg = k.load_w(k.w1_d[:, base:base + 384].rearrange("(k p) n -> p k n", p=128))
        wv, t_wvv = k.load_w(k.w1_d[:, 1824 + 192 * h:1824 + 192 * (h + 1)].rearrange("(k p) n -> p k n", p=128))
        wR, t_wR = k.load_w(k.w1_d[:, 1792:1824].rearrange("(k p) n -> p k n", p=128))

        def ev_q(pb, tb, t0, nt):
            S.op("act", lambda e: e.activation(out=qrow[:, t0:t0 + nt], in_=pb[0:96, 0:nt], func=AF.Copy, scale=96.0 ** -0.5), r=[tb], w=[t_rb[0]])
        k.proj_fm(wg, t_wg, 0, 96, ev_q)

        def ev_k(pb, tb, t0, nt):
            S.op("act", lambda e: e.activation(out=krow[:, t0:t0 + nt], in_=pb[0:96, 0:nt], func=AF.Copy), r=[tb], w=[t_rb[1]])
        k.proj_fm(wg, t_wg, 96, 96, ev_k)
        for a in range(2):
            def ev_g(pb, tb, t0, nt, a=a):
                S.op("act", lambda e: e.activation(out=B[:, t0:t0 + nt], in_=pb[0:96, 0:nt], func=AF.Sigmoid), r=[tb], w=[t_rb[3]])
                S.op("dve", lambda e: e.tensor_tensor(out=gsil[a][:, t0:t0 + nt], in0=pb[0:96, 0:nt], in1=B[:, t0:t0 + nt], op=ALU.mult),
                     r=[tb, t_rb[3]], w=[t_hb[2 + a]])
            k.proj_fm(wg, t_wg, 192 + 96 * a, 96, ev_g)

        def ev_v(pb, tb, i):
            S.op("act", lambda e: e.activation(out=vt[:, i, :], in_=pb[:, 0:192], func=AF.Copy), r=[tb], w=[t_hb[4], t_hb[5]])
        k.proj_tm(wv, t_wvv, 0, 192, ev_v)

        def make_logf(d, h=h, wR=wR, t_wR=t_wR):
            for (t0, nt) in TOKB:
                pr, tr = proj_block(k, wR, t_wR, d * 16, 16, t0, nt)
                S.op("act", lambda e, pr=pr, t0=t0, nt=nt: e.activation(out=B[0:16, t0:t0 + nt], in_=pr[0:16, 0:nt], func=AF.Copy), r=[tr], w=[t_rb[3]])
                pz, tz = k.bank()
                S.op("pe", lambda e, pz=pz, t0=t0, nt=nt: e.matmul(pz[0:96, 0:nt], lhsT=gw[0:16, d, h * 96:(h + 1) * 96], rhs=B[0:16, t0:t0 + nt],
                                                                   start=True, stop=True), r=[t_gw, t_rb[3]], w=[tz])
                S.op("act", lambda e, pz=pz, t0=t0, nt=nt: e.activation(out=A[:, t0:t0 + nt], in_=pz[0:96, 0:nt], func=AF.Exp, scale=-1.0,
                                                                        bias=col[0:96, 2 * h + d:2 * h + d + 1]), r=[tz, t_col], w=[t_rb[2]])
            S.op("act", lambda e: e.activation(out=A, in_=A, func=AF.Ln, bias=k.cst[0:96, 1:2]), r=[t_rb[2], k.t_c], w=[t_rb[2]])
            S.op("dve", lambda e: e.tensor_scalar(out=A, in0=A, scalar1=-1.0 / 16.0, scalar2=None, op0=ALU.mult), r=[t_rb[2]], w=[t_rb[2]])
        gated_scan(k, 96, 96, 2, qrow, t_rb[0], krow, t_rb[1], A, t_rb[2], B, t_rb[3], make_logf,
                   lambda i: vt[:, i, :], [t_hb[4], t_hb[5]], oacc, [t_rb[4], t_rb[5]], qts, [t_hb[0], t_hb[6]], kts, [t_hb[1], t_hb[7]], L=128)
        if h == 0:
            k.dump("gla_o0", RB[0:96, 4:6, :], [96, 2, N], F32, [t_rb[4], t_rb[5]])
        rms_gate_out(k, 96, 2, oacc, [t_rb[4], t_rb[5]], A, t_rb[2], B, t_rb[3], gsil, [t_hb[2], t_hb[3]],
                     [col[0:96, 8 + 2 * h:9 + 2 * h], col[0:96, 9 + 2 * h:10 + 2 * h]], t_col, [qt, kt], [t_hb[0], t_hb[1]], [2 * h, 2 * h + 1])
    k.dump("mixd1", k.mix_d[0:10, :, :], [10, 128, N], BF16, k.t_mixd[0:10])


_NC_CACHE = {}


def _f32(a):
    return np.ascontiguousarray(np.asarray(a, dtype=np.float32))


def kernel(x, c, ctx, c_ctx, w_ada, b_ada, ln_g, ln_b, w_in_even, attn_sink, hgrn_lb_logits, hgrn_norm,
           w_out_even, w_in_odd, gla_gate_w, gla_gate_b, gla_norm, w_out_odd, w_router, b_router,
           w_expert_gate, w_expert_up, w_expert_down):
    x = _f32(x); c = _f32(c); ctx = _f32(ctx); c_ctx = _f32(c_ctx)
    w0a = np.ascontiguousarray(_f32(w_in_even)[0][:, _cols0()])
    w1a = np.ascontiguousarray(_f32(w_in_odd)[0][:, _cols1()])
    shared = {
        "w_ada": _f32(w_ada), "b_ada": _f32(b_ada), "ln_g": _f32(ln_g), "ln_b": _f32(ln_b),
        "w0a": w0a, "attn_sink": _f32(attn_sink), "lb_logits": _f32(hgrn_lb_logits), "hgrn_norm": _f32(hgrn_norm),
        "w_out_even": _f32(w_out_even)[0], "w1a": w1a, "gla_gate_w": _f32(gla_gate_w)[0], "gla_gate_b": _f32(gla_gate_b)[0],
        "gla_norm": _f32(gla_norm), "w_out_odd": _f32(w_out_odd)[0], "w_router": _f32(w_router),
        "b_router": _f32(b_router)[None, :], "w_expert_gate": _f32(w_expert_gate), "w_expert_up": _f32(w_expert_up),
        "w_expert_down": _f32(w_expert_down),
    }
    nb = x.shape[0]
    in_maps = []
    for b in range(nb):
        m = dict(shared)
        m["x"] = np.ascontiguousarray(x[b])
        m["ctx"] = np.ascontiguousarray(ctx[b])
        m["cvec"] = np.ascontiguousarray(np.stack([c[b], c_ctx], 0))
        in_maps.append(m)
    if "nc" not in _NC_CACHE:
        _NC_CACHE["nc"] = build()
    res = run_bass_kernel_spmd(_NC_CACHE["nc"], in_maps, core_ids=list(range(nb)))
    return np.stack([np.asarray(r["out"], dtype=np.float32) for r in res.results], 0)
```

```python
import contextlib
import math
import numpy as np
import concourse.bass as bass
import concourse.mybir as mybir
from concourse.bass_utils import run_bass_kernel_spmd

F32 = mybir.dt.float32
BF16 = mybir.dt.bfloat16
I32 = mybir.dt.int32
AF = mybir.ActivationFunctionType
ALU = mybir.AluOpType
AX = mybir.AxisListType

ENG = ("pe", "act", "dve", "pool", "sp")


class Trk:
    __slots__ = ("w", "rs", "dsem", "dcnt", "name")

    def __init__(self, name=""):
        self.w = None
        self.rs = []
        self.dsem = None
        self.dcnt = 0
        self.name = name


class Sched:
    SEM_CHUNK = 20000

    def __init__(self, nc):
        self.nc = nc
        self.ops = {e: [] for e in ENG}
        self.waited = {e: {} for e in ENG}
        self.stack = contextlib.ExitStack()
        self.nsem = 0
        self.dma_ev = {}

    def sbuf(self, name, shape, dt):
        return self.stack.enter_context(self.nc.sbuf_tensor(name, list(shape), dt))

    def psum(self, name, shape, dt=F32):
        return self.stack.enter_context(self.nc.psum_tensor(name, list(shape), dt))

    def new_sem(self, name):
        self.nsem += 1
        return self.stack.enter_context(self.nc.semaphore(f"{name}_{self.nsem}"))

    def _filter(self, engine, deps):
        waits = []
        wd = self.waited[engine]
        for ev in deps:
            if ev[0] == "e":
                _, f, idx = ev
                if engine == "pe" and f == "pe":
                    continue
                if idx <= wd.get(f, -1):
                    continue
                wd[f] = idx
                self.ops[f][idx][2] = True
                waits.append(ev)
            else:
                _, sem, val = ev
                k = id(sem)
                if val <= wd.get(k, 0):
                    continue
                wd[k] = val
                waits.append(ev)
        return waits

    def _deps(self, engine, r, w):
        deps = []
        for t in r:
            if t.w is not None:
                deps.append(t.w)
        for t in w:
            if t.w is not None:
                deps.append(t.w)
            deps.extend(t.rs)
        return self._filter(engine, deps)

    def _post(self, ev, r, w):
        for t in w:
            t.w = ev
            t.rs = []
        for t in r:
            if t in w:
                continue
            if ev[0] == "e":
                t.rs = [x for x in t.rs if not (x[0] == "e" and x[1] == ev[1])]
            else:
                t.rs = [x for x in t.rs if not (x[0] == "d" and x[1] is ev[1])]
            t.rs.append(ev)

    def op(self, engine, fn, r=(), w=()):
        r = list(r)
        w = list(w)
        waits = self._deps(engine, r, w)
        idx = len(self.ops[engine])
        self.ops[engine].append([fn, waits, False, None])
        self._post(("e", engine, idx), r, w)

    def dma(self, out, in_, r=(), w=(), q="sp", **kw):
        r = list(r)
        w = list(w)
        waits = self._deps(q, r, w)
        t0 = w[0]
        if t0.dsem is None or t0.dcnt > 60000:
            t0.dsem = self.new_sem("d")
            t0.dcnt = 0
        t0.dcnt += 16
        ev = ("d", t0.dsem, t0.dcnt)
        self.dma_ev[id(t0.dsem)] = ev

        def fn(eng, out=out, in_=in_, kw=kw):
            return eng.dma_start(out=out, in_=in_, **kw)
        self.ops[q].append([fn, waits, False, t0.dsem])
        self._post(ev, r, w)

    def barrier(self):
        last = {}
        for f in ("pe", "act", "dve", "pool"):
            j = len(self.ops[f]) - 1
            while j >= 0 and (self.ops[f][j][0] is None or self.ops[f][j][3] is not None):
                j -= 1
            last[f] = j
        dm = list(self.dma_ev.values())
        for e in ENG:
            deps = [("e", f, last[f]) for f in ("pe", "act", "dve", "pool") if f != e and last[f] >= 0]
            deps += dm
            waits = self._filter(e, deps)
            self.ops[e].append([None, waits, False, None])

    def wait_all(self, engine, trks):
        waits = self._deps(engine, list(trks), [])
        self.ops[engine].append([None, waits, False, None])

    def emit(self):
        nc = self.nc
        cum = {}
        sems = {}
        for e in ENG:
            c = 0
            arr = []
            for rec in self.ops[e]:
                if rec[2]:
                    c += 1
                arr.append(c)
            cum[e] = arr
            sems[e] = [self.new_sem(f"s{e}") for _ in range(c // self.SEM_CHUNK + 1)]
        CH = self.SEM_CHUNK

        def semval(f, idx):
            c = cum[f][idx]
            ch = (c - 1) // CH
            return sems[f][ch], c - ch * CH

        def run(e, eng):
            for i, (fn, waits, sig, dsem) in enumerate(self.ops[e]):
                for ev in waits:
                    if ev[0] == "e":
                        s, v = semval(ev[1], ev[2])
                        eng.wait_ge(s, v)
                    else:
                        eng.wait_ge(ev[1], ev[2])
                if fn is None:
                    continue
                ins = fn(eng)
                if dsem is not None:
                    ins.then_inc(dsem, 16)
                elif sig:
                    s, v = semval(e, i)
                    ins.then_inc(s, 1)

        with nc.Block() as block:
            @block.tensor
            def _(eng):
                run("pe", eng)

            @block.scalar
            def _(eng):
                run("act", eng)

            @block.vector
            def _(eng):
                run("dve", eng)

            @block.gpsimd
            def _(eng):
                run("pool", eng)

            @block.sync
            def _(eng):
                run("sp", eng)

    def close(self):
        self.stack.close()


N = 2304
NT = 18
D = 1024
KC = 8
NLAT = 2048
ALPHA = 4.0 ** 0.25
EPS = 1e-5
TOKB = [(0, 512), (512, 512), (1024, 512), (1536, 512), (2048, 256)]
PI = math.pi

_SW = list(range(16, 32)) + list(range(0, 16)) + list(range(48, 64)) + list(range(32, 48))


def _cols0():
    cols = []
    for j in range(2):
        for blk in range(2):
            for hh in (4 * j + 2 * blk, 4 * j + 2 * blk + 1):
                cols += [hh * 64 + d for d in range(64)]
        for blk in range(2):
            for hh in (4 * j + 2 * blk, 4 * j + 2 * blk + 1):
                cols += [hh * 64 + d for d in _SW]
        cols += [512 + j * 64 + d for d in range(64)] * 2
        cols += [512 + j * 64 + d for d in _SW] * 2
    cols += list(range(640, 768))
    for h in range(4):
        cols += [768 + h * 128 + d for d in range(128)]
        cols += [1280 + h * 128 + d for d in range(128)]
        cols += [1792 + h * 128 + d for d in range(128)]
        cols += [2816 + h * 128 + d for d in range(128)]
    cols += list(range(2304, 2816))
    return cols


def _cols1():
    cols = []
    for h in range(4):
        cols += [h * 96 + d for d in range(96)]
        cols += [384 + h * 96 + d for d in range(96)]
        cols += [1568 + h * 192 + d for d in range(192)]
    cols += list(range(2336, 2592))
    cols += list(range(1536, 1568))
    cols += list(range(768, 1536))
    return cols


class K:
    pass


def build(dbg=(), stop=None):
    nc = bass.Bass("TRN2", target_bir_lowering=False)
    S = Sched(nc)
    k = K()
    k.nc = nc
    k.S = S
    k.dbg = set(dbg)
    k.sub = stop
    k.dbg_out = []

    def din(name, shape, dt=F32):
        return nc.dram_tensor(name, list(shape), dt, kind="ExternalInput").ap()

    x_d = din("x", [NLAT, D])
    ctx_d = din("ctx", [256, D])
    cv_d = din("cvec", [2, D])
    wada_d = din("w_ada", [2, D, 6 * D])
    bada_d = din("b_ada", [2, 6 * D])
    lng_d = din("ln_g", [2, 2, D])
    lnb_d = din("ln_b", [2, 2, D])
    w0_d = din("w0a", [D, 4224])
    sink_d = din("attn_sink", [1, 8])
    lbl_d = din("lb_logits", [2, 2, 512])
    hn_d = din("hgrn_norm", [1, 512])
    wo0_d = din("w_out_even", [D, D])
    w1_d = din("w1a", [D, 2592])
    gw_d = din("gla_gate_w", [2, 16, 384])
    gb_d = din("gla_gate_b", [2, 384])
    gn_d = din("gla_norm", [1, 768])
    wo1_d = din("w_out_odd", [D, D])
    wr_d = din("w_router", [D, 16])
    br_d = din("b_router", [1, 16])
    eg_d = din("w_expert_gate", [2, 16, D, 512])
    eu_d = din("w_expert_up", [2, 16, D, 512])
    ed_d = din("w_expert_down", [2, 16, 512, D])
    out_d = nc.dram_tensor("out", [NLAT, D], F32, kind="ExternalOutput").ap()
    hmid_d = nc.dram_tensor("hmid", [N, D], F32).ap()
    hout0_d = nc.dram_tensor("hout0", [N, D], F32).ap()
    mix_d = nc.dram_tensor("mixd", [10, 128, N], BF16).ap()
    t_hmid = [Trk() for _ in range(NT)]
    t_hout0 = [Trk() for _ in range(NT)]
    t_mixd = [Trk() for _ in range(10)]
    t_out = [Trk() for _ in range(16)]

    def dump(name, src_ap, shape, dt, r):
        if name not in k.dbg:
            return
        d = nc.dram_tensor("dbg_" + name, list(shape), dt, kind="ExternalOutput").ap()
        t = Trk()
        S.dma(d, src_ap, r=r, w=[t])
        k.dbg_out.append(t)

    PS = [S.psum(f"ps{i}", [128, 1024]) for i in range(4)]
    PT = [[Trk(), Trk()] for _ in range(4)]
    k.bank_i = 0
    k.pair_i = 0

    k.reserved = set()

    def bank():
        i = k.bank_i
        while i in k.reserved:
            i = (i + 1) % 8
        k.bank_i = (i + 1) % 8
        k.last_bank = i
        return PS[i // 2][:, (i % 2) * 512:(i % 2 + 1) * 512], PT[i // 2][i % 2]

    def pair():
        i = k.pair_i
        k.pair_i = (i + 1) % 4
        k.bank_i = (2 * i + 2) % 8
        return PS[i], PT[i]

    identF = S.sbuf("identF", [128, 128], F32); t_c = Trk()
    identB = S.sbuf("identB", [128, 128], BF16)
    onesF = S.sbuf("onesF", [128, 128], F32)
    onesB = S.sbuf("onesB", [128, 128], BF16)
    cst = S.sbuf("cst", [128, 8], F32)
    Mf = S.sbuf("Mf", [128, 128], BF16)
    Mb = S.sbuf("Mb", [128, 128], BF16)
    MP = S.sbuf("MP", [128, 512], BF16)
    MN = S.sbuf("MN", [128, 512], BF16)
    rmask = S.sbuf("rmask", [128, N], BF16)
    modT = S.sbuf("modT", [128, 2, 48, 2], F32); t_modT = Trk()
    mod_d = nc.dram_tensor("mod_d", [2, 2, 6 * D], F32).ap(); t_mod = Trk()
    bc = S.sbuf("bc", [128, 4, D], F32); t_bc = [Trk() for _ in range(4)]
    uT = S.sbuf("uT", [128, KC, N], BF16); t_uT = [Trk() for _ in range(NT)]
    wbuf = S.sbuf("wbuf", [128, 5, 4096], BF16); t_wb = [Trk() for _ in range(5)]
    RB = S.sbuf("RB", [128, 6, N], F32); t_rb = [Trk() for _ in range(6)]
    HB = S.sbuf("HB", [128, 8, N], BF16); t_hb = [Trk() for _ in range(8)]
    sm = S.sbuf("sm", [128, 640], F32)
    gates = S.sbuf("gates", [128, NT, 16], F32); t_gates = [Trk() for _ in range(NT)]
    wrt = S.sbuf("wrt", [128, KC, 16], F32); t_wr = Trk()
    brb = S.sbuf("brb", [128, 16], F32)

    S.op("pool", lambda e: e.memset(identF[:], 0.0), w=[t_c])
    S.op("pool", lambda e: e.affine_select(out=identF[:], in_=identF[:], pattern=[[-1, 128]], compare_op=ALU.not_equal,
                                           fill=1.0, base=0, channel_multiplier=1), r=[t_c], w=[t_c])
    S.op("pool", lambda e: e.tensor_copy(out=identB[:], in_=identF[:]), r=[t_c], w=[t_c])
    S.op("pool", lambda e: e.memset(onesF[:], 1.0), w=[t_c])
    S.op("pool", lambda e: e.memset(onesB[:], 1.0), w=[t_c])
    for j_, v_ in enumerate((EPS, 1.0, -PI, 0.0, PI)):
        S.op("pool", lambda e, j_=j_, v_=v_: e.memset(cst[:, j_:j_ + 1], v_), w=[t_c])
    S.op("pool", lambda e: e.memset(Mf[:], 1.0), w=[t_c])
    S.op("pool", lambda e: e.affine_select(out=Mf[:], in_=Mf[:], pattern=[[1, 128]], compare_op=ALU.is_ge, fill=0.0,
                                           base=0, channel_multiplier=-1), r=[t_c], w=[t_c])
    S.op("pool", lambda e: e.memset(Mf[0:64, 64:128], 0.0), r=[t_c], w=[t_c])
    S.op("pool", lambda e: e.memset(Mb[:], 1.0), w=[t_c])
    S.op("pool", lambda e: e.affine_select(out=Mb[:], in_=Mb[:], pattern=[[-1, 128]], compare_op=ALU.is_ge, fill=0.0,
                                           base=0, channel_multiplier=1), r=[t_c], w=[t_c])
    S.op("pool", lambda e: e.memset(Mb[64:128, 0:64], 0.0), r=[t_c], w=[t_c])
    S.op("pool", lambda e: e.memset(MP[:], 1.0), w=[t_c])
    S.op("pool", lambda e: e.affine_select(out=MP[:], in_=MP[:], pattern=[[0, 4], [-1, 128]], compare_op=ALU.is_ge,
                                           fill=0.0, base=0, channel_multiplier=1), r=[t_c], w=[t_c])
    S.op("pool", lambda e: e.memset(MN[:], 1.0), w=[t_c])
    S.op("pool", lambda e: e.affine_select(out=MN[:], in_=MN[:], pattern=[[0, 4], [1, 128]], compare_op=ALU.is_ge,
                                           fill=0.0, base=0, channel_multiplier=-1), r=[t_c], w=[t_c])
    S.op("pool", lambda e: e.memset(rmask[:], 1.0), w=[t_c])
    S.op("pool", lambda e: e.memset(rmask[:].rearrange("p (c l) -> p c l", l=64)[:, :, 0:1], 0.0), r=[t_c], w=[t_c])
    S.dma(wrt[:], wr_d.rearrange("(k p) n -> p k n", p=128), w=[t_wr])
    S.dma(brb[:], br_d[0:1, :].to_broadcast([128, 16]), w=[t_c])
    S.barrier()

    cs = bc[0:2, 0, 0:1024]
    sg_ = bc[0:2, 1, 0:1024]
    csT = sm[:, 520:536].rearrange("p (k r) -> p k r", r=2)
    t_cs = Trk(); t_csT = Trk()
    k.t_bb = [[Trk(), Trk()], [Trk(), Trk()]]
    S.dma(cs, cv_d[:, :], w=[t_cs])
    S.op("act", lambda e: e.activation(out=sg_, in_=cs, func=AF.Sigmoid), r=[t_cs], w=[t_csT])
    S.op("dve", lambda e: e.tensor_tensor(out=cs, in0=cs, in1=sg_, op=ALU.mult), r=[t_cs, t_csT], w=[t_cs])
    pb, tb = bank()
    for kk in range(KC):
        S.op("pe", lambda e, kk=kk, pb=pb: e.transpose(out=pb[:, 2 * kk:2 * kk + 2], in_=cs[:, kk * 128:(kk + 1) * 128],
                                                       identity=identF[0:2, 0:2]), r=[t_cs, t_c], w=[tb])
    S.op("dve", lambda e, pb=pb: e.tensor_copy(out=csT, in_=pb[:, 0:16].rearrange("p (k r) -> p k r", r=2)), r=[tb], w=[t_csT])
    t_wa = [Trk(), Trk()]
    t_mb = [Trk(), Trk()]
    k.t_modTg = [[Trk() for _ in range(6)] for _ in range(2)]
    mbuf = bc[0:2, 2, :]

    k.ada_list = [(l, j) for l in range(2) for j in range(24)]
    k.ada_pos = 0
    k.ada_pending_store = None

    def ada_load(idx):
        l, j = k.ada_list[idx]
        slot = idx % 2
        wa = RB[:, 4 + slot, 0:2048].rearrange("p (k n) -> p k n", n=256)
        S.dma(wa, wada_d[l, :, j * 256:(j + 1) * 256].rearrange("(k p) n -> p k n", p=128), w=[t_wa[slot]])
        for r_ in range(2):
            S.dma(mbuf[r_:r_ + 1, 512 + slot * 256:512 + (slot + 1) * 256], bada_d[l:l + 1, j * 256:(j + 1) * 256], w=[k.t_bb[slot][r_]])

    def ada_step():
        idx = k.ada_pos
        if idx >= len(k.ada_list):
            return False
        k.ada_pos = idx + 1
        if idx == 0:
            ada_load(0)
        if idx + 1 < len(k.ada_list):
            ada_load(idx + 1)
        if k.ada_pending_store is not None:
            k.ada_pending_store()
            k.ada_pending_store = None
        l, j = k.ada_list[idx]
        slot = idx % 2
        wa = RB[:, 4 + slot, 0:2048].rearrange("p (k n) -> p k n", n=256)
        mb = mbuf[:, slot * 256:(slot + 1) * 256]
        bb = mbuf[:, 512 + slot * 256:512 + (slot + 1) * 256]
        pb, tb = bank()
        for kk in range(KC):
            S.op("pe", lambda e, kk=kk: e.matmul(pb[0:2, 0:256], lhsT=csT[:, kk, :], rhs=wa[:, kk, :], start=(kk == 0), stop=(kk == KC - 1)),
                 r=[t_csT, t_wa[slot]], w=[tb])
        S.op("dve", lambda e: e.tensor_tensor(out=mb, in0=pb[0:2, 0:256], in1=bb, op=ALU.add), r=[tb] + k.t_bb[slot], w=[t_mb[slot]])
        which = j // 4
        if which in (1, 4):
            S.op("dve", lambda e: e.tensor_scalar(out=mb, in0=mb, scalar1=1.0, scalar2=None, op0=ALU.add), r=[t_mb[slot]], w=[t_mb[slot]])
        k.ada_pending_store = lambda: S.dma(mod_d[l, :, j * 256:(j + 1) * 256], mb, r=[t_mb[slot]], w=[t_mod])
        pT, tT = bank()
        for q_ in range(2):
            S.op("pe", lambda e, q_=q_: e.transpose(out=pT[:, 2 * q_:2 * q_ + 2], in_=mb[:, q_ * 128:(q_ + 1) * 128], identity=identF[0:2, 0:2]),
                 r=[t_mb[slot], t_c], w=[tT])
        S.op("dve", lambda e: e.tensor_copy(out=modT[:, l, 2 * j:2 * j + 2, :], in_=pT[:, 0:4].rearrange("p (j r) -> p j r", r=2)),
             r=[tT], w=[k.t_modTg[l][which]])
        return True

    def ada_flush():
        while ada_step():
            pass
        if k.ada_pending_store is not None:
            k.ada_pending_store()
            k.ada_pending_store = None
    k.ada_step = ada_step
    k.ada_flush = ada_flush
    for _ in range(8):
        ada_step()

    def bcast_rows(l, which):
        gi = 2 if which == "mix" else 5
        li = 0 if which == "mix" else 1
        S.dma(bc[:, 0, :], mod_d[l, 0:1, gi * D:(gi + 1) * D].to_broadcast([128, D]), r=[t_mod], w=[t_bc[0]])
        S.dma(bc[:, 1, :], mod_d[l, 1:2, gi * D:(gi + 1) * D].to_broadcast([128, D]), r=[t_mod], w=[t_bc[1]])
        S.dma(bc[:, 2, :], lng_d[l, li:li + 1, :].to_broadcast([128, D]), w=[t_bc[2]])
        S.dma(bc[:, 3, :], lnb_d[l, li:li + 1, :].to_broadcast([128, D]), w=[t_bc[3]])

    def ln_stats(src, t_src, st_ap, mv_ap, rs_ap, nb_ap, t_st):
        for j in range(2):
            S.op("dve", lambda e, j=j: e.bn_stats(out=st_ap[:, j, :], in_=src[:, j * 512:(j + 1) * 512]), r=[t_src], w=[t_st])
        S.op("dve", lambda e: e.bn_aggr(out=mv_ap, in_=st_ap), r=[t_st], w=[t_st])
        S.op("act", lambda e: e.activation(out=rs_ap, in_=mv_ap[:, 1:2], func=AF.Sqrt, bias=cst[:, 0:1]), r=[t_st, t_c], w=[t_st])
        S.op("dve", lambda e: e.reciprocal(out=rs_ap, in_=rs_ap), r=[t_st], w=[t_st])
        S.op("dve", lambda e: e.tensor_scalar(out=nb_ap, in0=mv_ap[:, 0:1], scalar1=rs_ap, scalar2=-1.0, op0=ALU.mult, op1=ALU.mult),
             r=[t_st], w=[t_st])

    k.st_i = 0

    def st_slot():
        i = k.st_i
        k.st_i = (i + 1) % 8
        base = i * 24
        return (sm[:, base:base + 12].rearrange("p (a b) -> p a b", b=6), sm[:, base + 12:base + 14],
                sm[:, base + 14:base + 15], sm[:, base + 15:base + 16], k.t_st[i])
    k.t_st = [Trk() for _ in range(8)]

    def ln_part1(src, t_src, xn, t_xn):
        st_ap, mv_ap, rs_ap, nb_ap, t_st = st_slot()
        ln_stats(src, t_src, st_ap, mv_ap, rs_ap, nb_ap, t_st)
        S.op("act", lambda e: e.activation(out=xn, in_=src, func=AF.Identity, scale=rs_ap, bias=nb_ap), r=[t_src, t_st], w=[t_xn])

    def ln_part2(l, xn, t_xn, i, which, router, uf=None, t_uf=None):
        r_ = 1 if i < 2 else 0
        pp, tp = pair()
        for kk in range(KC):
            S.op("pe", lambda e, kk=kk: e.transpose(out=pp[:, kk * 128:(kk + 1) * 128], in_=xn[:, kk * 128:(kk + 1) * 128],
                                                    identity=identF[:]), r=[t_xn, t_c], w=[tp[kk // 4]])
        for kk in range(KC):
            sc_ap = modT[:, l, (which + 1) * 8 + kk, r_:r_ + 1]
            sh_ap = modT[:, l, which * 8 + kk, r_:r_ + 1]
            if router:
                o_ap = uf[:, kk, :]
                tw = t_uf
            else:
                o_ap = uT[:, kk, i * 128:(i + 1) * 128]
                tw = t_uT[i]
            if kk % 2 == 0:
                S.op("act", lambda e, kk=kk, o_ap=o_ap, sc_ap=sc_ap, sh_ap=sh_ap: e.activation(
                    out=o_ap, in_=pp[:, kk * 128:(kk + 1) * 128], func=AF.Identity, scale=sc_ap, bias=sh_ap),
                    r=[tp[kk // 4], k.t_modTg[l][which], k.t_modTg[l][which + 1]], w=[tw])
            else:
                S.op("dve", lambda e, kk=kk, o_ap=o_ap, sc_ap=sc_ap, sh_ap=sh_ap: e.tensor_scalar(
                    out=o_ap, in0=pp[:, kk * 128:(kk + 1) * 128], scalar1=sc_ap, scalar2=sh_ap, op0=ALU.mult, op1=ALU.add),
                    r=[tp[kk // 4], k.t_modTg[l][which], k.t_modTg[l][which + 1]], w=[tw])
        if router:
            S.op("pool", lambda e: e.tensor_copy(out=uT[:, :, i * 128:(i + 1) * 128], in_=uf[:, :, :]), r=[t_uf], w=[t_uT[i]])

    def ln_to_uT(l, src, t_src, xn, t_xn, i, which, router, uf=None, t_uf=None):
        ln_part1(src, t_src, xn, t_xn)
        ln_part2(l, xn, t_xn, i, which, router, uf, t_uf)
        if router:
            route(i, uf, t_uf)

    def route(i, uf, t_uf):
        pb, tb = bank()
        for kk in range(KC):
            S.op("pe", lambda e, kk=kk: e.matmul(pb[:, 0:16], lhsT=uf[:, kk, :], rhs=wrt[:, kk, :], start=(kk == 0),
                                                 stop=(kk == KC - 1)), r=[t_uf, t_wr], w=[tb])
        S.op("act", lambda e: e.activation(out=gates[:, i, :], in_=pb[:, 0:16], func=AF.Copy), r=[tb], w=[t_gates[i]])

    def route_all(t0, nt, scr, t_scr):
        n16 = nt * 16
        o = [0]

        def take(n):
            a = scr[:, o[0]:o[0] + n]
            o[0] += n
            return a
        lg = gates[:, t0:t0 + nt, :]
        pr, sl, s2, eq, eq2 = [take(n16).rearrange("p (t e) -> p t e", e=16) for _ in range(5)]
        g4, g4b, g4c = [take(nt * 4).rearrange("p (t g) -> p t g", g=4) for _ in range(3)]
        sc1, sc2 = take(nt), take(nt)
        BIG = 1.0e4
        tg = t_gates[t0:t0 + nt]

        def dv(fn):
            S.op("dve", fn, r=t_scr + [k.t_c] + tg, w=t_scr)

        def bc16(a):
            return a.unsqueeze(2).to_broadcast([128, nt, 16])

        def g4v(a):
            return a.rearrange("p t (g e) -> p (t g) e", e=4)

        def g4f(a):
            return a.rearrange("p t g -> p (t g)")
        dv(lambda e: e.tensor_reduce(out=sc1, in_=lg, axis=AX.X, op=ALU.max))
        dv(lambda e: e.tensor_tensor(out=pr, in0=lg, in1=bc16(sc1), op=ALU.subtract))
        S.op("act", lambda e: e.activation(out=pr, in_=pr, func=AF.Exp), r=t_scr, w=t_scr)
        dv(lambda e: e.tensor_reduce(out=sc2, in_=pr, axis=AX.X, op=ALU.add))
        dv(lambda e: e.reciprocal(out=sc2, in_=sc2))
        dv(lambda e: e.tensor_tensor(out=pr, in0=pr, in1=bc16(sc2), op=ALU.mult))
        dv(lambda e: e.tensor_tensor(out=sl, in0=pr, in1=brb[:].unsqueeze(1).to_broadcast([128, nt, 16]), op=ALU.add))
        dv(lambda e: e.tensor_reduce(out=g4f(g4), in_=g4v(sl), axis=AX.X, op=ALU.max))
        dv(lambda e: e.tensor_tensor(out=g4v(eq), in0=g4v(sl), in1=g4f(g4).unsqueeze(2).to_broadcast([128, nt * 4, 4]), op=ALU.is_equal))
        dv(lambda e: e.scalar_tensor_tensor(out=s2.rearrange("p t e -> p (t e)"), in0=eq.rearrange("p t e -> p (t e)"), scalar=-BIG,
                                            in1=sl.rearrange("p t e -> p (t e)"), op0=ALU.mult, op1=ALU.add))
        dv(lambda e: e.tensor_reduce(out=g4f(g4b), in_=g4v(s2), axis=AX.X, op=ALU.max))
        dv(lambda e: e.tensor_tensor(out=g4f(g4), in0=g4f(g4), in1=g4f(g4b), op=ALU.add))
        dv(lambda e: e.tensor_reduce(out=sc1, in_=g4, axis=AX.X, op=ALU.max))
        dv(lambda e: e.tensor_tensor(out=g4c, in0=g4, in1=sc1.unsqueeze(2).to_broadcast([128, nt, 4]), op=ALU.is_equal))
        dv(lambda e: e.tensor_scalar(out=g4f(g4c), in0=g4f(g4c), scalar1=-1.0, scalar2=BIG, op0=ALU.add, op1=ALU.mult))
        dv(lambda e: e.tensor_tensor(out=g4v(s2), in0=g4v(sl), in1=g4f(g4c).unsqueeze(2).to_broadcast([128, nt * 4, 4]), op=ALU.add))
        dv(lambda e: e.tensor_reduce(out=sc1, in_=s2, axis=AX.X, op=ALU.max))
        dv(lambda e: e.tensor_tensor(out=eq, in0=s2, in1=bc16(sc1), op=ALU.is_equal))
        dv(lambda e: e.scalar_tensor_tensor(out=s2.rearrange("p t e -> p (t e)"), in0=eq.rearrange("p t e -> p (t e)"), scalar=-BIG,
                                            in1=s2.rearrange("p t e -> p (t e)"), op0=ALU.mult, op1=ALU.add))
        dv(lambda e: e.tensor_reduce(out=sc1, in_=s2, axis=AX.X, op=ALU.max))
        dv(lambda e: e.tensor_tensor(out=eq2, in0=s2, in1=bc16(sc1), op=ALU.is_equal))
        dv(lambda e: e.tensor_tensor(out=eq, in0=eq, in1=eq2, op=ALU.add))
        dv(lambda e: e.tensor_tensor(out=eq, in0=eq, in1=pr, op=ALU.mult))
        dv(lambda e: e.tensor_reduce(out=sc2, in_=eq, axis=AX.X, op=ALU.add))
        dv(lambda e: e.reciprocal(out=sc2, in_=sc2))
        S.op("dve", lambda e: e.tensor_tensor(out=lg, in0=eq, in1=bc16(sc2), op=ALU.mult), r=t_scr, w=tg)
    k.route_all = route_all
    k.t_route = [Trk(), Trk()]
    k.t_tile = [Trk() for _ in range(12)]

    k.wslot = 0

    def load_w(src_ap, nslots=1, parts=128):
        s0 = k.wslot
        if s0 + nslots > 5:
            s0 = 0
        k.wslot = (s0 + nslots) % 5
        a, b = src_ap.shape[1], src_ap.shape[2]
        dst = wbuf[0:parts, s0:s0 + nslots, :].rearrange("p s n -> p (s n)")[:, 0:a * b].rearrange("p (a b) -> p a b", b=b)
        trks = t_wb[s0:s0 + nslots]
        S.dma(dst, src_ap, w=trks, q="pool")
        return dst, trks

    def proj_fm(wv, t_w, c0, M, evac, toks=TOKB):
        for (t0, nt) in toks:
            pb, tb = bank()
            for kk in range(KC):
                S.op("pe", lambda e, kk=kk, pb=pb, t0=t0, nt=nt: e.matmul(pb[0:M, 0:nt], lhsT=wv[:, kk, c0:c0 + M],
                                                                          rhs=uT[:, kk, t0:t0 + nt], start=(kk == 0), stop=(kk == KC - 1)),
                     r=t_w + t_uT[t0 // 128:(t0 + nt) // 128], w=[tb])
            evac(pb, tb, t0, nt)

    def proj_tm(wv, t_w, c0, ncol, evac, tiles=range(NT)):
        for i in tiles:
            pb, tb = bank()
            for kk in range(KC):
                S.op("pe", lambda e, kk=kk, pb=pb, i=i: e.matmul(pb[:, 0:ncol], lhsT=uT[:, kk, i * 128:(i + 1) * 128],
                                                                 rhs=wv[:, kk, c0:c0 + ncol], start=(kk == 0), stop=(kk == KC - 1)),
                     r=t_w + [t_uT[i]], w=[tb])
            evac(pb, tb, i)

    k.proj_fm = proj_fm
    k.proj_tm = proj_tm
    k.load_w = load_w
    k.bank = bank
    k.pair = pair
    k.dump = dump
    k.ln_to_uT = ln_to_uT
    k.ln_part1 = ln_part1
    k.ln_part2 = ln_part2
    k.route = route
    k.ln_stats = ln_stats
    k.st_slot = st_slot
    k.bcast_rows = bcast_rows
    for nm in ("x_d ctx_d w0_d sink_d lbl_d hn_d wo0_d w1_d gw_d gb_d gn_d wo1_d eg_d eu_d ed_d out_d hmid_d hout0_d mix_d "
               "t_hmid t_hout0 t_mixd t_out identF identB onesF onesB cst Mf Mb MP MN rmask modT t_modT mod_d t_mod bc t_bc uT t_uT "
               "wbuf t_wb RB t_rb HB t_hb sm gates t_gates t_c PS PT").split():
        setattr(k, nm, locals()[nm])

    for ph, l in [("B", 0), ("M", 0), ("D", 0), ("E", 0), ("B", 1), ("M", 1), ("D", 1), ("E", 1)]:
        if ph == "B":
            phase_B(k, l)
        elif ph == "M":
            (mixer_even if l == 0 else mixer_odd)(k)
        elif ph == "D":
            phase_D(k, l)
        else:
            phase_E(k, l)
        S.barrier()
        if stop is not None and stop[0:2] == f"{ph}{l}":
            break

    S.wait_all("sp", t_out + k.dbg_out)
    S.emit()
    S.close()
    return nc


def phase_B(k, l):
    S = k.S
    bufs = {}

    def stA(i):
        slot = i % 2
        ht = k.RB[:, slot, 0:1024]
        t_ht = k.t_tile[slot]
        xn = k.RB[:, 2 + slot, 0:1024]
        t_xn = k.t_tile[2 + slot]
        if l == 0:
            src = k.ctx_d[i * 128:(i + 1) * 128, :] if i < 2 else k.x_d[(i - 2) * 128:(i - 1) * 128, :]
            S.dma(ht, src, w=[t_ht])
        else:
            S.dma(ht, k.hout0_d[i * 128:(i + 1) * 128, :], r=[k.t_hout0[i]], w=[t_ht])
        k.ln_part1(ht, t_ht, xn, t_xn)
        bufs[i] = (xn, t_xn)

    def stB(i):
        xn, t_xn = bufs.pop(i)
        k.ln_part2(l, xn, t_xn, i, 0, False)
    for n in range(NT + 1):
        if n < NT:
            stA(n)
        if n >= 1:
            stB(n - 1)
        if l == 0:
            for _ in range(3):
                k.ada_step()
    if l == 0:
        k.ada_flush()
        for l_ in range(2):
            k.dump(f"mod{l_}", k.mod_d[l_, :, :], [2, 6 * D], F32, [k.t_mod])
    k.dump(f"uT{l}", k.uT[:, :, :], [128, KC, N], BF16, k.t_uT)


def row_to_col(k, src, nr, n, dst, t_src, t_dst):
    S = k.S
    pb, tb = k.bank()
    S.op("pe", lambda e: e.transpose(out=pb[0:n, 0:nr], in_=src, identity=k.identF[0:nr, 0:nr]), r=[t_src, k.t_c], w=[tb])
    S.op("dve", lambda e: e.tensor_copy(out=dst, in_=pb[0:n, 0:nr]), r=[tb], w=[t_dst])


def proj_block(k, wv, t_w, c0, M, t0, nt):
    S = k.S
    pb, tb = k.bank()
    for kk in range(KC):
        S.op("pe", lambda e, kk=kk: e.matmul(pb[0:M, 0:nt], lhsT=wv[:, kk, c0:c0 + M], rhs=k.uT[:, kk, t0:t0 + nt],
                                             start=(kk == 0), stop=(kk == KC - 1)),
             r=t_w + k.t_uT[t0 // 128:(t0 + nt) // 128], w=[tb])
    return pb, tb


def gated_scan(k, dk, dvh, nh, q_ap, t_q, k_ap, t_k, A, t_A, B, t_B, make_logf, v_fn, t_v, o_acc, t_o, qts, t_qts, kts, t_kts, L=64):
    S = k.S
    sc = k.scn
    NC_ = N // L
    cpt = 128 // L
    dvt = dvh * nh
    B3 = B.rearrange("p (c l) -> p c l", l=L)
    for a in range(nh):
        S.op("pool", lambda e, a=a: e.memset(o_acc[a], 0.0), w=[t_o[a]])
    D_ = []
    for d in range(2):
        qt, kt, t_qt, t_kt = qts[d], kts[d], t_qts[d], t_kts[d]
        t_sc = k.t_scn[d]
        rr = sc["rr"][0:dk, d, 0:NC_]
        gg = sc["gg"][0:dk, d, 0:NC_]
        X1 = sc["X1"][0:dk, d, 0:NC_]
        X2 = sc["X2"][0:dk, d, 0:NC_]
        EG = sc["EG"][0:dk, d, 0:NC_]
        make_logf(d)
        S.op("dve", lambda e: e.tensor_tensor_scan(out=B, data0=k.rmask[0:dk, :], data1=A, initial=0.0, op0=ALU.mult, op1=ALU.add),
             r=[t_A, k.t_c], w=[t_B])
        S.op("pool", lambda e, gg=gg: e.tensor_copy(out=gg, in_=B3[:, :, L - 1]), r=[t_B], w=[t_sc])
        if d == 1:
            S.op("pool", lambda e: e.tensor_tensor(out=B, in0=B, in1=A, op=ALU.subtract), r=[t_B, t_A], w=[t_B])
        S.op("pool", lambda e, rr=rr: e.tensor_copy(out=rr, in_=B3[:, :, L // 2]), r=[t_B], w=[t_sc])
        S.op("dve", lambda e, rr=rr: e.tensor_tensor(out=B3, in0=B3, in1=rr.unsqueeze(2).to_broadcast([dk, NC_, L]), op=ALU.subtract),
             r=[t_B, t_sc], w=[t_B])
        sgn = 1.0 if d == 0 else -1.0
        S.op("act", lambda e, sgn=sgn: e.activation(out=A, in_=B, func=AF.Exp, scale=sgn), r=[t_B], w=[t_A])
        S.op("dve", lambda e, qt=qt: e.tensor_tensor(out=qt, in0=q_ap, in1=A, op=ALU.mult), r=[t_q, t_A], w=[t_qt])
        S.op("act", lambda e, sgn=sgn: e.activation(out=A, in_=B, func=AF.Exp, scale=-sgn), r=[t_B, t_qt], w=[t_A])
        S.op("dve", lambda e, kt=kt: e.tensor_tensor(out=kt, in0=k_ap, in1=A, op=ALU.mult), r=[t_k, t_A], w=[t_kt])
        S.op("act", lambda e, X1=X1, rr=rr: e.activation(out=X1, in_=rr, func=AF.Exp), r=[t_sc], w=[t_sc])
        S.op("act", lambda e, EG=EG, gg=gg: e.activation(out=EG, in_=gg, func=AF.Exp), r=[t_sc], w=[t_sc])
        S.op("dve", lambda e, X2=X2, gg=gg, rr=rr: e.tensor_tensor(out=X2, in0=gg, in1=rr, op=ALU.subtract), r=[t_sc], w=[t_sc])
        S.op("act", lambda e, X2=X2: e.activation(out=X2, in_=X2, func=AF.Exp), r=[t_sc], w=[t_sc])
        a_s, c_s = (X1, X2) if d == 0 else (X2, X1)
        fo = tuple(range(cpt))
        bo = tuple(reversed(range(cpt)))
        if d == 0:
            tiles = [(i, fo) for i in range(NT)]
        else:
            tiles = [(i, bo) for i in (1, 0)] + [(i, bo) for i in range(NT - 1, 1, -1)]
        st = {"d": d, "qt": qt, "kt": kt, "t_qt": t_qt, "t_kt": t_kt, "t_sc": t_sc, "EG": EG, "a_s": a_s, "c_s": c_s,
              "M": k.Mf if d == 0 else k.Mb, "tiles": tiles, "s_i": 0, "sb_i": 0, "am_i": 0, "pend": None, "nchunk": 0, "ds_i": 0}
        S.op("pool", lambda e, d=d: e.memset(sc["Sst"][0:dk, d, 0, 0:dvt], 0.0), w=[k.t_Sst[d][0]])
        S.op("pool", lambda e, d=d: e.memset(sc["Sbf"][0:dk, d, 0, 0:dvt], 0.0), w=[k.t_Sbf[d][0]])
        D_.append(st)

    def stage1(st, i):
        d = st["d"]
        tk0 = i * 128
        vt = v_fn(i)
        am_i = st["am_i"]
        st["am_i"] = 1 - am_i
        Am = sc["Am"][:, d, am_i, :]
        ktok = sc["ktok"][:, d, am_i, 0:dk]
        t_Am = k.t_Am2[d][am_i]
        t_kk = k.t_ktok2[d][am_i]
        qt, kt = st["qt"], st["kt"]
        pA, tA = k.bank()
        S.op("pe", lambda e: e.matmul(pA[:, 0:128], lhsT=kt[:, tk0:tk0 + 128], rhs=qt[:, tk0:tk0 + 128], start=True, stop=True),
             r=[st["t_kt"], st["t_qt"]], w=[tA])
        M_ = st["M"]
        S.op("dve", lambda e: e.tensor_tensor(out=Am, in0=pA[:, 0:128], in1=M_[:], op=ALU.mult), r=[tA, k.t_c], w=[t_Am])
        pK, tK = k.bank()
        S.op("pe", lambda e: e.matmul(pK[:, 0:dk], lhsT=kt[:, tk0:tk0 + 128], rhs=k.identB[0:dk, 0:dk], start=True, stop=True),
             r=[st["t_kt"], k.t_c], w=[tK])
        S.op("act", lambda e: e.activation(out=ktok, in_=pK[:, 0:dk], func=AF.Copy), r=[tK], w=[t_kk])
        pI, tI = k.bank()
        for a in range(nh):
            S.op("pe", lambda e, a=a: e.matmul(pI[0:dvh, a * 128:(a + 1) * 128], lhsT=vt[:, a * dvh:(a + 1) * dvh], rhs=Am[:, :], start=True, stop=True),
                 r=t_v + [t_Am], w=[tI])
        pS = [None] * cpt
        c_s = st["c_s"]
        for hf in range(cpt):
            pS_, tS_ = k.bank()
            rows = slice(hf * L, hf * L + L)
            S.op("pe", lambda e, pS_=pS_, rows=rows: e.matmul(pS_[0:dk, 0:dvt], lhsT=ktok[rows, :], rhs=vt[rows, 0:dvt], start=True, stop=True),
                 r=[t_kk] + t_v, w=[tS_])
            ds_i = st["ds_i"]
            st["ds_i"] = (ds_i + 1) % 4
            dSs = sc["dSs"][0:dk, d, ds_i, 0:dvt]
            c = cpt * i + hf
            S.op("act", lambda e, pS_=pS_, dSs=dSs, c=c: e.activation(out=dSs, in_=pS_[0:dk, 0:dvt], func=AF.Identity, scale=c_s[:, c:c + 1]),
                 r=[tS_, st["t_sc"]], w=[k.t_dSs[d][ds_i]])
            pS[hf] = (dSs, k.t_dSs[d][ds_i])
        for a in range(nh):
            S.op("dve", lambda e, a=a: e.tensor_tensor(out=o_acc[a][:, tk0:tk0 + 128], in0=pI[0:dvh, a * 128:(a + 1) * 128],
                                                       in1=o_acc[a][:, tk0:tk0 + 128], op=ALU.add), r=[tI, t_o[a]], w=[t_o[a]])
        st["pend"] = (i, pS)

    def stage2_chunk(st, i, hf, pS, po, tpo, last):
        d = st["d"]
        c = cpt * i + hf
        tok0 = c * L
        qt = st["qt"]
        sb_i = st["sb_i"]
        Sb = sc["Sbf"][0:dk, d, sb_i, 0:dvt]
        for a in range(nh):
            S.op("pe", lambda e, a=a: e.matmul(po[0:dvh, a * 128 + hf * L:a * 128 + hf * L + L], lhsT=Sb[:, a * dvh:(a + 1) * dvh],
                                               rhs=qt[:, tok0:tok0 + L], start=True, stop=True), r=[k.t_Sbf[d][sb_i], st["t_qt"]], w=[tpo])
        if last:
            return
        s_i = st["s_i"]
        Sc = sc["Sst"][0:dk, d, s_i, 0:dvt]
        Sn = sc["Sst"][0:dk, d, 1 - s_i, 0:dvt]
        EG, a_s = st["EG"], st["a_s"]
        dSs, t_dS = pS[hf]
        S.op("dve", lambda e: e.scalar_tensor_tensor(out=Sn, in0=Sc, scalar=EG[:, c:c + 1], in1=dSs, op0=ALU.mult, op1=ALU.add),
             r=[k.t_Sst[d][s_i], t_dS, st["t_sc"]], w=[k.t_Sst[d][1 - s_i]])
        st["s_i"] = 1 - s_i
        n_ = st["nchunk"] + 1
        ti, hfo = st["tiles"][n_ // cpt]
        cn = cpt * ti + hfo[n_ % cpt]
        Sbn = sc["Sbf"][0:dk, d, 1 - sb_i, 0:dvt]
        S.op("act", lambda e: e.activation(out=Sbn, in_=Sn, func=AF.Identity, scale=a_s[:, cn:cn + 1]),
             r=[k.t_Sst[d][1 - s_i], st["t_sc"]], w=[k.t_Sbf[d][1 - sb_i]])
        st["sb_i"] = 1 - sb_i

    nT = NT
    for step in range(nT + 1):
        if step < nT:
            for st in D_:
                stage1(st, st["tiles"][step][0])
        if step >= 1:
            pend = []
            for st in D_:
                i, hfo = st["tiles"][step - 1]
                po, tpo = k.bank()
                pend.append((st, i, hfo, po, tpo))
            for which in range(cpt):
                for (st, i, hfs, po, tpo) in pend:
                    pS = st["pS_prev"]
                    last = (st["nchunk"] == cpt * nT - 1)
                    stage2_chunk(st, i, hfs[which], pS, po, tpo, last)
                    st["nchunk"] += 1
            for (st, i, hfs, po, tpo) in pend:
                tk0 = i * 128
                for a in range(nh):
                    S.op("dve", lambda e, a=a, po=po, tk0=tk0: e.tensor_tensor(out=o_acc[a][:, tk0:tk0 + 128], in0=po[0:dvh, a * 128:(a + 1) * 128],
                                                                               in1=o_acc[a][:, tk0:tk0 + 128], op=ALU.add), r=[tpo, t_o[a]], w=[t_o[a]])
        for st in D_:
            if st["pend"] is not None:
                st["pS_prev"] = st["pend"][1]


def rms_gate_out(k, dvh, nh, o_acc, t_o, A, t_A, B, t_B, gsil, t_gs, gn_cols, t_gn, mixrow, t_mix, chunk_ids):
    S = k.S
    dv = dvh * nh
    for (t0, nt) in TOKB:
        pb, tb = k.bank()
        for a in range(nh):
            S.op("act", lambda e, a=a, t0=t0, nt=nt: e.activation(out=A[0:dvh, a * 512:a * 512 + nt], in_=o_acc[a][:, t0:t0 + nt], func=AF.Square),
                 r=[t_o[a]], w=[t_A])
        for a in range(nh):
            S.op("pe", lambda e, a=a, pb=pb, nt=nt: e.matmul(pb[0:dvh, 0:nt], lhsT=k.onesF[0:dvh, 0:dvh], rhs=A[0:dvh, a * 512:a * 512 + nt],
                                                             start=(a == 0), stop=(a == nh - 1)), r=[t_A, k.t_c], w=[tb])
        S.op("act", lambda e, pb=pb, nt=nt: e.activation(out=B[0:dvh, 0:nt], in_=pb[0:dvh, 0:nt], func=AF.Sqrt, scale=1.0 / dv, bias=k.cst[0:dvh, 0:1]),
             r=[tb, k.t_c], w=[t_B])
        S.op("dve", lambda e, nt=nt: e.reciprocal(out=B[0:dvh, 0:nt], in_=B[0:dvh, 0:nt]), r=[t_B], w=[t_B])
        for a in range(nh):
            S.op("dve", lambda e, a=a, t0=t0, nt=nt: e.tensor_tensor(out=B[0:dvh, 512 + a * 512:512 + a * 512 + nt], in0=o_acc[a][:, t0:t0 + nt],
                                                                     in1=B[0:dvh, 0:nt], op=ALU.mult), r=[t_o[a], t_B], w=[t_B])
            S.op("dve", lambda e, a=a, t0=t0, nt=nt: e.scalar_tensor_tensor(out=mixrow[a][0:dvh, t0:t0 + nt], in0=B[0:dvh, 512 + a * 512:512 + a * 512 + nt],
                                                                            scalar=gn_cols[a], in1=gsil[a][0:dvh, t0:t0 + nt], op0=ALU.mult, op1=ALU.mult),
                 r=[t_B, t_gn, t_gs[a]], w=[t_mix[a]])
    for a in range(nh):
        S.dma(k.mix_d[chunk_ids[a], 0:dvh, :], mixrow[a][0:dvh, :], r=[t_mix[a]], w=[k.t_mixd[chunk_ids[a]]])


def scan_setup(k):
    S = k.S
    if hasattr(k, "scn"):
        return
    sc = {}
    for nm in ("rr", "gg", "X1", "X2", "EG"):
        sc[nm] = S.sbuf("sc_" + nm, [128, 2, 36], F32)
    sc["Sst"] = S.sbuf("sc_Sst", [128, 2, 2, 192], F32)
    sc["dSs"] = S.sbuf("sc_dSs", [128, 2, 4, 192], BF16)
    sc["Sbf"] = S.sbuf("sc_Sbf", [128, 2, 2, 192], BF16)
    sc["Am"] = S.sbuf("sc_Am", [128, 2, 2, 128], BF16)
    sc["ktok"] = S.sbuf("sc_ktok", [128, 2, 2, 128], BF16)
    sc["col"] = S.sbuf("sc_col", [128, 64], F32)
    sc["row"] = k.RB[0:8, 5, 0:768]
    k.scn = sc
    k.t_scn = [Trk(), Trk()]
    k.t_Sst = [[Trk(), Trk()], [Trk(), Trk()]]
    k.t_dSs = [[Trk() for _ in range(4)], [Trk() for _ in range(4)]]
    k.t_Sbf = [[Trk(), Trk()], [Trk(), Trk()]]
    k.t_Am2 = [[Trk(), Trk()], [Trk(), Trk()]]
    k.t_ktok2 = [[Trk(), Trk()], [Trk(), Trk()]]
    k.t_Am = [k.t_Am2[0][0], k.t_Am2[0][1]]
    k.t_col = Trk()
    k.t_row = k.t_rb[5]


ATOK = [(0, 256), (256, 512), (768, 512), (1280, 512), (1792, 512)]


def mixer_even(k):
    S = k.S
    scan_setup(k)
    sc = k.scn
    RB, HB, t_rb, t_hb = k.RB, k.HB, k.t_rb, k.t_hb
    Ct = RB[:, 4, 0:2048]
    St = RB[:, 5, 0:2048]
    col = sc["col"]
    t_col = k.t_col
    ci = col[:, 0:8].bitcast(I32)
    S.op("pool", lambda e: e.iota(ci[:, 0:1], pattern=[[0, 1]], base=0, channel_multiplier=1), w=[t_col])
    S.op("dve", lambda e: e.tensor_single_scalar(out=ci[:, 1:2], in_=ci[:, 0:1], scalar=15, op=ALU.bitwise_and), r=[t_col], w=[t_col])
    S.op("dve", lambda e: e.tensor_scalar(out=ci[:, 2:3], in0=ci[:, 0:1], scalar1=5, scalar2=1, op0=ALU.logical_shift_right, op1=ALU.bitwise_and),
         r=[t_col], w=[t_col])
    S.op("dve", lambda e: e.tensor_scalar(out=ci[:, 3:4], in0=ci[:, 0:1], scalar1=4, scalar2=1, op0=ALU.logical_shift_right, op1=ALU.bitwise_and),
         r=[t_col], w=[t_col])
    S.op("dve", lambda e: e.tensor_copy(out=col[:, 8:11], in_=ci[:, 1:4]), r=[t_col], w=[t_col])
    S.op("act", lambda e: e.activation(out=col[:, 11:12], in_=col[:, 8:9], func=AF.Exp, scale=-math.log(10000.0) / 16.0), r=[t_col], w=[t_col])
    S.op("dve", lambda e: e.tensor_tensor(out=col[:, 13:14], in0=col[:, 11:12], in1=col[:, 9:10], op=ALU.mult), r=[t_col], w=[t_col])
    S.op("dve", lambda e: e.tensor_tensor(out=col[:, 12:13], in0=col[:, 11:12], in1=col[:, 13:14], op=ALU.subtract), r=[t_col], w=[t_col])
    S.op("dve", lambda e: e.tensor_scalar(out=col[:, 14:15], in0=col[:, 10:11], scalar1=2.0, scalar2=-1.0, op0=ALU.mult, op1=ALU.add),
         r=[t_col], w=[t_col])
    ri = RB[:, 0, 0:2048].bitcast(I32)
    qi = RB[:, 1, 0:2048].bitcast(I32)
    S.op("pool", lambda e: e.iota(ri, pattern=[[1, 32], [0, 64]], base=0, channel_multiplier=0), w=[t_rb[0]])
    S.op("pool", lambda e: e.iota(qi, pattern=[[0, 32], [1, 64]], base=0, channel_multiplier=0), w=[t_rb[1]])
    rf = RB[:, 2, 0:2048]
    qf = RB[:, 3, 0:2048]
    S.op("dve", lambda e: e.tensor_copy(out=rf, in_=ri), r=[t_rb[0]], w=[t_rb[2]])
    S.op("dve", lambda e: e.tensor_copy(out=qf, in_=qi), r=[t_rb[1]], w=[t_rb[3]])
    ang = RB[:, 0, 0:2048]
    S.op("dve", lambda e: e.tensor_scalar(out=ang, in0=rf, scalar1=col[:, 12:13], scalar2=None, op0=ALU.mult), r=[t_rb[2], t_col], w=[t_rb[0]])
    S.op("dve", lambda e: e.scalar_tensor_tensor(out=ang, in0=qf, scalar=col[:, 13:14], in1=ang, op0=ALU.mult, op1=ALU.add),
         r=[t_rb[3], t_rb[0], t_col], w=[t_rb[0]])
    def range_reduce(dst, t_dst, add, tmpi, t_tmpi, tmpf_, t_tmpf):
        S.op("dve", lambda e: e.tensor_scalar(out=tmpi, in0=ang, scalar1=add, scalar2=1.0 / (2 * PI), op0=ALU.add, op1=ALU.mult),
             r=[t_rb[0]], w=[t_tmpi])
        S.op("dve", lambda e: e.tensor_copy(out=tmpf_, in_=tmpi), r=[t_tmpi], w=[t_tmpf])
        S.op("dve", lambda e: e.scalar_tensor_tensor(out=dst, in0=tmpf_, scalar=-2 * PI, in1=ang, op0=ALU.mult, op1=ALU.add),
             r=[t_tmpf, t_rb[0]], w=[t_dst])
        if add != 0.0:
            S.op("dve", lambda e: e.tensor_scalar(out=dst, in0=dst, scalar1=add, scalar2=None, op0=ALU.add), r=[t_dst], w=[t_dst])
        S.op("dve", lambda e: e.tensor_scalar(out=tmpf_, in0=dst, scalar1=PI, scalar2=-2 * PI, op0=ALU.is_gt, op1=ALU.mult),
             r=[t_dst], w=[t_tmpf])
        S.op("dve", lambda e: e.tensor_tensor(out=dst, in0=dst, in1=tmpf_, op=ALU.add), r=[t_dst, t_tmpf], w=[t_dst])
        S.op("dve", lambda e: e.tensor_scalar(out=tmpf_, in0=dst, scalar1=-PI, scalar2=2 * PI, op0=ALU.is_lt, op1=ALU.mult),
             r=[t_dst], w=[t_tmpf])
        S.op("dve", lambda e: e.tensor_tensor(out=dst, in0=dst, in1=tmpf_, op=ALU.add), r=[t_dst, t_tmpf], w=[t_dst])
        S.op("dve", lambda e: e.tensor_scalar(out=dst, in0=dst, scalar1=PI, scalar2=-PI, op0=ALU.min, op1=ALU.max), r=[t_dst], w=[t_dst])
    m1 = RB[:, 1, 0:2048]
    tmpi = RB[:, 2, 0:2048].bitcast(I32)
    tmpf_ = RB[:, 3, 0:2048]
    range_reduce(m1, t_rb[1], 0.0, tmpi, t_rb[2], tmpf_, t_rb[3])
    S.op("act", lambda e: e.activation(out=St, in_=m1, func=AF.Sin, scale=col[:, 14:15]), r=[t_rb[1], t_col], w=[t_rb[5]])
    range_reduce(m1, t_rb[1], PI / 2, tmpi, t_rb[2], tmpf_, t_rb[3])
    S.op("act", lambda e: e.activation(out=Ct, in_=m1, func=AF.Sin), r=[t_rb[1]], w=[t_rb[4]])
    k.dump("ropeC", Ct, [128, 2048], F32, [t_rb[4]])
    k.dump("ropeS", St, [128, 2048], F32, [t_rb[5]])
    if k.sub == "M0a":
        return
    S.dma(col[:, 16:24], k.sink_d[0:1, :].to_broadcast([128, 8]), w=[t_col])
    S.op("act", lambda e: e.activation(out=col[:, 16:24], in_=col[:, 16:24], func=AF.Exp), r=[t_col], w=[t_col])

    qT = HB[:, 0:2, :]
    kAB = [HB[:, 2, :], HB[:, 3, :]]
    vdup = HB[:, 4, :].rearrange("p (i c) -> p i c", c=128)
    mixA = [HB[:, 5, :], HB[:, 6, :]]
    et = [HB[:, 7, ei * 512:(ei + 1) * 512] for ei in range(4)] + [RB[:, 3, 0:256].bitcast(BF16)]
    S.op("pool", lambda e: e.memset(kAB[0][64:128, :], 0.0), w=[t_hb[2]])
    S.op("pool", lambda e: e.memset(kAB[1][0:64, :], 0.0), w=[t_hb[3]])
    t_et = [Trk() for _ in range(5)]
    k.et_i = 0
    tmpf = [RB[:, 0, 0:512], RB[:, 0, 512:1024], RB[:, 1, 0:512], RB[:, 1, 512:1024]]
    t_tmp = [Trk() for _ in range(4)]
    dn = RB[:, 2, 0:512]
    t_dn = t_rb[2]
    wv_v, t_wv = k.load_w(k.w0_d[:, 1536:1664].rearrange("(k p) n -> p k n", p=128))
    for j in range(2):
        base = j * 768
        wq, t_wq = k.load_w(k.w0_d[:, base:base + 512].rearrange("(k p) n -> p k n", p=128))
        wk, t_wk = k.load_w(k.w0_d[:, base + 512:base + 768].rearrange("(k p) n -> p k n", p=128))
        tmp_i = 0
        for (t0, nt) in ATOK:
            for blk in range(3):
                if blk < 2:
                    pq, tq = proj_block(k, wq, t_wq, blk * 128, 128, t0, nt)
                    dst = qT[:, blk, t0:t0 + nt]
                    tdst = t_hb[blk]
                else:
                    pq, tq = proj_block(k, wk, t_wk, 0, 128, t0, nt)
                    dst = None
                if t0 == 0:
                    if blk < 2:
                        S.op("act", lambda e, pq=pq, dst=dst, nt=nt: e.activation(out=dst, in_=pq[:, 0:nt], func=AF.Copy), r=[tq], w=[tdst])
                    else:
                        S.op("act", lambda e, pq=pq, nt=nt, t0=t0: e.activation(out=kAB[0][0:64, t0:t0 + nt], in_=pq[0:64, 0:nt], func=AF.Copy), r=[tq], w=[t_hb[2]])
                        S.op("act", lambda e, pq=pq, nt=nt, t0=t0: e.activation(out=kAB[1][64:128, t0:t0 + nt], in_=pq[64:128, 0:nt], func=AF.Copy), r=[tq], w=[t_hb[3]])
                    continue
                if blk < 2:
                    ps_, ts_ = proj_block(k, wq, t_wq, 256 + blk * 128, 128, t0, nt)
                else:
                    ps_, ts_ = proj_block(k, wk, t_wk, 128, 128, t0, nt)
                l0 = t0 - 256
                ta, tb_ = tmp_i % 4, (tmp_i + 1) % 4
                tmp_i += 2
                S.op("dve", lambda e, pq=pq, ta=ta, l0=l0, nt=nt: e.tensor_tensor(out=tmpf[ta][:, 0:nt], in0=pq[:, 0:nt], in1=Ct[:, l0:l0 + nt], op=ALU.mult),
                     r=[tq, t_rb[4]], w=[t_tmp[ta]])
                S.op("dve", lambda e, ps_=ps_, tb_=tb_, l0=l0, nt=nt: e.tensor_tensor(out=tmpf[tb_][:, 0:nt], in0=ps_[:, 0:nt], in1=St[:, l0:l0 + nt], op=ALU.mult),
                     r=[ts_, t_rb[5]], w=[t_tmp[tb_]])
                if blk < 2:
                    S.op("pool", lambda e, dst=dst, ta=ta, tb_=tb_, nt=nt: e.tensor_tensor(out=dst, in0=tmpf[ta][:, 0:nt], in1=tmpf[tb_][:, 0:nt], op=ALU.add),
                         r=[t_tmp[ta], t_tmp[tb_]], w=[tdst])
                else:
                    S.op("pool", lambda e, ta=ta, tb_=tb_, nt=nt, t0=t0: e.tensor_tensor(out=kAB[0][0:64, t0:t0 + nt], in0=tmpf[ta][0:64, 0:nt], in1=tmpf[tb_][0:64, 0:nt], op=ALU.add),
                         r=[t_tmp[ta], t_tmp[tb_]], w=[t_hb[2]])
                    S.op("pool", lambda e, ta=ta, tb_=tb_, nt=nt, t0=t0: e.tensor_tensor(out=kAB[1][64:128, t0:t0 + nt], in0=tmpf[ta][64:128, 0:nt], in1=tmpf[tb_][64:128, 0:nt], op=ALU.add),
                         r=[t_tmp[ta], t_tmp[tb_]], w=[t_hb[3]])

        if k.sub == "M0p":
            k.dump("qT0", qT, [128, 2, N], BF16, [t_hb[0], t_hb[1]])
            return

        def ev_v(pb, tb, i, j=j):
            S.op("act", lambda e: e.activation(out=vdup[:, i, 0:64], in_=pb[:, j * 64:(j + 1) * 64], func=AF.Copy), r=[tb], w=[t_hb[4]])
            S.op("dve", lambda e: e.tensor_copy(out=vdup[:, i, 64:128], in_=pb[:, j * 64:(j + 1) * 64]), r=[tb], w=[t_hb[4]])
        k.proj_tm(wv_v, t_wv, 0, 128, ev_v)
        if j == 0:
            k.dump("qT0", qT, [128, 2, N], BF16, [t_hb[0], t_hb[1]])
            k.dump("kT0", HB[:, 2:4, :], [128, 2, N], BF16, [t_hb[2], t_hb[3]])
        if k.sub == "M0v":
            return
        for qb in range(NT):
            q0 = qb * 128
            if (k.sub == "M0q1" and qb == 1) or (k.sub == "M0q3" and qb == 3):
                k.dump("mixA", HB[:, 5:7, :], [128, 2, N], BF16, [t_hb[5], t_hb[6]])
                return
            if qb < 2:
                chunks = [(0, None), (1, None)]
            else:
                n_ = qb - 2
                chunks = [(0, None), (1, None)]
                if n_ > 0:
                    chunks.append((qb - 1, k.MP))
                chunks.append((qb, None))
                if n_ < 15:
                    chunks.append((qb + 1, k.MN))
            po, tpo = k.bank()
            pd, tpd = k.bank()
            nch = len(chunks)
            pss_l = []
            for ci_, (kc, msk) in enumerate(chunks):
                pss, tss = k.bank()
                for hh in range(4):
                    blk, half = hh // 2, hh % 2
                    S.op("pe", lambda e, pss=pss, hh=hh, blk=blk, half=half, kc=kc, q0=q0: e.matmul(
                        pss[:, hh * 128:(hh + 1) * 128], lhsT=kAB[half][:, kc * 128:(kc + 1) * 128], rhs=qT[:, blk, q0:q0 + 128],
                        start=True, stop=True), r=[t_hb[2 + half], t_hb[blk]], w=[tss])
                pss_l.append((pss, tss))
            for ci_, (kc, msk) in enumerate(chunks):
                pss, tss = pss_l[ci_]
                ei = ci_
                S.op("act", lambda e, pss=pss, ei=ei: e.activation(out=et[ei], in_=pss[:, :], func=AF.Exp, scale=0.125), r=[tss], w=[t_et[ei]])
                if msk is not None:
                    S.op("dve", lambda e, ei=ei, msk=msk: e.tensor_tensor(out=et[ei], in0=et[ei], in1=msk[:], op=ALU.mult),
                         r=[t_et[ei], k.t_c], w=[t_et[ei]])
            for ci_, (kc, msk) in enumerate(chunks):
                ei = ci_
                S.op("pe", lambda e, ei=ei, kc=kc, ci_=ci_, po=po, nch=nch: e.matmul(
                    po[:, :], lhsT=vdup[:, kc, :], rhs=et[ei][:, :], start=(ci_ == 0), stop=(ci_ == nch - 1)),
                    r=[t_hb[4], t_et[ei]], w=[tpo])
                S.op("pe", lambda e, ei=ei, ci_=ci_, pd=pd, nch=nch: e.matmul(
                    pd[:, :], lhsT=k.onesB[:], rhs=et[ei][:, :], start=(ci_ == 0), stop=(ci_ == nch - 1)),
                    r=[k.t_c, t_et[ei]], w=[tpd])
            if k.sub == "M0qb":
                S.op("dve", lambda e, po=po: e.tensor_copy(out=RB[:, 0, 0:512], in_=po[:, :]), r=[tpo], w=[t_rb[0]])
                S.op("dve", lambda e, pd=pd: e.tensor_copy(out=RB[:, 0, 512:1024], in_=pd[:, :]), r=[tpd], w=[t_rb[0]])
                k.dump("popd", RB[:, 0, 0:1024], [128, 1024], F32, [t_rb[0]])
                return
            for hh in range(4):
                S.op("dve", lambda e, hh=hh, pd=pd, j=j: e.tensor_scalar(out=dn[:, hh * 128:(hh + 1) * 128], in0=pd[:, hh * 128:(hh + 1) * 128],
                                                                    scalar1=col[:, 16 + 4 * j + hh:17 + 4 * j + hh], scalar2=None, op0=ALU.add),
                     r=[tpd, t_col], w=[t_dn])
            S.op("dve", lambda e: e.reciprocal(out=dn, in_=dn), r=[t_dn], w=[t_dn])
            for hh in range(4):
                blk, half = hh // 2, hh % 2
                rows = slice(half * 64, half * 64 + 64)
                S.op("dve", lambda e, hh=hh, blk=blk, rows=rows, po=po, q0=q0: e.tensor_tensor(
                    out=mixA[blk][rows, q0:q0 + 128], in0=po[rows, hh * 128:(hh + 1) * 128], in1=dn[rows, hh * 128:(hh + 1) * 128], op=ALU.mult),
                    r=[tpo, t_dn], w=[t_hb[5 + blk]])
        for blk in range(2):
            S.dma(k.mix_d[2 * j + blk, :, :], mixA[blk], r=[t_hb[5 + blk]], w=[k.t_mixd[2 * j + blk]])
    k.dump("mixd_att", k.mix_d[0:4, :, :], [4, 128, N], BF16, k.t_mixd[0:4])
    S.barrier()
    if k.sub == "M0b":
        return

    row = sc["row"]
    t_row = k.t_row
    S.dma(row[0:4, 0:512], k.lbl_d.rearrange("r a n -> (r a) n"), w=[t_row])
    for h in range(4):
        row_to_col(k, row[0:4, h * 128:(h + 1) * 128], 4, 128, col[:, 24 + 4 * h:28 + 4 * h], t_row, t_col)
    lbT = col[:, 24:40].rearrange("p (h r a) -> p h r a", r=2, a=2)
    lbv = col[:, 44:52].rearrange("p (h r) -> p h r", r=2)
    omv = col[:, 52:60].rearrange("p (h r) -> p h r", r=2)
    S.op("dve", lambda e: e.tensor_tensor(out=lbv, in0=lbT[:, :, :, 0], in1=lbT[:, :, :, 1], op=ALU.subtract), r=[t_col], w=[t_col])
    S.op("act", lambda e: e.activation(out=lbv, in_=lbv, func=AF.Sigmoid), r=[t_col], w=[t_col])
    S.op("dve", lambda e: e.tensor_scalar(out=omv, in0=lbv, scalar1=-1.0, scalar2=1.0, op0=ALU.mult, op1=ALU.add), r=[t_col], w=[t_col])
    S.dma(row[0:1, 0:512], k.hn_d[:, :], w=[t_row])
    for h in range(4):
        row_to_col(k, row[0:1, h * 128:(h + 1) * 128], 1, 128, col[:, 40 + h:41 + h], t_row, t_col)

    qrow, Krow, A, B, oacc = RB[:, 0, :], RB[:, 1, :], RB[:, 2, :], RB[:, 3, :], RB[:, 4, :]
    gsil, mixrow = HB[:, 2, :], HB[:, 4, :]
    qts, kts = [HB[:, 0, :], HB[:, 5, :]], [HB[:, 1, :], HB[:, 6, :]]
    vtm = HB[:, 3, :].rearrange("p (i c) -> p i c", c=128)
    for h in range(4):
        base = 1664 + 512 * h
        wg, t_wg = k.load_w(k.w0_d[:, base:base + 512].rearrange("(k p) n -> p k n", p=128))
        wvv, t_wvv = k.load_w(k.w0_d[:, 3712 + 128 * h:3712 + 128 * (h + 1)].rearrange("(k p) n -> p k n", p=128))

        def ev_q(pb, tb, t0, nt):
            S.op("act", lambda e: e.activation(out=qrow[:, t0:t0 + nt], in_=pb[:, 0:nt], func=AF.Copy), r=[tb], w=[t_rb[0]])
        k.proj_fm(wg, t_wg, 256, 128, ev_q)

        def ev_g(pb, tb, t0, nt):
            S.op("act", lambda e: e.activation(out=B[:, t0:t0 + nt], in_=pb[:, 0:nt], func=AF.Sigmoid), r=[tb], w=[t_rb[3]])
            S.op("dve", lambda e: e.tensor_tensor(out=gsil[:, t0:t0 + nt], in0=pb[:, 0:nt], in1=B[:, t0:t0 + nt], op=ALU.mult),
                 r=[tb, t_rb[3]], w=[t_hb[2]])
        k.proj_fm(wg, t_wg, 384, 128, ev_g)

        def ev_v2(pb, tb, i):
            S.op("act", lambda e: e.activation(out=vtm[:, i, :], in_=pb[:, 0:128], func=AF.Copy), r=[tb], w=[t_hb[3]])
        k.proj_tm(wvv, t_wvv, 0, 128, ev_v2)

        def make_logf(d, h=h, wg=wg, t_wg=t_wg):
            def ev_z(pb, tb, t0, nt):
                S.op("act", lambda e: e.activation(out=A[:, t0:t0 + nt], in_=pb[:, 0:nt], func=AF.Sigmoid), r=[tb], w=[t_rb[2]])
            k.proj_fm(wg, t_wg, d * 128, 128, ev_z)
            S.op("dve", lambda e: e.tensor_scalar(out=A, in0=A, scalar1=omv[:, h, d:d + 1], scalar2=lbv[:, h, d:d + 1], op0=ALU.mult, op1=ALU.add),
                 r=[t_rb[2], t_col], w=[t_rb[2]])
            S.op("pool", lambda e: e.tensor_scalar(out=Krow, in0=A, scalar1=-1.0, scalar2=1.0, op0=ALU.mult, op1=ALU.add),
                 r=[t_rb[2]], w=[t_rb[1]])
            S.op("act", lambda e: e.activation(out=A, in_=A, func=AF.Ln), r=[t_rb[2]], w=[t_rb[2]])
        gated_scan(k, 128, 128, 1, qrow, t_rb[0], Krow, t_rb[1], A, t_rb[2], B, t_rb[3], make_logf,
                   lambda i: vtm[:, i, :], [t_hb[3]], [oacc], [t_rb[4]], qts, [t_hb[0], t_hb[5]], kts, [t_hb[1], t_hb[6]])
        if h == 0:
            k.dump("oacc0", oacc, [128, N], F32, [t_rb[4]])
        rms_gate_out(k, 128, 1, [oacc], [t_rb[4]], A, t_rb[2], B, t_rb[3], [gsil], [t_hb[2]], [col[:, 40 + h:41 + h]], t_col,
                     [mixrow], [t_hb[4]], [4 + h])
    k.dump("mixd0", k.mix_d[0:8, :, :], [8, 128, N], BF16, k.t_mixd[0:8])


def phase_D(k, l):
    S = k.S
    RB, HB = k.RB, k.HB
    k.bcast_rows(l, "mix")
    if l == 0:
        wo, t_wo = k.load_w(k.wo0_d.rearrange("(c p) n -> p c n", p=128), nslots=2)
        chunks = [(c, 128, wo, t_wo, c) for c in range(8)]
        tiles = list(range(NT))
        nch = 8
    else:
        wo, t_wo = k.load_w(k.wo1_d[0:768, :].rearrange("(c p) n -> p c n", p=96), nslots=2, parts=96)
        wf, t_wf = k.load_w(k.wo1_d[768:1024, :].rearrange("(c p) n -> p c n", p=128), nslots=1)
        chunks = [(c, 96, wo, t_wo, c) for c in range(8)] + [(8 + c, 128, wf, t_wf, c) for c in range(2)]
        tiles = list(range(2, NT))
        nch = 10
    tb_ = [RB[:, r, hf * 1024:(hf + 1) * 1024] for r in range(6) for hf in range(2)]
    tt = k.t_tile
    nchunks = len(chunks)

    def bufs_for(n_):
        s6 = (n_ % 2) * 6
        return [tb_[s6 + q] for q in range(6)], [tt[s6 + q] for q in range(6)]

    def stA(n_):
        i = tiles[n_]
        (ht, tmp, xn1, hnew, xn2, ufb), (t_ht, t_tmp, t_xn1, t_hn, t_xn2, t_uf) = bufs_for(n_)
        mt = HB[:, n_ % 2, 0:nch * 128].rearrange("p (c n) -> p c n", n=128)
        t_mt = k.t_hb[n_ % 2]
        S.dma(mt, k.mix_d[0:nch, :, i * 128:(i + 1) * 128].rearrange("c p n -> p c n"), r=k.t_mixd[0:nch], w=[t_mt])
        if l == 0:
            src = k.ctx_d[i * 128:(i + 1) * 128, :] if i < 2 else k.x_d[(i - 2) * 128:(i - 1) * 128, :]
            S.dma(ht, src, w=[t_ht])
        else:
            S.dma(ht, k.hout0_d[i * 128:(i + 1) * 128, :], r=[k.t_hout0[i]], w=[t_ht])
        pp, tp = k.pair()
        for hf in range(2):
            for ci_, (c, KR, wv, t_wv, wc) in enumerate(chunks):
                S.op("pe", lambda e, hf=hf, c=c, KR=KR, wv=wv, wc=wc, ci_=ci_: e.matmul(
                    pp[:, hf * 512:(hf + 1) * 512], lhsT=mt[0:KR, c, :], rhs=wv[0:KR, wc, hf * 512:(hf + 1) * 512],
                    start=(ci_ == 0), stop=(ci_ == nchunks - 1)), r=[t_mt] + t_wv, w=[tp[hf]])
        r_ = 1 if i < 2 else 0
        for hf in range(2):
            S.op("dve", lambda e, hf=hf: e.tensor_tensor(out=tmp[:, hf * 512:(hf + 1) * 512], in0=pp[:, hf * 512:(hf + 1) * 512],
                                                         in1=k.bc[:, r_, hf * 512:(hf + 1) * 512], op=ALU.mult),
                 r=[tp[hf], k.t_bc[r_]], w=[t_tmp])
        S.op("dve", lambda e: e.scalar_tensor_tensor(out=tmp, in0=ht, scalar=ALPHA, in1=tmp, op0=ALU.mult, op1=ALU.add),
             r=[t_ht, t_tmp], w=[t_tmp])
        st_ap, mv_ap, rs_ap, nb_ap, t_st = k.st_slot()
        k.ln_stats(tmp, t_tmp, st_ap, mv_ap, rs_ap, nb_ap, t_st)
        S.op("act", lambda e: e.activation(out=xn1, in_=tmp, func=AF.Identity, scale=rs_ap, bias=nb_ap), r=[t_tmp, t_st], w=[t_xn1])
        S.op("pool", lambda e: e.tensor_tensor(out=xn1, in0=xn1, in1=k.bc[:, 2, :], op=ALU.mult), r=[t_xn1, k.t_bc[2]], w=[t_xn1])
        S.op("pool", lambda e: e.tensor_tensor(out=hnew, in0=xn1, in1=k.bc[:, 3, :], op=ALU.add), r=[t_xn1, k.t_bc[3]], w=[t_hn])
        k.ln_part1(hnew, t_hn, xn2, t_xn2)

    def stB(n_):
        i = tiles[n_]
        (ht, tmp, xn1, hnew, xn2, ufb), (t_ht, t_tmp, t_xn1, t_hn, t_xn2, t_uf) = bufs_for(n_)
        uf = ufb.rearrange("p (k n) -> p k n", n=128)
        S.dma(k.hmid_d[i * 128:(i + 1) * 128, :], hnew, r=[t_hn], w=[k.t_hmid[i]])
        k.ln_part2(l, xn2, t_xn2, i, 3, True, uf, t_uf)

    def stC(n_):
        i = tiles[n_]
        (ht, tmp, xn1, hnew, xn2, ufb), (t_ht, t_tmp, t_xn1, t_hn, t_xn2, t_uf) = bufs_for(n_)
        uf = ufb.rearrange("p (k n) -> p k n", n=128)
        k.route(i, uf, t_uf)
    nT_ = len(tiles)
    for n in range(nT_ + 2):
        if n < nT_:
            stA(n)
        if 1 <= n <= nT_:
            stB(n - 1)
        if n >= 2:
            stC(n - 2)
    k.route_all(tiles[0], len(tiles), RB[:, 0, :], [k.t_tile[0], k.t_tile[1]])
    k.dump(f"hmid{l}", k.hmid_d[:, :], [N, D], F32, k.t_hmid)
    k.dump(f"u2T{l}", k.uT[:, :, :], [128, KC, N], BF16, k.t_uT)
    k.dump(f"gates{l}", k.gates[:, :, :], [128, NT, 16], F32, k.t_gates)


def phase_E(k, l):
    S = k.S
    RB = k.RB
    k.bcast_rows(l, "moe")
    if l == 0:
        halves = [list(range(0, 9)), list(range(9, 18))]
        bsz = 384
    else:
        halves = [list(range(2, 10)), list(range(10, 18))]
        bsz = 512
    yacc = RB[:, 0:4, :].rearrange("p a n -> p (a n)").rearrange("p (t d) -> p t d", d=1024)
    t_y = k.t_tile[0:9]
    hTb = RB[:, 4, :].bitcast(BF16)
    hT = [hTb[:, q * 2048:(q + 1) * 2048].rearrange("p (f n) -> p f n", n=512) for q in range(2)]
    t_hT = [k.t_tile[9], k.t_tile[10]]
    sg = [RB[:, 5, q * 512:(q + 1) * 512] for q in range(2)]
    t_sg = [k.t_rb[4], k.t_rb[5]]
    Hb = RB[:, 5, 1024:2048]
    t_H = k.t_tile[11]
    dst_d = k.hout0_d if l == 0 else k.out_d
    k.h_i = 0
    k.s_i = 0
    for tiles in halves:
        tok0 = tiles[0] * 128
        ntok = len(tiles) * 128
        blocks = [(tok0 + b0, bsz) for b0 in range(0, ntok, bsz)]
        for e_ in range(16):
            w1, t_w1 = k.load_w(k.eg_d[l, e_].rearrange("(c p) n -> p c n", p=128))
            w3, t_w3 = k.load_w(k.eu_d[l, e_].rearrange("(c p) n -> p c n", p=128))
            w2, t_w2 = k.load_w(k.ed_d[l, e_].rearrange("(c p) n -> p c n", p=128))
            for (b0, nb) in blocks:
                hi = k.h_i
                k.h_i = 1 - hi
                hTc = hT[hi]
                for f in range(4):
                    p1, tp1 = k.bank()
                    for kk in range(KC):
                        S.op("pe", lambda e, kk=kk, f=f, p1=p1, w1=w1, b0=b0, nb=nb: e.matmul(
                            p1[:, 0:nb], lhsT=w1[:, kk, f * 128:(f + 1) * 128], rhs=k.uT[:, kk, b0:b0 + nb], start=(kk == 0), stop=(kk == KC - 1)),
                            r=t_w1 + k.t_uT[b0 // 128:(b0 + nb) // 128], w=[tp1])
                    p3, tp3 = k.bank()
                    for kk in range(KC):
                        S.op("pe", lambda e, kk=kk, f=f, p3=p3, w3=w3, b0=b0, nb=nb: e.matmul(
                            p3[:, 0:nb], lhsT=w3[:, kk, f * 128:(f + 1) * 128], rhs=k.uT[:, kk, b0:b0 + nb], start=(kk == 0), stop=(kk == KC - 1)),
                            r=t_w3 + k.t_uT[b0 // 128:(b0 + nb) // 128], w=[tp3])
                    si = k.s_i
                    k.s_i = 1 - si
                    S.op("act", lambda e, p1=p1, si=si, nb=nb: e.activation(out=sg[si][:, 0:nb], in_=p1[:, 0:nb], func=AF.Sigmoid), r=[tp1], w=[t_sg[si]])
                    S.op("dve", lambda e, p1=p1, si=si, nb=nb: e.tensor_tensor(out=sg[si][:, 0:nb], in0=p1[:, 0:nb], in1=sg[si][:, 0:nb], op=ALU.mult),
                         r=[tp1, t_sg[si]], w=[t_sg[si]])
                    S.op("dve", lambda e, p3=p3, si=si, nb=nb, f=f, hTc=hTc: e.tensor_tensor(out=hTc[:, f, 0:nb], in0=p3[:, 0:nb], in1=sg[si][:, 0:nb], op=ALU.mult),
                         r=[tp3, t_sg[si]], w=[t_hT[hi]])
                for tl in range(nb // 128):
                    gi = (b0 // 128) + tl
                    yi = gi - tiles[0]
                    for dh in range(2):
                        py, tpy = k.bank()
                        for f in range(4):
                            S.op("pe", lambda e, f=f, py=py, hTc=hTc, tl=tl, dh=dh, w2=w2: e.matmul(
                                py[:, :], lhsT=hTc[:, f, tl * 128:(tl + 1) * 128], rhs=w2[:, f, dh * 512:(dh + 1) * 512], start=(f == 0), stop=(f == 3)),
                                r=[t_hT[hi]] + t_w2, w=[tpy])
                        ya = yacc[:, yi, dh * 512:(dh + 1) * 512]
                        gs = k.gates[:, gi, e_:e_ + 1]
                        if e_ == 0:
                            S.op("dve", lambda e, py=py, ya=ya, gs=gs: e.tensor_scalar(out=ya, in0=py[:, :], scalar1=gs, scalar2=None, op0=ALU.mult),
                                 r=[tpy, k.t_gates[gi]], w=[t_y[yi]])
                        else:
                            S.op("dve", lambda e, py=py, ya=ya, gs=gs: e.scalar_tensor_tensor(out=ya, in0=py[:, :], scalar=gs, in1=ya, op0=ALU.mult, op1=ALU.add),
                                 r=[tpy, k.t_gates[gi], t_y[yi]], w=[t_y[yi]])
        Hbs = [RB[:, 5, 1024:2048], RB[:, 5, 0:1024]]
        t_Hs = [[k.t_tile[11]], [k.t_rb[4], k.t_rb[5]]]

        def fin_load(yi):
            gi = tiles[yi]
            S.dma(Hbs[yi % 2], k.hmid_d[gi * 128:(gi + 1) * 128, :], r=[k.t_hmid[gi]], w=t_Hs[yi % 2])

        def fin_store(yi):
            gi = tiles[yi]
            yt = yacc[:, yi, :]
            if l == 0:
                S.dma(k.hout0_d[gi * 128:(gi + 1) * 128, :], yt, r=[t_y[yi]], w=[k.t_hout0[gi]])
            else:
                S.dma(k.out_d[(gi - 2) * 128:(gi - 1) * 128, :], yt, r=[t_y[yi]], w=[k.t_out[gi - 2]])
        fin_load(0)
        for yi, gi in enumerate(tiles):
            yt = yacc[:, yi, :]
            Hb = Hbs[yi % 2]
            t_H = t_Hs[yi % 2]
            r_ = 1 if gi < 2 else 0
            if l == 0 and yi == 0 and tiles[0] == 0:
                k.dump("ymoe_t0", yt, [128, D], F32, [t_y[yi]])
            if yi + 1 < len(tiles):
                fin_load(yi + 1)
            S.op("dve", lambda e, yt=yt, r_=r_: e.tensor_tensor(out=yt, in0=yt, in1=k.bc[:, r_, :], op=ALU.mult), r=[t_y[yi], k.t_bc[r_]], w=[t_y[yi]])
            S.op("dve", lambda e, yt=yt, Hb=Hb: e.scalar_tensor_tensor(out=yt, in0=Hb, scalar=ALPHA, in1=yt, op0=ALU.mult, op1=ALU.add),
                 r=t_H + [t_y[yi]], w=[t_y[yi]])
            st_ap, mv_ap, rs_ap, nb_ap, t_st = k.st_slot()
            k.ln_stats(yt, t_y[yi], st_ap, mv_ap, rs_ap, nb_ap, t_st)
            S.op("act", lambda e, yt=yt, Hb=Hb, rs_ap=rs_ap, nb_ap=nb_ap: e.activation(out=Hb, in_=yt, func=AF.Identity, scale=rs_ap, bias=nb_ap),
                 r=[t_y[yi], t_st], w=t_H)
            S.op("pool", lambda e, Hb=Hb: e.tensor_tensor(out=Hb, in0=Hb, in1=k.bc[:, 2, :], op=ALU.mult), r=t_H + [k.t_bc[2]], w=t_H)
            S.op("pool", lambda e, yt=yt, Hb=Hb: e.tensor_tensor(out=yt, in0=Hb, in1=k.bc[:, 3, :], op=ALU.add), r=t_H + [k.t_bc[3]], w=[t_y[yi]])
            if yi >= 1:
                fin_store(yi - 1)
        fin_store(len(tiles) - 1)
    if l == 0:
        k.dump("hout0", k.hout0_d[:, :], [N, D], F32, k.t_hout0)


def mixer_odd(k):
    S = k.S
    scan_setup(k)
    sc = k.scn
    RB, HB, t_rb, t_hb = k.RB, k.HB, k.t_rb, k.t_hb
    col, t_col, row, t_row = sc["col"], k.t_col, sc["row"], k.t_row
    LATB = [(256, 512), (768, 512), (1280, 512), (1792, 512)]
    BC = sc["Am"][:, 0, 0, :]
    BS = sc["Am"][:, 0, 1, :]
    ci = col[:, 0:32].bitcast(I32)
    S.op("pool", lambda e: e.iota(ci[:, 0:1], pattern=[[0, 1]], base=0, channel_multiplier=1), w=[t_col])
    S.op("dve", lambda e: e.tensor_single_scalar(out=ci[:, 1:2], in_=ci[:, 0:1], scalar=63, op=ALU.bitwise_and), r=[t_col], w=[t_col])
    S.op("dve", lambda e: e.tensor_copy(out=col[:, 32:33], in_=ci[:, 1:2]), r=[t_col], w=[t_col])
    S.op("pool", lambda e: e.iota(ci[:, 2:18], pattern=[[128, 16]], base=0, channel_multiplier=1), r=[t_col], w=[t_col])
    S.op("dve", lambda e: e.tensor_copy(out=col[:, 40:56], in_=ci[:, 2:18]), r=[t_col], w=[t_col])
    qi = RB[:, 0, 0:128].bitcast(I32)
    qf = RB[:, 0, 128:256]
    ki = RB[:, 0, 256:384].bitcast(I32)
    kci = RB[:, 0, 384:512].bitcast(I32)
    tq = t_rb[0]
    S.op("pool", lambda e: e.iota(qi, pattern=[[1, 128]], base=0, channel_multiplier=0), w=[tq])
    S.op("dve", lambda e: e.tensor_single_scalar(out=qi, in_=qi, scalar=63, op=ALU.bitwise_and), r=[tq], w=[tq])
    S.op("dve", lambda e: e.tensor_copy(out=qf, in_=qi), r=[tq], w=[tq])
    S.op("dve", lambda e: e.tensor_scalar(out=ki, in0=qf, scalar1=col[:, 32:33], scalar2=None, op0=ALU.mult), r=[tq, t_col], w=[tq])
    S.op("dve", lambda e: e.tensor_single_scalar(out=ki, in_=ki, scalar=63, op=ALU.bitwise_and), r=[tq], w=[tq])
    S.op("dve", lambda e: e.tensor_scalar(out=kci, in0=ki, scalar1=16, scalar2=None, op0=ALU.add), r=[tq], w=[tq])
    S.op("dve", lambda e: e.tensor_single_scalar(out=kci, in_=kci, scalar=63, op=ALU.bitwise_and), r=[tq], w=[tq])
    S.op("act", lambda e: e.activation(out=BS, in_=ki, func=AF.Sin, scale=-2 * PI / 64, bias=k.cst[:, 4:5]), r=[tq, k.t_c], w=[k.t_Am[1]])
    S.op("act", lambda e: e.activation(out=BC, in_=kci, func=AF.Sin, scale=-2 * PI / 64, bias=k.cst[:, 4:5]), r=[tq, k.t_c], w=[k.t_Am[0]])
    for M_, tM in ((BC, k.t_Am[0]), (BS, k.t_Am[1])):
        S.op("pool", lambda e, M_=M_: e.memset(M_[0:64, 64:128], 0.0), r=[tM], w=[tM])
        S.op("pool", lambda e, M_=M_: e.memset(M_[64:128, 0:64], 0.0), r=[tM], w=[tM])
    if k.sub == "M1a":
        k.dump("BCS", sc["Am"][:, 0, :, :], [128, 2, 128], BF16, k.t_Am)
        return
    wz, t_wz = k.load_w(k.w1_d[:, 1536:1792].rearrange("(k p) n -> p k n", p=128))
    zT = HB[:, 2:4, :]
    zc = HB[:, 4:6, :].rearrange("p a n -> p (a n)")[:, 0:4096].rearrange("p (i c) -> p i c", c=256)
    zs = HB[:, 6:8, :].rearrange("p a n -> p (a n)")[:, 0:4096].rearrange("p (i c) -> p i c", c=256)
    for m in range(2):
        for (t0, nt) in LATB:
            pb, tb = proj_block(k, wz, t_wz, m * 128, 128, t0, nt)
            S.op("act", lambda e, pb=pb, m=m, t0=t0, nt=nt: e.activation(out=zT[:, m, t0:t0 + nt], in_=pb[:, 0:nt], func=AF.Copy), r=[tb], w=[t_hb[2 + m]])
    if k.sub == "M1z":
        k.dump("zT", HB[:, 2:4, :], [128, 2, N], BF16, [t_hb[2], t_hb[3]])
        return
    for a in range(16):
        tok = (a + 2) * 128
        pb, tb = k.bank()
        for m in range(2):
            S.op("pe", lambda e, pb=pb, m=m, tok=tok: e.matmul(pb[:, m * 128:(m + 1) * 128], lhsT=zT[:, m, tok:tok + 128], rhs=BC[:, :], start=True, stop=True),
                 r=[t_hb[2 + m], k.t_Am[0]], w=[tb])
            S.op("pe", lambda e, pb=pb, m=m, tok=tok: e.matmul(pb[:, 256 + m * 128:256 + (m + 1) * 128], lhsT=zT[:, m, tok:tok + 128], rhs=BS[:, :], start=True, stop=True),
                 r=[t_hb[2 + m], k.t_Am[1]], w=[tb])
        S.op("act", lambda e, pb=pb, a=a: e.activation(out=zc[:, a, :], in_=pb[:, 0:256], func=AF.Copy), r=[tb], w=[t_hb[4], t_hb[5]])
        if k.sub != "M1d":
            S.op("act", lambda e, pb=pb, a=a: e.activation(out=zs[:, a, :], in_=pb[:, 256:512], func=AF.Copy, scale=-1.0), r=[tb], w=[t_hb[6], t_hb[7]])
        if k.sub in ("M1c", "M1d") and a == 0:
            k.dump("zc", HB[:, 4:6, :], [128, 2, N], BF16, [t_hb[4], t_hb[5]])
            return
    S.barrier()
    if k.sub == "M1b":
        k.dump("zc", HB[:, 4:6, :], [128, 2, N], BF16, [t_hb[4], t_hb[5]])
        return
    fidx = RB[:, 0, 0:2048]
    fi_i = RB[:, 1, 0:2048].bitcast(I32)
    S.op("pool", lambda e: e.iota(fi_i, pattern=[[1, 2048]], base=0, channel_multiplier=0), w=[t_rb[1]])
    S.op("dve", lambda e: e.tensor_copy(out=fidx, in_=fi_i), r=[t_rb[1]], w=[t_rb[0]])
    tabs = []
    for q in range(2):
        rowb = RB[:, 2 + q, :].bitcast(BF16)
        tabs.append((rowb[:, 0:2048], rowb[:, 2048:4096], t_rb[2 + q]))
    kib = [RB[:, 4, 0:2048].bitcast(I32), RB[:, 5, 0:2048].bitcast(I32)]
    banks = [(k.PS[i // 2][:, (i % 2) * 512:(i % 2 + 1) * 512], k.PT[i // 2][i % 2]) for i in range(8)]
    for a in range(16):
        Cb, Sb, t_tab = tabs[a % 2]
        S.op("dve", lambda e, a=a: e.tensor_scalar(out=kib[0], in0=fidx, scalar1=col[:, 40 + a:41 + a], scalar2=None, op0=ALU.mult),
             r=[t_rb[0], t_col], w=[t_rb[4]])
        S.op("dve", lambda e: e.tensor_single_scalar(out=kib[0], in_=kib[0], scalar=2047, op=ALU.bitwise_and), r=[t_rb[4]], w=[t_rb[4]])
        S.op("dve", lambda e: e.tensor_scalar(out=kib[1], in0=kib[0], scalar1=512, scalar2=None, op0=ALU.add), r=[t_rb[4]], w=[t_rb[5]])
        S.op("dve", lambda e: e.tensor_single_scalar(out=kib[1], in_=kib[1], scalar=2047, op=ALU.bitwise_and), r=[t_rb[5]], w=[t_rb[5]])
        S.op("act", lambda e, Sb=Sb: e.activation(out=Sb, in_=kib[0], func=AF.Sin, scale=-2 * PI / 2048, bias=k.cst[:, 4:5]), r=[t_rb[4], k.t_c], w=[t_tab])
        S.op("act", lambda e, Cb=Cb: e.activation(out=Cb, in_=kib[1], func=AF.Sin, scale=-2 * PI / 2048, bias=k.cst[:, 4:5]), r=[t_rb[5], k.t_c], w=[t_tab])
        for m in range(2):
            for fb in range(4):
                pbk, tbk = banks[m * 4 + fb]
                S.op("pe", lambda e, pbk=pbk, a=a, m=m, fb=fb, Cb=Cb: e.matmul(pbk[:, :], lhsT=zc[:, a, m * 128:(m + 1) * 128], rhs=Cb[:, fb * 512:(fb + 1) * 512],
                                                                            start=(a == 0), stop=False), r=[t_hb[4], t_hb[5], t_tab], w=[tbk])
                S.op("pe", lambda e, pbk=pbk, a=a, m=m, fb=fb, Sb=Sb: e.matmul(pbk[:, :], lhsT=zs[:, a, m * 128:(m + 1) * 128], rhs=Sb[:, fb * 512:(fb + 1) * 512],
                                                                            start=False, stop=(a == 15)), r=[t_hb[6], t_hb[7], t_tab], w=[tbk])
    fsc = 1.0 / math.sqrt(2048.0 * 64.0)
    for m in range(2):
        for fb in range(4):
            pbk, tbk = banks[m * 4 + fb]
            S.op("act", lambda e, pbk=pbk, m=m, fb=fb: e.activation(out=HB[:, m, fb * 512:(fb + 1) * 512], in_=pbk[:, :], func=AF.Copy, scale=fsc), r=[tbk], w=[t_hb[m]])
        S.dma(k.mix_d[8 + m, :, 256:2304], HB[:, m, 0:2048], r=[t_hb[m]], w=[k.t_mixd[8 + m]])
    k.dump("mixd_f", k.mix_d[8:10, :, :], [2, 128, N], BF16, k.t_mixd[8:10])
    S.barrier()
    if k.sub == "M1f":
        return

    S.op("pool", lambda e: e.memset(k.rmask[:], 1.0), w=[k.t_c])
    S.op("pool", lambda e: e.memset(k.rmask[:].rearrange("p (c l) -> p c l", l=128)[:, :, 0:1], 0.0), r=[k.t_c], w=[k.t_c])
    S.op("pool", lambda e: e.memset(k.Mf[:], 1.0), w=[k.t_c])
    S.op("pool", lambda e: e.affine_select(out=k.Mf[:], in_=k.Mf[:], pattern=[[1, 128]], compare_op=ALU.is_ge, fill=0.0,
                                           base=0, channel_multiplier=-1), r=[k.t_c], w=[k.t_c])
    S.op("pool", lambda e: e.memset(k.Mb[:], 1.0), w=[k.t_c])
    S.op("pool", lambda e: e.affine_select(out=k.Mb[:], in_=k.Mb[:], pattern=[[-1, 128]], compare_op=ALU.is_ge, fill=0.0,
                                           base=0, channel_multiplier=1), r=[k.t_c], w=[k.t_c])
    gw = k.bc[0:16, 0, 0:768].rearrange("p (r n) -> p r n", n=384)
    t_gw = k.t_bc[0]
    S.dma(gw[:, :, :], k.gw_d.rearrange("r k n -> k r n"), w=[t_gw])
    S.dma(row[0:2, 0:384], k.gb_d[:, :], w=[t_row])
    for h in range(4):
        row_to_col(k, row[0:2, h * 96:(h + 1) * 96], 2, 96, col[0:96, 2 * h:2 * h + 2], t_row, t_col)
    S.op("dve", lambda e: e.tensor_scalar(out=col[0:96, 0:8], in0=col[0:96, 0:8], scalar1=-1.0, scalar2=None, op0=ALU.mult), r=[t_col], w=[t_col])
    S.dma(row[0:1, 0:768], k.gn_d[:, :], r=[t_col], w=[t_row])
    for c in range(8):
        row_to_col(k, row[0:1, c * 96:(c + 1) * 96], 1, 96, col[0:96, 8 + c:9 + c], t_row, t_col)
    qrow, krow, A, B = RB[0:96, 0, :], RB[0:96, 1, :], RB[0:96, 2, :], RB[0:96, 3, :]
    oacc = [RB[0:96, 4, :], RB[0:96, 5, :]]
    qt, kt = HB[0:96, 0, :], HB[0:96, 1, :]
    qts, kts = [HB[0:96, 0, :], HB[0:96, 6, :]], [HB[0:96, 1, :], HB[0:96, 7, :]]
    gsil = [HB[0:96, 2, :], HB[0:96, 3, :]]
    vt = HB[:, 4:6, :].rearrange("p a n -> p (a n)")[:, 0:3456].rearrange("p (i c) -> p i c", c=192)
    for h in range(4):
        base = 384 * h
        wg, t_wg = k.load_w(k.w1_d[:, base:base + 384].rearrange("(k p) n -> p k n", p=128))
        wv, t_wvv = k.load_w(k.w1_d[:, 1824 + 192 * h:1824 + 192 * (h + 1)].rearrange("(k p) n -> p k n", p=128))
        wR, t_wR = k.load_w(k.w1_d[:, 1792:1824].rearrange("(k p) n -> p k n", p=128))

        def ev_q(pb, tb, t0, nt):
            S.op("act", lambda e: e.activation(out=qrow[:, t0:t0 + nt], in_=pb[0:96, 0:nt], func=AF.Copy, scale=96.0 ** -0.5), r=[tb], w=[t_rb[0]])
        k.proj_fm(wg, t_wg, 0, 96, ev_q)

        def ev_k(pb, tb, t0, nt):
            S.op("act", lambda e: e.activation(out=krow[:, t0:t0 + nt], in_=pb[0:96, 0:nt], func=AF.Copy), r=[tb], w=[t_rb[1]])
        k.proj_fm(wg, t_wg, 96, 96, ev_k)
        for a in range(2):
            def ev_g(pb, tb, t0, nt, a=a):
                S.op("act", lambda e: e.activation(out=B[:, t0:t0 + nt], in_=pb[0:96, 0:nt], func=AF.Sigmoid), r=[tb], w=[t_rb[3]])
                S.op("dve", lambda e: e.tensor_tensor(out=gsil[a][:, t0:t0 + nt], in0=pb[0:96, 0:nt], in1=B[:, t0:t0 + nt], op=ALU.mult),
                     r=[tb, t_rb[3]], w=[t_hb[2 + a]])
            k.proj_fm(wg, t_wg, 192 + 96 * a, 96, ev_g)

        def ev_v(pb, tb, i):
            S.op("act", lambda e: e.activation(out=vt[:, i, :], in_=pb[:, 0:192], func=AF.Copy), r=[tb], w=[t_hb[4], t_hb[5]])
        k.proj_tm(wv, t_wvv, 0, 192, ev_v)

        def make_logf(d, h=h, wR=wR, t_wR=t_wR):
            for (t0, nt) in TOKB:
                pr, tr = proj_block(k, wR, t_wR, d * 16, 16, t0, nt)
                S.op("act", lambda e, pr=pr, t0=t0, nt=nt: e.activation(out=B[0:16, t0:t0 + nt], in_=pr[0:16, 0:nt], func=AF.Copy), r=[tr], w=[t_rb[3]])
                pz, tz = k.bank()
                S.op("pe", lambda e, pz=pz, t0=t0, nt=nt: e.matmul(pz[0:96, 0:nt], lhsT=gw[0:16, d, h * 96:(h + 1) * 96], rhs=B[0:16, t0:t0 + nt],
                                                                   start=True, stop=True), r=[t_gw, t_rb[3]], w=[tz])
                S.op("act", lambda e, pz=pz, t0=t0, nt=nt: e.activation(out=A[:, t0:t0 + nt], in_=pz[0:96, 0:nt], func=AF.Exp, scale=-1.0,
                                                                        bias=col[0:96, 2 * h + d:2 * h + d + 1]), r=[tz, t_col], w=[t_rb[2]])
            S.op("act", lambda e: e.activation(out=A, in_=A, func=AF.Ln, bias=k.cst[0:96, 1:2]), r=[t_rb[2], k.t_c], w=[t_rb[2]])
            S.op("dve", lambda e: e.tensor_scalar(out=A, in0=A, scalar1=-1.0 / 16.0, scalar2=None, op0=ALU.mult), r=[t_rb[2]], w=[t_rb[2]])
        gated_scan(k, 96, 96, 2, qrow, t_rb[0], krow, t_rb[1], A, t_rb[2], B, t_rb[3], make_logf,
                   lambda i: vt[:, i, :], [t_hb[4], t_hb[5]], oacc, [t_rb[4], t_rb[5]], qts, [t_hb[0], t_hb[6]], kts, [t_hb[1], t_hb[7]], L=128)
        if h == 0:
            k.dump("gla_o0", RB[0:96, 4:6, :], [96, 2, N], F32, [t_rb[4], t_rb[5]])
        rms_gate_out(k, 96, 2, oacc, [t_rb[4], t_rb[5]], A, t_rb[2], B, t_rb[3], gsil, [t_hb[2], t_hb[3]],
                     [col[0:96, 8 + 2 * h:9 + 2 * h], col[0:96, 9 + 2 * h:10 + 2 * h]], t_col, [qt, kt], [t_hb[0], t_hb[1]], [2 * h, 2 * h + 1])
    k.dump("mixd1", k.mix_d[0:10, :, :], [10, 128, N], BF16, k.t_mixd[0:10])


_NC_CACHE = {}


def _f32(a):
    return np.ascontiguousarray(np.asarray(a, dtype=np.float32))


def kernel(x, c, ctx, c_ctx, w_ada, b_ada, ln_g, ln_b, w_in_even, attn_sink, hgrn_lb_logits, hgrn_norm,
           w_out_even, w_in_odd, gla_gate_w, gla_gate_b, gla_norm, w_out_odd, w_router, b_router,
           w_expert_gate, w_expert_up, w_expert_down):
    x = _f32(x); c = _f32(c); ctx = _f32(ctx); c_ctx = _f32(c_ctx)
    w0a = np.ascontiguousarray(_f32(w_in_even)[0][:, _cols0()])
    w1a = np.ascontiguousarray(_f32(w_in_odd)[0][:, _cols1()])
    shared = {
        "w_ada": _f32(w_ada), "b_ada": _f32(b_ada), "ln_g": _f32(ln_g), "ln_b": _f32(ln_b),
        "w0a": w0a, "attn_sink": _f32(attn_sink), "lb_logits": _f32(hgrn_lb_logits), "hgrn_norm": _f32(hgrn_norm),
        "w_out_even": _f32(w_out_even)[0], "w1a": w1a, "gla_gate_w": _f32(gla_gate_w)[0], "gla_gate_b": _f32(gla_gate_b)[0],
        "gla_norm": _f32(gla_norm), "w_out_odd": _f32(w_out_odd)[0], "w_router": _f32(w_router),
        "b_router": _f32(b_router)[None, :], "w_expert_gate": _f32(w_expert_gate), "w_expert_up": _f32(w_expert_up),
        "w_expert_down": _f32(w_expert_down),
    }
    nb = x.shape[0]
    in_maps = []
    for b in range(nb):
        m = dict(shared)
        m["x"] = np.ascontiguousarray(x[b])
        m["ctx"] = np.ascontiguousarray(ctx[b])
        m["cvec"] = np.ascontiguousarray(np.stack([c[b], c_ctx], 0))
        in_maps.append(m)
    if "nc" not in _NC_CACHE:
        _NC_CACHE["nc"] = build()
    res = run_bass_kernel_spmd(_NC_CACHE["nc"], in_maps, core_ids=list(range(nb)))
    return np.stack([np.asarray(r["out"], dtype=np.float32) for r in res.results], 0)
```
